# Optimizing a Trainium2 kernel written in Bass

```python
import jax, jax.numpy as jnp
from jax import lax
import numpy as np

D_MODEL = 1024
BATCH = 2
SEQ = 8192
DEPTH = 2

N_MEM = 256
MLSTM_HEADS = 4
MLSTM_HEAD_DIM = D_MODEL // 16
MLSTM_WIDTH = MLSTM_HEADS * MLSTM_HEAD_DIM
MLSTM_CHUNK = 64
MLSTM_CONV = 4
CONF_CHANNELS = D_MODEL // 4
CONF_KERNEL = 31
FOX_HEADS = 8
FOX_HEAD_DIM = D_MODEL // 16
FOX_WIDTH = FOX_HEADS * FOX_HEAD_DIM
FOX_BLOCK = 128
MIX_WIDTH = MLSTM_WIDTH + CONF_CHANNELS + FOX_WIDTH
XATTN_HEADS = 4
XATTN_HEAD_DIM = D_MODEL // 8
XATTN_WIDTH = XATTN_HEADS * XATTN_HEAD_DIM
D_FF = ((8 * D_MODEL // 3) + 255) // 256 * 256
N_EXPERTS = 8
TOP_K = 2
EXPERT_FF = D_FF
N_DENSE_LAYERS = (DEPTH + 1) // 2
N_MOE_LAYERS = DEPTH // 2
EPS = 1e-6

IN_SPLITS = (2 * MLSTM_WIDTH, MLSTM_WIDTH, MLSTM_WIDTH, MLSTM_HEADS, MLSTM_HEADS,
             2 * CONF_CHANNELS, FOX_WIDTH, FOX_WIDTH, FOX_WIDTH, FOX_HEADS)
IN_WIDTH = sum(IN_SPLITS)
IN_SPLIT_IDX = tuple(int(i) for i in np.cumsum(IN_SPLITS)[:-1])

kernel_name = "hybrid_mlstm_conformer_fox_moe_block"


def rmsnorm(x, w):
    xf = x.astype(jnp.float32)
    y = xf * lax.rsqrt(jnp.mean(xf * xf, axis=-1, keepdims=True) + EPS)
    return (y * w.astype(jnp.float32)).astype(x.dtype)


def layernorm_f32(x, w, b):
    mu = jnp.mean(x, axis=-1, keepdims=True)
    xc = x - mu
    var = jnp.mean(xc * xc, axis=-1, keepdims=True)
    return xc * lax.rsqrt(var + EPS) * w.astype(jnp.float32) + b.astype(jnp.float32)


def causal_dwconv(x, w, b):
    K, C = w.shape
    y = lax.conv_general_dilated(
        x, w.astype(jnp.float32)[:, None, :], window_strides=(1,), padding=[(K - 1, 0)],
        dimension_numbers=("NWC", "WIO", "NWC"), feature_group_count=C)
    return y + b.astype(jnp.float32)


def mlstm_chunkwise(q, k, v, i_pre, f_pre):
    B, S, H, Dh = q.shape
    L = MLSTM_CHUNK
    nc = S // L
    log_f = jax.nn.log_sigmoid(f_pre)
    log_i = i_pre

    def chunks(t):
        return t.reshape(B, nc, L, H, Dh).transpose(1, 0, 3, 2, 4)

    def gchunks(t):
        return t.reshape(B, nc, L, H).transpose(1, 0, 3, 2)

    causal = jnp.tril(jnp.ones((L, L), dtype=bool))

    def step(carry, xs):
        C, n, m = carry
        qc, kc, vc, lic, lfc = xs
        b = jnp.cumsum(lfc, axis=-1)
        d_log = b[..., :, None] - b[..., None, :] + lic[..., None, :]
        d_log = jnp.where(causal, d_log, -jnp.inf)
        inter = b + m[..., None]
        m_t = jnp.maximum(jnp.max(d_log, axis=-1), inter)
        s = jnp.einsum("bhtd,bhsd->bhts", qc, kc) * jnp.exp(d_log - m_t[..., None])
        w_inter = jnp.exp(inter - m_t)
        num = (jnp.einsum("bhts,bhsd->bhtd", s, vc)
               + w_inter[..., None] * jnp.einsum("bhtk,bhkv->bhtv", qc, C))
        den = jnp.sum(s, axis=-1) + w_inter * jnp.einsum("bhtk,bhk->bht", qc, n)
        h = num / jnp.maximum(jnp.abs(den), jnp.exp(-m_t))[..., None]
        b_tot = b[..., -1]
        g = b_tot[..., None] - b + lic
        m_new = jnp.maximum(b_tot + m, jnp.max(g, axis=-1))
        wg = jnp.exp(g - m_new[..., None])
        decay = jnp.exp(b_tot + m - m_new)
        C_new = decay[..., None, None] * C + jnp.einsum("bhsk,bhsv->bhkv", kc * wg[..., None], vc)
        n_new = decay[..., None] * n + jnp.einsum("bhs,bhsk->bhk", wg, kc)
        return (C_new, n_new, m_new), h

    init = (jnp.zeros((B, H, Dh, Dh), jnp.float32), jnp.zeros((B, H, Dh), jnp.float32),
            jnp.zeros((B, H), jnp.float32))
    _, hs = lax.scan(step, init, (chunks(q), chunks(k), chunks(v), gchunks(log_i), gchunks(log_f)))
    return hs.transpose(1, 0, 3, 2, 4).reshape(B, S, H, Dh)


def forgetting_attention(q, k, v, log_f):
    B, S, H, Dh = q.shape
    nb = S // FOX_BLOCK
    c = jnp.cumsum(log_f, axis=1).transpose(0, 2, 1)
    kh = k.transpose(0, 2, 1, 3)
    vh = v.transpose(0, 2, 1, 3)
    qb = (q * (Dh ** -0.5)).reshape(B, nb, FOX_BLOCK, H, Dh).transpose(1, 0, 3, 2, 4)
    cb = c.reshape(B, H, nb, FOX_BLOCK).transpose(2, 0, 1, 3)
    qpos = jnp.arange(S).reshape(nb, FOX_BLOCK)
    kpos = jnp.arange(S)

    def one_block(args):
        qi, ci, pi = args
        logits = jnp.einsum("bhqd,bhkd->bhqk", qi, kh) + ci[..., None] - c[:, :, None, :]
        logits = jnp.where(pi[:, None] >= kpos[None, :], logits, -jnp.inf)
        p = jax.nn.softmax(logits, axis=-1)
        return jnp.einsum("bhqk,bhkd->bhqd", p, vh)

    o = lax.map(one_block, (qb, cb, qpos))
    return o.transpose(1, 0, 3, 2, 4).reshape(B, S, H * Dh)


def hybrid_mixer(h, w_in, mlstm_conv_w, mlstm_conv_b, mlstm_b_i, mlstm_b_f, mlstm_norm_w,
                 conf_conv_w, conf_conv_b, conf_ln_w, conf_ln_b, fox_b_f, w_out):
    B, S, _ = h.shape
    z = jnp.einsum("bsd,de->bse", h, w_in).astype(jnp.float32)
    m_qk, m_v, m_o, m_i, m_f, c_glu, f_q, f_k, f_v, f_f = jnp.split(z, IN_SPLIT_IDX, axis=-1)

    qk = jax.nn.silu(causal_dwconv(m_qk, mlstm_conv_w, mlstm_conv_b))
    mq, mk = jnp.split(qk, 2, axis=-1)
    hd = (B, S, MLSTM_HEADS, MLSTM_HEAD_DIM)
    hm = mlstm_chunkwise(mq.reshape(hd), mk.reshape(hd) * (MLSTM_HEAD_DIM ** -0.5), m_v.reshape(hd),
                         m_i + mlstm_b_i.astype(jnp.float32), m_f + mlstm_b_f.astype(jnp.float32))
    hm = hm * lax.rsqrt(jnp.mean(hm * hm, axis=-1, keepdims=True) + EPS)
    hm = hm.reshape(B, S, MLSTM_WIDTH) * mlstm_norm_w.astype(jnp.float32) * jax.nn.sigmoid(m_o)

    a, g = jnp.split(c_glu, 2, axis=-1)
    hc = causal_dwconv(a * jax.nn.sigmoid(g), conf_conv_w, conf_conv_b)
    hc = jax.nn.silu(layernorm_f32(hc, conf_ln_w, conf_ln_b))

    fd = (B, S, FOX_HEADS, FOX_HEAD_DIM)
    log_f = jax.nn.log_sigmoid(f_f + fox_b_f.astype(jnp.float32))
    hf = forgetting_attention(f_q.reshape(fd), f_k.reshape(fd), f_v.reshape(fd), log_f)

    cat = jnp.concatenate([hm, hc, hf], axis=-1).astype(h.dtype)
    return jnp.einsum("bse,ed->bsd", cat, w_out)


def memory_cross_attention(h, mem_n, w_q, w_kv, w_o):
    B, S, _ = h.shape
    q = jnp.einsum("bsd,de->bse", h, w_q).reshape(B, S, XATTN_HEADS, XATTN_HEAD_DIM)
    kv = jnp.einsum("bmd,de->bme", mem_n, w_kv)
    k, v = jnp.split(kv, 2, axis=-1)
    k = k.reshape(B, N_MEM, XATTN_HEADS, XATTN_HEAD_DIM)
    v = v.reshape(B, N_MEM, XATTN_HEADS, XATTN_HEAD_DIM)
    logits = jnp.einsum("bshd,bmhd->bhsm", q.astype(jnp.float32), k.astype(jnp.float32))
    p = jax.nn.softmax(logits * (XATTN_HEAD_DIM ** -0.5), axis=-1)
    o = jnp.einsum("bhsm,bmhd->bshd", p, v.astype(jnp.float32)).reshape(B, S, XATTN_WIDTH)
    return jnp.einsum("bse,ed->bsd", o.astype(h.dtype), w_o)


def swiglu(h, w_gate, w_up, w_down):
    a = jnp.einsum("bsd,df->bsf", h, w_gate)
    u = jnp.einsum("bsd,df->bsf", h, w_up)
    return jnp.einsum("bsf,fd->bsd", jax.nn.silu(a) * u, w_down)


def moe_swiglu(h, router_w, w_gate, w_up, w_down):
    logits = jnp.einsum("bsd,de->bse", h, router_w).astype(jnp.float32)
    top_v, top_i = lax.top_k(logits, TOP_K)
    top_w = jax.nn.softmax(top_v, axis=-1)
    gates = jnp.sum(jax.nn.one_hot(top_i, N_EXPERTS, dtype=jnp.float32) * top_w[..., None], axis=-2)
    y = jnp.zeros(h.shape, jnp.float32)
    for e in range(N_EXPERTS):
        y = y + gates[..., e:e + 1] * swiglu(h, w_gate[e], w_up[e], w_down[e]).astype(jnp.float32)
    return y.astype(h.dtype)


def setup_inputs(seed: int = 0) -> dict:
    key = jax.random.key(seed)
    ks = jax.random.split(key, 32)

    def nrm(k, shape, scale):
        return jax.random.normal(k, shape, jnp.float32) * scale

    D, L = D_MODEL, DEPTH
    return {
        "x": nrm(ks[0], (BATCH, SEQ, D), 1.0),
        "mem": nrm(ks[1], (BATCH, N_MEM, D), 1.0),
        "norm_mix_w": 1.0 + nrm(ks[2], (L, D), 0.02),
        "w_in": nrm(ks[3], (L, D, IN_WIDTH), D ** -0.5),
        "mlstm_conv_w": nrm(ks[4], (L, MLSTM_CONV, 2 * MLSTM_WIDTH), MLSTM_CONV ** -0.5),
        "mlstm_conv_b": nrm(ks[5], (L, 2 * MLSTM_WIDTH), 0.02),
        "mlstm_b_i": nrm(ks[6], (L, MLSTM_HEADS), 0.1),
        "mlstm_b_f": 3.0 + nrm(ks[7], (L, MLSTM_HEADS), 0.5),
        "mlstm_norm_w": 1.0 + nrm(ks[8], (L, MLSTM_WIDTH), 0.02),
        "conf_conv_w": nrm(ks[9], (L, CONF_KERNEL, CONF_CHANNELS), CONF_KERNEL ** -0.5),
        "conf_conv_b": nrm(ks[10], (L, CONF_CHANNELS), 0.02),
        "conf_ln_w": 1.0 + nrm(ks[11], (L, CONF_CHANNELS), 0.02),
        "conf_ln_b": nrm(ks[12], (L, CONF_CHANNELS), 0.02),
        "fox_b_f": 2.0 + nrm(ks[13], (L, FOX_HEADS), 0.5),
        "w_out": nrm(ks[14], (L, MIX_WIDTH, D), MIX_WIDTH ** -0.5),
        "norm_xattn_w": 1.0 + nrm(ks[15], (L, D), 0.02),
        "norm_mem_w": 1.0 + nrm(ks[16], (L, D), 0.02),
        "xattn_w_q": nrm(ks[17], (L, D, XATTN_WIDTH), D ** -0.5),
        "xattn_w_kv": nrm(ks[18], (L, D, 2 * XATTN_WIDTH), D ** -0.5),
        "xattn_w_o": nrm(ks[19], (L, XATTN_WIDTH, D), XATTN_WIDTH ** -0.5),
        "norm_ffn_w": 1.0 + nrm(ks[20], (L, D), 0.02),
        "ffn_w_gate": nrm(ks[21], (N_DENSE_LAYERS, D, D_FF), D ** -0.5),
        "ffn_w_up": nrm(ks[22], (N_DENSE_LAYERS, D, D_FF), D ** -0.5),
        "ffn_w_down": nrm(ks[23], (N_DENSE_LAYERS, D_FF, D), D_FF ** -0.5),
        "router_w": nrm(ks[24], (N_MOE_LAYERS, D, N_EXPERTS), D ** -0.5),
        "moe_w_gate": nrm(ks[25], (N_MOE_LAYERS, N_EXPERTS, D, EXPERT_FF), D ** -0.5),
        "moe_w_up": nrm(ks[26], (N_MOE_LAYERS, N_EXPERTS, D, EXPERT_FF), D ** -0.5),
        "moe_w_down": nrm(ks[27], (N_MOE_LAYERS, N_EXPERTS, EXPERT_FF, D), EXPERT_FF ** -0.5),
        "norm_final_w": 1.0 + nrm(ks[28], (D,), 0.02),
    }


def reference(x, mem, norm_mix_w, w_in, mlstm_conv_w, mlstm_conv_b, mlstm_b_i, mlstm_b_f,
              mlstm_norm_w, conf_conv_w, conf_conv_b, conf_ln_w, conf_ln_b, fox_b_f, w_out,
              norm_xattn_w, norm_mem_w, xattn_w_q, xattn_w_kv, xattn_w_o, norm_ffn_w,
              ffn_w_gate, ffn_w_up, ffn_w_down, router_w, moe_w_gate, moe_w_up, moe_w_down,
              norm_final_w):
    for l in range(DEPTH):
        h = rmsnorm(x, norm_mix_w[l])
        x = x + hybrid_mixer(h, w_in[l], mlstm_conv_w[l], mlstm_conv_b[l], mlstm_b_i[l], mlstm_b_f[l],
                             mlstm_norm_w[l], conf_conv_w[l], conf_conv_b[l], conf_ln_w[l],
                             conf_ln_b[l], fox_b_f[l], w_out[l])
        h = rmsnorm(x, norm_xattn_w[l])
        mem_n = rmsnorm(mem, norm_mem_w[l])
        x = x + memory_cross_attention(h, mem_n, xattn_w_q[l], xattn_w_kv[l], xattn_w_o[l])
        h = rmsnorm(x, norm_ffn_w[l])
        if l % 2 == 0:
            j = l // 2
            x = x + swiglu(h, ffn_w_gate[j], ffn_w_up[j], ffn_w_down[j])
        else:
            j = l // 2
            x = x + moe_swiglu(h, router_w[j], moe_w_gate[j], moe_w_up[j], moe_w_down[j])
    return rmsnorm(x, norm_final_w)
```

```python
import numpy as np
import concourse.bass as bass
import concourse.mybir as mybir
from concourse.bass_utils import run_bass_kernel_spmd

F32 = mybir.dt.float32
BF16 = mybir.dt.bfloat16
AF = mybir.ActivationFunctionType
ALU = mybir.AluOpType
AX = mybir.AxisListType

ENGS = ("pe", "act", "dve", "pool", "sp")


class KB:
    SEM_ROLL = 2000

    def __init__(self, nc, n_dma_sems=32):
        self.nc = nc
        self.q = {e: [] for e in ENGS}
        self.cnt = {e: 0 for e in ENGS}
        self.cur_sem = {}
        self.sem_pool = []
        self.waited = {e: {} for e in ENGS}
        self.last_w = {}
        self.reads = {}
        self.n_dma_sems = n_dma_sems
        self.dma_sems = []
        self.dma_cnt = []
        self.dma_rr = 0
        self.dma_rr_sw = 0
        self._stack = None
        self.n_inst = 0

    def _new_sem(self, name):
        s = self._stack.enter_context(self.nc.semaphore(name))
        return s

    def start(self, stack):
        self._stack = stack
        for e in ENGS:
            self.cur_sem[e] = self._new_sem(f"p_{e}_0")
        for i in range(self.n_dma_sems):
            self.dma_sems.append(self._new_sem(f"dma{i}"))
            self.dma_cnt.append(0)

    def _wait(self, eng, ev):
        if ev is None:
            return
        if len(ev) == 3 and ev[2] == "pe" and eng == "pe":
            return
        sem, val = ev[0], ev[1]
        w = self.waited[eng]
        if w.get(id(sem), (None, 0))[1] >= val:
            return
        w[id(sem)] = (sem, val)
        self.q[eng].append(lambda e, sem=sem, val=val: e.wait_ge(sem, val))

    def _wait_w(self, eng, k):
        lw = self.last_w.get(k)
        if isinstance(lw, list):
            for ev in lw:
                self._wait(eng, ev)
        else:
            self._wait(eng, lw)

    def _deps(self, eng, reads, writes):
        for k in reads:
            self._wait_w(eng, k)
        for k in writes:
            self._wait_w(eng, k)
            for ev in self.reads.get(k, ()):
                self._wait(eng, ev)

    def _commit(self, ev, reads, writes, is_dma=False):
        for k in writes:
            lw = self.last_w.get(k)
            if is_dma and isinstance(lw, list) and not self.reads.get(k):
                lw.append(ev)
            else:
                self.last_w[k] = [ev] if is_dma else ev
            self.reads[k] = []
        for k in reads:
            self.reads.setdefault(k, []).append(ev)

    def op(self, eng, fn, reads=(), writes=()):
        self._deps(eng, reads, writes)
        if self.cnt[eng] >= self.SEM_ROLL:
            self.cur_sem[eng] = self._new_sem(f"p_{eng}_{self.n_inst}")
            self.cnt[eng] = 0
        self.cnt[eng] += 1
        sem = self.cur_sem[eng]
        ev = (sem, self.cnt[eng], eng)
        self.q[eng].append(lambda e, sem=sem: fn(e).then_inc(sem, 1))
        self._commit(ev, reads, writes)
        self.n_inst += 1
        return ev

    def dma(self, eng, out, in_, reads=(), writes=(), **kw):
        self._deps(eng, reads, writes)
        half = self.n_dma_sems // 2
        if eng == "pool":
            i = half + self.dma_rr_sw
            self.dma_rr_sw = (self.dma_rr_sw + 1) % (self.n_dma_sems - half)
        else:
            i = self.dma_rr
            self.dma_rr = (self.dma_rr + 1) % half
        sem = self.dma_sems[i]
        if self.dma_cnt[i] >= 2048:
            self.dma_sems[i] = self._new_sem(f"dma{i}_{self.n_inst}")
            self.dma_cnt[i] = 0
            sem = self.dma_sems[i]
        if self.dma_cnt[i] > 0:
            self._wait(eng, (sem, self.dma_cnt[i]))
        self.dma_cnt[i] += 16
        ev = (sem, self.dma_cnt[i])
        self.q[eng].append(lambda e, sem=sem: e.dma_start(out=out, in_=in_, **kw).then_inc(sem, 16))
        self._commit(ev, reads, writes, is_dma=True)
        self.n_inst += 1
        return ev

    def collective(self, kind, rg, in_ap, out_ap, reads=(), writes=()):
        eng = "pool"
        self._deps(eng, reads, writes)
        sem = self._new_sem(f"cc_{self.n_inst}")
        ev = (sem, 1)
        self.q[eng].append(lambda e: e.collective_compute(kind, ALU.bypass, replica_groups=rg, ins=[in_ap.opt()],
                                                          outs=[out_ap.opt()]).then_inc(sem, 1))
        self._commit(ev, reads, writes)
        self.n_inst += 1
        self.cc_events = getattr(self, "cc_events", []) + [ev]
        return ev

    def wait_all(self, eng, evs):
        for ev in evs:
            self._wait(eng, ev)

    def flush(self):
        nc = self.nc
        q = self.q
        with nc.Block() as block:
            @block.tensor
            def _(e):
                for f in q["pe"]:
                    f(e)

            @block.scalar
            def _(e):
                for f in q["act"]:
                    f(e)

            @block.vector
            def _(e):
                for f in q["dve"]:
                    f(e)

            @block.gpsimd
            def _(e):
                for f in q["pool"]:
                    f(e)

            @block.sync
            def _(e):
                for f in q["sp"]:
                    f(e)
        self.q = {e: [] for e in ENGS}


D = 1024
NT = 2048
TT = 512
NTT = NT // TT
DFF = 2816
NF = DFF // 128
SEQ = 8192
EPS = 1e-6
NV_T = 100


class Ctx:
    def __init__(self, nc, st):
        self.nc = nc
        self.st = st
        self.kb = KB(nc)
        self.kb.start(st)
        self.ps_rr = 0
        self.uid = 0

    def sb(self, name, shape, dt, st=None):
        self.uid += 1
        return (st or self.st).enter_context(self.nc.sbuf_tensor(f"{name}_u{self.uid}", shape, dt))

    def barrier(self):
        kb = self.kb
        evs = []
        for e in ENGS:
            if kb.cnt[e] > 0:
                evs.append((kb.cur_sem[e], kb.cnt[e]))
        for i, s in enumerate(kb.dma_sems):
            if kb.dma_cnt[i] > 0:
                evs.append((s, kb.dma_cnt[i]))
        evs += getattr(kb, "cc_events", [])
        kb.cc_events = []
        for e in ENGS:
            for ev in evs:
                kb._wait(e, ev)
        kb.last_w = {}
        kb.reads = {}

    def setup(self):
        nc, kb = self.nc, self.kb
        self.ident_f = self.sb("ident_f", [128, 128], F32)
        self.ident_b = self.sb("ident_b", [128, 128], BF16)
        self.ones_b = self.sb("ones_b", [128, 128], BF16)
        self.ones_f = self.sb("ones_f", [128, 128], F32)
        self.psb = [self.st.enter_context(nc.psum_tensor(f"psb{i}", [128, 512], F32)) for i in range(8)]
        idf, idb, ob, of = self.ident_f, self.ident_b, self.ones_b, self.ones_f
        kb.op("pool", lambda e: e.memset(idf[:], 0.0), writes=["ident_f"])
        kb.op("pool", lambda e: e.affine_select(out=idf[:], in_=idf[:], pattern=[[-1, 128]],
                                                compare_op=ALU.not_equal, fill=1.0, base=0,
                                                channel_multiplier=1),
              reads=["ident_f"], writes=["ident_f"])
        kb.op("pool", lambda e: e.tensor_copy(out=idb[:], in_=idf[:]), reads=["ident_f"], writes=["ident_b"])
        kb.op("pool", lambda e: e.memset(ob[:], 1.0), writes=["ones_b"])
        kb.op("pool", lambda e: e.memset(of[:], 1.0), writes=["ones_f"])

    def ps(self):
        rot = getattr(self, "rot", None) or list(range(8))
        i = rot[self.ps_rr % len(rot)]
        self.ps_rr += 1
        return self.psb[i], f"psb{i}"


def h_store(c, dst, hT, c0, n, reads, writes=()):
    if isinstance(dst, list):
        for a, d in enumerate(dst):
            c.kb.dma("sp", d[:, c0:c0 + n].rearrange("(k p) n -> p k n", p=128), hT[:, 2 * a:2 * a + 2, 0:n], reads=reads, writes=writes)
    else:
        c.kb.dma("sp", dst[:, c0:c0 + n].rearrange("(k p) n -> p k n", p=128), hT[:, :, 0:n], reads=reads, writes=writes)


def h_load(c, src, hT, c0, n, writes, j=None):
    if isinstance(src, list):
        for a, d in enumerate(src):
            v = d if j is None else d.rearrange("(j r) n -> j r n", j=4)[j]
            c.kb.dma("sp", hT[:, 2 * a:2 * a + 2, 0:n], v[:, c0:c0 + n].rearrange("(k p) n -> p k n", p=128), writes=writes)
    else:
        v = src if j is None else src[j]
        c.kb.dma("sp", hT[:, :, 0:n], v[:, c0:c0 + n].rearrange("(k p) n -> p k n", p=128), writes=writes)


def mm(c, out, lhsT, rhs, start, stop, reads, writes):
    return c.kb.op("pe", lambda e: e.matmul(out, lhsT=lhsT, rhs=rhs, start=start, stop=stop),
                   reads=reads, writes=writes)


def rmsnorm_tile(c, xT, xkey, t0, n, wv, tmp, out_bf, okey, out_f=None):
    kb = c.kb
    sq, rstd = tmp["sq"], tmp["rstd"]
    for k in range(8):
        kb.op("act", lambda e, k=k: e.activation(out=sq[:, k, 0:n], in_=xT[:, k, t0:t0 + n], func=AF.Square),
              reads=[(xkey, k)], writes=[("sq", k)])
    p, pk = c.ps()
    for k in range(8):
        mm(c, p[:, 0:n], c.ones_b[:], sq[:, k, 0:n], k == 0, k == 7, ["ones_b", ("sq", k)], [pk])
    kb.op("act", lambda e: e.activation(out=rstd[:, 0:n], in_=p[:, 0:n], func=AF.Sqrt, scale=1.0 / D, bias=tmp["eps"][:, 0:1]),
          reads=[pk, "eps"], writes=["rstd"])
    kb.op("dve", lambda e: e.reciprocal(out=rstd[:, 0:n], in_=rstd[:, 0:n]), reads=["rstd"], writes=["rstd"])
    for k in range(8):
        kb.op("dve", lambda e, k=k: e.scalar_tensor_tensor(out=out_bf[:, k, 0:n], in0=xT[:, k, t0:t0 + n],
                                                           scalar=wv[:, k:k + 1], in1=rstd[:, 0:n],
                                                           op0=ALU.mult, op1=ALU.mult),
              reads=[(xkey, k), "rstd", "vecs"], writes=[(okey, k)])
        if out_f is not None:
            kb.op("dve", lambda e, k=k: e.scalar_tensor_tensor(out=out_f[:, k, 0:n], in0=xT[:, k, t0:t0 + n],
                                                                scalar=wv[:, k:k + 1], in1=rstd[:, 0:n],
                                                                op0=ALU.mult, op1=ALU.mult),
                  reads=[(xkey, k), "rstd", "vecs"], writes=[(okey + "_f", k)])


def phase_T(c, io, E, last, xT):
    nc, kb = c.nc, c.kb
    from contextlib import ExitStack
    vec_st = ExitStack()
    vecs = c.sb("vecsT", [128, NV_T], F32, vec_st)
    eps_t = c.sb("eps_t", [128, 1], F32, vec_st)
    sq = c.sb("sq", [128, 8, TT], BF16, vec_st)
    rstd = c.sb("rstd", [128, TT], F32, vec_st)
    hT = c.sb("hT", [128, 8, TT], BF16, vec_st)
    tmp = {"sq": sq, "rstd": rstd, "eps": eps_t}
    kb.dma("sp", vecs[:], io["vecs"], writes=["vecs"])
    kb.op("pool", lambda e: e.memset(eps_t[:], EPS), writes=["eps"])
    V_XA, V_MEM, V_FFN, V_NEXT, V_CB, V_LNW, V_LNB, V_CW = 0, 8, 16, 24, 32, 34, 36, 38

    with ExitStack() as s1:
        gluT = c.sb("gluT", [128, 2, 32 + NT], BF16, s1)
        hcT = c.sb("hcT", [128, 2, NT], BF16, s1)
        wc = c.sb("wc", [128, 8, 512], BF16, s1)
        dg = c.sb("dg", [128, 62, 128], BF16, s1)
        sig = c.sb("sig", [128, 2, TT], F32, s1)
        hcv = c.sb("hcv", [128, 2, TT], F32, s1)
        hsq = c.sb("hsq", [128, 2, TT], F32, s1)
        mean = c.sb("mean", [128, TT], F32, s1)
        var = c.sb("var", [128, TT], F32, s1)
        wo_m = c.sb("wo_m", [64, 4, D], BF16, s1)
        wo_c = c.sb("wo_c", [128, 2, D], BF16, s1)
        wo_f = c.sb("wo_f", [128, 4, D], BF16, s1)
        mT = c.sb("mT", [64, 4, TT], BF16, s1)
        fT = c.sb("fT", [128, 4, TT], BF16, s1)
        if "sel" in io:
            halo4 = c.sb("halo4", [128, 4, 8, 32], BF16, s1)
            selt = c.sb("selt", [128, 8], F32, s1)
            m4 = [c.sb(f"m4_{i}", [64, 4, TT], BF16, s1) for i in range(2)]
            f4 = [c.sb(f"f4_{i}", [128, 4, TT], BF16, s1) for i in range(2)]
            kb.dma("sp", selt[:], io["sel"], writes=["selt"])
        kb.dma("pool", wc[:], io["w_c"].rearrange("(k p) n -> p k n", p=128), writes=["wc"])
        kb.dma("pool", wo_m[:], io["w_out"][0:256, :].rearrange("(g p) n -> p g n", p=64), writes=["wo_m"])
        kb.dma("pool", wo_c[:], io["w_out"][256:512, :].rearrange("(g p) n -> p g n", p=128), writes=["wo_c"])
        kb.dma("pool", wo_f[:], io["w_out"][512:1024, :].rearrange("(g p) n -> p g n", p=128), writes=["wo_f"])
        for j in range(31):
            for ch in range(2):
                kb.op("dve", lambda e, j=j, ch=ch: e.tensor_scalar(
                    out=dg[:, j * 2 + ch, :], in0=c.ident_b[:], scalar1=vecs[:, V_CW + j * 2 + ch:V_CW + j * 2 + ch + 1],
                    scalar2=None, op0=ALU.mult), reads=["ident_b", "vecs"], writes=[("dg", j, ch)])
        tiles = [("halo", 0, 32)] + [("own", t * TT, TT) for t in range(NTT)]
        for kind, t0, n in tiles:
            if kind == "halo" and "sel" in io:
                tl = io["tails"].rearrange("(j k p) n -> j p k n", j=4, p=128)
                for jj in range(4):
                    kb.dma("sp", halo4[:, jj, :, :], tl[jj], writes=[("halo4", jj)])
                kb.op("dve", lambda e: e.tensor_scalar(out=hT[:, :, 0:32], in0=halo4[:, 0, :, :], scalar1=selt[:, 4:5], scalar2=None, op0=ALU.mult),
                      reads=[("halo4", 0), "selt"], writes=[("hT", k) for k in range(8)])
                for jj in range(1, 4):
                    kb.op("dve", lambda e, jj=jj: e.scalar_tensor_tensor(out=hT[:, :, 0:32], in0=halo4[:, jj, :, :], scalar=selt[:, 4 + jj:5 + jj], in1=hT[:, :, 0:32],
                                                                         op0=ALU.mult, op1=ALU.add),
                          reads=[("halo4", jj), "selt"] + [("hT", k) for k in range(8)], writes=[("hT", k) for k in range(8)])
                g0 = 0
            elif kind == "halo":
                kb.dma("sp", hT[:, :, 0:n], io["h_halo"].rearrange("(k p) n -> p k n", p=128),
                       writes=[("hT", k) for k in range(8)])
                g0 = 0
            else:
                h_load(c, io["h_own"], hT, t0, n, [("hT", k) for k in range(8)])
                g0 = 32 + t0
            for ch in range(2):
                pa, pak = c.ps()
                pg, pgk = c.ps()
                for k in range(8):
                    mm(c, pa[:, 0:n], wc[:, k, ch * 128:(ch + 1) * 128], hT[:, k, 0:n], k == 0, k == 7,
                       ["wc", ("hT", k)], [pak])
                for k in range(8):
                    mm(c, pg[:, 0:n], wc[:, k, 256 + ch * 128:256 + (ch + 1) * 128], hT[:, k, 0:n], k == 0, k == 7,
                       ["wc", ("hT", k)], [pgk])
                kb.op("act", lambda e, ch=ch, pg=pg, n=n: e.activation(out=sig[:, ch, 0:n], in_=pg[:, 0:n], func=AF.Sigmoid),
                      reads=[pgk], writes=[("sig", ch)])
                kb.op("dve", lambda e, ch=ch, pa=pa, n=n, g0=g0: e.tensor_tensor(
                    out=gluT[:, ch, g0:g0 + n], in0=pa[:, 0:n], in1=sig[:, ch, 0:n], op=ALU.mult),
                    reads=[pak, ("sig", ch)], writes=[("glu", ch, g0 // TT), ("glu", ch, (g0 + n - 1) // TT)])
        for t in range(NTT):
            t0 = t * TT
            gk = lambda ch: [("glu", ch, (32 + t0 - 30) // TT), ("glu", ch, (32 + t0 + TT - 1) // TT)]
            for ch in range(2):
                p, pk = c.ps()
                for j in range(31):
                    o = 32 + t0 - 30 + j
                    mm(c, p[:, :], dg[:, j * 2 + ch, :], gluT[:, ch, o:o + TT], j == 0, j == 30,
                       [("dg", j, ch)] + gk(ch), [pk])
                kb.op("act", lambda e, ch=ch, p=p: e.activation(out=hcv[:, ch, :], in_=p[:, :], func=AF.Identity,
                                                                bias=vecs[:, V_CB + ch:V_CB + ch + 1]),
                      reads=[pk, "vecs"], writes=[("hcv", ch)])
                kb.op("act", lambda e, ch=ch: e.activation(out=hsq[:, ch, :], in_=hcv[:, ch, :], func=AF.Square),
                      reads=[("hcv", ch)], writes=[("hsq", ch)])
            p1, p1k = c.ps()
            p2, p2k = c.ps()
            for ch in range(2):
                mm(c, p1[:, :], c.ones_f[:], hcv[:, ch, :], ch == 0, ch == 1, ["ones_f", ("hcv", ch)], [p1k])
            for ch in range(2):
                mm(c, p2[:, :], c.ones_f[:], hsq[:, ch, :], ch == 0, ch == 1, ["ones_f", ("hsq", ch)], [p2k])
            kb.op("dve", lambda e, p1=p1: e.tensor_scalar(out=mean[:], in0=p1[:, :], scalar1=1.0 / 256, scalar2=None, op0=ALU.mult),
                  reads=[p1k], writes=["mean"])
            kb.op("dve", lambda e: e.tensor_tensor(out=var[:], in0=mean[:], in1=mean[:], op=ALU.mult),
                  reads=["mean"], writes=["var"])
            kb.op("dve", lambda e, p2=p2: e.scalar_tensor_tensor(out=var[:], in0=p2[:, :], scalar=1.0 / 256, in1=var[:],
                                                                 op0=ALU.mult, op1=ALU.subtract),
                  reads=[p2k, "var"], writes=["var"])
            kb.op("act", lambda e: e.activation(out=var[:], in_=var[:], func=AF.Sqrt, bias=eps_t[:, 0:1]),
                  reads=["var", "eps"], writes=["var"])
            kb.op("dve", lambda e: e.reciprocal(out=var[:], in_=var[:]), reads=["var"], writes=["var"])
            for ch in range(2):
                kb.op("dve", lambda e, ch=ch: e.tensor_tensor(out=hcv[:, ch, :], in0=hcv[:, ch, :], in1=mean[:], op=ALU.subtract),
                      reads=[("hcv", ch), "mean"], writes=[("hcv", ch)])
                kb.op("dve", lambda e, ch=ch: e.tensor_tensor(out=hcv[:, ch, :], in0=hcv[:, ch, :], in1=var[:], op=ALU.mult),
                      reads=[("hcv", ch), "var"], writes=[("hcv", ch)])
                kb.op("dve", lambda e, ch=ch: e.tensor_scalar(out=hcv[:, ch, :], in0=hcv[:, ch, :],
                                                              scalar1=vecs[:, V_LNW + ch:V_LNW + ch + 1],
                                                              scalar2=vecs[:, V_LNB + ch:V_LNB + ch + 1],
                                                              op0=ALU.mult, op1=ALU.add),
                      reads=[("hcv", ch), "vecs"], writes=[("hcv", ch)])
                kb.op("act", lambda e, ch=ch, t0=t0: e.activation(out=hcT[:, ch, t0:t0 + TT], in_=hcv[:, ch, :], func=AF.Silu),
                      reads=[("hcv", ch)], writes=[("hcT", ch, t)])
            if "sel" in io:
                cm = io["catm_all"].rearrange("(g p) n -> p g n", p=64)
                cf = [a.rearrange("(g p) n -> p g n", p=64) for a in io["catf_all"]]
                for jj in range(4):
                    for dst, stg, src, nm, npart in ((mT, m4, cm, "m4", 64), (fT, f4, cf, "f4", 128)):
                        dk = "mT" if nm == "m4" else "fT"
                        sg = stg[jj % 2]
                        sk = f"{nm}_{jj % 2}"
                        if nm == "m4":
                            kb.dma("sp", sg[:], src[:, :, jj * NT + t0:jj * NT + t0 + TT], writes=[sk])
                        else:
                            for hh in range(2):
                                kb.dma("sp", sg[hh * 64:(hh + 1) * 64, :, :], src[hh][:, :, jj * NT + t0:jj * NT + t0 + TT], writes=[sk])
                        if jj == 0:
                            kb.op("dve", lambda e, dst=dst, sg=sg, npart=npart: e.tensor_scalar(out=dst[:], in0=sg[:], scalar1=selt[0:npart, 0:1], scalar2=None, op0=ALU.mult),
                                  reads=[sk, "selt"], writes=[dk])
                        else:
                            kb.op("dve", lambda e, dst=dst, sg=sg, jj=jj, npart=npart: e.scalar_tensor_tensor(out=dst[:], in0=sg[:], scalar=selt[0:npart, jj:jj + 1], in1=dst[:],
                                                                                                          op0=ALU.mult, op1=ALU.add),
                                  reads=[sk, "selt", dk], writes=[dk])
            else:
                kb.dma("sp", mT[:], io["catm"][:, :, t0:t0 + TT].rearrange("g p n -> p g n"), writes=["mT"])
                kb.dma("sp", fT[:], io["catf"][:, :, t0:t0 + TT].rearrange("g p n -> p g n"), writes=["fT"])
            for d in range(8):
                p, pk = c.ps()
                ds = slice(d * 128, (d + 1) * 128)
                for g in range(4):
                    mm(c, p[:, :], wo_m[:, g, ds], mT[:, g, :], g == 0, False, ["wo_m", "mT"], [pk])
                for ch in range(2):
                    mm(c, p[:, :], wo_c[:, ch, ds], hcT[:, ch, t0:t0 + TT], False, False, ["wo_c", ("hcT", ch, t)], [pk])
                for g in range(4):
                    mm(c, p[:, :], wo_f[:, g, ds], fT[:, g, :], False, g == 3, ["wo_f", "fT"], [pk])
                kb.op("dve", lambda e, d=d, p=p, t0=t0: e.tensor_tensor(out=xT[:, d, t0:t0 + TT], in0=xT[:, d, t0:t0 + TT],
                                                                        in1=p[:, :], op=ALU.add),
                      reads=[pk, ("xT", d)], writes=[("xT", d)])
    c.barrier()
    if io.get("dbg_stage") == 1:
        vec_st.close()
        return

    with ExitStack() as s2:
        memt = c.sb("memt", [128, 2, D], F32, s2)
        mss = c.sb("mss", [128, 2], F32, s2)
        junk = c.sb("junk", [128, D], F32, s2)
        memnT = c.sb("memnT", [128, 8, 256], BF16, s2)
        wkv = c.sb("wkv", [128, 8, D], BF16, s2)
        wq = c.sb("wq", [128, 8, 512], BF16, s2)
        wo = c.sb("wo", [128, 4, D], BF16, s2)
        kT = c.sb("kT", [128, 4, 256], BF16, s2)
        Vt = c.sb("Vt", [128, 2, 512], BF16, s2)
        qT = c.sb("qT", [128, 4, TT], BF16, s2)
        pT = c.sb("pT", [128, 8, TT], BF16, s2)
        rden = c.sb("rden", [128, TT], F32, s2)
        oT = c.sb("oT", [128, 4, TT], BF16, s2)
        kb.dma("sp", memt[:], io["mem"].rearrange("(t p) d -> p t d", p=128), writes=["memt"])
        kb.dma("pool", wkv[:], io["w_kv"].rearrange("(k p) n -> p k n", p=128), writes=["wkv"])
        kb.dma("pool", wq[:], io["w_q"].rearrange("(k p) n -> p k n", p=128), writes=["wq"])
        kb.dma("pool", wo[:], io["w_o"].rearrange("(k p) n -> p k n", p=128), writes=["wo"])
        for mt in range(2):
            kb.op("act", lambda e, mt=mt: e.activation(out=junk[:], in_=memt[:, mt, :], func=AF.Square,
                                                       accum_out=mss[:, mt:mt + 1]),
                  reads=["memt"], writes=["junk", ("mss", mt)])
            kb.op("act", lambda e, mt=mt: e.activation(out=mss[:, mt:mt + 1], in_=mss[:, mt:mt + 1], func=AF.Sqrt,
                                                       scale=1.0 / D, bias=eps_t[:, 0:1]),
                  reads=[("mss", mt), "eps"], writes=[("mss", mt)])
            kb.op("dve", lambda e, mt=mt: e.reciprocal(out=mss[:, mt:mt + 1], in_=mss[:, mt:mt + 1]),
                  reads=[("mss", mt)], writes=[("mss", mt)])
            kb.op("dve", lambda e, mt=mt: e.tensor_scalar(out=memt[:, mt, :], in0=memt[:, mt, :], scalar1=mss[:, mt:mt + 1],
                                                          scalar2=None, op0=ALU.mult),
                  reads=["memt", ("mss", mt)], writes=["memt"])
        for k in range(8):
            p, pk = c.ps()
            for mt in range(2):
                kb.op("pe", lambda e, k=k, mt=mt, p=p: e.transpose(out=p[:, mt * 128:(mt + 1) * 128],
                                                                   in_=memt[:, mt, k * 128:(k + 1) * 128], identity=c.ident_f[:]),
                      reads=["memt", "ident_f"], writes=[pk])
            kb.op("dve", lambda e, k=k, p=p: e.tensor_scalar(out=memnT[:, k, :], in0=p[:, 0:256],
                                                             scalar1=vecs[:, V_MEM + k:V_MEM + k + 1], scalar2=None, op0=ALU.mult),
                  reads=[pk, "vecs"], writes=[("memnT", k)])
        for h in range(4):
            p, pk = c.ps()
            for k in range(8):
                mm(c, p[:, 0:256], wkv[:, k, h * 128:(h + 1) * 128], memnT[:, k, :], k == 0, k == 7, ["wkv", ("memnT", k)], [pk])
            kb.op("act", lambda e, h=h, p=p: e.activation(out=kT[:, h, :], in_=p[:, 0:256], func=AF.Copy),
                  reads=[pk], writes=[("kT", h)])
        for mt in range(2):
            p, pk = c.ps()
            for k in range(8):
                mm(c, p[:, :], memnT[:, k, mt * 128:(mt + 1) * 128], wkv[:, k, 512:1024], k == 0, k == 7, ["wkv", ("memnT", k)], [pk])
            kb.op("act", lambda e, mt=mt, p=p: e.activation(out=Vt[:, mt, :], in_=p[:, :], func=AF.Copy),
                  reads=[pk], writes=[("Vt", mt)])
        sc = 128 ** -0.5
        for t in range(NTT):
            t0 = t * TT
            rmsnorm_tile(c, xT, "xT", t0, TT, vecs[:, V_XA:V_XA + 8], tmp, hT, "hT")
            for h in range(4):
                p, pk = c.ps()
                for k in range(8):
                    mm(c, p[:, :], wq[:, k, h * 128:(h + 1) * 128], hT[:, k, :], k == 0, k == 7, ["wq", ("hT", k)], [pk])
                kb.op("act", lambda e, h=h, p=p: e.activation(out=qT[:, h, :], in_=p[:, :], func=AF.Copy),
                      reads=[pk], writes=[("qT", h)])
            for h in range(4):
                for mt in range(2):
                    p, pk = c.ps()
                    mm(c, p[:, :], kT[:, h, mt * 128:(mt + 1) * 128], qT[:, h, :], True, True, [("kT", h), ("qT", h)], [pk])
                    kb.op("act", lambda e, h=h, mt=mt, p=p: e.activation(out=pT[:, h * 2 + mt, :], in_=p[:, :], func=AF.Exp, scale=sc),
                          reads=[pk], writes=[("pT", h, mt)])
                pd, pdk = c.ps()
                for mt in range(2):
                    mm(c, pd[:, :], c.ones_b[:], pT[:, h * 2 + mt, :], mt == 0, mt == 1, ["ones_b", ("pT", h, mt)], [pdk])
                kb.op("dve", lambda e, pd=pd: e.reciprocal(out=rden[:], in_=pd[:, :]), reads=[pdk], writes=["rden"])
                po, pok = c.ps()
                for mt in range(2):
                    mm(c, po[:, :], Vt[:, mt, h * 128:(h + 1) * 128], pT[:, h * 2 + mt, :], mt == 0, mt == 1,
                       [("Vt", mt), ("pT", h, mt)], [pok])
                kb.op("dve", lambda e, h=h, po=po: e.tensor_tensor(out=oT[:, h, :], in0=po[:, :], in1=rden[:], op=ALU.mult),
                      reads=[pok, "rden"], writes=[("oT", h)])
            for d in range(8):
                p, pk = c.ps()
                for h in range(4):
                    mm(c, p[:, :], wo[:, h, d * 128:(d + 1) * 128], oT[:, h, :], h == 0, h == 3, ["wo", ("oT", h)], [pk])
                kb.op("dve", lambda e, d=d, p=p, t0=t0: e.tensor_tensor(out=xT[:, d, t0:t0 + TT], in0=xT[:, d, t0:t0 + TT],
                                                                        in1=p[:, :], op=ALU.add),
                      reads=[pk, ("xT", d)], writes=[("xT", d)])
    c.barrier()
    if io.get("dbg_stage") == 2:
        vec_st.close()
        return

    with ExitStack() as s3:
        NG = 6
        wgu = [c.sb(f"wgu{i}", [128, 8, 512], BF16, s3) for i in range(4)]
        wdr = [c.sb(f"wdr{i}", [128, D], BF16, s3) for i in range(6)]
        actT = c.sb("actT", [128, NF, TT], BF16, s3)
        sil = [c.sb(f"sil{i}", [128, TT], BF16, s3) for i in range(2)]
        if E > 1:
            hF = c.sb("hF", [128, 8, TT], F32, s3)
            wr = c.sb("wr", [128, 8, 8], F32, s3)
            lg = c.sb("lg", [128, 4, 8], F32, s3)
            top8 = c.sb("top8", [128, 4, 8], F32, s3)
            gts = c.sb("gts", [128, 4, 8], F32, s3)
            gsc = c.sb("gsc", [128, 4, 4], F32, s3)
            dgate = c.sb("dgate", [128, 128], F32, s3)
            gB = [c.sb(f"gB{i}", [128, TT], F32, s3) for i in range(2)]
            ytmps = [c.sb(f"ytmp{i}", [128, TT], F32, s3) for i in range(2)]
            kb.dma("sp", wr[:], io["router_w"].rearrange("(k p) n -> p k n", p=128), writes=["wr"])
        wgu_i = 0
        wdr_i = 0
        for t in range(NTT):
            t0 = t * TT
            rmsnorm_tile(c, xT, "xT", t0, TT, vecs[:, V_FFN:V_FFN + 8], tmp, hT, "hT", out_f=(hF if E > 1 else None))
            if E > 1:
                for s in range(4):
                    p, pk = c.ps()
                    for k in range(8):
                        mm(c, p[:, 0:8], hF[:, k, s * 128:(s + 1) * 128], wr[:, k, :], k == 0, k == 7, [("hT_f", k), "wr"], [pk])
                    kb.op("dve", lambda e, s=s, p=p: e.tensor_copy(out=lg[:, s, :], in_=p[:, 0:8]), reads=[pk], writes=[("lg", s)])
                    kb.op("dve", lambda e, s=s: e.max(out=top8[:, s, :], in_=lg[:, s, :]), reads=[("lg", s)], writes=[("top8", s)])
                    kb.op("dve", lambda e, s=s: e.tensor_scalar(out=gsc[:, s, 0:1], in0=top8[:, s, 0:1], scalar1=-1.0, scalar2=None, op0=ALU.mult),
                          reads=[("top8", s)], writes=[("gsc", s, 0)])
                    kb.op("act", lambda e, s=s: e.activation(out=gsc[:, s, 1:2], in_=top8[:, s, 1:2], func=AF.Exp, bias=gsc[:, s, 0:1]),
                          reads=[("top8", s), ("gsc", s, 0)], writes=[("gsc", s, 1)])
                    kb.op("dve", lambda e, s=s: e.tensor_scalar(out=gsc[:, s, 1:2], in0=gsc[:, s, 1:2], scalar1=1.0, scalar2=None, op0=ALU.add),
                          reads=[("gsc", s, 1)], writes=[("gsc", s, 1)])
                    kb.op("dve", lambda e, s=s: e.reciprocal(out=gsc[:, s, 1:2], in_=gsc[:, s, 1:2]),
                          reads=[("gsc", s, 1)], writes=[("gsc", s, 1)])
                    kb.op("act", lambda e, s=s: e.activation(out=gts[:, s, :], in_=lg[:, s, :], func=AF.Exp, bias=gsc[:, s, 0:1]),
                          reads=[("lg", s), ("gsc", s, 0)], writes=[("gts", s)])
                    kb.op("dve", lambda e, s=s: e.tensor_scalar(out=lg[:, s, :], in0=lg[:, s, :], scalar1=top8[:, s, 1:2], scalar2=None, op0=ALU.is_ge),
                          reads=[("lg", s), ("top8", s)], writes=[("lg", s)])
                    kb.op("dve", lambda e, s=s: e.scalar_tensor_tensor(out=gts[:, s, :], in0=gts[:, s, :], scalar=gsc[:, s, 1:2], in1=lg[:, s, :],
                                                                       op0=ALU.mult, op1=ALU.mult),
                          reads=[("gts", s), ("gsc", s, 1), ("lg", s)], writes=[("gts", s)])
            for ex in range(E):
                if E > 1:
                    gb = gB[ex % 2]
                    gbk = f"gB{ex % 2}"
                    for s in range(4):
                        kb.op("dve", lambda e, s=s, ex=ex: e.tensor_scalar(out=dgate[:], in0=c.ident_f[:], scalar1=gts[:, s, ex:ex + 1],
                                                                           scalar2=None, op0=ALU.mult),
                              reads=["ident_f", ("gts", s)], writes=["dgate"])
                        p, pk = c.ps()
                        mm(c, p[:, 0:128], c.ones_f[:], dgate[:], True, True, ["ones_f", "dgate"], [pk])
                        kb.op("act", lambda e, s=s, p=p, gb=gb: e.activation(out=gb[:, s * 128:(s + 1) * 128], in_=p[:, 0:128], func=AF.Copy),
                              reads=[pk], writes=[gbk])
                for g in range(NG):
                    nf = 4 if g < 5 else 2
                    f0 = g * 512
                    sg, su = wgu[wgu_i % 4], wgu[(wgu_i + 1) % 4]
                    sgk, suk = f"wgu{wgu_i % 4}", f"wgu{(wgu_i + 1) % 4}"
                    wgu_i += 2
                    kb.dma("pool", sg[:, :, 0:nf * 128], io["w_gate"][ex][:, f0:f0 + nf * 128].rearrange("(k p) n -> p k n", p=128), writes=[sgk])
                    kb.dma("pool", su[:, :, 0:nf * 128], io["w_up"][ex][:, f0:f0 + nf * 128].rearrange("(k p) n -> p k n", p=128), writes=[suk])
                    for fi in range(nf):
                        f = g * 4 + fi
                        pg, pgk = c.ps()
                        pu, puk = c.ps()
                        for k in range(8):
                            mm(c, pg[:, :], sg[:, k, fi * 128:(fi + 1) * 128], hT[:, k, :], k == 0, k == 7, [sgk, ("hT", k)], [pgk])
                        for k in range(8):
                            mm(c, pu[:, :], su[:, k, fi * 128:(fi + 1) * 128], hT[:, k, :], k == 0, k == 7, [suk, ("hT", k)], [puk])
                        sl = sil[f % 2]
                        slk = f"sil{f % 2}"
                        kb.op("act", lambda e, pg=pg, sl=sl: e.activation(out=sl[:], in_=pg[:, :], func=AF.Silu), reads=[pgk], writes=[slk])
                        kb.op("dve", lambda e, pu=pu, sl=sl, f=f: e.tensor_tensor(out=actT[:, f, :], in0=pu[:, :], in1=sl[:], op=ALU.mult),
                              reads=[puk, slk], writes=[("actT", f)])
                banks = [c.ps() for _ in range(8)]
                for f in range(NF):
                    wd = wdr[wdr_i % 6]
                    wdk = f"wdr{wdr_i % 6}"
                    wdr_i += 1
                    kb.dma("pool", wd[:], io["w_down"][ex][f * 128:(f + 1) * 128, :], writes=[wdk])
                    for d in range(8):
                        p, pk = banks[d]
                        mm(c, p[:, :], wd[:, d * 128:(d + 1) * 128], actT[:, f, :], f == 0, f == NF - 1, [wdk, ("actT", f)], [pk])
                for d in range(8):
                    p, pk = banks[d]
                    if E > 1:
                        ytmp = ytmps[d % 2]
                        ytk = f"ytmp{d % 2}"
                        kb.op("dve", lambda e, p=p, gb=gb, ytmp=ytmp: e.tensor_tensor(out=ytmp[:], in0=p[:, :], in1=gb[:], op=ALU.mult),
                              reads=[pk, gbk], writes=[ytk])
                        kb.op("pool", lambda e, d=d, t0=t0, ytmp=ytmp: e.tensor_tensor(out=xT[:, d, t0:t0 + TT], in0=xT[:, d, t0:t0 + TT], in1=ytmp[:], op=ALU.add),
                              reads=[ytk, ("xT", d)], writes=[("xT", d)])
                    else:
                        kb.op("dve", lambda e, d=d, p=p, t0=t0: e.tensor_tensor(out=xT[:, d, t0:t0 + TT], in0=xT[:, d, t0:t0 + TT], in1=p[:, :], op=ALU.add),
                              reads=[pk, ("xT", d)], writes=[("xT", d)])
    c.barrier()

    with ExitStack() as s4:
        if not last:
            for t in range(NTT):
                t0 = t * TT
                rmsnorm_tile(c, xT, "xT", t0, TT, vecs[:, V_NEXT:V_NEXT + 8], tmp, hT, "hT")
                h_store(c, io["h_next"], hT, t0, TT, [("hT", k) for k in range(8)], ["h_next_d"])
                if t == NTT - 1 and "tail_next" in io:
                    kb.dma("sp", io["tail_next"].rearrange("(k p) n -> p k n", p=128), hT[:, :, TT - 32:TT],
                           reads=[("hT", k) for k in range(8)], writes=["tail_next_d"])
        else:
            hF2 = c.sb("hF2", [128, 8, TT], F32, s4)
            otm = c.sb("otm", [128, 4, D], F32, s4)
            for t in range(NTT):
                t0 = t * TT
                rmsnorm_tile(c, xT, "xT", t0, TT, vecs[:, V_NEXT:V_NEXT + 8], tmp, hT, "hT", out_f=hF2)
                for s in range(4):
                    for kk in range(2):
                        p, pk = c.ps()
                        for k4 in range(4):
                            k = kk * 4 + k4
                            kb.op("pe", lambda e, k=k, k4=k4, s=s, p=p: e.transpose(out=p[:, k4 * 128:(k4 + 1) * 128],
                                                                                    in_=hF2[:, k, s * 128:(s + 1) * 128], identity=c.ident_f[:]),
                                  reads=[("hT_f", k), "ident_f"], writes=[pk])
                        kb.op("act", lambda e, s=s, kk=kk, p=p: e.activation(out=otm[:, s, kk * 512:(kk + 1) * 512], in_=p[:, :], func=AF.Copy),
                              reads=[pk], writes=[("otm", s)])
                kb.dma("sp", io["out"][t0:t0 + TT, :].rearrange("(s p) d -> p s d", p=128), otm[:],
                       reads=[("otm", s) for s in range(4)])
    c.barrier()
    vec_st.close()


from contextlib import ExitStack as _ES
import ml_dtypes as _mld

NPBF = _mld.bfloat16


def build_T(E, last, dbg_stage=0):
    nc = bass.Bass("TRN2", target_bir_lowering=False)
    io = {}

    def din(name, shape, dt=F32):
        io[name] = nc.dram_tensor(name, shape, dt, kind="ExternalInput").ap()

    def dout(name, shape, dt=F32):
        io[name] = nc.dram_tensor(name, shape, dt, kind="ExternalOutput").ap()

    din("xT_in", [D, NT]); din("h_own", [D, NT], BF16); din("h_halo", [D, 32], BF16)
    din("catm", [4, 64, NT], BF16); din("catf", [4, 128, NT], BF16); din("mem", [256, D])
    din("vecs", [128, NV_T]); din("w_c", [D, 512]); din("w_out", [D, D]); din("w_q", [D, 512])
    din("w_kv", [D, D]); din("w_o", [512, D])
    din("w_gate", [E, D, DFF]); din("w_up", [E, D, DFF]); din("w_down", [E, DFF, D])
    if E > 1:
        din("router_w", [D, 8])
    if last:
        dout("out", [NT, D])
    else:
        dout("xT_out", [D, NT]); dout("h_next", [D, NT], BF16)
    io["dbg_stage"] = dbg_stage
    with _ES() as st:
        c = Ctx(nc, st)
        c.setup()
        xT = c.sb("xT", [128, 8, NT], F32)
        c.kb.dma("sp", xT[:], io["xT_in"].rearrange("(k p) n -> p k n", p=128), writes=[("xT", k) for k in range(8)])
        phase_T(c, io, E, last and not dbg_stage, xT)
        evs = []
        if not last or dbg_stage:
            key = "xT_out" if not last else "out"
            if last:
                io["xT_dbg"] = None
            evs.append(c.kb.dma("sp", io["xT_out"].rearrange("(k p) n -> p k n", p=128), xT[:],
                                reads=[("xT", k) for k in range(8)]))
        c.barrier()
        c.kb.flush()
    return nc


def vecs_T(inp, l, last):
    v = np.zeros((128, NV_T), np.float32)
    fm = lambda w: np.asarray(w, np.float32).reshape(-1, 128).T
    v[:, 0:8] = fm(inp["norm_xattn_w"][l]); v[:, 8:16] = fm(inp["norm_mem_w"][l]); v[:, 16:24] = fm(inp["norm_ffn_w"][l])
    v[:, 24:32] = fm(inp["norm_final_w"]) if last else fm(inp["norm_mix_w"][l + 1])
    v[:, 32:34] = fm(inp["conf_conv_b"][l]); v[:, 34:36] = fm(inp["conf_ln_w"][l]); v[:, 36:38] = fm(inp["conf_ln_b"][l])
    cw = np.asarray(inp["conf_conv_w"][l], np.float32)
    for j in range(31):
        v[:, 38 + 2 * j:40 + 2 * j] = fm(cw[j])
    return v


NCH = SEQ // 64
NKT = SEQ // 128
NQT = SEQ // TT
GRP = 4


def AP3(t, off, dims):
    return bass.AP(t[:].tensor, off, [list(d) for d in dims])


def log_sigmoid_tile(c, x, out, tmp1, tmp2, bias_ap, keys):
    kb = c.kb
    kx, ko, k1, k2 = keys
    kb.op("dve", lambda e: e.tensor_scalar(out=x, in0=x, scalar1=bias_ap, scalar2=None, op0=ALU.add), reads=[kx, "vecsM"], writes=[kx])
    kb.op("dve", lambda e: e.tensor_scalar(out=tmp1, in0=x, scalar1=-1.0, scalar2=None, op0=ALU.mult), reads=[kx], writes=[k1])
    kb.op("dve", lambda e: e.tensor_tensor(out=tmp1, in0=tmp1, in1=x, op=ALU.max), reads=[kx, k1], writes=[k1])
    kb.op("act", lambda e: e.activation(out=tmp1, in_=tmp1, func=AF.Exp, scale=-1.0), reads=[k1], writes=[k1])
    kb.op("dve", lambda e: e.tensor_scalar(out=tmp1, in0=tmp1, scalar1=1.0, scalar2=None, op0=ALU.add), reads=[k1], writes=[k1])
    kb.op("act", lambda e: e.activation(out=tmp1, in_=tmp1, func=AF.Ln), reads=[k1], writes=[k1])
    kb.op("dve", lambda e: e.tensor_scalar(out=tmp2, in0=x, scalar1=0.0, scalar2=None, op0=ALU.min), reads=[kx], writes=[k2])
    kb.op("dve", lambda e: e.tensor_tensor(out=out, in0=tmp2, in1=tmp1, op=ALU.subtract), reads=[k1, k2], writes=[ko])


def phase_M_mlstm(c, io):
    nc, kb = c.nc, c.kb
    from contextlib import ExitStack
    with ExitStack() as s0:
        vm = c.sb("vecsM_sb", [128, 16], F32, s0)
        wml = c.sb("wml", [128, 8, 258], BF16, s0)
        qT = c.sb("m_qT", [64, SEQ], BF16, s0)
        kT = c.sb("m_kT", [64, SEQ], BF16, s0)
        Vaug = c.sb("m_Vaug", [64, NCH, 65], BF16, s0)
        og = c.sb("m_og", [64, SEQ], BF16, s0)
        iC = c.sb("m_iC", [128, 64], F32, s0)
        fC = c.sb("m_fC", [128, 64], F32, s0)
        eps_t = c.sb("m_eps", [128, 1], F32, s0)
        kb.dma("sp", vm[:], io["vecsM"], writes=["vecsM"])
        kb.dma("pool", wml[:], io["w_ml"].rearrange("(k p) n -> p k n", p=128), writes=["wml"])
        kb.op("pool", lambda e: e.memset(Vaug[:, :, 64:65], 1.0), writes=["Vaug1"])
        kb.op("pool", lambda e: e.memset(eps_t[:], EPS), writes=["m_eps"])
        with ExitStack() as s1:
            hTb = [c.sb(f"m_hT{i}", [128, 8, TT], BF16, s1) for i in range(2)]
            zq = c.sb("m_zq", [64, TT + 3], F32, s1)
            zk = c.sb("m_zk", [64, TT + 3], F32, s1)
            cacc = [c.sb(f"m_cacc{i}", [64, TT], F32, s1) for i in range(2)]
            vt = c.sb("m_vt", [64, TT], F32, s1)
            rows = [c.sb(f"m_rows{i}", [2, TT], F32, s1) for i in range(2)]
            kb.op("pool", lambda e: e.memset(zq[:, 0:3], 0.0), writes=["zq"])
            kb.op("pool", lambda e: e.memset(zk[:, 0:3], 0.0), writes=["zk"])
            for tt in range(NQT):
                j, off = tt // 4, (tt % 4) * TT
                hT = hTb[tt % 2]
                hk = f"m_hT{tt % 2}"
                tok = slice(tt * TT, (tt + 1) * TT)
                h_load(c, io["hT_all"], hT, off, TT, [hk], j=j)
                for nm, z, col0, vc, dst in (("q", zq, 0, 0, qT), ("k", zk, 64, 5, kT)):
                    p, pk = c.ps()
                    for k in range(8):
                        mm(c, p[0:64, :], wml[:, k, col0:col0 + 64], hT[:, k, :], k == 0, k == 7, ["wml", hk], [pk])
                    zkey = "z" + nm
                    kb.op("act", lambda e, z=z, p=p: e.activation(out=z[:, 3:TT + 3], in_=p[0:64, :], func=AF.Copy), reads=[pk], writes=[zkey])
                    ca = cacc[0 if nm == "q" else 1]
                    ck = "cacc" + nm
                    kb.op("dve", lambda e, z=z, ca=ca, vc=vc: e.tensor_scalar(out=ca[:], in0=z[:, 0:TT], scalar1=vm[0:64, vc:vc + 1], scalar2=vm[0:64, vc + 4:vc + 5],
                                                                              op0=ALU.mult, op1=ALU.add), reads=[zkey, "vecsM"], writes=[ck])
                    for jj in range(1, 4):
                        kb.op("dve", lambda e, z=z, ca=ca, vc=vc, jj=jj: e.scalar_tensor_tensor(out=ca[:], in0=z[:, jj:jj + TT], scalar=vm[0:64, vc + jj:vc + jj + 1], in1=ca[:],
                                                                                                 op0=ALU.mult, op1=ALU.add), reads=[zkey, ck, "vecsM"], writes=[ck])
                    kb.op("act", lambda e, ca=ca, dst=dst, tok=tok: e.activation(out=dst[:, tok], in_=ca[:], func=AF.Silu), reads=[ck], writes=[("m_" + nm + "T", tt)])
                    kb.op("dve", lambda e, z=z: e.tensor_copy(out=z[:, 0:3], in_=z[:, TT:TT + 3]), reads=[zkey], writes=[zkey])
                p, pk = c.ps()
                for k in range(8):
                    mm(c, p[0:64, :], wml[:, k, 128:192], hT[:, k, :], k == 0, k == 7, ["wml", hk], [pk])
                kb.op("act", lambda e, p=p: e.activation(out=vt[:], in_=p[0:64, :], func=AF.Copy), reads=[pk], writes=["m_vt"])
                p2, p2k = c.ps()
                for ci in range(8):
                    kb.op("pe", lambda e, ci=ci, p2=p2: e.transpose(out=p2[0:64, ci * 64:(ci + 1) * 64], in_=vt[:, ci * 64:(ci + 1) * 64], identity=c.ident_f[0:64, 0:64]),
                          reads=["m_vt", "ident_f"], writes=[p2k])
                kb.op("dve", lambda e, p2=p2, tt=tt: e.tensor_copy(out=Vaug[:, tt * 8:(tt + 1) * 8, 0:64], in_=p2[0:64, :].rearrange("p (c d) -> p c d", d=64)),
                      reads=[p2k], writes=[("Vaug", tt)])
                p, pk = c.ps()
                for k in range(8):
                    mm(c, p[0:64, :], wml[:, k, 192:256], hT[:, k, :], k == 0, k == 7, ["wml", hk], [pk])
                kb.op("act", lambda e, p=p, tok=tok: e.activation(out=og[:, tok], in_=p[0:64, :], func=AF.Sigmoid), reads=[pk], writes=[("og", tt)])
                p, pk = c.ps()
                for k in range(8):
                    mm(c, p[0:2, :], wml[:, k, 256:258], hT[:, k, :], k == 0, k == 7, ["wml", hk], [pk])
                rw = rows[tt % 2]
                rk = f"m_rows{tt % 2}"
                kb.op("act", lambda e, p=p, rw=rw: e.activation(out=rw[:], in_=p[0:2, :], func=AF.Copy), reads=[pk], writes=[rk])
                kb.dma("sp", iC[tt * 8:(tt + 1) * 8, :], AP3(rw, 0, [[TT, 1], [64, 8], [1, 64]]), reads=[rk], writes=["iC"])
                kb.dma("sp", fC[tt * 8:(tt + 1) * 8, :], AP3(rw, TT, [[TT, 1], [64, 8], [1, 64]]), reads=[rk], writes=["fC"])
        c.barrier()
        Uall = c.sb("m_Uall", [64, 65, NCH], F32, s0)
        wgT = c.sb("m_wgT", [64, NCH], F32, s0)
        flT = c.sb("m_flT", [64, NCH], F32, s0)
        dB = c.sb("m_dB", [64, NCH], F32, s0)
        dB0 = c.sb("m_dB0", [64, NCH], F32, s0)
        with ExitStack() as s2:
            t1 = c.sb("g_t1", [128, 64], F32, s2)
            t2 = c.sb("g_t2", [128, 64], F32, s2)
            lf = c.sb("g_lf", [128, 64], F32, s2)
            bb = c.sb("g_b", [128, 64], F32, s2)
            aa = c.sb("g_a", [128, 64], F32, s2)
            AA = c.sb("g_A", [128, 64], F32, s2)
            MM = c.sb("g_M", [128, 64], F32, s2)
            wg = c.sb("g_wg", [128, 64], F32, s2)
            fl = c.sb("g_fl", [128, 64], F32, s2)
            on = c.sb("g_on", [128, 64], F32, s2)
            r1 = c.sb("g_r1", [1, 128], F32, s2)
            r2 = c.sb("g_r2", [1, 128], F32, s2)
            r3 = c.sb("g_r3", [1, 128], F32, s2)
            r4 = c.sb("g_r4", [1, 128], F32, s2)
            mcol = c.sb("g_mcol", [128, 1], F32, s2)
            nM63 = c.sb("g_nM63", [128, 1], F32, s2)
            dec = c.sb("g_dec", [128, 1], F32, s2)
            dgd = c.sb("g_dgd", [128, 128], F32, s2)
            Xb = [c.sb(f"g_X{i}", [128, 8, 64], F32, s2) for i in range(2)]
            kw32 = [c.sb(f"g_kw32{i}", [64, TT], F32, s2) for i in range(2)]
            kwTok = c.sb("g_kwTok", [64, NCH, 64], BF16, s2)
            log_sigmoid_tile(c, fC[:], lf[:], t1[:], t2[:], vm[:, 12:13], ("fC", "g_lf", "g_t1", "g_t2"))
            kb.op("dve", lambda e: e.tensor_scalar(out=iC[:], in0=iC[:], scalar1=vm[:, 11:12], scalar2=None, op0=ALU.add), reads=["iC", "vecsM"], writes=["iC"])
            kb.op("pool", lambda e: e.memset(on[:], 1.0), writes=["g_on"])
            kb.op("dve", lambda e: e.tensor_tensor_scan(out=bb[:], data0=on[:], data1=lf[:], initial=0.0, op0=ALU.mult, op1=ALU.add),
                  reads=["g_on", "g_lf"], writes=["g_b"])
            kb.op("dve", lambda e: e.tensor_tensor(out=aa[:], in0=iC[:], in1=bb[:], op=ALU.subtract), reads=["iC", "g_b"], writes=["g_a"])
            kb.op("dve", lambda e: e.tensor_tensor_scan(out=AA[:], data0=aa[:], data1=aa[:], initial=-1e30, op0=ALU.max, op1=ALU.max),
                  reads=["g_a"], writes=["g_A"])
            p, pk = c.ps()
            kb.op("pe", lambda e, p=p: e.transpose(out=p[0:1, 0:128], in_=AA[:, 63:64], identity=c.ident_f[:]), reads=["g_A", "ident_f"], writes=[pk])
            kb.op("pe", lambda e, p=p: e.transpose(out=p[0:1, 128:256], in_=bb[:, 63:64], identity=c.ident_f[:]), reads=["g_b", "ident_f"], writes=[pk])
            kb.op("dve", lambda e, p=p: e.tensor_copy(out=r1[:], in_=p[0:1, 0:128]), reads=[pk], writes=["g_r1"])
            kb.op("dve", lambda e, p=p: e.tensor_copy(out=r2[:], in_=p[0:1, 128:256]), reads=[pk], writes=["g_r2"])
            kb.op("dve", lambda e: e.tensor_tensor_scan(out=r3[:], data0=r1[:], data1=r2[:], initial=0.0, op0=ALU.max, op1=ALU.add),
                  reads=["g_r1", "g_r2"], writes=["g_r3"])
            kb.op("pool", lambda e: e.memset(r4[:, 0:1], 0.0), writes=["g_r4a"])
            kb.op("dve", lambda e: e.tensor_copy(out=r4[:, 1:128], in_=r3[:, 0:127]), reads=["g_r3"], writes=["g_r4b"])
            p, pk = c.ps()
            kb.op("pe", lambda e, p=p: e.transpose(out=p[:, 0:1], in_=r4[:], identity=c.ident_f[0:1, 0:1]), reads=["g_r4a", "g_r4b", "ident_f"], writes=[pk])
            kb.op("dve", lambda e, p=p: e.tensor_copy(out=mcol[:], in_=p[:, 0:1]), reads=[pk], writes=["g_mcol"])
            kb.op("dve", lambda e: e.tensor_scalar(out=MM[:], in0=AA[:], scalar1=mcol[:, 0:1], scalar2=None, op0=ALU.max), reads=["g_A", "g_mcol"], writes=["g_M"])
            kb.op("dve", lambda e: e.tensor_scalar(out=nM63[:], in0=MM[:, 63:64], scalar1=-1.0, scalar2=None, op0=ALU.mult), reads=["g_M"], writes=["g_nM63"])
            kb.op("act", lambda e: e.activation(out=wg[:], in_=aa[:], func=AF.Exp, bias=nM63[:, 0:1]), reads=["g_a", "g_nM63"], writes=["g_wg"])
            kb.op("act", lambda e: e.activation(out=dec[:], in_=mcol[:], func=AF.Exp, bias=nM63[:, 0:1]), reads=["g_mcol", "g_nM63"], writes=["g_dec"])
            kb.op("act", lambda e: e.activation(out=fl[:], in_=bb[:], func=AF.Exp, scale=-1.0, bias=nM63[:, 0:1]), reads=["g_b", "g_nM63"], writes=["g_fl"])
            p, pk = c.ps()
            kb.op("pe", lambda e, p=p: e.transpose(out=p[0:64, 0:128], in_=wg[:], identity=c.ident_f[:]), reads=["g_wg", "ident_f"], writes=[pk])
            kb.op("pe", lambda e, p=p: e.transpose(out=p[0:64, 128:256], in_=fl[:], identity=c.ident_f[:]), reads=["g_fl", "ident_f"], writes=[pk])
            kb.op("dve", lambda e, p=p: e.tensor_copy(out=wgT[:], in_=p[0:64, 0:128]), reads=[pk], writes=["m_wgT"])
            kb.op("dve", lambda e, p=p: e.tensor_copy(out=flT[:], in_=p[0:64, 128:256]), reads=[pk], writes=["m_flT"])
            kb.op("dve", lambda e: e.tensor_scalar(out=dgd[:], in0=c.ident_f[:], scalar1=dec[:, 0:1], scalar2=None, op0=ALU.mult), reads=["ident_f", "g_dec"], writes=["g_dgd"])
            p, pk = c.ps()
            mm(c, p[0:64, 0:128], c.ones_f[:, 0:64], dgd[:], True, True, ["ones_f", "g_dgd"], [pk])
            kb.op("dve", lambda e, p=p: e.tensor_copy(out=dB[:], in_=p[0:64, 0:128]), reads=[pk], writes=["m_dB"])
            kb.op("dve", lambda e, p=p: e.tensor_copy(out=dB0[:], in_=p[0:64, 0:128]), reads=[pk], writes=["m_dB0"])
            kb.op("pool", lambda e: e.memset(dB0[:, 0:1], 0.0), reads=["m_dB0"], writes=["m_dB0"])
            for tt in range(NQT):
                tok = slice(tt * TT, (tt + 1) * TT)
                X = Xb[tt % 2]
                Xk = f"g_X{tt % 2}"
                kb.op("dve", lambda e, X=X, tt=tt: e.tensor_tensor(out=X[:], in0=AP3(c.ident_f, 8 * tt, [[128, 128], [1, 8], [0, 64]]),
                                                                   in1=AP3(wg, 0, [[64, 128], [0, 8], [1, 64]]), op=ALU.mult),
                      reads=["ident_f", "g_wg"], writes=[Xk])
                p, pk = c.ps()
                mm(c, p[0:64, :], c.ones_f[:, 0:64], X[:].rearrange("p c s -> p (c s)"), True, True, ["ones_f", Xk], [pk])
                k32 = kw32[tt % 2]
                k32k = f"g_kw32{tt % 2}"
                kb.op("dve", lambda e, p=p, k32=k32, tok=tok: e.scalar_tensor_tensor(out=k32[:], in0=kT[:, tok], scalar=0.125, in1=p[0:64, :], op0=ALU.mult, op1=ALU.mult),
                      reads=[pk, ("m_kT", tt)], writes=[k32k])
                kb.op("act", lambda e, k32=k32, tok=tok: e.activation(out=kT[:, tok], in_=k32[:], func=AF.Copy), reads=[k32k], writes=[("m_kT", tt)])
                p2, p2k = c.ps()
                for ci in range(8):
                    kb.op("pe", lambda e, ci=ci, p2=p2, k32=k32: e.transpose(out=p2[0:64, ci * 64:(ci + 1) * 64], in_=k32[:, ci * 64:(ci + 1) * 64], identity=c.ident_f[0:64, 0:64]),
                          reads=[k32k, "ident_f"], writes=[p2k])
                kb.op("dve", lambda e, p2=p2, tt=tt: e.tensor_copy(out=kwTok[:, tt * 8:(tt + 1) * 8, :], in_=p2[0:64, :].rearrange("p (c d) -> p c d", d=64)),
                      reads=[p2k], writes=[("kwTok", tt)])
            for g in range(NCH // GRP):
                p, pk = c.ps()
                for ci in range(GRP):
                    ch = g * GRP + ci
                    mm(c, p[0:64, ci * 65:(ci + 1) * 65], kwTok[:, ch, :], Vaug[:, ch, :], True, True, [("kwTok", ch // 8), ("Vaug", ch // 8), "Vaug1"], [pk])
                kb.op("dve", lambda e, p=p, g=g: e.tensor_copy(out=AP3(Uall, g * GRP, [[65 * NCH, 64], [1, GRP], [NCH, 65]]),
                                                               in_=p[0:64, 0:GRP * 65].rearrange("p (c d) -> p c d", d=65)),
                      reads=[pk], writes=["Uall"])
        c.barrier()
        Cn = Uall
        Eb = c.sb("m_E", [64, NCH, 65], BF16, s0)
        for dv in range(65):
            kb.op("dve", lambda e, dv=dv: e.tensor_tensor_scan(out=Cn[:, dv, :], data0=dB0[:], data1=Uall[:, dv, :], initial=0.0, op0=ALU.mult, op1=ALU.add),
                  reads=["m_dB0", "Uall"], writes=[("Cn", dv), "Uall"])
        kb.op("pool", lambda e: e.memset(Eb[:, 0:1, :], 0.0), writes=["E0"])
        kb.op("dve", lambda e: e.tensor_tensor(out=Eb[:, 1:NCH, :], in0=AP3(Cn, 0, [[65 * NCH, 64], [1, NCH - 1], [NCH, 65]]),
                                               in1=AP3(dB, 1, [[NCH, 64], [1, NCH - 1], [0, 65]]), op=ALU.mult),
              reads=[("Cn", dv) for dv in range(65)] + ["m_dB"], writes=["E"])
        with ExitStack() as s4:
            mask = c.sb("o_mask", [64, 64], F32, s4)
            sT = [c.sb(f"o_sT{i}", [64, GRP * 64], BF16, s4) for i in range(2)]
            den = c.sb("o_den", [64, GRP], F32, s4)
            hn = c.sb("o_hn", [64, GRP, 64], F32, s4)
            hsq = c.sb("o_hsq", [64, GRP, 64], F32, s4)
            ss = c.sb("o_ss", [64, GRP], F32, s4)
            cst = [c.sb(f"o_cst{i}", [64, GRP * 64], BF16, s4) for i in range(2)]
            kb.op("pool", lambda e: e.memset(mask[:], 1.0), writes=["o_mask"])
            kb.op("pool", lambda e: e.affine_select(out=mask[:], in_=mask[:], pattern=[[1, 64]], compare_op=ALU.is_ge, fill=0.0, base=0, channel_multiplier=-1),
                  reads=["o_mask"], writes=["o_mask"])
            for g in range(NCH // GRP):
                c0 = g * GRP
                p, pk = c.ps()
                for ci in range(GRP):
                    ch = c0 + ci
                    cs = slice(ch * 64, (ch + 1) * 64)
                    mm(c, p[0:64, ci * 64:(ci + 1) * 64], kT[:, cs], qT[:, cs], True, True, [("m_kT", ch // 8), ("m_qT", ch // 8)], [pk])
                st_ = sT[g % 2]
                stk = f"o_sT{g % 2}"
                kb.op("dve", lambda e, p=p, st_=st_: e.tensor_tensor(out=st_[:].rearrange("p (c t) -> p c t", t=64), in0=p[0:64, 0:GRP * 64].rearrange("p (c t) -> p c t", t=64),
                                                                     in1=AP3(mask, 0, [[64, 64], [0, GRP], [1, 64]]), op=ALU.mult),
                      reads=[pk, "o_mask"], writes=[stk])
                po, pok = c.ps()
                for ci in range(GRP):
                    ch = c0 + ci
                    cs = slice(ch * 64, (ch + 1) * 64)
                    mm(c, po[0:64, ci * 65:(ci + 1) * 65], st_[:, ci * 64:(ci + 1) * 64], Vaug[:, ch, :], True, False, [stk, ("Vaug", ch // 8), "Vaug1"], [pok])
                    mm(c, po[0:64, ci * 65:(ci + 1) * 65], qT[:, cs], Eb[:, ch, :], False, True, [("m_qT", ch // 8), "E", "E0"], [pok])
                po3 = po[0:64, 0:GRP * 65].rearrange("p (c d) -> p c d", d=65)
                den3 = den[:].rearrange("p (c o) -> p c o", o=1)
                kb.op("dve", lambda e, po3=po3, den3=den3: e.tensor_scalar(out=den3, in0=po3[:, :, 64:65], scalar1=-1.0, scalar2=None, op0=ALU.mult),
                      reads=[pok], writes=["o_den"])
                kb.op("dve", lambda e, po3=po3, den3=den3: e.tensor_tensor(out=den3, in0=po3[:, :, 64:65], in1=den3, op=ALU.max),
                      reads=[pok, "o_den"], writes=["o_den"])
                kb.op("dve", lambda e, c0=c0: e.tensor_tensor(out=den[:], in0=den[:], in1=flT[:, c0:c0 + GRP], op=ALU.max),
                      reads=["o_den", "m_flT"], writes=["o_den"])
                kb.op("dve", lambda e: e.reciprocal(out=den[:], in_=den[:]), reads=["o_den"], writes=["o_den"])
                kb.op("dve", lambda e, po3=po3: e.tensor_tensor(out=hn[:], in0=po3[:, :, 0:64], in1=AP3(den, 0, [[GRP, 64], [1, GRP], [0, 64]]), op=ALU.mult),
                      reads=[pok, "o_den"], writes=["o_hn"])
                kb.op("act", lambda e: e.activation(out=hsq[:], in_=hn[:], func=AF.Square), reads=["o_hn"], writes=["o_hsq"])
                kb.op("dve", lambda e: e.tensor_reduce(out=ss[:], in_=hsq[:], axis=AX.X, op=ALU.add), reads=["o_hsq"], writes=["o_ss"])
                kb.op("act", lambda e: e.activation(out=ss[:], in_=ss[:], func=AF.Sqrt, scale=1.0 / 64, bias=eps_t[0:64, 0:1]), reads=["o_ss", "m_eps"], writes=["o_ss"])
                kb.op("dve", lambda e: e.reciprocal(out=ss[:], in_=ss[:]), reads=["o_ss"], writes=["o_ss"])
                kb.op("dve", lambda e: e.tensor_tensor(out=hn[:], in0=hn[:], in1=AP3(ss, 0, [[GRP, 64], [1, GRP], [0, 64]]), op=ALU.mult),
                      reads=["o_hn", "o_ss"], writes=["o_hn"])
                pt, ptk = c.ps()
                for ci in range(GRP):
                    kb.op("pe", lambda e, ci=ci, pt=pt: e.transpose(out=pt[0:64, ci * 64:(ci + 1) * 64], in_=hn[:, ci, :], identity=c.ident_f[0:64, 0:64]),
                          reads=["o_hn", "ident_f"], writes=[ptk])
                cs_ = cst[g % 2]
                csk = f"o_cst{g % 2}"
                toks = slice(c0 * 64, (c0 + GRP) * 64)
                kb.op("dve", lambda e, pt=pt, cs_=cs_, toks=toks: e.scalar_tensor_tensor(out=cs_[:], in0=pt[0:64, 0:GRP * 64], scalar=vm[0:64, 10:11], in1=og[:, toks],
                                                                                       op0=ALU.mult, op1=ALU.mult),
                      reads=[ptk, "vecsM", ("og", (c0 * 64) // TT)], writes=[csk])
                kb.dma("sp", io["catm_out"][:, toks], cs_[:], reads=[csk])
        c.barrier()


def phase_M_fox(c, io):
    nc, kb = c.nc, c.kb
    from contextlib import ExitStack
    NEG = -30000.0
    with ExitStack() as s0:
        vm = c.sb("vecsMf_sb", [128, 16], F32, s0)
        wfx = c.sb("wfx", [128, 8, 386], BF16, s0)
        fq = [c.sb(f"f_q{h}", [64, SEQ], BF16, s0) for h in range(2)]
        fk = [c.sb(f"f_k{h}", [64, SEQ], BF16, s0) for h in range(2)]
        fV = [c.sb(f"f_V{h}", [128, NKT, 65], BF16, s0) for h in range(2)]
        fC = [c.sb(f"f_C{h}", [64, 128], F32, s0) for h in range(2)]
        kb.dma("sp", vm[:], io["vecsM"], writes=["vecsM"])
        kb.dma("pool", wfx[:], io["w_fx"].rearrange("(k p) n -> p k n", p=128), writes=["wfx"])
        for h in range(2):
            kb.op("pool", lambda e, h=h: e.memset(fV[h][:, :, 64:65], 1.0), writes=[("fV1", h)])
        with ExitStack() as s1:
            hTb = [c.sb(f"f_hT{i}", [128, 8, TT], BF16, s1) for i in range(2)]
            vt = c.sb("f_vt", [64, TT], F32, s1)
            rows = [c.sb(f"f_rows{i}", [2, TT], F32, s1) for i in range(2)]
            for tt in range(NQT):
                j, off = tt // 4, (tt % 4) * TT
                hT = hTb[tt % 2]
                hk = f"f_hT{tt % 2}"
                tok = slice(tt * TT, (tt + 1) * TT)
                h_load(c, io["hT_all"], hT, off, TT, [hk], j=j)
                for h in range(2):
                    b0 = h * 192
                    p, pk = c.ps()
                    for k in range(8):
                        mm(c, p[0:64, :], wfx[:, k, b0:b0 + 64], hT[:, k, :], k == 0, k == 7, ["wfx", hk], [pk])
                    kb.op("act", lambda e, p=p, h=h, tok=tok: e.activation(out=fq[h][:, tok], in_=p[0:64, :], func=AF.Copy, scale=0.125), reads=[pk], writes=[("fq", h, tt)])
                    p, pk = c.ps()
                    for k in range(8):
                        mm(c, p[0:64, :], wfx[:, k, b0 + 64:b0 + 128], hT[:, k, :], k == 0, k == 7, ["wfx", hk], [pk])
                    kb.op("act", lambda e, p=p, h=h, tok=tok: e.activation(out=fk[h][:, tok], in_=p[0:64, :], func=AF.Copy), reads=[pk], writes=[("fk", h, tt)])
                    p, pk = c.ps()
                    for k in range(8):
                        mm(c, p[0:64, :], wfx[:, k, b0 + 128:b0 + 192], hT[:, k, :], k == 0, k == 7, ["wfx", hk], [pk])
                    kb.op("act", lambda e, p=p: e.activation(out=vt[:], in_=p[0:64, :], func=AF.Copy), reads=[pk], writes=["f_vt"])
                    p2, p2k = c.ps()
                    for ci in range(4):
                        kb.op("pe", lambda e, ci=ci, p2=p2: e.transpose(out=p2[:, ci * 64:(ci + 1) * 64], in_=vt[:, ci * 128:(ci + 1) * 128], identity=c.ident_f[0:64, 0:64]),
                              reads=["f_vt", "ident_f"], writes=[p2k])
                    kb.op("dve", lambda e, p2=p2, tt=tt, h=h: e.tensor_copy(out=fV[h][:, tt * 4:(tt + 1) * 4, 0:64], in_=p2[:, 0:256].rearrange("p (c d) -> p c d", d=64)),
                          reads=[p2k], writes=[("fV", h, tt)])
                p, pk = c.ps()
                for k in range(8):
                    mm(c, p[0:2, :], wfx[:, k, 384:386], hT[:, k, :], k == 0, k == 7, ["wfx", hk], [pk])
                rw = rows[tt % 2]
                rk = f"f_rows{tt % 2}"
                kb.op("act", lambda e, p=p, rw=rw: e.activation(out=rw[:], in_=p[0:2, :], func=AF.Copy), reads=[pk], writes=[rk])
                for h in range(2):
                    kb.dma("sp", fC[h][tt * 4:(tt + 1) * 4, :], AP3(rw, h * TT, [[TT, 1], [128, 4], [1, 128]]), reads=[rk], writes=[("fC", h)])
        c.barrier()
        ckT = [c.sb(f"f_ckT{h}", [128, NKT], F32, s0) for h in range(2)]
        cC = [c.sb(f"f_cC{h}", [64, 128], F32, s0) for h in range(2)]
        negm = c.sb("f_negm", [128, 4, TT], F32, s0)
        Ls = c.sb("f_Ls", [64, 64], F32, s0)
        with ExitStack() as s2:
            t1 = c.sb("f_t1", [64, 128], F32, s2)
            t2 = c.sb("f_t2", [64, 128], F32, s2)
            lf = c.sb("f_lf", [64, 128], F32, s2)
            on = c.sb("f_on", [64, 128], F32, s2)
            pre = c.sb("f_pre", [64, 1], F32, s2)
            kb.op("pool", lambda e: e.memset(on[:], 1.0), writes=["f_on"])
            kb.op("pool", lambda e: e.memset(Ls[:], 1.0), writes=["f_Ls"])
            kb.op("pool", lambda e: e.affine_select(out=Ls[:], in_=Ls[:], pattern=[[1, 64]], compare_op=ALU.is_ge, fill=0.0, base=-1, channel_multiplier=-1),
                  reads=["f_Ls"], writes=["f_Ls"])
            for r in range(4):
                kb.op("pool", lambda e, r=r: e.memset(negm[:, r, :], 0.0), writes=[("negm", r)])
                kb.op("pool", lambda e, r=r: e.affine_select(out=negm[:, r, :], in_=negm[:, r, :], pattern=[[1, TT]], compare_op=ALU.is_ge, fill=NEG,
                                                             base=-128 * r, channel_multiplier=-1), reads=[("negm", r)], writes=[("negm", r)])
            for h in range(2):
                log_sigmoid_tile(c, fC[h][:], lf[:], t1[:], t2[:], vm[0:64, 13 + h:14 + h], (("fC", h), "f_lf", "f_t1", "f_t2"))
                kb.op("dve", lambda e, h=h: e.tensor_tensor_scan(out=cC[h][:], data0=on[:], data1=lf[:], initial=0.0, op0=ALU.mult, op1=ALU.add),
                      reads=["f_on", "f_lf"], writes=[("cC", h)])
                p, pk = c.ps()
                mm(c, p[0:64, 0:1], Ls[:], cC[h][:, 127:128], True, True, ["f_Ls", ("cC", h)], [pk])
                kb.op("dve", lambda e, p=p: e.tensor_copy(out=pre[:], in_=p[0:64, 0:1]), reads=[pk], writes=["f_pre"])
                kb.op("dve", lambda e, h=h: e.tensor_scalar(out=cC[h][:], in0=cC[h][:], scalar1=pre[:, 0:1], scalar2=None, op0=ALU.add), reads=[("cC", h), "f_pre"], writes=[("cC", h)])
                p, pk = c.ps()
                kb.op("pe", lambda e, p=p, h=h: e.transpose(out=p[:, 0:64], in_=cC[h][:], identity=c.ident_f[0:64, 0:64]), reads=[("cC", h), "ident_f"], writes=[pk])
                kb.op("dve", lambda e, p=p, h=h: e.tensor_scalar(out=ckT[h][:], in0=p[:, 0:64], scalar1=-1.0, scalar2=None, op0=ALU.mult), reads=[pk], writes=[("ckT", h)])
        c.barrier()
        with ExitStack() as s3:
            X = c.sb("f_X", [64, 4, 128], F32, s3)
            cqB = c.sb("f_cqB", [128, TT], F32, s3)
            cqD = c.sb("f_cqD", [128, 4, TT], F32, s3)
            tmpb = [c.sb(f"f_tmp{i}", [128, TT], F32, s3) for i in range(3)]
            pTb = [c.sb(f"f_pT{i}", [128, TT], BF16, s3) for i in range(3)]
            osb = c.sb("f_osb", [65, TT], F32, s3)
            rden = c.sb("f_rden", [64, TT], F32, s3)
            outb = [c.sb(f"f_out{i}", [64, TT], BF16, s3) for i in range(2)]
            it = 0
            for h in range(2):
                for qi in range(NQT):
                    qs = slice(qi * TT, (qi + 1) * TT)
                    kb.op("dve", lambda e, h=h, qi=qi: e.tensor_tensor(out=X[:], in0=AP3(c.ident_f, 4 * qi, [[128, 64], [1, 4], [0, 128]]),
                                                                       in1=AP3(cC[h], 0, [[128, 64], [0, 4], [1, 128]]), op=ALU.mult),
                          reads=["ident_f", ("cC", h)], writes=["f_X"])
                    p, pk = c.ps()
                    mm(c, p[:, :], c.ones_f[0:64, :], X[:].rearrange("p r s -> p (r s)"), True, True, ["ones_f", "f_X"], [pk])
                    kb.op("act", lambda e, p=p: e.activation(out=cqB[:], in_=p[:, :], func=AF.Copy), reads=[pk], writes=["f_cqB"])
                    for r in range(4):
                        kb.op("pool", lambda e, r=r: e.tensor_tensor(out=cqD[:, r, :], in0=cqB[:], in1=negm[:, r, :], op=ALU.add),
                              reads=["f_cqB", ("negm", r)], writes=[("f_cqD", r)])
                    c.rot = list(range(6))
                    po, pok = c.psb[6 + qi % 2], f"psb{6 + qi % 2}"
                    nk = 4 * (qi + 1)
                    for kt in range(nk):
                        ps_, psk = c.ps()
                        mm(c, ps_[:, :], fk[h][:, kt * 128:(kt + 1) * 128], fq[h][:, qs], True, True, [("fk", h, kt // 4), ("fq", h, qi)], [psk])
                        tb = tmpb[it % 3]; tbk = f"f_tmp{it % 3}"
                        pb = pTb[it % 3]; pbk = f"f_pT{it % 3}"
                        it += 1
                        r = kt - 4 * qi
                        if r >= 0:
                            kb.op("dve", lambda e, ps_=ps_, tb=tb, r=r: e.tensor_tensor(out=tb[:], in0=ps_[:, :], in1=cqD[:, r, :], op=ALU.add),
                                  reads=[psk, ("f_cqD", r)], writes=[tbk])
                        else:
                            kb.op("dve", lambda e, ps_=ps_, tb=tb: e.tensor_tensor(out=tb[:], in0=ps_[:, :], in1=cqB[:], op=ALU.add),
                                  reads=[psk, "f_cqB"], writes=[tbk])
                        kb.op("act", lambda e, tb=tb, pb=pb, h=h, kt=kt: e.activation(out=pb[:], in_=tb[:], func=AF.Exp, bias=ckT[h][:, kt:kt + 1]),
                              reads=[tbk, ("ckT", h)], writes=[pbk])
                        mm(c, po[0:65, :], fV[h][:, kt, :], pb[:], kt == 0, kt == nk - 1, [("fV", h, kt // 4), ("fV1", h), pbk], [pok])
                    kb.op("act", lambda e, po=po: e.activation(out=osb[:], in_=po[0:65, :], func=AF.Copy), reads=[pok], writes=["f_osb"])
                    pd, pdk = c.ps()
                    mm(c, pd[0:64, :], c.ones_f[64:65, 0:64], osb[64:65, :], True, True, ["ones_f", "f_osb"], [pdk])
                    kb.op("dve", lambda e, pd=pd: e.reciprocal(out=rden[:], in_=pd[0:64, :]), reads=[pdk], writes=["f_rden"])
                    ob = outb[qi % 2]; obk = f"f_out{qi % 2}"
                    kb.op("dve", lambda e, ob=ob: e.tensor_tensor(out=ob[:], in0=osb[0:64, :], in1=rden[:], op=ALU.mult), reads=["f_osb", "f_rden"], writes=[obk])
                    cfo = io["catf_out"]
                    kb.dma("sp", (cfo[h][:, qs] if isinstance(cfo, list) else cfo[h * 64:(h + 1) * 64, qs]), ob[:], reads=[obk])
            c.rot = None
        c.barrier()


def build_M(which="both"):
    nc = bass.Bass("TRN2", target_bir_lowering=False)
    io = {}

    def din(name, shape, dt=F32):
        io[name] = nc.dram_tensor(name, shape, dt, kind="ExternalInput").ap()

    def dout(name, shape, dt=F32):
        io[name] = nc.dram_tensor(name, shape, dt, kind="ExternalOutput").ap()

    din("hT_all", [4, D, NT], BF16); din("w_ml", [D, 258]); din("w_fx", [D, 386]); din("vecsM", [128, 16])
    dout("catm_out", [64, SEQ], BF16); dout("catf_out", [128, SEQ], BF16)
    with _ES() as st:
        c = Ctx(nc, st)
        c.setup()
        if which in ("both", "mlstm"):
            phase_M_mlstm(c, io)
        if which in ("both", "fox"):
            phase_M_fox(c, io)
        c.barrier()
        c.kb.flush()
    return nc


def inputs_M(inp, l, g):
    w_in = np.asarray(inp["w_in"][l], np.float32)
    cols = np.concatenate([
        np.arange(g * 64, g * 64 + 64), 256 + np.arange(g * 64, g * 64 + 64),
        512 + np.arange(g * 64, g * 64 + 64), 768 + np.arange(g * 64, g * 64 + 64),
        [1024 + g, 1028 + g]])
    w_ml = np.ascontiguousarray(w_in[:, cols])
    fcols = []
    for hh in (2 * g, 2 * g + 1):
        for base in (1544, 2056, 2568):
            fcols.append(base + np.arange(hh * 64, hh * 64 + 64))
    fcols.append(np.array([3080 + 2 * g, 3080 + 2 * g + 1]))
    w_fx = np.ascontiguousarray(w_in[:, np.concatenate(fcols)])
    v = np.zeros((128, 16), np.float32)
    cw = np.asarray(inp["mlstm_conv_w"][l], np.float32)
    cb = np.asarray(inp["mlstm_conv_b"][l], np.float32)
    v[0:64, 0:4] = cw[:, g * 64:g * 64 + 64].T
    v[0:64, 4] = cb[g * 64:g * 64 + 64]
    v[0:64, 5:9] = cw[:, 256 + g * 64:256 + g * 64 + 64].T
    v[0:64, 9] = cb[256 + g * 64:256 + g * 64 + 64]
    v[0:64, 10] = np.asarray(inp["mlstm_norm_w"][l], np.float32)[g * 64:g * 64 + 64]
    v[:, 11] = inp["mlstm_b_i"][l][g]
    v[:, 12] = inp["mlstm_b_f"][l][g]
    v[:, 13] = inp["fox_b_f"][l][2 * g]
    v[:, 14] = inp["fox_b_f"][l][2 * g + 1]
    return {"w_ml": w_ml, "w_fx": w_fx, "vecsM": v}


def phase_P(c, io, xT):
    kb = c.kb
    from contextlib import ExitStack
    with ExitStack() as s0:
        vecs = c.sb("vecsP_sb", [128, 8], F32, s0)
        eps_t = c.sb("p_eps", [128, 1], F32, s0)
        sq = c.sb("p_sq", [128, 8, TT], BF16, s0)
        rstd = c.sb("p_rstd", [128, TT], F32, s0)
        hT = c.sb("p_hT", [128, 8, TT], BF16, s0)
        xt = [c.sb(f"p_xt{i}", [128, 4, D], F32, s0) for i in range(2)]
        tmp = {"sq": sq, "rstd": rstd, "eps": eps_t}
        kb.dma("sp", vecs[:], io["vecsP"], writes=["vecs"])
        kb.op("pool", lambda e: e.memset(eps_t[:], EPS), writes=["eps"])
        for t in range(NTT):
            t0 = t * TT
            xb = xt[t % 2]
            xk = f"p_xt{t % 2}"
            kb.dma("sp", xb[:], io["x_tok"][t0:t0 + TT, :].rearrange("(s p) d -> p s d", p=128), writes=[xk])
            for k in range(8):
                p, pk = c.ps()
                for s in range(4):
                    kb.op("pe", lambda e, k=k, s=s, p=p, xb=xb: e.transpose(out=p[:, s * 128:(s + 1) * 128], in_=xb[:, s, k * 128:(k + 1) * 128], identity=c.ident_f[:]),
                          reads=[xk, "ident_f"], writes=[pk])
                kb.op("act", lambda e, k=k, p=p, t0=t0: e.activation(out=xT[:, k, t0:t0 + TT], in_=p[:, :], func=AF.Copy), reads=[pk], writes=[("xT", k)])
            rmsnorm_tile(c, xT, "xT", t0, TT, vecs[:, 0:8], tmp, hT, "hT")
            h_store(c, io["h_next"], hT, t0, TT, [("hT", k) for k in range(8)], ["h_next_d"])
            if t == NTT - 1 and "tail_next" in io:
                kb.dma("sp", io["tail_next"].rearrange("(k p) n -> p k n", p=128), hT[:, :, TT - 32:TT],
                       reads=[("hT", k) for k in range(8)], writes=["tail_next_d"])
    c.barrier()


def build_P():
    nc = bass.Bass("TRN2", target_bir_lowering=False)
    io = {}
    io["x_tok"] = nc.dram_tensor("x_tok", [NT, D], F32, kind="ExternalInput").ap()
    io["vecsP"] = nc.dram_tensor("vecsP", [128, 8], F32, kind="ExternalInput").ap()
    io["xT_out"] = nc.dram_tensor("xT_out", [D, NT], F32, kind="ExternalOutput").ap()
    io["h_next"] = nc.dram_tensor("h_next", [D, NT], BF16, kind="ExternalOutput").ap()
    with _ES() as st:
        c = Ctx(nc, st)
        c.setup()
        xT = c.sb("xT", [128, 8, NT], F32)
        phase_P(c, io, xT)
        c.kb.dma("sp", io["xT_out"].rearrange("(k p) n -> p k n", p=128), xT[:], reads=[("xT", k) for k in range(8)])
        c.barrier()
        c.kb.flush()
    return nc


_CACHE = {}


def _get(name, fn):
    if name not in _CACHE:
        _CACHE[name] = fn()
    return _CACHE[name]


def kernel(**inp):
    inp = {k: np.asarray(v) for k, v in inp.items()}
    cores = list(range(8))
    B = 2
    x = inp["x"].astype(np.float32, copy=False)
    fm = lambda w: np.ascontiguousarray(np.asarray(w, np.float32).reshape(-1, 128).T)
    ncP = _get("P", build_P)
    maps = []
    for cid in cores:
        b, j = cid // 4, cid % 4
        maps.append({"x_tok": np.ascontiguousarray(x[b, j * NT:(j + 1) * NT]), "vecsP": fm(inp["norm_mix_w"][0])})
    res = run_bass_kernel_spmd(ncP, maps, core_ids=cores).results
    xT = [r["xT_out"] for r in res]
    hN = [r["h_next"] for r in res]
    out = None
    for l in range(2):
        last = (l == 1)
        E = 1 if l == 0 else 8
        ncM = _get("M", build_M)
        maps = []
        for cid in cores:
            b, g = cid // 4, cid % 4
            m = inputs_M(inp, l, g)
            m["hT_all"] = np.ascontiguousarray(np.stack([hN[b * 4 + jj] for jj in range(4)], axis=0))
            maps.append(m)
        resM = run_bass_kernel_spmd(ncM, maps, core_ids=cores).results
        ncT = _get(("T", E, last), lambda: build_T(E, last))
        maps = []
        for cid in cores:
            b, j = cid // 4, cid % 4
            tk = slice(j * NT, (j + 1) * NT)
            m = {"xT_in": xT[cid], "h_own": hN[cid]}
            m["h_halo"] = (np.ascontiguousarray(hN[cid - 1][:, NT - 32:NT]) if j > 0 else np.zeros((D, 32), NPBF))
            m["catm"] = np.ascontiguousarray(np.stack([resM[b * 4 + g]["catm_out"][:, tk] for g in range(4)], axis=0))
            m["catf"] = np.ascontiguousarray(np.stack([resM[b * 4 + g]["catf_out"][:, tk] for g in range(4)], axis=0))
            m["mem"] = np.ascontiguousarray(inp["mem"][b], dtype=np.float32)
            m["vecs"] = vecs_T(inp, l, last)
            m["w_c"] = np.ascontiguousarray(inp["w_in"][l][:, 1032:1544])
            m["w_out"] = inp["w_out"][l]; m["w_q"] = inp["xattn_w_q"][l]
            m["w_kv"] = inp["xattn_w_kv"][l]; m["w_o"] = inp["xattn_w_o"][l]
            if E == 1:
                m["w_gate"] = inp["ffn_w_gate"]; m["w_up"] = inp["ffn_w_up"]; m["w_down"] = inp["ffn_w_down"]
            else:
                m["w_gate"] = inp["moe_w_gate"][0]; m["w_up"] = inp["moe_w_up"][0]; m["w_down"] = inp["moe_w_down"][0]
                m["router_w"] = inp["router_w"][0]
            maps.append(m)
        resT = run_bass_kernel_spmd(ncT, maps, core_ids=cores).results
        if not last:
            xT = [r["xT_out"] for r in resT]
            hN = [r["h_next"] for r in resT]
        else:
            out = np.zeros((B, SEQ, D), np.float32)
            for cid in cores:
                b, j = cid // 4, cid % 4
                out[b, j * NT:(j + 1) * NT] = resT[cid]["out"]
    return out


RG = [[0, 1, 2, 3], [4, 5, 6, 7]]
_STOP = None


def build_fused(stop=None):
    nc = bass.Bass("TRN2", target_bir_lowering=False)
    io = {}
    if stop:
        io["dbg1"] = nc.dram_tensor("dbg1", [4 * D, NT], BF16, kind="ExternalOutput").ap()
        io["dbg2"] = nc.dram_tensor("dbg2", [512, SEQ], BF16, kind="ExternalOutput").ap()
        io["dbg3"] = nc.dram_tensor("dbg3", [D, NT], F32, kind="ExternalOutput").ap()

    def din(name, shape, dt=F32):
        io[name] = nc.dram_tensor(name, shape, dt, kind="ExternalInput").ap()
        return io[name]

    def dint(name, shape, dt=BF16):
        io[name] = nc.dram_tensor(name, shape, dt, kind="Internal").ap()
        return io[name]

    din("x_tok", [NT, D]); din("vecsP", [128, 8])
    if stop != "AG":
        din("sel", [128, 8]); din("mem", [256, D])
    for l in range(2 if stop != "AG" else 0):
        din(f"w_ml{l}", [D, 258]); din(f"w_fx{l}", [D, 386]); din(f"vecsM{l}", [128, 16]); din(f"vecs{l}", [128, NV_T])
        din(f"w_c{l}", [D, 512]); din(f"w_out{l}", [D, D]); din(f"w_q{l}", [D, 512]); din(f"w_kv{l}", [D, D]); din(f"w_o{l}", [512, D])
    if stop != "AG":
        din("w_gate0", [1, D, DFF]); din("w_up0", [1, D, DFF]); din("w_down0", [1, DFF, D])
        din("w_gate1", [8, D, DFF]); din("w_up1", [8, D, DFF]); din("w_down1", [8, DFF, D]); din("router_w", [D, 8])
    io["out"] = nc.dram_tensor("out", [NT, D], F32, kind="ExternalOutput").ap()
    for l in range(2):
        io[f"h_own{l}"] = [dint(f"h_own{l}_{a}", [256, NT]) for a in range(4)]
        io[f"hT_all{l}"] = [dint(f"hT_all{l}_{a}", [4 * 256, NT]) for a in range(4)]
        dint(f"tail{l}", [D, 32]); dint(f"tails{l}", [4 * D, 32])
        dint(f"catm{l}", [64, SEQ]); dint(f"catm_all{l}", [256, SEQ])
        io[f"catf{l}"] = [dint(f"catf{l}_{h}", [64, SEQ]) for h in range(2)]
        io[f"catf_all{l}"] = [dint(f"catf_all{l}_{h}", [256, SEQ]) for h in range(2)]
    with _ES() as st:
        c = Ctx(nc, st)
        c.setup()
        kb = c.kb
        xT = c.sb("xT", [128, 8, NT], F32)
        phase_P(c, {"x_tok": io["x_tok"], "vecsP": io["vecsP"], "h_next": io["h_own0"], "tail_next": io["tail0"]}, xT)
        for l in range(2):
            last = (l == 1)
            for a in range(4):
                kb.collective("AllGather", RG, io[f"h_own{l}"][a], io[f"hT_all{l}"][a], reads=["h_next_d"], writes=["hT_all_d"])
            kb.collective("AllGather", RG, io[f"tail{l}"], io[f"tails{l}"], reads=["tail_next_d"], writes=["tails_d"])
            c.barrier()
            if stop == "AG":
                for a in range(4):
                    for jj in range(4):
                        kb.dma("sp", io["dbg1"][jj * D + a * 256:jj * D + (a + 1) * 256, :], io[f"hT_all{l}"][a][jj * 256:(jj + 1) * 256, :], reads=["hT_all_d"])
                break
            ioM = {"hT_all": io[f"hT_all{l}"], "w_ml": io[f"w_ml{l}"], "w_fx": io[f"w_fx{l}"],
                   "vecsM": io[f"vecsM{l}"], "catm_out": io[f"catm{l}"], "catf_out": io[f"catf{l}"]}
            c.sfx = f"_{l}"
            phase_M_mlstm(c, ioM)
            phase_M_fox(c, ioM)
            kb.collective("AllGather", RG, io[f"catm{l}"], io[f"catm_all{l}"], writes=["catm_all_d"])
            for h in range(2):
                kb.collective("AllGather", RG, io[f"catf{l}"][h], io[f"catf_all{l}"][h], writes=["catf_all_d"])
            c.barrier()
            if stop == "M":
                for h in range(2):
                    for g in range(4):
                        kb.dma("sp", io["dbg2"][g * 128 + h * 64:g * 128 + (h + 1) * 64, :], io[f"catf_all{l}"][h][g * 64:(g + 1) * 64, :], reads=["catf_all_d"])
                break
            ioT = {"h_own": io[f"h_own{l}"], "tails": io[f"tails{l}"], "sel": io["sel"], "catm_all": io[f"catm_all{l}"], "catf_all": io[f"catf_all{l}"],
                   "mem": io["mem"], "vecs": io[f"vecs{l}"], "w_c": io[f"w_c{l}"], "w_out": io[f"w_out{l}"], "w_q": io[f"w_q{l}"],
                   "w_kv": io[f"w_kv{l}"], "w_o": io[f"w_o{l}"], "w_gate": io[f"w_gate{l}"], "w_up": io[f"w_up{l}"], "w_down": io[f"w_down{l}"]}
            if last:
                ioT["router_w"] = io["router_w"]; ioT["out"] = io["out"]
            else:
                ioT["h_next"] = io["h_own1"]; ioT["tail_next"] = io["tail1"]
            phase_T(c, ioT, 8 if last else 1, last, xT)
            if stop == "T":
                kb.dma("sp", io["dbg3"].rearrange("(k p) n -> p k n", p=128), xT[:], reads=[("xT", k) for k in range(8)])
                break
        c.barrier()
        kb.flush()
    return nc


def kernel_unfused(**inp):
    return _kernel_unfused(**inp)


_kernel_unfused = kernel


def kernel(**inp):
    inp = {k: np.asarray(v) for k, v in inp.items()}
    cores = list(range(8))
    x = inp["x"].astype(np.float32, copy=False)
    fm = lambda w: np.ascontiguousarray(np.asarray(w, np.float32).reshape(-1, 128).T)
    nc = _get("fused", lambda: build_fused(_STOP))
    shared = {"vecsP": fm(inp["norm_mix_w"][0]),
              "w_gate0": inp["ffn_w_gate"], "w_up0": inp["ffn_w_up"], "w_down0": inp["ffn_w_down"],
              "w_gate1": inp["moe_w_gate"][0], "w_up1": inp["moe_w_up"][0], "w_down1": inp["moe_w_down"][0],
              "router_w": inp["router_w"][0]}
    for l in range(2):
        shared[f"vecs{l}"] = vecs_T(inp, l, l == 1)
        shared[f"w_c{l}"] = np.ascontiguousarray(inp["w_in"][l][:, 1032:1544])
        shared[f"w_out{l}"] = inp["w_out"][l]; shared[f"w_q{l}"] = inp["xattn_w_q"][l]
        shared[f"w_kv{l}"] = inp["xattn_w_kv"][l]; shared[f"w_o{l}"] = inp["xattn_w_o"][l]
    perg = []
    for g in range(4):
        d = {}
        for l in range(2):
            m = inputs_M(inp, l, g)
            d[f"w_ml{l}"] = m["w_ml"]; d[f"w_fx{l}"] = m["w_fx"]; d[f"vecsM{l}"] = m["vecsM"]
        perg.append(d)
    maps = []
    for cid in cores:
        b, j = cid // 4, cid % 4
        m = dict(shared)
        m.update(perg[j])
        m["x_tok"] = np.ascontiguousarray(x[b, j * NT:(j + 1) * NT])
        m["mem"] = np.ascontiguousarray(inp["mem"][b], dtype=np.float32)
        sel = np.zeros((128, 8), np.float32)
        sel[:, j] = 1.0
        if j > 0:
            sel[:, 4 + j - 1] = 1.0
        m["sel"] = sel
        maps.append(m)
    if _STOP == "AG":
        maps = [{k: m[k] for k in ("x_tok", "vecsP")} for m in maps]
    res = run_bass_kernel_spmd(nc, maps, core_ids=cores).results
    if _STOP:
        return res
    out = np.zeros((2, SEQ, D), np.float32)
    for cid in cores:
        b, j = cid // 4, cid % 4
        out[b, j * NT:(j + 1) * NT] = res[cid]["out"]
    return out
```

```python
import numpy as np
import concourse.bass as bass
import concourse.mybir as mybir
from concourse.bass_utils import run_bass_kernel_spmd

F32 = mybir.dt.float32
BF16 = mybir.dt.bfloat16
AF = mybir.ActivationFunctionType
ALU = mybir.AluOpType
AX = mybir.AxisListType

ENGS = ("pe", "act", "dve", "pool", "sp")


class KB:
    SEM_ROLL = 2000

    def __init__(self, nc, n_dma_sems=32):
        self.nc = nc
        self.q = {e: [] for e in ENGS}
        self.cnt = {e: 0 for e in ENGS}
        self.cur_sem = {}
        self.sem_pool = []
        self.waited = {e: {} for e in ENGS}
        self.last_w = {}
        self.reads = {}
        self.n_dma_sems = n_dma_sems
        self.dma_sems = []
        self.dma_cnt = []
        self.dma_rr = 0
        self.dma_rr_sw = 0
        self._stack = None
        self.n_inst = 0

    def _new_sem(self, name):
        s = self._stack.enter_context(self.nc.semaphore(name))
        return s

    def start(self, stack):
        self._stack = stack
        for e in ENGS:
            self.cur_sem[e] = self._new_sem(f"p_{e}_0")
        for i in range(self.n_dma_sems):
            self.dma_sems.append(self._new_sem(f"dma{i}"))
            self.dma_cnt.append(0)

    def _wait(self, eng, ev):
        if ev is None:
            return
        if len(ev) == 3 and ev[2] == "pe" and eng == "pe":
            return
        sem, val = ev[0], ev[1]
        w = self.waited[eng]
        if w.get(id(sem), (None, 0))[1] >= val:
            return
        w[id(sem)] = (sem, val)
        self.q[eng].append(lambda e, sem=sem, val=val: e.wait_ge(sem, val))

    def _wait_w(self, eng, k):
        lw = self.last_w.get(k)
        if isinstance(lw, list):
            for ev in lw:
                self._wait(eng, ev)
        else:
            self._wait(eng, lw)

    def _deps(self, eng, reads, writes):
        for k in reads:
            self._wait_w(eng, k)
        for k in writes:
            self._wait_w(eng, k)
            for ev in self.reads.get(k, ()):
                self._wait(eng, ev)

    def _commit(self, ev, reads, writes, is_dma=False):
        for k in writes:
            lw = self.last_w.get(k)
            if is_dma and isinstance(lw, list) and not self.reads.get(k):
                lw.append(ev)
            else:
                self.last_w[k] = [ev] if is_dma else ev
            self.reads[k] = []
        for k in reads:
            self.reads.setdefault(k, []).append(ev)

    def op(self, eng, fn, reads=(), writes=()):
        self._deps(eng, reads, writes)
        if self.cnt[eng] >= self.SEM_ROLL:
            self.cur_sem[eng] = self._new_sem(f"p_{eng}_{self.n_inst}")
            self.cnt[eng] = 0
        self.cnt[eng] += 1
        sem = self.cur_sem[eng]
        ev = (sem, self.cnt[eng], eng)
        self.q[eng].append(lambda e, sem=sem: fn(e).then_inc(sem, 1))
        self._commit(ev, reads, writes)
        self.n_inst += 1
        return ev

    def dma(self, eng, out, in_, reads=(), writes=(), **kw):
        self._deps(eng, reads, writes)
        half = self.n_dma_sems // 2
        if eng == "pool":
            i = half + self.dma_rr_sw
            self.dma_rr_sw = (self.dma_rr_sw + 1) % (self.n_dma_sems - half)
        else:
            i = self.dma_rr
            self.dma_rr = (self.dma_rr + 1) % half
        sem = self.dma_sems[i]
        if self.dma_cnt[i] >= 2048:
            self.dma_sems[i] = self._new_sem(f"dma{i}_{self.n_inst}")
            self.dma_cnt[i] = 0
            sem = self.dma_sems[i]
        if self.dma_cnt[i] > 0:
            self._wait(eng, (sem, self.dma_cnt[i]))
        self.dma_cnt[i] += 16
        ev = (sem, self.dma_cnt[i])
        self.q[eng].append(lambda e, sem=sem: e.dma_start(out=out, in_=in_, **kw).then_inc(sem, 16))
        self._commit(ev, reads, writes, is_dma=True)
        self.n_inst += 1
        return ev

    def collective(self, kind, rg, in_ap, out_ap, reads=(), writes=()):
        eng = "pool"
        self._deps(eng, reads, writes)
        sem = self._new_sem(f"cc_{self.n_inst}")
        ev = (sem, 1)
        self.q[eng].append(lambda e: e.collective_compute(kind, ALU.bypass, replica_groups=rg, ins=[in_ap.opt()],
                                                          outs=[out_ap.opt()]).then_inc(sem, 1))
        self._commit(ev, reads, writes)
        self.n_inst += 1
        self.cc_events = getattr(self, "cc_events", []) + [ev]
        return ev

    def wait_all(self, eng, evs):
        for ev in evs:
            self._wait(eng, ev)

    def flush(self):
        nc = self.nc
        q = self.q
        with nc.Block() as block:
            @block.tensor
            def _(e):
                for f in q["pe"]:
                    f(e)

            @block.scalar
            def _(e):
                for f in q["act"]:
                    f(e)

            @block.vector
            def _(e):
                for f in q["dve"]:
                    f(e)

            @block.gpsimd
            def _(e):
                for f in q["pool"]:
                    f(e)

            @block.sync
            def _(e):
                for f in q["sp"]:
                    f(e)
        self.q = {e: [] for e in ENGS}


D = 1024
NT = 2048
TT = 512
NTT = NT // TT
DFF = 2816
NF = DFF // 128
SEQ = 8192
EPS = 1e-6
NV_T = 100


class Ctx:
    def __init__(self, nc, st):
        self.nc = nc
        self.st = st
        self.kb = KB(nc)
        self.kb.start(st)
        self.ps_rr = 0
        self.uid = 0

    def sb(self, name, shape, dt, st=None):
        self.uid += 1
        return (st or self.st).enter_context(self.nc.sbuf_tensor(f"{name}_u{self.uid}", shape, dt))

    def barrier(self):
        kb = self.kb
        evs = []
        for e in ENGS:
            if kb.cnt[e] > 0:
                evs.append((kb.cur_sem[e], kb.cnt[e]))
        for i, s in enumerate(kb.dma_sems):
            if kb.dma_cnt[i] > 0:
                evs.append((s, kb.dma_cnt[i]))
        evs += getattr(kb, "cc_events", [])
        kb.cc_events = []
        for e in ENGS:
            for ev in evs:
                kb._wait(e, ev)
        kb.last_w = {}
        kb.reads = {}

    def setup(self):
        nc, kb = self.nc, self.kb
        self.ident_f = self.sb("ident_f", [128, 128], F32)
        self.ident_b = self.sb("ident_b", [128, 128], BF16)
        self.ones_b = self.sb("ones_b", [128, 128], BF16)
        self.ones_f = self.sb("ones_f", [128, 128], F32)
        self.psb = [self.st.enter_context(nc.psum_tensor(f"psb{i}", [128, 512], F32)) for i in range(8)]
        idf, idb, ob, of = self.ident_f, self.ident_b, self.ones_b, self.ones_f
        kb.op("pool", lambda e: e.memset(idf[:], 0.0), writes=["ident_f"])
        kb.op("pool", lambda e: e.affine_select(out=idf[:], in_=idf[:], pattern=[[-1, 128]],
                                                compare_op=ALU.not_equal, fill=1.0, base=0,
                                                channel_multiplier=1),
              reads=["ident_f"], writes=["ident_f"])
        kb.op("pool", lambda e: e.tensor_copy(out=idb[:], in_=idf[:]), reads=["ident_f"], writes=["ident_b"])
        kb.op("pool", lambda e: e.memset(ob[:], 1.0), writes=["ones_b"])
        kb.op("pool", lambda e: e.memset(of[:], 1.0), writes=["ones_f"])

    def ps(self):
        rot = getattr(self, "rot", None) or list(range(8))
        i = rot[self.ps_rr % len(rot)]
        self.ps_rr += 1
        return self.psb[i], f"psb{i}"


def h_store(c, dst, hT, c0, n, reads, writes=()):
    if isinstance(dst, list):
        for a, d in enumerate(dst):
            c.kb.dma("sp", d[:, c0:c0 + n].rearrange("(k p) n -> p k n", p=128), hT[:, 2 * a:2 * a + 2, 0:n], reads=reads, writes=writes)
    else:
        c.kb.dma("sp", dst[:, c0:c0 + n].rearrange("(k p) n -> p k n", p=128), hT[:, :, 0:n], reads=reads, writes=writes)


def h_load(c, src, hT, c0, n, writes, j=None):
    if isinstance(src, list):
        for a, d in enumerate(src):
            v = d if j is None else d.rearrange("(j r) n -> j r n", j=4)[j]
            c.kb.dma("sp", hT[:, 2 * a:2 * a + 2, 0:n], v[:, c0:c0 + n].rearrange("(k p) n -> p k n", p=128), writes=writes)
    else:
        v = src if j is None else src[j]
        c.kb.dma("sp", hT[:, :, 0:n], v[:, c0:c0 + n].rearrange("(k p) n -> p k n", p=128), writes=writes)


def mm(c, out, lhsT, rhs, start, stop, reads, writes):
    return c.kb.op("pe", lambda e: e.matmul(out, lhsT=lhsT, rhs=rhs, start=start, stop=stop),
                   reads=reads, writes=writes)


def rmsnorm_tile(c, xT, xkey, t0, n, wv, tmp, out_bf, okey, out_f=None):
    kb = c.kb
    sq, rstd = tmp["sq"], tmp["rstd"]
    for k in range(8):
        kb.op("act", lambda e, k=k: e.activation(out=sq[:, k, 0:n], in_=xT[:, k, t0:t0 + n], func=AF.Square),
              reads=[(xkey, k)], writes=[("sq", k)])
    p, pk = c.ps()
    for k in range(8):
        mm(c, p[:, 0:n], c.ones_b[:], sq[:, k, 0:n], k == 0, k == 7, ["ones_b", ("sq", k)], [pk])
    kb.op("act", lambda e: e.activation(out=rstd[:, 0:n], in_=p[:, 0:n], func=AF.Sqrt, scale=1.0 / D, bias=tmp["eps"][:, 0:1]),
          reads=[pk, "eps"], writes=["rstd"])
    kb.op("dve", lambda e: e.reciprocal(out=rstd[:, 0:n], in_=rstd[:, 0:n]), reads=["rstd"], writes=["rstd"])
    for k in range(8):
        kb.op("dve", lambda e, k=k: e.scalar_tensor_tensor(out=out_bf[:, k, 0:n], in0=xT[:, k, t0:t0 + n],
                                                           scalar=wv[:, k:k + 1], in1=rstd[:, 0:n],
                                                           op0=ALU.mult, op1=ALU.mult),
              reads=[(xkey, k), "rstd", "vecs"], writes=[(okey, k)])
        if out_f is not None:
            kb.op("dve", lambda e, k=k: e.scalar_tensor_tensor(out=out_f[:, k, 0:n], in0=xT[:, k, t0:t0 + n],
                                                                scalar=wv[:, k:k + 1], in1=rstd[:, 0:n],
                                                                op0=ALU.mult, op1=ALU.mult),
                  reads=[(xkey, k), "rstd", "vecs"], writes=[(okey + "_f", k)])


def phase_T(c, io, E, last, xT):
    nc, kb = c.nc, c.kb
    from contextlib import ExitStack
    vec_st = ExitStack()
    vecs = c.sb("vecsT", [128, NV_T], F32, vec_st)
    eps_t = c.sb("eps_t", [128, 1], F32, vec_st)
    sq = c.sb("sq", [128, 8, TT], BF16, vec_st)
    rstd = c.sb("rstd", [128, TT], F32, vec_st)
    tmp = {"sq": sq, "rstd": rstd, "eps": eps_t}
    kb.dma("sp", vecs[:], io["vecs"], writes=["vecs"])
    kb.op("pool", lambda e: e.memset(eps_t[:], EPS), writes=["eps"])
    V_XA, V_MEM, V_FFN, V_NEXT, V_CB, V_LNW, V_LNB, V_CW = 0, 8, 16, 24, 32, 34, 36, 38

    with ExitStack() as s1:
        hT = c.sb("hT", [128, 8, TT], BF16, s1)
        gluT = c.sb("gluT", [128, 2, 32 + NT], BF16, s1)
        hcT = c.sb("hcT", [128, 2, NT], BF16, s1)
        wc = c.sb("wc", [128, 8, 512], BF16, s1)
        dg = c.sb("dg", [128, 62, 128], BF16, s1)
        sig = c.sb("sig", [128, 2, TT], F32, s1)
        hcv = c.sb("hcv", [128, 2, TT], F32, s1)
        hsq = c.sb("hsq", [128, 2, TT], F32, s1)
        mean = c.sb("mean", [128, TT], F32, s1)
        var = c.sb("var", [128, TT], F32, s1)
        wo_m = c.sb("wo_m", [64, 4, D], BF16, s1)
        wo_c = c.sb("wo_c", [128, 2, D], BF16, s1)
        wo_f = c.sb("wo_f", [128, 4, D], BF16, s1)
        mT = c.sb("mT", [64, 4, TT], BF16, s1)
        fT = c.sb("fT", [128, 4, TT], BF16, s1)
        if "sel" in io:
            halo4 = c.sb("halo4", [128, 4, 8, 32], BF16, s1)
            selt = c.sb("selt", [128, 8], F32, s1)
            m4 = [c.sb(f"m4_{i}", [64, 4, TT], BF16, s1) for i in range(2)]
            f4 = [c.sb(f"f4_{i}", [128, 4, TT], BF16, s1) for i in range(2)]
            kb.dma("sp", selt[:], io["sel"], writes=["selt"])
        kb.dma("pool", wc[:], io["w_c"].rearrange("(k p) n -> p k n", p=128), writes=["wc"])
        kb.dma("pool", wo_m[:], io["w_out"][0:256, :].rearrange("(g p) n -> p g n", p=64), writes=["wo_m"])
        kb.dma("pool", wo_c[:], io["w_out"][256:512, :].rearrange("(g p) n -> p g n", p=128), writes=["wo_c"])
        kb.dma("pool", wo_f[:], io["w_out"][512:1024, :].rearrange("(g p) n -> p g n", p=128), writes=["wo_f"])
        for j in range(31):
            for ch in range(2):
                kb.op("dve", lambda e, j=j, ch=ch: e.tensor_scalar(
                    out=dg[:, j * 2 + ch, :], in0=c.ident_b[:], scalar1=vecs[:, V_CW + j * 2 + ch:V_CW + j * 2 + ch + 1],
                    scalar2=None, op0=ALU.mult), reads=["ident_b", "vecs"], writes=[("dg", j, ch)])
        tiles = [("halo", 0, 32)] + [("own", t * TT, TT) for t in range(NTT)]
        for kind, t0, n in tiles:
            if kind == "halo" and "sel" in io:
                tl = io["tails"].rearrange("(j k p) n -> j p k n", j=4, p=128)
                for jj in range(4):
                    kb.dma("sp", halo4[:, jj, :, :], tl[jj], writes=[("halo4", jj)])
                kb.op("dve", lambda e: e.tensor_scalar(out=hT[:, :, 0:32], in0=halo4[:, 0, :, :], scalar1=selt[:, 4:5], scalar2=None, op0=ALU.mult),
                      reads=[("halo4", 0), "selt"], writes=[("hT", k) for k in range(8)])
                for jj in range(1, 4):
                    kb.op("dve", lambda e, jj=jj: e.scalar_tensor_tensor(out=hT[:, :, 0:32], in0=halo4[:, jj, :, :], scalar=selt[:, 4 + jj:5 + jj], in1=hT[:, :, 0:32],
                                                                         op0=ALU.mult, op1=ALU.add),
                          reads=[("halo4", jj), "selt"] + [("hT", k) for k in range(8)], writes=[("hT", k) for k in range(8)])
                g0 = 0
            elif kind == "halo":
                kb.dma("sp", hT[:, :, 0:n], io["h_halo"].rearrange("(k p) n -> p k n", p=128),
                       writes=[("hT", k) for k in range(8)])
                g0 = 0
            else:
                h_load(c, io["h_own"], hT, t0, n, [("hT", k) for k in range(8)])
                g0 = 32 + t0
            for ch in range(2):
                pa, pak = c.ps()
                pg, pgk = c.ps()
                for k in range(8):
                    mm(c, pa[:, 0:n], wc[:, k, ch * 128:(ch + 1) * 128], hT[:, k, 0:n], k == 0, k == 7,
                       ["wc", ("hT", k)], [pak])
                for k in range(8):
                    mm(c, pg[:, 0:n], wc[:, k, 256 + ch * 128:256 + (ch + 1) * 128], hT[:, k, 0:n], k == 0, k == 7,
                       ["wc", ("hT", k)], [pgk])
                kb.op("act", lambda e, ch=ch, pg=pg, n=n: e.activation(out=sig[:, ch, 0:n], in_=pg[:, 0:n], func=AF.Sigmoid),
                      reads=[pgk], writes=[("sig", ch)])
                kb.op("dve", lambda e, ch=ch, pa=pa, n=n, g0=g0: e.tensor_tensor(
                    out=gluT[:, ch, g0:g0 + n], in0=pa[:, 0:n], in1=sig[:, ch, 0:n], op=ALU.mult),
                    reads=[pak, ("sig", ch)], writes=[("glu", ch, g0 // TT), ("glu", ch, (g0 + n - 1) // TT)])
        for t in range(NTT):
            t0 = t * TT
            gk = lambda ch: [("glu", ch, (32 + t0 - 30) // TT), ("glu", ch, (32 + t0 + TT - 1) // TT)]
            for ch in range(2):
                p, pk = c.ps()
                for j in range(31):
                    o = 32 + t0 - 30 + j
                    mm(c, p[:, :], dg[:, j * 2 + ch, :], gluT[:, ch, o:o + TT], j == 0, j == 30,
                       [("dg", j, ch)] + gk(ch), [pk])
                kb.op("act", lambda e, ch=ch, p=p: e.activation(out=hcv[:, ch, :], in_=p[:, :], func=AF.Identity,
                                                                bias=vecs[:, V_CB + ch:V_CB + ch + 1]),
                      reads=[pk, "vecs"], writes=[("hcv", ch)])
                kb.op("act", lambda e, ch=ch: e.activation(out=hsq[:, ch, :], in_=hcv[:, ch, :], func=AF.Square),
                      reads=[("hcv", ch)], writes=[("hsq", ch)])
            p1, p1k = c.ps()
            p2, p2k = c.ps()
            for ch in range(2):
                mm(c, p1[:, :], c.ones_f[:], hcv[:, ch, :], ch == 0, ch == 1, ["ones_f", ("hcv", ch)], [p1k])
            for ch in range(2):
                mm(c, p2[:, :], c.ones_f[:], hsq[:, ch, :], ch == 0, ch == 1, ["ones_f", ("hsq", ch)], [p2k])
            kb.op("dve", lambda e, p1=p1: e.tensor_scalar(out=mean[:], in0=p1[:, :], scalar1=1.0 / 256, scalar2=None, op0=ALU.mult),
                  reads=[p1k], writes=["mean"])
            kb.op("dve", lambda e: e.tensor_tensor(out=var[:], in0=mean[:], in1=mean[:], op=ALU.mult),
                  reads=["mean"], writes=["var"])
            kb.op("dve", lambda e, p2=p2: e.scalar_tensor_tensor(out=var[:], in0=p2[:, :], scalar=1.0 / 256, in1=var[:],
                                                                 op0=ALU.mult, op1=ALU.subtract),
                  reads=[p2k, "var"], writes=["var"])
            kb.op("act", lambda e: e.activation(out=var[:], in_=var[:], func=AF.Sqrt, bias=eps_t[:, 0:1]),
                  reads=["var", "eps"], writes=["var"])
            kb.op("dve", lambda e: e.reciprocal(out=var[:], in_=var[:]), reads=["var"], writes=["var"])
            for ch in range(2):
                kb.op("dve", lambda e, ch=ch: e.tensor_tensor(out=hcv[:, ch, :], in0=hcv[:, ch, :], in1=mean[:], op=ALU.subtract),
                      reads=[("hcv", ch), "mean"], writes=[("hcv", ch)])
                kb.op("dve", lambda e, ch=ch: e.tensor_tensor(out=hcv[:, ch, :], in0=hcv[:, ch, :], in1=var[:], op=ALU.mult),
                      reads=[("hcv", ch), "var"], writes=[("hcv", ch)])
                kb.op("dve", lambda e, ch=ch: e.tensor_scalar(out=hcv[:, ch, :], in0=hcv[:, ch, :],
                                                              scalar1=vecs[:, V_LNW + ch:V_LNW + ch + 1],
                                                              scalar2=vecs[:, V_LNB + ch:V_LNB + ch + 1],
                                                              op0=ALU.mult, op1=ALU.add),
                      reads=[("hcv", ch), "vecs"], writes=[("hcv", ch)])
                kb.op("act", lambda e, ch=ch, t0=t0: e.activation(out=hcT[:, ch, t0:t0 + TT], in_=hcv[:, ch, :], func=AF.Silu),
                      reads=[("hcv", ch)], writes=[("hcT", ch, t)])
            if "sel" in io:
                cm = io["catm_all"].rearrange("(g p) n -> p g n", p=64)
                cf = [a.rearrange("(g p) n -> p g n", p=64) for a in io["catf_all"]]
                for jj in range(4):
                    for dst, stg, src, nm, npart in ((mT, m4, cm, "m4", 64), (fT, f4, cf, "f4", 128)):
                        dk = "mT" if nm == "m4" else "fT"
                        sg = stg[jj % 2]
                        sk = f"{nm}_{jj % 2}"
                        if nm == "m4":
                            kb.dma("sp", sg[:], src[:, :, jj * NT + t0:jj * NT + t0 + TT], writes=[sk])
                        else:
                            for hh in range(2):
                                kb.dma("sp", sg[hh * 64:(hh + 1) * 64, :, :], src[hh][:, :, jj * NT + t0:jj * NT + t0 + TT], writes=[sk])
                        if jj == 0:
                            kb.op("dve", lambda e, dst=dst, sg=sg, npart=npart: e.tensor_scalar(out=dst[:], in0=sg[:], scalar1=selt[0:npart, 0:1], scalar2=None, op0=ALU.mult),
                                  reads=[sk, "selt"], writes=[dk])
                        else:
                            kb.op("dve", lambda e, dst=dst, sg=sg, jj=jj, npart=npart: e.scalar_tensor_tensor(out=dst[:], in0=sg[:], scalar=selt[0:npart, jj:jj + 1], in1=dst[:],
                                                                                                          op0=ALU.mult, op1=ALU.add),
                                  reads=[sk, "selt", dk], writes=[dk])
            else:
                kb.dma("sp", mT[:], io["catm"][:, :, t0:t0 + TT].rearrange("g p n -> p g n"), writes=["mT"])
                kb.dma("sp", fT[:], io["catf"][:, :, t0:t0 + TT].rearrange("g p n -> p g n"), writes=["fT"])
            for d in range(8):
                p, pk = c.ps()
                ds = slice(d * 128, (d + 1) * 128)
                for g in range(4):
                    mm(c, p[:, :], wo_m[:, g, ds], mT[:, g, :], g == 0, False, ["wo_m", "mT"], [pk])
                for ch in range(2):
                    mm(c, p[:, :], wo_c[:, ch, ds], hcT[:, ch, t0:t0 + TT], False, False, ["wo_c", ("hcT", ch, t)], [pk])
                for g in range(4):
                    mm(c, p[:, :], wo_f[:, g, ds], fT[:, g, :], False, g == 3, ["wo_f", "fT"], [pk])
                kb.op("dve", lambda e, d=d, p=p, t0=t0: e.tensor_tensor(out=xT[:, d, t0:t0 + TT], in0=xT[:, d, t0:t0 + TT],
                                                                        in1=p[:, :], op=ALU.add),
                      reads=[pk, ("xT", d)], writes=[("xT", d)])
    c.barrier()
    if io.get("dbg_stage") == 1:
        vec_st.close()
        return

    with ExitStack() as s2:
        hT = c.sb("hT", [128, 8, TT], BF16, s2)
        memt = c.sb("memt", [128, 2, D], F32, s2)
        mss = c.sb("mss", [128, 2], F32, s2)
        junk = c.sb("junk", [128, D], F32, s2)
        memnT = c.sb("memnT", [128, 8, 256], BF16, s2)
        wkv = c.sb("wkv", [128, 8, D], BF16, s2)
        wq = c.sb("wq", [128, 8, 512], BF16, s2)
        wo = c.sb("wo", [128, 4, D], BF16, s2)
        kT = c.sb("kT", [128, 4, 256], BF16, s2)
        Vt = c.sb("Vt", [128, 2, 512], BF16, s2)
        qT = c.sb("qT", [128, 4, TT], BF16, s2)
        pT = c.sb("pT", [128, 8, TT], BF16, s2)
        rden = c.sb("rden", [128, TT], F32, s2)
        oT = c.sb("oT", [128, 4, TT], BF16, s2)
        kb.dma("sp", memt[:], io["mem"].rearrange("(t p) d -> p t d", p=128), writes=["memt"])
        kb.dma("pool", wkv[:], io["w_kv"].rearrange("(k p) n -> p k n", p=128), writes=["wkv"])
        kb.dma("pool", wq[:], io["w_q"].rearrange("(k p) n -> p k n", p=128), writes=["wq"])
        kb.dma("pool", wo[:], io["w_o"].rearrange("(k p) n -> p k n", p=128), writes=["wo"])
        for mt in range(2):
            kb.op("act", lambda e, mt=mt: e.activation(out=junk[:], in_=memt[:, mt, :], func=AF.Square,
                                                       accum_out=mss[:, mt:mt + 1]),
                  reads=["memt"], writes=["junk", ("mss", mt)])
            kb.op("act", lambda e, mt=mt: e.activation(out=mss[:, mt:mt + 1], in_=mss[:, mt:mt + 1], func=AF.Sqrt,
                                                       scale=1.0 / D, bias=eps_t[:, 0:1]),
                  reads=[("mss", mt), "eps"], writes=[("mss", mt)])
            kb.op("dve", lambda e, mt=mt: e.reciprocal(out=mss[:, mt:mt + 1], in_=mss[:, mt:mt + 1]),
                  reads=[("mss", mt)], writes=[("mss", mt)])
            kb.op("dve", lambda e, mt=mt: e.tensor_scalar(out=memt[:, mt, :], in0=memt[:, mt, :], scalar1=mss[:, mt:mt + 1],
                                                          scalar2=None, op0=ALU.mult),
                  reads=["memt", ("mss", mt)], writes=["memt"])
        for k in range(8):
            p, pk = c.ps()
            for mt in range(2):
                kb.op("pe", lambda e, k=k, mt=mt, p=p: e.transpose(out=p[:, mt * 128:(mt + 1) * 128],
                                                                   in_=memt[:, mt, k * 128:(k + 1) * 128], identity=c.ident_f[:]),
                      reads=["memt", "ident_f"], writes=[pk])
            kb.op("dve", lambda e, k=k, p=p: e.tensor_scalar(out=memnT[:, k, :], in0=p[:, 0:256],
                                                             scalar1=vecs[:, V_MEM + k:V_MEM + k + 1], scalar2=None, op0=ALU.mult),
                  reads=[pk, "vecs"], writes=[("memnT", k)])
        for h in range(4):
            p, pk = c.ps()
            for k in range(8):
                mm(c, p[:, 0:256], wkv[:, k, h * 128:(h + 1) * 128], memnT[:, k, :], k == 0, k == 7, ["wkv", ("memnT", k)], [pk])
            kb.op("act", lambda e, h=h, p=p: e.activation(out=kT[:, h, :], in_=p[:, 0:256], func=AF.Copy),
                  reads=[pk], writes=[("kT", h)])
        for mt in range(2):
            p, pk = c.ps()
            for k in range(8):
                mm(c, p[:, :], memnT[:, k, mt * 128:(mt + 1) * 128], wkv[:, k, 512:1024], k == 0, k == 7, ["wkv", ("memnT", k)], [pk])
            kb.op("act", lambda e, mt=mt, p=p: e.activation(out=Vt[:, mt, :], in_=p[:, :], func=AF.Copy),
                  reads=[pk], writes=[("Vt", mt)])
        sc = 128 ** -0.5
        for t in range(NTT):
            t0 = t * TT
            rmsnorm_tile(c, xT, "xT", t0, TT, vecs[:, V_XA:V_XA + 8], tmp, hT, "hT")
            for h in range(4):
                p, pk = c.ps()
                for k in range(8):
                    mm(c, p[:, :], wq[:, k, h * 128:(h + 1) * 128], hT[:, k, :], k == 0, k == 7, ["wq", ("hT", k)], [pk])
                kb.op("act", lambda e, h=h, p=p: e.activation(out=qT[:, h, :], in_=p[:, :], func=AF.Copy),
                      reads=[pk], writes=[("qT", h)])
            for h in range(4):
                for mt in range(2):
                    p, pk = c.ps()
                    mm(c, p[:, :], kT[:, h, mt * 128:(mt + 1) * 128], qT[:, h, :], True, True, [("kT", h), ("qT", h)], [pk])
                    kb.op("act", lambda e, h=h, mt=mt, p=p: e.activation(out=pT[:, h * 2 + mt, :], in_=p[:, :], func=AF.Exp, scale=sc),
                          reads=[pk], writes=[("pT", h, mt)])
                pd, pdk = c.ps()
                for mt in range(2):
                    mm(c, pd[:, :], c.ones_b[:], pT[:, h * 2 + mt, :], mt == 0, mt == 1, ["ones_b", ("pT", h, mt)], [pdk])
                kb.op("dve", lambda e, pd=pd: e.reciprocal(out=rden[:], in_=pd[:, :]), reads=[pdk], writes=["rden"])
                po, pok = c.ps()
                for mt in range(2):
                    mm(c, po[:, :], Vt[:, mt, h * 128:(h + 1) * 128], pT[:, h * 2 + mt, :], mt == 0, mt == 1,
                       [("Vt", mt), ("pT", h, mt)], [pok])
                kb.op("dve", lambda e, h=h, po=po: e.tensor_tensor(out=oT[:, h, :], in0=po[:, :], in1=rden[:], op=ALU.mult),
                      reads=[pok, "rden"], writes=[("oT", h)])
            for d in range(8):
                p, pk = c.ps()
                for h in range(4):
                    mm(c, p[:, :], wo[:, h, d * 128:(d + 1) * 128], oT[:, h, :], h == 0, h == 3, ["wo", ("oT", h)], [pk])
                kb.op("dve", lambda e, d=d, p=p, t0=t0: e.tensor_tensor(out=xT[:, d, t0:t0 + TT], in0=xT[:, d, t0:t0 + TT],
                                                                        in1=p[:, :], op=ALU.add),
                      reads=[pk, ("xT", d)], writes=[("xT", d)])
    c.barrier()
    if io.get("dbg_stage") == 2:
        vec_st.close()
        return

    with ExitStack() as s3:
        hTall = c.sb("hTall", [128, 8, NT], BF16, s3)
        actT = c.sb("actT", [128, 8, NT], BF16, s3)
        wgu = [c.sb(f"wgu{i}", [128, 8, 256], BF16, s3) for i in range(3)]
        wdr = [c.sb(f"wdr{i}", [128, D], BF16, s3) for i in range(11)]
        sil = [c.sb(f"sil{i}", [128, TT], BF16, s3) for i in range(2)]
        if E > 1:
            wr = c.sb("wr", [128, 8, 8], F32, s3)
            lg = c.sb("lg", [128, 4, 8], F32, s3)
            top8 = c.sb("top8", [128, 4, 8], F32, s3)
            gts = c.sb("gts", [128, 16, 8], F32, s3)
            gsc = c.sb("gsc", [128, 4, 4], F32, s3)
            dgate = c.sb("dgate", [128, 128], F32, s3)
            gB = [c.sb(f"gB{i}", [128, NT], BF16, s3) for i in range(2)]
            ytmps = [c.sb(f"ytmp{i}", [128, TT], F32, s3) for i in range(2)]
            kb.dma("sp", wr[:], io["router_w"].rearrange("(k p) n -> p k n", p=128), writes=["wr"])
        with ExitStack() as s3a:
            hF = c.sb("hF", [128, 8, TT], F32, s3a) if E > 1 else None
            for t in range(NTT):
                t0 = t * TT
                rmsnorm_tile(c, xT, "xT", t0, TT, vecs[:, V_FFN:V_FFN + 8], tmp, hTall[:, :, t0:t0 + TT], f"hA{t}", out_f=hF)
                if E > 1:
                    for s in range(4):
                        p, pk = c.ps()
                        for k in range(8):
                            mm(c, p[:, 0:8], hF[:, k, s * 128:(s + 1) * 128], wr[:, k, :], k == 0, k == 7, [(f"hA{t}_f", k), "wr"], [pk])
                        kb.op("dve", lambda e, s=s, p=p: e.tensor_copy(out=lg[:, s, :], in_=p[:, 0:8]), reads=[pk], writes=[("lg", s)])
                        kb.op("dve", lambda e, s=s: e.max(out=top8[:, s, :], in_=lg[:, s, :]), reads=[("lg", s)], writes=[("top8", s)])
                        kb.op("dve", lambda e, s=s: e.tensor_scalar(out=gsc[:, s, 0:1], in0=top8[:, s, 0:1], scalar1=-1.0, scalar2=None, op0=ALU.mult),
                              reads=[("top8", s)], writes=[("gsc", s, 0)])
                        kb.op("act", lambda e, s=s: e.activation(out=gsc[:, s, 1:2], in_=top8[:, s, 1:2], func=AF.Exp, bias=gsc[:, s, 0:1]),
                              reads=[("top8", s), ("gsc", s, 0)], writes=[("gsc", s, 1)])
                        kb.op("dve", lambda e, s=s: e.tensor_scalar(out=gsc[:, s, 1:2], in0=gsc[:, s, 1:2], scalar1=1.0, scalar2=None, op0=ALU.add),
                              reads=[("gsc", s, 1)], writes=[("gsc", s, 1)])
                        kb.op("dve", lambda e, s=s: e.reciprocal(out=gsc[:, s, 1:2], in_=gsc[:, s, 1:2]),
                              reads=[("gsc", s, 1)], writes=[("gsc", s, 1)])
                        gi = t * 4 + s
                        kb.op("act", lambda e, s=s, gi=gi: e.activation(out=gts[:, gi, :], in_=lg[:, s, :], func=AF.Exp, bias=gsc[:, s, 0:1]),
                              reads=[("lg", s), ("gsc", s, 0)], writes=[("gts", gi)])
                        kb.op("dve", lambda e, s=s: e.tensor_scalar(out=lg[:, s, :], in0=lg[:, s, :], scalar1=top8[:, s, 1:2], scalar2=None, op0=ALU.is_ge),
                              reads=[("lg", s), ("top8", s)], writes=[("lg", s)])
                        kb.op("dve", lambda e, s=s, gi=gi: e.scalar_tensor_tensor(out=gts[:, gi, :], in0=gts[:, gi, :], scalar=gsc[:, s, 1:2], in1=lg[:, s, :],
                                                                                  op0=ALU.mult, op1=ALU.mult),
                              reads=[("gts", gi), ("gsc", s, 1), ("lg", s)], writes=[("gts", gi)])
        hkeys = lambda t: [(f"hA{t}", k) for k in range(8)]
        groups = [(0, 8), (8, 16), (16, 22)]
        wgu_i = 0
        wd_i = 0
        sil_i = 0
        for ex in range(E):
            if E > 1:
                gb = gB[ex % 2]
                gbk = f"gB{ex % 2}"
                for gi in range(16):
                    kb.op("dve", lambda e, gi=gi, ex=ex: e.tensor_scalar(out=dgate[:], in0=c.ident_f[:], scalar1=gts[:, gi, ex:ex + 1],
                                                                         scalar2=None, op0=ALU.mult),
                          reads=["ident_f", ("gts", gi)], writes=["dgate"])
                    p, pk = c.ps()
                    mm(c, p[:, 0:128], c.ones_f[:], dgate[:], True, True, ["ones_f", "dgate"], [pk])
                    kb.op("act", lambda e, gi=gi, p=p, gb=gb: e.activation(out=gb[:, gi * 128:(gi + 1) * 128], in_=p[:, 0:128], func=AF.Copy),
                          reads=[pk], writes=[(gbk, gi // 4)])
            for fa, fb in groups:
                nfg = fb - fa
                for f0 in range(fa, fb, 2):
                    nf = min(2, fb - f0)
                    sg, su = wgu[wgu_i % 3], wgu[(wgu_i + 1) % 3]
                    sgk, suk = f"wgu{wgu_i % 3}", f"wgu{(wgu_i + 1) % 3}"
                    wgu_i += 2
                    kb.dma("pool", sg[:, :, 0:nf * 128], io["w_gate"][ex][:, f0 * 128:(f0 + nf) * 128].rearrange("(k p) n -> p k n", p=128), writes=[sgk])
                    kb.dma("pool", su[:, :, 0:nf * 128], io["w_up"][ex][:, f0 * 128:(f0 + nf) * 128].rearrange("(k p) n -> p k n", p=128), writes=[suk])
                    for fi in range(nf):
                        fl = f0 + fi - fa
                        for t in range(NTT):
                            ts_ = slice(t * TT, (t + 1) * TT)
                            pg, pgk = c.ps()
                            pu, puk = c.ps()
                            for k in range(8):
                                mm(c, pg[:, :], sg[:, k, fi * 128:(fi + 1) * 128], hTall[:, k, ts_], k == 0, k == 7, [sgk, (f"hA{t}", k)], [pgk])
                            for k in range(8):
                                mm(c, pu[:, :], su[:, k, fi * 128:(fi + 1) * 128], hTall[:, k, ts_], k == 0, k == 7, [suk, (f"hA{t}", k)], [puk])
                            sl = sil[sil_i % 2]
                            slk = f"sil{sil_i % 2}"
                            sil_i += 1
                            kb.op("act", lambda e, pg=pg, sl=sl: e.activation(out=sl[:], in_=pg[:, :], func=AF.Silu), reads=[pgk], writes=[slk])
                            kb.op("dve", lambda e, pu=pu, sl=sl, fl=fl, ts_=ts_: e.tensor_tensor(out=actT[:, fl, ts_], in0=pu[:, :], in1=sl[:], op=ALU.mult),
                                  reads=[puk, slk], writes=[("actT", fl, t)])
                slots = []
                for fl in range(nfg):
                    f = fa + fl
                    wd = wdr[wd_i % 11]
                    wdk = f"wdr{wd_i % 11}"
                    wd_i += 1
                    kb.dma("pool", wd[:], io["w_down"][ex][f * 128:(f + 1) * 128, :], writes=[wdk])
                    slots.append((wd, wdk))
                for t in range(NTT):
                    ts_ = slice(t * TT, (t + 1) * TT)
                    for d in range(8):
                        p, pk = c.ps()
                        for fl in range(nfg):
                            wd, wdk = slots[fl]
                            mm(c, p[:, :], wd[:, d * 128:(d + 1) * 128], actT[:, fl, ts_], fl == 0, fl == nfg - 1, [wdk, ("actT", fl, t)], [pk])
                        if E > 1:
                            ytmp = ytmps[d % 2]
                            ytk = f"ytmp{d % 2}"
                            kb.op("dve", lambda e, p=p, gb=gb, ytmp=ytmp, ts_=ts_: e.tensor_tensor(out=ytmp[:], in0=p[:, :], in1=gb[:, ts_], op=ALU.mult),
                                  reads=[pk, (gbk, t)], writes=[ytk])
                            kb.op("pool", lambda e, d=d, ts_=ts_, ytmp=ytmp: e.tensor_tensor(out=xT[:, d, ts_], in0=xT[:, d, ts_], in1=ytmp[:], op=ALU.add),
                                  reads=[ytk, ("xT", d)], writes=[("xT", d)])
                        else:
                            kb.op("dve", lambda e, d=d, p=p, ts_=ts_: e.tensor_tensor(out=xT[:, d, ts_], in0=xT[:, d, ts_], in1=p[:, :], op=ALU.add),
                                  reads=[pk, ("xT", d)], writes=[("xT", d)])
    c.barrier()

    with ExitStack() as s4:
        hT = c.sb("hT", [128, 8, TT], BF16, s4)
        if not last:
            for t in range(NTT):
                t0 = t * TT
                rmsnorm_tile(c, xT, "xT", t0, TT, vecs[:, V_NEXT:V_NEXT + 8], tmp, hT, "hT")
                h_store(c, io["h_next"], hT, t0, TT, [("hT", k) for k in range(8)], ["h_next_d"])
                if t == NTT - 1 and "tail_next" in io:
                    kb.dma("sp", io["tail_next"].rearrange("(k p) n -> p k n", p=128), hT[:, :, TT - 32:TT],
                           reads=[("hT", k) for k in range(8)], writes=["tail_next_d"])
        else:
            hF2 = c.sb("hF2", [128, 8, TT], F32, s4)
            otm = c.sb("otm", [128, 4, D], F32, s4)
            for t in range(NTT):
                t0 = t * TT
                rmsnorm_tile(c, xT, "xT", t0, TT, vecs[:, V_NEXT:V_NEXT + 8], tmp, hT, "hT", out_f=hF2)
                for s in range(4):
                    for kk in range(2):
                        p, pk = c.ps()
                        for k4 in range(4):
                            k = kk * 4 + k4
                            kb.op("pe", lambda e, k=k, k4=k4, s=s, p=p: e.transpose(out=p[:, k4 * 128:(k4 + 1) * 128],
                                                                                    in_=hF2[:, k, s * 128:(s + 1) * 128], identity=c.ident_f[:]),
                                  reads=[("hT_f", k), "ident_f"], writes=[pk])
                        kb.op("act", lambda e, s=s, kk=kk, p=p: e.activation(out=otm[:, s, kk * 512:(kk + 1) * 512], in_=p[:, :], func=AF.Copy),
                              reads=[pk], writes=[("otm", s)])
                kb.dma("sp", io["out"][t0:t0 + TT, :].rearrange("(s p) d -> p s d", p=128), otm[:],
                       reads=[("otm", s) for s in range(4)])
    c.barrier()
    vec_st.close()


from contextlib import ExitStack as _ES
import ml_dtypes as _mld

NPBF = _mld.bfloat16


def build_T(E, last, dbg_stage=0):
    nc = bass.Bass("TRN2", target_bir_lowering=False)
    io = {}

    def din(name, shape, dt=F32):
        io[name] = nc.dram_tensor(name, shape, dt, kind="ExternalInput").ap()

    def dout(name, shape, dt=F32):
        io[name] = nc.dram_tensor(name, shape, dt, kind="ExternalOutput").ap()

    din("xT_in", [D, NT]); din("h_own", [D, NT], BF16); din("h_halo", [D, 32], BF16)
    din("catm", [4, 64, NT], BF16); din("catf", [4, 128, NT], BF16); din("mem", [256, D])
    din("vecs", [128, NV_T]); din("w_c", [D, 512]); din("w_out", [D, D]); din("w_q", [D, 512])
    din("w_kv", [D, D]); din("w_o", [512, D])
    din("w_gate", [E, D, DFF]); din("w_up", [E, D, DFF]); din("w_down", [E, DFF, D])
    if E > 1:
        din("router_w", [D, 8])
    if last:
        dout("out", [NT, D])
    else:
        dout("xT_out", [D, NT]); dout("h_next", [D, NT], BF16)
    io["dbg_stage"] = dbg_stage
    with _ES() as st:
        c = Ctx(nc, st)
        c.setup()
        xT = c.sb("xT", [128, 8, NT], F32)
        c.kb.dma("sp", xT[:], io["xT_in"].rearrange("(k p) n -> p k n", p=128), writes=[("xT", k) for k in range(8)])
        phase_T(c, io, E, last and not dbg_stage, xT)
        evs = []
        if not last or dbg_stage:
            key = "xT_out" if not last else "out"
            if last:
                io["xT_dbg"] = None
            evs.append(c.kb.dma("sp", io["xT_out"].rearrange("(k p) n -> p k n", p=128), xT[:],
                                reads=[("xT", k) for k in range(8)]))
        c.barrier()
        c.kb.flush()
    return nc


def vecs_T(inp, l, last):
    v = np.zeros((128, NV_T), np.float32)
    fm = lambda w: np.asarray(w, np.float32).reshape(-1, 128).T
    v[:, 0:8] = fm(inp["norm_xattn_w"][l]); v[:, 8:16] = fm(inp["norm_mem_w"][l]); v[:, 16:24] = fm(inp["norm_ffn_w"][l])
    v[:, 24:32] = fm(inp["norm_final_w"]) if last else fm(inp["norm_mix_w"][l + 1])
    v[:, 32:34] = fm(inp["conf_conv_b"][l]); v[:, 34:36] = fm(inp["conf_ln_w"][l]); v[:, 36:38] = fm(inp["conf_ln_b"][l])
    cw = np.asarray(inp["conf_conv_w"][l], np.float32)
    for j in range(31):
        v[:, 38 + 2 * j:40 + 2 * j] = fm(cw[j])
    return v


NCH = SEQ // 64
NKT = SEQ // 128
NQT = SEQ // TT
GRP = 4


def AP3(t, off, dims):
    return bass.AP(t[:].tensor, off, [list(d) for d in dims])


def log_sigmoid_tile(c, x, out, tmp1, tmp2, bias_ap, keys):
    kb = c.kb
    kx, ko, k1, k2 = keys
    kb.op("dve", lambda e: e.tensor_scalar(out=x, in0=x, scalar1=bias_ap, scalar2=None, op0=ALU.add), reads=[kx, "vecsM"], writes=[kx])
    kb.op("dve", lambda e: e.tensor_scalar(out=tmp1, in0=x, scalar1=-1.0, scalar2=None, op0=ALU.mult), reads=[kx], writes=[k1])
    kb.op("dve", lambda e: e.tensor_tensor(out=tmp1, in0=tmp1, in1=x, op=ALU.max), reads=[kx, k1], writes=[k1])
    kb.op("act", lambda e: e.activation(out=tmp1, in_=tmp1, func=AF.Exp, scale=-1.0), reads=[k1], writes=[k1])
    kb.op("dve", lambda e: e.tensor_scalar(out=tmp1, in0=tmp1, scalar1=1.0, scalar2=None, op0=ALU.add), reads=[k1], writes=[k1])
    kb.op("act", lambda e: e.activation(out=tmp1, in_=tmp1, func=AF.Ln), reads=[k1], writes=[k1])
    kb.op("dve", lambda e: e.tensor_scalar(out=tmp2, in0=x, scalar1=0.0, scalar2=None, op0=ALU.min), reads=[kx], writes=[k2])
    kb.op("dve", lambda e: e.tensor_tensor(out=out, in0=tmp2, in1=tmp1, op=ALU.subtract), reads=[k1, k2], writes=[ko])


def phase_M_mlstm(c, io):
    nc, kb = c.nc, c.kb
    from contextlib import ExitStack
    with ExitStack() as s0:
        vm = c.sb("vecsM_sb", [128, 16], F32, s0)
        wml = c.sb("wml", [128, 8, 258], BF16, s0)
        qT = c.sb("m_qT", [64, SEQ], BF16, s0)
        kT = c.sb("m_kT", [64, SEQ], BF16, s0)
        Vaug = c.sb("m_Vaug", [64, NCH, 65], BF16, s0)
        og = c.sb("m_og", [64, SEQ], BF16, s0)
        iC = c.sb("m_iC", [128, 64], F32, s0)
        fC = c.sb("m_fC", [128, 64], F32, s0)
        eps_t = c.sb("m_eps", [128, 1], F32, s0)
        kb.dma("sp", vm[:], io["vecsM"], writes=["vecsM"])
        kb.dma("pool", wml[:], io["w_ml"].rearrange("(k p) n -> p k n", p=128), writes=["wml"])
        kb.op("pool", lambda e: e.memset(Vaug[:, :, 64:65], 1.0), writes=["Vaug1"])
        kb.op("pool", lambda e: e.memset(eps_t[:], EPS), writes=["m_eps"])
        with ExitStack() as s1:
            hTb = [c.sb(f"m_hT{i}", [128, 8, TT], BF16, s1) for i in range(2)]
            zq = c.sb("m_zq", [64, TT + 3], F32, s1)
            zk = c.sb("m_zk", [64, TT + 3], F32, s1)
            cacc = [c.sb(f"m_cacc{i}", [64, TT], F32, s1) for i in range(2)]
            vt = c.sb("m_vt", [64, TT], F32, s1)
            rows = [c.sb(f"m_rows{i}", [2, TT], F32, s1) for i in range(2)]
            kb.op("pool", lambda e: e.memset(zq[:, 0:3], 0.0), writes=["zq"])
            kb.op("pool", lambda e: e.memset(zk[:, 0:3], 0.0), writes=["zk"])
            for tt in range(NQT):
                j, off = tt // 4, (tt % 4) * TT
                hT = hTb[tt % 2]
                hk = f"m_hT{tt % 2}"
                tok = slice(tt * TT, (tt + 1) * TT)
                h_load(c, io["hT_all"], hT, off, TT, [hk], j=j)
                for nm, z, col0, vc, dst in (("q", zq, 0, 0, qT), ("k", zk, 64, 5, kT)):
                    p, pk = c.ps()
                    for k in range(8):
                        mm(c, p[0:64, :], wml[:, k, col0:col0 + 64], hT[:, k, :], k == 0, k == 7, ["wml", hk], [pk])
                    zkey = "z" + nm
                    kb.op("act", lambda e, z=z, p=p: e.activation(out=z[:, 3:TT + 3], in_=p[0:64, :], func=AF.Copy), reads=[pk], writes=[zkey])
                    ca = cacc[0 if nm == "q" else 1]
                    ck = "cacc" + nm
                    kb.op("dve", lambda e, z=z, ca=ca, vc=vc: e.tensor_scalar(out=ca[:], in0=z[:, 0:TT], scalar1=vm[0:64, vc:vc + 1], scalar2=vm[0:64, vc + 4:vc + 5],
                                                                              op0=ALU.mult, op1=ALU.add), reads=[zkey, "vecsM"], writes=[ck])
                    for jj in range(1, 4):
                        kb.op("dve", lambda e, z=z, ca=ca, vc=vc, jj=jj: e.scalar_tensor_tensor(out=ca[:], in0=z[:, jj:jj + TT], scalar=vm[0:64, vc + jj:vc + jj + 1], in1=ca[:],
                                                                                                 op0=ALU.mult, op1=ALU.add), reads=[zkey, ck, "vecsM"], writes=[ck])
                    kb.op("act", lambda e, ca=ca, dst=dst, tok=tok: e.activation(out=dst[:, tok], in_=ca[:], func=AF.Silu), reads=[ck], writes=[("m_" + nm + "T", tt)])
                    kb.op("dve", lambda e, z=z: e.tensor_copy(out=z[:, 0:3], in_=z[:, TT:TT + 3]), reads=[zkey], writes=[zkey])
                p, pk = c.ps()
                for k in range(8):
                    mm(c, p[0:64, :], wml[:, k, 128:192], hT[:, k, :], k == 0, k == 7, ["wml", hk], [pk])
                kb.op("act", lambda e, p=p: e.activation(out=vt[:], in_=p[0:64, :], func=AF.Copy), reads=[pk], writes=["m_vt"])
                p2, p2k = c.ps()
                for ci in range(8):
                    kb.op("pe", lambda e, ci=ci, p2=p2: e.transpose(out=p2[0:64, ci * 64:(ci + 1) * 64], in_=vt[:, ci * 64:(ci + 1) * 64], identity=c.ident_f[0:64, 0:64]),
                          reads=["m_vt", "ident_f"], writes=[p2k])
                kb.op("dve", lambda e, p2=p2, tt=tt: e.tensor_copy(out=Vaug[:, tt * 8:(tt + 1) * 8, 0:64], in_=p2[0:64, :].rearrange("p (c d) -> p c d", d=64)),
                      reads=[p2k], writes=[("Vaug", tt)])
                p, pk = c.ps()
                for k in range(8):
                    mm(c, p[0:64, :], wml[:, k, 192:256], hT[:, k, :], k == 0, k == 7, ["wml", hk], [pk])
                kb.op("act", lambda e, p=p, tok=tok: e.activation(out=og[:, tok], in_=p[0:64, :], func=AF.Sigmoid), reads=[pk], writes=[("og", tt)])
                p, pk = c.ps()
                for k in range(8):
                    mm(c, p[0:2, :], wml[:, k, 256:258], hT[:, k, :], k == 0, k == 7, ["wml", hk], [pk])
                rw = rows[tt % 2]
                rk = f"m_rows{tt % 2}"
                kb.op("act", lambda e, p=p, rw=rw: e.activation(out=rw[:], in_=p[0:2, :], func=AF.Copy), reads=[pk], writes=[rk])
                kb.dma("sp", iC[tt * 8:(tt + 1) * 8, :], AP3(rw, 0, [[TT, 1], [64, 8], [1, 64]]), reads=[rk], writes=["iC"])
                kb.dma("sp", fC[tt * 8:(tt + 1) * 8, :], AP3(rw, TT, [[TT, 1], [64, 8], [1, 64]]), reads=[rk], writes=["fC"])
        c.barrier()
        Uall = c.sb("m_Uall", [64, 65, NCH], F32, s0)
        wgT = c.sb("m_wgT", [64, NCH], F32, s0)
        flT = c.sb("m_flT", [64, NCH], F32, s0)
        dB = c.sb("m_dB", [64, NCH], F32, s0)
        dB0 = c.sb("m_dB0", [64, NCH], F32, s0)
        with ExitStack() as s2:
            t1 = c.sb("g_t1", [128, 64], F32, s2)
            t2 = c.sb("g_t2", [128, 64], F32, s2)
            lf = c.sb("g_lf", [128, 64], F32, s2)
            bb = c.sb("g_b", [128, 64], F32, s2)
            aa = c.sb("g_a", [128, 64], F32, s2)
            AA = c.sb("g_A", [128, 64], F32, s2)
            MM = c.sb("g_M", [128, 64], F32, s2)
            wg = c.sb("g_wg", [128, 64], F32, s2)
            fl = c.sb("g_fl", [128, 64], F32, s2)
            on = c.sb("g_on", [128, 64], F32, s2)
            r1 = c.sb("g_r1", [1, 128], F32, s2)
            r2 = c.sb("g_r2", [1, 128], F32, s2)
            r3 = c.sb("g_r3", [1, 128], F32, s2)
            r4 = c.sb("g_r4", [1, 128], F32, s2)
            mcol = c.sb("g_mcol", [128, 1], F32, s2)
            nM63 = c.sb("g_nM63", [128, 1], F32, s2)
            dec = c.sb("g_dec", [128, 1], F32, s2)
            dgd = c.sb("g_dgd", [128, 128], F32, s2)
            Xb = [c.sb(f"g_X{i}", [128, 8, 64], F32, s2) for i in range(2)]
            kw32 = [c.sb(f"g_kw32{i}", [64, TT], F32, s2) for i in range(2)]
            kwTok = c.sb("g_kwTok", [64, NCH, 64], BF16, s2)
            log_sigmoid_tile(c, fC[:], lf[:], t1[:], t2[:], vm[:, 12:13], ("fC", "g_lf", "g_t1", "g_t2"))
            kb.op("dve", lambda e: e.tensor_scalar(out=iC[:], in0=iC[:], scalar1=vm[:, 11:12], scalar2=None, op0=ALU.add), reads=["iC", "vecsM"], writes=["iC"])
            kb.op("pool", lambda e: e.memset(on[:], 1.0), writes=["g_on"])
            kb.op("dve", lambda e: e.tensor_tensor_scan(out=bb[:], data0=on[:], data1=lf[:], initial=0.0, op0=ALU.mult, op1=ALU.add),
                  reads=["g_on", "g_lf"], writes=["g_b"])
            kb.op("dve", lambda e: e.tensor_tensor(out=aa[:], in0=iC[:], in1=bb[:], op=ALU.subtract), reads=["iC", "g_b"], writes=["g_a"])
            kb.op("dve", lambda e: e.tensor_tensor_scan(out=AA[:], data0=aa[:], data1=aa[:], initial=-1e30, op0=ALU.max, op1=ALU.max),
                  reads=["g_a"], writes=["g_A"])
            p, pk = c.ps()
            kb.op("pe", lambda e, p=p: e.transpose(out=p[0:1, 0:128], in_=AA[:, 63:64], identity=c.ident_f[:]), reads=["g_A", "ident_f"], writes=[pk])
            kb.op("pe", lambda e, p=p: e.transpose(out=p[0:1, 128:256], in_=bb[:, 63:64], identity=c.ident_f[:]), reads=["g_b", "ident_f"], writes=[pk])
            kb.op("dve", lambda e, p=p: e.tensor_copy(out=r1[:], in_=p[0:1, 0:128]), reads=[pk], writes=["g_r1"])
            kb.op("dve", lambda e, p=p: e.tensor_copy(out=r2[:], in_=p[0:1, 128:256]), reads=[pk], writes=["g_r2"])
            kb.op("dve", lambda e: e.tensor_tensor_scan(out=r3[:], data0=r1[:], data1=r2[:], initial=0.0, op0=ALU.max, op1=ALU.add),
                  reads=["g_r1", "g_r2"], writes=["g_r3"])
            kb.op("pool", lambda e: e.memset(r4[:, 0:1], 0.0), writes=["g_r4a"])
            kb.op("dve", lambda e: e.tensor_copy(out=r4[:, 1:128], in_=r3[:, 0:127]), reads=["g_r3"], writes=["g_r4b"])
            p, pk = c.ps()
            kb.op("pe", lambda e, p=p: e.transpose(out=p[:, 0:1], in_=r4[:], identity=c.ident_f[0:1, 0:1]), reads=["g_r4a", "g_r4b", "ident_f"], writes=[pk])
            kb.op("dve", lambda e, p=p: e.tensor_copy(out=mcol[:], in_=p[:, 0:1]), reads=[pk], writes=["g_mcol"])
            kb.op("dve", lambda e: e.tensor_scalar(out=MM[:], in0=AA[:], scalar1=mcol[:, 0:1], scalar2=None, op0=ALU.max), reads=["g_A", "g_mcol"], writes=["g_M"])
            kb.op("dve", lambda e: e.tensor_scalar(out=nM63[:], in0=MM[:, 63:64], scalar1=-1.0, scalar2=None, op0=ALU.mult), reads=["g_M"], writes=["g_nM63"])
            kb.op("act", lambda e: e.activation(out=wg[:], in_=aa[:], func=AF.Exp, bias=nM63[:, 0:1]), reads=["g_a", "g_nM63"], writes=["g_wg"])
            kb.op("act", lambda e: e.activation(out=dec[:], in_=mcol[:], func=AF.Exp, bias=nM63[:, 0:1]), reads=["g_mcol", "g_nM63"], writes=["g_dec"])
            kb.op("act", lambda e: e.activation(out=fl[:], in_=bb[:], func=AF.Exp, scale=-1.0, bias=nM63[:, 0:1]), reads=["g_b", "g_nM63"], writes=["g_fl"])
            p, pk = c.ps()
            kb.op("pe", lambda e, p=p: e.transpose(out=p[0:64, 0:128], in_=wg[:], identity=c.ident_f[:]), reads=["g_wg", "ident_f"], writes=[pk])
            kb.op("pe", lambda e, p=p: e.transpose(out=p[0:64, 128:256], in_=fl[:], identity=c.ident_f[:]), reads=["g_fl", "ident_f"], writes=[pk])
            kb.op("dve", lambda e, p=p: e.tensor_copy(out=wgT[:], in_=p[0:64, 0:128]), reads=[pk], writes=["m_wgT"])
            kb.op("dve", lambda e, p=p: e.tensor_copy(out=flT[:], in_=p[0:64, 128:256]), reads=[pk], writes=["m_flT"])
            kb.op("dve", lambda e: e.tensor_scalar(out=dgd[:], in0=c.ident_f[:], scalar1=dec[:, 0:1], scalar2=None, op0=ALU.mult), reads=["ident_f", "g_dec"], writes=["g_dgd"])
            p, pk = c.ps()
            mm(c, p[0:64, 0:128], c.ones_f[:, 0:64], dgd[:], True, True, ["ones_f", "g_dgd"], [pk])
            kb.op("dve", lambda e, p=p: e.tensor_copy(out=dB[:], in_=p[0:64, 0:128]), reads=[pk], writes=["m_dB"])
            kb.op("dve", lambda e, p=p: e.tensor_copy(out=dB0[:], in_=p[0:64, 0:128]), reads=[pk], writes=["m_dB0"])
            kb.op("pool", lambda e: e.memset(dB0[:, 0:1], 0.0), reads=["m_dB0"], writes=["m_dB0"])
            for tt in range(NQT):
                tok = slice(tt * TT, (tt + 1) * TT)
                X = Xb[tt % 2]
                Xk = f"g_X{tt % 2}"
                kb.op("dve", lambda e, X=X, tt=tt: e.tensor_tensor(out=X[:], in0=AP3(c.ident_f, 8 * tt, [[128, 128], [1, 8], [0, 64]]),
                                                                   in1=AP3(wg, 0, [[64, 128], [0, 8], [1, 64]]), op=ALU.mult),
                      reads=["ident_f", "g_wg"], writes=[Xk])
                p, pk = c.ps()
                mm(c, p[0:64, :], c.ones_f[:, 0:64], X[:].rearrange("p c s -> p (c s)"), True, True, ["ones_f", Xk], [pk])
                k32 = kw32[tt % 2]
                k32k = f"g_kw32{tt % 2}"
                kb.op("dve", lambda e, p=p, k32=k32, tok=tok: e.scalar_tensor_tensor(out=k32[:], in0=kT[:, tok], scalar=0.125, in1=p[0:64, :], op0=ALU.mult, op1=ALU.mult),
                      reads=[pk, ("m_kT", tt)], writes=[k32k])
                kb.op("act", lambda e, k32=k32, tok=tok: e.activation(out=kT[:, tok], in_=k32[:], func=AF.Copy), reads=[k32k], writes=[("m_kT", tt)])
                p2, p2k = c.ps()
                for ci in range(8):
                    kb.op("pe", lambda e, ci=ci, p2=p2, k32=k32: e.transpose(out=p2[0:64, ci * 64:(ci + 1) * 64], in_=k32[:, ci * 64:(ci + 1) * 64], identity=c.ident_f[0:64, 0:64]),
                          reads=[k32k, "ident_f"], writes=[p2k])
                kb.op("dve", lambda e, p2=p2, tt=tt: e.tensor_copy(out=kwTok[:, tt * 8:(tt + 1) * 8, :], in_=p2[0:64, :].rearrange("p (c d) -> p c d", d=64)),
                      reads=[p2k], writes=[("kwTok", tt)])
            for g in range(NCH // GRP):
                p, pk = c.ps()
                for ci in range(GRP):
                    ch = g * GRP + ci
                    mm(c, p[0:64, ci * 65:(ci + 1) * 65], kwTok[:, ch, :], Vaug[:, ch, :], True, True, [("kwTok", ch // 8), ("Vaug", ch // 8), "Vaug1"], [pk])
                kb.op("dve", lambda e, p=p, g=g: e.tensor_copy(out=AP3(Uall, g * GRP, [[65 * NCH, 64], [1, GRP], [NCH, 65]]),
                                                               in_=p[0:64, 0:GRP * 65].rearrange("p (c d) -> p c d", d=65)),
                      reads=[pk], writes=["Uall"])
        c.barrier()
        Cn = Uall
        Eb = c.sb("m_E", [64, NCH, 65], BF16, s0)
        for dv in range(65):
            kb.op("dve", lambda e, dv=dv: e.tensor_tensor_scan(out=Cn[:, dv, :], data0=dB0[:], data1=Uall[:, dv, :], initial=0.0, op0=ALU.mult, op1=ALU.add),
                  reads=["m_dB0", "Uall"], writes=[("Cn", dv), "Uall"])
        kb.op("pool", lambda e: e.memset(Eb[:, 0:1, :], 0.0), writes=["E0"])
        kb.op("dve", lambda e: e.tensor_tensor(out=Eb[:, 1:NCH, :], in0=AP3(Cn, 0, [[65 * NCH, 64], [1, NCH - 1], [NCH, 65]]),
                                               in1=AP3(dB, 1, [[NCH, 64], [1, NCH - 1], [0, 65]]), op=ALU.mult),
              reads=[("Cn", dv) for dv in range(65)] + ["m_dB"], writes=["E"])
        with ExitStack() as s4:
            mask = c.sb("o_mask", [64, 64], F32, s4)
            sT = [c.sb(f"o_sT{i}", [64, GRP * 64], BF16, s4) for i in range(2)]
            den = c.sb("o_den", [64, GRP], F32, s4)
            hn = c.sb("o_hn", [64, GRP, 64], F32, s4)
            hsq = c.sb("o_hsq", [64, GRP, 64], F32, s4)
            ss = c.sb("o_ss", [64, GRP], F32, s4)
            cst = [c.sb(f"o_cst{i}", [64, GRP * 64], BF16, s4) for i in range(2)]
            kb.op("pool", lambda e: e.memset(mask[:], 1.0), writes=["o_mask"])
            kb.op("pool", lambda e: e.affine_select(out=mask[:], in_=mask[:], pattern=[[1, 64]], compare_op=ALU.is_ge, fill=0.0, base=0, channel_multiplier=-1),
                  reads=["o_mask"], writes=["o_mask"])
            for g in range(NCH // GRP):
                c0 = g * GRP
                p, pk = c.ps()
                for ci in range(GRP):
                    ch = c0 + ci
                    cs = slice(ch * 64, (ch + 1) * 64)
                    mm(c, p[0:64, ci * 64:(ci + 1) * 64], kT[:, cs], qT[:, cs], True, True, [("m_kT", ch // 8), ("m_qT", ch // 8)], [pk])
                st_ = sT[g % 2]
                stk = f"o_sT{g % 2}"
                kb.op("dve", lambda e, p=p, st_=st_: e.tensor_tensor(out=st_[:].rearrange("p (c t) -> p c t", t=64), in0=p[0:64, 0:GRP * 64].rearrange("p (c t) -> p c t", t=64),
                                                                     in1=AP3(mask, 0, [[64, 64], [0, GRP], [1, 64]]), op=ALU.mult),
                      reads=[pk, "o_mask"], writes=[stk])
                po, pok = c.ps()
                for ci in range(GRP):
                    ch = c0 + ci
                    cs = slice(ch * 64, (ch + 1) * 64)
                    mm(c, po[0:64, ci * 65:(ci + 1) * 65], st_[:, ci * 64:(ci + 1) * 64], Vaug[:, ch, :], True, False, [stk, ("Vaug", ch // 8), "Vaug1"], [pok])
                    mm(c, po[0:64, ci * 65:(ci + 1) * 65], qT[:, cs], Eb[:, ch, :], False, True, [("m_qT", ch // 8), "E", "E0"], [pok])
                po3 = po[0:64, 0:GRP * 65].rearrange("p (c d) -> p c d", d=65)
                den3 = den[:].rearrange("p (c o) -> p c o", o=1)
                kb.op("dve", lambda e, po3=po3, den3=den3: e.tensor_scalar(out=den3, in0=po3[:, :, 64:65], scalar1=-1.0, scalar2=None, op0=ALU.mult),
                      reads=[pok], writes=["o_den"])
                kb.op("dve", lambda e, po3=po3, den3=den3: e.tensor_tensor(out=den3, in0=po3[:, :, 64:65], in1=den3, op=ALU.max),
                      reads=[pok, "o_den"], writes=["o_den"])
                kb.op("dve", lambda e, c0=c0: e.tensor_tensor(out=den[:], in0=den[:], in1=flT[:, c0:c0 + GRP], op=ALU.max),
                      reads=["o_den", "m_flT"], writes=["o_den"])
                kb.op("dve", lambda e: e.reciprocal(out=den[:], in_=den[:]), reads=["o_den"], writes=["o_den"])
                kb.op("dve", lambda e, po3=po3: e.tensor_tensor(out=hn[:], in0=po3[:, :, 0:64], in1=AP3(den, 0, [[GRP, 64], [1, GRP], [0, 64]]), op=ALU.mult),
                      reads=[pok, "o_den"], writes=["o_hn"])
                kb.op("act", lambda e: e.activation(out=hsq[:], in_=hn[:], func=AF.Square), reads=["o_hn"], writes=["o_hsq"])
                kb.op("dve", lambda e: e.tensor_reduce(out=ss[:], in_=hsq[:], axis=AX.X, op=ALU.add), reads=["o_hsq"], writes=["o_ss"])
                kb.op("act", lambda e: e.activation(out=ss[:], in_=ss[:], func=AF.Sqrt, scale=1.0 / 64, bias=eps_t[0:64, 0:1]), reads=["o_ss", "m_eps"], writes=["o_ss"])
                kb.op("dve", lambda e: e.reciprocal(out=ss[:], in_=ss[:]), reads=["o_ss"], writes=["o_ss"])
                kb.op("dve", lambda e: e.tensor_tensor(out=hn[:], in0=hn[:], in1=AP3(ss, 0, [[GRP, 64], [1, GRP], [0, 64]]), op=ALU.mult),
                      reads=["o_hn", "o_ss"], writes=["o_hn"])
                pt, ptk = c.ps()
                for ci in range(GRP):
                    kb.op("pe", lambda e, ci=ci, pt=pt: e.transpose(out=pt[0:64, ci * 64:(ci + 1) * 64], in_=hn[:, ci, :], identity=c.ident_f[0:64, 0:64]),
                          reads=["o_hn", "ident_f"], writes=[ptk])
                cs_ = cst[g % 2]
                csk = f"o_cst{g % 2}"
                toks = slice(c0 * 64, (c0 + GRP) * 64)
                kb.op("dve", lambda e, pt=pt, cs_=cs_, toks=toks: e.scalar_tensor_tensor(out=cs_[:], in0=pt[0:64, 0:GRP * 64], scalar=vm[0:64, 10:11], in1=og[:, toks],
                                                                                       op0=ALU.mult, op1=ALU.mult),
                      reads=[ptk, "vecsM", ("og", (c0 * 64) // TT)], writes=[csk])
                kb.dma("sp", io["catm_out"][:, toks], cs_[:], reads=[csk])
        c.barrier()


def phase_M_fox(c, io):
    nc, kb = c.nc, c.kb
    from contextlib import ExitStack
    NEG = -30000.0
    with ExitStack() as s0:
        vm = c.sb("vecsMf_sb", [128, 16], F32, s0)
        wfx = c.sb("wfx", [128, 8, 386], BF16, s0)
        fq = [c.sb(f"f_q{h}", [64, SEQ], BF16, s0) for h in range(2)]
        fk = [c.sb(f"f_k{h}", [64, SEQ], BF16, s0) for h in range(2)]
        fV = [c.sb(f"f_V{h}", [128, NKT, 65], BF16, s0) for h in range(2)]
        fC = [c.sb(f"f_C{h}", [64, 128], F32, s0) for h in range(2)]
        kb.dma("sp", vm[:], io["vecsM"], writes=["vecsM"])
        kb.dma("pool", wfx[:], io["w_fx"].rearrange("(k p) n -> p k n", p=128), writes=["wfx"])
        for h in range(2):
            kb.op("pool", lambda e, h=h: e.memset(fV[h][:, :, 64:65], 1.0), writes=[("fV1", h)])
        with ExitStack() as s1:
            hTb = [c.sb(f"f_hT{i}", [128, 8, TT], BF16, s1) for i in range(2)]
            vt = c.sb("f_vt", [64, TT], F32, s1)
            rows = [c.sb(f"f_rows{i}", [2, TT], F32, s1) for i in range(2)]
            for tt in range(NQT):
                j, off = tt // 4, (tt % 4) * TT
                hT = hTb[tt % 2]
                hk = f"f_hT{tt % 2}"
                tok = slice(tt * TT, (tt + 1) * TT)
                h_load(c, io["hT_all"], hT, off, TT, [hk], j=j)
                for h in range(2):
                    b0 = h * 192
                    p, pk = c.ps()
                    for k in range(8):
                        mm(c, p[0:64, :], wfx[:, k, b0:b0 + 64], hT[:, k, :], k == 0, k == 7, ["wfx", hk], [pk])
                    kb.op("act", lambda e, p=p, h=h, tok=tok: e.activation(out=fq[h][:, tok], in_=p[0:64, :], func=AF.Copy, scale=0.125), reads=[pk], writes=[("fq", h, tt)])
                    p, pk = c.ps()
                    for k in range(8):
                        mm(c, p[0:64, :], wfx[:, k, b0 + 64:b0 + 128], hT[:, k, :], k == 0, k == 7, ["wfx", hk], [pk])
                    kb.op("act", lambda e, p=p, h=h, tok=tok: e.activation(out=fk[h][:, tok], in_=p[0:64, :], func=AF.Copy), reads=[pk], writes=[("fk", h, tt)])
                    p, pk = c.ps()
                    for k in range(8):
                        mm(c, p[0:64, :], wfx[:, k, b0 + 128:b0 + 192], hT[:, k, :], k == 0, k == 7, ["wfx", hk], [pk])
                    kb.op("act", lambda e, p=p: e.activation(out=vt[:], in_=p[0:64, :], func=AF.Copy), reads=[pk], writes=["f_vt"])
                    p2, p2k = c.ps()
                    for ci in range(4):
                        kb.op("pe", lambda e, ci=ci, p2=p2: e.transpose(out=p2[:, ci * 64:(ci + 1) * 64], in_=vt[:, ci * 128:(ci + 1) * 128], identity=c.ident_f[0:64, 0:64]),
                              reads=["f_vt", "ident_f"], writes=[p2k])
                    kb.op("dve", lambda e, p2=p2, tt=tt, h=h: e.tensor_copy(out=fV[h][:, tt * 4:(tt + 1) * 4, 0:64], in_=p2[:, 0:256].rearrange("p (c d) -> p c d", d=64)),
                          reads=[p2k], writes=[("fV", h, tt)])
                p, pk = c.ps()
                for k in range(8):
                    mm(c, p[0:2, :], wfx[:, k, 384:386], hT[:, k, :], k == 0, k == 7, ["wfx", hk], [pk])
                rw = rows[tt % 2]
                rk = f"f_rows{tt % 2}"
                kb.op("act", lambda e, p=p, rw=rw: e.activation(out=rw[:], in_=p[0:2, :], func=AF.Copy), reads=[pk], writes=[rk])
                for h in range(2):
                    kb.dma("sp", fC[h][tt * 4:(tt + 1) * 4, :], AP3(rw, h * TT, [[TT, 1], [128, 4], [1, 128]]), reads=[rk], writes=[("fC", h)])
        c.barrier()
        ckT = [c.sb(f"f_ckT{h}", [128, NKT], F32, s0) for h in range(2)]
        cC = [c.sb(f"f_cC{h}", [64, 128], F32, s0) for h in range(2)]
        negm = c.sb("f_negm", [128, 4, TT], F32, s0)
        Ls = c.sb("f_Ls", [64, 64], F32, s0)
        with ExitStack() as s2:
            t1 = c.sb("f_t1", [64, 128], F32, s2)
            t2 = c.sb("f_t2", [64, 128], F32, s2)
            lf = c.sb("f_lf", [64, 128], F32, s2)
            on = c.sb("f_on", [64, 128], F32, s2)
            pre = c.sb("f_pre", [64, 1], F32, s2)
            kb.op("pool", lambda e: e.memset(on[:], 1.0), writes=["f_on"])
            kb.op("pool", lambda e: e.memset(Ls[:], 1.0), writes=["f_Ls"])
            kb.op("pool", lambda e: e.affine_select(out=Ls[:], in_=Ls[:], pattern=[[1, 64]], compare_op=ALU.is_ge, fill=0.0, base=-1, channel_multiplier=-1),
                  reads=["f_Ls"], writes=["f_Ls"])
            for r in range(4):
                kb.op("pool", lambda e, r=r: e.memset(negm[:, r, :], 0.0), writes=[("negm", r)])
                kb.op("pool", lambda e, r=r: e.affine_select(out=negm[:, r, :], in_=negm[:, r, :], pattern=[[1, TT]], compare_op=ALU.is_ge, fill=NEG,
                                                             base=-128 * r, channel_multiplier=-1), reads=[("negm", r)], writes=[("negm", r)])
            for h in range(2):
                log_sigmoid_tile(c, fC[h][:], lf[:], t1[:], t2[:], vm[0:64, 13 + h:14 + h], (("fC", h), "f_lf", "f_t1", "f_t2"))
                kb.op("dve", lambda e, h=h: e.tensor_tensor_scan(out=cC[h][:], data0=on[:], data1=lf[:], initial=0.0, op0=ALU.mult, op1=ALU.add),
                      reads=["f_on", "f_lf"], writes=[("cC", h)])
                p, pk = c.ps()
                mm(c, p[0:64, 0:1], Ls[:], cC[h][:, 127:128], True, True, ["f_Ls", ("cC", h)], [pk])
                kb.op("dve", lambda e, p=p: e.tensor_copy(out=pre[:], in_=p[0:64, 0:1]), reads=[pk], writes=["f_pre"])
                kb.op("dve", lambda e, h=h: e.tensor_scalar(out=cC[h][:], in0=cC[h][:], scalar1=pre[:, 0:1], scalar2=None, op0=ALU.add), reads=[("cC", h), "f_pre"], writes=[("cC", h)])
                p, pk = c.ps()
                kb.op("pe", lambda e, p=p, h=h: e.transpose(out=p[:, 0:64], in_=cC[h][:], identity=c.ident_f[0:64, 0:64]), reads=[("cC", h), "ident_f"], writes=[pk])
                kb.op("dve", lambda e, p=p, h=h: e.tensor_scalar(out=ckT[h][:], in0=p[:, 0:64], scalar1=-1.0, scalar2=None, op0=ALU.mult), reads=[pk], writes=[("ckT", h)])
        c.barrier()
        with ExitStack() as s3:
            X = c.sb("f_X", [64, 4, 128], F32, s3)
            cqB = c.sb("f_cqB", [128, TT], F32, s3)
            cqD = c.sb("f_cqD", [128, 4, TT], F32, s3)
            tmpb = [c.sb(f"f_tmp{i}", [128, TT], F32, s3) for i in range(3)]
            pTb = [c.sb(f"f_pT{i}", [128, TT], BF16, s3) for i in range(3)]
            osb = c.sb("f_osb", [65, TT], F32, s3)
            rden = c.sb("f_rden", [64, TT], F32, s3)
            outb = [c.sb(f"f_out{i}", [64, TT], BF16, s3) for i in range(2)]
            it = 0
            for h in range(2):
                for qi in range(NQT):
                    qs = slice(qi * TT, (qi + 1) * TT)
                    kb.op("dve", lambda e, h=h, qi=qi: e.tensor_tensor(out=X[:], in0=AP3(c.ident_f, 4 * qi, [[128, 64], [1, 4], [0, 128]]),
                                                                       in1=AP3(cC[h], 0, [[128, 64], [0, 4], [1, 128]]), op=ALU.mult),
                          reads=["ident_f", ("cC", h)], writes=["f_X"])
                    p, pk = c.ps()
                    mm(c, p[:, :], c.ones_f[0:64, :], X[:].rearrange("p r s -> p (r s)"), True, True, ["ones_f", "f_X"], [pk])
                    kb.op("act", lambda e, p=p: e.activation(out=cqB[:], in_=p[:, :], func=AF.Copy), reads=[pk], writes=["f_cqB"])
                    for r in range(4):
                        kb.op("pool", lambda e, r=r: e.tensor_tensor(out=cqD[:, r, :], in0=cqB[:], in1=negm[:, r, :], op=ALU.add),
                              reads=["f_cqB", ("negm", r)], writes=[("f_cqD", r)])
                    c.rot = list(range(6))
                    po, pok = c.psb[6 + qi % 2], f"psb{6 + qi % 2}"
                    nk = 4 * (qi + 1)
                    for kt in range(nk):
                        ps_, psk = c.ps()
                        mm(c, ps_[:, :], fk[h][:, kt * 128:(kt + 1) * 128], fq[h][:, qs], True, True, [("fk", h, kt // 4), ("fq", h, qi)], [psk])
                        tb = tmpb[it % 3]; tbk = f"f_tmp{it % 3}"
                        pb = pTb[it % 3]; pbk = f"f_pT{it % 3}"
                        it += 1
                        r = kt - 4 * qi
                        if r >= 0:
                            kb.op("dve", lambda e, ps_=ps_, tb=tb, r=r: e.tensor_tensor(out=tb[:], in0=ps_[:, :], in1=cqD[:, r, :], op=ALU.add),
                                  reads=[psk, ("f_cqD", r)], writes=[tbk])
                        else:
                            kb.op("dve", lambda e, ps_=ps_, tb=tb: e.tensor_tensor(out=tb[:], in0=ps_[:, :], in1=cqB[:], op=ALU.add),
                                  reads=[psk, "f_cqB"], writes=[tbk])
                        kb.op("act", lambda e, tb=tb, pb=pb, h=h, kt=kt: e.activation(out=pb[:], in_=tb[:], func=AF.Exp, bias=ckT[h][:, kt:kt + 1]),
                              reads=[tbk, ("ckT", h)], writes=[pbk])
                        mm(c, po[0:65, :], fV[h][:, kt, :], pb[:], kt == 0, kt == nk - 1, [("fV", h, kt // 4), ("fV1", h), pbk], [pok])
                    kb.op("act", lambda e, po=po: e.activation(out=osb[:], in_=po[0:65, :], func=AF.Copy), reads=[pok], writes=["f_osb"])
                    pd, pdk = c.ps()
                    mm(c, pd[0:64, :], c.ones_f[64:65, 0:64], osb[64:65, :], True, True, ["ones_f", "f_osb"], [pdk])
                    kb.op("dve", lambda e, pd=pd: e.reciprocal(out=rden[:], in_=pd[0:64, :]), reads=[pdk], writes=["f_rden"])
                    ob = outb[qi % 2]; obk = f"f_out{qi % 2}"
                    kb.op("dve", lambda e, ob=ob: e.tensor_tensor(out=ob[:], in0=osb[0:64, :], in1=rden[:], op=ALU.mult), reads=["f_osb", "f_rden"], writes=[obk])
                    cfo = io["catf_out"]
                    kb.dma("sp", (cfo[h][:, qs] if isinstance(cfo, list) else cfo[h * 64:(h + 1) * 64, qs]), ob[:], reads=[obk])
            c.rot = None
        c.barrier()


def build_M(which="both"):
    nc = bass.Bass("TRN2", target_bir_lowering=False)
    io = {}

    def din(name, shape, dt=F32):
        io[name] = nc.dram_tensor(name, shape, dt, kind="ExternalInput").ap()

    def dout(name, shape, dt=F32):
        io[name] = nc.dram_tensor(name, shape, dt, kind="ExternalOutput").ap()

    din("hT_all", [4, D, NT], BF16); din("w_ml", [D, 258]); din("w_fx", [D, 386]); din("vecsM", [128, 16])
    dout("catm_out", [64, SEQ], BF16); dout("catf_out", [128, SEQ], BF16)
    with _ES() as st:
        c = Ctx(nc, st)
        c.setup()
        if which in ("both", "mlstm"):
            phase_M_mlstm(c, io)
        if which in ("both", "fox"):
            phase_M_fox(c, io)
        c.barrier()
        c.kb.flush()
    return nc


def inputs_M(inp, l, g):
    w_in = np.asarray(inp["w_in"][l], np.float32)
    cols = np.concatenate([
        np.arange(g * 64, g * 64 + 64), 256 + np.arange(g * 64, g * 64 + 64),
        512 + np.arange(g * 64, g * 64 + 64), 768 + np.arange(g * 64, g * 64 + 64),
        [1024 + g, 1028 + g]])
    w_ml = np.ascontiguousarray(w_in[:, cols])
    fcols = []
    for hh in (2 * g, 2 * g + 1):
        for base in (1544, 2056, 2568):
            fcols.append(base + np.arange(hh * 64, hh * 64 + 64))
    fcols.append(np.array([3080 + 2 * g, 3080 + 2 * g + 1]))
    w_fx = np.ascontiguousarray(w_in[:, np.concatenate(fcols)])
    v = np.zeros((128, 16), np.float32)
    cw = np.asarray(inp["mlstm_conv_w"][l], np.float32)
    cb = np.asarray(inp["mlstm_conv_b"][l], np.float32)
    v[0:64, 0:4] = cw[:, g * 64:g * 64 + 64].T
    v[0:64, 4] = cb[g * 64:g * 64 + 64]
    v[0:64, 5:9] = cw[:, 256 + g * 64:256 + g * 64 + 64].T
    v[0:64, 9] = cb[256 + g * 64:256 + g * 64 + 64]
    v[0:64, 10] = np.asarray(inp["mlstm_norm_w"][l], np.float32)[g * 64:g * 64 + 64]
    v[:, 11] = inp["mlstm_b_i"][l][g]
    v[:, 12] = inp["mlstm_b_f"][l][g]
    v[:, 13] = inp["fox_b_f"][l][2 * g]
    v[:, 14] = inp["fox_b_f"][l][2 * g + 1]
    return {"w_ml": w_ml, "w_fx": w_fx, "vecsM": v}


def phase_P(c, io, xT):
    kb = c.kb
    from contextlib import ExitStack
    with ExitStack() as s0:
        vecs = c.sb("vecsP_sb", [128, 8], F32, s0)
        eps_t = c.sb("p_eps", [128, 1], F32, s0)
        sq = c.sb("p_sq", [128, 8, TT], BF16, s0)
        rstd = c.sb("p_rstd", [128, TT], F32, s0)
        hT = c.sb("p_hT", [128, 8, TT], BF16, s0)
        xt = [c.sb(f"p_xt{i}", [128, 4, D], F32, s0) for i in range(2)]
        tmp = {"sq": sq, "rstd": rstd, "eps": eps_t}
        kb.dma("sp", vecs[:], io["vecsP"], writes=["vecs"])
        kb.op("pool", lambda e: e.memset(eps_t[:], EPS), writes=["eps"])
        for t in range(NTT):
            t0 = t * TT
            xb = xt[t % 2]
            xk = f"p_xt{t % 2}"
            kb.dma("sp", xb[:], io["x_tok"][t0:t0 + TT, :].rearrange("(s p) d -> p s d", p=128), writes=[xk])
            for k in range(8):
                p, pk = c.ps()
                for s in range(4):
                    kb.op("pe", lambda e, k=k, s=s, p=p, xb=xb: e.transpose(out=p[:, s * 128:(s + 1) * 128], in_=xb[:, s, k * 128:(k + 1) * 128], identity=c.ident_f[:]),
                          reads=[xk, "ident_f"], writes=[pk])
                kb.op("act", lambda e, k=k, p=p, t0=t0: e.activation(out=xT[:, k, t0:t0 + TT], in_=p[:, :], func=AF.Copy), reads=[pk], writes=[("xT", k)])
            rmsnorm_tile(c, xT, "xT", t0, TT, vecs[:, 0:8], tmp, hT, "hT")
            h_store(c, io["h_next"], hT, t0, TT, [("hT", k) for k in range(8)], ["h_next_d"])
            if t == NTT - 1 and "tail_next" in io:
                kb.dma("sp", io["tail_next"].rearrange("(k p) n -> p k n", p=128), hT[:, :, TT - 32:TT],
                       reads=[("hT", k) for k in range(8)], writes=["tail_next_d"])
    c.barrier()


def build_P():
    nc = bass.Bass("TRN2", target_bir_lowering=False)
    io = {}
    io["x_tok"] = nc.dram_tensor("x_tok", [NT, D], F32, kind="ExternalInput").ap()
    io["vecsP"] = nc.dram_tensor("vecsP", [128, 8], F32, kind="ExternalInput").ap()
    io["xT_out"] = nc.dram_tensor("xT_out", [D, NT], F32, kind="ExternalOutput").ap()
    io["h_next"] = nc.dram_tensor("h_next", [D, NT], BF16, kind="ExternalOutput").ap()
    with _ES() as st:
        c = Ctx(nc, st)
        c.setup()
        xT = c.sb("xT", [128, 8, NT], F32)
        phase_P(c, io, xT)
        c.kb.dma("sp", io["xT_out"].rearrange("(k p) n -> p k n", p=128), xT[:], reads=[("xT", k) for k in range(8)])
        c.barrier()
        c.kb.flush()
    return nc


_CACHE = {}


def _get(name, fn):
    if name not in _CACHE:
        _CACHE[name] = fn()
    return _CACHE[name]


def kernel(**inp):
    inp = {k: np.asarray(v) for k, v in inp.items()}
    cores = list(range(8))
    B = 2
    x = inp["x"].astype(np.float32, copy=False)
    fm = lambda w: np.ascontiguousarray(np.asarray(w, np.float32).reshape(-1, 128).T)
    ncP = _get("P", build_P)
    maps = []
    for cid in cores:
        b, j = cid // 4, cid % 4
        maps.append({"x_tok": np.ascontiguousarray(x[b, j * NT:(j + 1) * NT]), "vecsP": fm(inp["norm_mix_w"][0])})
    res = run_bass_kernel_spmd(ncP, maps, core_ids=cores).results
    xT = [r["xT_out"] for r in res]
    hN = [r["h_next"] for r in res]
    out = None
    for l in range(2):
        last = (l == 1)
        E = 1 if l == 0 else 8
        ncM = _get("M", build_M)
        maps = []
        for cid in cores:
            b, g = cid // 4, cid % 4
            m = inputs_M(inp, l, g)
            m["hT_all"] = np.ascontiguousarray(np.stack([hN[b * 4 + jj] for jj in range(4)], axis=0))
            maps.append(m)
        resM = run_bass_kernel_spmd(ncM, maps, core_ids=cores).results
        ncT = _get(("T", E, last), lambda: build_T(E, last))
        maps = []
        for cid in cores:
            b, j = cid // 4, cid % 4
            tk = slice(j * NT, (j + 1) * NT)
            m = {"xT_in": xT[cid], "h_own": hN[cid]}
            m["h_halo"] = (np.ascontiguousarray(hN[cid - 1][:, NT - 32:NT]) if j > 0 else np.zeros((D, 32), NPBF))
            m["catm"] = np.ascontiguousarray(np.stack([resM[b * 4 + g]["catm_out"][:, tk] for g in range(4)], axis=0))
            m["catf"] = np.ascontiguousarray(np.stack([resM[b * 4 + g]["catf_out"][:, tk] for g in range(4)], axis=0))
            m["mem"] = np.ascontiguousarray(inp["mem"][b], dtype=np.float32)
            m["vecs"] = vecs_T(inp, l, last)
            m["w_c"] = np.ascontiguousarray(inp["w_in"][l][:, 1032:1544])
            m["w_out"] = inp["w_out"][l]; m["w_q"] = inp["xattn_w_q"][l]
            m["w_kv"] = inp["xattn_w_kv"][l]; m["w_o"] = inp["xattn_w_o"][l]
            if E == 1:
                m["w_gate"] = inp["ffn_w_gate"]; m["w_up"] = inp["ffn_w_up"]; m["w_down"] = inp["ffn_w_down"]
            else:
                m["w_gate"] = inp["moe_w_gate"][0]; m["w_up"] = inp["moe_w_up"][0]; m["w_down"] = inp["moe_w_down"][0]
                m["router_w"] = inp["router_w"][0]
            maps.append(m)
        resT = run_bass_kernel_spmd(ncT, maps, core_ids=cores).results
        if not last:
            xT = [r["xT_out"] for r in resT]
            hN = [r["h_next"] for r in resT]
        else:
            out = np.zeros((B, SEQ, D), np.float32)
            for cid in cores:
                b, j = cid // 4, cid % 4
                out[b, j * NT:(j + 1) * NT] = resT[cid]["out"]
    return out


RG = [[0, 1, 2, 3], [4, 5, 6, 7]]
_STOP = None


def build_fused(stop=None):
    nc = bass.Bass("TRN2", target_bir_lowering=False)
    io = {}
    if stop:
        io["dbg1"] = nc.dram_tensor("dbg1", [4 * D, NT], BF16, kind="ExternalOutput").ap()
        io["dbg2"] = nc.dram_tensor("dbg2", [512, SEQ], BF16, kind="ExternalOutput").ap()
        io["dbg3"] = nc.dram_tensor("dbg3", [D, NT], F32, kind="ExternalOutput").ap()

    def din(name, shape, dt=F32):
        io[name] = nc.dram_tensor(name, shape, dt, kind="ExternalInput").ap()
        return io[name]

    def dint(name, shape, dt=BF16):
        io[name] = nc.dram_tensor(name, shape, dt, kind="Internal").ap()
        return io[name]

    din("x_tok", [NT, D]); din("vecsP", [128, 8])
    if stop != "AG":
        din("sel", [128, 8]); din("mem", [256, D])
    for l in range(2 if stop != "AG" else 0):
        din(f"w_ml{l}", [D, 258]); din(f"w_fx{l}", [D, 386]); din(f"vecsM{l}", [128, 16]); din(f"vecs{l}", [128, NV_T])
        din(f"w_c{l}", [D, 512]); din(f"w_out{l}", [D, D]); din(f"w_q{l}", [D, 512]); din(f"w_kv{l}", [D, D]); din(f"w_o{l}", [512, D])
    if stop != "AG":
        din("w_gate0", [1, D, DFF]); din("w_up0", [1, D, DFF]); din("w_down0", [1, DFF, D])
        din("w_gate1", [8, D, DFF]); din("w_up1", [8, D, DFF]); din("w_down1", [8, DFF, D]); din("router_w", [D, 8])
    io["out"] = nc.dram_tensor("out", [NT, D], F32, kind="ExternalOutput").ap()
    for l in range(2):
        io[f"h_own{l}"] = [dint(f"h_own{l}_{a}", [256, NT]) for a in range(4)]
        io[f"hT_all{l}"] = [dint(f"hT_all{l}_{a}", [4 * 256, NT]) for a in range(4)]
        dint(f"tail{l}", [D, 32]); dint(f"tails{l}", [4 * D, 32])
        dint(f"catm{l}", [64, SEQ]); dint(f"catm_all{l}", [256, SEQ])
        io[f"catf{l}"] = [dint(f"catf{l}_{h}", [64, SEQ]) for h in range(2)]
        io[f"catf_all{l}"] = [dint(f"catf_all{l}_{h}", [256, SEQ]) for h in range(2)]
    with _ES() as st:
        c = Ctx(nc, st)
        c.setup()
        kb = c.kb
        xT = c.sb("xT", [128, 8, NT], F32)
        phase_P(c, {"x_tok": io["x_tok"], "vecsP": io["vecsP"], "h_next": io["h_own0"], "tail_next": io["tail0"]}, xT)
        for l in range(2):
            last = (l == 1)
            for a in range(4):
                kb.collective("AllGather", RG, io[f"h_own{l}"][a], io[f"hT_all{l}"][a], reads=["h_next_d"], writes=["hT_all_d"])
            kb.collective("AllGather", RG, io[f"tail{l}"], io[f"tails{l}"], reads=["tail_next_d"], writes=["tails_d"])
            c.barrier()
            if stop == "AG":
                for a in range(4):
                    for jj in range(4):
                        kb.dma("sp", io["dbg1"][jj * D + a * 256:jj * D + (a + 1) * 256, :], io[f"hT_all{l}"][a][jj * 256:(jj + 1) * 256, :], reads=["hT_all_d"])
                break
            ioM = {"hT_all": io[f"hT_all{l}"], "w_ml": io[f"w_ml{l}"], "w_fx": io[f"w_fx{l}"],
                   "vecsM": io[f"vecsM{l}"], "catm_out": io[f"catm{l}"], "catf_out": io[f"catf{l}"]}
            c.sfx = f"_{l}"
            phase_M_mlstm(c, ioM)
            phase_M_fox(c, ioM)
            kb.collective("AllGather", RG, io[f"catm{l}"], io[f"catm_all{l}"], writes=["catm_all_d"])
            for h in range(2):
                kb.collective("AllGather", RG, io[f"catf{l}"][h], io[f"catf_all{l}"][h], writes=["catf_all_d"])
            c.barrier()
            if stop == "M":
                for h in range(2):
                    for g in range(4):
                        kb.dma("sp", io["dbg2"][g * 128 + h * 64:g * 128 + (h + 1) * 64, :], io[f"catf_all{l}"][h][g * 64:(g + 1) * 64, :], reads=["catf_all_d"])
                break
            ioT = {"h_own": io[f"h_own{l}"], "tails": io[f"tails{l}"], "sel": io["sel"], "catm_all": io[f"catm_all{l}"], "catf_all": io[f"catf_all{l}"],
                   "mem": io["mem"], "vecs": io[f"vecs{l}"], "w_c": io[f"w_c{l}"], "w_out": io[f"w_out{l}"], "w_q": io[f"w_q{l}"],
                   "w_kv": io[f"w_kv{l}"], "w_o": io[f"w_o{l}"], "w_gate": io[f"w_gate{l}"], "w_up": io[f"w_up{l}"], "w_down": io[f"w_down{l}"]}
            if last:
                ioT["router_w"] = io["router_w"]; ioT["out"] = io["out"]
            else:
                ioT["h_next"] = io["h_own1"]; ioT["tail_next"] = io["tail1"]
            phase_T(c, ioT, 8 if last else 1, last, xT)
            if stop == "T":
                kb.dma("sp", io["dbg3"].rearrange("(k p) n -> p k n", p=128), xT[:], reads=[("xT", k) for k in range(8)])
                break
        c.barrier()
        kb.flush()
    return nc


def kernel_unfused(**inp):
    return _kernel_unfused(**inp)


_kernel_unfused = kernel


def kernel(**inp):
    inp = {k: np.asarray(v) for k, v in inp.items()}
    cores = list(range(8))
    x = inp["x"].astype(np.float32, copy=False)
    fm = lambda w: np.ascontiguousarray(np.asarray(w, np.float32).reshape(-1, 128).T)
    nc = _get("fused", lambda: build_fused(_STOP))
    shared = {"vecsP": fm(inp["norm_mix_w"][0]),
              "w_gate0": inp["ffn_w_gate"], "w_up0": inp["ffn_w_up"], "w_down0": inp["ffn_w_down"],
              "w_gate1": inp["moe_w_gate"][0], "w_up1": inp["moe_w_up"][0], "w_down1": inp["moe_w_down"][0],
              "router_w": inp["router_w"][0]}
    for l in range(2):
        shared[f"vecs{l}"] = vecs_T(inp, l, l == 1)
        shared[f"w_c{l}"] = np.ascontiguousarray(inp["w_in"][l][:, 1032:1544])
        shared[f"w_out{l}"] = inp["w_out"][l]; shared[f"w_q{l}"] = inp["xattn_w_q"][l]
        shared[f"w_kv{l}"] = inp["xattn_w_kv"][l]; shared[f"w_o{l}"] = inp["xattn_w_o"][l]
    perg = []
    for g in range(4):
        d = {}
        for l in range(2):
            m = inputs_M(inp, l, g)
            d[f"w_ml{l}"] = m["w_ml"]; d[f"w_fx{l}"] = m["w_fx"]; d[f"vecsM{l}"] = m["vecsM"]
        perg.append(d)
    maps = []
    for cid in cores:
        b, j = cid // 4, cid % 4
        m = dict(shared)
        m.update(perg[j])
        m["x_tok"] = np.ascontiguousarray(x[b, j * NT:(j + 1) * NT])
        m["mem"] = np.ascontiguousarray(inp["mem"][b], dtype=np.float32)
        sel = np.zeros((128, 8), np.float32)
        sel[:, j] = 1.0
        if j > 0:
            sel[:, 4 + j - 1] = 1.0
        m["sel"] = sel
        maps.append(m)
    if _STOP == "AG":
        maps = [{k: m[k] for k in ("x_tok", "vecsP")} for m in maps]
    res = run_bass_kernel_spmd(nc, maps, core_ids=cores).results
    if _STOP:
        return res
    out = np.zeros((2, SEQ, D), np.float32)
    for cid in cores:
        b, j = cid // 4, cid % 4
        out[b, j * NT:(j + 1) * NT] = res[cid]["out"]
    return out
```

```python
import numpy as np
import concourse.bass as bass
import concourse.mybir as mybir
from concourse.bass_utils import run_bass_kernel_spmd

F32 = mybir.dt.float32
BF16 = mybir.dt.bfloat16
AF = mybir.ActivationFunctionType
ALU = mybir.AluOpType
AX = mybir.AxisListType

ENGS = ("pe", "act", "dve", "pool", "sp")


class KB:
    SEM_ROLL = 2000

    def __init__(self, nc, n_dma_sems=32):
        self.nc = nc
        self.q = {e: [] for e in ENGS}
        self.cnt = {e: 0 for e in ENGS}
        self.cur_sem = {}
        self.sem_pool = []
        self.waited = {e: {} for e in ENGS}
        self.last_w = {}
        self.reads = {}
        self.n_dma_sems = n_dma_sems
        self.dma_sems = []
        self.dma_cnt = []
        self.dma_rr = 0
        self.dma_rr_sw = 0
        self._stack = None
        self.n_inst = 0

    def _new_sem(self, name):
        s = self._stack.enter_context(self.nc.semaphore(name))
        return s

    def start(self, stack):
        self._stack = stack
        for e in ENGS:
            self.cur_sem[e] = self._new_sem(f"p_{e}_0")
        for i in range(self.n_dma_sems):
            self.dma_sems.append(self._new_sem(f"dma{i}"))
            self.dma_cnt.append(0)

    def _wait(self, eng, ev):
        if ev is None:
            return
        if len(ev) == 3 and ev[2] == "pe" and eng == "pe":
            return
        sem, val = ev[0], ev[1]
        w = self.waited[eng]
        if w.get(id(sem), (None, 0))[1] >= val:
            return
        w[id(sem)] = (sem, val)
        self.q[eng].append(lambda e, sem=sem, val=val: e.wait_ge(sem, val))

    def _wait_w(self, eng, k):
        lw = self.last_w.get(k)
        if isinstance(lw, list):
            for ev in lw:
                self._wait(eng, ev)
        else:
            self._wait(eng, lw)

    def _deps(self, eng, reads, writes):
        for k in reads:
            self._wait_w(eng, k)
        for k in writes:
            self._wait_w(eng, k)
            for ev in self.reads.get(k, ()):
                self._wait(eng, ev)

    def _commit(self, ev, reads, writes, is_dma=False):
        for k in writes:
            lw = self.last_w.get(k)
            if is_dma and isinstance(lw, list) and not self.reads.get(k):
                lw.append(ev)
            else:
                self.last_w[k] = [ev] if is_dma else ev
            self.reads[k] = []
        for k in reads:
            self.reads.setdefault(k, []).append(ev)

    def op(self, eng, fn, reads=(), writes=()):
        self._deps(eng, reads, writes)
        if self.cnt[eng] >= self.SEM_ROLL:
            self.cur_sem[eng] = self._new_sem(f"p_{eng}_{self.n_inst}")
            self.cnt[eng] = 0
        self.cnt[eng] += 1
        sem = self.cur_sem[eng]
        ev = (sem, self.cnt[eng], eng)
        self.q[eng].append(lambda e, sem=sem: fn(e).then_inc(sem, 1))
        self._commit(ev, reads, writes)
        self.n_inst += 1
        return ev

    def dma(self, eng, out, in_, reads=(), writes=(), **kw):
        self._deps(eng, reads, writes)
        half = self.n_dma_sems // 2
        if eng == "pool":
            i = half + self.dma_rr_sw
            self.dma_rr_sw = (self.dma_rr_sw + 1) % (self.n_dma_sems - half)
        else:
            i = self.dma_rr
            self.dma_rr = (self.dma_rr + 1) % half
        sem = self.dma_sems[i]
        if self.dma_cnt[i] >= 2048:
            self.dma_sems[i] = self._new_sem(f"dma{i}_{self.n_inst}")
            self.dma_cnt[i] = 0
            sem = self.dma_sems[i]
        if self.dma_cnt[i] > 0:
            self._wait(eng, (sem, self.dma_cnt[i]))
        self.dma_cnt[i] += 16
        ev = (sem, self.dma_cnt[i])
        self.q[eng].append(lambda e, sem=sem: e.dma_start(out=out, in_=in_, **kw).then_inc(sem, 16))
        self._commit(ev, reads, writes, is_dma=True)
        self.n_inst += 1
        return ev

    def collective(self, kind, rg, in_ap, out_ap, reads=(), writes=()):
        eng = "pool"
        self._deps(eng, reads, writes)
        sem = self._new_sem(f"cc_{self.n_inst}")
        ev = (sem, 1)
        self.q[eng].append(lambda e: e.collective_compute(kind, ALU.bypass, replica_groups=rg, ins=[in_ap.opt()],
                                                          outs=[out_ap.opt()]).then_inc(sem, 1))
        self._commit(ev, reads, writes)
        self.n_inst += 1
        self.cc_events = getattr(self, "cc_events", []) + [ev]
        return ev

    def wait_all(self, eng, evs):
        for ev in evs:
            self._wait(eng, ev)

    def flush(self):
        nc = self.nc
        q = self.q
        with nc.Block() as block:
            @block.tensor
            def _(e):
                for f in q["pe"]:
                    f(e)

            @block.scalar
            def _(e):
                for f in q["act"]:
                    f(e)

            @block.vector
            def _(e):
                for f in q["dve"]:
                    f(e)

            @block.gpsimd
            def _(e):
                for f in q["pool"]:
                    f(e)

            @block.sync
            def _(e):
                for f in q["sp"]:
                    f(e)
        self.q = {e: [] for e in ENGS}


D = 1024
NT = 2048
TT = 512
NTT = NT // TT
DFF = 2816
NF = DFF // 128
SEQ = 8192
EPS = 1e-6
NV_T = 100


class Ctx:
    def __init__(self, nc, st):
        self.nc = nc
        self.st = st
        self.kb = KB(nc)
        self.kb.start(st)
        self.ps_rr = 0
        self.uid = 0

    def sb(self, name, shape, dt, st=None):
        self.uid += 1
        return (st or self.st).enter_context(self.nc.sbuf_tensor(f"{name}_u{self.uid}", shape, dt))

    def barrier(self):
        kb = self.kb
        evs = []
        for e in ENGS:
            if kb.cnt[e] > 0:
                evs.append((kb.cur_sem[e], kb.cnt[e]))
        for i, s in enumerate(kb.dma_sems):
            if kb.dma_cnt[i] > 0:
                evs.append((s, kb.dma_cnt[i]))
        evs += getattr(kb, "cc_events", [])
        kb.cc_events = []
        for e in ENGS:
            for ev in evs:
                kb._wait(e, ev)
        kb.last_w = {}
        kb.reads = {}

    def setup(self):
        nc, kb = self.nc, self.kb
        self.ident_f = self.sb("ident_f", [128, 128], F32)
        self.ident_b = self.sb("ident_b", [128, 128], BF16)
        self.ones_b = self.sb("ones_b", [128, 128], BF16)
        self.ones_f = self.sb("ones_f", [128, 128], F32)
        self.psb = [self.st.enter_context(nc.psum_tensor(f"psb{i}", [128, 512], F32)) for i in range(8)]
        idf, idb, ob, of = self.ident_f, self.ident_b, self.ones_b, self.ones_f
        kb.op("pool", lambda e: e.memset(idf[:], 0.0), writes=["ident_f"])
        kb.op("pool", lambda e: e.affine_select(out=idf[:], in_=idf[:], pattern=[[-1, 128]],
                                                compare_op=ALU.not_equal, fill=1.0, base=0,
                                                channel_multiplier=1),
              reads=["ident_f"], writes=["ident_f"])
        kb.op("pool", lambda e: e.tensor_copy(out=idb[:], in_=idf[:]), reads=["ident_f"], writes=["ident_b"])
        kb.op("pool", lambda e: e.memset(ob[:], 1.0), writes=["ones_b"])
        kb.op("pool", lambda e: e.memset(of[:], 1.0), writes=["ones_f"])

    def ps(self):
        rot = getattr(self, "rot", None) or list(range(8))
        i = rot[self.ps_rr % len(rot)]
        self.ps_rr += 1
        return self.psb[i], f"psb{i}"


def h_store(c, dst, hT, c0, n, reads, writes=()):
    if isinstance(dst, list):
        for a, d in enumerate(dst):
            c.kb.dma("sp", d[:, c0:c0 + n].rearrange("(k p) n -> p k n", p=128), hT[:, 2 * a:2 * a + 2, 0:n], reads=reads, writes=writes)
    else:
        c.kb.dma("sp", dst[:, c0:c0 + n].rearrange("(k p) n -> p k n", p=128), hT[:, :, 0:n], reads=reads, writes=writes)


def h_load(c, src, hT, c0, n, writes, j=None):
    if isinstance(src, list):
        for a, d in enumerate(src):
            v = d if j is None else d.rearrange("(j r) n -> j r n", j=4)[j]
            c.kb.dma("sp", hT[:, 2 * a:2 * a + 2, 0:n], v[:, c0:c0 + n].rearrange("(k p) n -> p k n", p=128), writes=writes)
    else:
        v = src if j is None else src[j]
        c.kb.dma("sp", hT[:, :, 0:n], v[:, c0:c0 + n].rearrange("(k p) n -> p k n", p=128), writes=writes)


def mm(c, out, lhsT, rhs, start, stop, reads, writes):
    return c.kb.op("pe", lambda e: e.matmul(out, lhsT=lhsT, rhs=rhs, start=start, stop=stop),
                   reads=reads, writes=writes)


def rmsnorm_tile(c, xT, xkey, t0, n, wv, tmp, out_bf, okey, out_f=None):
    kb = c.kb
    sq, rstd = tmp["sq"], tmp["rstd"]
    for k in range(8):
        kb.op("act", lambda e, k=k: e.activation(out=sq[:, k, 0:n], in_=xT[:, k, t0:t0 + n], func=AF.Square),
              reads=[(xkey, k)], writes=[("sq", k)])
    p, pk = c.ps()
    for k in range(8):
        mm(c, p[:, 0:n], c.ones_b[:], sq[:, k, 0:n], k == 0, k == 7, ["ones_b", ("sq", k)], [pk])
    kb.op("act", lambda e: e.activation(out=rstd[:, 0:n], in_=p[:, 0:n], func=AF.Sqrt, scale=1.0 / D, bias=tmp["eps"][:, 0:1]),
          reads=[pk, "eps"], writes=["rstd"])
    kb.op("dve", lambda e: e.reciprocal(out=rstd[:, 0:n], in_=rstd[:, 0:n]), reads=["rstd"], writes=["rstd"])
    for k in range(8):
        kb.op("dve", lambda e, k=k: e.scalar_tensor_tensor(out=out_bf[:, k, 0:n], in0=xT[:, k, t0:t0 + n],
                                                           scalar=wv[:, k:k + 1], in1=rstd[:, 0:n],
                                                           op0=ALU.mult, op1=ALU.mult),
              reads=[(xkey, k), "rstd", "vecs"], writes=[(okey, k)])
        if out_f is not None:
            kb.op("dve", lambda e, k=k: e.scalar_tensor_tensor(out=out_f[:, k, 0:n], in0=xT[:, k, t0:t0 + n],
                                                                scalar=wv[:, k:k + 1], in1=rstd[:, 0:n],
                                                                op0=ALU.mult, op1=ALU.mult),
                  reads=[(xkey, k), "rstd", "vecs"], writes=[(okey + "_f", k)])


def phase_T(c, io, E, last, xT):
    nc, kb = c.nc, c.kb
    from contextlib import ExitStack
    vec_st = ExitStack()
    vecs = c.sb("vecsT", [128, NV_T], F32, vec_st)
    eps_t = c.sb("eps_t", [128, 1], F32, vec_st)
    sq = c.sb("sq", [128, 8, TT], BF16, vec_st)
    rstd = c.sb("rstd", [128, TT], F32, vec_st)
    tmp = {"sq": sq, "rstd": rstd, "eps": eps_t}
    kb.dma("sp", vecs[:], io["vecs"], writes=["vecs"])
    kb.op("pool", lambda e: e.memset(eps_t[:], EPS), writes=["eps"])
    V_XA, V_MEM, V_FFN, V_NEXT, V_CB, V_LNW, V_LNB, V_CW = 0, 8, 16, 24, 32, 34, 36, 38

    with ExitStack() as s1:
        hT = c.sb("hT", [128, 8, TT], BF16, s1)
        gluT = c.sb("gluT", [128, 2, 32 + NT], BF16, s1)
        hcT = c.sb("hcT", [128, 2, NT], BF16, s1)
        wc = c.sb("wc", [128, 8, 512], BF16, s1)
        dg = c.sb("dg", [128, 62, 128], BF16, s1)
        sig = c.sb("sig", [128, 2, TT], F32, s1)
        hcv = c.sb("hcv", [128, 2, TT], F32, s1)
        hsq = c.sb("hsq", [128, 2, TT], F32, s1)
        mean = c.sb("mean", [128, TT], F32, s1)
        var = c.sb("var", [128, TT], F32, s1)
        wo_m = c.sb("wo_m", [64, 4, D], BF16, s1)
        wo_c = c.sb("wo_c", [128, 2, D], BF16, s1)
        wo_f = c.sb("wo_f", [128, 4, D], BF16, s1)
        mT = c.sb("mT", [64, 4, TT], BF16, s1)
        fT = c.sb("fT", [128, 4, TT], BF16, s1)
        if "sel" in io:
            halo4 = c.sb("halo4", [128, 4, 8, 32], BF16, s1)
            selt = c.sb("selt", [128, 8], F32, s1)
            m4 = [c.sb(f"m4_{i}", [64, 4, TT], BF16, s1) for i in range(2)]
            f4 = [c.sb(f"f4_{i}", [128, 4, TT], BF16, s1) for i in range(2)]
            kb.dma("sp", selt[:], io["sel"], writes=["selt"])
        kb.dma("pool", wc[:], io["w_c"].rearrange("(k p) n -> p k n", p=128), writes=["wc"])
        kb.dma("pool", wo_m[:], io["w_out"][0:256, :].rearrange("(g p) n -> p g n", p=64), writes=["wo_m"])
        kb.dma("pool", wo_c[:], io["w_out"][256:512, :].rearrange("(g p) n -> p g n", p=128), writes=["wo_c"])
        kb.dma("pool", wo_f[:], io["w_out"][512:1024, :].rearrange("(g p) n -> p g n", p=128), writes=["wo_f"])
        for j in range(31):
            for ch in range(2):
                kb.op("dve", lambda e, j=j, ch=ch: e.tensor_scalar(
                    out=dg[:, j * 2 + ch, :], in0=c.ident_b[:], scalar1=vecs[:, V_CW + j * 2 + ch:V_CW + j * 2 + ch + 1],
                    scalar2=None, op0=ALU.mult), reads=["ident_b", "vecs"], writes=[("dg", j, ch)])
        tiles = [("halo", 0, 32)] + [("own", t * TT, TT) for t in range(NTT)]
        for kind, t0, n in tiles:
            if kind == "halo" and "sel" in io:
                tl = io["tails"].rearrange("(j k p) n -> j p k n", j=4, p=128)
                for jj in range(4):
                    kb.dma("sp", halo4[:, jj, :, :], tl[jj], writes=[("halo4", jj)])
                kb.op("dve", lambda e: e.tensor_scalar(out=hT[:, :, 0:32], in0=halo4[:, 0, :, :], scalar1=selt[:, 4:5], scalar2=None, op0=ALU.mult),
                      reads=[("halo4", 0), "selt"], writes=[("hT", k) for k in range(8)])
                for jj in range(1, 4):
                    kb.op("dve", lambda e, jj=jj: e.scalar_tensor_tensor(out=hT[:, :, 0:32], in0=halo4[:, jj, :, :], scalar=selt[:, 4 + jj:5 + jj], in1=hT[:, :, 0:32],
                                                                         op0=ALU.mult, op1=ALU.add),
                          reads=[("halo4", jj), "selt"] + [("hT", k) for k in range(8)], writes=[("hT", k) for k in range(8)])
                g0 = 0
            elif kind == "halo":
                kb.dma("sp", hT[:, :, 0:n], io["h_halo"].rearrange("(k p) n -> p k n", p=128),
                       writes=[("hT", k) for k in range(8)])
                g0 = 0
            else:
                h_load(c, io["h_own"], hT, t0, n, [("hT", k) for k in range(8)])
                g0 = 32 + t0
            for ch in range(2):
                pa, pak = c.ps()
                pg, pgk = c.ps()
                for k in range(8):
                    mm(c, pa[:, 0:n], wc[:, k, ch * 128:(ch + 1) * 128], hT[:, k, 0:n], k == 0, k == 7,
                       ["wc", ("hT", k)], [pak])
                for k in range(8):
                    mm(c, pg[:, 0:n], wc[:, k, 256 + ch * 128:256 + (ch + 1) * 128], hT[:, k, 0:n], k == 0, k == 7,
                       ["wc", ("hT", k)], [pgk])
                kb.op("act", lambda e, ch=ch, pg=pg, n=n: e.activation(out=sig[:, ch, 0:n], in_=pg[:, 0:n], func=AF.Sigmoid),
                      reads=[pgk], writes=[("sig", ch)])
                kb.op("dve", lambda e, ch=ch, pa=pa, n=n, g0=g0: e.tensor_tensor(
                    out=gluT[:, ch, g0:g0 + n], in0=pa[:, 0:n], in1=sig[:, ch, 0:n], op=ALU.mult),
                    reads=[pak, ("sig", ch)], writes=[("glu", ch, g0 // TT), ("glu", ch, (g0 + n - 1) // TT)])
        for t in range(NTT):
            t0 = t * TT
            gk = lambda ch: [("glu", ch, (32 + t0 - 30) // TT), ("glu", ch, (32 + t0 + TT - 1) // TT)]
            for ch in range(2):
                p, pk = c.ps()
                for j in range(31):
                    o = 32 + t0 - 30 + j
                    mm(c, p[:, :], dg[:, j * 2 + ch, :], gluT[:, ch, o:o + TT], j == 0, j == 30,
                       [("dg", j, ch)] + gk(ch), [pk])
                kb.op("act", lambda e, ch=ch, p=p: e.activation(out=hcv[:, ch, :], in_=p[:, :], func=AF.Identity,
                                                                bias=vecs[:, V_CB + ch:V_CB + ch + 1]),
                      reads=[pk, "vecs"], writes=[("hcv", ch)])
                kb.op("act", lambda e, ch=ch: e.activation(out=hsq[:, ch, :], in_=hcv[:, ch, :], func=AF.Square),
                      reads=[("hcv", ch)], writes=[("hsq", ch)])
            p1, p1k = c.ps()
            p2, p2k = c.ps()
            for ch in range(2):
                mm(c, p1[:, :], c.ones_f[:], hcv[:, ch, :], ch == 0, ch == 1, ["ones_f", ("hcv", ch)], [p1k])
            for ch in range(2):
                mm(c, p2[:, :], c.ones_f[:], hsq[:, ch, :], ch == 0, ch == 1, ["ones_f", ("hsq", ch)], [p2k])
            kb.op("dve", lambda e, p1=p1: e.tensor_scalar(out=mean[:], in0=p1[:, :], scalar1=1.0 / 256, scalar2=None, op0=ALU.mult),
                  reads=[p1k], writes=["mean"])
            kb.op("dve", lambda e: e.tensor_tensor(out=var[:], in0=mean[:], in1=mean[:], op=ALU.mult),
                  reads=["mean"], writes=["var"])
            kb.op("dve", lambda e, p2=p2: e.scalar_tensor_tensor(out=var[:], in0=p2[:, :], scalar=1.0 / 256, in1=var[:],
                                                                 op0=ALU.mult, op1=ALU.subtract),
                  reads=[p2k, "var"], writes=["var"])
            kb.op("act", lambda e: e.activation(out=var[:], in_=var[:], func=AF.Sqrt, bias=eps_t[:, 0:1]),
                  reads=["var", "eps"], writes=["var"])
            kb.op("dve", lambda e: e.reciprocal(out=var[:], in_=var[:]), reads=["var"], writes=["var"])
            for ch in range(2):
                kb.op("dve", lambda e, ch=ch: e.tensor_tensor(out=hcv[:, ch, :], in0=hcv[:, ch, :], in1=mean[:], op=ALU.subtract),
                      reads=[("hcv", ch), "mean"], writes=[("hcv", ch)])
                kb.op("dve", lambda e, ch=ch: e.tensor_tensor(out=hcv[:, ch, :], in0=hcv[:, ch, :], in1=var[:], op=ALU.mult),
                      reads=[("hcv", ch), "var"], writes=[("hcv", ch)])
                kb.op("dve", lambda e, ch=ch: e.tensor_scalar(out=hcv[:, ch, :], in0=hcv[:, ch, :],
                                                              scalar1=vecs[:, V_LNW + ch:V_LNW + ch + 1],
                                                              scalar2=vecs[:, V_LNB + ch:V_LNB + ch + 1],
                                                              op0=ALU.mult, op1=ALU.add),
                      reads=[("hcv", ch), "vecs"], writes=[("hcv", ch)])
                kb.op("act", lambda e, ch=ch, t0=t0: e.activation(out=hcT[:, ch, t0:t0 + TT], in_=hcv[:, ch, :], func=AF.Silu),
                      reads=[("hcv", ch)], writes=[("hcT", ch, t)])
            if "sel" in io:
                cm = io["catm_all"].rearrange("(g p) n -> p g n", p=64)
                cf = [a.rearrange("(g p) n -> p g n", p=64) for a in io["catf_all"]]
                for jj in range(4):
                    for dst, stg, src, nm, npart in ((mT, m4, cm, "m4", 64), (fT, f4, cf, "f4", 128)):
                        dk = "mT" if nm == "m4" else "fT"
                        sg = stg[jj % 2]
                        sk = f"{nm}_{jj % 2}"
                        if nm == "m4":
                            kb.dma("sp", sg[:], src[:, :, jj * NT + t0:jj * NT + t0 + TT], writes=[sk])
                        else:
                            for hh in range(2):
                                kb.dma("sp", sg[hh * 64:(hh + 1) * 64, :, :], src[hh][:, :, jj * NT + t0:jj * NT + t0 + TT], writes=[sk])
                        if jj == 0:
                            kb.op("dve", lambda e, dst=dst, sg=sg, npart=npart: e.tensor_scalar(out=dst[:], in0=sg[:], scalar1=selt[0:npart, 0:1], scalar2=None, op0=ALU.mult),
                                  reads=[sk, "selt"], writes=[dk])
                        else:
                            kb.op("dve", lambda e, dst=dst, sg=sg, jj=jj, npart=npart: e.scalar_tensor_tensor(out=dst[:], in0=sg[:], scalar=selt[0:npart, jj:jj + 1], in1=dst[:],
                                                                                                          op0=ALU.mult, op1=ALU.add),
                                  reads=[sk, "selt", dk], writes=[dk])
            else:
                kb.dma("sp", mT[:], io["catm"][:, :, t0:t0 + TT].rearrange("g p n -> p g n"), writes=["mT"])
                kb.dma("sp", fT[:], io["catf"][:, :, t0:t0 + TT].rearrange("g p n -> p g n"), writes=["fT"])
            for d in range(8):
                p, pk = c.ps()
                ds = slice(d * 128, (d + 1) * 128)
                for g in range(4):
                    mm(c, p[:, :], wo_m[:, g, ds], mT[:, g, :], g == 0, False, ["wo_m", "mT"], [pk])
                for ch in range(2):
                    mm(c, p[:, :], wo_c[:, ch, ds], hcT[:, ch, t0:t0 + TT], False, False, ["wo_c", ("hcT", ch, t)], [pk])
                for g in range(4):
                    mm(c, p[:, :], wo_f[:, g, ds], fT[:, g, :], False, g == 3, ["wo_f", "fT"], [pk])
                kb.op("dve", lambda e, d=d, p=p, t0=t0: e.tensor_tensor(out=xT[:, d, t0:t0 + TT], in0=xT[:, d, t0:t0 + TT],
                                                                        in1=p[:, :], op=ALU.add),
                      reads=[pk, ("xT", d)], writes=[("xT", d)])
    c.barrier()
    if io.get("dbg_stage") == 1:
        vec_st.close()
        return

    with ExitStack() as s2:
        hT = c.sb("hT", [128, 8, TT], BF16, s2)
        memt = c.sb("memt", [128, 2, D], F32, s2)
        mss = c.sb("mss", [128, 2], F32, s2)
        junk = c.sb("junk", [128, D], F32, s2)
        memnT = c.sb("memnT", [128, 8, 256], BF16, s2)
        wkv = c.sb("wkv", [128, 8, D], BF16, s2)
        wq = c.sb("wq", [128, 8, 512], BF16, s2)
        wo = c.sb("wo", [128, 4, D], BF16, s2)
        kT = c.sb("kT", [128, 4, 256], BF16, s2)
        Vt = c.sb("Vt", [128, 2, 512], BF16, s2)
        qT = c.sb("qT", [128, 4, TT], BF16, s2)
        pT = c.sb("pT", [128, 8, TT], BF16, s2)
        rden = c.sb("rden", [128, TT], F32, s2)
        oT = c.sb("oT", [128, 4, TT], BF16, s2)
        kb.dma("sp", memt[:], io["mem"].rearrange("(t p) d -> p t d", p=128), writes=["memt"])
        kb.dma("pool", wkv[:], io["w_kv"].rearrange("(k p) n -> p k n", p=128), writes=["wkv"])
        kb.dma("pool", wq[:], io["w_q"].rearrange("(k p) n -> p k n", p=128), writes=["wq"])
        kb.dma("pool", wo[:], io["w_o"].rearrange("(k p) n -> p k n", p=128), writes=["wo"])
        for mt in range(2):
            kb.op("act", lambda e, mt=mt: e.activation(out=junk[:], in_=memt[:, mt, :], func=AF.Square,
                                                       accum_out=mss[:, mt:mt + 1]),
                  reads=["memt"], writes=["junk", ("mss", mt)])
            kb.op("act", lambda e, mt=mt: e.activation(out=mss[:, mt:mt + 1], in_=mss[:, mt:mt + 1], func=AF.Sqrt,
                                                       scale=1.0 / D, bias=eps_t[:, 0:1]),
                  reads=[("mss", mt), "eps"], writes=[("mss", mt)])
            kb.op("dve", lambda e, mt=mt: e.reciprocal(out=mss[:, mt:mt + 1], in_=mss[:, mt:mt + 1]),
                  reads=[("mss", mt)], writes=[("mss", mt)])
            kb.op("dve", lambda e, mt=mt: e.tensor_scalar(out=memt[:, mt, :], in0=memt[:, mt, :], scalar1=mss[:, mt:mt + 1],
                                                          scalar2=None, op0=ALU.mult),
                  reads=["memt", ("mss", mt)], writes=["memt"])
        for k in range(8):
            p, pk = c.ps()
            for mt in range(2):
                kb.op("pe", lambda e, k=k, mt=mt, p=p: e.transpose(out=p[:, mt * 128:(mt + 1) * 128],
                                                                   in_=memt[:, mt, k * 128:(k + 1) * 128], identity=c.ident_f[:]),
                      reads=["memt", "ident_f"], writes=[pk])
            kb.op("dve", lambda e, k=k, p=p: e.tensor_scalar(out=memnT[:, k, :], in0=p[:, 0:256],
                                                             scalar1=vecs[:, V_MEM + k:V_MEM + k + 1], scalar2=None, op0=ALU.mult),
                  reads=[pk, "vecs"], writes=[("memnT", k)])
        for h in range(4):
            p, pk = c.ps()
            for k in range(8):
                mm(c, p[:, 0:256], wkv[:, k, h * 128:(h + 1) * 128], memnT[:, k, :], k == 0, k == 7, ["wkv", ("memnT", k)], [pk])
            kb.op("act", lambda e, h=h, p=p: e.activation(out=kT[:, h, :], in_=p[:, 0:256], func=AF.Copy),
                  reads=[pk], writes=[("kT", h)])
        for mt in range(2):
            p, pk = c.ps()
            for k in range(8):
                mm(c, p[:, :], memnT[:, k, mt * 128:(mt + 1) * 128], wkv[:, k, 512:1024], k == 0, k == 7, ["wkv", ("memnT", k)], [pk])
            kb.op("act", lambda e, mt=mt, p=p: e.activation(out=Vt[:, mt, :], in_=p[:, :], func=AF.Copy),
                  reads=[pk], writes=[("Vt", mt)])
        sc = 128 ** -0.5
        for t in range(NTT):
            t0 = t * TT
            rmsnorm_tile(c, xT, "xT", t0, TT, vecs[:, V_XA:V_XA + 8], tmp, hT, "hT")
            for h in range(4):
                p, pk = c.ps()
                for k in range(8):
                    mm(c, p[:, :], wq[:, k, h * 128:(h + 1) * 128], hT[:, k, :], k == 0, k == 7, ["wq", ("hT", k)], [pk])
                kb.op("act", lambda e, h=h, p=p: e.activation(out=qT[:, h, :], in_=p[:, :], func=AF.Copy),
                      reads=[pk], writes=[("qT", h)])
            for h in range(4):
                for mt in range(2):
                    p, pk = c.ps()
                    mm(c, p[:, :], kT[:, h, mt * 128:(mt + 1) * 128], qT[:, h, :], True, True, [("kT", h), ("qT", h)], [pk])
                    kb.op("act", lambda e, h=h, mt=mt, p=p: e.activation(out=pT[:, h * 2 + mt, :], in_=p[:, :], func=AF.Exp, scale=sc),
                          reads=[pk], writes=[("pT", h, mt)])
                pd, pdk = c.ps()
                for mt in range(2):
                    mm(c, pd[:, :], c.ones_b[:], pT[:, h * 2 + mt, :], mt == 0, mt == 1, ["ones_b", ("pT", h, mt)], [pdk])
                kb.op("dve", lambda e, pd=pd: e.reciprocal(out=rden[:], in_=pd[:, :]), reads=[pdk], writes=["rden"])
                po, pok = c.ps()
                for mt in range(2):
                    mm(c, po[:, :], Vt[:, mt, h * 128:(h + 1) * 128], pT[:, h * 2 + mt, :], mt == 0, mt == 1,
                       [("Vt", mt), ("pT", h, mt)], [pok])
                kb.op("dve", lambda e, h=h, po=po: e.tensor_tensor(out=oT[:, h, :], in0=po[:, :], in1=rden[:], op=ALU.mult),
                      reads=[pok, "rden"], writes=[("oT", h)])
            for d in range(8):
                p, pk = c.ps()
                for h in range(4):
                    mm(c, p[:, :], wo[:, h, d * 128:(d + 1) * 128], oT[:, h, :], h == 0, h == 3, ["wo", ("oT", h)], [pk])
                kb.op("dve", lambda e, d=d, p=p, t0=t0: e.tensor_tensor(out=xT[:, d, t0:t0 + TT], in0=xT[:, d, t0:t0 + TT],
                                                                        in1=p[:, :], op=ALU.add),
                      reads=[pk, ("xT", d)], writes=[("xT", d)])
    c.barrier()
    if io.get("dbg_stage") == 2:
        vec_st.close()
        return

    with ExitStack() as s3:
        hTall = c.sb("hTall", [128, 8, NT], BF16, s3)
        actT = c.sb("actT", [128, 8, NT], BF16, s3)
        wgu = [c.sb(f"wgu{i}", [128, 8, 256], BF16, s3) for i in range(3)]
        wdr = [c.sb(f"wdr{i}", [128, D], BF16, s3) for i in range(11)]
        sil = [c.sb(f"sil{i}", [128, TT], BF16, s3) for i in range(2)]
        if E > 1:
            wr = c.sb("wr", [128, 8, 8], F32, s3)
            lg = c.sb("lg", [128, 4, 8], F32, s3)
            top8 = c.sb("top8", [128, 4, 8], F32, s3)
            gts = c.sb("gts", [128, 16, 8], F32, s3)
            gsc = c.sb("gsc", [128, 4, 4], F32, s3)
            dgate = c.sb("dgate", [128, 128], F32, s3)
            gB = [c.sb(f"gB{i}", [128, NT], BF16, s3) for i in range(2)]
            ytmps = [c.sb(f"ytmp{i}", [128, TT], F32, s3) for i in range(2)]
            kb.dma("sp", wr[:], io["router_w"].rearrange("(k p) n -> p k n", p=128), writes=["wr"])
        with ExitStack() as s3a:
            hF = c.sb("hF", [128, 8, TT], F32, s3a) if E > 1 else None
            for t in range(NTT):
                t0 = t * TT
                rmsnorm_tile(c, xT, "xT", t0, TT, vecs[:, V_FFN:V_FFN + 8], tmp, hTall[:, :, t0:t0 + TT], f"hA{t}", out_f=hF)
                if E > 1:
                    for s in range(4):
                        p, pk = c.ps()
                        for k in range(8):
                            mm(c, p[:, 0:8], hF[:, k, s * 128:(s + 1) * 128], wr[:, k, :], k == 0, k == 7, [(f"hA{t}_f", k), "wr"], [pk])
                        kb.op("dve", lambda e, s=s, p=p: e.tensor_copy(out=lg[:, s, :], in_=p[:, 0:8]), reads=[pk], writes=[("lg", s)])
                        kb.op("dve", lambda e, s=s: e.max(out=top8[:, s, :], in_=lg[:, s, :]), reads=[("lg", s)], writes=[("top8", s)])
                        kb.op("dve", lambda e, s=s: e.tensor_scalar(out=gsc[:, s, 0:1], in0=top8[:, s, 0:1], scalar1=-1.0, scalar2=None, op0=ALU.mult),
                              reads=[("top8", s)], writes=[("gsc", s, 0)])
                        kb.op("act", lambda e, s=s: e.activation(out=gsc[:, s, 1:2], in_=top8[:, s, 1:2], func=AF.Exp, bias=gsc[:, s, 0:1]),
                              reads=[("top8", s), ("gsc", s, 0)], writes=[("gsc", s, 1)])
                        kb.op("dve", lambda e, s=s: e.tensor_scalar(out=gsc[:, s, 1:2], in0=gsc[:, s, 1:2], scalar1=1.0, scalar2=None, op0=ALU.add),
                              reads=[("gsc", s, 1)], writes=[("gsc", s, 1)])
                        kb.op("dve", lambda e, s=s: e.reciprocal(out=gsc[:, s, 1:2], in_=gsc[:, s, 1:2]),
                              reads=[("gsc", s, 1)], writes=[("gsc", s, 1)])
                        gi = t * 4 + s
                        kb.op("act", lambda e, s=s, gi=gi: e.activation(out=gts[:, gi, :], in_=lg[:, s, :], func=AF.Exp, bias=gsc[:, s, 0:1]),
                              reads=[("lg", s), ("gsc", s, 0)], writes=[("gts", gi)])
                        kb.op("dve", lambda e, s=s: e.tensor_scalar(out=lg[:, s, :], in0=lg[:, s, :], scalar1=top8[:, s, 1:2], scalar2=None, op0=ALU.is_ge),
                              reads=[("lg", s), ("top8", s)], writes=[("lg", s)])
                        kb.op("dve", lambda e, s=s, gi=gi: e.scalar_tensor_tensor(out=gts[:, gi, :], in0=gts[:, gi, :], scalar=gsc[:, s, 1:2], in1=lg[:, s, :],
                                                                                  op0=ALU.mult, op1=ALU.mult),
                              reads=[("gts", gi), ("gsc", s, 1), ("lg", s)], writes=[("gts", gi)])
        hkeys = lambda t: [(f"hA{t}", k) for k in range(8)]
        groups = [(0, 8), (8, 16), (16, 22)]
        wgu_i = 0
        wd_i = 0
        sil_i = 0
        for ex in range(E):
            if E > 1:
                gb = gB[ex % 2]
                gbk = f"gB{ex % 2}"
                for gi in range(16):
                    kb.op("dve", lambda e, gi=gi, ex=ex: e.tensor_scalar(out=dgate[:], in0=c.ident_f[:], scalar1=gts[:, gi, ex:ex + 1],
                                                                         scalar2=None, op0=ALU.mult),
                          reads=["ident_f", ("gts", gi)], writes=["dgate"])
                    p, pk = c.ps()
                    mm(c, p[:, 0:128], c.ones_f[:], dgate[:], True, True, ["ones_f", "dgate"], [pk])
                    kb.op("act", lambda e, gi=gi, p=p, gb=gb: e.activation(out=gb[:, gi * 128:(gi + 1) * 128], in_=p[:, 0:128], func=AF.Copy),
                          reads=[pk], writes=[(gbk, gi // 4)])
            for fa, fb in groups:
                nfg = fb - fa
                for f0 in range(fa, fb, 2):
                    nf = min(2, fb - f0)
                    sg, su = wgu[wgu_i % 3], wgu[(wgu_i + 1) % 3]
                    sgk, suk = f"wgu{wgu_i % 3}", f"wgu{(wgu_i + 1) % 3}"
                    wgu_i += 2
                    kb.dma("pool", sg[:, :, 0:nf * 128], io["w_gate"][ex][:, f0 * 128:(f0 + nf) * 128].rearrange("(k p) n -> p k n", p=128), writes=[sgk])
                    kb.dma("pool", su[:, :, 0:nf * 128], io["w_up"][ex][:, f0 * 128:(f0 + nf) * 128].rearrange("(k p) n -> p k n", p=128), writes=[suk])
                    for fi in range(nf):
                        fl = f0 + fi - fa
                        for t in range(NTT):
                            ts_ = slice(t * TT, (t + 1) * TT)
                            pg, pgk = c.ps()
                            pu, puk = c.ps()
                            for k in range(8):
                                mm(c, pg[:, :], sg[:, k, fi * 128:(fi + 1) * 128], hTall[:, k, ts_], k == 0, k == 7, [sgk, (f"hA{t}", k)], [pgk])
                            for k in range(8):
                                mm(c, pu[:, :], su[:, k, fi * 128:(fi + 1) * 128], hTall[:, k, ts_], k == 0, k == 7, [suk, (f"hA{t}", k)], [puk])
                            sl = sil[sil_i % 2]
                            slk = f"sil{sil_i % 2}"
                            sil_i += 1
                            kb.op("act", lambda e, pg=pg, sl=sl: e.activation(out=sl[:], in_=pg[:, :], func=AF.Silu), reads=[pgk], writes=[slk])
                            kb.op("dve", lambda e, pu=pu, sl=sl, fl=fl, ts_=ts_: e.tensor_tensor(out=actT[:, fl, ts_], in0=pu[:, :], in1=sl[:], op=ALU.mult),
                                  reads=[puk, slk], writes=[("actT", fl, t)])
                slots = []
                for fl in range(nfg):
                    f = fa + fl
                    wd = wdr[wd_i % 11]
                    wdk = f"wdr{wd_i % 11}"
                    wd_i += 1
                    kb.dma("pool", wd[:], io["w_down"][ex][f * 128:(f + 1) * 128, :], writes=[wdk])
                    slots.append((wd, wdk))
                for t in range(NTT):
                    ts_ = slice(t * TT, (t + 1) * TT)
                    for d in range(8):
                        p, pk = c.ps()
                        for fl in range(nfg):
                            wd, wdk = slots[fl]
                            mm(c, p[:, :], wd[:, d * 128:(d + 1) * 128], actT[:, fl, ts_], fl == 0, fl == nfg - 1, [wdk, ("actT", fl, t)], [pk])
                        if E > 1:
                            ytmp = ytmps[d % 2]
                            ytk = f"ytmp{d % 2}"
                            kb.op("dve", lambda e, p=p, gb=gb, ytmp=ytmp, ts_=ts_: e.tensor_tensor(out=ytmp[:], in0=p[:, :], in1=gb[:, ts_], op=ALU.mult),
                                  reads=[pk, (gbk, t)], writes=[ytk])
                            kb.op("pool", lambda e, d=d, ts_=ts_, ytmp=ytmp: e.tensor_tensor(out=xT[:, d, ts_], in0=xT[:, d, ts_], in1=ytmp[:], op=ALU.add),
                                  reads=[ytk, ("xT", d)], writes=[("xT", d)])
                        else:
                            kb.op("dve", lambda e, d=d, p=p, ts_=ts_: e.tensor_tensor(out=xT[:, d, ts_], in0=xT[:, d, ts_], in1=p[:, :], op=ALU.add),
                                  reads=[pk, ("xT", d)], writes=[("xT", d)])
    c.barrier()

    with ExitStack() as s4:
        hT = c.sb("hT", [128, 8, TT], BF16, s4)
        if not last:
            for t in range(NTT):
                t0 = t * TT
                rmsnorm_tile(c, xT, "xT", t0, TT, vecs[:, V_NEXT:V_NEXT + 8], tmp, hT, "hT")
                h_store(c, io["h_next"], hT, t0, TT, [("hT", k) for k in range(8)], ["h_next_d"])
                if t == NTT - 1 and "tail_next" in io:
                    kb.dma("sp", io["tail_next"].rearrange("(k p) n -> p k n", p=128), hT[:, :, TT - 32:TT],
                           reads=[("hT", k) for k in range(8)], writes=["tail_next_d"])
        else:
            hF2 = c.sb("hF2", [128, 8, TT], F32, s4)
            otm = c.sb("otm", [128, 4, D], F32, s4)
            for t in range(NTT):
                t0 = t * TT
                rmsnorm_tile(c, xT, "xT", t0, TT, vecs[:, V_NEXT:V_NEXT + 8], tmp, hT, "hT", out_f=hF2)
                for s in range(4):
                    for kk in range(2):
                        p, pk = c.ps()
                        for k4 in range(4):
                            k = kk * 4 + k4
                            kb.op("pe", lambda e, k=k, k4=k4, s=s, p=p: e.transpose(out=p[:, k4 * 128:(k4 + 1) * 128],
                                                                                    in_=hF2[:, k, s * 128:(s + 1) * 128], identity=c.ident_f[:]),
                                  reads=[("hT_f", k), "ident_f"], writes=[pk])
                        kb.op("act", lambda e, s=s, kk=kk, p=p: e.activation(out=otm[:, s, kk * 512:(kk + 1) * 512], in_=p[:, :], func=AF.Copy),
                              reads=[pk], writes=[("otm", s)])
                kb.dma("sp", io["out"][t0:t0 + TT, :].rearrange("(s p) d -> p s d", p=128), otm[:],
                       reads=[("otm", s) for s in range(4)])
    c.barrier()
    vec_st.close()


from contextlib import ExitStack as _ES
import ml_dtypes as _mld

NPBF = _mld.bfloat16


def build_T(E, last, dbg_stage=0):
    nc = bass.Bass("TRN2", target_bir_lowering=False)
    io = {}

    def din(name, shape, dt=F32):
        io[name] = nc.dram_tensor(name, shape, dt, kind="ExternalInput").ap()

    def dout(name, shape, dt=F32):
        io[name] = nc.dram_tensor(name, shape, dt, kind="ExternalOutput").ap()

    din("xT_in", [D, NT]); din("h_own", [D, NT], BF16); din("h_halo", [D, 32], BF16)
    din("catm", [4, 64, NT], BF16); din("catf", [4, 128, NT], BF16); din("mem", [256, D])
    din("vecs", [128, NV_T]); din("w_c", [D, 512]); din("w_out", [D, D]); din("w_q", [D, 512])
    din("w_kv", [D, D]); din("w_o", [512, D])
    din("w_gate", [E, D, DFF]); din("w_up", [E, D, DFF]); din("w_down", [E, DFF, D])
    if E > 1:
        din("router_w", [D, 8])
    if last:
        dout("out", [NT, D])
    else:
        dout("xT_out", [D, NT]); dout("h_next", [D, NT], BF16)
    io["dbg_stage"] = dbg_stage
    with _ES() as st:
        c = Ctx(nc, st)
        c.setup()
        xT = c.sb("xT", [128, 8, NT], F32)
        c.kb.dma("sp", xT[:], io["xT_in"].rearrange("(k p) n -> p k n", p=128), writes=[("xT", k) for k in range(8)])
        phase_T(c, io, E, last and not dbg_stage, xT)
        evs = []
        if not last or dbg_stage:
            key = "xT_out" if not last else "out"
            if last:
                io["xT_dbg"] = None
            evs.append(c.kb.dma("sp", io["xT_out"].rearrange("(k p) n -> p k n", p=128), xT[:],
                                reads=[("xT", k) for k in range(8)]))
        c.barrier()
        c.kb.flush()
    return nc


def vecs_T(inp, l, last):
    v = np.zeros((128, NV_T), np.float32)
    fm = lambda w: np.asarray(w, np.float32).reshape(-1, 128).T
    v[:, 0:8] = fm(inp["norm_xattn_w"][l]); v[:, 8:16] = fm(inp["norm_mem_w"][l]); v[:, 16:24] = fm(inp["norm_ffn_w"][l])
    v[:, 24:32] = fm(inp["norm_final_w"]) if last else fm(inp["norm_mix_w"][l + 1])
    v[:, 32:34] = fm(inp["conf_conv_b"][l]); v[:, 34:36] = fm(inp["conf_ln_w"][l]); v[:, 36:38] = fm(inp["conf_ln_b"][l])
    cw = np.asarray(inp["conf_conv_w"][l], np.float32)
    for j in range(31):
        v[:, 38 + 2 * j:40 + 2 * j] = fm(cw[j])
    return v


NCH = SEQ // 64
NKT = SEQ // 128
NQT = SEQ // TT
GRP = 4


def AP3(t, off, dims):
    return bass.AP(t[:].tensor, off, [list(d) for d in dims])


def log_sigmoid_tile(c, x, out, tmp1, tmp2, bias_ap, keys):
    kb = c.kb
    kx, ko, k1, k2 = keys
    kb.op("dve", lambda e: e.tensor_scalar(out=x, in0=x, scalar1=bias_ap, scalar2=None, op0=ALU.add), reads=[kx, "vecsM"], writes=[kx])
    kb.op("dve", lambda e: e.tensor_scalar(out=tmp1, in0=x, scalar1=-1.0, scalar2=None, op0=ALU.mult), reads=[kx], writes=[k1])
    kb.op("dve", lambda e: e.tensor_tensor(out=tmp1, in0=tmp1, in1=x, op=ALU.max), reads=[kx, k1], writes=[k1])
    kb.op("act", lambda e: e.activation(out=tmp1, in_=tmp1, func=AF.Exp, scale=-1.0), reads=[k1], writes=[k1])
    kb.op("dve", lambda e: e.tensor_scalar(out=tmp1, in0=tmp1, scalar1=1.0, scalar2=None, op0=ALU.add), reads=[k1], writes=[k1])
    kb.op("act", lambda e: e.activation(out=tmp1, in_=tmp1, func=AF.Ln), reads=[k1], writes=[k1])
    kb.op("dve", lambda e: e.tensor_scalar(out=tmp2, in0=x, scalar1=0.0, scalar2=None, op0=ALU.min), reads=[kx], writes=[k2])
    kb.op("dve", lambda e: e.tensor_tensor(out=out, in0=tmp2, in1=tmp1, op=ALU.subtract), reads=[k1, k2], writes=[ko])


def phase_M_mlstm(c, io):
    nc, kb = c.nc, c.kb
    from contextlib import ExitStack
    with ExitStack() as s0:
        vm = c.sb("vecsM_sb", [128, 16], F32, s0)
        wml = c.sb("wml", [128, 8, 258], BF16, s0)
        qT = c.sb("m_qT", [64, SEQ], BF16, s0)
        kT = c.sb("m_kT", [64, SEQ], BF16, s0)
        Vaug = c.sb("m_Vaug", [64, NCH, 65], BF16, s0)
        og = c.sb("m_og", [64, SEQ], BF16, s0)
        iC = c.sb("m_iC", [128, 64], F32, s0)
        fC = c.sb("m_fC", [128, 64], F32, s0)
        eps_t = c.sb("m_eps", [128, 1], F32, s0)
        kb.dma("sp", vm[:], io["vecsM"], writes=["vecsM"])
        kb.dma("pool", wml[:], io["w_ml"].rearrange("(k p) n -> p k n", p=128), writes=["wml"])
        kb.op("pool", lambda e: e.memset(Vaug[:, :, 64:65], 1.0), writes=["Vaug1"])
        kb.op("pool", lambda e: e.memset(eps_t[:], EPS), writes=["m_eps"])
        with ExitStack() as s1:
            hTb = [c.sb(f"m_hT{i}", [128, 8, TT], BF16, s1) for i in range(2)]
            zq = c.sb("m_zq", [64, TT + 3], F32, s1)
            zk = c.sb("m_zk", [64, TT + 3], F32, s1)
            cacc = [c.sb(f"m_cacc{i}", [64, TT], F32, s1) for i in range(2)]
            vt = c.sb("m_vt", [64, TT], F32, s1)
            rows = [c.sb(f"m_rows{i}", [2, TT], F32, s1) for i in range(2)]
            kb.op("pool", lambda e: e.memset(zq[:, 0:3], 0.0), writes=["zq"])
            kb.op("pool", lambda e: e.memset(zk[:, 0:3], 0.0), writes=["zk"])
            for tt in range(NQT):
                j, off = tt // 4, (tt % 4) * TT
                hT = hTb[tt % 2]
                hk = f"m_hT{tt % 2}"
                tok = slice(tt * TT, (tt + 1) * TT)
                h_load(c, io["hT_all"], hT, off, TT, [hk], j=j)
                for nm, z, col0, vc, dst in (("q", zq, 0, 0, qT), ("k", zk, 64, 5, kT)):
                    p, pk = c.ps()
                    for k in range(8):
                        mm(c, p[0:64, :], wml[:, k, col0:col0 + 64], hT[:, k, :], k == 0, k == 7, ["wml", hk], [pk])
                    zkey = "z" + nm
                    kb.op("act", lambda e, z=z, p=p: e.activation(out=z[:, 3:TT + 3], in_=p[0:64, :], func=AF.Copy), reads=[pk], writes=[zkey])
                    ca = cacc[0 if nm == "q" else 1]
                    ck = "cacc" + nm
                    kb.op("dve", lambda e, z=z, ca=ca, vc=vc: e.tensor_scalar(out=ca[:], in0=z[:, 0:TT], scalar1=vm[0:64, vc:vc + 1], scalar2=vm[0:64, vc + 4:vc + 5],
                                                                              op0=ALU.mult, op1=ALU.add), reads=[zkey, "vecsM"], writes=[ck])
                    for jj in range(1, 4):
                        kb.op("dve", lambda e, z=z, ca=ca, vc=vc, jj=jj: e.scalar_tensor_tensor(out=ca[:], in0=z[:, jj:jj + TT], scalar=vm[0:64, vc + jj:vc + jj + 1], in1=ca[:],
                                                                                                 op0=ALU.mult, op1=ALU.add), reads=[zkey, ck, "vecsM"], writes=[ck])
                    kb.op("act", lambda e, ca=ca, dst=dst, tok=tok: e.activation(out=dst[:, tok], in_=ca[:], func=AF.Silu), reads=[ck], writes=[("m_" + nm + "T", tt)])
                    kb.op("dve", lambda e, z=z: e.tensor_copy(out=z[:, 0:3], in_=z[:, TT:TT + 3]), reads=[zkey], writes=[zkey])
                p, pk = c.ps()
                for k in range(8):
                    mm(c, p[0:64, :], wml[:, k, 128:192], hT[:, k, :], k == 0, k == 7, ["wml", hk], [pk])
                kb.op("act", lambda e, p=p: e.activation(out=vt[:], in_=p[0:64, :], func=AF.Copy), reads=[pk], writes=["m_vt"])
                p2, p2k = c.ps()
                for ci in range(8):
                    kb.op("pe", lambda e, ci=ci, p2=p2: e.transpose(out=p2[0:64, ci * 64:(ci + 1) * 64], in_=vt[:, ci * 64:(ci + 1) * 64], identity=c.ident_f[0:64, 0:64]),
                          reads=["m_vt", "ident_f"], writes=[p2k])
                kb.op("dve", lambda e, p2=p2, tt=tt: e.tensor_copy(out=Vaug[:, tt * 8:(tt + 1) * 8, 0:64], in_=p2[0:64, :].rearrange("p (c d) -> p c d", d=64)),
                      reads=[p2k], writes=[("Vaug", tt)])
                p, pk = c.ps()
                for k in range(8):
                    mm(c, p[0:64, :], wml[:, k, 192:256], hT[:, k, :], k == 0, k == 7, ["wml", hk], [pk])
                kb.op("act", lambda e, p=p, tok=tok: e.activation(out=og[:, tok], in_=p[0:64, :], func=AF.Sigmoid), reads=[pk], writes=[("og", tt)])
                p, pk = c.ps()
                for k in range(8):
                    mm(c, p[0:2, :], wml[:, k, 256:258], hT[:, k, :], k == 0, k == 7, ["wml", hk], [pk])
                rw = rows[tt % 2]
                rk = f"m_rows{tt % 2}"
                kb.op("act", lambda e, p=p, rw=rw: e.activation(out=rw[:], in_=p[0:2, :], func=AF.Copy), reads=[pk], writes=[rk])
                kb.dma("sp", iC[tt * 8:(tt + 1) * 8, :], AP3(rw, 0, [[TT, 1], [64, 8], [1, 64]]), reads=[rk], writes=["iC"])
                kb.dma("sp", fC[tt * 8:(tt + 1) * 8, :], AP3(rw, TT, [[TT, 1], [64, 8], [1, 64]]), reads=[rk], writes=["fC"])
        c.barrier()
        Uall = c.sb("m_Uall", [64, 65, NCH], F32, s0)
        wgT = c.sb("m_wgT", [64, NCH], F32, s0)
        flT = c.sb("m_flT", [64, NCH], F32, s0)
        dB = c.sb("m_dB", [64, NCH], F32, s0)
        dB0 = c.sb("m_dB0", [64, NCH], F32, s0)
        with ExitStack() as s2:
            t1 = c.sb("g_t1", [128, 64], F32, s2)
            t2 = c.sb("g_t2", [128, 64], F32, s2)
            lf = c.sb("g_lf", [128, 64], F32, s2)
            bb = c.sb("g_b", [128, 64], F32, s2)
            aa = c.sb("g_a", [128, 64], F32, s2)
            AA = c.sb("g_A", [128, 64], F32, s2)
            MM = c.sb("g_M", [128, 64], F32, s2)
            wg = c.sb("g_wg", [128, 64], F32, s2)
            fl = c.sb("g_fl", [128, 64], F32, s2)
            on = c.sb("g_on", [128, 64], F32, s2)
            r1 = c.sb("g_r1", [1, 128], F32, s2)
            r2 = c.sb("g_r2", [1, 128], F32, s2)
            r3 = c.sb("g_r3", [1, 128], F32, s2)
            r4 = c.sb("g_r4", [1, 128], F32, s2)
            mcol = c.sb("g_mcol", [128, 1], F32, s2)
            nM63 = c.sb("g_nM63", [128, 1], F32, s2)
            dec = c.sb("g_dec", [128, 1], F32, s2)
            dgd = c.sb("g_dgd", [128, 128], F32, s2)
            Xb = [c.sb(f"g_X{i}", [128, 8, 64], F32, s2) for i in range(2)]
            kw32 = [c.sb(f"g_kw32{i}", [64, TT], F32, s2) for i in range(2)]
            kwTok = c.sb("g_kwTok", [64, NCH, 64], BF16, s2)
            log_sigmoid_tile(c, fC[:], lf[:], t1[:], t2[:], vm[:, 12:13], ("fC", "g_lf", "g_t1", "g_t2"))
            kb.op("dve", lambda e: e.tensor_scalar(out=iC[:], in0=iC[:], scalar1=vm[:, 11:12], scalar2=None, op0=ALU.add), reads=["iC", "vecsM"], writes=["iC"])
            kb.op("pool", lambda e: e.memset(on[:], 1.0), writes=["g_on"])
            kb.op("dve", lambda e: e.tensor_tensor_scan(out=bb[:], data0=on[:], data1=lf[:], initial=0.0, op0=ALU.mult, op1=ALU.add),
                  reads=["g_on", "g_lf"], writes=["g_b"])
            kb.op("dve", lambda e: e.tensor_tensor(out=aa[:], in0=iC[:], in1=bb[:], op=ALU.subtract), reads=["iC", "g_b"], writes=["g_a"])
            kb.op("dve", lambda e: e.tensor_tensor_scan(out=AA[:], data0=aa[:], data1=aa[:], initial=-1e30, op0=ALU.max, op1=ALU.max),
                  reads=["g_a"], writes=["g_A"])
            p, pk = c.ps()
            kb.op("pe", lambda e, p=p: e.transpose(out=p[0:1, 0:128], in_=AA[:, 63:64], identity=c.ident_f[:]), reads=["g_A", "ident_f"], writes=[pk])
            kb.op("pe", lambda e, p=p: e.transpose(out=p[0:1, 128:256], in_=bb[:, 63:64], identity=c.ident_f[:]), reads=["g_b", "ident_f"], writes=[pk])
            kb.op("dve", lambda e, p=p: e.tensor_copy(out=r1[:], in_=p[0:1, 0:128]), reads=[pk], writes=["g_r1"])
            kb.op("dve", lambda e, p=p: e.tensor_copy(out=r2[:], in_=p[0:1, 128:256]), reads=[pk], writes=["g_r2"])
            kb.op("dve", lambda e: e.tensor_tensor_scan(out=r3[:], data0=r1[:], data1=r2[:], initial=0.0, op0=ALU.max, op1=ALU.add),
                  reads=["g_r1", "g_r2"], writes=["g_r3"])
            kb.op("pool", lambda e: e.memset(r4[:, 0:1], 0.0), writes=["g_r4a"])
            kb.op("dve", lambda e: e.tensor_copy(out=r4[:, 1:128], in_=r3[:, 0:127]), reads=["g_r3"], writes=["g_r4b"])
            p, pk = c.ps()
            kb.op("pe", lambda e, p=p: e.transpose(out=p[:, 0:1], in_=r4[:], identity=c.ident_f[0:1, 0:1]), reads=["g_r4a", "g_r4b", "ident_f"], writes=[pk])
            kb.op("dve", lambda e, p=p: e.tensor_copy(out=mcol[:], in_=p[:, 0:1]), reads=[pk], writes=["g_mcol"])
            kb.op("dve", lambda e: e.tensor_scalar(out=MM[:], in0=AA[:], scalar1=mcol[:, 0:1], scalar2=None, op0=ALU.max), reads=["g_A", "g_mcol"], writes=["g_M"])
            kb.op("dve", lambda e: e.tensor_scalar(out=nM63[:], in0=MM[:, 63:64], scalar1=-1.0, scalar2=None, op0=ALU.mult), reads=["g_M"], writes=["g_nM63"])
            kb.op("act", lambda e: e.activation(out=wg[:], in_=aa[:], func=AF.Exp, bias=nM63[:, 0:1]), reads=["g_a", "g_nM63"], writes=["g_wg"])
            kb.op("act", lambda e: e.activation(out=dec[:], in_=mcol[:], func=AF.Exp, bias=nM63[:, 0:1]), reads=["g_mcol", "g_nM63"], writes=["g_dec"])
            kb.op("act", lambda e: e.activation(out=fl[:], in_=bb[:], func=AF.Exp, scale=-1.0, bias=nM63[:, 0:1]), reads=["g_b", "g_nM63"], writes=["g_fl"])
            p, pk = c.ps()
            kb.op("pe", lambda e, p=p: e.transpose(out=p[0:64, 0:128], in_=wg[:], identity=c.ident_f[:]), reads=["g_wg", "ident_f"], writes=[pk])
            kb.op("pe", lambda e, p=p: e.transpose(out=p[0:64, 128:256], in_=fl[:], identity=c.ident_f[:]), reads=["g_fl", "ident_f"], writes=[pk])
            kb.op("dve", lambda e, p=p: e.tensor_copy(out=wgT[:], in_=p[0:64, 0:128]), reads=[pk], writes=["m_wgT"])
            kb.op("dve", lambda e, p=p: e.tensor_copy(out=flT[:], in_=p[0:64, 128:256]), reads=[pk], writes=["m_flT"])
            kb.op("dve", lambda e: e.tensor_scalar(out=dgd[:], in0=c.ident_f[:], scalar1=dec[:, 0:1], scalar2=None, op0=ALU.mult), reads=["ident_f", "g_dec"], writes=["g_dgd"])
            p, pk = c.ps()
            mm(c, p[0:64, 0:128], c.ones_f[:, 0:64], dgd[:], True, True, ["ones_f", "g_dgd"], [pk])
            kb.op("dve", lambda e, p=p: e.tensor_copy(out=dB[:], in_=p[0:64, 0:128]), reads=[pk], writes=["m_dB"])
            kb.op("dve", lambda e, p=p: e.tensor_copy(out=dB0[:], in_=p[0:64, 0:128]), reads=[pk], writes=["m_dB0"])
            kb.op("pool", lambda e: e.memset(dB0[:, 0:1], 0.0), reads=["m_dB0"], writes=["m_dB0"])
            for tt in range(NQT):
                tok = slice(tt * TT, (tt + 1) * TT)
                X = Xb[tt % 2]
                Xk = f"g_X{tt % 2}"
                kb.op("dve", lambda e, X=X, tt=tt: e.tensor_tensor(out=X[:], in0=AP3(c.ident_f, 8 * tt, [[128, 128], [1, 8], [0, 64]]),
                                                                   in1=AP3(wg, 0, [[64, 128], [0, 8], [1, 64]]), op=ALU.mult),
                      reads=["ident_f", "g_wg"], writes=[Xk])
                p, pk = c.ps()
                mm(c, p[0:64, :], c.ones_f[:, 0:64], X[:].rearrange("p c s -> p (c s)"), True, True, ["ones_f", Xk], [pk])
                k32 = kw32[tt % 2]
                k32k = f"g_kw32{tt % 2}"
                kb.op("dve", lambda e, p=p, k32=k32, tok=tok: e.scalar_tensor_tensor(out=k32[:], in0=kT[:, tok], scalar=0.125, in1=p[0:64, :], op0=ALU.mult, op1=ALU.mult),
                      reads=[pk, ("m_kT", tt)], writes=[k32k])
                kb.op("act", lambda e, k32=k32, tok=tok: e.activation(out=kT[:, tok], in_=k32[:], func=AF.Copy), reads=[k32k], writes=[("m_kT", tt)])
                p2, p2k = c.ps()
                for ci in range(8):
                    kb.op("pe", lambda e, ci=ci, p2=p2, k32=k32: e.transpose(out=p2[0:64, ci * 64:(ci + 1) * 64], in_=k32[:, ci * 64:(ci + 1) * 64], identity=c.ident_f[0:64, 0:64]),
                          reads=[k32k, "ident_f"], writes=[p2k])
                kb.op("dve", lambda e, p2=p2, tt=tt: e.tensor_copy(out=kwTok[:, tt * 8:(tt + 1) * 8, :], in_=p2[0:64, :].rearrange("p (c d) -> p c d", d=64)),
                      reads=[p2k], writes=[("kwTok", tt)])
            for g in range(NCH // GRP):
                p, pk = c.ps()
                for ci in range(GRP):
                    ch = g * GRP + ci
                    mm(c, p[0:64, ci * 65:(ci + 1) * 65], kwTok[:, ch, :], Vaug[:, ch, :], True, True, [("kwTok", ch // 8), ("Vaug", ch // 8), "Vaug1"], [pk])
                kb.op("dve", lambda e, p=p, g=g: e.tensor_copy(out=AP3(Uall, g * GRP, [[65 * NCH, 64], [1, GRP], [NCH, 65]]),
                                                               in_=p[0:64, 0:GRP * 65].rearrange("p (c d) -> p c d", d=65)),
                      reads=[pk], writes=["Uall"])
        c.barrier()
        Cn = Uall
        Eb = c.sb("m_E", [64, NCH, 65], BF16, s0)
        for dv in range(65):
            kb.op("dve", lambda e, dv=dv: e.tensor_tensor_scan(out=Cn[:, dv, :], data0=dB0[:], data1=Uall[:, dv, :], initial=0.0, op0=ALU.mult, op1=ALU.add),
                  reads=["m_dB0", "Uall"], writes=[("Cn", dv), "Uall"])
        kb.op("pool", lambda e: e.memset(Eb[:, 0:1, :], 0.0), writes=["E0"])
        kb.op("dve", lambda e: e.tensor_tensor(out=Eb[:, 1:NCH, :], in0=AP3(Cn, 0, [[65 * NCH, 64], [1, NCH - 1], [NCH, 65]]),
                                               in1=AP3(dB, 1, [[NCH, 64], [1, NCH - 1], [0, 65]]), op=ALU.mult),
              reads=[("Cn", dv) for dv in range(65)] + ["m_dB"], writes=["E"])
        with ExitStack() as s4:
            mask = c.sb("o_mask", [64, 64], F32, s4)
            sT = [c.sb(f"o_sT{i}", [64, GRP * 64], BF16, s4) for i in range(2)]
            den = c.sb("o_den", [64, GRP], F32, s4)
            hn = c.sb("o_hn", [64, GRP, 64], F32, s4)
            hsq = c.sb("o_hsq", [64, GRP, 64], F32, s4)
            ss = c.sb("o_ss", [64, GRP], F32, s4)
            cst = [c.sb(f"o_cst{i}", [64, GRP * 64], BF16, s4) for i in range(2)]
            kb.op("pool", lambda e: e.memset(mask[:], 1.0), writes=["o_mask"])
            kb.op("pool", lambda e: e.affine_select(out=mask[:], in_=mask[:], pattern=[[1, 64]], compare_op=ALU.is_ge, fill=0.0, base=0, channel_multiplier=-1),
                  reads=["o_mask"], writes=["o_mask"])
            for g in range(NCH // GRP):
                c0 = g * GRP
                p, pk = c.ps()
                for ci in range(GRP):
                    ch = c0 + ci
                    cs = slice(ch * 64, (ch + 1) * 64)
                    mm(c, p[0:64, ci * 64:(ci + 1) * 64], kT[:, cs], qT[:, cs], True, True, [("m_kT", ch // 8), ("m_qT", ch // 8)], [pk])
                st_ = sT[g % 2]
                stk = f"o_sT{g % 2}"
                kb.op("dve", lambda e, p=p, st_=st_: e.tensor_tensor(out=st_[:].rearrange("p (c t) -> p c t", t=64), in0=p[0:64, 0:GRP * 64].rearrange("p (c t) -> p c t", t=64),
                                                                     in1=AP3(mask, 0, [[64, 64], [0, GRP], [1, 64]]), op=ALU.mult),
                      reads=[pk, "o_mask"], writes=[stk])
                po, pok = c.ps()
                for ci in range(GRP):
                    ch = c0 + ci
                    cs = slice(ch * 64, (ch + 1) * 64)
                    mm(c, po[0:64, ci * 65:(ci + 1) * 65], st_[:, ci * 64:(ci + 1) * 64], Vaug[:, ch, :], True, False, [stk, ("Vaug", ch // 8), "Vaug1"], [pok])
                    mm(c, po[0:64, ci * 65:(ci + 1) * 65], qT[:, cs], Eb[:, ch, :], False, True, [("m_qT", ch // 8), "E", "E0"], [pok])
                po3 = po[0:64, 0:GRP * 65].rearrange("p (c d) -> p c d", d=65)
                den3 = den[:].rearrange("p (c o) -> p c o", o=1)
                kb.op("dve", lambda e, po3=po3, den3=den3: e.tensor_scalar(out=den3, in0=po3[:, :, 64:65], scalar1=-1.0, scalar2=None, op0=ALU.mult),
                      reads=[pok], writes=["o_den"])
                kb.op("dve", lambda e, po3=po3, den3=den3: e.tensor_tensor(out=den3, in0=po3[:, :, 64:65], in1=den3, op=ALU.max),
                      reads=[pok, "o_den"], writes=["o_den"])
                kb.op("dve", lambda e, c0=c0: e.tensor_tensor(out=den[:], in0=den[:], in1=flT[:, c0:c0 + GRP], op=ALU.max),
                      reads=["o_den", "m_flT"], writes=["o_den"])
                kb.op("dve", lambda e: e.reciprocal(out=den[:], in_=den[:]), reads=["o_den"], writes=["o_den"])
                kb.op("dve", lambda e, po3=po3: e.tensor_tensor(out=hn[:], in0=po3[:, :, 0:64], in1=AP3(den, 0, [[GRP, 64], [1, GRP], [0, 64]]), op=ALU.mult),
                      reads=[pok, "o_den"], writes=["o_hn"])
                kb.op("act", lambda e: e.activation(out=hsq[:], in_=hn[:], func=AF.Square), reads=["o_hn"], writes=["o_hsq"])
                kb.op("dve", lambda e: e.tensor_reduce(out=ss[:], in_=hsq[:], axis=AX.X, op=ALU.add), reads=["o_hsq"], writes=["o_ss"])
                kb.op("act", lambda e: e.activation(out=ss[:], in_=ss[:], func=AF.Sqrt, scale=1.0 / 64, bias=eps_t[0:64, 0:1]), reads=["o_ss", "m_eps"], writes=["o_ss"])
                kb.op("dve", lambda e: e.reciprocal(out=ss[:], in_=ss[:]), reads=["o_ss"], writes=["o_ss"])
                kb.op("dve", lambda e: e.tensor_tensor(out=hn[:], in0=hn[:], in1=AP3(ss, 0, [[GRP, 64], [1, GRP], [0, 64]]), op=ALU.mult),
                      reads=["o_hn", "o_ss"], writes=["o_hn"])
                pt, ptk = c.ps()
                for ci in range(GRP):
                    kb.op("pe", lambda e, ci=ci, pt=pt: e.transpose(out=pt[0:64, ci * 64:(ci + 1) * 64], in_=hn[:, ci, :], identity=c.ident_f[0:64, 0:64]),
                          reads=["o_hn", "ident_f"], writes=[ptk])
                cs_ = cst[g % 2]
                csk = f"o_cst{g % 2}"
                toks = slice(c0 * 64, (c0 + GRP) * 64)
                kb.op("dve", lambda e, pt=pt, cs_=cs_, toks=toks: e.scalar_tensor_tensor(out=cs_[:], in0=pt[0:64, 0:GRP * 64], scalar=vm[0:64, 10:11], in1=og[:, toks],
                                                                                       op0=ALU.mult, op1=ALU.mult),
                      reads=[ptk, "vecsM", ("og", (c0 * 64) // TT)], writes=[csk])
                kb.dma("sp", io["catm_out"][:, toks], cs_[:], reads=[csk])
        c.barrier()


def phase_M_fox(c, io):
    nc, kb = c.nc, c.kb
    from contextlib import ExitStack
    NEG = -30000.0
    with ExitStack() as s0:
        vm = c.sb("vecsMf_sb", [128, 16], F32, s0)
        wfx = c.sb("wfx", [128, 8, 386], BF16, s0)
        fq = [c.sb(f"f_q{h}", [64, SEQ], BF16, s0) for h in range(2)]
        fk = [c.sb(f"f_k{h}", [64, SEQ], BF16, s0) for h in range(2)]
        fV = [c.sb(f"f_V{h}", [128, NKT, 65], BF16, s0) for h in range(2)]
        fC = [c.sb(f"f_C{h}", [64, 128], F32, s0) for h in range(2)]
        kb.dma("sp", vm[:], io["vecsM"], writes=["vecsM"])
        kb.dma("pool", wfx[:], io["w_fx"].rearrange("(k p) n -> p k n", p=128), writes=["wfx"])
        for h in range(2):
            kb.op("pool", lambda e, h=h: e.memset(fV[h][:, :, 64:65], 1.0), writes=[("fV1", h)])
        with ExitStack() as s1:
            hTb = [c.sb(f"f_hT{i}", [128, 8, TT], BF16, s1) for i in range(2)]
            vt = c.sb("f_vt", [64, TT], F32, s1)
            rows = [c.sb(f"f_rows{i}", [2, TT], F32, s1) for i in range(2)]
            for tt in range(NQT):
                j, off = tt // 4, (tt % 4) * TT
                hT = hTb[tt % 2]
                hk = f"f_hT{tt % 2}"
                tok = slice(tt * TT, (tt + 1) * TT)
                h_load(c, io["hT_all"], hT, off, TT, [hk], j=j)
                for h in range(2):
                    b0 = h * 192
                    p, pk = c.ps()
                    for k in range(8):
                        mm(c, p[0:64, :], wfx[:, k, b0:b0 + 64], hT[:, k, :], k == 0, k == 7, ["wfx", hk], [pk])
                    kb.op("act", lambda e, p=p, h=h, tok=tok: e.activation(out=fq[h][:, tok], in_=p[0:64, :], func=AF.Copy, scale=0.125), reads=[pk], writes=[("fq", h, tt)])
                    p, pk = c.ps()
                    for k in range(8):
                        mm(c, p[0:64, :], wfx[:, k, b0 + 64:b0 + 128], hT[:, k, :], k == 0, k == 7, ["wfx", hk], [pk])
                    kb.op("act", lambda e, p=p, h=h, tok=tok: e.activation(out=fk[h][:, tok], in_=p[0:64, :], func=AF.Copy), reads=[pk], writes=[("fk", h, tt)])
                    p, pk = c.ps()
                    for k in range(8):
                        mm(c, p[0:64, :], wfx[:, k, b0 + 128:b0 + 192], hT[:, k, :], k == 0, k == 7, ["wfx", hk], [pk])
                    kb.op("act", lambda e, p=p: e.activation(out=vt[:], in_=p[0:64, :], func=AF.Copy), reads=[pk], writes=["f_vt"])
                    p2, p2k = c.ps()
                    for ci in range(4):
                        kb.op("pe", lambda e, ci=ci, p2=p2: e.transpose(out=p2[:, ci * 64:(ci + 1) * 64], in_=vt[:, ci * 128:(ci + 1) * 128], identity=c.ident_f[0:64, 0:64]),
                              reads=["f_vt", "ident_f"], writes=[p2k])
                    kb.op("dve", lambda e, p2=p2, tt=tt, h=h: e.tensor_copy(out=fV[h][:, tt * 4:(tt + 1) * 4, 0:64], in_=p2[:, 0:256].rearrange("p (c d) -> p c d", d=64)),
                          reads=[p2k], writes=[("fV", h, tt)])
                p, pk = c.ps()
                for k in range(8):
                    mm(c, p[0:2, :], wfx[:, k, 384:386], hT[:, k, :], k == 0, k == 7, ["wfx", hk], [pk])
                rw = rows[tt % 2]
                rk = f"f_rows{tt % 2}"
                kb.op("act", lambda e, p=p, rw=rw: e.activation(out=rw[:], in_=p[0:2, :], func=AF.Copy), reads=[pk], writes=[rk])
                for h in range(2):
                    kb.dma("sp", fC[h][tt * 4:(tt + 1) * 4, :], AP3(rw, h * TT, [[TT, 1], [128, 4], [1, 128]]), reads=[rk], writes=[("fC", h)])
        c.barrier()
        ckT = [c.sb(f"f_ckT{h}", [128, NKT], F32, s0) for h in range(2)]
        cC = [c.sb(f"f_cC{h}", [64, 128], F32, s0) for h in range(2)]
        negm = c.sb("f_negm", [128, 4, TT], F32, s0)
        Ls = c.sb("f_Ls", [64, 64], F32, s0)
        with ExitStack() as s2:
            t1 = c.sb("f_t1", [64, 128], F32, s2)
            t2 = c.sb("f_t2", [64, 128], F32, s2)
            lf = c.sb("f_lf", [64, 128], F32, s2)
            on = c.sb("f_on", [64, 128], F32, s2)
            pre = c.sb("f_pre", [64, 1], F32, s2)
            kb.op("pool", lambda e: e.memset(on[:], 1.0), writes=["f_on"])
            kb.op("pool", lambda e: e.memset(Ls[:], 1.0), writes=["f_Ls"])
            kb.op("pool", lambda e: e.affine_select(out=Ls[:], in_=Ls[:], pattern=[[1, 64]], compare_op=ALU.is_ge, fill=0.0, base=-1, channel_multiplier=-1),
                  reads=["f_Ls"], writes=["f_Ls"])
            for r in range(4):
                kb.op("pool", lambda e, r=r: e.memset(negm[:, r, :], 0.0), writes=[("negm", r)])
                kb.op("pool", lambda e, r=r: e.affine_select(out=negm[:, r, :], in_=negm[:, r, :], pattern=[[1, TT]], compare_op=ALU.is_ge, fill=NEG,
                                                             base=-128 * r, channel_multiplier=-1), reads=[("negm", r)], writes=[("negm", r)])
            for h in range(2):
                log_sigmoid_tile(c, fC[h][:], lf[:], t1[:], t2[:], vm[0:64, 13 + h:14 + h], (("fC", h), "f_lf", "f_t1", "f_t2"))
                kb.op("dve", lambda e, h=h: e.tensor_tensor_scan(out=cC[h][:], data0=on[:], data1=lf[:], initial=0.0, op0=ALU.mult, op1=ALU.add),
                      reads=["f_on", "f_lf"], writes=[("cC", h)])
                p, pk = c.ps()
                mm(c, p[0:64, 0:1], Ls[:], cC[h][:, 127:128], True, True, ["f_Ls", ("cC", h)], [pk])
                kb.op("dve", lambda e, p=p: e.tensor_copy(out=pre[:], in_=p[0:64, 0:1]), reads=[pk], writes=["f_pre"])
                kb.op("dve", lambda e, h=h: e.tensor_scalar(out=cC[h][:], in0=cC[h][:], scalar1=pre[:, 0:1], scalar2=None, op0=ALU.add), reads=[("cC", h), "f_pre"], writes=[("cC", h)])
                p, pk = c.ps()
                kb.op("pe", lambda e, p=p, h=h: e.transpose(out=p[:, 0:64], in_=cC[h][:], identity=c.ident_f[0:64, 0:64]), reads=[("cC", h), "ident_f"], writes=[pk])
                kb.op("dve", lambda e, p=p, h=h: e.tensor_scalar(out=ckT[h][:], in0=p[:, 0:64], scalar1=-1.0, scalar2=None, op0=ALU.mult), reads=[pk], writes=[("ckT", h)])
        c.barrier()
        with ExitStack() as s3:
            X = c.sb("f_X", [64, 4, 128], F32, s3)
            cqB = c.sb("f_cqB", [128, TT], F32, s3)
            cqD = c.sb("f_cqD", [128, 4, TT], F32, s3)
            tmpb = [c.sb(f"f_tmp{i}", [128, TT], F32, s3) for i in range(3)]
            pTb = [c.sb(f"f_pT{i}", [128, TT], BF16, s3) for i in range(3)]
            osb = c.sb("f_osb", [65, TT], F32, s3)
            rden = c.sb("f_rden", [64, TT], F32, s3)
            outb = [c.sb(f"f_out{i}", [64, TT], BF16, s3) for i in range(2)]
            it = 0
            for h in range(2):
                for qi in range(NQT):
                    qs = slice(qi * TT, (qi + 1) * TT)
                    kb.op("dve", lambda e, h=h, qi=qi: e.tensor_tensor(out=X[:], in0=AP3(c.ident_f, 4 * qi, [[128, 64], [1, 4], [0, 128]]),
                                                                       in1=AP3(cC[h], 0, [[128, 64], [0, 4], [1, 128]]), op=ALU.mult),
                          reads=["ident_f", ("cC", h)], writes=["f_X"])
                    p, pk = c.ps()
                    mm(c, p[:, :], c.ones_f[0:64, :], X[:].rearrange("p r s -> p (r s)"), True, True, ["ones_f", "f_X"], [pk])
                    kb.op("act", lambda e, p=p: e.activation(out=cqB[:], in_=p[:, :], func=AF.Copy), reads=[pk], writes=["f_cqB"])
                    for r in range(4):
                        kb.op("pool", lambda e, r=r: e.tensor_tensor(out=cqD[:, r, :], in0=cqB[:], in1=negm[:, r, :], op=ALU.add),
                              reads=["f_cqB", ("negm", r)], writes=[("f_cqD", r)])
                    c.rot = list(range(6))
                    po, pok = c.psb[6 + qi % 2], f"psb{6 + qi % 2}"
                    nk = 4 * (qi + 1)
                    LA = 3
                    sbank = {}

                    def emit_S(kt):
                        ps_, psk = c.ps()
                        mm(c, ps_[:, :], fk[h][:, kt * 128:(kt + 1) * 128], fq[h][:, qs], True, True, [("fk", h, kt // 4), ("fq", h, qi)], [psk])
                        sbank[kt] = (ps_, psk)

                    for kt in range(min(LA, nk)):
                        emit_S(kt)
                    for kt in range(nk):
                        ps_, psk = sbank.pop(kt)
                        tb = tmpb[it % 3]; tbk = f"f_tmp{it % 3}"
                        pb = pTb[it % 3]; pbk = f"f_pT{it % 3}"
                        it += 1
                        r = kt - 4 * qi
                        if r >= 0:
                            kb.op("dve", lambda e, ps_=ps_, tb=tb, r=r: e.tensor_tensor(out=tb[:], in0=ps_[:, :], in1=cqD[:, r, :], op=ALU.add),
                                  reads=[psk, ("f_cqD", r)], writes=[tbk])
                        else:
                            kb.op("dve", lambda e, ps_=ps_, tb=tb: e.tensor_tensor(out=tb[:], in0=ps_[:, :], in1=cqB[:], op=ALU.add),
                                  reads=[psk, "f_cqB"], writes=[tbk])
                        kb.op("act", lambda e, tb=tb, pb=pb, h=h, kt=kt: e.activation(out=pb[:], in_=tb[:], func=AF.Exp, bias=ckT[h][:, kt:kt + 1]),
                              reads=[tbk, ("ckT", h)], writes=[pbk])
                        if kt + LA < nk:
                            emit_S(kt + LA)
                        mm(c, po[0:65, :], fV[h][:, kt, :], pb[:], kt == 0, kt == nk - 1, [("fV", h, kt // 4), ("fV1", h), pbk], [pok])
                    kb.op("act", lambda e, po=po: e.activation(out=osb[:], in_=po[0:65, :], func=AF.Copy), reads=[pok], writes=["f_osb"])
                    pd, pdk = c.ps()
                    mm(c, pd[0:64, :], c.ones_f[64:65, 0:64], osb[64:65, :], True, True, ["ones_f", "f_osb"], [pdk])
                    kb.op("dve", lambda e, pd=pd: e.reciprocal(out=rden[:], in_=pd[0:64, :]), reads=[pdk], writes=["f_rden"])
                    ob = outb[qi % 2]; obk = f"f_out{qi % 2}"
                    kb.op("dve", lambda e, ob=ob: e.tensor_tensor(out=ob[:], in0=osb[0:64, :], in1=rden[:], op=ALU.mult), reads=["f_osb", "f_rden"], writes=[obk])
                    cfo = io["catf_out"]
                    kb.dma("sp", (cfo[h][:, qs] if isinstance(cfo, list) else cfo[h * 64:(h + 1) * 64, qs]), ob[:], reads=[obk])
            c.rot = None
        c.barrier()


def build_M(which="both"):
    nc = bass.Bass("TRN2", target_bir_lowering=False)
    io = {}

    def din(name, shape, dt=F32):
        io[name] = nc.dram_tensor(name, shape, dt, kind="ExternalInput").ap()

    def dout(name, shape, dt=F32):
        io[name] = nc.dram_tensor(name, shape, dt, kind="ExternalOutput").ap()

    din("hT_all", [4, D, NT], BF16); din("w_ml", [D, 258]); din("w_fx", [D, 386]); din("vecsM", [128, 16])
    dout("catm_out", [64, SEQ], BF16); dout("catf_out", [128, SEQ], BF16)
    with _ES() as st:
        c = Ctx(nc, st)
        c.setup()
        if which in ("both", "mlstm"):
            phase_M_mlstm(c, io)
        if which in ("both", "fox"):
            phase_M_fox(c, io)
        c.barrier()
        c.kb.flush()
    return nc


def inputs_M(inp, l, g):
    w_in = np.asarray(inp["w_in"][l], np.float32)
    cols = np.concatenate([
        np.arange(g * 64, g * 64 + 64), 256 + np.arange(g * 64, g * 64 + 64),
        512 + np.arange(g * 64, g * 64 + 64), 768 + np.arange(g * 64, g * 64 + 64),
        [1024 + g, 1028 + g]])
    w_ml = np.ascontiguousarray(w_in[:, cols])
    fcols = []
    for hh in (2 * g, 2 * g + 1):
        for base in (1544, 2056, 2568):
            fcols.append(base + np.arange(hh * 64, hh * 64 + 64))
    fcols.append(np.array([3080 + 2 * g, 3080 + 2 * g + 1]))
    w_fx = np.ascontiguousarray(w_in[:, np.concatenate(fcols)])
    v = np.zeros((128, 16), np.float32)
    cw = np.asarray(inp["mlstm_conv_w"][l], np.float32)
    cb = np.asarray(inp["mlstm_conv_b"][l], np.float32)
    v[0:64, 0:4] = cw[:, g * 64:g * 64 + 64].T
    v[0:64, 4] = cb[g * 64:g * 64 + 64]
    v[0:64, 5:9] = cw[:, 256 + g * 64:256 + g * 64 + 64].T
    v[0:64, 9] = cb[256 + g * 64:256 + g * 64 + 64]
    v[0:64, 10] = np.asarray(inp["mlstm_norm_w"][l], np.float32)[g * 64:g * 64 + 64]
    v[:, 11] = inp["mlstm_b_i"][l][g]
    v[:, 12] = inp["mlstm_b_f"][l][g]
    v[:, 13] = inp["fox_b_f"][l][2 * g]
    v[:, 14] = inp["fox_b_f"][l][2 * g + 1]
    return {"w_ml": w_ml, "w_fx": w_fx, "vecsM": v}


def phase_P(c, io, xT):
    kb = c.kb
    from contextlib import ExitStack
    with ExitStack() as s0:
        vecs = c.sb("vecsP_sb", [128, 8], F32, s0)
        eps_t = c.sb("p_eps", [128, 1], F32, s0)
        sq = c.sb("p_sq", [128, 8, TT], BF16, s0)
        rstd = c.sb("p_rstd", [128, TT], F32, s0)
        hT = c.sb("p_hT", [128, 8, TT], BF16, s0)
        xt = [c.sb(f"p_xt{i}", [128, 4, D], F32, s0) for i in range(2)]
        tmp = {"sq": sq, "rstd": rstd, "eps": eps_t}
        kb.dma("sp", vecs[:], io["vecsP"], writes=["vecs"])
        kb.op("pool", lambda e: e.memset(eps_t[:], EPS), writes=["eps"])
        for t in range(NTT):
            t0 = t * TT
            xb = xt[t % 2]
            xk = f"p_xt{t % 2}"
            kb.dma("sp", xb[:], io["x_tok"][t0:t0 + TT, :].rearrange("(s p) d -> p s d", p=128), writes=[xk])
            for k in range(8):
                p, pk = c.ps()
                for s in range(4):
                    kb.op("pe", lambda e, k=k, s=s, p=p, xb=xb: e.transpose(out=p[:, s * 128:(s + 1) * 128], in_=xb[:, s, k * 128:(k + 1) * 128], identity=c.ident_f[:]),
                          reads=[xk, "ident_f"], writes=[pk])
                kb.op("act", lambda e, k=k, p=p, t0=t0: e.activation(out=xT[:, k, t0:t0 + TT], in_=p[:, :], func=AF.Copy), reads=[pk], writes=[("xT", k)])
            rmsnorm_tile(c, xT, "xT", t0, TT, vecs[:, 0:8], tmp, hT, "hT")
            h_store(c, io["h_next"], hT, t0, TT, [("hT", k) for k in range(8)], ["h_next_d"])
            if t == NTT - 1 and "tail_next" in io:
                kb.dma("sp", io["tail_next"].rearrange("(k p) n -> p k n", p=128), hT[:, :, TT - 32:TT],
                       reads=[("hT", k) for k in range(8)], writes=["tail_next_d"])
    c.barrier()


def build_P():
    nc = bass.Bass("TRN2", target_bir_lowering=False)
    io = {}
    io["x_tok"] = nc.dram_tensor("x_tok", [NT, D], F32, kind="ExternalInput").ap()
    io["vecsP"] = nc.dram_tensor("vecsP", [128, 8], F32, kind="ExternalInput").ap()
    io["xT_out"] = nc.dram_tensor("xT_out", [D, NT], F32, kind="ExternalOutput").ap()
    io["h_next"] = nc.dram_tensor("h_next", [D, NT], BF16, kind="ExternalOutput").ap()
    with _ES() as st:
        c = Ctx(nc, st)
        c.setup()
        xT = c.sb("xT", [128, 8, NT], F32)
        phase_P(c, io, xT)
        c.kb.dma("sp", io["xT_out"].rearrange("(k p) n -> p k n", p=128), xT[:], reads=[("xT", k) for k in range(8)])
        c.barrier()
        c.kb.flush()
    return nc


_CACHE = {}


def _get(name, fn):
    if name not in _CACHE:
        _CACHE[name] = fn()
    return _CACHE[name]


def kernel(**inp):
    inp = {k: np.asarray(v) for k, v in inp.items()}
    cores = list(range(8))
    B = 2
    x = inp["x"].astype(np.float32, copy=False)
    fm = lambda w: np.ascontiguousarray(np.asarray(w, np.float32).reshape(-1, 128).T)
    ncP = _get("P", build_P)
    maps = []
    for cid in cores:
        b, j = cid // 4, cid % 4
        maps.append({"x_tok": np.ascontiguousarray(x[b, j * NT:(j + 1) * NT]), "vecsP": fm(inp["norm_mix_w"][0])})
    res = run_bass_kernel_spmd(ncP, maps, core_ids=cores).results
    xT = [r["xT_out"] for r in res]
    hN = [r["h_next"] for r in res]
    out = None
    for l in range(2):
        last = (l == 1)
        E = 1 if l == 0 else 8
        ncM = _get("M", build_M)
        maps = []
        for cid in cores:
            b, g = cid // 4, cid % 4
            m = inputs_M(inp, l, g)
            m["hT_all"] = np.ascontiguousarray(np.stack([hN[b * 4 + jj] for jj in range(4)], axis=0))
            maps.append(m)
        resM = run_bass_kernel_spmd(ncM, maps, core_ids=cores).results
        ncT = _get(("T", E, last), lambda: build_T(E, last))
        maps = []
        for cid in cores:
            b, j = cid // 4, cid % 4
            tk = slice(j * NT, (j + 1) * NT)
            m = {"xT_in": xT[cid], "h_own": hN[cid]}
            m["h_halo"] = (np.ascontiguousarray(hN[cid - 1][:, NT - 32:NT]) if j > 0 else np.zeros((D, 32), NPBF))
            m["catm"] = np.ascontiguousarray(np.stack([resM[b * 4 + g]["catm_out"][:, tk] for g in range(4)], axis=0))
            m["catf"] = np.ascontiguousarray(np.stack([resM[b * 4 + g]["catf_out"][:, tk] for g in range(4)], axis=0))
            m["mem"] = np.ascontiguousarray(inp["mem"][b], dtype=np.float32)
            m["vecs"] = vecs_T(inp, l, last)
            m["w_c"] = np.ascontiguousarray(inp["w_in"][l][:, 1032:1544])
            m["w_out"] = inp["w_out"][l]; m["w_q"] = inp["xattn_w_q"][l]
            m["w_kv"] = inp["xattn_w_kv"][l]; m["w_o"] = inp["xattn_w_o"][l]
            if E == 1:
                m["w_gate"] = inp["ffn_w_gate"]; m["w_up"] = inp["ffn_w_up"]; m["w_down"] = inp["ffn_w_down"]
            else:
                m["w_gate"] = inp["moe_w_gate"][0]; m["w_up"] = inp["moe_w_up"][0]; m["w_down"] = inp["moe_w_down"][0]
                m["router_w"] = inp["router_w"][0]
            maps.append(m)
        resT = run_bass_kernel_spmd(ncT, maps, core_ids=cores).results
        if not last:
            xT = [r["xT_out"] for r in resT]
            hN = [r["h_next"] for r in resT]
        else:
            out = np.zeros((B, SEQ, D), np.float32)
            for cid in cores:
                b, j = cid // 4, cid % 4
                out[b, j * NT:(j + 1) * NT] = resT[cid]["out"]
    return out


RG = [[0, 1, 2, 3], [4, 5, 6, 7]]
_STOP = None


def build_fused(stop=None):
    nc = bass.Bass("TRN2", target_bir_lowering=False)
    io = {}
    if stop:
        io["dbg1"] = nc.dram_tensor("dbg1", [4 * D, NT], BF16, kind="ExternalOutput").ap()
        io["dbg2"] = nc.dram_tensor("dbg2", [512, SEQ], BF16, kind="ExternalOutput").ap()
        io["dbg3"] = nc.dram_tensor("dbg3", [D, NT], F32, kind="ExternalOutput").ap()

    def din(name, shape, dt=F32):
        io[name] = nc.dram_tensor(name, shape, dt, kind="ExternalInput").ap()
        return io[name]

    def dint(name, shape, dt=BF16):
        io[name] = nc.dram_tensor(name, shape, dt, kind="Internal").ap()
        return io[name]

    din("x_tok", [NT, D]); din("vecsP", [128, 8])
    if stop != "AG":
        din("sel", [128, 8]); din("mem", [256, D])
    for l in range(2 if stop != "AG" else 0):
        din(f"w_ml{l}", [D, 258]); din(f"w_fx{l}", [D, 386]); din(f"vecsM{l}", [128, 16]); din(f"vecs{l}", [128, NV_T])
        din(f"w_c{l}", [D, 512]); din(f"w_out{l}", [D, D]); din(f"w_q{l}", [D, 512]); din(f"w_kv{l}", [D, D]); din(f"w_o{l}", [512, D])
    if stop != "AG":
        din("w_gate0", [1, D, DFF]); din("w_up0", [1, D, DFF]); din("w_down0", [1, DFF, D])
        din("w_gate1", [8, D, DFF]); din("w_up1", [8, D, DFF]); din("w_down1", [8, DFF, D]); din("router_w", [D, 8])
    io["out"] = nc.dram_tensor("out", [NT, D], F32, kind="ExternalOutput").ap()
    for l in range(2):
        io[f"h_own{l}"] = [dint(f"h_own{l}_{a}", [256, NT]) for a in range(4)]
        io[f"hT_all{l}"] = [dint(f"hT_all{l}_{a}", [4 * 256, NT]) for a in range(4)]
        dint(f"tail{l}", [D, 32]); dint(f"tails{l}", [4 * D, 32])
        dint(f"catm{l}", [64, SEQ]); dint(f"catm_all{l}", [256, SEQ])
        io[f"catf{l}"] = [dint(f"catf{l}_{h}", [64, SEQ]) for h in range(2)]
        io[f"catf_all{l}"] = [dint(f"catf_all{l}_{h}", [256, SEQ]) for h in range(2)]
    with _ES() as st:
        c = Ctx(nc, st)
        c.setup()
        kb = c.kb
        xT = c.sb("xT", [128, 8, NT], F32)
        phase_P(c, {"x_tok": io["x_tok"], "vecsP": io["vecsP"], "h_next": io["h_own0"], "tail_next": io["tail0"]}, xT)
        for l in range(2):
            last = (l == 1)
            for a in range(4):
                kb.collective("AllGather", RG, io[f"h_own{l}"][a], io[f"hT_all{l}"][a], reads=["h_next_d"], writes=["hT_all_d"])
            kb.collective("AllGather", RG, io[f"tail{l}"], io[f"tails{l}"], reads=["tail_next_d"], writes=["tails_d"])
            c.barrier()
            if stop == "AG":
                for a in range(4):
                    for jj in range(4):
                        kb.dma("sp", io["dbg1"][jj * D + a * 256:jj * D + (a + 1) * 256, :], io[f"hT_all{l}"][a][jj * 256:(jj + 1) * 256, :], reads=["hT_all_d"])
                break
            ioM = {"hT_all": io[f"hT_all{l}"], "w_ml": io[f"w_ml{l}"], "w_fx": io[f"w_fx{l}"],
                   "vecsM": io[f"vecsM{l}"], "catm_out": io[f"catm{l}"], "catf_out": io[f"catf{l}"]}
            c.sfx = f"_{l}"
            phase_M_mlstm(c, ioM)
            phase_M_fox(c, ioM)
            kb.collective("AllGather", RG, io[f"catm{l}"], io[f"catm_all{l}"], writes=["catm_all_d"])
            for h in range(2):
                kb.collective("AllGather", RG, io[f"catf{l}"][h], io[f"catf_all{l}"][h], writes=["catf_all_d"])
            c.barrier()
            if stop == "M":
                for h in range(2):
                    for g in range(4):
                        kb.dma("sp", io["dbg2"][g * 128 + h * 64:g * 128 + (h + 1) * 64, :], io[f"catf_all{l}"][h][g * 64:(g + 1) * 64, :], reads=["catf_all_d"])
                break
            ioT = {"h_own": io[f"h_own{l}"], "tails": io[f"tails{l}"], "sel": io["sel"], "catm_all": io[f"catm_all{l}"], "catf_all": io[f"catf_all{l}"],
                   "mem": io["mem"], "vecs": io[f"vecs{l}"], "w_c": io[f"w_c{l}"], "w_out": io[f"w_out{l}"], "w_q": io[f"w_q{l}"],
                   "w_kv": io[f"w_kv{l}"], "w_o": io[f"w_o{l}"], "w_gate": io[f"w_gate{l}"], "w_up": io[f"w_up{l}"], "w_down": io[f"w_down{l}"]}
            if last:
                ioT["router_w"] = io["router_w"]; ioT["out"] = io["out"]
            else:
                ioT["h_next"] = io["h_own1"]; ioT["tail_next"] = io["tail1"]
            phase_T(c, ioT, 8 if last else 1, last, xT)
            if stop == "T":
                kb.dma("sp", io["dbg3"].rearrange("(k p) n -> p k n", p=128), xT[:], reads=[("xT", k) for k in range(8)])
                break
        c.barrier()
        kb.flush()
    return nc


def kernel_unfused(**inp):
    return _kernel_unfused(**inp)


_kernel_unfused = kernel


def kernel(**inp):
    inp = {k: np.asarray(v) for k, v in inp.items()}
    cores = list(range(8))
    x = inp["x"].astype(np.float32, copy=False)
    fm = lambda w: np.ascontiguousarray(np.asarray(w, np.float32).reshape(-1, 128).T)
    nc = _get("fused", lambda: build_fused(_STOP))
    shared = {"vecsP": fm(inp["norm_mix_w"][0]),
              "w_gate0": inp["ffn_w_gate"], "w_up0": inp["ffn_w_up"], "w_down0": inp["ffn_w_down"],
              "w_gate1": inp["moe_w_gate"][0], "w_up1": inp["moe_w_up"][0], "w_down1": inp["moe_w_down"][0],
              "router_w": inp["router_w"][0]}
    for l in range(2):
        shared[f"vecs{l}"] = vecs_T(inp, l, l == 1)
        shared[f"w_c{l}"] = np.ascontiguousarray(inp["w_in"][l][:, 1032:1544])
        shared[f"w_out{l}"] = inp["w_out"][l]; shared[f"w_q{l}"] = inp["xattn_w_q"][l]
        shared[f"w_kv{l}"] = inp["xattn_w_kv"][l]; shared[f"w_o{l}"] = inp["xattn_w_o"][l]
    perg = []
    for g in range(4):
        d = {}
        for l in range(2):
            m = inputs_M(inp, l, g)
            d[f"w_ml{l}"] = m["w_ml"]; d[f"w_fx{l}"] = m["w_fx"]; d[f"vecsM{l}"] = m["vecsM"]
        perg.append(d)
    maps = []
    for cid in cores:
        b, j = cid // 4, cid % 4
        m = dict(shared)
        m.update(perg[j])
        m["x_tok"] = np.ascontiguousarray(x[b, j * NT:(j + 1) * NT])
        m["mem"] = np.ascontiguousarray(inp["mem"][b], dtype=np.float32)
        sel = np.zeros((128, 8), np.float32)
        sel[:, j] = 1.0
        if j > 0:
            sel[:, 4 + j - 1] = 1.0
        m["sel"] = sel
        maps.append(m)
    if _STOP == "AG":
        maps = [{k: m[k] for k in ("x_tok", "vecsP")} for m in maps]
    res = run_bass_kernel_spmd(nc, maps, core_ids=cores).results
    if _STOP:
        return res
    out = np.zeros((2, SEQ, D), np.float32)
    for cid in cores:
        b, j = cid // 4, cid % 4
        out[b, j * NT:(j + 1) * NT] = res[cid]["out"]
    return out
```

```python
import numpy as np
import concourse.bass as bass
import concourse.mybir as mybir
from concourse.bass_utils import run_bass_kernel_spmd

F32 = mybir.dt.float32
BF16 = mybir.dt.bfloat16
AF = mybir.ActivationFunctionType
ALU = mybir.AluOpType
AX = mybir.AxisListType

ENGS = ("pe", "act", "dve", "pool", "sp")


class KB:
    SEM_ROLL = 2000

    def __init__(self, nc, n_dma_sems=32):
        self.nc = nc
        self.q = {e: [] for e in ENGS}
        self.cnt = {e: 0 for e in ENGS}
        self.cur_sem = {}
        self.sem_pool = []
        self.waited = {e: {} for e in ENGS}
        self.last_w = {}
        self.reads = {}
        self.n_dma_sems = n_dma_sems
        self.dma_sems = []
        self.dma_cnt = []
        self.dma_rr = 0
        self.dma_rr_sw = 0
        self._stack = None
        self.n_inst = 0

    def _new_sem(self, name):
        s = self._stack.enter_context(self.nc.semaphore(name))
        return s

    def start(self, stack):
        self._stack = stack
        for e in ENGS:
            self.cur_sem[e] = self._new_sem(f"p_{e}_0")
        for i in range(self.n_dma_sems):
            self.dma_sems.append(self._new_sem(f"dma{i}"))
            self.dma_cnt.append(0)

    def _wait(self, eng, ev):
        if ev is None:
            return
        if len(ev) == 3 and ev[2] == "pe" and eng == "pe":
            return
        sem, val = ev[0], ev[1]
        w = self.waited[eng]
        if w.get(id(sem), (None, 0))[1] >= val:
            return
        w[id(sem)] = (sem, val)
        self.q[eng].append(lambda e, sem=sem, val=val: e.wait_ge(sem, val))

    def _wait_w(self, eng, k):
        lw = self.last_w.get(k)
        if isinstance(lw, list):
            for ev in lw:
                self._wait(eng, ev)
        else:
            self._wait(eng, lw)

    def _deps(self, eng, reads, writes):
        for k in reads:
            self._wait_w(eng, k)
        for k in writes:
            self._wait_w(eng, k)
            for ev in self.reads.get(k, ()):
                self._wait(eng, ev)

    def _commit(self, ev, reads, writes, is_dma=False):
        for k in writes:
            lw = self.last_w.get(k)
            if is_dma and isinstance(lw, list) and not self.reads.get(k):
                lw.append(ev)
            else:
                self.last_w[k] = [ev] if is_dma else ev
            self.reads[k] = []
        for k in reads:
            self.reads.setdefault(k, []).append(ev)

    def op(self, eng, fn, reads=(), writes=()):
        self._deps(eng, reads, writes)
        if self.cnt[eng] >= self.SEM_ROLL:
            self.cur_sem[eng] = self._new_sem(f"p_{eng}_{self.n_inst}")
            self.cnt[eng] = 0
        self.cnt[eng] += 1
        sem = self.cur_sem[eng]
        ev = (sem, self.cnt[eng], eng)
        self.q[eng].append(lambda e, sem=sem: fn(e).then_inc(sem, 1))
        self._commit(ev, reads, writes)
        self.n_inst += 1
        return ev

    def dma(self, eng, out, in_, reads=(), writes=(), **kw):
        self._deps(eng, reads, writes)
        half = self.n_dma_sems // 2
        if eng == "pool":
            i = half + self.dma_rr_sw
            self.dma_rr_sw = (self.dma_rr_sw + 1) % (self.n_dma_sems - half)
        else:
            i = self.dma_rr
            self.dma_rr = (self.dma_rr + 1) % half
        sem = self.dma_sems[i]
        if self.dma_cnt[i] >= 2048:
            self.dma_sems[i] = self._new_sem(f"dma{i}_{self.n_inst}")
            self.dma_cnt[i] = 0
            sem = self.dma_sems[i]
        if self.dma_cnt[i] > 0:
            self._wait(eng, (sem, self.dma_cnt[i]))
        self.dma_cnt[i] += 16
        ev = (sem, self.dma_cnt[i])
        self.q[eng].append(lambda e, sem=sem: e.dma_start(out=out, in_=in_, **kw).then_inc(sem, 16))
        self._commit(ev, reads, writes, is_dma=True)
        self.n_inst += 1
        return ev

    def collective(self, kind, rg, in_ap, out_ap, reads=(), writes=()):
        eng = "pool"
        self._deps(eng, reads, writes)
        sem = self._new_sem(f"cc_{self.n_inst}")
        ev = (sem, 1)
        self.q[eng].append(lambda e: e.collective_compute(kind, ALU.bypass, replica_groups=rg, ins=[in_ap.opt()],
                                                          outs=[out_ap.opt()]).then_inc(sem, 1))
        self._commit(ev, reads, writes)
        self.n_inst += 1
        self.cc_events = getattr(self, "cc_events", []) + [ev]
        return ev

    def wait_all(self, eng, evs):
        for ev in evs:
            self._wait(eng, ev)

    def flush(self):
        nc = self.nc
        q = self.q
        with nc.Block() as block:
            @block.tensor
            def _(e):
                for f in q["pe"]:
                    f(e)

            @block.scalar
            def _(e):
                for f in q["act"]:
                    f(e)

            @block.vector
            def _(e):
                for f in q["dve"]:
                    f(e)

            @block.gpsimd
            def _(e):
                for f in q["pool"]:
                    f(e)

            @block.sync
            def _(e):
                for f in q["sp"]:
                    f(e)
        self.q = {e: [] for e in ENGS}


D = 1024
NT = 2048
TT = 512
NTT = NT // TT
DFF = 2816
NF = DFF // 128
SEQ = 8192
EPS = 1e-6
NV_T = 100


class Ctx:
    def __init__(self, nc, st):
        self.nc = nc
        self.st = st
        self.kb = KB(nc)
        self.kb.start(st)
        self.ps_rr = 0
        self.uid = 0

    def sb(self, name, shape, dt, st=None):
        self.uid += 1
        return (st or self.st).enter_context(self.nc.sbuf_tensor(f"{name}_u{self.uid}", shape, dt))

    def barrier(self):
        kb = self.kb
        evs = []
        for e in ENGS:
            if kb.cnt[e] > 0:
                evs.append((kb.cur_sem[e], kb.cnt[e]))
        for i, s in enumerate(kb.dma_sems):
            if kb.dma_cnt[i] > 0:
                evs.append((s, kb.dma_cnt[i]))
        evs += getattr(kb, "cc_events", [])
        kb.cc_events = []
        for e in ENGS:
            for ev in evs:
                kb._wait(e, ev)
        kb.last_w = {}
        kb.reads = {}

    def setup(self):
        nc, kb = self.nc, self.kb
        self.ident_f = self.sb("ident_f", [128, 128], F32)
        self.ident_b = self.sb("ident_b", [128, 128], BF16)
        self.ones_b = self.sb("ones_b", [128, 128], BF16)
        self.ones_f = self.sb("ones_f", [128, 128], F32)
        self.psb = [self.st.enter_context(nc.psum_tensor(f"psb{i}", [128, 512], F32)) for i in range(8)]
        idf, idb, ob, of = self.ident_f, self.ident_b, self.ones_b, self.ones_f
        kb.op("pool", lambda e: e.memset(idf[:], 0.0), writes=["ident_f"])
        kb.op("pool", lambda e: e.affine_select(out=idf[:], in_=idf[:], pattern=[[-1, 128]],
                                                compare_op=ALU.not_equal, fill=1.0, base=0,
                                                channel_multiplier=1),
              reads=["ident_f"], writes=["ident_f"])
        kb.op("pool", lambda e: e.tensor_copy(out=idb[:], in_=idf[:]), reads=["ident_f"], writes=["ident_b"])
        kb.op("pool", lambda e: e.memset(ob[:], 1.0), writes=["ones_b"])
        kb.op("pool", lambda e: e.memset(of[:], 1.0), writes=["ones_f"])

    def ps(self):
        rot = getattr(self, "rot", None) or list(range(8))
        i = rot[self.ps_rr % len(rot)]
        self.ps_rr += 1
        return self.psb[i], f"psb{i}"


def h_store(c, dst, hT, c0, n, reads, writes=()):
    if isinstance(dst, list):
        for a, d in enumerate(dst):
            c.kb.dma("sp", d[:, c0:c0 + n].rearrange("(k p) n -> p k n", p=128), hT[:, 2 * a:2 * a + 2, 0:n], reads=reads, writes=writes)
    else:
        c.kb.dma("sp", dst[:, c0:c0 + n].rearrange("(k p) n -> p k n", p=128), hT[:, :, 0:n], reads=reads, writes=writes)


def h_load(c, src, hT, c0, n, writes, j=None):
    if isinstance(src, list):
        for a, d in enumerate(src):
            v = d if j is None else d.rearrange("(j r) n -> j r n", j=4)[j]
            c.kb.dma("sp", hT[:, 2 * a:2 * a + 2, 0:n], v[:, c0:c0 + n].rearrange("(k p) n -> p k n", p=128), writes=writes)
    else:
        v = src if j is None else src[j]
        c.kb.dma("sp", hT[:, :, 0:n], v[:, c0:c0 + n].rearrange("(k p) n -> p k n", p=128), writes=writes)


def mm(c, out, lhsT, rhs, start, stop, reads, writes):
    return c.kb.op("pe", lambda e: e.matmul(out, lhsT=lhsT, rhs=rhs, start=start, stop=stop),
                   reads=reads, writes=writes)


def rmsnorm_tile(c, xT, xkey, t0, n, wv, tmp, out_bf, okey, out_f=None):
    kb = c.kb
    sq, rstd = tmp["sq"], tmp["rstd"]
    for k in range(8):
        kb.op("act", lambda e, k=k: e.activation(out=sq[:, k, 0:n], in_=xT[:, k, t0:t0 + n], func=AF.Square),
              reads=[(xkey, k)], writes=[("sq", k)])
    p, pk = c.ps()
    for k in range(8):
        mm(c, p[:, 0:n], c.ones_b[:], sq[:, k, 0:n], k == 0, k == 7, ["ones_b", ("sq", k)], [pk])
    kb.op("act", lambda e: e.activation(out=rstd[:, 0:n], in_=p[:, 0:n], func=AF.Sqrt, scale=1.0 / D, bias=tmp["eps"][:, 0:1]),
          reads=[pk, "eps"], writes=["rstd"])
    kb.op("dve", lambda e: e.reciprocal(out=rstd[:, 0:n], in_=rstd[:, 0:n]), reads=["rstd"], writes=["rstd"])
    for k in range(8):
        kb.op("dve", lambda e, k=k: e.scalar_tensor_tensor(out=out_bf[:, k, 0:n], in0=xT[:, k, t0:t0 + n],
                                                           scalar=wv[:, k:k + 1], in1=rstd[:, 0:n],
                                                           op0=ALU.mult, op1=ALU.mult),
              reads=[(xkey, k), "rstd", "vecs"], writes=[(okey, k)])
        if out_f is not None:
            kb.op("dve", lambda e, k=k: e.scalar_tensor_tensor(out=out_f[:, k, 0:n], in0=xT[:, k, t0:t0 + n],
                                                                scalar=wv[:, k:k + 1], in1=rstd[:, 0:n],
                                                                op0=ALU.mult, op1=ALU.mult),
                  reads=[(xkey, k), "rstd", "vecs"], writes=[(okey + "_f", k)])


def phase_T(c, io, E, last, xT):
    nc, kb = c.nc, c.kb
    from contextlib import ExitStack
    vec_st = ExitStack()
    vecs = c.sb("vecsT", [128, NV_T], F32, vec_st)
    eps_t = c.sb("eps_t", [128, 1], F32, vec_st)
    sq = c.sb("sq", [128, 8, TT], BF16, vec_st)
    rstd = c.sb("rstd", [128, TT], F32, vec_st)
    tmp = {"sq": sq, "rstd": rstd, "eps": eps_t}
    kb.dma("sp", vecs[:], io["vecs"], writes=["vecs"])
    kb.op("pool", lambda e: e.memset(eps_t[:], EPS), writes=["eps"])
    V_XA, V_MEM, V_FFN, V_NEXT, V_CB, V_LNW, V_LNB, V_CW = 0, 8, 16, 24, 32, 34, 36, 38

    with ExitStack() as s1:
        hT = c.sb("hT", [128, 8, TT], BF16, s1)
        gluT = c.sb("gluT", [128, 2, 32 + NT], BF16, s1)
        hcT = c.sb("hcT", [128, 2, NT], BF16, s1)
        wc = c.sb("wc", [128, 8, 512], BF16, s1)
        dg = c.sb("dg", [128, 62, 128], BF16, s1)
        sig = c.sb("sig", [128, 2, TT], F32, s1)
        hcv = c.sb("hcv", [128, 2, TT], F32, s1)
        hsq = c.sb("hsq", [128, 2, TT], F32, s1)
        mean = c.sb("mean", [128, TT], F32, s1)
        var = c.sb("var", [128, TT], F32, s1)
        wo_m = c.sb("wo_m", [64, 4, D], BF16, s1)
        wo_c = c.sb("wo_c", [128, 2, D], BF16, s1)
        wo_f = c.sb("wo_f", [128, 4, D], BF16, s1)
        mT = c.sb("mT", [64, 4, TT], BF16, s1)
        fT = c.sb("fT", [128, 4, TT], BF16, s1)
        if "sel" in io:
            halo4 = c.sb("halo4", [128, 4, 8, 32], BF16, s1)
            selt = c.sb("selt", [128, 8], F32, s1)
            m4 = [c.sb(f"m4_{i}", [64, 4, TT], BF16, s1) for i in range(2)]
            f4 = [c.sb(f"f4_{i}", [128, 4, TT], BF16, s1) for i in range(2)]
            kb.dma("sp", selt[:], io["sel"], writes=["selt"])
        kb.dma("pool", wc[:], io["w_c"].rearrange("(k p) n -> p k n", p=128), writes=["wc"])
        kb.dma("pool", wo_m[:], io["w_out"][0:256, :].rearrange("(g p) n -> p g n", p=64), writes=["wo_m"])
        kb.dma("pool", wo_c[:], io["w_out"][256:512, :].rearrange("(g p) n -> p g n", p=128), writes=["wo_c"])
        kb.dma("pool", wo_f[:], io["w_out"][512:1024, :].rearrange("(g p) n -> p g n", p=128), writes=["wo_f"])
        for j in range(31):
            for ch in range(2):
                kb.op("dve", lambda e, j=j, ch=ch: e.tensor_scalar(
                    out=dg[:, j * 2 + ch, :], in0=c.ident_b[:], scalar1=vecs[:, V_CW + j * 2 + ch:V_CW + j * 2 + ch + 1],
                    scalar2=None, op0=ALU.mult), reads=["ident_b", "vecs"], writes=[("dg", j, ch)])
        tiles = [("halo", 0, 32)] + [("own", t * TT, TT) for t in range(NTT)]
        for kind, t0, n in tiles:
            if kind == "halo" and "sel" in io:
                tl = io["tails"].rearrange("(j k p) n -> j p k n", j=4, p=128)
                for jj in range(4):
                    kb.dma("sp", halo4[:, jj, :, :], tl[jj], writes=[("halo4", jj)])
                kb.op("dve", lambda e: e.tensor_scalar(out=hT[:, :, 0:32], in0=halo4[:, 0, :, :], scalar1=selt[:, 4:5], scalar2=None, op0=ALU.mult),
                      reads=[("halo4", 0), "selt"], writes=[("hT", k) for k in range(8)])
                for jj in range(1, 4):
                    kb.op("dve", lambda e, jj=jj: e.scalar_tensor_tensor(out=hT[:, :, 0:32], in0=halo4[:, jj, :, :], scalar=selt[:, 4 + jj:5 + jj], in1=hT[:, :, 0:32],
                                                                         op0=ALU.mult, op1=ALU.add),
                          reads=[("halo4", jj), "selt"] + [("hT", k) for k in range(8)], writes=[("hT", k) for k in range(8)])
                g0 = 0
            elif kind == "halo":
                kb.dma("sp", hT[:, :, 0:n], io["h_halo"].rearrange("(k p) n -> p k n", p=128),
                       writes=[("hT", k) for k in range(8)])
                g0 = 0
            else:
                h_load(c, io["h_own"], hT, t0, n, [("hT", k) for k in range(8)])
                g0 = 32 + t0
            for ch in range(2):
                pa, pak = c.ps()
                pg, pgk = c.ps()
                for k in range(8):
                    mm(c, pa[:, 0:n], wc[:, k, ch * 128:(ch + 1) * 128], hT[:, k, 0:n], k == 0, k == 7,
                       ["wc", ("hT", k)], [pak])
                for k in range(8):
                    mm(c, pg[:, 0:n], wc[:, k, 256 + ch * 128:256 + (ch + 1) * 128], hT[:, k, 0:n], k == 0, k == 7,
                       ["wc", ("hT", k)], [pgk])
                kb.op("act", lambda e, ch=ch, pg=pg, n=n: e.activation(out=sig[:, ch, 0:n], in_=pg[:, 0:n], func=AF.Sigmoid),
                      reads=[pgk], writes=[("sig", ch)])
                kb.op("dve", lambda e, ch=ch, pa=pa, n=n, g0=g0: e.tensor_tensor(
                    out=gluT[:, ch, g0:g0 + n], in0=pa[:, 0:n], in1=sig[:, ch, 0:n], op=ALU.mult),
                    reads=[pak, ("sig", ch)], writes=[("glu", ch, g0 // TT), ("glu", ch, (g0 + n - 1) // TT)])
        for t in range(NTT):
            t0 = t * TT
            gk = lambda ch: [("glu", ch, (32 + t0 - 30) // TT), ("glu", ch, (32 + t0 + TT - 1) // TT)]
            for ch in range(2):
                p, pk = c.ps()
                for j in range(31):
                    o = 32 + t0 - 30 + j
                    mm(c, p[:, :], dg[:, j * 2 + ch, :], gluT[:, ch, o:o + TT], j == 0, j == 30,
                       [("dg", j, ch)] + gk(ch), [pk])
                kb.op("act", lambda e, ch=ch, p=p: e.activation(out=hcv[:, ch, :], in_=p[:, :], func=AF.Identity,
                                                                bias=vecs[:, V_CB + ch:V_CB + ch + 1]),
                      reads=[pk, "vecs"], writes=[("hcv", ch)])
                kb.op("act", lambda e, ch=ch: e.activation(out=hsq[:, ch, :], in_=hcv[:, ch, :], func=AF.Square),
                      reads=[("hcv", ch)], writes=[("hsq", ch)])
            p1, p1k = c.ps()
            p2, p2k = c.ps()
            for ch in range(2):
                mm(c, p1[:, :], c.ones_f[:], hcv[:, ch, :], ch == 0, ch == 1, ["ones_f", ("hcv", ch)], [p1k])
            for ch in range(2):
                mm(c, p2[:, :], c.ones_f[:], hsq[:, ch, :], ch == 0, ch == 1, ["ones_f", ("hsq", ch)], [p2k])
            kb.op("dve", lambda e, p1=p1: e.tensor_scalar(out=mean[:], in0=p1[:, :], scalar1=1.0 / 256, scalar2=None, op0=ALU.mult),
                  reads=[p1k], writes=["mean"])
            kb.op("dve", lambda e: e.tensor_tensor(out=var[:], in0=mean[:], in1=mean[:], op=ALU.mult),
                  reads=["mean"], writes=["var"])
            kb.op("dve", lambda e, p2=p2: e.scalar_tensor_tensor(out=var[:], in0=p2[:, :], scalar=1.0 / 256, in1=var[:],
                                                                 op0=ALU.mult, op1=ALU.subtract),
                  reads=[p2k, "var"], writes=["var"])
            kb.op("act", lambda e: e.activation(out=var[:], in_=var[:], func=AF.Sqrt, bias=eps_t[:, 0:1]),
                  reads=["var", "eps"], writes=["var"])
            kb.op("dve", lambda e: e.reciprocal(out=var[:], in_=var[:]), reads=["var"], writes=["var"])
            for ch in range(2):
                kb.op("dve", lambda e, ch=ch: e.tensor_tensor(out=hcv[:, ch, :], in0=hcv[:, ch, :], in1=mean[:], op=ALU.subtract),
                      reads=[("hcv", ch), "mean"], writes=[("hcv", ch)])
                kb.op("dve", lambda e, ch=ch: e.tensor_tensor(out=hcv[:, ch, :], in0=hcv[:, ch, :], in1=var[:], op=ALU.mult),
                      reads=[("hcv", ch), "var"], writes=[("hcv", ch)])
                kb.op("dve", lambda e, ch=ch: e.tensor_scalar(out=hcv[:, ch, :], in0=hcv[:, ch, :],
                                                              scalar1=vecs[:, V_LNW + ch:V_LNW + ch + 1],
                                                              scalar2=vecs[:, V_LNB + ch:V_LNB + ch + 1],
                                                              op0=ALU.mult, op1=ALU.add),
                      reads=[("hcv", ch), "vecs"], writes=[("hcv", ch)])
                kb.op("act", lambda e, ch=ch, t0=t0: e.activation(out=hcT[:, ch, t0:t0 + TT], in_=hcv[:, ch, :], func=AF.Silu),
                      reads=[("hcv", ch)], writes=[("hcT", ch, t)])
            if "sel" in io:
                cm = io["catm_all"].rearrange("(g p) n -> p g n", p=64)
                cf = [a.rearrange("(g p) n -> p g n", p=64) for a in io["catf_all"]]
                for jj in range(4):
                    for dst, stg, src, nm, npart in ((mT, m4, cm, "m4", 64), (fT, f4, cf, "f4", 128)):
                        dk = "mT" if nm == "m4" else "fT"
                        sg = stg[jj % 2]
                        sk = f"{nm}_{jj % 2}"
                        if nm == "m4":
                            kb.dma("sp", sg[:], src[:, :, jj * NT + t0:jj * NT + t0 + TT], writes=[sk])
                        else:
                            for hh in range(2):
                                kb.dma("sp", sg[hh * 64:(hh + 1) * 64, :, :], src[hh][:, :, jj * NT + t0:jj * NT + t0 + TT], writes=[sk])
                        if jj == 0:
                            kb.op("dve", lambda e, dst=dst, sg=sg, npart=npart: e.tensor_scalar(out=dst[:], in0=sg[:], scalar1=selt[0:npart, 0:1], scalar2=None, op0=ALU.mult),
                                  reads=[sk, "selt"], writes=[dk])
                        else:
                            kb.op("dve", lambda e, dst=dst, sg=sg, jj=jj, npart=npart: e.scalar_tensor_tensor(out=dst[:], in0=sg[:], scalar=selt[0:npart, jj:jj + 1], in1=dst[:],
                                                                                                          op0=ALU.mult, op1=ALU.add),
                                  reads=[sk, "selt", dk], writes=[dk])
            else:
                kb.dma("sp", mT[:], io["catm"][:, :, t0:t0 + TT].rearrange("g p n -> p g n"), writes=["mT"])
                kb.dma("sp", fT[:], io["catf"][:, :, t0:t0 + TT].rearrange("g p n -> p g n"), writes=["fT"])
            for d in range(8):
                p, pk = c.ps()
                ds = slice(d * 128, (d + 1) * 128)
                for g in range(4):
                    mm(c, p[:, :], wo_m[:, g, ds], mT[:, g, :], g == 0, False, ["wo_m", "mT"], [pk])
                for ch in range(2):
                    mm(c, p[:, :], wo_c[:, ch, ds], hcT[:, ch, t0:t0 + TT], False, False, ["wo_c", ("hcT", ch, t)], [pk])
                for g in range(4):
                    mm(c, p[:, :], wo_f[:, g, ds], fT[:, g, :], False, g == 3, ["wo_f", "fT"], [pk])
                kb.op("dve", lambda e, d=d, p=p, t0=t0: e.tensor_tensor(out=xT[:, d, t0:t0 + TT], in0=xT[:, d, t0:t0 + TT],
                                                                        in1=p[:, :], op=ALU.add),
                      reads=[pk, ("xT", d)], writes=[("xT", d)])
    c.barrier()
    if io.get("dbg_stage") == 1:
        vec_st.close()
        return

    with ExitStack() as s2:
        hT = c.sb("hT", [128, 8, TT], BF16, s2)
        memt = c.sb("memt", [128, 2, D], F32, s2)
        mss = c.sb("mss", [128, 2], F32, s2)
        junk = c.sb("junk", [128, D], F32, s2)
        memnT = c.sb("memnT", [128, 8, 256], BF16, s2)
        wkv = c.sb("wkv", [128, 8, D], BF16, s2)
        wq = c.sb("wq", [128, 8, 512], BF16, s2)
        wo = c.sb("wo", [128, 4, D], BF16, s2)
        kT = c.sb("kT", [128, 4, 256], BF16, s2)
        Vt = c.sb("Vt", [128, 2, 512], BF16, s2)
        qT = c.sb("qT", [128, 4, TT], BF16, s2)
        pT = c.sb("pT", [128, 8, TT], BF16, s2)
        rden = c.sb("rden", [128, TT], F32, s2)
        oT = c.sb("oT", [128, 4, TT], BF16, s2)
        kb.dma("sp", memt[:], io["mem"].rearrange("(t p) d -> p t d", p=128), writes=["memt"])
        kb.dma("pool", wkv[:], io["w_kv"].rearrange("(k p) n -> p k n", p=128), writes=["wkv"])
        kb.dma("pool", wq[:], io["w_q"].rearrange("(k p) n -> p k n", p=128), writes=["wq"])
        kb.dma("pool", wo[:], io["w_o"].rearrange("(k p) n -> p k n", p=128), writes=["wo"])
        for mt in range(2):
            kb.op("act", lambda e, mt=mt: e.activation(out=junk[:], in_=memt[:, mt, :], func=AF.Square,
                                                       accum_out=mss[:, mt:mt + 1]),
                  reads=["memt"], writes=["junk", ("mss", mt)])
            kb.op("act", lambda e, mt=mt: e.activation(out=mss[:, mt:mt + 1], in_=mss[:, mt:mt + 1], func=AF.Sqrt,
                                                       scale=1.0 / D, bias=eps_t[:, 0:1]),
                  reads=[("mss", mt), "eps"], writes=[("mss", mt)])
            kb.op("dve", lambda e, mt=mt: e.reciprocal(out=mss[:, mt:mt + 1], in_=mss[:, mt:mt + 1]),
                  reads=[("mss", mt)], writes=[("mss", mt)])
            kb.op("dve", lambda e, mt=mt: e.tensor_scalar(out=memt[:, mt, :], in0=memt[:, mt, :], scalar1=mss[:, mt:mt + 1],
                                                          scalar2=None, op0=ALU.mult),
                  reads=["memt", ("mss", mt)], writes=["memt"])
        for k in range(8):
            p, pk = c.ps()
            for mt in range(2):
                kb.op("pe", lambda e, k=k, mt=mt, p=p: e.transpose(out=p[:, mt * 128:(mt + 1) * 128],
                                                                   in_=memt[:, mt, k * 128:(k + 1) * 128], identity=c.ident_f[:]),
                      reads=["memt", "ident_f"], writes=[pk])
            kb.op("dve", lambda e, k=k, p=p: e.tensor_scalar(out=memnT[:, k, :], in0=p[:, 0:256],
                                                             scalar1=vecs[:, V_MEM + k:V_MEM + k + 1], scalar2=None, op0=ALU.mult),
                  reads=[pk, "vecs"], writes=[("memnT", k)])
        for h in range(4):
            p, pk = c.ps()
            for k in range(8):
                mm(c, p[:, 0:256], wkv[:, k, h * 128:(h + 1) * 128], memnT[:, k, :], k == 0, k == 7, ["wkv", ("memnT", k)], [pk])
            kb.op("act", lambda e, h=h, p=p: e.activation(out=kT[:, h, :], in_=p[:, 0:256], func=AF.Copy),
                  reads=[pk], writes=[("kT", h)])
        for mt in range(2):
            p, pk = c.ps()
            for k in range(8):
                mm(c, p[:, :], memnT[:, k, mt * 128:(mt + 1) * 128], wkv[:, k, 512:1024], k == 0, k == 7, ["wkv", ("memnT", k)], [pk])
            kb.op("act", lambda e, mt=mt, p=p: e.activation(out=Vt[:, mt, :], in_=p[:, :], func=AF.Copy),
                  reads=[pk], writes=[("Vt", mt)])
        sc = 128 ** -0.5
        for t in range(NTT):
            t0 = t * TT
            rmsnorm_tile(c, xT, "xT", t0, TT, vecs[:, V_XA:V_XA + 8], tmp, hT, "hT")
            for h in range(4):
                p, pk = c.ps()
                for k in range(8):
                    mm(c, p[:, :], wq[:, k, h * 128:(h + 1) * 128], hT[:, k, :], k == 0, k == 7, ["wq", ("hT", k)], [pk])
                kb.op("act", lambda e, h=h, p=p: e.activation(out=qT[:, h, :], in_=p[:, :], func=AF.Copy),
                      reads=[pk], writes=[("qT", h)])
            for h in range(4):
                for mt in range(2):
                    p, pk = c.ps()
                    mm(c, p[:, :], kT[:, h, mt * 128:(mt + 1) * 128], qT[:, h, :], True, True, [("kT", h), ("qT", h)], [pk])
                    kb.op("act", lambda e, h=h, mt=mt, p=p: e.activation(out=pT[:, h * 2 + mt, :], in_=p[:, :], func=AF.Exp, scale=sc),
                          reads=[pk], writes=[("pT", h, mt)])
                pd, pdk = c.ps()
                for mt in range(2):
                    mm(c, pd[:, :], c.ones_b[:], pT[:, h * 2 + mt, :], mt == 0, mt == 1, ["ones_b", ("pT", h, mt)], [pdk])
                kb.op("dve", lambda e, pd=pd: e.reciprocal(out=rden[:], in_=pd[:, :]), reads=[pdk], writes=["rden"])
                po, pok = c.ps()
                for mt in range(2):
                    mm(c, po[:, :], Vt[:, mt, h * 128:(h + 1) * 128], pT[:, h * 2 + mt, :], mt == 0, mt == 1,
                       [("Vt", mt), ("pT", h, mt)], [pok])
                kb.op("dve", lambda e, h=h, po=po: e.tensor_tensor(out=oT[:, h, :], in0=po[:, :], in1=rden[:], op=ALU.mult),
                      reads=[pok, "rden"], writes=[("oT", h)])
            for d in range(8):
                p, pk = c.ps()
                for h in range(4):
                    mm(c, p[:, :], wo[:, h, d * 128:(d + 1) * 128], oT[:, h, :], h == 0, h == 3, ["wo", ("oT", h)], [pk])
                kb.op("dve", lambda e, d=d, p=p, t0=t0: e.tensor_tensor(out=xT[:, d, t0:t0 + TT], in0=xT[:, d, t0:t0 + TT],
                                                                        in1=p[:, :], op=ALU.add),
                      reads=[pk, ("xT", d)], writes=[("xT", d)])
    c.barrier()
    if io.get("dbg_stage") == 2:
        vec_st.close()
        return

    with ExitStack() as s3:
        hTall = c.sb("hTall", [128, 8, NT], BF16, s3)
        actT = c.sb("actT", [128, 8, NT], BF16, s3)
        wgu = [c.sb(f"wgu{i}", [128, 8, 256], BF16, s3) for i in range(3)]
        wdr = [c.sb(f"wdr{i}", [128, D], BF16, s3) for i in range(11)]
        sil = [c.sb(f"sil{i}", [128, TT], BF16, s3) for i in range(2)]
        if E > 1:
            wr = c.sb("wr", [128, 8, 8], F32, s3)
            lg = c.sb("lg", [128, 4, 8], F32, s3)
            top8 = c.sb("top8", [128, 4, 8], F32, s3)
            gts = c.sb("gts", [128, 16, 8], F32, s3)
            gsc = c.sb("gsc", [128, 4, 4], F32, s3)
            dgate = c.sb("dgate", [128, 128], F32, s3)
            gB = [c.sb(f"gB{i}", [128, NT], BF16, s3) for i in range(2)]
            ytmps = [c.sb(f"ytmp{i}", [128, TT], F32, s3) for i in range(2)]
            kb.dma("sp", wr[:], io["router_w"].rearrange("(k p) n -> p k n", p=128), writes=["wr"])
        with ExitStack() as s3a:
            hF = c.sb("hF", [128, 8, TT], F32, s3a) if E > 1 else None
            for t in range(NTT):
                t0 = t * TT
                rmsnorm_tile(c, xT, "xT", t0, TT, vecs[:, V_FFN:V_FFN + 8], tmp, hTall[:, :, t0:t0 + TT], f"hA{t}", out_f=hF)
                if E > 1:
                    for s in range(4):
                        p, pk = c.ps()
                        for k in range(8):
                            mm(c, p[:, 0:8], hF[:, k, s * 128:(s + 1) * 128], wr[:, k, :], k == 0, k == 7, [(f"hA{t}_f", k), "wr"], [pk])
                        kb.op("dve", lambda e, s=s, p=p: e.tensor_copy(out=lg[:, s, :], in_=p[:, 0:8]), reads=[pk], writes=[("lg", s)])
                        kb.op("dve", lambda e, s=s: e.max(out=top8[:, s, :], in_=lg[:, s, :]), reads=[("lg", s)], writes=[("top8", s)])
                        kb.op("dve", lambda e, s=s: e.tensor_scalar(out=gsc[:, s, 0:1], in0=top8[:, s, 0:1], scalar1=-1.0, scalar2=None, op0=ALU.mult),
                              reads=[("top8", s)], writes=[("gsc", s, 0)])
                        kb.op("act", lambda e, s=s: e.activation(out=gsc[:, s, 1:2], in_=top8[:, s, 1:2], func=AF.Exp, bias=gsc[:, s, 0:1]),
                              reads=[("top8", s), ("gsc", s, 0)], writes=[("gsc", s, 1)])
                        kb.op("dve", lambda e, s=s: e.tensor_scalar(out=gsc[:, s, 1:2], in0=gsc[:, s, 1:2], scalar1=1.0, scalar2=None, op0=ALU.add),
                              reads=[("gsc", s, 1)], writes=[("gsc", s, 1)])
                        kb.op("dve", lambda e, s=s: e.reciprocal(out=gsc[:, s, 1:2], in_=gsc[:, s, 1:2]),
                              reads=[("gsc", s, 1)], writes=[("gsc", s, 1)])
                        gi = t * 4 + s
                        kb.op("act", lambda e, s=s, gi=gi: e.activation(out=gts[:, gi, :], in_=lg[:, s, :], func=AF.Exp, bias=gsc[:, s, 0:1]),
                              reads=[("lg", s), ("gsc", s, 0)], writes=[("gts", gi)])
                        kb.op("dve", lambda e, s=s: e.tensor_scalar(out=lg[:, s, :], in0=lg[:, s, :], scalar1=top8[:, s, 1:2], scalar2=None, op0=ALU.is_ge),
                              reads=[("lg", s), ("top8", s)], writes=[("lg", s)])
                        kb.op("dve", lambda e, s=s, gi=gi: e.scalar_tensor_tensor(out=gts[:, gi, :], in0=gts[:, gi, :], scalar=gsc[:, s, 1:2], in1=lg[:, s, :],
                                                                                  op0=ALU.mult, op1=ALU.mult),
                              reads=[("gts", gi), ("gsc", s, 1), ("lg", s)], writes=[("gts", gi)])
        hkeys = lambda t: [(f"hA{t}", k) for k in range(8)]
        groups = [(0, 8), (8, 16), (16, 22)]
        wgu_i = 0
        wd_i = 0
        sil_i = 0
        for ex in range(E):
            if E > 1:
                gb = gB[ex % 2]
                gbk = f"gB{ex % 2}"
                for gi in range(16):
                    kb.op("dve", lambda e, gi=gi, ex=ex: e.tensor_scalar(out=dgate[:], in0=c.ident_f[:], scalar1=gts[:, gi, ex:ex + 1],
                                                                         scalar2=None, op0=ALU.mult),
                          reads=["ident_f", ("gts", gi)], writes=["dgate"])
                    p, pk = c.ps()
                    mm(c, p[:, 0:128], c.ones_f[:], dgate[:], True, True, ["ones_f", "dgate"], [pk])
                    kb.op("act", lambda e, gi=gi, p=p, gb=gb: e.activation(out=gb[:, gi * 128:(gi + 1) * 128], in_=p[:, 0:128], func=AF.Copy),
                          reads=[pk], writes=[(gbk, gi // 4)])
            for fa, fb in groups:
                nfg = fb - fa
                for f0 in range(fa, fb, 2):
                    nf = min(2, fb - f0)
                    sg, su = wgu[wgu_i % 3], wgu[(wgu_i + 1) % 3]
                    sgk, suk = f"wgu{wgu_i % 3}", f"wgu{(wgu_i + 1) % 3}"
                    wgu_i += 2
                    kb.dma("pool", sg[:, :, 0:nf * 128], io["w_gate"][ex][:, f0 * 128:(f0 + nf) * 128].rearrange("(k p) n -> p k n", p=128), writes=[sgk])
                    kb.dma("pool", su[:, :, 0:nf * 128], io["w_up"][ex][:, f0 * 128:(f0 + nf) * 128].rearrange("(k p) n -> p k n", p=128), writes=[suk])
                    for fi in range(nf):
                        fl = f0 + fi - fa
                        for t in range(NTT):
                            ts_ = slice(t * TT, (t + 1) * TT)
                            pg, pgk = c.ps()
                            pu, puk = c.ps()
                            for k in range(8):
                                mm(c, pg[:, :], sg[:, k, fi * 128:(fi + 1) * 128], hTall[:, k, ts_], k == 0, k == 7, [sgk, (f"hA{t}", k)], [pgk])
                            for k in range(8):
                                mm(c, pu[:, :], su[:, k, fi * 128:(fi + 1) * 128], hTall[:, k, ts_], k == 0, k == 7, [suk, (f"hA{t}", k)], [puk])
                            sl = sil[sil_i % 2]
                            slk = f"sil{sil_i % 2}"
                            sil_i += 1
                            kb.op("act", lambda e, pg=pg, sl=sl: e.activation(out=sl[:], in_=pg[:, :], func=AF.Silu), reads=[pgk], writes=[slk])
                            kb.op("dve", lambda e, pu=pu, sl=sl, fl=fl, ts_=ts_: e.tensor_tensor(out=actT[:, fl, ts_], in0=pu[:, :], in1=sl[:], op=ALU.mult),
                                  reads=[puk, slk], writes=[("actT", fl, t)])
                slots = []
                for fl in range(nfg):
                    f = fa + fl
                    wd = wdr[wd_i % 11]
                    wdk = f"wdr{wd_i % 11}"
                    wd_i += 1
                    kb.dma("pool", wd[:], io["w_down"][ex][f * 128:(f + 1) * 128, :], writes=[wdk])
                    slots.append((wd, wdk))
                for t in range(NTT):
                    ts_ = slice(t * TT, (t + 1) * TT)
                    for d in range(8):
                        p, pk = c.ps()
                        for fl in range(nfg):
                            wd, wdk = slots[fl]
                            mm(c, p[:, :], wd[:, d * 128:(d + 1) * 128], actT[:, fl, ts_], fl == 0, fl == nfg - 1, [wdk, ("actT", fl, t)], [pk])
                        if E > 1:
                            ytmp = ytmps[d % 2]
                            ytk = f"ytmp{d % 2}"
                            kb.op("dve", lambda e, p=p, gb=gb, ytmp=ytmp, ts_=ts_: e.tensor_tensor(out=ytmp[:], in0=p[:, :], in1=gb[:, ts_], op=ALU.mult),
                                  reads=[pk, (gbk, t)], writes=[ytk])
                            kb.op("dve", lambda e, d=d, ts_=ts_, ytmp=ytmp: e.tensor_tensor(out=xT[:, d, ts_], in0=xT[:, d, ts_], in1=ytmp[:], op=ALU.add),
                                  reads=[ytk, ("xT", d)], writes=[("xT", d)])
                        else:
                            kb.op("dve", lambda e, d=d, p=p, ts_=ts_: e.tensor_tensor(out=xT[:, d, ts_], in0=xT[:, d, ts_], in1=p[:, :], op=ALU.add),
                                  reads=[pk, ("xT", d)], writes=[("xT", d)])
    c.barrier()

    with ExitStack() as s4:
        hT = c.sb("hT", [128, 8, TT], BF16, s4)
        if not last:
            for t in range(NTT):
                t0 = t * TT
                rmsnorm_tile(c, xT, "xT", t0, TT, vecs[:, V_NEXT:V_NEXT + 8], tmp, hT, "hT")
                h_store(c, io["h_next"], hT, t0, TT, [("hT", k) for k in range(8)], ["h_next_d"])
                if t == NTT - 1 and "tail_next" in io:
                    kb.dma("sp", io["tail_next"].rearrange("(k p) n -> p k n", p=128), hT[:, :, TT - 32:TT],
                           reads=[("hT", k) for k in range(8)], writes=["tail_next_d"])
        else:
            hF2 = c.sb("hF2", [128, 8, TT], F32, s4)
            otm = c.sb("otm", [128, 4, D], F32, s4)
            for t in range(NTT):
                t0 = t * TT
                rmsnorm_tile(c, xT, "xT", t0, TT, vecs[:, V_NEXT:V_NEXT + 8], tmp, hT, "hT", out_f=hF2)
                for s in range(4):
                    for kk in range(2):
                        p, pk = c.ps()
                        for k4 in range(4):
                            k = kk * 4 + k4
                            kb.op("pe", lambda e, k=k, k4=k4, s=s, p=p: e.transpose(out=p[:, k4 * 128:(k4 + 1) * 128],
                                                                                    in_=hF2[:, k, s * 128:(s + 1) * 128], identity=c.ident_f[:]),
                                  reads=[("hT_f", k), "ident_f"], writes=[pk])
                        kb.op("act", lambda e, s=s, kk=kk, p=p: e.activation(out=otm[:, s, kk * 512:(kk + 1) * 512], in_=p[:, :], func=AF.Copy),
                              reads=[pk], writes=[("otm", s)])
                kb.dma("sp", io["out"][t0:t0 + TT, :].rearrange("(s p) d -> p s d", p=128), otm[:],
                       reads=[("otm", s) for s in range(4)])
    c.barrier()
    vec_st.close()


from contextlib import ExitStack as _ES
import ml_dtypes as _mld

NPBF = _mld.bfloat16


def build_T(E, last, dbg_stage=0):
    nc = bass.Bass("TRN2", target_bir_lowering=False)
    io = {}

    def din(name, shape, dt=F32):
        io[name] = nc.dram_tensor(name, shape, dt, kind="ExternalInput").ap()

    def dout(name, shape, dt=F32):
        io[name] = nc.dram_tensor(name, shape, dt, kind="ExternalOutput").ap()

    din("xT_in", [D, NT]); din("h_own", [D, NT], BF16); din("h_halo", [D, 32], BF16)
    din("catm", [4, 64, NT], BF16); din("catf", [4, 128, NT], BF16); din("mem", [256, D])
    din("vecs", [128, NV_T]); din("w_c", [D, 512]); din("w_out", [D, D]); din("w_q", [D, 512])
    din("w_kv", [D, D]); din("w_o", [512, D])
    din("w_gate", [E, D, DFF]); din("w_up", [E, D, DFF]); din("w_down", [E, DFF, D])
    if E > 1:
        din("router_w", [D, 8])
    if last:
        dout("out", [NT, D])
    else:
        dout("xT_out", [D, NT]); dout("h_next", [D, NT], BF16)
    io["dbg_stage"] = dbg_stage
    with _ES() as st:
        c = Ctx(nc, st)
        c.setup()
        xT = c.sb("xT", [128, 8, NT], F32)
        c.kb.dma("sp", xT[:], io["xT_in"].rearrange("(k p) n -> p k n", p=128), writes=[("xT", k) for k in range(8)])
        phase_T(c, io, E, last and not dbg_stage, xT)
        evs = []
        if not last or dbg_stage:
            key = "xT_out" if not last else "out"
            if last:
                io["xT_dbg"] = None
            evs.append(c.kb.dma("sp", io["xT_out"].rearrange("(k p) n -> p k n", p=128), xT[:],
                                reads=[("xT", k) for k in range(8)]))
        c.barrier()
        c.kb.flush()
    return nc


def vecs_T(inp, l, last):
    v = np.zeros((128, NV_T), np.float32)
    fm = lambda w: np.asarray(w, np.float32).reshape(-1, 128).T
    v[:, 0:8] = fm(inp["norm_xattn_w"][l]); v[:, 8:16] = fm(inp["norm_mem_w"][l]); v[:, 16:24] = fm(inp["norm_ffn_w"][l])
    v[:, 24:32] = fm(inp["norm_final_w"]) if last else fm(inp["norm_mix_w"][l + 1])
    v[:, 32:34] = fm(inp["conf_conv_b"][l]); v[:, 34:36] = fm(inp["conf_ln_w"][l]); v[:, 36:38] = fm(inp["conf_ln_b"][l])
    cw = np.asarray(inp["conf_conv_w"][l], np.float32)
    for j in range(31):
        v[:, 38 + 2 * j:40 + 2 * j] = fm(cw[j])
    return v


NCH = SEQ // 64
NKT = SEQ // 128
NQT = SEQ // TT
GRP = 4


def AP3(t, off, dims):
    return bass.AP(t[:].tensor, off, [list(d) for d in dims])


def log_sigmoid_tile(c, x, out, tmp1, tmp2, bias_ap, keys):
    kb = c.kb
    kx, ko, k1, k2 = keys
    kb.op("dve", lambda e: e.tensor_scalar(out=x, in0=x, scalar1=bias_ap, scalar2=None, op0=ALU.add), reads=[kx, "vecsM"], writes=[kx])
    kb.op("dve", lambda e: e.tensor_scalar(out=tmp1, in0=x, scalar1=-1.0, scalar2=None, op0=ALU.mult), reads=[kx], writes=[k1])
    kb.op("dve", lambda e: e.tensor_tensor(out=tmp1, in0=tmp1, in1=x, op=ALU.max), reads=[kx, k1], writes=[k1])
    kb.op("act", lambda e: e.activation(out=tmp1, in_=tmp1, func=AF.Exp, scale=-1.0), reads=[k1], writes=[k1])
    kb.op("dve", lambda e: e.tensor_scalar(out=tmp1, in0=tmp1, scalar1=1.0, scalar2=None, op0=ALU.add), reads=[k1], writes=[k1])
    kb.op("act", lambda e: e.activation(out=tmp1, in_=tmp1, func=AF.Ln), reads=[k1], writes=[k1])
    kb.op("dve", lambda e: e.tensor_scalar(out=tmp2, in0=x, scalar1=0.0, scalar2=None, op0=ALU.min), reads=[kx], writes=[k2])
    kb.op("dve", lambda e: e.tensor_tensor(out=out, in0=tmp2, in1=tmp1, op=ALU.subtract), reads=[k1, k2], writes=[ko])


def phase_M_mlstm(c, io):
    nc, kb = c.nc, c.kb
    from contextlib import ExitStack
    with ExitStack() as s0:
        vm = c.sb("vecsM_sb", [128, 16], F32, s0)
        wml = c.sb("wml", [128, 8, 258], BF16, s0)
        qT = c.sb("m_qT", [64, SEQ], BF16, s0)
        kT = c.sb("m_kT", [64, SEQ], BF16, s0)
        Vaug = c.sb("m_Vaug", [64, NCH, 65], BF16, s0)
        og = c.sb("m_og", [64, SEQ], BF16, s0)
        iC = c.sb("m_iC", [128, 64], F32, s0)
        fC = c.sb("m_fC", [128, 64], F32, s0)
        eps_t = c.sb("m_eps", [128, 1], F32, s0)
        kb.dma("sp", vm[:], io["vecsM"], writes=["vecsM"])
        kb.dma("pool", wml[:], io["w_ml"].rearrange("(k p) n -> p k n", p=128), writes=["wml"])
        kb.op("pool", lambda e: e.memset(Vaug[:, :, 64:65], 1.0), writes=["Vaug1"])
        kb.op("pool", lambda e: e.memset(eps_t[:], EPS), writes=["m_eps"])
        with ExitStack() as s1:
            hTb = [c.sb(f"m_hT{i}", [128, 8, TT], BF16, s1) for i in range(2)]
            zq = c.sb("m_zq", [64, TT + 3], F32, s1)
            zk = c.sb("m_zk", [64, TT + 3], F32, s1)
            cacc = [c.sb(f"m_cacc{i}", [64, TT], F32, s1) for i in range(2)]
            vt = c.sb("m_vt", [64, TT], F32, s1)
            rows = [c.sb(f"m_rows{i}", [2, TT], F32, s1) for i in range(2)]
            kb.op("pool", lambda e: e.memset(zq[:, 0:3], 0.0), writes=["zq"])
            kb.op("pool", lambda e: e.memset(zk[:, 0:3], 0.0), writes=["zk"])
            for tt in range(NQT):
                j, off = tt // 4, (tt % 4) * TT
                hT = hTb[tt % 2]
                hk = f"m_hT{tt % 2}"
                tok = slice(tt * TT, (tt + 1) * TT)
                h_load(c, io["hT_all"], hT, off, TT, [hk], j=j)
                for nm, z, col0, vc, dst in (("q", zq, 0, 0, qT), ("k", zk, 64, 5, kT)):
                    p, pk = c.ps()
                    for k in range(8):
                        mm(c, p[0:64, :], wml[:, k, col0:col0 + 64], hT[:, k, :], k == 0, k == 7, ["wml", hk], [pk])
                    zkey = "z" + nm
                    kb.op("act", lambda e, z=z, p=p: e.activation(out=z[:, 3:TT + 3], in_=p[0:64, :], func=AF.Copy), reads=[pk], writes=[zkey])
                    ca = cacc[0 if nm == "q" else 1]
                    ck = "cacc" + nm
                    kb.op("dve", lambda e, z=z, ca=ca, vc=vc: e.tensor_scalar(out=ca[:], in0=z[:, 0:TT], scalar1=vm[0:64, vc:vc + 1], scalar2=vm[0:64, vc + 4:vc + 5],
                                                                              op0=ALU.mult, op1=ALU.add), reads=[zkey, "vecsM"], writes=[ck])
                    for jj in range(1, 4):
                        kb.op("dve", lambda e, z=z, ca=ca, vc=vc, jj=jj: e.scalar_tensor_tensor(out=ca[:], in0=z[:, jj:jj + TT], scalar=vm[0:64, vc + jj:vc + jj + 1], in1=ca[:],
                                                                                                 op0=ALU.mult, op1=ALU.add), reads=[zkey, ck, "vecsM"], writes=[ck])
                    kb.op("act", lambda e, ca=ca, dst=dst, tok=tok: e.activation(out=dst[:, tok], in_=ca[:], func=AF.Silu), reads=[ck], writes=[("m_" + nm + "T", tt)])
                    kb.op("dve", lambda e, z=z: e.tensor_copy(out=z[:, 0:3], in_=z[:, TT:TT + 3]), reads=[zkey], writes=[zkey])
                p, pk = c.ps()
                for k in range(8):
                    mm(c, p[0:64, :], wml[:, k, 128:192], hT[:, k, :], k == 0, k == 7, ["wml", hk], [pk])
                kb.op("act", lambda e, p=p: e.activation(out=vt[:], in_=p[0:64, :], func=AF.Copy), reads=[pk], writes=["m_vt"])
                p2, p2k = c.ps()
                for ci in range(8):
                    kb.op("pe", lambda e, ci=ci, p2=p2: e.transpose(out=p2[0:64, ci * 64:(ci + 1) * 64], in_=vt[:, ci * 64:(ci + 1) * 64], identity=c.ident_f[0:64, 0:64]),
                          reads=["m_vt", "ident_f"], writes=[p2k])
                kb.op("dve", lambda e, p2=p2, tt=tt: e.tensor_copy(out=Vaug[:, tt * 8:(tt + 1) * 8, 0:64], in_=p2[0:64, :].rearrange("p (c d) -> p c d", d=64)),
                      reads=[p2k], writes=[("Vaug", tt)])
                p, pk = c.ps()
                for k in range(8):
                    mm(c, p[0:64, :], wml[:, k, 192:256], hT[:, k, :], k == 0, k == 7, ["wml", hk], [pk])
                kb.op("act", lambda e, p=p, tok=tok: e.activation(out=og[:, tok], in_=p[0:64, :], func=AF.Sigmoid), reads=[pk], writes=[("og", tt)])
                p, pk = c.ps()
                for k in range(8):
                    mm(c, p[0:2, :], wml[:, k, 256:258], hT[:, k, :], k == 0, k == 7, ["wml", hk], [pk])
                rw = rows[tt % 2]
                rk = f"m_rows{tt % 2}"
                kb.op("act", lambda e, p=p, rw=rw: e.activation(out=rw[:], in_=p[0:2, :], func=AF.Copy), reads=[pk], writes=[rk])
                kb.dma("sp", iC[tt * 8:(tt + 1) * 8, :], AP3(rw, 0, [[TT, 1], [64, 8], [1, 64]]), reads=[rk], writes=["iC"])
                kb.dma("sp", fC[tt * 8:(tt + 1) * 8, :], AP3(rw, TT, [[TT, 1], [64, 8], [1, 64]]), reads=[rk], writes=["fC"])
        c.barrier()
        Uall = c.sb("m_Uall", [64, 65, NCH], F32, s0)
        wgT = c.sb("m_wgT", [64, NCH], F32, s0)
        flT = c.sb("m_flT", [64, NCH], F32, s0)
        dB = c.sb("m_dB", [64, NCH], F32, s0)
        dB0 = c.sb("m_dB0", [64, NCH], F32, s0)
        with ExitStack() as s2:
            t1 = c.sb("g_t1", [128, 64], F32, s2)
            t2 = c.sb("g_t2", [128, 64], F32, s2)
            lf = c.sb("g_lf", [128, 64], F32, s2)
            bb = c.sb("g_b", [128, 64], F32, s2)
            aa = c.sb("g_a", [128, 64], F32, s2)
            AA = c.sb("g_A", [128, 64], F32, s2)
            MM = c.sb("g_M", [128, 64], F32, s2)
            wg = c.sb("g_wg", [128, 64], F32, s2)
            fl = c.sb("g_fl", [128, 64], F32, s2)
            on = c.sb("g_on", [128, 64], F32, s2)
            r1 = c.sb("g_r1", [1, 128], F32, s2)
            r2 = c.sb("g_r2", [1, 128], F32, s2)
            r3 = c.sb("g_r3", [1, 128], F32, s2)
            r4 = c.sb("g_r4", [1, 128], F32, s2)
            mcol = c.sb("g_mcol", [128, 1], F32, s2)
            nM63 = c.sb("g_nM63", [128, 1], F32, s2)
            dec = c.sb("g_dec", [128, 1], F32, s2)
            dgd = c.sb("g_dgd", [128, 128], F32, s2)
            Xb = [c.sb(f"g_X{i}", [128, 8, 64], F32, s2) for i in range(2)]
            kw32 = [c.sb(f"g_kw32{i}", [64, TT], F32, s2) for i in range(2)]
            kwTok = c.sb("g_kwTok", [64, NCH, 64], BF16, s2)
            log_sigmoid_tile(c, fC[:], lf[:], t1[:], t2[:], vm[:, 12:13], ("fC", "g_lf", "g_t1", "g_t2"))
            kb.op("dve", lambda e: e.tensor_scalar(out=iC[:], in0=iC[:], scalar1=vm[:, 11:12], scalar2=None, op0=ALU.add), reads=["iC", "vecsM"], writes=["iC"])
            kb.op("pool", lambda e: e.memset(on[:], 1.0), writes=["g_on"])
            kb.op("dve", lambda e: e.tensor_tensor_scan(out=bb[:], data0=on[:], data1=lf[:], initial=0.0, op0=ALU.mult, op1=ALU.add),
                  reads=["g_on", "g_lf"], writes=["g_b"])
            kb.op("dve", lambda e: e.tensor_tensor(out=aa[:], in0=iC[:], in1=bb[:], op=ALU.subtract), reads=["iC", "g_b"], writes=["g_a"])
            kb.op("dve", lambda e: e.tensor_tensor_scan(out=AA[:], data0=aa[:], data1=aa[:], initial=-1e30, op0=ALU.max, op1=ALU.max),
                  reads=["g_a"], writes=["g_A"])
            p, pk = c.ps()
            kb.op("pe", lambda e, p=p: e.transpose(out=p[0:1, 0:128], in_=AA[:, 63:64], identity=c.ident_f[:]), reads=["g_A", "ident_f"], writes=[pk])
            kb.op("pe", lambda e, p=p: e.transpose(out=p[0:1, 128:256], in_=bb[:, 63:64], identity=c.ident_f[:]), reads=["g_b", "ident_f"], writes=[pk])
            kb.op("dve", lambda e, p=p: e.tensor_copy(out=r1[:], in_=p[0:1, 0:128]), reads=[pk], writes=["g_r1"])
            kb.op("dve", lambda e, p=p: e.tensor_copy(out=r2[:], in_=p[0:1, 128:256]), reads=[pk], writes=["g_r2"])
            kb.op("dve", lambda e: e.tensor_tensor_scan(out=r3[:], data0=r1[:], data1=r2[:], initial=0.0, op0=ALU.max, op1=ALU.add),
                  reads=["g_r1", "g_r2"], writes=["g_r3"])
            kb.op("pool", lambda e: e.memset(r4[:, 0:1], 0.0), writes=["g_r4a"])
            kb.op("dve", lambda e: e.tensor_copy(out=r4[:, 1:128], in_=r3[:, 0:127]), reads=["g_r3"], writes=["g_r4b"])
            p, pk = c.ps()
            kb.op("pe", lambda e, p=p: e.transpose(out=p[:, 0:1], in_=r4[:], identity=c.ident_f[0:1, 0:1]), reads=["g_r4a", "g_r4b", "ident_f"], writes=[pk])
            kb.op("dve", lambda e, p=p: e.tensor_copy(out=mcol[:], in_=p[:, 0:1]), reads=[pk], writes=["g_mcol"])
            kb.op("dve", lambda e: e.tensor_scalar(out=MM[:], in0=AA[:], scalar1=mcol[:, 0:1], scalar2=None, op0=ALU.max), reads=["g_A", "g_mcol"], writes=["g_M"])
            kb.op("dve", lambda e: e.tensor_scalar(out=nM63[:], in0=MM[:, 63:64], scalar1=-1.0, scalar2=None, op0=ALU.mult), reads=["g_M"], writes=["g_nM63"])
            kb.op("act", lambda e: e.activation(out=wg[:], in_=aa[:], func=AF.Exp, bias=nM63[:, 0:1]), reads=["g_a", "g_nM63"], writes=["g_wg"])
            kb.op("act", lambda e: e.activation(out=dec[:], in_=mcol[:], func=AF.Exp, bias=nM63[:, 0:1]), reads=["g_mcol", "g_nM63"], writes=["g_dec"])
            kb.op("act", lambda e: e.activation(out=fl[:], in_=bb[:], func=AF.Exp, scale=-1.0, bias=nM63[:, 0:1]), reads=["g_b", "g_nM63"], writes=["g_fl"])
            p, pk = c.ps()
            kb.op("pe", lambda e, p=p: e.transpose(out=p[0:64, 0:128], in_=wg[:], identity=c.ident_f[:]), reads=["g_wg", "ident_f"], writes=[pk])
            kb.op("pe", lambda e, p=p: e.transpose(out=p[0:64, 128:256], in_=fl[:], identity=c.ident_f[:]), reads=["g_fl", "ident_f"], writes=[pk])
            kb.op("dve", lambda e, p=p: e.tensor_copy(out=wgT[:], in_=p[0:64, 0:128]), reads=[pk], writes=["m_wgT"])
            kb.op("dve", lambda e, p=p: e.tensor_copy(out=flT[:], in_=p[0:64, 128:256]), reads=[pk], writes=["m_flT"])
            kb.op("dve", lambda e: e.tensor_scalar(out=dgd[:], in0=c.ident_f[:], scalar1=dec[:, 0:1], scalar2=None, op0=ALU.mult), reads=["ident_f", "g_dec"], writes=["g_dgd"])
            p, pk = c.ps()
            mm(c, p[0:64, 0:128], c.ones_f[:, 0:64], dgd[:], True, True, ["ones_f", "g_dgd"], [pk])
            kb.op("dve", lambda e, p=p: e.tensor_copy(out=dB[:], in_=p[0:64, 0:128]), reads=[pk], writes=["m_dB"])
            kb.op("dve", lambda e, p=p: e.tensor_copy(out=dB0[:], in_=p[0:64, 0:128]), reads=[pk], writes=["m_dB0"])
            kb.op("pool", lambda e: e.memset(dB0[:, 0:1], 0.0), reads=["m_dB0"], writes=["m_dB0"])
            for tt in range(NQT):
                tok = slice(tt * TT, (tt + 1) * TT)
                X = Xb[tt % 2]
                Xk = f"g_X{tt % 2}"
                kb.op("dve", lambda e, X=X, tt=tt: e.tensor_tensor(out=X[:], in0=AP3(c.ident_f, 8 * tt, [[128, 128], [1, 8], [0, 64]]),
                                                                   in1=AP3(wg, 0, [[64, 128], [0, 8], [1, 64]]), op=ALU.mult),
                      reads=["ident_f", "g_wg"], writes=[Xk])
                p, pk = c.ps()
                mm(c, p[0:64, :], c.ones_f[:, 0:64], X[:].rearrange("p c s -> p (c s)"), True, True, ["ones_f", Xk], [pk])
                k32 = kw32[tt % 2]
                k32k = f"g_kw32{tt % 2}"
                kb.op("dve", lambda e, p=p, k32=k32, tok=tok: e.scalar_tensor_tensor(out=k32[:], in0=kT[:, tok], scalar=0.125, in1=p[0:64, :], op0=ALU.mult, op1=ALU.mult),
                      reads=[pk, ("m_kT", tt)], writes=[k32k])
                kb.op("act", lambda e, k32=k32, tok=tok: e.activation(out=kT[:, tok], in_=k32[:], func=AF.Copy), reads=[k32k], writes=[("m_kT", tt)])
                p2, p2k = c.ps()
                for ci in range(8):
                    kb.op("pe", lambda e, ci=ci, p2=p2, k32=k32: e.transpose(out=p2[0:64, ci * 64:(ci + 1) * 64], in_=k32[:, ci * 64:(ci + 1) * 64], identity=c.ident_f[0:64, 0:64]),
                          reads=[k32k, "ident_f"], writes=[p2k])
                kb.op("dve", lambda e, p2=p2, tt=tt: e.tensor_copy(out=kwTok[:, tt * 8:(tt + 1) * 8, :], in_=p2[0:64, :].rearrange("p (c d) -> p c d", d=64)),
                      reads=[p2k], writes=[("kwTok", tt)])
            for g in range(NCH // GRP):
                p, pk = c.ps()
                for ci in range(GRP):
                    ch = g * GRP + ci
                    mm(c, p[0:64, ci * 65:(ci + 1) * 65], kwTok[:, ch, :], Vaug[:, ch, :], True, True, [("kwTok", ch // 8), ("Vaug", ch // 8), "Vaug1"], [pk])
                kb.op("dve", lambda e, p=p, g=g: e.tensor_copy(out=AP3(Uall, g * GRP, [[65 * NCH, 64], [1, GRP], [NCH, 65]]),
                                                               in_=p[0:64, 0:GRP * 65].rearrange("p (c d) -> p c d", d=65)),
                      reads=[pk], writes=["Uall"])
        c.barrier()
        Cn = Uall
        Eb = c.sb("m_E", [64, NCH, 65], BF16, s0)
        for dv in range(65):
            kb.op("dve", lambda e, dv=dv: e.tensor_tensor_scan(out=Cn[:, dv, :], data0=dB0[:], data1=Uall[:, dv, :], initial=0.0, op0=ALU.mult, op1=ALU.add),
                  reads=["m_dB0", "Uall"], writes=[("Cn", dv), "Uall"])
        kb.op("pool", lambda e: e.memset(Eb[:, 0:1, :], 0.0), writes=["E0"])
        kb.op("dve", lambda e: e.tensor_tensor(out=Eb[:, 1:NCH, :], in0=AP3(Cn, 0, [[65 * NCH, 64], [1, NCH - 1], [NCH, 65]]),
                                               in1=AP3(dB, 1, [[NCH, 64], [1, NCH - 1], [0, 65]]), op=ALU.mult),
              reads=[("Cn", dv) for dv in range(65)] + ["m_dB"], writes=["E"])
        with ExitStack() as s4:
            mask = c.sb("o_mask", [64, 64], F32, s4)
            sT = [c.sb(f"o_sT{i}", [64, GRP * 64], BF16, s4) for i in range(2)]
            den = c.sb("o_den", [64, GRP], F32, s4)
            hn = c.sb("o_hn", [64, GRP, 64], F32, s4)
            hsq = c.sb("o_hsq", [64, GRP, 64], F32, s4)
            ss = c.sb("o_ss", [64, GRP], F32, s4)
            cst = [c.sb(f"o_cst{i}", [64, GRP * 64], BF16, s4) for i in range(2)]
            kb.op("pool", lambda e: e.memset(mask[:], 1.0), writes=["o_mask"])
            kb.op("pool", lambda e: e.affine_select(out=mask[:], in_=mask[:], pattern=[[1, 64]], compare_op=ALU.is_ge, fill=0.0, base=0, channel_multiplier=-1),
                  reads=["o_mask"], writes=["o_mask"])
            for g in range(NCH // GRP):
                c0 = g * GRP
                p, pk = c.ps()
                for ci in range(GRP):
                    ch = c0 + ci
                    cs = slice(ch * 64, (ch + 1) * 64)
                    mm(c, p[0:64, ci * 64:(ci + 1) * 64], kT[:, cs], qT[:, cs], True, True, [("m_kT", ch // 8), ("m_qT", ch // 8)], [pk])
                st_ = sT[g % 2]
                stk = f"o_sT{g % 2}"
                kb.op("dve", lambda e, p=p, st_=st_: e.tensor_tensor(out=st_[:].rearrange("p (c t) -> p c t", t=64), in0=p[0:64, 0:GRP * 64].rearrange("p (c t) -> p c t", t=64),
                                                                     in1=AP3(mask, 0, [[64, 64], [0, GRP], [1, 64]]), op=ALU.mult),
                      reads=[pk, "o_mask"], writes=[stk])
                po, pok = c.ps()
                for ci in range(GRP):
                    ch = c0 + ci
                    cs = slice(ch * 64, (ch + 1) * 64)
                    mm(c, po[0:64, ci * 65:(ci + 1) * 65], st_[:, ci * 64:(ci + 1) * 64], Vaug[:, ch, :], True, False, [stk, ("Vaug", ch // 8), "Vaug1"], [pok])
                    mm(c, po[0:64, ci * 65:(ci + 1) * 65], qT[:, cs], Eb[:, ch, :], False, True, [("m_qT", ch // 8), "E", "E0"], [pok])
                po3 = po[0:64, 0:GRP * 65].rearrange("p (c d) -> p c d", d=65)
                den3 = den[:].rearrange("p (c o) -> p c o", o=1)
                kb.op("dve", lambda e, po3=po3, den3=den3: e.tensor_scalar(out=den3, in0=po3[:, :, 64:65], scalar1=-1.0, scalar2=None, op0=ALU.mult),
                      reads=[pok], writes=["o_den"])
                kb.op("dve", lambda e, po3=po3, den3=den3: e.tensor_tensor(out=den3, in0=po3[:, :, 64:65], in1=den3, op=ALU.max),
                      reads=[pok, "o_den"], writes=["o_den"])
                kb.op("dve", lambda e, c0=c0: e.tensor_tensor(out=den[:], in0=den[:], in1=flT[:, c0:c0 + GRP], op=ALU.max),
                      reads=["o_den", "m_flT"], writes=["o_den"])
                kb.op("dve", lambda e: e.reciprocal(out=den[:], in_=den[:]), reads=["o_den"], writes=["o_den"])
                kb.op("dve", lambda e, po3=po3: e.tensor_tensor(out=hn[:], in0=po3[:, :, 0:64], in1=AP3(den, 0, [[GRP, 64], [1, GRP], [0, 64]]), op=ALU.mult),
                      reads=[pok, "o_den"], writes=["o_hn"])
                kb.op("act", lambda e: e.activation(out=hsq[:], in_=hn[:], func=AF.Square), reads=["o_hn"], writes=["o_hsq"])
                kb.op("dve", lambda e: e.tensor_reduce(out=ss[:], in_=hsq[:], axis=AX.X, op=ALU.add), reads=["o_hsq"], writes=["o_ss"])
                kb.op("act", lambda e: e.activation(out=ss[:], in_=ss[:], func=AF.Sqrt, scale=1.0 / 64, bias=eps_t[0:64, 0:1]), reads=["o_ss", "m_eps"], writes=["o_ss"])
                kb.op("dve", lambda e: e.reciprocal(out=ss[:], in_=ss[:]), reads=["o_ss"], writes=["o_ss"])
                kb.op("dve", lambda e: e.tensor_tensor(out=hn[:], in0=hn[:], in1=AP3(ss, 0, [[GRP, 64], [1, GRP], [0, 64]]), op=ALU.mult),
                      reads=["o_hn", "o_ss"], writes=["o_hn"])
                pt, ptk = c.ps()
                for ci in range(GRP):
                    kb.op("pe", lambda e, ci=ci, pt=pt: e.transpose(out=pt[0:64, ci * 64:(ci + 1) * 64], in_=hn[:, ci, :], identity=c.ident_f[0:64, 0:64]),
                          reads=["o_hn", "ident_f"], writes=[ptk])
                cs_ = cst[g % 2]
                csk = f"o_cst{g % 2}"
                toks = slice(c0 * 64, (c0 + GRP) * 64)
                kb.op("dve", lambda e, pt=pt, cs_=cs_, toks=toks: e.scalar_tensor_tensor(out=cs_[:], in0=pt[0:64, 0:GRP * 64], scalar=vm[0:64, 10:11], in1=og[:, toks],
                                                                                       op0=ALU.mult, op1=ALU.mult),
                      reads=[ptk, "vecsM", ("og", (c0 * 64) // TT)], writes=[csk])
                kb.dma("sp", io["catm_out"][:, toks], cs_[:], reads=[csk])
        c.barrier()


def phase_M_fox(c, io):
    nc, kb = c.nc, c.kb
    from contextlib import ExitStack
    NEG = -30000.0
    with ExitStack() as s0:
        vm = c.sb("vecsMf_sb", [128, 16], F32, s0)
        wfx = c.sb("wfx", [128, 8, 386], BF16, s0)
        fq = [c.sb(f"f_q{h}", [128, SEQ], BF16, s0) for h in range(2)]
        fkk = c.sb("f_kk", [128, SEQ], BF16, s0)
        kb.op("pool", lambda e: e.memset(fq[0][64:128, :], 0.0), writes=[("fqz", 0)])
        kb.op("pool", lambda e: e.memset(fq[1][0:64, :], 0.0), writes=[("fqz", 1)])
        fV = [c.sb(f"f_V{h}", [128, NKT, 65], BF16, s0) for h in range(2)]
        fC = [c.sb(f"f_C{h}", [64, 128], F32, s0) for h in range(2)]
        kb.dma("sp", vm[:], io["vecsM"], writes=["vecsM"])
        kb.dma("pool", wfx[:], io["w_fx"].rearrange("(k p) n -> p k n", p=128), writes=["wfx"])
        for h in range(2):
            kb.op("pool", lambda e, h=h: e.memset(fV[h][:, :, 64:65], 1.0), writes=[("fV1", h)])
        with ExitStack() as s1:
            hTb = [c.sb(f"f_hT{i}", [128, 8, TT], BF16, s1) for i in range(2)]
            vt = c.sb("f_vt", [128, TT], F32, s1)
            rows = [c.sb(f"f_rows{i}", [2, TT], F32, s1) for i in range(2)]
            for tt in range(NQT):
                j, off = tt // 4, (tt % 4) * TT
                hT = hTb[tt % 2]
                hk = f"f_hT{tt % 2}"
                tok = slice(tt * TT, (tt + 1) * TT)
                h_load(c, io["hT_all"], hT, off, TT, [hk], j=j)
                p, pk = c.ps()
                for k in range(8):
                    mm(c, p[:, :], wfx[:, k, 0:128], hT[:, k, :], k == 0, k == 7, ["wfx", hk], [pk])
                kb.op("act", lambda e, p=p, tok=tok: e.activation(out=fq[0][0:64, tok], in_=p[0:64, :], func=AF.Copy, scale=0.125), reads=[pk], writes=[("fq", 0, tt)])
                kb.op("act", lambda e, p=p, tok=tok: e.activation(out=fq[1][64:128, tok], in_=p[64:128, :], func=AF.Copy, scale=0.125), reads=[pk], writes=[("fq", 1, tt)])
                p, pk = c.ps()
                for k in range(8):
                    mm(c, p[:, :], wfx[:, k, 128:256], hT[:, k, :], k == 0, k == 7, ["wfx", hk], [pk])
                kb.op("act", lambda e, p=p, tok=tok: e.activation(out=fkk[:, tok], in_=p[:, :], func=AF.Copy), reads=[pk], writes=[("fk", tt)])
                p, pk = c.ps()
                for k in range(8):
                    mm(c, p[:, :], wfx[:, k, 256:384], hT[:, k, :], k == 0, k == 7, ["wfx", hk], [pk])
                kb.op("act", lambda e, p=p: e.activation(out=vt[:], in_=p[:, :], func=AF.Copy), reads=[pk], writes=["f_vt"])
                p2, p2k = c.ps()
                for ci in range(4):
                    kb.op("pe", lambda e, ci=ci, p2=p2: e.transpose(out=p2[:, ci * 128:(ci + 1) * 128], in_=vt[:, ci * 128:(ci + 1) * 128], identity=c.ident_f[:]),
                          reads=["f_vt", "ident_f"], writes=[p2k])
                for h in range(2):
                    kb.op("dve", lambda e, p2=p2, tt=tt, h=h: e.tensor_copy(out=fV[h][:, tt * 4:(tt + 1) * 4, 0:64],
                                                                         in_=p2[:, :].rearrange("p (c d) -> p c d", d=128)[:, :, h * 64:(h + 1) * 64]),
                          reads=[p2k], writes=[("fV", h, tt)])
                p, pk = c.ps()
                for k in range(8):
                    mm(c, p[0:2, :], wfx[:, k, 384:386], hT[:, k, :], k == 0, k == 7, ["wfx", hk], [pk])
                rw = rows[tt % 2]
                rk = f"f_rows{tt % 2}"
                kb.op("act", lambda e, p=p, rw=rw: e.activation(out=rw[:], in_=p[0:2, :], func=AF.Copy), reads=[pk], writes=[rk])
                for h in range(2):
                    kb.dma("sp", fC[h][tt * 4:(tt + 1) * 4, :], AP3(rw, h * TT, [[TT, 1], [128, 4], [1, 128]]), reads=[rk], writes=[("fC", h)])
        c.barrier()
        ckT = [c.sb(f"f_ckT{h}", [128, NKT], F32, s0) for h in range(2)]
        cC = [c.sb(f"f_cC{h}", [64, 128], F32, s0) for h in range(2)]
        negm = c.sb("f_negm", [128, 4, TT], F32, s0)
        Ls = c.sb("f_Ls", [64, 64], F32, s0)
        with ExitStack() as s2:
            t1 = c.sb("f_t1", [64, 128], F32, s2)
            t2 = c.sb("f_t2", [64, 128], F32, s2)
            lf = c.sb("f_lf", [64, 128], F32, s2)
            on = c.sb("f_on", [64, 128], F32, s2)
            pre = c.sb("f_pre", [64, 1], F32, s2)
            kb.op("pool", lambda e: e.memset(on[:], 1.0), writes=["f_on"])
            kb.op("pool", lambda e: e.memset(Ls[:], 1.0), writes=["f_Ls"])
            kb.op("pool", lambda e: e.affine_select(out=Ls[:], in_=Ls[:], pattern=[[1, 64]], compare_op=ALU.is_ge, fill=0.0, base=-1, channel_multiplier=-1),
                  reads=["f_Ls"], writes=["f_Ls"])
            for r in range(4):
                kb.op("pool", lambda e, r=r: e.memset(negm[:, r, :], 0.0), writes=[("negm", r)])
                kb.op("pool", lambda e, r=r: e.affine_select(out=negm[:, r, :], in_=negm[:, r, :], pattern=[[1, TT]], compare_op=ALU.is_ge, fill=NEG,
                                                             base=-128 * r, channel_multiplier=-1), reads=[("negm", r)], writes=[("negm", r)])
            for h in range(2):
                log_sigmoid_tile(c, fC[h][:], lf[:], t1[:], t2[:], vm[0:64, 13 + h:14 + h], (("fC", h), "f_lf", "f_t1", "f_t2"))
                kb.op("dve", lambda e, h=h: e.tensor_tensor_scan(out=cC[h][:], data0=on[:], data1=lf[:], initial=0.0, op0=ALU.mult, op1=ALU.add),
                      reads=["f_on", "f_lf"], writes=[("cC", h)])
                p, pk = c.ps()
                mm(c, p[0:64, 0:1], Ls[:], cC[h][:, 127:128], True, True, ["f_Ls", ("cC", h)], [pk])
                kb.op("dve", lambda e, p=p: e.tensor_copy(out=pre[:], in_=p[0:64, 0:1]), reads=[pk], writes=["f_pre"])
                kb.op("dve", lambda e, h=h: e.tensor_scalar(out=cC[h][:], in0=cC[h][:], scalar1=pre[:, 0:1], scalar2=None, op0=ALU.add), reads=[("cC", h), "f_pre"], writes=[("cC", h)])
                p, pk = c.ps()
                kb.op("pe", lambda e, p=p, h=h: e.transpose(out=p[:, 0:64], in_=cC[h][:], identity=c.ident_f[0:64, 0:64]), reads=[("cC", h), "ident_f"], writes=[pk])
                kb.op("dve", lambda e, p=p, h=h: e.tensor_scalar(out=ckT[h][:], in0=p[:, 0:64], scalar1=-1.0, scalar2=None, op0=ALU.mult), reads=[pk], writes=[("ckT", h)])
        c.barrier()
        with ExitStack() as s3:
            X = c.sb("f_X", [64, 4, 128], F32, s3)
            cqB = c.sb("f_cqB", [128, TT], F32, s3)
            cqD = c.sb("f_cqD", [128, 4, TT], F32, s3)
            tmpb = [c.sb(f"f_tmp{i}", [128, TT], F32, s3) for i in range(3)]
            pTb = [c.sb(f"f_pT{i}", [128, TT], BF16, s3) for i in range(3)]
            osb = c.sb("f_osb", [65, TT], F32, s3)
            rden = c.sb("f_rden", [64, TT], F32, s3)
            outb = [c.sb(f"f_out{i}", [64, TT], BF16, s3) for i in range(2)]
            it = 0
            for h in range(2):
                for qi in range(NQT):
                    qs = slice(qi * TT, (qi + 1) * TT)
                    kb.op("dve", lambda e, h=h, qi=qi: e.tensor_tensor(out=X[:], in0=AP3(c.ident_f, 4 * qi, [[128, 64], [1, 4], [0, 128]]),
                                                                       in1=AP3(cC[h], 0, [[128, 64], [0, 4], [1, 128]]), op=ALU.mult),
                          reads=["ident_f", ("cC", h)], writes=["f_X"])
                    p, pk = c.ps()
                    mm(c, p[:, :], c.ones_f[0:64, :], X[:].rearrange("p r s -> p (r s)"), True, True, ["ones_f", "f_X"], [pk])
                    kb.op("act", lambda e, p=p: e.activation(out=cqB[:], in_=p[:, :], func=AF.Copy), reads=[pk], writes=["f_cqB"])
                    for r in range(4):
                        kb.op("pool", lambda e, r=r: e.tensor_tensor(out=cqD[:, r, :], in0=cqB[:], in1=negm[:, r, :], op=ALU.add),
                              reads=["f_cqB", ("negm", r)], writes=[("f_cqD", r)])
                    c.rot = list(range(6))
                    po, pok = c.psb[6 + qi % 2], f"psb{6 + qi % 2}"
                    nk = 4 * (qi + 1)
                    LA = 3
                    sbank = {}

                    def emit_S(kt):
                        ps_, psk = c.ps()
                        mm(c, ps_[:, :], fkk[:, kt * 128:(kt + 1) * 128], fq[h][:, qs], True, True, [("fk", kt // 4), ("fq", h, qi), ("fqz", h)], [psk])
                        sbank[kt] = (ps_, psk)

                    for kt in range(min(LA, nk)):
                        emit_S(kt)
                    for kt in range(nk):
                        ps_, psk = sbank.pop(kt)
                        tb = tmpb[it % 3]; tbk = f"f_tmp{it % 3}"
                        pb = pTb[it % 3]; pbk = f"f_pT{it % 3}"
                        it += 1
                        r = kt - 4 * qi
                        if r >= 0:
                            kb.op("dve", lambda e, ps_=ps_, tb=tb, r=r: e.tensor_tensor(out=tb[:], in0=ps_[:, :], in1=cqD[:, r, :], op=ALU.add),
                                  reads=[psk, ("f_cqD", r)], writes=[tbk])
                        else:
                            kb.op("dve", lambda e, ps_=ps_, tb=tb: e.tensor_tensor(out=tb[:], in0=ps_[:, :], in1=cqB[:], op=ALU.add),
                                  reads=[psk, "f_cqB"], writes=[tbk])
                        kb.op("act", lambda e, tb=tb, pb=pb, h=h, kt=kt: e.activation(out=pb[:], in_=tb[:], func=AF.Exp, bias=ckT[h][:, kt:kt + 1]),
                              reads=[tbk, ("ckT", h)], writes=[pbk])
                        if kt + LA < nk:
                            emit_S(kt + LA)
                        mm(c, po[0:65, :], fV[h][:, kt, :], pb[:], kt == 0, kt == nk - 1, [("fV", h, kt // 4), ("fV1", h), pbk], [pok])
                    kb.op("act", lambda e, po=po: e.activation(out=osb[:], in_=po[0:65, :], func=AF.Copy), reads=[pok], writes=["f_osb"])
                    pd, pdk = c.ps()
                    mm(c, pd[0:64, :], c.ones_f[64:65, 0:64], osb[64:65, :], True, True, ["ones_f", "f_osb"], [pdk])
                    kb.op("dve", lambda e, pd=pd: e.reciprocal(out=rden[:], in_=pd[0:64, :]), reads=[pdk], writes=["f_rden"])
                    ob = outb[qi % 2]; obk = f"f_out{qi % 2}"
                    kb.op("dve", lambda e, ob=ob: e.tensor_tensor(out=ob[:], in0=osb[0:64, :], in1=rden[:], op=ALU.mult), reads=["f_osb", "f_rden"], writes=[obk])
                    cfo = io["catf_out"]
                    kb.dma("sp", (cfo[h][:, qs] if isinstance(cfo, list) else cfo[h * 64:(h + 1) * 64, qs]), ob[:], reads=[obk])
            c.rot = None
        c.barrier()


def build_M(which="both"):
    nc = bass.Bass("TRN2", target_bir_lowering=False)
    io = {}

    def din(name, shape, dt=F32):
        io[name] = nc.dram_tensor(name, shape, dt, kind="ExternalInput").ap()

    def dout(name, shape, dt=F32):
        io[name] = nc.dram_tensor(name, shape, dt, kind="ExternalOutput").ap()

    din("hT_all", [4, D, NT], BF16); din("w_ml", [D, 258]); din("w_fx", [D, 386]); din("vecsM", [128, 16])
    dout("catm_out", [64, SEQ], BF16); dout("catf_out", [128, SEQ], BF16)
    with _ES() as st:
        c = Ctx(nc, st)
        c.setup()
        if which in ("both", "mlstm"):
            phase_M_mlstm(c, io)
        if which in ("both", "fox"):
            phase_M_fox(c, io)
        c.barrier()
        c.kb.flush()
    return nc


def inputs_M(inp, l, g):
    w_in = np.asarray(inp["w_in"][l], np.float32)
    cols = np.concatenate([
        np.arange(g * 64, g * 64 + 64), 256 + np.arange(g * 64, g * 64 + 64),
        512 + np.arange(g * 64, g * 64 + 64), 768 + np.arange(g * 64, g * 64 + 64),
        [1024 + g, 1028 + g]])
    w_ml = np.ascontiguousarray(w_in[:, cols])
    fcols = []
    for base in (1544, 2056, 2568):
        for hh in (2 * g, 2 * g + 1):
            fcols.append(base + np.arange(hh * 64, hh * 64 + 64))
    fcols.append(np.array([3080 + 2 * g, 3080 + 2 * g + 1]))
    w_fx = np.ascontiguousarray(w_in[:, np.concatenate(fcols)])
    v = np.zeros((128, 16), np.float32)
    cw = np.asarray(inp["mlstm_conv_w"][l], np.float32)
    cb = np.asarray(inp["mlstm_conv_b"][l], np.float32)
    v[0:64, 0:4] = cw[:, g * 64:g * 64 + 64].T
    v[0:64, 4] = cb[g * 64:g * 64 + 64]
    v[0:64, 5:9] = cw[:, 256 + g * 64:256 + g * 64 + 64].T
    v[0:64, 9] = cb[256 + g * 64:256 + g * 64 + 64]
    v[0:64, 10] = np.asarray(inp["mlstm_norm_w"][l], np.float32)[g * 64:g * 64 + 64]
    v[:, 11] = inp["mlstm_b_i"][l][g]
    v[:, 12] = inp["mlstm_b_f"][l][g]
    v[:, 13] = inp["fox_b_f"][l][2 * g]
    v[:, 14] = inp["fox_b_f"][l][2 * g + 1]
    return {"w_ml": w_ml, "w_fx": w_fx, "vecsM": v}


def phase_P(c, io, xT):
    kb = c.kb
    from contextlib import ExitStack
    with ExitStack() as s0:
        vecs = c.sb("vecsP_sb", [128, 8], F32, s0)
        eps_t = c.sb("p_eps", [128, 1], F32, s0)
        sq = c.sb("p_sq", [128, 8, TT], BF16, s0)
        rstd = c.sb("p_rstd", [128, TT], F32, s0)
        hT = c.sb("p_hT", [128, 8, TT], BF16, s0)
        xt = [c.sb(f"p_xt{i}", [128, 4, D], F32, s0) for i in range(2)]
        tmp = {"sq": sq, "rstd": rstd, "eps": eps_t}
        kb.dma("sp", vecs[:], io["vecsP"], writes=["vecs"])
        kb.op("pool", lambda e: e.memset(eps_t[:], EPS), writes=["eps"])
        for t in range(NTT):
            t0 = t * TT
            xb = xt[t % 2]
            xk = f"p_xt{t % 2}"
            kb.dma("sp", xb[:], io["x_tok"][t0:t0 + TT, :].rearrange("(s p) d -> p s d", p=128), writes=[xk])
            for k in range(8):
                p, pk = c.ps()
                for s in range(4):
                    kb.op("pe", lambda e, k=k, s=s, p=p, xb=xb: e.transpose(out=p[:, s * 128:(s + 1) * 128], in_=xb[:, s, k * 128:(k + 1) * 128], identity=c.ident_f[:]),
                          reads=[xk, "ident_f"], writes=[pk])
                kb.op("act", lambda e, k=k, p=p, t0=t0: e.activation(out=xT[:, k, t0:t0 + TT], in_=p[:, :], func=AF.Copy), reads=[pk], writes=[("xT", k)])
            rmsnorm_tile(c, xT, "xT", t0, TT, vecs[:, 0:8], tmp, hT, "hT")
            h_store(c, io["h_next"], hT, t0, TT, [("hT", k) for k in range(8)], ["h_next_d"])
            if t == NTT - 1 and "tail_next" in io:
                kb.dma("sp", io["tail_next"].rearrange("(k p) n -> p k n", p=128), hT[:, :, TT - 32:TT],
                       reads=[("hT", k) for k in range(8)], writes=["tail_next_d"])
    c.barrier()


def build_P():
    nc = bass.Bass("TRN2", target_bir_lowering=False)
    io = {}
    io["x_tok"] = nc.dram_tensor("x_tok", [NT, D], F32, kind="ExternalInput").ap()
    io["vecsP"] = nc.dram_tensor("vecsP", [128, 8], F32, kind="ExternalInput").ap()
    io["xT_out"] = nc.dram_tensor("xT_out", [D, NT], F32, kind="ExternalOutput").ap()
    io["h_next"] = nc.dram_tensor("h_next", [D, NT], BF16, kind="ExternalOutput").ap()
    with _ES() as st:
        c = Ctx(nc, st)
        c.setup()
        xT = c.sb("xT", [128, 8, NT], F32)
        phase_P(c, io, xT)
        c.kb.dma("sp", io["xT_out"].rearrange("(k p) n -> p k n", p=128), xT[:], reads=[("xT", k) for k in range(8)])
        c.barrier()
        c.kb.flush()
    return nc


_CACHE = {}


def _get(name, fn):
    if name not in _CACHE:
        _CACHE[name] = fn()
    return _CACHE[name]


def kernel(**inp):
    inp = {k: np.asarray(v) for k, v in inp.items()}
    cores = list(range(8))
    B = 2
    x = inp["x"].astype(np.float32, copy=False)
    fm = lambda w: np.ascontiguousarray(np.asarray(w, np.float32).reshape(-1, 128).T)
    ncP = _get("P", build_P)
    maps = []
    for cid in cores:
        b, j = cid // 4, cid % 4
        maps.append({"x_tok": np.ascontiguousarray(x[b, j * NT:(j + 1) * NT]), "vecsP": fm(inp["norm_mix_w"][0])})
    res = run_bass_kernel_spmd(ncP, maps, core_ids=cores).results
    xT = [r["xT_out"] for r in res]
    hN = [r["h_next"] for r in res]
    out = None
    for l in range(2):
        last = (l == 1)
        E = 1 if l == 0 else 8
        ncM = _get("M", build_M)
        maps = []
        for cid in cores:
            b, g = cid // 4, cid % 4
            m = inputs_M(inp, l, g)
            m["hT_all"] = np.ascontiguousarray(np.stack([hN[b * 4 + jj] for jj in range(4)], axis=0))
            maps.append(m)
        resM = run_bass_kernel_spmd(ncM, maps, core_ids=cores).results
        ncT = _get(("T", E, last), lambda: build_T(E, last))
        maps = []
        for cid in cores:
            b, j = cid // 4, cid % 4
            tk = slice(j * NT, (j + 1) * NT)
            m = {"xT_in": xT[cid], "h_own": hN[cid]}
            m["h_halo"] = (np.ascontiguousarray(hN[cid - 1][:, NT - 32:NT]) if j > 0 else np.zeros((D, 32), NPBF))
            m["catm"] = np.ascontiguousarray(np.stack([resM[b * 4 + g]["catm_out"][:, tk] for g in range(4)], axis=0))
            m["catf"] = np.ascontiguousarray(np.stack([resM[b * 4 + g]["catf_out"][:, tk] for g in range(4)], axis=0))
            m["mem"] = np.ascontiguousarray(inp["mem"][b], dtype=np.float32)
            m["vecs"] = vecs_T(inp, l, last)
            m["w_c"] = np.ascontiguousarray(inp["w_in"][l][:, 1032:1544])
            m["w_out"] = inp["w_out"][l]; m["w_q"] = inp["xattn_w_q"][l]
            m["w_kv"] = inp["xattn_w_kv"][l]; m["w_o"] = inp["xattn_w_o"][l]
            if E == 1:
                m["w_gate"] = inp["ffn_w_gate"]; m["w_up"] = inp["ffn_w_up"]; m["w_down"] = inp["ffn_w_down"]
            else:
                m["w_gate"] = inp["moe_w_gate"][0]; m["w_up"] = inp["moe_w_up"][0]; m["w_down"] = inp["moe_w_down"][0]
                m["router_w"] = inp["router_w"][0]
            maps.append(m)
        resT = run_bass_kernel_spmd(ncT, maps, core_ids=cores).results
        if not last:
            xT = [r["xT_out"] for r in resT]
            hN = [r["h_next"] for r in resT]
        else:
            out = np.zeros((B, SEQ, D), np.float32)
            for cid in cores:
                b, j = cid // 4, cid % 4
                out[b, j * NT:(j + 1) * NT] = resT[cid]["out"]
    return out


RG = [[0, 1, 2, 3], [4, 5, 6, 7]]
_STOP = None


def build_fused(stop=None):
    nc = bass.Bass("TRN2", target_bir_lowering=False)
    io = {}
    if stop:
        io["dbg1"] = nc.dram_tensor("dbg1", [4 * D, NT], BF16, kind="ExternalOutput").ap()
        io["dbg2"] = nc.dram_tensor("dbg2", [512, SEQ], BF16, kind="ExternalOutput").ap()
        io["dbg3"] = nc.dram_tensor("dbg3", [D, NT], F32, kind="ExternalOutput").ap()

    def din(name, shape, dt=F32):
        io[name] = nc.dram_tensor(name, shape, dt, kind="ExternalInput").ap()
        return io[name]

    def dint(name, shape, dt=BF16):
        io[name] = nc.dram_tensor(name, shape, dt, kind="Internal").ap()
        return io[name]

    din("x_tok", [NT, D]); din("vecsP", [128, 8])
    if stop != "AG":
        din("sel", [128, 8]); din("mem", [256, D])
    for l in range(2 if stop != "AG" else 0):
        din(f"w_ml{l}", [D, 258]); din(f"w_fx{l}", [D, 386]); din(f"vecsM{l}", [128, 16]); din(f"vecs{l}", [128, NV_T])
        din(f"w_c{l}", [D, 512]); din(f"w_out{l}", [D, D]); din(f"w_q{l}", [D, 512]); din(f"w_kv{l}", [D, D]); din(f"w_o{l}", [512, D])
    if stop != "AG":
        din("w_gate0", [1, D, DFF]); din("w_up0", [1, D, DFF]); din("w_down0", [1, DFF, D])
        din("w_gate1", [8, D, DFF]); din("w_up1", [8, D, DFF]); din("w_down1", [8, DFF, D]); din("router_w", [D, 8])
    io["out"] = nc.dram_tensor("out", [NT, D], F32, kind="ExternalOutput").ap()
    for l in range(2):
        io[f"h_own{l}"] = [dint(f"h_own{l}_{a}", [256, NT]) for a in range(4)]
        io[f"hT_all{l}"] = [dint(f"hT_all{l}_{a}", [4 * 256, NT]) for a in range(4)]
        dint(f"tail{l}", [D, 32]); dint(f"tails{l}", [4 * D, 32])
        dint(f"catm{l}", [64, SEQ]); dint(f"catm_all{l}", [256, SEQ])
        io[f"catf{l}"] = [dint(f"catf{l}_{h}", [64, SEQ]) for h in range(2)]
        io[f"catf_all{l}"] = [dint(f"catf_all{l}_{h}", [256, SEQ]) for h in range(2)]
    with _ES() as st:
        c = Ctx(nc, st)
        c.setup()
        kb = c.kb
        xT = c.sb("xT", [128, 8, NT], F32)
        phase_P(c, {"x_tok": io["x_tok"], "vecsP": io["vecsP"], "h_next": io["h_own0"], "tail_next": io["tail0"]}, xT)
        for l in range(2):
            last = (l == 1)
            for a in range(4):
                kb.collective("AllGather", RG, io[f"h_own{l}"][a], io[f"hT_all{l}"][a], reads=["h_next_d"], writes=["hT_all_d"])
            kb.collective("AllGather", RG, io[f"tail{l}"], io[f"tails{l}"], reads=["tail_next_d"], writes=["tails_d"])
            c.barrier()
            if stop == "AG":
                for a in range(4):
                    for jj in range(4):
                        kb.dma("sp", io["dbg1"][jj * D + a * 256:jj * D + (a + 1) * 256, :], io[f"hT_all{l}"][a][jj * 256:(jj + 1) * 256, :], reads=["hT_all_d"])
                break
            ioM = {"hT_all": io[f"hT_all{l}"], "w_ml": io[f"w_ml{l}"], "w_fx": io[f"w_fx{l}"],
                   "vecsM": io[f"vecsM{l}"], "catm_out": io[f"catm{l}"], "catf_out": io[f"catf{l}"]}
            c.sfx = f"_{l}"
            phase_M_mlstm(c, ioM)
            phase_M_fox(c, ioM)
            kb.collective("AllGather", RG, io[f"catm{l}"], io[f"catm_all{l}"], writes=["catm_all_d"])
            for h in range(2):
                kb.collective("AllGather", RG, io[f"catf{l}"][h], io[f"catf_all{l}"][h], writes=["catf_all_d"])
            c.barrier()
            if stop == "M":
                for h in range(2):
                    for g in range(4):
                        kb.dma("sp", io["dbg2"][g * 128 + h * 64:g * 128 + (h + 1) * 64, :], io[f"catf_all{l}"][h][g * 64:(g + 1) * 64, :], reads=["catf_all_d"])
                break
            ioT = {"h_own": io[f"h_own{l}"], "tails": io[f"tails{l}"], "sel": io["sel"], "catm_all": io[f"catm_all{l}"], "catf_all": io[f"catf_all{l}"],
                   "mem": io["mem"], "vecs": io[f"vecs{l}"], "w_c": io[f"w_c{l}"], "w_out": io[f"w_out{l}"], "w_q": io[f"w_q{l}"],
                   "w_kv": io[f"w_kv{l}"], "w_o": io[f"w_o{l}"], "w_gate": io[f"w_gate{l}"], "w_up": io[f"w_up{l}"], "w_down": io[f"w_down{l}"]}
            if last:
                ioT["router_w"] = io["router_w"]; ioT["out"] = io["out"]
            else:
                ioT["h_next"] = io["h_own1"]; ioT["tail_next"] = io["tail1"]
            phase_T(c, ioT, 8 if last else 1, last, xT)
            if stop == "T":
                kb.dma("sp", io["dbg3"].rearrange("(k p) n -> p k n", p=128), xT[:], reads=[("xT", k) for k in range(8)])
                break
        c.barrier()
        kb.flush()
    return nc


def kernel_unfused(**inp):
    return _kernel_unfused(**inp)


_kernel_unfused = kernel


def kernel(**inp):
    inp = {k: np.asarray(v) for k, v in inp.items()}
    cores = list(range(8))
    x = inp["x"].astype(np.float32, copy=False)
    fm = lambda w: np.ascontiguousarray(np.asarray(w, np.float32).reshape(-1, 128).T)
    nc = _get("fused", lambda: build_fused(_STOP))
    shared = {"vecsP": fm(inp["norm_mix_w"][0]),
              "w_gate0": inp["ffn_w_gate"], "w_up0": inp["ffn_w_up"], "w_down0": inp["ffn_w_down"],
              "w_gate1": inp["moe_w_gate"][0], "w_up1": inp["moe_w_up"][0], "w_down1": inp["moe_w_down"][0],
              "router_w": inp["router_w"][0]}
    for l in range(2):
        shared[f"vecs{l}"] = vecs_T(inp, l, l == 1)
        shared[f"w_c{l}"] = np.ascontiguousarray(inp["w_in"][l][:, 1032:1544])
        shared[f"w_out{l}"] = inp["w_out"][l]; shared[f"w_q{l}"] = inp["xattn_w_q"][l]
        shared[f"w_kv{l}"] = inp["xattn_w_kv"][l]; shared[f"w_o{l}"] = inp["xattn_w_o"][l]
    perg = []
    for g in range(4):
        d = {}
        for l in range(2):
            m = inputs_M(inp, l, g)
            d[f"w_ml{l}"] = m["w_ml"]; d[f"w_fx{l}"] = m["w_fx"]; d[f"vecsM{l}"] = m["vecsM"]
        perg.append(d)
    maps = []
    for cid in cores:
        b, j = cid // 4, cid % 4
        m = dict(shared)
        m.update(perg[j])
        m["x_tok"] = np.ascontiguousarray(x[b, j * NT:(j + 1) * NT])
        m["mem"] = np.ascontiguousarray(inp["mem"][b], dtype=np.float32)
        sel = np.zeros((128, 8), np.float32)
        sel[:, j] = 1.0
        if j > 0:
            sel[:, 4 + j - 1] = 1.0
        m["sel"] = sel
        maps.append(m)
    if _STOP == "AG":
        maps = [{k: m[k] for k in ("x_tok", "vecsP")} for m in maps]
    res = run_bass_kernel_spmd(nc, maps, core_ids=cores).results
    if _STOP:
        return res
    out = np.zeros((2, SEQ, D), np.float32)
    for cid in cores:
        b, j = cid // 4, cid % 4
        out[b, j * NT:(j + 1) * NT] = res[cid]["out"]
    return out
```

```python
import numpy as np
import concourse.bass as bass
import concourse.mybir as mybir
from concourse.bass_utils import run_bass_kernel_spmd

F32 = mybir.dt.float32
BF16 = mybir.dt.bfloat16
AF = mybir.ActivationFunctionType
ALU = mybir.AluOpType
AX = mybir.AxisListType

ENGS = ("pe", "act", "dve", "pool", "sp")


class KB:
    SEM_ROLL = 2000

    def __init__(self, nc, n_dma_sems=32):
        self.nc = nc
        self.q = {e: [] for e in ENGS}
        self.cnt = {e: 0 for e in ENGS}
        self.cur_sem = {}
        self.sem_pool = []
        self.waited = {e: {} for e in ENGS}
        self.last_w = {}
        self.reads = {}
        self.n_dma_sems = n_dma_sems
        self.dma_sems = []
        self.dma_cnt = []
        self.dma_rr = 0
        self.dma_rr_sw = 0
        self._stack = None
        self.n_inst = 0

    def _new_sem(self, name):
        s = self._stack.enter_context(self.nc.semaphore(name))
        return s

    def start(self, stack):
        self._stack = stack
        for e in ENGS:
            self.cur_sem[e] = self._new_sem(f"p_{e}_0")
        for i in range(self.n_dma_sems):
            self.dma_sems.append(self._new_sem(f"dma{i}"))
            self.dma_cnt.append(0)

    def _wait(self, eng, ev):
        if ev is None:
            return
        if len(ev) == 3 and ev[2] == "pe" and eng == "pe":
            return
        sem, val = ev[0], ev[1]
        w = self.waited[eng]
        if w.get(id(sem), (None, 0))[1] >= val:
            return
        w[id(sem)] = (sem, val)
        self.q[eng].append(lambda e, sem=sem, val=val: e.wait_ge(sem, val))

    def _wait_w(self, eng, k):
        lw = self.last_w.get(k)
        if isinstance(lw, list):
            for ev in lw:
                self._wait(eng, ev)
        else:
            self._wait(eng, lw)

    def _deps(self, eng, reads, writes):
        for k in reads:
            self._wait_w(eng, k)
        for k in writes:
            self._wait_w(eng, k)
            for ev in self.reads.get(k, ()):
                self._wait(eng, ev)

    def _commit(self, ev, reads, writes, is_dma=False):
        for k in writes:
            lw = self.last_w.get(k)
            if is_dma and isinstance(lw, list) and not self.reads.get(k):
                lw.append(ev)
            else:
                self.last_w[k] = [ev] if is_dma else ev
            self.reads[k] = []
        for k in reads:
            self.reads.setdefault(k, []).append(ev)

    def op(self, eng, fn, reads=(), writes=()):
        self._deps(eng, reads, writes)
        if self.cnt[eng] >= self.SEM_ROLL:
            self.cur_sem[eng] = self._new_sem(f"p_{eng}_{self.n_inst}")
            self.cnt[eng] = 0
        self.cnt[eng] += 1
        sem = self.cur_sem[eng]
        ev = (sem, self.cnt[eng], eng)
        self.q[eng].append(lambda e, sem=sem: fn(e).then_inc(sem, 1))
        self._commit(ev, reads, writes)
        self.n_inst += 1
        return ev

    def dma(self, eng, out, in_, reads=(), writes=(), **kw):
        self._deps(eng, reads, writes)
        half = self.n_dma_sems // 2
        if eng == "pool":
            i = half + self.dma_rr_sw
            self.dma_rr_sw = (self.dma_rr_sw + 1) % (self.n_dma_sems - half)
        else:
            i = self.dma_rr
            self.dma_rr = (self.dma_rr + 1) % half
        sem = self.dma_sems[i]
        if self.dma_cnt[i] >= 2048:
            self.dma_sems[i] = self._new_sem(f"dma{i}_{self.n_inst}")
            self.dma_cnt[i] = 0
            sem = self.dma_sems[i]
        if self.dma_cnt[i] > 0:
            self._wait(eng, (sem, self.dma_cnt[i]))
        self.dma_cnt[i] += 16
        ev = (sem, self.dma_cnt[i])
        self.q[eng].append(lambda e, sem=sem: e.dma_start(out=out, in_=in_, **kw).then_inc(sem, 16))
        self._commit(ev, reads, writes, is_dma=True)
        self.n_inst += 1
        return ev

    def collective(self, kind, rg, in_ap, out_ap, reads=(), writes=()):
        eng = "pool"
        self._deps(eng, reads, writes)
        sem = self._new_sem(f"cc_{self.n_inst}")
        ev = (sem, 1)
        self.q[eng].append(lambda e: e.collective_compute(kind, ALU.bypass, replica_groups=rg, ins=[in_ap.opt()],
                                                          outs=[out_ap.opt()]).then_inc(sem, 1))
        self._commit(ev, reads, writes)
        self.n_inst += 1
        self.cc_events = getattr(self, "cc_events", []) + [ev]
        return ev

    def wait_all(self, eng, evs):
        for ev in evs:
            self._wait(eng, ev)

    def flush(self):
        nc = self.nc
        q = self.q
        with nc.Block() as block:
            @block.tensor
            def _(e):
                for f in q["pe"]:
                    f(e)

            @block.scalar
            def _(e):
                for f in q["act"]:
                    f(e)

            @block.vector
            def _(e):
                for f in q["dve"]:
                    f(e)

            @block.gpsimd
            def _(e):
                for f in q["pool"]:
                    f(e)

            @block.sync
            def _(e):
                for f in q["sp"]:
                    f(e)
        self.q = {e: [] for e in ENGS}


D = 1024
NT = 2048
TT = 512
NTT = NT // TT
DFF = 2816
NF = DFF // 128
SEQ = 8192
EPS = 1e-6
NV_T = 100


class Ctx:
    def __init__(self, nc, st):
        self.nc = nc
        self.st = st
        self.kb = KB(nc)
        self.kb.start(st)
        self.ps_rr = 0
        self.uid = 0

    def sb(self, name, shape, dt, st=None):
        self.uid += 1
        return (st or self.st).enter_context(self.nc.sbuf_tensor(f"{name}_u{self.uid}", shape, dt))

    def barrier(self):
        kb = self.kb
        evs = []
        for e in ENGS:
            if kb.cnt[e] > 0:
                evs.append((kb.cur_sem[e], kb.cnt[e]))
        for i, s in enumerate(kb.dma_sems):
            if kb.dma_cnt[i] > 0:
                evs.append((s, kb.dma_cnt[i]))
        evs += getattr(kb, "cc_events", [])
        kb.cc_events = []
        for e in ENGS:
            for ev in evs:
                kb._wait(e, ev)
        kb.last_w = {}
        kb.reads = {}

    def setup(self):
        nc, kb = self.nc, self.kb
        self.ident_f = self.sb("ident_f", [128, 128], F32)
        self.ident_b = self.sb("ident_b", [128, 128], BF16)
        self.ones_b = self.sb("ones_b", [128, 128], BF16)
        self.ones_f = self.sb("ones_f", [128, 128], F32)
        self.psb = [self.st.enter_context(nc.psum_tensor(f"psb{i}", [128, 512], F32)) for i in range(8)]
        idf, idb, ob, of = self.ident_f, self.ident_b, self.ones_b, self.ones_f
        kb.op("pool", lambda e: e.memset(idf[:], 0.0), writes=["ident_f"])
        kb.op("pool", lambda e: e.affine_select(out=idf[:], in_=idf[:], pattern=[[-1, 128]],
                                                compare_op=ALU.not_equal, fill=1.0, base=0,
                                                channel_multiplier=1),
              reads=["ident_f"], writes=["ident_f"])
        kb.op("pool", lambda e: e.tensor_copy(out=idb[:], in_=idf[:]), reads=["ident_f"], writes=["ident_b"])
        kb.op("pool", lambda e: e.memset(ob[:], 1.0), writes=["ones_b"])
        kb.op("pool", lambda e: e.memset(of[:], 1.0), writes=["ones_f"])

    def ps(self):
        rot = getattr(self, "rot", None) or list(range(8))
        i = rot[self.ps_rr % len(rot)]
        self.ps_rr += 1
        return self.psb[i], f"psb{i}"


def h_store(c, dst, hT, c0, n, reads, writes=()):
    if isinstance(dst, list):
        for a, d in enumerate(dst):
            c.kb.dma("sp", d[:, c0:c0 + n].rearrange("(k p) n -> p k n", p=128), hT[:, 2 * a:2 * a + 2, 0:n], reads=reads, writes=writes)
    else:
        c.kb.dma("sp", dst[:, c0:c0 + n].rearrange("(k p) n -> p k n", p=128), hT[:, :, 0:n], reads=reads, writes=writes)


def h_load(c, src, hT, c0, n, writes, j=None):
    if isinstance(src, list):
        for a, d in enumerate(src):
            v = d if j is None else d.rearrange("(j r) n -> j r n", j=4)[j]
            c.kb.dma("sp", hT[:, 2 * a:2 * a + 2, 0:n], v[:, c0:c0 + n].rearrange("(k p) n -> p k n", p=128), writes=writes)
    else:
        v = src if j is None else src[j]
        c.kb.dma("sp", hT[:, :, 0:n], v[:, c0:c0 + n].rearrange("(k p) n -> p k n", p=128), writes=writes)


def mm(c, out, lhsT, rhs, start, stop, reads, writes):
    return c.kb.op("pe", lambda e: e.matmul(out, lhsT=lhsT, rhs=rhs, start=start, stop=stop),
                   reads=reads, writes=writes)


def rmsnorm_tile(c, xT, xkey, t0, n, wv, tmp, out_bf, okey, out_f=None):
    kb = c.kb
    sq, rstd = tmp["sq"], tmp["rstd"]
    for k in range(8):
        kb.op("act", lambda e, k=k: e.activation(out=sq[:, k, 0:n], in_=xT[:, k, t0:t0 + n], func=AF.Square),
              reads=[(xkey, k)], writes=[("sq", k)])
    p, pk = c.ps()
    for k in range(8):
        mm(c, p[:, 0:n], c.ones_b[:], sq[:, k, 0:n], k == 0, k == 7, ["ones_b", ("sq", k)], [pk])
    kb.op("act", lambda e: e.activation(out=rstd[:, 0:n], in_=p[:, 0:n], func=AF.Sqrt, scale=1.0 / D, bias=tmp["eps"][:, 0:1]),
          reads=[pk, "eps"], writes=["rstd"])
    kb.op("dve", lambda e: e.reciprocal(out=rstd[:, 0:n], in_=rstd[:, 0:n]), reads=["rstd"], writes=["rstd"])
    for k in range(8):
        kb.op("dve", lambda e, k=k: e.scalar_tensor_tensor(out=out_bf[:, k, 0:n], in0=xT[:, k, t0:t0 + n],
                                                           scalar=wv[:, k:k + 1], in1=rstd[:, 0:n],
                                                           op0=ALU.mult, op1=ALU.mult),
              reads=[(xkey, k), "rstd", "vecs"], writes=[(okey, k)])
        if out_f is not None:
            kb.op("dve", lambda e, k=k: e.scalar_tensor_tensor(out=out_f[:, k, 0:n], in0=xT[:, k, t0:t0 + n],
                                                                scalar=wv[:, k:k + 1], in1=rstd[:, 0:n],
                                                                op0=ALU.mult, op1=ALU.mult),
                  reads=[(xkey, k), "rstd", "vecs"], writes=[(okey + "_f", k)])


def phase_T(c, io, E, last, xT):
    nc, kb = c.nc, c.kb
    from contextlib import ExitStack
    vec_st = ExitStack()
    vecs = c.sb("vecsT", [128, NV_T], F32, vec_st)
    eps_t = c.sb("eps_t", [128, 1], F32, vec_st)
    sq = c.sb("sq", [128, 8, TT], BF16, vec_st)
    rstd = c.sb("rstd", [128, TT], F32, vec_st)
    tmp = {"sq": sq, "rstd": rstd, "eps": eps_t}
    kb.dma("sp", vecs[:], io["vecs"], writes=["vecs"])
    kb.op("pool", lambda e: e.memset(eps_t[:], EPS), writes=["eps"])
    V_XA, V_MEM, V_FFN, V_NEXT, V_CB, V_LNW, V_LNB, V_CW = 0, 8, 16, 24, 32, 34, 36, 38

    with ExitStack() as s1:
        hT = c.sb("hT", [128, 8, TT], BF16, s1)
        gluT = c.sb("gluT", [128, 2, 32 + NT], BF16, s1)
        hcT = c.sb("hcT", [128, 2, NT], BF16, s1)
        wc = c.sb("wc", [128, 8, 512], BF16, s1)
        dg = c.sb("dg", [128, 62, 128], BF16, s1)
        sig = c.sb("sig", [128, 2, TT], F32, s1)
        hcv = c.sb("hcv", [128, 2, TT], F32, s1)
        hsq = c.sb("hsq", [128, 2, TT], F32, s1)
        mean = c.sb("mean", [128, TT], F32, s1)
        var = c.sb("var", [128, TT], F32, s1)
        wo_m = c.sb("wo_m", [64, 4, D], BF16, s1)
        wo_c = c.sb("wo_c", [128, 2, D], BF16, s1)
        wo_f = c.sb("wo_f", [128, 4, D], BF16, s1)
        mT = c.sb("mT", [64, 4, TT], BF16, s1)
        fT = c.sb("fT", [128, 4, TT], BF16, s1)
        if "sel" in io:
            halo4 = c.sb("halo4", [128, 4, 8, 32], BF16, s1)
            selt = c.sb("selt", [128, 8], F32, s1)
            m4 = [c.sb(f"m4_{i}", [64, 4, TT], BF16, s1) for i in range(2)]
            f4 = [c.sb(f"f4_{i}", [128, 4, TT], BF16, s1) for i in range(2)]
            kb.dma("sp", selt[:], io["sel"], writes=["selt"])
        kb.dma("pool", wc[:], io["w_c"].rearrange("(k p) n -> p k n", p=128), writes=["wc"])
        kb.dma("pool", wo_m[:], io["w_out"][0:256, :].rearrange("(g p) n -> p g n", p=64), writes=["wo_m"])
        kb.dma("pool", wo_c[:], io["w_out"][256:512, :].rearrange("(g p) n -> p g n", p=128), writes=["wo_c"])
        kb.dma("pool", wo_f[:], io["w_out"][512:1024, :].rearrange("(g p) n -> p g n", p=128), writes=["wo_f"])
        for j in range(31):
            for ch in range(2):
                kb.op("dve", lambda e, j=j, ch=ch: e.tensor_scalar(
                    out=dg[:, j * 2 + ch, :], in0=c.ident_b[:], scalar1=vecs[:, V_CW + j * 2 + ch:V_CW + j * 2 + ch + 1],
                    scalar2=None, op0=ALU.mult), reads=["ident_b", "vecs"], writes=[("dg", j, ch)])
        tiles = [("halo", 0, 32)] + [("own", t * TT, TT) for t in range(NTT)]
        for kind, t0, n in tiles:
            if kind == "halo" and "sel" in io:
                tl = io["tails"].rearrange("(j k p) n -> j p k n", j=4, p=128)
                for jj in range(4):
                    kb.dma("sp", halo4[:, jj, :, :], tl[jj], writes=[("halo4", jj)])
                kb.op("dve", lambda e: e.tensor_scalar(out=hT[:, :, 0:32], in0=halo4[:, 0, :, :], scalar1=selt[:, 4:5], scalar2=None, op0=ALU.mult),
                      reads=[("halo4", 0), "selt"], writes=[("hT", k) for k in range(8)])
                for jj in range(1, 4):
                    kb.op("dve", lambda e, jj=jj: e.scalar_tensor_tensor(out=hT[:, :, 0:32], in0=halo4[:, jj, :, :], scalar=selt[:, 4 + jj:5 + jj], in1=hT[:, :, 0:32],
                                                                         op0=ALU.mult, op1=ALU.add),
                          reads=[("halo4", jj), "selt"] + [("hT", k) for k in range(8)], writes=[("hT", k) for k in range(8)])
                g0 = 0
            elif kind == "halo":
                kb.dma("sp", hT[:, :, 0:n], io["h_halo"].rearrange("(k p) n -> p k n", p=128),
                       writes=[("hT", k) for k in range(8)])
                g0 = 0
            else:
                h_load(c, io["h_own"], hT, t0, n, [("hT", k) for k in range(8)])
                g0 = 32 + t0
            for ch in range(2):
                pa, pak = c.ps()
                pg, pgk = c.ps()
                for k in range(8):
                    mm(c, pa[:, 0:n], wc[:, k, ch * 128:(ch + 1) * 128], hT[:, k, 0:n], k == 0, k == 7,
                       ["wc", ("hT", k)], [pak])
                for k in range(8):
                    mm(c, pg[:, 0:n], wc[:, k, 256 + ch * 128:256 + (ch + 1) * 128], hT[:, k, 0:n], k == 0, k == 7,
                       ["wc", ("hT", k)], [pgk])
                kb.op("act", lambda e, ch=ch, pg=pg, n=n: e.activation(out=sig[:, ch, 0:n], in_=pg[:, 0:n], func=AF.Sigmoid),
                      reads=[pgk], writes=[("sig", ch)])
                kb.op("dve", lambda e, ch=ch, pa=pa, n=n, g0=g0: e.tensor_tensor(
                    out=gluT[:, ch, g0:g0 + n], in0=pa[:, 0:n], in1=sig[:, ch, 0:n], op=ALU.mult),
                    reads=[pak, ("sig", ch)], writes=[("glu", ch, g0 // TT), ("glu", ch, (g0 + n - 1) // TT)])
        for t in range(NTT):
            t0 = t * TT
            gk = lambda ch: [("glu", ch, (32 + t0 - 30) // TT), ("glu", ch, (32 + t0 + TT - 1) // TT)]
            for ch in range(2):
                p, pk = c.ps()
                for j in range(31):
                    o = 32 + t0 - 30 + j
                    mm(c, p[:, :], dg[:, j * 2 + ch, :], gluT[:, ch, o:o + TT], j == 0, j == 30,
                       [("dg", j, ch)] + gk(ch), [pk])
                kb.op("act", lambda e, ch=ch, p=p: e.activation(out=hcv[:, ch, :], in_=p[:, :], func=AF.Identity,
                                                                bias=vecs[:, V_CB + ch:V_CB + ch + 1]),
                      reads=[pk, "vecs"], writes=[("hcv", ch)])
                kb.op("act", lambda e, ch=ch: e.activation(out=hsq[:, ch, :], in_=hcv[:, ch, :], func=AF.Square),
                      reads=[("hcv", ch)], writes=[("hsq", ch)])
            p1, p1k = c.ps()
            p2, p2k = c.ps()
            for ch in range(2):
                mm(c, p1[:, :], c.ones_f[:], hcv[:, ch, :], ch == 0, ch == 1, ["ones_f", ("hcv", ch)], [p1k])
            for ch in range(2):
                mm(c, p2[:, :], c.ones_f[:], hsq[:, ch, :], ch == 0, ch == 1, ["ones_f", ("hsq", ch)], [p2k])
            kb.op("dve", lambda e, p1=p1: e.tensor_scalar(out=mean[:], in0=p1[:, :], scalar1=1.0 / 256, scalar2=None, op0=ALU.mult),
                  reads=[p1k], writes=["mean"])
            kb.op("dve", lambda e: e.tensor_tensor(out=var[:], in0=mean[:], in1=mean[:], op=ALU.mult),
                  reads=["mean"], writes=["var"])
            kb.op("dve", lambda e, p2=p2: e.scalar_tensor_tensor(out=var[:], in0=p2[:, :], scalar=1.0 / 256, in1=var[:],
                                                                 op0=ALU.mult, op1=ALU.subtract),
                  reads=[p2k, "var"], writes=["var"])
            kb.op("act", lambda e: e.activation(out=var[:], in_=var[:], func=AF.Sqrt, bias=eps_t[:, 0:1]),
                  reads=["var", "eps"], writes=["var"])
            kb.op("dve", lambda e: e.reciprocal(out=var[:], in_=var[:]), reads=["var"], writes=["var"])
            for ch in range(2):
                kb.op("dve", lambda e, ch=ch: e.tensor_tensor(out=hcv[:, ch, :], in0=hcv[:, ch, :], in1=mean[:], op=ALU.subtract),
                      reads=[("hcv", ch), "mean"], writes=[("hcv", ch)])
                kb.op("dve", lambda e, ch=ch: e.tensor_tensor(out=hcv[:, ch, :], in0=hcv[:, ch, :], in1=var[:], op=ALU.mult),
                      reads=[("hcv", ch), "var"], writes=[("hcv", ch)])
                kb.op("dve", lambda e, ch=ch: e.tensor_scalar(out=hcv[:, ch, :], in0=hcv[:, ch, :],
                                                              scalar1=vecs[:, V_LNW + ch:V_LNW + ch + 1],
                                                              scalar2=vecs[:, V_LNB + ch:V_LNB + ch + 1],
                                                              op0=ALU.mult, op1=ALU.add),
                      reads=[("hcv", ch), "vecs"], writes=[("hcv", ch)])
                kb.op("act", lambda e, ch=ch, t0=t0: e.activation(out=hcT[:, ch, t0:t0 + TT], in_=hcv[:, ch, :], func=AF.Silu),
                      reads=[("hcv", ch)], writes=[("hcT", ch, t)])
            if "sel" in io:
                cm = io["catm_all"].rearrange("(g p) n -> p g n", p=64)
                cf = [a.rearrange("(g p) n -> p g n", p=64) for a in io["catf_all"]]
                for jj in range(4):
                    for dst, stg, src, nm, npart in ((mT, m4, cm, "m4", 64), (fT, f4, cf, "f4", 128)):
                        dk = "mT" if nm == "m4" else "fT"
                        sg = stg[jj % 2]
                        sk = f"{nm}_{jj % 2}"
                        if nm == "m4":
                            kb.dma("sp", sg[:], src[:, :, jj * NT + t0:jj * NT + t0 + TT], writes=[sk])
                        else:
                            for hh in range(2):
                                kb.dma("sp", sg[hh * 64:(hh + 1) * 64, :, :], src[hh][:, :, jj * NT + t0:jj * NT + t0 + TT], writes=[sk])
                        if jj == 0:
                            kb.op("dve", lambda e, dst=dst, sg=sg, npart=npart: e.tensor_scalar(out=dst[:], in0=sg[:], scalar1=selt[0:npart, 0:1], scalar2=None, op0=ALU.mult),
                                  reads=[sk, "selt"], writes=[dk])
                        else:
                            kb.op("dve", lambda e, dst=dst, sg=sg, jj=jj, npart=npart: e.scalar_tensor_tensor(out=dst[:], in0=sg[:], scalar=selt[0:npart, jj:jj + 1], in1=dst[:],
                                                                                                          op0=ALU.mult, op1=ALU.add),
                                  reads=[sk, "selt", dk], writes=[dk])
            else:
                kb.dma("sp", mT[:], io["catm"][:, :, t0:t0 + TT].rearrange("g p n -> p g n"), writes=["mT"])
                kb.dma("sp", fT[:], io["catf"][:, :, t0:t0 + TT].rearrange("g p n -> p g n"), writes=["fT"])
            for d in range(8):
                p, pk = c.ps()
                ds = slice(d * 128, (d + 1) * 128)
                for g in range(4):
                    mm(c, p[:, :], wo_m[:, g, ds], mT[:, g, :], g == 0, False, ["wo_m", "mT"], [pk])
                for ch in range(2):
                    mm(c, p[:, :], wo_c[:, ch, ds], hcT[:, ch, t0:t0 + TT], False, False, ["wo_c", ("hcT", ch, t)], [pk])
                for g in range(4):
                    mm(c, p[:, :], wo_f[:, g, ds], fT[:, g, :], False, g == 3, ["wo_f", "fT"], [pk])
                kb.op("dve", lambda e, d=d, p=p, t0=t0: e.tensor_tensor(out=xT[:, d, t0:t0 + TT], in0=xT[:, d, t0:t0 + TT],
                                                                        in1=p[:, :], op=ALU.add),
                      reads=[pk, ("xT", d)], writes=[("xT", d)])
    c.barrier()
    if io.get("dbg_stage") == 1:
        vec_st.close()
        return

    with ExitStack() as s2:
        hT = c.sb("hT", [128, 8, TT], BF16, s2)
        memt = c.sb("memt", [128, 2, D], F32, s2)
        mss = c.sb("mss", [128, 2], F32, s2)
        junk = c.sb("junk", [128, D], F32, s2)
        memnT = c.sb("memnT", [128, 8, 256], BF16, s2)
        wkv = c.sb("wkv", [128, 8, D], BF16, s2)
        wq = c.sb("wq", [128, 8, 512], BF16, s2)
        wo = c.sb("wo", [128, 4, D], BF16, s2)
        kT = c.sb("kT", [128, 4, 256], BF16, s2)
        Vt = c.sb("Vt", [128, 2, 512], BF16, s2)
        qT = c.sb("qT", [128, 4, TT], BF16, s2)
        pT = c.sb("pT", [128, 8, TT], BF16, s2)
        rden = c.sb("rden", [128, TT], F32, s2)
        oT = c.sb("oT", [128, 4, TT], BF16, s2)
        kb.dma("sp", memt[:], io["mem"].rearrange("(t p) d -> p t d", p=128), writes=["memt"])
        kb.dma("pool", wkv[:], io["w_kv"].rearrange("(k p) n -> p k n", p=128), writes=["wkv"])
        kb.dma("pool", wq[:], io["w_q"].rearrange("(k p) n -> p k n", p=128), writes=["wq"])
        kb.dma("pool", wo[:], io["w_o"].rearrange("(k p) n -> p k n", p=128), writes=["wo"])
        for mt in range(2):
            kb.op("act", lambda e, mt=mt: e.activation(out=junk[:], in_=memt[:, mt, :], func=AF.Square,
                                                       accum_out=mss[:, mt:mt + 1]),
                  reads=["memt"], writes=["junk", ("mss", mt)])
            kb.op("act", lambda e, mt=mt: e.activation(out=mss[:, mt:mt + 1], in_=mss[:, mt:mt + 1], func=AF.Sqrt,
                                                       scale=1.0 / D, bias=eps_t[:, 0:1]),
                  reads=[("mss", mt), "eps"], writes=[("mss", mt)])
            kb.op("dve", lambda e, mt=mt: e.reciprocal(out=mss[:, mt:mt + 1], in_=mss[:, mt:mt + 1]),
                  reads=[("mss", mt)], writes=[("mss", mt)])
            kb.op("dve", lambda e, mt=mt: e.tensor_scalar(out=memt[:, mt, :], in0=memt[:, mt, :], scalar1=mss[:, mt:mt + 1],
                                                          scalar2=None, op0=ALU.mult),
                  reads=["memt", ("mss", mt)], writes=["memt"])
        for k in range(8):
            p, pk = c.ps()
            for mt in range(2):
                kb.op("pe", lambda e, k=k, mt=mt, p=p: e.transpose(out=p[:, mt * 128:(mt + 1) * 128],
                                                                   in_=memt[:, mt, k * 128:(k + 1) * 128], identity=c.ident_f[:]),
                      reads=["memt", "ident_f"], writes=[pk])
            kb.op("dve", lambda e, k=k, p=p: e.tensor_scalar(out=memnT[:, k, :], in0=p[:, 0:256],
                                                             scalar1=vecs[:, V_MEM + k:V_MEM + k + 1], scalar2=None, op0=ALU.mult),
                  reads=[pk, "vecs"], writes=[("memnT", k)])
        for h in range(4):
            p, pk = c.ps()
            for k in range(8):
                mm(c, p[:, 0:256], wkv[:, k, h * 128:(h + 1) * 128], memnT[:, k, :], k == 0, k == 7, ["wkv", ("memnT", k)], [pk])
            kb.op("act", lambda e, h=h, p=p: e.activation(out=kT[:, h, :], in_=p[:, 0:256], func=AF.Copy),
                  reads=[pk], writes=[("kT", h)])
        for mt in range(2):
            p, pk = c.ps()
            for k in range(8):
                mm(c, p[:, :], memnT[:, k, mt * 128:(mt + 1) * 128], wkv[:, k, 512:1024], k == 0, k == 7, ["wkv", ("memnT", k)], [pk])
            kb.op("act", lambda e, mt=mt, p=p: e.activation(out=Vt[:, mt, :], in_=p[:, :], func=AF.Copy),
                  reads=[pk], writes=[("Vt", mt)])
        sc = 128 ** -0.5
        for t in range(NTT):
            t0 = t * TT
            rmsnorm_tile(c, xT, "xT", t0, TT, vecs[:, V_XA:V_XA + 8], tmp, hT, "hT")
            for h in range(4):
                p, pk = c.ps()
                for k in range(8):
                    mm(c, p[:, :], wq[:, k, h * 128:(h + 1) * 128], hT[:, k, :], k == 0, k == 7, ["wq", ("hT", k)], [pk])
                kb.op("act", lambda e, h=h, p=p: e.activation(out=qT[:, h, :], in_=p[:, :], func=AF.Copy),
                      reads=[pk], writes=[("qT", h)])
            for h in range(4):
                for mt in range(2):
                    p, pk = c.ps()
                    mm(c, p[:, :], kT[:, h, mt * 128:(mt + 1) * 128], qT[:, h, :], True, True, [("kT", h), ("qT", h)], [pk])
                    kb.op("act", lambda e, h=h, mt=mt, p=p: e.activation(out=pT[:, h * 2 + mt, :], in_=p[:, :], func=AF.Exp, scale=sc),
                          reads=[pk], writes=[("pT", h, mt)])
                pd, pdk = c.ps()
                for mt in range(2):
                    mm(c, pd[:, :], c.ones_b[:], pT[:, h * 2 + mt, :], mt == 0, mt == 1, ["ones_b", ("pT", h, mt)], [pdk])
                kb.op("dve", lambda e, pd=pd: e.reciprocal(out=rden[:], in_=pd[:, :]), reads=[pdk], writes=["rden"])
                po, pok = c.ps()
                for mt in range(2):
                    mm(c, po[:, :], Vt[:, mt, h * 128:(h + 1) * 128], pT[:, h * 2 + mt, :], mt == 0, mt == 1,
                       [("Vt", mt), ("pT", h, mt)], [pok])
                kb.op("dve", lambda e, h=h, po=po: e.tensor_tensor(out=oT[:, h, :], in0=po[:, :], in1=rden[:], op=ALU.mult),
                      reads=[pok, "rden"], writes=[("oT", h)])
            for d in range(8):
                p, pk = c.ps()
                for h in range(4):
                    mm(c, p[:, :], wo[:, h, d * 128:(d + 1) * 128], oT[:, h, :], h == 0, h == 3, ["wo", ("oT", h)], [pk])
                kb.op("dve", lambda e, d=d, p=p, t0=t0: e.tensor_tensor(out=xT[:, d, t0:t0 + TT], in0=xT[:, d, t0:t0 + TT],
                                                                        in1=p[:, :], op=ALU.add),
                      reads=[pk, ("xT", d)], writes=[("xT", d)])
    c.barrier()
    if io.get("dbg_stage") == 2:
        vec_st.close()
        return

    with ExitStack() as s3:
        hTall = c.sb("hTall", [128, 8, NT], BF16, s3)
        actT = c.sb("actT", [128, 8, NT], BF16, s3)
        wgu = [c.sb(f"wgu{i}", [128, 8, 256], BF16, s3) for i in range(3)]
        wdr = [c.sb(f"wdr{i}", [128, D], BF16, s3) for i in range(11)]
        sil = [c.sb(f"sil{i}", [128, TT], BF16, s3) for i in range(2)]
        if E > 1:
            wr = c.sb("wr", [128, 8, 8], F32, s3)
            lg = c.sb("lg", [128, 4, 8], F32, s3)
            top8 = c.sb("top8", [128, 4, 8], F32, s3)
            gts = c.sb("gts", [128, 16, 8], F32, s3)
            gsc = c.sb("gsc", [128, 4, 4], F32, s3)
            dgate = c.sb("dgate", [128, 128], F32, s3)
            gB = [c.sb(f"gB{i}", [128, NT], BF16, s3) for i in range(2)]
            ytmps = [c.sb(f"ytmp{i}", [128, TT], F32, s3) for i in range(2)]
            kb.dma("sp", wr[:], io["router_w"].rearrange("(k p) n -> p k n", p=128), writes=["wr"])
        with ExitStack() as s3a:
            hF = c.sb("hF", [128, 8, TT], F32, s3a) if E > 1 else None
            for t in range(NTT):
                t0 = t * TT
                rmsnorm_tile(c, xT, "xT", t0, TT, vecs[:, V_FFN:V_FFN + 8], tmp, hTall[:, :, t0:t0 + TT], f"hA{t}", out_f=hF)
                if E > 1:
                    for s in range(4):
                        p, pk = c.ps()
                        for k in range(8):
                            mm(c, p[:, 0:8], hF[:, k, s * 128:(s + 1) * 128], wr[:, k, :], k == 0, k == 7, [(f"hA{t}_f", k), "wr"], [pk])
                        kb.op("dve", lambda e, s=s, p=p: e.tensor_copy(out=lg[:, s, :], in_=p[:, 0:8]), reads=[pk], writes=[("lg", s)])
                        kb.op("dve", lambda e, s=s: e.max(out=top8[:, s, :], in_=lg[:, s, :]), reads=[("lg", s)], writes=[("top8", s)])
                        kb.op("dve", lambda e, s=s: e.tensor_scalar(out=gsc[:, s, 0:1], in0=top8[:, s, 0:1], scalar1=-1.0, scalar2=None, op0=ALU.mult),
                              reads=[("top8", s)], writes=[("gsc", s, 0)])
                        kb.op("act", lambda e, s=s: e.activation(out=gsc[:, s, 1:2], in_=top8[:, s, 1:2], func=AF.Exp, bias=gsc[:, s, 0:1]),
                              reads=[("top8", s), ("gsc", s, 0)], writes=[("gsc", s, 1)])
                        kb.op("dve", lambda e, s=s: e.tensor_scalar(out=gsc[:, s, 1:2], in0=gsc[:, s, 1:2], scalar1=1.0, scalar2=None, op0=ALU.add),
                              reads=[("gsc", s, 1)], writes=[("gsc", s, 1)])
                        kb.op("dve", lambda e, s=s: e.reciprocal(out=gsc[:, s, 1:2], in_=gsc[:, s, 1:2]),
                              reads=[("gsc", s, 1)], writes=[("gsc", s, 1)])
                        gi = t * 4 + s
                        kb.op("act", lambda e, s=s, gi=gi: e.activation(out=gts[:, gi, :], in_=lg[:, s, :], func=AF.Exp, bias=gsc[:, s, 0:1]),
                              reads=[("lg", s), ("gsc", s, 0)], writes=[("gts", gi)])
                        kb.op("dve", lambda e, s=s: e.tensor_scalar(out=lg[:, s, :], in0=lg[:, s, :], scalar1=top8[:, s, 1:2], scalar2=None, op0=ALU.is_ge),
                              reads=[("lg", s), ("top8", s)], writes=[("lg", s)])
                        kb.op("dve", lambda e, s=s, gi=gi: e.scalar_tensor_tensor(out=gts[:, gi, :], in0=gts[:, gi, :], scalar=gsc[:, s, 1:2], in1=lg[:, s, :],
                                                                                  op0=ALU.mult, op1=ALU.mult),
                              reads=[("gts", gi), ("gsc", s, 1), ("lg", s)], writes=[("gts", gi)])
        hkeys = lambda t: [(f"hA{t}", k) for k in range(8)]
        groups = [(0, 8), (8, 16), (16, 22)]
        wgu_i = 0
        wd_i = 0
        sil_i = 0
        for ex in range(E):
            if E > 1:
                gb = gB[ex % 2]
                gbk = f"gB{ex % 2}"
                for gi in range(16):
                    kb.op("dve", lambda e, gi=gi, ex=ex: e.tensor_scalar(out=dgate[:], in0=c.ident_f[:], scalar1=gts[:, gi, ex:ex + 1],
                                                                         scalar2=None, op0=ALU.mult),
                          reads=["ident_f", ("gts", gi)], writes=["dgate"])
                    p, pk = c.ps()
                    mm(c, p[:, 0:128], c.ones_f[:], dgate[:], True, True, ["ones_f", "dgate"], [pk])
                    kb.op("act", lambda e, gi=gi, p=p, gb=gb: e.activation(out=gb[:, gi * 128:(gi + 1) * 128], in_=p[:, 0:128], func=AF.Copy),
                          reads=[pk], writes=[(gbk, gi // 4)])
            for fa, fb in groups:
                nfg = fb - fa
                for f0 in range(fa, fb, 2):
                    nf = min(2, fb - f0)
                    sg, su = wgu[wgu_i % 3], wgu[(wgu_i + 1) % 3]
                    sgk, suk = f"wgu{wgu_i % 3}", f"wgu{(wgu_i + 1) % 3}"
                    wgu_i += 2
                    kb.dma("pool", sg[:, :, 0:nf * 128], io["w_gate"][ex][:, f0 * 128:(f0 + nf) * 128].rearrange("(k p) n -> p k n", p=128), writes=[sgk])
                    kb.dma("pool", su[:, :, 0:nf * 128], io["w_up"][ex][:, f0 * 128:(f0 + nf) * 128].rearrange("(k p) n -> p k n", p=128), writes=[suk])
                    for fi in range(nf):
                        fl = f0 + fi - fa
                        for t in range(NTT):
                            ts_ = slice(t * TT, (t + 1) * TT)
                            pg, pgk = c.ps()
                            pu, puk = c.ps()
                            for k in range(8):
                                mm(c, pg[:, :], sg[:, k, fi * 128:(fi + 1) * 128], hTall[:, k, ts_], k == 0, k == 7, [sgk, (f"hA{t}", k)], [pgk])
                            for k in range(8):
                                mm(c, pu[:, :], su[:, k, fi * 128:(fi + 1) * 128], hTall[:, k, ts_], k == 0, k == 7, [suk, (f"hA{t}", k)], [puk])
                            sl = sil[sil_i % 2]
                            slk = f"sil{sil_i % 2}"
                            sil_i += 1
                            kb.op("act", lambda e, pg=pg, sl=sl: e.activation(out=sl[:], in_=pg[:, :], func=AF.Silu), reads=[pgk], writes=[slk])
                            kb.op("dve", lambda e, pu=pu, sl=sl, fl=fl, ts_=ts_: e.tensor_tensor(out=actT[:, fl, ts_], in0=pu[:, :], in1=sl[:], op=ALU.mult),
                                  reads=[puk, slk], writes=[("actT", fl, t)])
                slots = []
                for fl in range(nfg):
                    f = fa + fl
                    wd = wdr[wd_i % 11]
                    wdk = f"wdr{wd_i % 11}"
                    wd_i += 1
                    kb.dma("pool", wd[:], io["w_down"][ex][f * 128:(f + 1) * 128, :], writes=[wdk])
                    slots.append((wd, wdk))
                for t in range(NTT):
                    ts_ = slice(t * TT, (t + 1) * TT)
                    for d in range(8):
                        p, pk = c.ps()
                        for fl in range(nfg):
                            wd, wdk = slots[fl]
                            mm(c, p[:, :], wd[:, d * 128:(d + 1) * 128], actT[:, fl, ts_], fl == 0, fl == nfg - 1, [wdk, ("actT", fl, t)], [pk])
                        if E > 1:
                            ytmp = ytmps[d % 2]
                            ytk = f"ytmp{d % 2}"
                            kb.op("dve", lambda e, p=p, gb=gb, ytmp=ytmp, ts_=ts_: e.tensor_tensor(out=ytmp[:], in0=p[:, :], in1=gb[:, ts_], op=ALU.mult),
                                  reads=[pk, (gbk, t)], writes=[ytk])
                            kb.op("dve", lambda e, d=d, ts_=ts_, ytmp=ytmp: e.tensor_tensor(out=xT[:, d, ts_], in0=xT[:, d, ts_], in1=ytmp[:], op=ALU.add),
                                  reads=[ytk, ("xT", d)], writes=[("xT", d)])
                        else:
                            kb.op("dve", lambda e, d=d, p=p, ts_=ts_: e.tensor_tensor(out=xT[:, d, ts_], in0=xT[:, d, ts_], in1=p[:, :], op=ALU.add),
                                  reads=[pk, ("xT", d)], writes=[("xT", d)])
    c.barrier()

    with ExitStack() as s4:
        hT = c.sb("hT", [128, 8, TT], BF16, s4)
        if not last:
            for t in range(NTT):
                t0 = t * TT
                rmsnorm_tile(c, xT, "xT", t0, TT, vecs[:, V_NEXT:V_NEXT + 8], tmp, hT, "hT")
                h_store(c, io["h_next"], hT, t0, TT, [("hT", k) for k in range(8)], ["h_next_d"])
                if t == NTT - 1 and "tail_next" in io:
                    kb.dma("sp", io["tail_next"].rearrange("(k p) n -> p k n", p=128), hT[:, :, TT - 32:TT],
                           reads=[("hT", k) for k in range(8)], writes=["tail_next_d"])
        else:
            hF2 = c.sb("hF2", [128, 8, TT], F32, s4)
            otm = c.sb("otm", [128, 4, D], F32, s4)
            for t in range(NTT):
                t0 = t * TT
                rmsnorm_tile(c, xT, "xT", t0, TT, vecs[:, V_NEXT:V_NEXT + 8], tmp, hT, "hT", out_f=hF2)
                for s in range(4):
                    for kk in range(2):
                        p, pk = c.ps()
                        for k4 in range(4):
                            k = kk * 4 + k4
                            kb.op("pe", lambda e, k=k, k4=k4, s=s, p=p: e.transpose(out=p[:, k4 * 128:(k4 + 1) * 128],
                                                                                    in_=hF2[:, k, s * 128:(s + 1) * 128], identity=c.ident_f[:]),
                                  reads=[("hT_f", k), "ident_f"], writes=[pk])
                        kb.op("act", lambda e, s=s, kk=kk, p=p: e.activation(out=otm[:, s, kk * 512:(kk + 1) * 512], in_=p[:, :], func=AF.Copy),
                              reads=[pk], writes=[("otm", s)])
                kb.dma("sp", io["out"][t0:t0 + TT, :].rearrange("(s p) d -> p s d", p=128), otm[:],
                       reads=[("otm", s) for s in range(4)])
    c.barrier()
    vec_st.close()


from contextlib import ExitStack as _ES
import ml_dtypes as _mld

NPBF = _mld.bfloat16


def build_T(E, last, dbg_stage=0):
    nc = bass.Bass("TRN2", target_bir_lowering=False)
    io = {}

    def din(name, shape, dt=F32):
        io[name] = nc.dram_tensor(name, shape, dt, kind="ExternalInput").ap()

    def dout(name, shape, dt=F32):
        io[name] = nc.dram_tensor(name, shape, dt, kind="ExternalOutput").ap()

    din("xT_in", [D, NT]); din("h_own", [D, NT], BF16); din("h_halo", [D, 32], BF16)
    din("catm", [4, 64, NT], BF16); din("catf", [4, 128, NT], BF16); din("mem", [256, D])
    din("vecs", [128, NV_T]); din("w_c", [D, 512]); din("w_out", [D, D]); din("w_q", [D, 512])
    din("w_kv", [D, D]); din("w_o", [512, D])
    din("w_gate", [E, D, DFF]); din("w_up", [E, D, DFF]); din("w_down", [E, DFF, D])
    if E > 1:
        din("router_w", [D, 8])
    if last:
        dout("out", [NT, D])
    else:
        dout("xT_out", [D, NT]); dout("h_next", [D, NT], BF16)
    io["dbg_stage"] = dbg_stage
    with _ES() as st:
        c = Ctx(nc, st)
        c.setup()
        xT = c.sb("xT", [128, 8, NT], F32)
        c.kb.dma("sp", xT[:], io["xT_in"].rearrange("(k p) n -> p k n", p=128), writes=[("xT", k) for k in range(8)])
        phase_T(c, io, E, last and not dbg_stage, xT)
        evs = []
        if not last or dbg_stage:
            key = "xT_out" if not last else "out"
            if last:
                io["xT_dbg"] = None
            evs.append(c.kb.dma("sp", io["xT_out"].rearrange("(k p) n -> p k n", p=128), xT[:],
                                reads=[("xT", k) for k in range(8)]))
        c.barrier()
        c.kb.flush()
    return nc


def vecs_T(inp, l, last):
    v = np.zeros((128, NV_T), np.float32)
    fm = lambda w: np.asarray(w, np.float32).reshape(-1, 128).T
    v[:, 0:8] = fm(inp["norm_xattn_w"][l]); v[:, 8:16] = fm(inp["norm_mem_w"][l]); v[:, 16:24] = fm(inp["norm_ffn_w"][l])
    v[:, 24:32] = fm(inp["norm_final_w"]) if last else fm(inp["norm_mix_w"][l + 1])
    v[:, 32:34] = fm(inp["conf_conv_b"][l]); v[:, 34:36] = fm(inp["conf_ln_w"][l]); v[:, 36:38] = fm(inp["conf_ln_b"][l])
    cw = np.asarray(inp["conf_conv_w"][l], np.float32)
    for j in range(31):
        v[:, 38 + 2 * j:40 + 2 * j] = fm(cw[j])
    return v


NCH = SEQ // 64
NKT = SEQ // 128
NQT = SEQ // TT
GRP = 4


def AP3(t, off, dims):
    return bass.AP(t[:].tensor, off, [list(d) for d in dims])


def log_sigmoid_tile(c, x, out, tmp1, tmp2, bias_ap, keys):
    kb = c.kb
    kx, ko, k1, k2 = keys
    kb.op("dve", lambda e: e.tensor_scalar(out=x, in0=x, scalar1=bias_ap, scalar2=None, op0=ALU.add), reads=[kx, "vecsM"], writes=[kx])
    kb.op("dve", lambda e: e.tensor_scalar(out=tmp1, in0=x, scalar1=-1.0, scalar2=None, op0=ALU.mult), reads=[kx], writes=[k1])
    kb.op("dve", lambda e: e.tensor_tensor(out=tmp1, in0=tmp1, in1=x, op=ALU.max), reads=[kx, k1], writes=[k1])
    kb.op("act", lambda e: e.activation(out=tmp1, in_=tmp1, func=AF.Exp, scale=-1.0), reads=[k1], writes=[k1])
    kb.op("dve", lambda e: e.tensor_scalar(out=tmp1, in0=tmp1, scalar1=1.0, scalar2=None, op0=ALU.add), reads=[k1], writes=[k1])
    kb.op("act", lambda e: e.activation(out=tmp1, in_=tmp1, func=AF.Ln), reads=[k1], writes=[k1])
    kb.op("dve", lambda e: e.tensor_scalar(out=tmp2, in0=x, scalar1=0.0, scalar2=None, op0=ALU.min), reads=[kx], writes=[k2])
    kb.op("dve", lambda e: e.tensor_tensor(out=out, in0=tmp2, in1=tmp1, op=ALU.subtract), reads=[k1, k2], writes=[ko])


def phase_M_mlstm(c, io):
    nc, kb = c.nc, c.kb
    from contextlib import ExitStack
    with ExitStack() as s0:
        vm = c.sb("vecsM_sb", [128, 16], F32, s0)
        wml = c.sb("wml", [128, 8, 258], BF16, s0)
        qT = c.sb("m_qT", [64, SEQ], BF16, s0)
        kT = c.sb("m_kT", [64, SEQ], BF16, s0)
        Vaug = c.sb("m_Vaug", [64, NCH, 65], BF16, s0)
        og = c.sb("m_og", [64, SEQ], BF16, s0)
        iC = c.sb("m_iC", [128, 64], F32, s0)
        fC = c.sb("m_fC", [128, 64], F32, s0)
        eps_t = c.sb("m_eps", [128, 1], F32, s0)
        kb.dma("sp", vm[:], io["vecsM"], writes=["vecsM"])
        kb.dma("pool", wml[:], io["w_ml"].rearrange("(k p) n -> p k n", p=128), writes=["wml"])
        kb.op("pool", lambda e: e.memset(Vaug[:, :, 64:65], 1.0), writes=["Vaug1"])
        kb.op("pool", lambda e: e.memset(eps_t[:], EPS), writes=["m_eps"])
        with ExitStack() as s1:
            hTb = [c.sb(f"m_hT{i}", [128, 8, TT], BF16, s1) for i in range(2)]
            zq = c.sb("m_zq", [64, TT + 3], F32, s1)
            zk = c.sb("m_zk", [64, TT + 3], F32, s1)
            cacc = [c.sb(f"m_cacc{i}", [64, TT], F32, s1) for i in range(2)]
            vt = c.sb("m_vt", [64, TT], F32, s1)
            rows = [c.sb(f"m_rows{i}", [2, TT], F32, s1) for i in range(2)]
            kb.op("pool", lambda e: e.memset(zq[:, 0:3], 0.0), writes=["zq"])
            kb.op("pool", lambda e: e.memset(zk[:, 0:3], 0.0), writes=["zk"])
            for tt in range(NQT):
                j, off = tt // 4, (tt % 4) * TT
                hT = hTb[tt % 2]
                hk = f"m_hT{tt % 2}"
                tok = slice(tt * TT, (tt + 1) * TT)
                h_load(c, io["hT_all"], hT, off, TT, [hk], j=j)
                for nm, z, col0, vc, dst in (("q", zq, 0, 0, qT), ("k", zk, 64, 5, kT)):
                    p, pk = c.ps()
                    for k in range(8):
                        mm(c, p[0:64, :], wml[:, k, col0:col0 + 64], hT[:, k, :], k == 0, k == 7, ["wml", hk], [pk])
                    zkey = "z" + nm
                    kb.op("act", lambda e, z=z, p=p: e.activation(out=z[:, 3:TT + 3], in_=p[0:64, :], func=AF.Copy), reads=[pk], writes=[zkey])
                    ca = cacc[0 if nm == "q" else 1]
                    ck = "cacc" + nm
                    kb.op("dve", lambda e, z=z, ca=ca, vc=vc: e.tensor_scalar(out=ca[:], in0=z[:, 0:TT], scalar1=vm[0:64, vc:vc + 1], scalar2=vm[0:64, vc + 4:vc + 5],
                                                                              op0=ALU.mult, op1=ALU.add), reads=[zkey, "vecsM"], writes=[ck])
                    for jj in range(1, 4):
                        kb.op("dve", lambda e, z=z, ca=ca, vc=vc, jj=jj: e.scalar_tensor_tensor(out=ca[:], in0=z[:, jj:jj + TT], scalar=vm[0:64, vc + jj:vc + jj + 1], in1=ca[:],
                                                                                                 op0=ALU.mult, op1=ALU.add), reads=[zkey, ck, "vecsM"], writes=[ck])
                    kb.op("act", lambda e, ca=ca, dst=dst, tok=tok: e.activation(out=dst[:, tok], in_=ca[:], func=AF.Silu), reads=[ck], writes=[("m_" + nm + "T", tt)])
                    kb.op("dve", lambda e, z=z: e.tensor_copy(out=z[:, 0:3], in_=z[:, TT:TT + 3]), reads=[zkey], writes=[zkey])
                p, pk = c.ps()
                for k in range(8):
                    mm(c, p[0:64, :], wml[:, k, 128:192], hT[:, k, :], k == 0, k == 7, ["wml", hk], [pk])
                kb.op("act", lambda e, p=p: e.activation(out=vt[:], in_=p[0:64, :], func=AF.Copy), reads=[pk], writes=["m_vt"])
                p2, p2k = c.ps()
                for ci in range(8):
                    kb.op("pe", lambda e, ci=ci, p2=p2: e.transpose(out=p2[0:64, ci * 64:(ci + 1) * 64], in_=vt[:, ci * 64:(ci + 1) * 64], identity=c.ident_f[0:64, 0:64]),
                          reads=["m_vt", "ident_f"], writes=[p2k])
                kb.op("dve", lambda e, p2=p2, tt=tt: e.tensor_copy(out=Vaug[:, tt * 8:(tt + 1) * 8, 0:64], in_=p2[0:64, :].rearrange("p (c d) -> p c d", d=64)),
                      reads=[p2k], writes=[("Vaug", tt)])
                p, pk = c.ps()
                for k in range(8):
                    mm(c, p[0:64, :], wml[:, k, 192:256], hT[:, k, :], k == 0, k == 7, ["wml", hk], [pk])
                kb.op("act", lambda e, p=p, tok=tok: e.activation(out=og[:, tok], in_=p[0:64, :], func=AF.Sigmoid), reads=[pk], writes=[("og", tt)])
                p, pk = c.ps()
                for k in range(8):
                    mm(c, p[0:2, :], wml[:, k, 256:258], hT[:, k, :], k == 0, k == 7, ["wml", hk], [pk])
                rw = rows[tt % 2]
                rk = f"m_rows{tt % 2}"
                kb.op("act", lambda e, p=p, rw=rw: e.activation(out=rw[:], in_=p[0:2, :], func=AF.Copy), reads=[pk], writes=[rk])
                kb.dma("sp", iC[tt * 8:(tt + 1) * 8, :], AP3(rw, 0, [[TT, 1], [64, 8], [1, 64]]), reads=[rk], writes=["iC"])
                kb.dma("sp", fC[tt * 8:(tt + 1) * 8, :], AP3(rw, TT, [[TT, 1], [64, 8], [1, 64]]), reads=[rk], writes=["fC"])
        c.barrier()
        Uall = c.sb("m_Uall", [64, 65, NCH], F32, s0)
        wgT = c.sb("m_wgT", [64, NCH], F32, s0)
        flT = c.sb("m_flT", [64, NCH], F32, s0)
        dB = c.sb("m_dB", [64, NCH], F32, s0)
        dB0 = c.sb("m_dB0", [64, NCH], F32, s0)
        with ExitStack() as s2:
            t1 = c.sb("g_t1", [128, 64], F32, s2)
            t2 = c.sb("g_t2", [128, 64], F32, s2)
            lf = c.sb("g_lf", [128, 64], F32, s2)
            bb = c.sb("g_b", [128, 64], F32, s2)
            aa = c.sb("g_a", [128, 64], F32, s2)
            AA = c.sb("g_A", [128, 64], F32, s2)
            MM = c.sb("g_M", [128, 64], F32, s2)
            wg = c.sb("g_wg", [128, 64], F32, s2)
            fl = c.sb("g_fl", [128, 64], F32, s2)
            on = c.sb("g_on", [128, 64], F32, s2)
            r1 = c.sb("g_r1", [1, 128], F32, s2)
            r2 = c.sb("g_r2", [1, 128], F32, s2)
            r3 = c.sb("g_r3", [1, 128], F32, s2)
            r4 = c.sb("g_r4", [1, 128], F32, s2)
            mcol = c.sb("g_mcol", [128, 1], F32, s2)
            nM63 = c.sb("g_nM63", [128, 1], F32, s2)
            dec = c.sb("g_dec", [128, 1], F32, s2)
            dgd = c.sb("g_dgd", [128, 128], F32, s2)
            Xb = [c.sb(f"g_X{i}", [128, 8, 64], F32, s2) for i in range(2)]
            kw32 = [c.sb(f"g_kw32{i}", [64, TT], F32, s2) for i in range(2)]
            kwTok = c.sb("g_kwTok", [64, NCH, 64], BF16, s2)
            log_sigmoid_tile(c, fC[:], lf[:], t1[:], t2[:], vm[:, 12:13], ("fC", "g_lf", "g_t1", "g_t2"))
            kb.op("dve", lambda e: e.tensor_scalar(out=iC[:], in0=iC[:], scalar1=vm[:, 11:12], scalar2=None, op0=ALU.add), reads=["iC", "vecsM"], writes=["iC"])
            kb.op("pool", lambda e: e.memset(on[:], 1.0), writes=["g_on"])
            kb.op("dve", lambda e: e.tensor_tensor_scan(out=bb[:], data0=on[:], data1=lf[:], initial=0.0, op0=ALU.mult, op1=ALU.add),
                  reads=["g_on", "g_lf"], writes=["g_b"])
            kb.op("dve", lambda e: e.tensor_tensor(out=aa[:], in0=iC[:], in1=bb[:], op=ALU.subtract), reads=["iC", "g_b"], writes=["g_a"])
            kb.op("dve", lambda e: e.tensor_tensor_scan(out=AA[:], data0=aa[:], data1=aa[:], initial=-1e30, op0=ALU.max, op1=ALU.max),
                  reads=["g_a"], writes=["g_A"])
            p, pk = c.ps()
            kb.op("pe", lambda e, p=p: e.transpose(out=p[0:1, 0:128], in_=AA[:, 63:64], identity=c.ident_f[:]), reads=["g_A", "ident_f"], writes=[pk])
            kb.op("pe", lambda e, p=p: e.transpose(out=p[0:1, 128:256], in_=bb[:, 63:64], identity=c.ident_f[:]), reads=["g_b", "ident_f"], writes=[pk])
            kb.op("dve", lambda e, p=p: e.tensor_copy(out=r1[:], in_=p[0:1, 0:128]), reads=[pk], writes=["g_r1"])
            kb.op("dve", lambda e, p=p: e.tensor_copy(out=r2[:], in_=p[0:1, 128:256]), reads=[pk], writes=["g_r2"])
            kb.op("dve", lambda e: e.tensor_tensor_scan(out=r3[:], data0=r1[:], data1=r2[:], initial=0.0, op0=ALU.max, op1=ALU.add),
                  reads=["g_r1", "g_r2"], writes=["g_r3"])
            kb.op("pool", lambda e: e.memset(r4[:, 0:1], 0.0), writes=["g_r4a"])
            kb.op("dve", lambda e: e.tensor_copy(out=r4[:, 1:128], in_=r3[:, 0:127]), reads=["g_r3"], writes=["g_r4b"])
            p, pk = c.ps()
            kb.op("pe", lambda e, p=p: e.transpose(out=p[:, 0:1], in_=r4[:], identity=c.ident_f[0:1, 0:1]), reads=["g_r4a", "g_r4b", "ident_f"], writes=[pk])
            kb.op("dve", lambda e, p=p: e.tensor_copy(out=mcol[:], in_=p[:, 0:1]), reads=[pk], writes=["g_mcol"])
            kb.op("dve", lambda e: e.tensor_scalar(out=MM[:], in0=AA[:], scalar1=mcol[:, 0:1], scalar2=None, op0=ALU.max), reads=["g_A", "g_mcol"], writes=["g_M"])
            kb.op("dve", lambda e: e.tensor_scalar(out=nM63[:], in0=MM[:, 63:64], scalar1=-1.0, scalar2=None, op0=ALU.mult), reads=["g_M"], writes=["g_nM63"])
            kb.op("act", lambda e: e.activation(out=wg[:], in_=aa[:], func=AF.Exp, bias=nM63[:, 0:1]), reads=["g_a", "g_nM63"], writes=["g_wg"])
            kb.op("act", lambda e: e.activation(out=dec[:], in_=mcol[:], func=AF.Exp, bias=nM63[:, 0:1]), reads=["g_mcol", "g_nM63"], writes=["g_dec"])
            kb.op("act", lambda e: e.activation(out=fl[:], in_=bb[:], func=AF.Exp, scale=-1.0, bias=nM63[:, 0:1]), reads=["g_b", "g_nM63"], writes=["g_fl"])
            p, pk = c.ps()
            kb.op("pe", lambda e, p=p: e.transpose(out=p[0:64, 0:128], in_=wg[:], identity=c.ident_f[:]), reads=["g_wg", "ident_f"], writes=[pk])
            kb.op("pe", lambda e, p=p: e.transpose(out=p[0:64, 128:256], in_=fl[:], identity=c.ident_f[:]), reads=["g_fl", "ident_f"], writes=[pk])
            kb.op("dve", lambda e, p=p: e.tensor_copy(out=wgT[:], in_=p[0:64, 0:128]), reads=[pk], writes=["m_wgT"])
            kb.op("dve", lambda e, p=p: e.tensor_copy(out=flT[:], in_=p[0:64, 128:256]), reads=[pk], writes=["m_flT"])
            kb.op("dve", lambda e: e.tensor_scalar(out=dgd[:], in0=c.ident_f[:], scalar1=dec[:, 0:1], scalar2=None, op0=ALU.mult), reads=["ident_f", "g_dec"], writes=["g_dgd"])
            p, pk = c.ps()
            mm(c, p[0:64, 0:128], c.ones_f[:, 0:64], dgd[:], True, True, ["ones_f", "g_dgd"], [pk])
            kb.op("dve", lambda e, p=p: e.tensor_copy(out=dB[:], in_=p[0:64, 0:128]), reads=[pk], writes=["m_dB"])
            kb.op("dve", lambda e, p=p: e.tensor_copy(out=dB0[:], in_=p[0:64, 0:128]), reads=[pk], writes=["m_dB0"])
            kb.op("pool", lambda e: e.memset(dB0[:, 0:1], 0.0), reads=["m_dB0"], writes=["m_dB0"])
            for tt in range(NQT):
                tok = slice(tt * TT, (tt + 1) * TT)
                X = Xb[tt % 2]
                Xk = f"g_X{tt % 2}"
                kb.op("dve", lambda e, X=X, tt=tt: e.tensor_tensor(out=X[:], in0=AP3(c.ident_f, 8 * tt, [[128, 128], [1, 8], [0, 64]]),
                                                                   in1=AP3(wg, 0, [[64, 128], [0, 8], [1, 64]]), op=ALU.mult),
                      reads=["ident_f", "g_wg"], writes=[Xk])
                p, pk = c.ps()
                mm(c, p[0:64, :], c.ones_f[:, 0:64], X[:].rearrange("p c s -> p (c s)"), True, True, ["ones_f", Xk], [pk])
                k32 = kw32[tt % 2]
                k32k = f"g_kw32{tt % 2}"
                kb.op("dve", lambda e, p=p, k32=k32, tok=tok: e.scalar_tensor_tensor(out=k32[:], in0=kT[:, tok], scalar=0.125, in1=p[0:64, :], op0=ALU.mult, op1=ALU.mult),
                      reads=[pk, ("m_kT", tt)], writes=[k32k])
                kb.op("act", lambda e, k32=k32, tok=tok: e.activation(out=kT[:, tok], in_=k32[:], func=AF.Copy), reads=[k32k], writes=[("m_kT", tt)])
                p2, p2k = c.ps()
                for ci in range(8):
                    kb.op("pe", lambda e, ci=ci, p2=p2, k32=k32: e.transpose(out=p2[0:64, ci * 64:(ci + 1) * 64], in_=k32[:, ci * 64:(ci + 1) * 64], identity=c.ident_f[0:64, 0:64]),
                          reads=[k32k, "ident_f"], writes=[p2k])
                kb.op("dve", lambda e, p2=p2, tt=tt: e.tensor_copy(out=kwTok[:, tt * 8:(tt + 1) * 8, :], in_=p2[0:64, :].rearrange("p (c d) -> p c d", d=64)),
                      reads=[p2k], writes=[("kwTok", tt)])
            for g in range(NCH // GRP):
                p, pk = c.ps()
                for ci in range(GRP):
                    ch = g * GRP + ci
                    mm(c, p[0:64, ci * 65:(ci + 1) * 65], kwTok[:, ch, :], Vaug[:, ch, :], True, True, [("kwTok", ch // 8), ("Vaug", ch // 8), "Vaug1"], [pk])
                kb.op("dve", lambda e, p=p, g=g: e.tensor_copy(out=AP3(Uall, g * GRP, [[65 * NCH, 64], [1, GRP], [NCH, 65]]),
                                                               in_=p[0:64, 0:GRP * 65].rearrange("p (c d) -> p c d", d=65)),
                      reads=[pk], writes=["Uall"])
        c.barrier()
        Cn = Uall
        Eb = c.sb("m_E", [64, NCH, 65], BF16, s0)
        for dv in range(65):
            kb.op("dve", lambda e, dv=dv: e.tensor_tensor_scan(out=Cn[:, dv, :], data0=dB0[:], data1=Uall[:, dv, :], initial=0.0, op0=ALU.mult, op1=ALU.add),
                  reads=["m_dB0", "Uall"], writes=[("Cn", dv), "Uall"])
        kb.op("pool", lambda e: e.memset(Eb[:, 0:1, :], 0.0), writes=["E0"])
        kb.op("dve", lambda e: e.tensor_tensor(out=Eb[:, 1:NCH, :], in0=AP3(Cn, 0, [[65 * NCH, 64], [1, NCH - 1], [NCH, 65]]),
                                               in1=AP3(dB, 1, [[NCH, 64], [1, NCH - 1], [0, 65]]), op=ALU.mult),
              reads=[("Cn", dv) for dv in range(65)] + ["m_dB"], writes=["E"])
        with ExitStack() as s4:
            mask = c.sb("o_mask", [64, 64], F32, s4)
            sT = [c.sb(f"o_sT{i}", [64, GRP * 64], BF16, s4) for i in range(2)]
            den = c.sb("o_den", [64, GRP], F32, s4)
            hn = c.sb("o_hn", [64, GRP, 64], F32, s4)
            hsq = c.sb("o_hsq", [64, GRP, 64], F32, s4)
            ss = c.sb("o_ss", [64, GRP], F32, s4)
            cst = [c.sb(f"o_cst{i}", [64, GRP * 64], BF16, s4) for i in range(2)]
            kb.op("pool", lambda e: e.memset(mask[:], 1.0), writes=["o_mask"])
            kb.op("pool", lambda e: e.affine_select(out=mask[:], in_=mask[:], pattern=[[1, 64]], compare_op=ALU.is_ge, fill=0.0, base=0, channel_multiplier=-1),
                  reads=["o_mask"], writes=["o_mask"])
            for g in range(NCH // GRP):
                c0 = g * GRP
                p, pk = c.ps()
                for ci in range(GRP):
                    ch = c0 + ci
                    cs = slice(ch * 64, (ch + 1) * 64)
                    mm(c, p[0:64, ci * 64:(ci + 1) * 64], kT[:, cs], qT[:, cs], True, True, [("m_kT", ch // 8), ("m_qT", ch // 8)], [pk])
                st_ = sT[g % 2]
                stk = f"o_sT{g % 2}"
                kb.op("dve", lambda e, p=p, st_=st_: e.tensor_tensor(out=st_[:].rearrange("p (c t) -> p c t", t=64), in0=p[0:64, 0:GRP * 64].rearrange("p (c t) -> p c t", t=64),
                                                                     in1=AP3(mask, 0, [[64, 64], [0, GRP], [1, 64]]), op=ALU.mult),
                      reads=[pk, "o_mask"], writes=[stk])
                po, pok = c.ps()
                for ci in range(GRP):
                    ch = c0 + ci
                    cs = slice(ch * 64, (ch + 1) * 64)
                    mm(c, po[0:64, ci * 65:(ci + 1) * 65], st_[:, ci * 64:(ci + 1) * 64], Vaug[:, ch, :], True, False, [stk, ("Vaug", ch // 8), "Vaug1"], [pok])
                    mm(c, po[0:64, ci * 65:(ci + 1) * 65], qT[:, cs], Eb[:, ch, :], False, True, [("m_qT", ch // 8), "E", "E0"], [pok])
                po3 = po[0:64, 0:GRP * 65].rearrange("p (c d) -> p c d", d=65)
                den3 = den[:].rearrange("p (c o) -> p c o", o=1)
                kb.op("dve", lambda e, po3=po3, den3=den3: e.tensor_scalar(out=den3, in0=po3[:, :, 64:65], scalar1=-1.0, scalar2=None, op0=ALU.mult),
                      reads=[pok], writes=["o_den"])
                kb.op("dve", lambda e, po3=po3, den3=den3: e.tensor_tensor(out=den3, in0=po3[:, :, 64:65], in1=den3, op=ALU.max),
                      reads=[pok, "o_den"], writes=["o_den"])
                kb.op("dve", lambda e, c0=c0: e.tensor_tensor(out=den[:], in0=den[:], in1=flT[:, c0:c0 + GRP], op=ALU.max),
                      reads=["o_den", "m_flT"], writes=["o_den"])
                kb.op("dve", lambda e: e.reciprocal(out=den[:], in_=den[:]), reads=["o_den"], writes=["o_den"])
                kb.op("dve", lambda e, po3=po3: e.tensor_tensor(out=hn[:], in0=po3[:, :, 0:64], in1=AP3(den, 0, [[GRP, 64], [1, GRP], [0, 64]]), op=ALU.mult),
                      reads=[pok, "o_den"], writes=["o_hn"])
                kb.op("act", lambda e: e.activation(out=hsq[:], in_=hn[:], func=AF.Square), reads=["o_hn"], writes=["o_hsq"])
                kb.op("dve", lambda e: e.tensor_reduce(out=ss[:], in_=hsq[:], axis=AX.X, op=ALU.add), reads=["o_hsq"], writes=["o_ss"])
                kb.op("act", lambda e: e.activation(out=ss[:], in_=ss[:], func=AF.Sqrt, scale=1.0 / 64, bias=eps_t[0:64, 0:1]), reads=["o_ss", "m_eps"], writes=["o_ss"])
                kb.op("dve", lambda e: e.reciprocal(out=ss[:], in_=ss[:]), reads=["o_ss"], writes=["o_ss"])
                kb.op("dve", lambda e: e.tensor_tensor(out=hn[:], in0=hn[:], in1=AP3(ss, 0, [[GRP, 64], [1, GRP], [0, 64]]), op=ALU.mult),
                      reads=["o_hn", "o_ss"], writes=["o_hn"])
                pt, ptk = c.ps()
                for ci in range(GRP):
                    kb.op("pe", lambda e, ci=ci, pt=pt: e.transpose(out=pt[0:64, ci * 64:(ci + 1) * 64], in_=hn[:, ci, :], identity=c.ident_f[0:64, 0:64]),
                          reads=["o_hn", "ident_f"], writes=[ptk])
                cs_ = cst[g % 2]
                csk = f"o_cst{g % 2}"
                toks = slice(c0 * 64, (c0 + GRP) * 64)
                kb.op("dve", lambda e, pt=pt, cs_=cs_, toks=toks: e.scalar_tensor_tensor(out=cs_[:], in0=pt[0:64, 0:GRP * 64], scalar=vm[0:64, 10:11], in1=og[:, toks],
                                                                                       op0=ALU.mult, op1=ALU.mult),
                      reads=[ptk, "vecsM", ("og", (c0 * 64) // TT)], writes=[csk])
                kb.dma("sp", io["catm_out"][:, toks], cs_[:], reads=[csk])
        c.barrier()


def phase_M_fox(c, io):
    nc, kb = c.nc, c.kb
    from contextlib import ExitStack
    NEG = -30000.0
    with ExitStack() as s0:
        vm = c.sb("vecsMf_sb", [128, 16], F32, s0)
        wfx = c.sb("wfx", [128, 8, 386], BF16, s0)
        fq = [c.sb(f"f_q{h}", [128, SEQ], BF16, s0) for h in range(2)]
        fkk = c.sb("f_kk", [128, SEQ], BF16, s0)
        kb.op("pool", lambda e: e.memset(fq[0][64:128, :], 0.0), writes=[("fqz", 0)])
        kb.op("pool", lambda e: e.memset(fq[1][0:64, :], 0.0), writes=[("fqz", 1)])
        fV = [c.sb(f"f_V{h}", [128, NKT, 65], BF16, s0) for h in range(2)]
        fC = [c.sb(f"f_C{h}", [64, 128], F32, s0) for h in range(2)]
        kb.dma("sp", vm[:], io["vecsM"], writes=["vecsM"])
        kb.dma("pool", wfx[:], io["w_fx"].rearrange("(k p) n -> p k n", p=128), writes=["wfx"])
        for h in range(2):
            kb.op("pool", lambda e, h=h: e.memset(fV[h][:, :, 64:65], 1.0), writes=[("fV1", h)])
        with ExitStack() as s1:
            hTb = [c.sb(f"f_hT{i}", [128, 8, TT], BF16, s1) for i in range(2)]
            vt = c.sb("f_vt", [128, TT], F32, s1)
            rows = [c.sb(f"f_rows{i}", [2, TT], F32, s1) for i in range(2)]
            for tt in range(NQT):
                j, off = tt // 4, (tt % 4) * TT
                hT = hTb[tt % 2]
                hk = f"f_hT{tt % 2}"
                tok = slice(tt * TT, (tt + 1) * TT)
                h_load(c, io["hT_all"], hT, off, TT, [hk], j=j)
                p, pk = c.ps()
                for k in range(8):
                    mm(c, p[:, :], wfx[:, k, 0:128], hT[:, k, :], k == 0, k == 7, ["wfx", hk], [pk])
                kb.op("act", lambda e, p=p, tok=tok: e.activation(out=fq[0][0:64, tok], in_=p[0:64, :], func=AF.Copy, scale=0.125), reads=[pk], writes=[("fq", 0, tt)])
                kb.op("act", lambda e, p=p, tok=tok: e.activation(out=fq[1][64:128, tok], in_=p[64:128, :], func=AF.Copy, scale=0.125), reads=[pk], writes=[("fq", 1, tt)])
                p, pk = c.ps()
                for k in range(8):
                    mm(c, p[:, :], wfx[:, k, 128:256], hT[:, k, :], k == 0, k == 7, ["wfx", hk], [pk])
                kb.op("act", lambda e, p=p, tok=tok: e.activation(out=fkk[:, tok], in_=p[:, :], func=AF.Copy), reads=[pk], writes=[("fk", tt)])
                p, pk = c.ps()
                for k in range(8):
                    mm(c, p[:, :], wfx[:, k, 256:384], hT[:, k, :], k == 0, k == 7, ["wfx", hk], [pk])
                kb.op("act", lambda e, p=p: e.activation(out=vt[:], in_=p[:, :], func=AF.Copy), reads=[pk], writes=["f_vt"])
                p2, p2k = c.ps()
                for ci in range(4):
                    kb.op("pe", lambda e, ci=ci, p2=p2: e.transpose(out=p2[:, ci * 128:(ci + 1) * 128], in_=vt[:, ci * 128:(ci + 1) * 128], identity=c.ident_f[:]),
                          reads=["f_vt", "ident_f"], writes=[p2k])
                for h in range(2):
                    kb.op("dve", lambda e, p2=p2, tt=tt, h=h: e.tensor_copy(out=fV[h][:, tt * 4:(tt + 1) * 4, 0:64],
                                                                         in_=p2[:, :].rearrange("p (c d) -> p c d", d=128)[:, :, h * 64:(h + 1) * 64]),
                          reads=[p2k], writes=[("fV", h, tt)])
                p, pk = c.ps()
                for k in range(8):
                    mm(c, p[0:2, :], wfx[:, k, 384:386], hT[:, k, :], k == 0, k == 7, ["wfx", hk], [pk])
                rw = rows[tt % 2]
                rk = f"f_rows{tt % 2}"
                kb.op("act", lambda e, p=p, rw=rw: e.activation(out=rw[:], in_=p[0:2, :], func=AF.Copy), reads=[pk], writes=[rk])
                for h in range(2):
                    kb.dma("sp", fC[h][tt * 4:(tt + 1) * 4, :], AP3(rw, h * TT, [[TT, 1], [128, 4], [1, 128]]), reads=[rk], writes=[("fC", h)])
        c.barrier()
        ckT = [c.sb(f"f_ckT{h}", [128, NKT], F32, s0) for h in range(2)]
        cC = [c.sb(f"f_cC{h}", [64, 128], F32, s0) for h in range(2)]
        negm = c.sb("f_negm", [128, 4, TT], F32, s0)
        Ls = c.sb("f_Ls", [64, 64], F32, s0)
        with ExitStack() as s2:
            t1 = c.sb("f_t1", [64, 128], F32, s2)
            t2 = c.sb("f_t2", [64, 128], F32, s2)
            lf = c.sb("f_lf", [64, 128], F32, s2)
            on = c.sb("f_on", [64, 128], F32, s2)
            pre = c.sb("f_pre", [64, 1], F32, s2)
            kb.op("pool", lambda e: e.memset(on[:], 1.0), writes=["f_on"])
            kb.op("pool", lambda e: e.memset(Ls[:], 1.0), writes=["f_Ls"])
            kb.op("pool", lambda e: e.affine_select(out=Ls[:], in_=Ls[:], pattern=[[1, 64]], compare_op=ALU.is_ge, fill=0.0, base=-1, channel_multiplier=-1),
                  reads=["f_Ls"], writes=["f_Ls"])
            for r in range(4):
                kb.op("pool", lambda e, r=r: e.memset(negm[:, r, :], 0.0), writes=[("negm", r)])
                kb.op("pool", lambda e, r=r: e.affine_select(out=negm[:, r, :], in_=negm[:, r, :], pattern=[[1, TT]], compare_op=ALU.is_ge, fill=NEG,
                                                             base=-128 * r, channel_multiplier=-1), reads=[("negm", r)], writes=[("negm", r)])
            for h in range(2):
                log_sigmoid_tile(c, fC[h][:], lf[:], t1[:], t2[:], vm[0:64, 13 + h:14 + h], (("fC", h), "f_lf", "f_t1", "f_t2"))
                kb.op("dve", lambda e, h=h: e.tensor_tensor_scan(out=cC[h][:], data0=on[:], data1=lf[:], initial=0.0, op0=ALU.mult, op1=ALU.add),
                      reads=["f_on", "f_lf"], writes=[("cC", h)])
                p, pk = c.ps()
                mm(c, p[0:64, 0:1], Ls[:], cC[h][:, 127:128], True, True, ["f_Ls", ("cC", h)], [pk])
                kb.op("dve", lambda e, p=p: e.tensor_copy(out=pre[:], in_=p[0:64, 0:1]), reads=[pk], writes=["f_pre"])
                kb.op("dve", lambda e, h=h: e.tensor_scalar(out=cC[h][:], in0=cC[h][:], scalar1=pre[:, 0:1], scalar2=None, op0=ALU.add), reads=[("cC", h), "f_pre"], writes=[("cC", h)])
                p, pk = c.ps()
                kb.op("pe", lambda e, p=p, h=h: e.transpose(out=p[:, 0:64], in_=cC[h][:], identity=c.ident_f[0:64, 0:64]), reads=[("cC", h), "ident_f"], writes=[pk])
                kb.op("dve", lambda e, p=p, h=h: e.tensor_scalar(out=ckT[h][:], in0=p[:, 0:64], scalar1=-1.0, scalar2=None, op0=ALU.mult), reads=[pk], writes=[("ckT", h)])
        c.barrier()
        with ExitStack() as s3:
            X = c.sb("f_X", [64, 4, 128], F32, s3)
            cqB = c.sb("f_cqB", [128, TT], F32, s3)
            cqD = c.sb("f_cqD", [128, 4, TT], F32, s3)
            NB = 5
            tmpb = [c.sb(f"f_tmp{i}", [128, TT], F32, s3) for i in range(NB)]
            pTb = [c.sb(f"f_pT{i}", [128, TT], BF16, s3) for i in range(NB)]
            osb = c.sb("f_osb", [65, TT], F32, s3)
            rden = c.sb("f_rden", [64, TT], F32, s3)
            outb = [c.sb(f"f_out{i}", [64, TT], BF16, s3) for i in range(2)]
            it = 0
            for h in range(2):
                for qi in range(NQT):
                    qs = slice(qi * TT, (qi + 1) * TT)
                    kb.op("dve", lambda e, h=h, qi=qi: e.tensor_tensor(out=X[:], in0=AP3(c.ident_f, 4 * qi, [[128, 64], [1, 4], [0, 128]]),
                                                                       in1=AP3(cC[h], 0, [[128, 64], [0, 4], [1, 128]]), op=ALU.mult),
                          reads=["ident_f", ("cC", h)], writes=["f_X"])
                    p, pk = c.ps()
                    mm(c, p[:, :], c.ones_f[0:64, :], X[:].rearrange("p r s -> p (r s)"), True, True, ["ones_f", "f_X"], [pk])
                    kb.op("act", lambda e, p=p: e.activation(out=cqB[:], in_=p[:, :], func=AF.Copy), reads=[pk], writes=["f_cqB"])
                    for r in range(4):
                        kb.op("pool", lambda e, r=r: e.tensor_tensor(out=cqD[:, r, :], in0=cqB[:], in1=negm[:, r, :], op=ALU.add),
                              reads=["f_cqB", ("negm", r)], writes=[("f_cqD", r)])
                    c.rot = list(range(6))
                    po, pok = c.psb[6 + qi % 2], f"psb{6 + qi % 2}"
                    nk = 4 * (qi + 1)
                    LA = 4
                    sbank = {}

                    def emit_S(kt):
                        ps_, psk = c.ps()
                        mm(c, ps_[:, :], fkk[:, kt * 128:(kt + 1) * 128], fq[h][:, qs], True, True, [("fk", kt // 4), ("fq", h, qi), ("fqz", h)], [psk])
                        sbank[kt] = (ps_, psk)

                    for kt in range(min(LA, nk)):
                        emit_S(kt)
                    for kt in range(nk):
                        ps_, psk = sbank.pop(kt)
                        tb = tmpb[it % NB]; tbk = f"f_tmp{it % NB}"
                        pb = pTb[it % NB]; pbk = f"f_pT{it % NB}"
                        it += 1
                        r = kt - 4 * qi
                        if r >= 0:
                            kb.op("dve", lambda e, ps_=ps_, tb=tb, r=r: e.tensor_tensor(out=tb[:], in0=ps_[:, :], in1=cqD[:, r, :], op=ALU.add),
                                  reads=[psk, ("f_cqD", r)], writes=[tbk])
                        else:
                            kb.op("dve", lambda e, ps_=ps_, tb=tb: e.tensor_tensor(out=tb[:], in0=ps_[:, :], in1=cqB[:], op=ALU.add),
                                  reads=[psk, "f_cqB"], writes=[tbk])
                        kb.op("act", lambda e, tb=tb, pb=pb, h=h, kt=kt: e.activation(out=pb[:], in_=tb[:], func=AF.Exp, bias=ckT[h][:, kt:kt + 1]),
                              reads=[tbk, ("ckT", h)], writes=[pbk])
                        if kt + LA < nk:
                            emit_S(kt + LA)
                        mm(c, po[0:65, :], fV[h][:, kt, :], pb[:], kt == 0, kt == nk - 1, [("fV", h, kt // 4), ("fV1", h), pbk], [pok])
                    kb.op("act", lambda e, po=po: e.activation(out=osb[:], in_=po[0:65, :], func=AF.Copy), reads=[pok], writes=["f_osb"])
                    pd, pdk = c.ps()
                    mm(c, pd[0:64, :], c.ones_f[64:65, 0:64], osb[64:65, :], True, True, ["ones_f", "f_osb"], [pdk])
                    kb.op("dve", lambda e, pd=pd: e.reciprocal(out=rden[:], in_=pd[0:64, :]), reads=[pdk], writes=["f_rden"])
                    ob = outb[qi % 2]; obk = f"f_out{qi % 2}"
                    kb.op("dve", lambda e, ob=ob: e.tensor_tensor(out=ob[:], in0=osb[0:64, :], in1=rden[:], op=ALU.mult), reads=["f_osb", "f_rden"], writes=[obk])
                    cfo = io["catf_out"]
                    kb.dma("sp", (cfo[h][:, qs] if isinstance(cfo, list) else cfo[h * 64:(h + 1) * 64, qs]), ob[:], reads=[obk])
            c.rot = None
        c.barrier()


def build_M(which="both"):
    nc = bass.Bass("TRN2", target_bir_lowering=False)
    io = {}

    def din(name, shape, dt=F32):
        io[name] = nc.dram_tensor(name, shape, dt, kind="ExternalInput").ap()

    def dout(name, shape, dt=F32):
        io[name] = nc.dram_tensor(name, shape, dt, kind="ExternalOutput").ap()

    din("hT_all", [4, D, NT], BF16); din("w_ml", [D, 258]); din("w_fx", [D, 386]); din("vecsM", [128, 16])
    dout("catm_out", [64, SEQ], BF16); dout("catf_out", [128, SEQ], BF16)
    with _ES() as st:
        c = Ctx(nc, st)
        c.setup()
        if which in ("both", "mlstm"):
            phase_M_mlstm(c, io)
        if which in ("both", "fox"):
            phase_M_fox(c, io)
        c.barrier()
        c.kb.flush()
    return nc


def inputs_M(inp, l, g):
    w_in = np.asarray(inp["w_in"][l], np.float32)
    cols = np.concatenate([
        np.arange(g * 64, g * 64 + 64), 256 + np.arange(g * 64, g * 64 + 64),
        512 + np.arange(g * 64, g * 64 + 64), 768 + np.arange(g * 64, g * 64 + 64),
        [1024 + g, 1028 + g]])
    w_ml = np.ascontiguousarray(w_in[:, cols])
    fcols = []
    for base in (1544, 2056, 2568):
        for hh in (2 * g, 2 * g + 1):
            fcols.append(base + np.arange(hh * 64, hh * 64 + 64))
    fcols.append(np.array([3080 + 2 * g, 3080 + 2 * g + 1]))
    w_fx = np.ascontiguousarray(w_in[:, np.concatenate(fcols)])
    v = np.zeros((128, 16), np.float32)
    cw = np.asarray(inp["mlstm_conv_w"][l], np.float32)
    cb = np.asarray(inp["mlstm_conv_b"][l], np.float32)
    v[0:64, 0:4] = cw[:, g * 64:g * 64 + 64].T
    v[0:64, 4] = cb[g * 64:g * 64 + 64]
    v[0:64, 5:9] = cw[:, 256 + g * 64:256 + g * 64 + 64].T
    v[0:64, 9] = cb[256 + g * 64:256 + g * 64 + 64]
    v[0:64, 10] = np.asarray(inp["mlstm_norm_w"][l], np.float32)[g * 64:g * 64 + 64]
    v[:, 11] = inp["mlstm_b_i"][l][g]
    v[:, 12] = inp["mlstm_b_f"][l][g]
    v[:, 13] = inp["fox_b_f"][l][2 * g]
    v[:, 14] = inp["fox_b_f"][l][2 * g + 1]
    return {"w_ml": w_ml, "w_fx": w_fx, "vecsM": v}


def phase_P(c, io, xT):
    kb = c.kb
    from contextlib import ExitStack
    with ExitStack() as s0:
        vecs = c.sb("vecsP_sb", [128, 8], F32, s0)
        eps_t = c.sb("p_eps", [128, 1], F32, s0)
        sq = c.sb("p_sq", [128, 8, TT], BF16, s0)
        rstd = c.sb("p_rstd", [128, TT], F32, s0)
        hT = c.sb("p_hT", [128, 8, TT], BF16, s0)
        xt = [c.sb(f"p_xt{i}", [128, 4, D], F32, s0) for i in range(2)]
        tmp = {"sq": sq, "rstd": rstd, "eps": eps_t}
        kb.dma("sp", vecs[:], io["vecsP"], writes=["vecs"])
        kb.op("pool", lambda e: e.memset(eps_t[:], EPS), writes=["eps"])
        for t in range(NTT):
            t0 = t * TT
            xb = xt[t % 2]
            xk = f"p_xt{t % 2}"
            kb.dma("sp", xb[:], io["x_tok"][t0:t0 + TT, :].rearrange("(s p) d -> p s d", p=128), writes=[xk])
            for k in range(8):
                p, pk = c.ps()
                for s in range(4):
                    kb.op("pe", lambda e, k=k, s=s, p=p, xb=xb: e.transpose(out=p[:, s * 128:(s + 1) * 128], in_=xb[:, s, k * 128:(k + 1) * 128], identity=c.ident_f[:]),
                          reads=[xk, "ident_f"], writes=[pk])
                kb.op("act", lambda e, k=k, p=p, t0=t0: e.activation(out=xT[:, k, t0:t0 + TT], in_=p[:, :], func=AF.Copy), reads=[pk], writes=[("xT", k)])
            rmsnorm_tile(c, xT, "xT", t0, TT, vecs[:, 0:8], tmp, hT, "hT")
            h_store(c, io["h_next"], hT, t0, TT, [("hT", k) for k in range(8)], ["h_next_d"])
            if t == NTT - 1 and "tail_next" in io:
                kb.dma("sp", io["tail_next"].rearrange("(k p) n -> p k n", p=128), hT[:, :, TT - 32:TT],
                       reads=[("hT", k) for k in range(8)], writes=["tail_next_d"])
    c.barrier()


def build_P():
    nc = bass.Bass("TRN2", target_bir_lowering=False)
    io = {}
    io["x_tok"] = nc.dram_tensor("x_tok", [NT, D], F32, kind="ExternalInput").ap()
    io["vecsP"] = nc.dram_tensor("vecsP", [128, 8], F32, kind="ExternalInput").ap()
    io["xT_out"] = nc.dram_tensor("xT_out", [D, NT], F32, kind="ExternalOutput").ap()
    io["h_next"] = nc.dram_tensor("h_next", [D, NT], BF16, kind="ExternalOutput").ap()
    with _ES() as st:
        c = Ctx(nc, st)
        c.setup()
        xT = c.sb("xT", [128, 8, NT], F32)
        phase_P(c, io, xT)
        c.kb.dma("sp", io["xT_out"].rearrange("(k p) n -> p k n", p=128), xT[:], reads=[("xT", k) for k in range(8)])
        c.barrier()
        c.kb.flush()
    return nc


_CACHE = {}


def _get(name, fn):
    if name not in _CACHE:
        _CACHE[name] = fn()
    return _CACHE[name]


def kernel(**inp):
    inp = {k: np.asarray(v) for k, v in inp.items()}
    cores = list(range(8))
    B = 2
    x = inp["x"].astype(np.float32, copy=False)
    fm = lambda w: np.ascontiguousarray(np.asarray(w, np.float32).reshape(-1, 128).T)
    ncP = _get("P", build_P)
    maps = []
    for cid in cores:
        b, j = cid // 4, cid % 4
        maps.append({"x_tok": np.ascontiguousarray(x[b, j * NT:(j + 1) * NT]), "vecsP": fm(inp["norm_mix_w"][0])})
    res = run_bass_kernel_spmd(ncP, maps, core_ids=cores).results
    xT = [r["xT_out"] for r in res]
    hN = [r["h_next"] for r in res]
    out = None
    for l in range(2):
        last = (l == 1)
        E = 1 if l == 0 else 8
        ncM = _get("M", build_M)
        maps = []
        for cid in cores:
            b, g = cid // 4, cid % 4
            m = inputs_M(inp, l, g)
            m["hT_all"] = np.ascontiguousarray(np.stack([hN[b * 4 + jj] for jj in range(4)], axis=0))
            maps.append(m)
        resM = run_bass_kernel_spmd(ncM, maps, core_ids=cores).results
        ncT = _get(("T", E, last), lambda: build_T(E, last))
        maps = []
        for cid in cores:
            b, j = cid // 4, cid % 4
            tk = slice(j * NT, (j + 1) * NT)
            m = {"xT_in": xT[cid], "h_own": hN[cid]}
            m["h_halo"] = (np.ascontiguousarray(hN[cid - 1][:, NT - 32:NT]) if j > 0 else np.zeros((D, 32), NPBF))
            m["catm"] = np.ascontiguousarray(np.stack([resM[b * 4 + g]["catm_out"][:, tk] for g in range(4)], axis=0))
            m["catf"] = np.ascontiguousarray(np.stack([resM[b * 4 + g]["catf_out"][:, tk] for g in range(4)], axis=0))
            m["mem"] = np.ascontiguousarray(inp["mem"][b], dtype=np.float32)
            m["vecs"] = vecs_T(inp, l, last)
            m["w_c"] = np.ascontiguousarray(inp["w_in"][l][:, 1032:1544])
            m["w_out"] = inp["w_out"][l]; m["w_q"] = inp["xattn_w_q"][l]
            m["w_kv"] = inp["xattn_w_kv"][l]; m["w_o"] = inp["xattn_w_o"][l]
            if E == 1:
                m["w_gate"] = inp["ffn_w_gate"]; m["w_up"] = inp["ffn_w_up"]; m["w_down"] = inp["ffn_w_down"]
            else:
                m["w_gate"] = inp["moe_w_gate"][0]; m["w_up"] = inp["moe_w_up"][0]; m["w_down"] = inp["moe_w_down"][0]
                m["router_w"] = inp["router_w"][0]
            maps.append(m)
        resT = run_bass_kernel_spmd(ncT, maps, core_ids=cores).results
        if not last:
            xT = [r["xT_out"] for r in resT]
            hN = [r["h_next"] for r in resT]
        else:
            out = np.zeros((B, SEQ, D), np.float32)
            for cid in cores:
                b, j = cid // 4, cid % 4
                out[b, j * NT:(j + 1) * NT] = resT[cid]["out"]
    return out


RG = [[0, 1, 2, 3], [4, 5, 6, 7]]
_STOP = None


def build_fused(stop=None):
    nc = bass.Bass("TRN2", target_bir_lowering=False)
    io = {}
    if stop:
        io["dbg1"] = nc.dram_tensor("dbg1", [4 * D, NT], BF16, kind="ExternalOutput").ap()
        io["dbg2"] = nc.dram_tensor("dbg2", [512, SEQ], BF16, kind="ExternalOutput").ap()
        io["dbg3"] = nc.dram_tensor("dbg3", [D, NT], F32, kind="ExternalOutput").ap()

    def din(name, shape, dt=F32):
        io[name] = nc.dram_tensor(name, shape, dt, kind="ExternalInput").ap()
        return io[name]

    def dint(name, shape, dt=BF16):
        io[name] = nc.dram_tensor(name, shape, dt, kind="Internal").ap()
        return io[name]

    din("x_tok", [NT, D]); din("vecsP", [128, 8])
    if stop != "AG":
        din("sel", [128, 8]); din("mem", [256, D])
    for l in range(2 if stop != "AG" else 0):
        din(f"w_ml{l}", [D, 258]); din(f"w_fx{l}", [D, 386]); din(f"vecsM{l}", [128, 16]); din(f"vecs{l}", [128, NV_T])
        din(f"w_c{l}", [D, 512]); din(f"w_out{l}", [D, D]); din(f"w_q{l}", [D, 512]); din(f"w_kv{l}", [D, D]); din(f"w_o{l}", [512, D])
    if stop != "AG":
        din("w_gate0", [1, D, DFF]); din("w_up0", [1, D, DFF]); din("w_down0", [1, DFF, D])
        din("w_gate1", [8, D, DFF]); din("w_up1", [8, D, DFF]); din("w_down1", [8, DFF, D]); din("router_w", [D, 8])
    io["out"] = nc.dram_tensor("out", [NT, D], F32, kind="ExternalOutput").ap()
    for l in range(2):
        io[f"h_own{l}"] = [dint(f"h_own{l}_{a}", [256, NT]) for a in range(4)]
        io[f"hT_all{l}"] = [dint(f"hT_all{l}_{a}", [4 * 256, NT]) for a in range(4)]
        dint(f"tail{l}", [D, 32]); dint(f"tails{l}", [4 * D, 32])
        dint(f"catm{l}", [64, SEQ]); dint(f"catm_all{l}", [256, SEQ])
        io[f"catf{l}"] = [dint(f"catf{l}_{h}", [64, SEQ]) for h in range(2)]
        io[f"catf_all{l}"] = [dint(f"catf_all{l}_{h}", [256, SEQ]) for h in range(2)]
    with _ES() as st:
        c = Ctx(nc, st)
        c.setup()
        kb = c.kb
        xT = c.sb("xT", [128, 8, NT], F32)
        phase_P(c, {"x_tok": io["x_tok"], "vecsP": io["vecsP"], "h_next": io["h_own0"], "tail_next": io["tail0"]}, xT)
        for l in range(2):
            last = (l == 1)
            for a in range(4):
                kb.collective("AllGather", RG, io[f"h_own{l}"][a], io[f"hT_all{l}"][a], reads=["h_next_d"], writes=["hT_all_d"])
            kb.collective("AllGather", RG, io[f"tail{l}"], io[f"tails{l}"], reads=["tail_next_d"], writes=["tails_d"])
            c.barrier()
            if stop == "AG":
                for a in range(4):
                    for jj in range(4):
                        kb.dma("sp", io["dbg1"][jj * D + a * 256:jj * D + (a + 1) * 256, :], io[f"hT_all{l}"][a][jj * 256:(jj + 1) * 256, :], reads=["hT_all_d"])
                break
            ioM = {"hT_all": io[f"hT_all{l}"], "w_ml": io[f"w_ml{l}"], "w_fx": io[f"w_fx{l}"],
                   "vecsM": io[f"vecsM{l}"], "catm_out": io[f"catm{l}"], "catf_out": io[f"catf{l}"]}
            c.sfx = f"_{l}"
            phase_M_mlstm(c, ioM)
            phase_M_fox(c, ioM)
            kb.collective("AllGather", RG, io[f"catm{l}"], io[f"catm_all{l}"], writes=["catm_all_d"])
            for h in range(2):
                kb.collective("AllGather", RG, io[f"catf{l}"][h], io[f"catf_all{l}"][h], writes=["catf_all_d"])
            c.barrier()
            if stop == "M":
                for h in range(2):
                    for g in range(4):
                        kb.dma("sp", io["dbg2"][g * 128 + h * 64:g * 128 + (h + 1) * 64, :], io[f"catf_all{l}"][h][g * 64:(g + 1) * 64, :], reads=["catf_all_d"])
                break
            ioT = {"h_own": io[f"h_own{l}"], "tails": io[f"tails{l}"], "sel": io["sel"], "catm_all": io[f"catm_all{l}"], "catf_all": io[f"catf_all{l}"],
                   "mem": io["mem"], "vecs": io[f"vecs{l}"], "w_c": io[f"w_c{l}"], "w_out": io[f"w_out{l}"], "w_q": io[f"w_q{l}"],
                   "w_kv": io[f"w_kv{l}"], "w_o": io[f"w_o{l}"], "w_gate": io[f"w_gate{l}"], "w_up": io[f"w_up{l}"], "w_down": io[f"w_down{l}"]}
            if last:
                ioT["router_w"] = io["router_w"]; ioT["out"] = io["out"]
            else:
                ioT["h_next"] = io["h_own1"]; ioT["tail_next"] = io["tail1"]
            phase_T(c, ioT, 8 if last else 1, last, xT)
            if stop == "T":
                kb.dma("sp", io["dbg3"].rearrange("(k p) n -> p k n", p=128), xT[:], reads=[("xT", k) for k in range(8)])
                break
        c.barrier()
        kb.flush()
    return nc


def kernel_unfused(**inp):
    return _kernel_unfused(**inp)


_kernel_unfused = kernel


def kernel(**inp):
    inp = {k: np.asarray(v) for k, v in inp.items()}
    cores = list(range(8))
    x = inp["x"].astype(np.float32, copy=False)
    fm = lambda w: np.ascontiguousarray(np.asarray(w, np.float32).reshape(-1, 128).T)
    nc = _get("fused", lambda: build_fused(_STOP))
    shared = {"vecsP": fm(inp["norm_mix_w"][0]),
              "w_gate0": inp["ffn_w_gate"], "w_up0": inp["ffn_w_up"], "w_down0": inp["ffn_w_down"],
              "w_gate1": inp["moe_w_gate"][0], "w_up1": inp["moe_w_up"][0], "w_down1": inp["moe_w_down"][0],
              "router_w": inp["router_w"][0]}
    for l in range(2):
        shared[f"vecs{l}"] = vecs_T(inp, l, l == 1)
        shared[f"w_c{l}"] = np.ascontiguousarray(inp["w_in"][l][:, 1032:1544])
        shared[f"w_out{l}"] = inp["w_out"][l]; shared[f"w_q{l}"] = inp["xattn_w_q"][l]
        shared[f"w_kv{l}"] = inp["xattn_w_kv"][l]; shared[f"w_o{l}"] = inp["xattn_w_o"][l]
    perg = []
    for g in range(4):
        d = {}
        for l in range(2):
            m = inputs_M(inp, l, g)
            d[f"w_ml{l}"] = m["w_ml"]; d[f"w_fx{l}"] = m["w_fx"]; d[f"vecsM{l}"] = m["vecsM"]
        perg.append(d)
    maps = []
    for cid in cores:
        b, j = cid // 4, cid % 4
        m = dict(shared)
        m.update(perg[j])
        m["x_tok"] = np.ascontiguousarray(x[b, j * NT:(j + 1) * NT])
        m["mem"] = np.ascontiguousarray(inp["mem"][b], dtype=np.float32)
        sel = np.zeros((128, 8), np.float32)
        sel[:, j] = 1.0
        if j > 0:
            sel[:, 4 + j - 1] = 1.0
        m["sel"] = sel
        maps.append(m)
    if _STOP == "AG":
        maps = [{k: m[k] for k in ("x_tok", "vecsP")} for m in maps]
    res = run_bass_kernel_spmd(nc, maps, core_ids=cores).results
    if _STOP:
        return res
    out = np.zeros((2, SEQ, D), np.float32)
    for cid in cores:
        b, j = cid // 4, cid % 4
        out[b, j * NT:(j + 1) * NT] = res[cid]["out"]
    return out
```

```python
import numpy as np
import concourse.bass as bass
import concourse.mybir as mybir
from concourse.bass_utils import run_bass_kernel_spmd

F32 = mybir.dt.float32
BF16 = mybir.dt.bfloat16
AF = mybir.ActivationFunctionType
ALU = mybir.AluOpType
AX = mybir.AxisListType

ENGS = ("pe", "act", "dve", "pool", "sp")


class KB:
    SEM_ROLL = 2000

    def __init__(self, nc, n_dma_sems=32):
        self.nc = nc
        self.q = {e: [] for e in ENGS}
        self.cnt = {e: 0 for e in ENGS}
        self.cur_sem = {}
        self.sem_pool = []
        self.waited = {e: {} for e in ENGS}
        self.last_w = {}
        self.reads = {}
        self.n_dma_sems = n_dma_sems
        self.dma_sems = []
        self.dma_cnt = []
        self.dma_rr = 0
        self.dma_rr_sw = 0
        self._stack = None
        self.n_inst = 0

    def _new_sem(self, name):
        s = self._stack.enter_context(self.nc.semaphore(name))
        return s

    def start(self, stack):
        self._stack = stack
        for e in ENGS:
            self.cur_sem[e] = self._new_sem(f"p_{e}_0")
        for i in range(self.n_dma_sems):
            self.dma_sems.append(self._new_sem(f"dma{i}"))
            self.dma_cnt.append(0)

    def _wait(self, eng, ev):
        if ev is None:
            return
        if len(ev) == 3 and ev[2] == "pe" and eng == "pe":
            return
        sem, val = ev[0], ev[1]
        w = self.waited[eng]
        if w.get(id(sem), (None, 0))[1] >= val:
            return
        w[id(sem)] = (sem, val)
        self.q[eng].append(lambda e, sem=sem, val=val: e.wait_ge(sem, val))

    def _wait_w(self, eng, k):
        lw = self.last_w.get(k)
        if isinstance(lw, list):
            for ev in lw:
                self._wait(eng, ev)
        else:
            self._wait(eng, lw)

    def _deps(self, eng, reads, writes):
        for k in reads:
            self._wait_w(eng, k)
        for k in writes:
            self._wait_w(eng, k)
            for ev in self.reads.get(k, ()):
                self._wait(eng, ev)

    def _commit(self, ev, reads, writes, is_dma=False):
        for k in writes:
            lw = self.last_w.get(k)
            if is_dma and isinstance(lw, list) and not self.reads.get(k):
                lw.append(ev)
            else:
                self.last_w[k] = [ev] if is_dma else ev
            self.reads[k] = []
        for k in reads:
            self.reads.setdefault(k, []).append(ev)

    def op(self, eng, fn, reads=(), writes=()):
        self._deps(eng, reads, writes)
        if self.cnt[eng] >= self.SEM_ROLL:
            self.cur_sem[eng] = self._new_sem(f"p_{eng}_{self.n_inst}")
            self.cnt[eng] = 0
        self.cnt[eng] += 1
        sem = self.cur_sem[eng]
        ev = (sem, self.cnt[eng], eng)
        self.q[eng].append(lambda e, sem=sem: fn(e).then_inc(sem, 1))
        self._commit(ev, reads, writes)
        self.n_inst += 1
        return ev

    def dma(self, eng, out, in_, reads=(), writes=(), **kw):
        self._deps(eng, reads, writes)
        half = self.n_dma_sems // 2
        if eng == "pool":
            i = half + self.dma_rr_sw
            self.dma_rr_sw = (self.dma_rr_sw + 1) % (self.n_dma_sems - half)
        else:
            i = self.dma_rr
            self.dma_rr = (self.dma_rr + 1) % half
        sem = self.dma_sems[i]
        if self.dma_cnt[i] >= 2048:
            self.dma_sems[i] = self._new_sem(f"dma{i}_{self.n_inst}")
            self.dma_cnt[i] = 0
            sem = self.dma_sems[i]
        if self.dma_cnt[i] > 0:
            self._wait(eng, (sem, self.dma_cnt[i]))
        self.dma_cnt[i] += 16
        ev = (sem, self.dma_cnt[i])
        self.q[eng].append(lambda e, sem=sem: e.dma_start(out=out, in_=in_, **kw).then_inc(sem, 16))
        self._commit(ev, reads, writes, is_dma=True)
        self.n_inst += 1
        return ev

    def collective(self, kind, rg, in_ap, out_ap, reads=(), writes=()):
        eng = "pool"
        self._deps(eng, reads, writes)
        sem = self._new_sem(f"cc_{self.n_inst}")
        ev = (sem, 1)
        self.q[eng].append(lambda e: e.collective_compute(kind, ALU.bypass, replica_groups=rg, ins=[in_ap.opt()],
                                                          outs=[out_ap.opt()]).then_inc(sem, 1))
        self._commit(ev, reads, writes)
        self.n_inst += 1
        self.cc_events = getattr(self, "cc_events", []) + [ev]
        return ev

    def wait_all(self, eng, evs):
        for ev in evs:
            self._wait(eng, ev)

    def flush(self):
        nc = self.nc
        q = self.q
        with nc.Block() as block:
            @block.tensor
            def _(e):
                for f in q["pe"]:
                    f(e)

            @block.scalar
            def _(e):
                for f in q["act"]:
                    f(e)

            @block.vector
            def _(e):
                for f in q["dve"]:
                    f(e)

            @block.gpsimd
            def _(e):
                for f in q["pool"]:
                    f(e)

            @block.sync
            def _(e):
                for f in q["sp"]:
                    f(e)
        self.q = {e: [] for e in ENGS}


D = 1024
NT = 2048
TT = 512
NTT = NT // TT
DFF = 2816
NF = DFF // 128
SEQ = 8192
EPS = 1e-6
NV_T = 100


class Ctx:
    def __init__(self, nc, st):
        self.nc = nc
        self.st = st
        self.kb = KB(nc)
        self.kb.start(st)
        self.ps_rr = 0
        self.uid = 0

    def sb(self, name, shape, dt, st=None):
        self.uid += 1
        return (st or self.st).enter_context(self.nc.sbuf_tensor(f"{name}_u{self.uid}", shape, dt))

    def barrier(self):
        kb = self.kb
        evs = []
        for e in ENGS:
            if kb.cnt[e] > 0:
                evs.append((kb.cur_sem[e], kb.cnt[e]))
        for i, s in enumerate(kb.dma_sems):
            if kb.dma_cnt[i] > 0:
                evs.append((s, kb.dma_cnt[i]))
        evs += getattr(kb, "cc_events", [])
        kb.cc_events = []
        for e in ENGS:
            for ev in evs:
                kb._wait(e, ev)
        kb.last_w = {}
        kb.reads = {}

    def setup(self):
        nc, kb = self.nc, self.kb
        self.ident_f = self.sb("ident_f", [128, 128], F32)
        self.ident_b = self.sb("ident_b", [128, 128], BF16)
        self.ones_b = self.sb("ones_b", [128, 128], BF16)
        self.ones_f = self.sb("ones_f", [128, 128], F32)
        self.psb = [self.st.enter_context(nc.psum_tensor(f"psb{i}", [128, 512], F32)) for i in range(8)]
        idf, idb, ob, of = self.ident_f, self.ident_b, self.ones_b, self.ones_f
        kb.op("pool", lambda e: e.memset(idf[:], 0.0), writes=["ident_f"])
        kb.op("pool", lambda e: e.affine_select(out=idf[:], in_=idf[:], pattern=[[-1, 128]],
                                                compare_op=ALU.not_equal, fill=1.0, base=0,
                                                channel_multiplier=1),
              reads=["ident_f"], writes=["ident_f"])
        kb.op("pool", lambda e: e.tensor_copy(out=idb[:], in_=idf[:]), reads=["ident_f"], writes=["ident_b"])
        kb.op("pool", lambda e: e.memset(ob[:], 1.0), writes=["ones_b"])
        kb.op("pool", lambda e: e.memset(of[:], 1.0), writes=["ones_f"])

    def ps(self):
        rot = getattr(self, "rot", None) or list(range(8))
        i = rot[self.ps_rr % len(rot)]
        self.ps_rr += 1
        return self.psb[i], f"psb{i}"


def h_store(c, dst, hT, c0, n, reads, writes=()):
    if isinstance(dst, list):
        for a, d in enumerate(dst):
            c.kb.dma("sp", d[:, c0:c0 + n].rearrange("(k p) n -> p k n", p=128), hT[:, 2 * a:2 * a + 2, 0:n], reads=reads, writes=writes)
    else:
        c.kb.dma("sp", dst[:, c0:c0 + n].rearrange("(k p) n -> p k n", p=128), hT[:, :, 0:n], reads=reads, writes=writes)


def h_load(c, src, hT, c0, n, writes, j=None):
    if isinstance(src, list):
        for a, d in enumerate(src):
            v = d if j is None else d.rearrange("(j r) n -> j r n", j=4)[j]
            c.kb.dma("sp", hT[:, 2 * a:2 * a + 2, 0:n], v[:, c0:c0 + n].rearrange("(k p) n -> p k n", p=128), writes=writes)
    else:
        v = src if j is None else src[j]
        c.kb.dma("sp", hT[:, :, 0:n], v[:, c0:c0 + n].rearrange("(k p) n -> p k n", p=128), writes=writes)


def mm(c, out, lhsT, rhs, start, stop, reads, writes):
    return c.kb.op("pe", lambda e: e.matmul(out, lhsT=lhsT, rhs=rhs, start=start, stop=stop),
                   reads=reads, writes=writes)


def rmsnorm_tile(c, xT, xkey, t0, n, wv, tmp, out_bf, okey, out_f=None):
    kb = c.kb
    sq, rstd = tmp["sq"], tmp["rstd"]
    for k in range(8):
        kb.op("act", lambda e, k=k: e.activation(out=sq[:, k, 0:n], in_=xT[:, k, t0:t0 + n], func=AF.Square),
              reads=[(xkey, k)], writes=[("sq", k)])
    p, pk = c.ps()
    for k in range(8):
        mm(c, p[:, 0:n], c.ones_b[:], sq[:, k, 0:n], k == 0, k == 7, ["ones_b", ("sq", k)], [pk])
    kb.op("act", lambda e: e.activation(out=rstd[:, 0:n], in_=p[:, 0:n], func=AF.Sqrt, scale=1.0 / D, bias=tmp["eps"][:, 0:1]),
          reads=[pk, "eps"], writes=["rstd"])
    kb.op("dve", lambda e: e.reciprocal(out=rstd[:, 0:n], in_=rstd[:, 0:n]), reads=["rstd"], writes=["rstd"])
    for k in range(8):
        kb.op("dve", lambda e, k=k: e.scalar_tensor_tensor(out=out_bf[:, k, 0:n], in0=xT[:, k, t0:t0 + n],
                                                           scalar=wv[:, k:k + 1], in1=rstd[:, 0:n],
                                                           op0=ALU.mult, op1=ALU.mult),
              reads=[(xkey, k), "rstd", "vecs"], writes=[(okey, k)])
        if out_f is not None:
            kb.op("dve", lambda e, k=k: e.scalar_tensor_tensor(out=out_f[:, k, 0:n], in0=xT[:, k, t0:t0 + n],
                                                                scalar=wv[:, k:k + 1], in1=rstd[:, 0:n],
                                                                op0=ALU.mult, op1=ALU.mult),
                  reads=[(xkey, k), "rstd", "vecs"], writes=[(okey + "_f", k)])


def phase_T(c, io, E, last, xT):
    nc, kb = c.nc, c.kb
    from contextlib import ExitStack
    vec_st = ExitStack()
    vecs = c.sb("vecsT", [128, NV_T], F32, vec_st)
    eps_t = c.sb("eps_t", [128, 1], F32, vec_st)
    sq = c.sb("sq", [128, 8, TT], BF16, vec_st)
    rstd = c.sb("rstd", [128, TT], F32, vec_st)
    tmp = {"sq": sq, "rstd": rstd, "eps": eps_t}
    kb.dma("sp", vecs[:], io["vecs"], writes=["vecs"])
    kb.op("pool", lambda e: e.memset(eps_t[:], EPS), writes=["eps"])
    V_XA, V_MEM, V_FFN, V_NEXT, V_CB, V_LNW, V_LNB, V_CW = 0, 8, 16, 24, 32, 34, 36, 38

    with ExitStack() as s1:
        hT = c.sb("hT", [128, 8, TT], BF16, s1)
        gluT = c.sb("gluT", [128, 2, 32 + NT], BF16, s1)
        hcT = c.sb("hcT", [128, 2, NT], BF16, s1)
        wc = c.sb("wc", [128, 8, 512], BF16, s1)
        dg = c.sb("dg", [128, 62, 128], BF16, s1)
        sig = c.sb("sig", [128, 2, TT], F32, s1)
        hcv = c.sb("hcv", [128, 2, TT], F32, s1)
        hsq = c.sb("hsq", [128, 2, TT], F32, s1)
        mean = c.sb("mean", [128, TT], F32, s1)
        var = c.sb("var", [128, TT], F32, s1)
        wo_m = c.sb("wo_m", [64, 4, D], BF16, s1)
        wo_c = c.sb("wo_c", [128, 2, D], BF16, s1)
        wo_f = c.sb("wo_f", [128, 4, D], BF16, s1)
        mT = c.sb("mT", [64, 4, TT], BF16, s1)
        fT = c.sb("fT", [128, 4, TT], BF16, s1)
        if "sel" in io:
            halo4 = c.sb("halo4", [128, 4, 8, 32], BF16, s1)
            selt = c.sb("selt", [128, 8], F32, s1)
            m4 = [c.sb(f"m4_{i}", [64, 4, TT], BF16, s1) for i in range(2)]
            f4 = [c.sb(f"f4_{i}", [128, 4, TT], BF16, s1) for i in range(2)]
            kb.dma("sp", selt[:], io["sel"], writes=["selt"])
        kb.dma("pool", wc[:], io["w_c"].rearrange("(k p) n -> p k n", p=128), writes=["wc"])
        kb.dma("pool", wo_m[:], io["w_out"][0:256, :].rearrange("(g p) n -> p g n", p=64), writes=["wo_m"])
        kb.dma("pool", wo_c[:], io["w_out"][256:512, :].rearrange("(g p) n -> p g n", p=128), writes=["wo_c"])
        kb.dma("pool", wo_f[:], io["w_out"][512:1024, :].rearrange("(g p) n -> p g n", p=128), writes=["wo_f"])
        for j in range(31):
            for ch in range(2):
                kb.op("dve", lambda e, j=j, ch=ch: e.tensor_scalar(
                    out=dg[:, j * 2 + ch, :], in0=c.ident_b[:], scalar1=vecs[:, V_CW + j * 2 + ch:V_CW + j * 2 + ch + 1],
                    scalar2=None, op0=ALU.mult), reads=["ident_b", "vecs"], writes=[("dg", j, ch)])
        tiles = [("halo", 0, 32)] + [("own", t * TT, TT) for t in range(NTT)]
        for kind, t0, n in tiles:
            if kind == "halo" and "sel" in io:
                tl = io["tails"].rearrange("(j k p) n -> j p k n", j=4, p=128)
                for jj in range(4):
                    kb.dma("sp", halo4[:, jj, :, :], tl[jj], writes=[("halo4", jj)])
                kb.op("dve", lambda e: e.tensor_scalar(out=hT[:, :, 0:32], in0=halo4[:, 0, :, :], scalar1=selt[:, 4:5], scalar2=None, op0=ALU.mult),
                      reads=[("halo4", 0), "selt"], writes=[("hT", k) for k in range(8)])
                for jj in range(1, 4):
                    kb.op("dve", lambda e, jj=jj: e.scalar_tensor_tensor(out=hT[:, :, 0:32], in0=halo4[:, jj, :, :], scalar=selt[:, 4 + jj:5 + jj], in1=hT[:, :, 0:32],
                                                                         op0=ALU.mult, op1=ALU.add),
                          reads=[("halo4", jj), "selt"] + [("hT", k) for k in range(8)], writes=[("hT", k) for k in range(8)])
                g0 = 0
            elif kind == "halo":
                kb.dma("sp", hT[:, :, 0:n], io["h_halo"].rearrange("(k p) n -> p k n", p=128),
                       writes=[("hT", k) for k in range(8)])
                g0 = 0
            else:
                h_load(c, io["h_own"], hT, t0, n, [("hT", k) for k in range(8)])
                g0 = 32 + t0
            for ch in range(2):
                pa, pak = c.ps()
                pg, pgk = c.ps()
                for k in range(8):
                    mm(c, pa[:, 0:n], wc[:, k, ch * 128:(ch + 1) * 128], hT[:, k, 0:n], k == 0, k == 7,
                       ["wc", ("hT", k)], [pak])
                for k in range(8):
                    mm(c, pg[:, 0:n], wc[:, k, 256 + ch * 128:256 + (ch + 1) * 128], hT[:, k, 0:n], k == 0, k == 7,
                       ["wc", ("hT", k)], [pgk])
                kb.op("act", lambda e, ch=ch, pg=pg, n=n: e.activation(out=sig[:, ch, 0:n], in_=pg[:, 0:n], func=AF.Sigmoid),
                      reads=[pgk], writes=[("sig", ch)])
                kb.op("dve", lambda e, ch=ch, pa=pa, n=n, g0=g0: e.tensor_tensor(
                    out=gluT[:, ch, g0:g0 + n], in0=pa[:, 0:n], in1=sig[:, ch, 0:n], op=ALU.mult),
                    reads=[pak, ("sig", ch)], writes=[("glu", ch, g0 // TT), ("glu", ch, (g0 + n - 1) // TT)])
        for t in range(NTT):
            t0 = t * TT
            gk = lambda ch: [("glu", ch, (32 + t0 - 30) // TT), ("glu", ch, (32 + t0 + TT - 1) // TT)]
            for ch in range(2):
                p, pk = c.ps()
                for j in range(31):
                    o = 32 + t0 - 30 + j
                    mm(c, p[:, :], dg[:, j * 2 + ch, :], gluT[:, ch, o:o + TT], j == 0, j == 30,
                       [("dg", j, ch)] + gk(ch), [pk])
                kb.op("act", lambda e, ch=ch, p=p: e.activation(out=hcv[:, ch, :], in_=p[:, :], func=AF.Identity,
                                                                bias=vecs[:, V_CB + ch:V_CB + ch + 1]),
                      reads=[pk, "vecs"], writes=[("hcv", ch)])
                kb.op("act", lambda e, ch=ch: e.activation(out=hsq[:, ch, :], in_=hcv[:, ch, :], func=AF.Square),
                      reads=[("hcv", ch)], writes=[("hsq", ch)])
            p1, p1k = c.ps()
            p2, p2k = c.ps()
            for ch in range(2):
                mm(c, p1[:, :], c.ones_f[:], hcv[:, ch, :], ch == 0, ch == 1, ["ones_f", ("hcv", ch)], [p1k])
            for ch in range(2):
                mm(c, p2[:, :], c.ones_f[:], hsq[:, ch, :], ch == 0, ch == 1, ["ones_f", ("hsq", ch)], [p2k])
            kb.op("dve", lambda e, p1=p1: e.tensor_scalar(out=mean[:], in0=p1[:, :], scalar1=1.0 / 256, scalar2=None, op0=ALU.mult),
                  reads=[p1k], writes=["mean"])
            kb.op("dve", lambda e: e.tensor_tensor(out=var[:], in0=mean[:], in1=mean[:], op=ALU.mult),
                  reads=["mean"], writes=["var"])
            kb.op("dve", lambda e, p2=p2: e.scalar_tensor_tensor(out=var[:], in0=p2[:, :], scalar=1.0 / 256, in1=var[:],
                                                                 op0=ALU.mult, op1=ALU.subtract),
                  reads=[p2k, "var"], writes=["var"])
            kb.op("act", lambda e: e.activation(out=var[:], in_=var[:], func=AF.Sqrt, bias=eps_t[:, 0:1]),
                  reads=["var", "eps"], writes=["var"])
            kb.op("dve", lambda e: e.reciprocal(out=var[:], in_=var[:]), reads=["var"], writes=["var"])
            for ch in range(2):
                kb.op("dve", lambda e, ch=ch: e.tensor_tensor(out=hcv[:, ch, :], in0=hcv[:, ch, :], in1=mean[:], op=ALU.subtract),
                      reads=[("hcv", ch), "mean"], writes=[("hcv", ch)])
                kb.op("dve", lambda e, ch=ch: e.tensor_tensor(out=hcv[:, ch, :], in0=hcv[:, ch, :], in1=var[:], op=ALU.mult),
                      reads=[("hcv", ch), "var"], writes=[("hcv", ch)])
                kb.op("dve", lambda e, ch=ch: e.tensor_scalar(out=hcv[:, ch, :], in0=hcv[:, ch, :],
                                                              scalar1=vecs[:, V_LNW + ch:V_LNW + ch + 1],
                                                              scalar2=vecs[:, V_LNB + ch:V_LNB + ch + 1],
                                                              op0=ALU.mult, op1=ALU.add),
                      reads=[("hcv", ch), "vecs"], writes=[("hcv", ch)])
                kb.op("act", lambda e, ch=ch, t0=t0: e.activation(out=hcT[:, ch, t0:t0 + TT], in_=hcv[:, ch, :], func=AF.Silu),
                      reads=[("hcv", ch)], writes=[("hcT", ch, t)])
            if "sel" in io:
                cm = io["catm_all"].rearrange("(g p) n -> p g n", p=64)
                cf = [a.rearrange("(g p) n -> p g n", p=64) for a in io["catf_all"]]
                for jj in range(4):
                    for dst, stg, src, nm, npart in ((mT, m4, cm, "m4", 64), (fT, f4, cf, "f4", 128)):
                        dk = "mT" if nm == "m4" else "fT"
                        sg = stg[jj % 2]
                        sk = f"{nm}_{jj % 2}"
                        if nm == "m4":
                            kb.dma("sp", sg[:], src[:, :, jj * NT + t0:jj * NT + t0 + TT], writes=[sk])
                        else:
                            for hh in range(2):
                                kb.dma("sp", sg[hh * 64:(hh + 1) * 64, :, :], src[hh][:, :, jj * NT + t0:jj * NT + t0 + TT], writes=[sk])
                        if jj == 0:
                            kb.op("dve", lambda e, dst=dst, sg=sg, npart=npart: e.tensor_scalar(out=dst[:], in0=sg[:], scalar1=selt[0:npart, 0:1], scalar2=None, op0=ALU.mult),
                                  reads=[sk, "selt"], writes=[dk])
                        else:
                            kb.op("dve", lambda e, dst=dst, sg=sg, jj=jj, npart=npart: e.scalar_tensor_tensor(out=dst[:], in0=sg[:], scalar=selt[0:npart, jj:jj + 1], in1=dst[:],
                                                                                                          op0=ALU.mult, op1=ALU.add),
                                  reads=[sk, "selt", dk], writes=[dk])
            else:
                kb.dma("sp", mT[:], io["catm"][:, :, t0:t0 + TT].rearrange("g p n -> p g n"), writes=["mT"])
                kb.dma("sp", fT[:], io["catf"][:, :, t0:t0 + TT].rearrange("g p n -> p g n"), writes=["fT"])
            for d in range(8):
                p, pk = c.ps()
                ds = slice(d * 128, (d + 1) * 128)
                for g in range(4):
                    mm(c, p[:, :], wo_m[:, g, ds], mT[:, g, :], g == 0, False, ["wo_m", "mT"], [pk])
                for ch in range(2):
                    mm(c, p[:, :], wo_c[:, ch, ds], hcT[:, ch, t0:t0 + TT], False, False, ["wo_c", ("hcT", ch, t)], [pk])
                for g in range(4):
                    mm(c, p[:, :], wo_f[:, g, ds], fT[:, g, :], False, g == 3, ["wo_f", "fT"], [pk])
                kb.op("dve", lambda e, d=d, p=p, t0=t0: e.tensor_tensor(out=xT[:, d, t0:t0 + TT], in0=xT[:, d, t0:t0 + TT],
                                                                        in1=p[:, :], op=ALU.add),
                      reads=[pk, ("xT", d)], writes=[("xT", d)])
    c.barrier()
    if io.get("dbg_stage") == 1:
        vec_st.close()
        return

    with ExitStack() as s2:
        hT = c.sb("hT", [128, 8, TT], BF16, s2)
        memt = c.sb("memt", [128, 2, D], F32, s2)
        mss = c.sb("mss", [128, 2], F32, s2)
        junk = c.sb("junk", [128, D], F32, s2)
        memnT = c.sb("memnT", [128, 8, 256], BF16, s2)
        wkv = c.sb("wkv", [128, 8, D], BF16, s2)
        wq = c.sb("wq", [128, 8, 512], BF16, s2)
        wo = c.sb("wo", [128, 4, D], BF16, s2)
        kT = c.sb("kT", [128, 4, 256], BF16, s2)
        Vt = c.sb("Vt", [128, 2, 512], BF16, s2)
        qT = c.sb("qT", [128, 4, TT], BF16, s2)
        pT = c.sb("pT", [128, 8, TT], BF16, s2)
        rden = c.sb("rden", [128, TT], F32, s2)
        oT = c.sb("oT", [128, 4, TT], BF16, s2)
        kb.dma("sp", memt[:], io["mem"].rearrange("(t p) d -> p t d", p=128), writes=["memt"])
        kb.dma("pool", wkv[:], io["w_kv"].rearrange("(k p) n -> p k n", p=128), writes=["wkv"])
        kb.dma("pool", wq[:], io["w_q"].rearrange("(k p) n -> p k n", p=128), writes=["wq"])
        kb.dma("pool", wo[:], io["w_o"].rearrange("(k p) n -> p k n", p=128), writes=["wo"])
        for mt in range(2):
            kb.op("act", lambda e, mt=mt: e.activation(out=junk[:], in_=memt[:, mt, :], func=AF.Square,
                                                       accum_out=mss[:, mt:mt + 1]),
                  reads=["memt"], writes=["junk", ("mss", mt)])
            kb.op("act", lambda e, mt=mt: e.activation(out=mss[:, mt:mt + 1], in_=mss[:, mt:mt + 1], func=AF.Sqrt,
                                                       scale=1.0 / D, bias=eps_t[:, 0:1]),
                  reads=[("mss", mt), "eps"], writes=[("mss", mt)])
            kb.op("dve", lambda e, mt=mt: e.reciprocal(out=mss[:, mt:mt + 1], in_=mss[:, mt:mt + 1]),
                  reads=[("mss", mt)], writes=[("mss", mt)])
            kb.op("dve", lambda e, mt=mt: e.tensor_scalar(out=memt[:, mt, :], in0=memt[:, mt, :], scalar1=mss[:, mt:mt + 1],
                                                          scalar2=None, op0=ALU.mult),
                  reads=["memt", ("mss", mt)], writes=["memt"])
        for k in range(8):
            p, pk = c.ps()
            for mt in range(2):
                kb.op("pe", lambda e, k=k, mt=mt, p=p: e.transpose(out=p[:, mt * 128:(mt + 1) * 128],
                                                                   in_=memt[:, mt, k * 128:(k + 1) * 128], identity=c.ident_f[:]),
                      reads=["memt", "ident_f"], writes=[pk])
            kb.op("dve", lambda e, k=k, p=p: e.tensor_scalar(out=memnT[:, k, :], in0=p[:, 0:256],
                                                             scalar1=vecs[:, V_MEM + k:V_MEM + k + 1], scalar2=None, op0=ALU.mult),
                  reads=[pk, "vecs"], writes=[("memnT", k)])
        for h in range(4):
            p, pk = c.ps()
            for k in range(8):
                mm(c, p[:, 0:256], wkv[:, k, h * 128:(h + 1) * 128], memnT[:, k, :], k == 0, k == 7, ["wkv", ("memnT", k)], [pk])
            kb.op("act", lambda e, h=h, p=p: e.activation(out=kT[:, h, :], in_=p[:, 0:256], func=AF.Copy),
                  reads=[pk], writes=[("kT", h)])
        for mt in range(2):
            p, pk = c.ps()
            for k in range(8):
                mm(c, p[:, :], memnT[:, k, mt * 128:(mt + 1) * 128], wkv[:, k, 512:1024], k == 0, k == 7, ["wkv", ("memnT", k)], [pk])
            kb.op("act", lambda e, mt=mt, p=p: e.activation(out=Vt[:, mt, :], in_=p[:, :], func=AF.Copy),
                  reads=[pk], writes=[("Vt", mt)])
        sc = 128 ** -0.5
        for t in range(NTT):
            t0 = t * TT
            rmsnorm_tile(c, xT, "xT", t0, TT, vecs[:, V_XA:V_XA + 8], tmp, hT, "hT")
            for h in range(4):
                p, pk = c.ps()
                for k in range(8):
                    mm(c, p[:, :], wq[:, k, h * 128:(h + 1) * 128], hT[:, k, :], k == 0, k == 7, ["wq", ("hT", k)], [pk])
                kb.op("act", lambda e, h=h, p=p: e.activation(out=qT[:, h, :], in_=p[:, :], func=AF.Copy),
                      reads=[pk], writes=[("qT", h)])
            for h in range(4):
                for mt in range(2):
                    p, pk = c.ps()
                    mm(c, p[:, :], kT[:, h, mt * 128:(mt + 1) * 128], qT[:, h, :], True, True, [("kT", h), ("qT", h)], [pk])
                    kb.op("act", lambda e, h=h, mt=mt, p=p: e.activation(out=pT[:, h * 2 + mt, :], in_=p[:, :], func=AF.Exp, scale=sc),
                          reads=[pk], writes=[("pT", h, mt)])
                pd, pdk = c.ps()
                for mt in range(2):
                    mm(c, pd[:, :], c.ones_b[:], pT[:, h * 2 + mt, :], mt == 0, mt == 1, ["ones_b", ("pT", h, mt)], [pdk])
                kb.op("dve", lambda e, pd=pd: e.reciprocal(out=rden[:], in_=pd[:, :]), reads=[pdk], writes=["rden"])
                po, pok = c.ps()
                for mt in range(2):
                    mm(c, po[:, :], Vt[:, mt, h * 128:(h + 1) * 128], pT[:, h * 2 + mt, :], mt == 0, mt == 1,
                       [("Vt", mt), ("pT", h, mt)], [pok])
                kb.op("dve", lambda e, h=h, po=po: e.tensor_tensor(out=oT[:, h, :], in0=po[:, :], in1=rden[:], op=ALU.mult),
                      reads=[pok, "rden"], writes=[("oT", h)])
            for d in range(8):
                p, pk = c.ps()
                for h in range(4):
                    mm(c, p[:, :], wo[:, h, d * 128:(d + 1) * 128], oT[:, h, :], h == 0, h == 3, ["wo", ("oT", h)], [pk])
                kb.op("dve", lambda e, d=d, p=p, t0=t0: e.tensor_tensor(out=xT[:, d, t0:t0 + TT], in0=xT[:, d, t0:t0 + TT],
                                                                        in1=p[:, :], op=ALU.add),
                      reads=[pk, ("xT", d)], writes=[("xT", d)])
    c.barrier()
    if io.get("dbg_stage") == 2:
        vec_st.close()
        return

    with ExitStack() as s3:
        hTall = c.sb("hTall", [128, 8, NT], BF16, s3)
        actT = c.sb("actT", [128, 8, NT], BF16, s3)
        wgu = [c.sb(f"wgu{i}", [128, 8, 256], BF16, s3) for i in range(3)]
        wdr = [c.sb(f"wdr{i}", [128, D], BF16, s3) for i in range(11)]
        sil = [c.sb(f"sil{i}", [128, TT], BF16, s3) for i in range(2)]
        if E > 1:
            wr = c.sb("wr", [128, 8, 8], F32, s3)
            lg = c.sb("lg", [128, 4, 8], F32, s3)
            top8 = c.sb("top8", [128, 4, 8], F32, s3)
            gts = c.sb("gts", [128, 16, 8], F32, s3)
            gsc = c.sb("gsc", [128, 4, 4], F32, s3)
            dgate = c.sb("dgate", [128, 128], F32, s3)
            gB = [c.sb(f"gB{i}", [128, NT], BF16, s3) for i in range(2)]
            ytmps = [c.sb(f"ytmp{i}", [128, TT], F32, s3) for i in range(2)]
            kb.dma("sp", wr[:], io["router_w"].rearrange("(k p) n -> p k n", p=128), writes=["wr"])
        with ExitStack() as s3a:
            hF = c.sb("hF", [128, 8, TT], F32, s3a) if E > 1 else None
            for t in range(NTT):
                t0 = t * TT
                rmsnorm_tile(c, xT, "xT", t0, TT, vecs[:, V_FFN:V_FFN + 8], tmp, hTall[:, :, t0:t0 + TT], f"hA{t}", out_f=hF)
                if E > 1:
                    for s in range(4):
                        p, pk = c.ps()
                        for k in range(8):
                            mm(c, p[:, 0:8], hF[:, k, s * 128:(s + 1) * 128], wr[:, k, :], k == 0, k == 7, [(f"hA{t}_f", k), "wr"], [pk])
                        kb.op("dve", lambda e, s=s, p=p: e.tensor_copy(out=lg[:, s, :], in_=p[:, 0:8]), reads=[pk], writes=[("lg", s)])
                        kb.op("dve", lambda e, s=s: e.max(out=top8[:, s, :], in_=lg[:, s, :]), reads=[("lg", s)], writes=[("top8", s)])
                        kb.op("dve", lambda e, s=s: e.tensor_scalar(out=gsc[:, s, 0:1], in0=top8[:, s, 0:1], scalar1=-1.0, scalar2=None, op0=ALU.mult),
                              reads=[("top8", s)], writes=[("gsc", s, 0)])
                        kb.op("act", lambda e, s=s: e.activation(out=gsc[:, s, 1:2], in_=top8[:, s, 1:2], func=AF.Exp, bias=gsc[:, s, 0:1]),
                              reads=[("top8", s), ("gsc", s, 0)], writes=[("gsc", s, 1)])
                        kb.op("dve", lambda e, s=s: e.tensor_scalar(out=gsc[:, s, 1:2], in0=gsc[:, s, 1:2], scalar1=1.0, scalar2=None, op0=ALU.add),
                              reads=[("gsc", s, 1)], writes=[("gsc", s, 1)])
                        kb.op("dve", lambda e, s=s: e.reciprocal(out=gsc[:, s, 1:2], in_=gsc[:, s, 1:2]),
                              reads=[("gsc", s, 1)], writes=[("gsc", s, 1)])
                        gi = t * 4 + s
                        kb.op("act", lambda e, s=s, gi=gi: e.activation(out=gts[:, gi, :], in_=lg[:, s, :], func=AF.Exp, bias=gsc[:, s, 0:1]),
                              reads=[("lg", s), ("gsc", s, 0)], writes=[("gts", gi)])
                        kb.op("dve", lambda e, s=s: e.tensor_scalar(out=lg[:, s, :], in0=lg[:, s, :], scalar1=top8[:, s, 1:2], scalar2=None, op0=ALU.is_ge),
                              reads=[("lg", s), ("top8", s)], writes=[("lg", s)])
                        kb.op("dve", lambda e, s=s, gi=gi: e.scalar_tensor_tensor(out=gts[:, gi, :], in0=gts[:, gi, :], scalar=gsc[:, s, 1:2], in1=lg[:, s, :],
                                                                                  op0=ALU.mult, op1=ALU.mult),
                              reads=[("gts", gi), ("gsc", s, 1), ("lg", s)], writes=[("gts", gi)])
        hkeys = lambda t: [(f"hA{t}", k) for k in range(8)]
        groups = [(0, 8), (8, 16), (16, 22)]
        wgu_i = 0
        wd_i = 0
        sil_i = 0
        for ex in range(E):
            if E > 1:
                gb = gB[ex % 2]
                gbk = f"gB{ex % 2}"
                for gi in range(16):
                    kb.op("dve", lambda e, gi=gi, ex=ex: e.tensor_scalar(out=dgate[:], in0=c.ident_f[:], scalar1=gts[:, gi, ex:ex + 1],
                                                                         scalar2=None, op0=ALU.mult),
                          reads=["ident_f", ("gts", gi)], writes=["dgate"])
                    p, pk = c.ps()
                    mm(c, p[:, 0:128], c.ones_f[:], dgate[:], True, True, ["ones_f", "dgate"], [pk])
                    kb.op("act", lambda e, gi=gi, p=p, gb=gb: e.activation(out=gb[:, gi * 128:(gi + 1) * 128], in_=p[:, 0:128], func=AF.Copy),
                          reads=[pk], writes=[(gbk, gi // 4)])
            for fa, fb in groups:
                nfg = fb - fa
                for f0 in range(fa, fb, 2):
                    nf = min(2, fb - f0)
                    sg, su = wgu[wgu_i % 3], wgu[(wgu_i + 1) % 3]
                    sgk, suk = f"wgu{wgu_i % 3}", f"wgu{(wgu_i + 1) % 3}"
                    wgu_i += 2
                    kb.dma("pool", sg[:, :, 0:nf * 128], io["w_gate"][ex][:, f0 * 128:(f0 + nf) * 128].rearrange("(k p) n -> p k n", p=128), writes=[sgk])
                    kb.dma("pool", su[:, :, 0:nf * 128], io["w_up"][ex][:, f0 * 128:(f0 + nf) * 128].rearrange("(k p) n -> p k n", p=128), writes=[suk])
                    for fi in range(nf):
                        fl = f0 + fi - fa
                        for t in range(NTT):
                            ts_ = slice(t * TT, (t + 1) * TT)
                            pg, pgk = c.ps()
                            pu, puk = c.ps()
                            for k in range(8):
                                mm(c, pg[:, :], sg[:, k, fi * 128:(fi + 1) * 128], hTall[:, k, ts_], k == 0, k == 7, [sgk, (f"hA{t}", k)], [pgk])
                            for k in range(8):
                                mm(c, pu[:, :], su[:, k, fi * 128:(fi + 1) * 128], hTall[:, k, ts_], k == 0, k == 7, [suk, (f"hA{t}", k)], [puk])
                            sl = sil[sil_i % 2]
                            slk = f"sil{sil_i % 2}"
                            sil_i += 1
                            kb.op("act", lambda e, pg=pg, sl=sl: e.activation(out=sl[:], in_=pg[:, :], func=AF.Silu), reads=[pgk], writes=[slk])
                            kb.op("dve", lambda e, pu=pu, sl=sl, fl=fl, ts_=ts_: e.tensor_tensor(out=actT[:, fl, ts_], in0=pu[:, :], in1=sl[:], op=ALU.mult),
                                  reads=[puk, slk], writes=[("actT", fl, t)])
                slots = []
                for fl in range(nfg):
                    f = fa + fl
                    wd = wdr[wd_i % 11]
                    wdk = f"wdr{wd_i % 11}"
                    wd_i += 1
                    kb.dma("pool", wd[:], io["w_down"][ex][f * 128:(f + 1) * 128, :], writes=[wdk])
                    slots.append((wd, wdk))
                for t in range(NTT):
                    ts_ = slice(t * TT, (t + 1) * TT)
                    for d in range(8):
                        p, pk = c.ps()
                        for fl in range(nfg):
                            wd, wdk = slots[fl]
                            mm(c, p[:, :], wd[:, d * 128:(d + 1) * 128], actT[:, fl, ts_], fl == 0, fl == nfg - 1, [wdk, ("actT", fl, t)], [pk])
                        if E > 1:
                            ytmp = ytmps[d % 2]
                            ytk = f"ytmp{d % 2}"
                            kb.op("dve", lambda e, p=p, gb=gb, ytmp=ytmp, ts_=ts_: e.tensor_tensor(out=ytmp[:], in0=p[:, :], in1=gb[:, ts_], op=ALU.mult),
                                  reads=[pk, (gbk, t)], writes=[ytk])
                            kb.op("dve", lambda e, d=d, ts_=ts_, ytmp=ytmp: e.tensor_tensor(out=xT[:, d, ts_], in0=xT[:, d, ts_], in1=ytmp[:], op=ALU.add),
                                  reads=[ytk, ("xT", d)], writes=[("xT", d)])
                        else:
                            kb.op("dve", lambda e, d=d, p=p, ts_=ts_: e.tensor_tensor(out=xT[:, d, ts_], in0=xT[:, d, ts_], in1=p[:, :], op=ALU.add),
                                  reads=[pk, ("xT", d)], writes=[("xT", d)])
    c.barrier()

    with ExitStack() as s4:
        hT = c.sb("hT", [128, 8, TT], BF16, s4)
        if not last:
            for t in range(NTT):
                t0 = t * TT
                rmsnorm_tile(c, xT, "xT", t0, TT, vecs[:, V_NEXT:V_NEXT + 8], tmp, hT, "hT")
                h_store(c, io["h_next"], hT, t0, TT, [("hT", k) for k in range(8)], ["h_next_d"])
                if t == NTT - 1 and "tail_next" in io:
                    kb.dma("sp", io["tail_next"].rearrange("(k p) n -> p k n", p=128), hT[:, :, TT - 32:TT],
                           reads=[("hT", k) for k in range(8)], writes=["tail_next_d"])
        else:
            hF2 = c.sb("hF2", [128, 8, TT], F32, s4)
            otm = c.sb("otm", [128, 4, D], F32, s4)
            for t in range(NTT):
                t0 = t * TT
                rmsnorm_tile(c, xT, "xT", t0, TT, vecs[:, V_NEXT:V_NEXT + 8], tmp, hT, "hT", out_f=hF2)
                for s in range(4):
                    for kk in range(2):
                        p, pk = c.ps()
                        for k4 in range(4):
                            k = kk * 4 + k4
                            kb.op("pe", lambda e, k=k, k4=k4, s=s, p=p: e.transpose(out=p[:, k4 * 128:(k4 + 1) * 128],
                                                                                    in_=hF2[:, k, s * 128:(s + 1) * 128], identity=c.ident_f[:]),
                                  reads=[("hT_f", k), "ident_f"], writes=[pk])
                        kb.op("act", lambda e, s=s, kk=kk, p=p: e.activation(out=otm[:, s, kk * 512:(kk + 1) * 512], in_=p[:, :], func=AF.Copy),
                              reads=[pk], writes=[("otm", s)])
                kb.dma("sp", io["out"][t0:t0 + TT, :].rearrange("(s p) d -> p s d", p=128), otm[:],
                       reads=[("otm", s) for s in range(4)])
    c.barrier()
    vec_st.close()


from contextlib import ExitStack as _ES
import ml_dtypes as _mld

NPBF = _mld.bfloat16


def build_T(E, last, dbg_stage=0):
    nc = bass.Bass("TRN2", target_bir_lowering=False)
    io = {}

    def din(name, shape, dt=F32):
        io[name] = nc.dram_tensor(name, shape, dt, kind="ExternalInput").ap()

    def dout(name, shape, dt=F32):
        io[name] = nc.dram_tensor(name, shape, dt, kind="ExternalOutput").ap()

    din("xT_in", [D, NT]); din("h_own", [D, NT], BF16); din("h_halo", [D, 32], BF16)
    din("catm", [4, 64, NT], BF16); din("catf", [4, 128, NT], BF16); din("mem", [256, D])
    din("vecs", [128, NV_T]); din("w_c", [D, 512]); din("w_out", [D, D]); din("w_q", [D, 512])
    din("w_kv", [D, D]); din("w_o", [512, D])
    din("w_gate", [E, D, DFF]); din("w_up", [E, D, DFF]); din("w_down", [E, DFF, D])
    if E > 1:
        din("router_w", [D, 8])
    if last:
        dout("out", [NT, D])
    else:
        dout("xT_out", [D, NT]); dout("h_next", [D, NT], BF16)
    io["dbg_stage"] = dbg_stage
    with _ES() as st:
        c = Ctx(nc, st)
        c.setup()
        xT = c.sb("xT", [128, 8, NT], F32)
        c.kb.dma("sp", xT[:], io["xT_in"].rearrange("(k p) n -> p k n", p=128), writes=[("xT", k) for k in range(8)])
        phase_T(c, io, E, last and not dbg_stage, xT)
        evs = []
        if not last or dbg_stage:
            key = "xT_out" if not last else "out"
            if last:
                io["xT_dbg"] = None
            evs.append(c.kb.dma("sp", io["xT_out"].rearrange("(k p) n -> p k n", p=128), xT[:],
                                reads=[("xT", k) for k in range(8)]))
        c.barrier()
        c.kb.flush()
    return nc


def vecs_T(inp, l, last):
    v = np.zeros((128, NV_T), np.float32)
    fm = lambda w: np.asarray(w, np.float32).reshape(-1, 128).T
    v[:, 0:8] = fm(inp["norm_xattn_w"][l]); v[:, 8:16] = fm(inp["norm_mem_w"][l]); v[:, 16:24] = fm(inp["norm_ffn_w"][l])
    v[:, 24:32] = fm(inp["norm_final_w"]) if last else fm(inp["norm_mix_w"][l + 1])
    v[:, 32:34] = fm(inp["conf_conv_b"][l]); v[:, 34:36] = fm(inp["conf_ln_w"][l]); v[:, 36:38] = fm(inp["conf_ln_b"][l])
    cw = np.asarray(inp["conf_conv_w"][l], np.float32)
    for j in range(31):
        v[:, 38 + 2 * j:40 + 2 * j] = fm(cw[j])
    return v


NCH = SEQ // 64
NKT = SEQ // 128
NQT = SEQ // TT
GRP = 4


def AP3(t, off, dims):
    return bass.AP(t[:].tensor, off, [list(d) for d in dims])


def log_sigmoid_tile(c, x, out, tmp1, tmp2, bias_ap, keys):
    kb = c.kb
    kx, ko, k1, k2 = keys
    kb.op("dve", lambda e: e.tensor_scalar(out=x, in0=x, scalar1=bias_ap, scalar2=None, op0=ALU.add), reads=[kx, "vecsM"], writes=[kx])
    kb.op("dve", lambda e: e.tensor_scalar(out=tmp1, in0=x, scalar1=-1.0, scalar2=None, op0=ALU.mult), reads=[kx], writes=[k1])
    kb.op("dve", lambda e: e.tensor_tensor(out=tmp1, in0=tmp1, in1=x, op=ALU.max), reads=[kx, k1], writes=[k1])
    kb.op("act", lambda e: e.activation(out=tmp1, in_=tmp1, func=AF.Exp, scale=-1.0), reads=[k1], writes=[k1])
    kb.op("dve", lambda e: e.tensor_scalar(out=tmp1, in0=tmp1, scalar1=1.0, scalar2=None, op0=ALU.add), reads=[k1], writes=[k1])
    kb.op("act", lambda e: e.activation(out=tmp1, in_=tmp1, func=AF.Ln), reads=[k1], writes=[k1])
    kb.op("dve", lambda e: e.tensor_scalar(out=tmp2, in0=x, scalar1=0.0, scalar2=None, op0=ALU.min), reads=[kx], writes=[k2])
    kb.op("dve", lambda e: e.tensor_tensor(out=out, in0=tmp2, in1=tmp1, op=ALU.subtract), reads=[k1, k2], writes=[ko])


def phase_M_mlstm(c, io):
    nc, kb = c.nc, c.kb
    from contextlib import ExitStack
    with ExitStack() as s0:
        vm = c.sb("vecsM_sb", [128, 16], F32, s0)
        wml = c.sb("wml", [128, 8, 258], BF16, s0)
        qT = c.sb("m_qT", [64, SEQ], BF16, s0)
        kT = c.sb("m_kT", [64, SEQ], BF16, s0)
        Vaug = c.sb("m_Vaug", [64, NCH, 65], BF16, s0)
        og = c.sb("m_og", [64, SEQ], BF16, s0)
        iC = c.sb("m_iC", [128, 64], F32, s0)
        fC = c.sb("m_fC", [128, 64], F32, s0)
        eps_t = c.sb("m_eps", [128, 1], F32, s0)
        kb.dma("sp", vm[:], io["vecsM"], writes=["vecsM"])
        kb.dma("pool", wml[:], io["w_ml"].rearrange("(k p) n -> p k n", p=128), writes=["wml"])
        kb.op("pool", lambda e: e.memset(Vaug[:, :, 64:65], 1.0), writes=["Vaug1"])
        kb.op("pool", lambda e: e.memset(eps_t[:], EPS), writes=["m_eps"])
        with ExitStack() as s1:
            hTb = [c.sb(f"m_hT{i}", [128, 8, TT], BF16, s1) for i in range(2)]
            zq = c.sb("m_zq", [64, TT + 3], F32, s1)
            zk = c.sb("m_zk", [64, TT + 3], F32, s1)
            cacc = [c.sb(f"m_cacc{i}", [64, TT], F32, s1) for i in range(2)]
            vt = c.sb("m_vt", [64, TT], F32, s1)
            rows = [c.sb(f"m_rows{i}", [2, TT], F32, s1) for i in range(2)]
            kb.op("pool", lambda e: e.memset(zq[:, 0:3], 0.0), writes=["zq"])
            kb.op("pool", lambda e: e.memset(zk[:, 0:3], 0.0), writes=["zk"])
            for tt in range(NQT):
                j, off = tt // 4, (tt % 4) * TT
                hT = hTb[tt % 2]
                hk = f"m_hT{tt % 2}"
                tok = slice(tt * TT, (tt + 1) * TT)
                h_load(c, io["hT_all"], hT, off, TT, [hk], j=j)
                for nm, z, col0, vc, dst in (("q", zq, 0, 0, qT), ("k", zk, 64, 5, kT)):
                    p, pk = c.ps()
                    for k in range(8):
                        mm(c, p[0:64, :], wml[:, k, col0:col0 + 64], hT[:, k, :], k == 0, k == 7, ["wml", hk], [pk])
                    zkey = "z" + nm
                    kb.op("act", lambda e, z=z, p=p: e.activation(out=z[:, 3:TT + 3], in_=p[0:64, :], func=AF.Copy), reads=[pk], writes=[zkey])
                    ca = cacc[0 if nm == "q" else 1]
                    ck = "cacc" + nm
                    kb.op("dve", lambda e, z=z, ca=ca, vc=vc: e.tensor_scalar(out=ca[:], in0=z[:, 0:TT], scalar1=vm[0:64, vc:vc + 1], scalar2=vm[0:64, vc + 4:vc + 5],
                                                                              op0=ALU.mult, op1=ALU.add), reads=[zkey, "vecsM"], writes=[ck])
                    for jj in range(1, 4):
                        kb.op("dve", lambda e, z=z, ca=ca, vc=vc, jj=jj: e.scalar_tensor_tensor(out=ca[:], in0=z[:, jj:jj + TT], scalar=vm[0:64, vc + jj:vc + jj + 1], in1=ca[:],
                                                                                                 op0=ALU.mult, op1=ALU.add), reads=[zkey, ck, "vecsM"], writes=[ck])
                    kb.op("act", lambda e, ca=ca, dst=dst, tok=tok: e.activation(out=dst[:, tok], in_=ca[:], func=AF.Silu), reads=[ck], writes=[("m_" + nm + "T", tt)])
                    kb.op("dve", lambda e, z=z: e.tensor_copy(out=z[:, 0:3], in_=z[:, TT:TT + 3]), reads=[zkey], writes=[zkey])
                p, pk = c.ps()
                for k in range(8):
                    mm(c, p[0:64, :], wml[:, k, 128:192], hT[:, k, :], k == 0, k == 7, ["wml", hk], [pk])
                kb.op("act", lambda e, p=p: e.activation(out=vt[:], in_=p[0:64, :], func=AF.Copy), reads=[pk], writes=["m_vt"])
                p2, p2k = c.ps()
                for ci in range(8):
                    kb.op("pe", lambda e, ci=ci, p2=p2: e.transpose(out=p2[0:64, ci * 64:(ci + 1) * 64], in_=vt[:, ci * 64:(ci + 1) * 64], identity=c.ident_f[0:64, 0:64]),
                          reads=["m_vt", "ident_f"], writes=[p2k])
                kb.op("dve", lambda e, p2=p2, tt=tt: e.tensor_copy(out=Vaug[:, tt * 8:(tt + 1) * 8, 0:64], in_=p2[0:64, :].rearrange("p (c d) -> p c d", d=64)),
                      reads=[p2k], writes=[("Vaug", tt)])
                p, pk = c.ps()
                for k in range(8):
                    mm(c, p[0:64, :], wml[:, k, 192:256], hT[:, k, :], k == 0, k == 7, ["wml", hk], [pk])
                kb.op("act", lambda e, p=p, tok=tok: e.activation(out=og[:, tok], in_=p[0:64, :], func=AF.Sigmoid), reads=[pk], writes=[("og", tt)])
                p, pk = c.ps()
                for k in range(8):
                    mm(c, p[0:2, :], wml[:, k, 256:258], hT[:, k, :], k == 0, k == 7, ["wml", hk], [pk])
                rw = rows[tt % 2]
                rk = f"m_rows{tt % 2}"
                kb.op("act", lambda e, p=p, rw=rw: e.activation(out=rw[:], in_=p[0:2, :], func=AF.Copy), reads=[pk], writes=[rk])
                kb.dma("sp", iC[tt * 8:(tt + 1) * 8, :], AP3(rw, 0, [[TT, 1], [64, 8], [1, 64]]), reads=[rk], writes=["iC"])
                kb.dma("sp", fC[tt * 8:(tt + 1) * 8, :], AP3(rw, TT, [[TT, 1], [64, 8], [1, 64]]), reads=[rk], writes=["fC"])
        c.barrier()
        Uall = c.sb("m_Uall", [64, 65, NCH], F32, s0)
        wgT = c.sb("m_wgT", [64, NCH], F32, s0)
        flT = c.sb("m_flT", [64, NCH], F32, s0)
        dB = c.sb("m_dB", [64, NCH], F32, s0)
        dB0 = c.sb("m_dB0", [64, NCH], F32, s0)
        with ExitStack() as s2:
            t1 = c.sb("g_t1", [128, 64], F32, s2)
            t2 = c.sb("g_t2", [128, 64], F32, s2)
            lf = c.sb("g_lf", [128, 64], F32, s2)
            bb = c.sb("g_b", [128, 64], F32, s2)
            aa = c.sb("g_a", [128, 64], F32, s2)
            AA = c.sb("g_A", [128, 64], F32, s2)
            MM = c.sb("g_M", [128, 64], F32, s2)
            wg = c.sb("g_wg", [128, 64], F32, s2)
            fl = c.sb("g_fl", [128, 64], F32, s2)
            on = c.sb("g_on", [128, 64], F32, s2)
            r1 = c.sb("g_r1", [1, 128], F32, s2)
            r2 = c.sb("g_r2", [1, 128], F32, s2)
            r3 = c.sb("g_r3", [1, 128], F32, s2)
            r4 = c.sb("g_r4", [1, 128], F32, s2)
            mcol = c.sb("g_mcol", [128, 1], F32, s2)
            nM63 = c.sb("g_nM63", [128, 1], F32, s2)
            dec = c.sb("g_dec", [128, 1], F32, s2)
            dgd = c.sb("g_dgd", [128, 128], F32, s2)
            Xb = [c.sb(f"g_X{i}", [128, 8, 64], F32, s2) for i in range(2)]
            kw32 = [c.sb(f"g_kw32{i}", [64, TT], F32, s2) for i in range(2)]
            kwTok = c.sb("g_kwTok", [64, NCH, 64], BF16, s2)
            log_sigmoid_tile(c, fC[:], lf[:], t1[:], t2[:], vm[:, 12:13], ("fC", "g_lf", "g_t1", "g_t2"))
            kb.op("dve", lambda e: e.tensor_scalar(out=iC[:], in0=iC[:], scalar1=vm[:, 11:12], scalar2=None, op0=ALU.add), reads=["iC", "vecsM"], writes=["iC"])
            kb.op("pool", lambda e: e.memset(on[:], 1.0), writes=["g_on"])
            kb.op("dve", lambda e: e.tensor_tensor_scan(out=bb[:], data0=on[:], data1=lf[:], initial=0.0, op0=ALU.mult, op1=ALU.add),
                  reads=["g_on", "g_lf"], writes=["g_b"])
            kb.op("dve", lambda e: e.tensor_tensor(out=aa[:], in0=iC[:], in1=bb[:], op=ALU.subtract), reads=["iC", "g_b"], writes=["g_a"])
            kb.op("dve", lambda e: e.tensor_tensor_scan(out=AA[:], data0=aa[:], data1=aa[:], initial=-1e30, op0=ALU.max, op1=ALU.max),
                  reads=["g_a"], writes=["g_A"])
            p, pk = c.ps()
            kb.op("pe", lambda e, p=p: e.transpose(out=p[0:1, 0:128], in_=AA[:, 63:64], identity=c.ident_f[:]), reads=["g_A", "ident_f"], writes=[pk])
            kb.op("pe", lambda e, p=p: e.transpose(out=p[0:1, 128:256], in_=bb[:, 63:64], identity=c.ident_f[:]), reads=["g_b", "ident_f"], writes=[pk])
            kb.op("dve", lambda e, p=p: e.tensor_copy(out=r1[:], in_=p[0:1, 0:128]), reads=[pk], writes=["g_r1"])
            kb.op("dve", lambda e, p=p: e.tensor_copy(out=r2[:], in_=p[0:1, 128:256]), reads=[pk], writes=["g_r2"])
            kb.op("dve", lambda e: e.tensor_tensor_scan(out=r3[:], data0=r1[:], data1=r2[:], initial=0.0, op0=ALU.max, op1=ALU.add),
                  reads=["g_r1", "g_r2"], writes=["g_r3"])
            kb.op("pool", lambda e: e.memset(r4[:, 0:1], 0.0), writes=["g_r4a"])
            kb.op("dve", lambda e: e.tensor_copy(out=r4[:, 1:128], in_=r3[:, 0:127]), reads=["g_r3"], writes=["g_r4b"])
            p, pk = c.ps()
            kb.op("pe", lambda e, p=p: e.transpose(out=p[:, 0:1], in_=r4[:], identity=c.ident_f[0:1, 0:1]), reads=["g_r4a", "g_r4b", "ident_f"], writes=[pk])
            kb.op("dve", lambda e, p=p: e.tensor_copy(out=mcol[:], in_=p[:, 0:1]), reads=[pk], writes=["g_mcol"])
            kb.op("dve", lambda e: e.tensor_scalar(out=MM[:], in0=AA[:], scalar1=mcol[:, 0:1], scalar2=None, op0=ALU.max), reads=["g_A", "g_mcol"], writes=["g_M"])
            kb.op("dve", lambda e: e.tensor_scalar(out=nM63[:], in0=MM[:, 63:64], scalar1=-1.0, scalar2=None, op0=ALU.mult), reads=["g_M"], writes=["g_nM63"])
            kb.op("act", lambda e: e.activation(out=wg[:], in_=aa[:], func=AF.Exp, bias=nM63[:, 0:1]), reads=["g_a", "g_nM63"], writes=["g_wg"])
            kb.op("act", lambda e: e.activation(out=dec[:], in_=mcol[:], func=AF.Exp, bias=nM63[:, 0:1]), reads=["g_mcol", "g_nM63"], writes=["g_dec"])
            kb.op("act", lambda e: e.activation(out=fl[:], in_=bb[:], func=AF.Exp, scale=-1.0, bias=nM63[:, 0:1]), reads=["g_b", "g_nM63"], writes=["g_fl"])
            p, pk = c.ps()
            kb.op("pe", lambda e, p=p: e.transpose(out=p[0:64, 0:128], in_=wg[:], identity=c.ident_f[:]), reads=["g_wg", "ident_f"], writes=[pk])
            kb.op("pe", lambda e, p=p: e.transpose(out=p[0:64, 128:256], in_=fl[:], identity=c.ident_f[:]), reads=["g_fl", "ident_f"], writes=[pk])
            kb.op("dve", lambda e, p=p: e.tensor_copy(out=wgT[:], in_=p[0:64, 0:128]), reads=[pk], writes=["m_wgT"])
            kb.op("dve", lambda e, p=p: e.tensor_copy(out=flT[:], in_=p[0:64, 128:256]), reads=[pk], writes=["m_flT"])
            kb.op("dve", lambda e: e.tensor_scalar(out=dgd[:], in0=c.ident_f[:], scalar1=dec[:, 0:1], scalar2=None, op0=ALU.mult), reads=["ident_f", "g_dec"], writes=["g_dgd"])
            p, pk = c.ps()
            mm(c, p[0:64, 0:128], c.ones_f[:, 0:64], dgd[:], True, True, ["ones_f", "g_dgd"], [pk])
            kb.op("dve", lambda e, p=p: e.tensor_copy(out=dB[:], in_=p[0:64, 0:128]), reads=[pk], writes=["m_dB"])
            kb.op("dve", lambda e, p=p: e.tensor_copy(out=dB0[:], in_=p[0:64, 0:128]), reads=[pk], writes=["m_dB0"])
            kb.op("pool", lambda e: e.memset(dB0[:, 0:1], 0.0), reads=["m_dB0"], writes=["m_dB0"])
            for tt in range(NQT):
                tok = slice(tt * TT, (tt + 1) * TT)
                X = Xb[tt % 2]
                Xk = f"g_X{tt % 2}"
                kb.op("dve", lambda e, X=X, tt=tt: e.tensor_tensor(out=X[:], in0=AP3(c.ident_f, 8 * tt, [[128, 128], [1, 8], [0, 64]]),
                                                                   in1=AP3(wg, 0, [[64, 128], [0, 8], [1, 64]]), op=ALU.mult),
                      reads=["ident_f", "g_wg"], writes=[Xk])
                p, pk = c.ps()
                mm(c, p[0:64, :], c.ones_f[:, 0:64], X[:].rearrange("p c s -> p (c s)"), True, True, ["ones_f", Xk], [pk])
                k32 = kw32[tt % 2]
                k32k = f"g_kw32{tt % 2}"
                kb.op("dve", lambda e, p=p, k32=k32, tok=tok: e.scalar_tensor_tensor(out=k32[:], in0=kT[:, tok], scalar=0.125, in1=p[0:64, :], op0=ALU.mult, op1=ALU.mult),
                      reads=[pk, ("m_kT", tt)], writes=[k32k])
                kb.op("act", lambda e, k32=k32, tok=tok: e.activation(out=kT[:, tok], in_=k32[:], func=AF.Copy), reads=[k32k], writes=[("m_kT", tt)])
                p2, p2k = c.ps()
                for ci in range(8):
                    kb.op("pe", lambda e, ci=ci, p2=p2, k32=k32: e.transpose(out=p2[0:64, ci * 64:(ci + 1) * 64], in_=k32[:, ci * 64:(ci + 1) * 64], identity=c.ident_f[0:64, 0:64]),
                          reads=[k32k, "ident_f"], writes=[p2k])
                kb.op("dve", lambda e, p2=p2, tt=tt: e.tensor_copy(out=kwTok[:, tt * 8:(tt + 1) * 8, :], in_=p2[0:64, :].rearrange("p (c d) -> p c d", d=64)),
                      reads=[p2k], writes=[("kwTok", tt)])
            for g in range(NCH // GRP):
                p, pk = c.ps()
                for ci in range(GRP):
                    ch = g * GRP + ci
                    mm(c, p[0:64, ci * 65:(ci + 1) * 65], kwTok[:, ch, :], Vaug[:, ch, :], True, True, [("kwTok", ch // 8), ("Vaug", ch // 8), "Vaug1"], [pk])
                kb.op("dve", lambda e, p=p, g=g: e.tensor_copy(out=AP3(Uall, g * GRP, [[65 * NCH, 64], [1, GRP], [NCH, 65]]),
                                                               in_=p[0:64, 0:GRP * 65].rearrange("p (c d) -> p c d", d=65)),
                      reads=[pk], writes=["Uall"])
        c.barrier()
        Cn = Uall
        Eb = c.sb("m_E", [64, NCH, 65], BF16, s0)
        for dv in range(65):
            kb.op("dve", lambda e, dv=dv: e.tensor_tensor_scan(out=Cn[:, dv, :], data0=dB0[:], data1=Uall[:, dv, :], initial=0.0, op0=ALU.mult, op1=ALU.add),
                  reads=["m_dB0", "Uall"], writes=[("Cn", dv), "Uall"])
        kb.op("pool", lambda e: e.memset(Eb[:, 0:1, :], 0.0), writes=["E0"])
        kb.op("dve", lambda e: e.tensor_tensor(out=Eb[:, 1:NCH, :], in0=AP3(Cn, 0, [[65 * NCH, 64], [1, NCH - 1], [NCH, 65]]),
                                               in1=AP3(dB, 1, [[NCH, 64], [1, NCH - 1], [0, 65]]), op=ALU.mult),
              reads=[("Cn", dv) for dv in range(65)] + ["m_dB"], writes=["E"])
        with ExitStack() as s4:
            mask = c.sb("o_mask", [64, 64], F32, s4)
            sT = [c.sb(f"o_sT{i}", [64, GRP * 64], BF16, s4) for i in range(2)]
            den = c.sb("o_den", [64, GRP], F32, s4)
            hn = c.sb("o_hn", [64, GRP, 64], F32, s4)
            hsq = c.sb("o_hsq", [64, GRP, 64], F32, s4)
            ss = c.sb("o_ss", [64, GRP], F32, s4)
            cst = [c.sb(f"o_cst{i}", [64, GRP * 64], BF16, s4) for i in range(2)]
            kb.op("pool", lambda e: e.memset(mask[:], 1.0), writes=["o_mask"])
            kb.op("pool", lambda e: e.affine_select(out=mask[:], in_=mask[:], pattern=[[1, 64]], compare_op=ALU.is_ge, fill=0.0, base=0, channel_multiplier=-1),
                  reads=["o_mask"], writes=["o_mask"])
            pend = {}

            def a4_stage1(g):
                c0 = g * GRP
                p, pk = c.ps()
                for ci in range(GRP):
                    ch = c0 + ci
                    cs = slice(ch * 64, (ch + 1) * 64)
                    mm(c, p[0:64, ci * 64:(ci + 1) * 64], kT[:, cs], qT[:, cs], True, True, [("m_kT", ch // 8), ("m_qT", ch // 8)], [pk])
                st_ = sT[g % 2]
                stk = f"o_sT{g % 2}"
                kb.op("dve", lambda e, p=p, st_=st_: e.tensor_tensor(out=st_[:].rearrange("p (c t) -> p c t", t=64), in0=p[0:64, 0:GRP * 64].rearrange("p (c t) -> p c t", t=64),
                                                                     in1=AP3(mask, 0, [[64, 64], [0, GRP], [1, 64]]), op=ALU.mult),
                      reads=[pk, "o_mask"], writes=[stk])
                po, pok = c.ps()
                for ci in range(GRP):
                    ch = c0 + ci
                    cs = slice(ch * 64, (ch + 1) * 64)
                    mm(c, po[0:64, ci * 65:(ci + 1) * 65], st_[:, ci * 64:(ci + 1) * 64], Vaug[:, ch, :], True, False, [stk, ("Vaug", ch // 8), "Vaug1"], [pok])
                    mm(c, po[0:64, ci * 65:(ci + 1) * 65], qT[:, cs], Eb[:, ch, :], False, True, [("m_qT", ch // 8), "E", "E0"], [pok])
                pend[g] = (po, pok)

            def a4_stage2(g):
                c0 = g * GRP
                po, pok = pend.pop(g)
                po3 = po[0:64, 0:GRP * 65].rearrange("p (c d) -> p c d", d=65)
                den3 = den[:].rearrange("p (c o) -> p c o", o=1)
                kb.op("dve", lambda e, po3=po3, den3=den3: e.tensor_scalar(out=den3, in0=po3[:, :, 64:65], scalar1=-1.0, scalar2=None, op0=ALU.mult),
                      reads=[pok], writes=["o_den"])
                kb.op("dve", lambda e, po3=po3, den3=den3: e.tensor_tensor(out=den3, in0=po3[:, :, 64:65], in1=den3, op=ALU.max),
                      reads=[pok, "o_den"], writes=["o_den"])
                kb.op("dve", lambda e, c0=c0: e.tensor_tensor(out=den[:], in0=den[:], in1=flT[:, c0:c0 + GRP], op=ALU.max),
                      reads=["o_den", "m_flT"], writes=["o_den"])
                kb.op("dve", lambda e: e.reciprocal(out=den[:], in_=den[:]), reads=["o_den"], writes=["o_den"])
                kb.op("dve", lambda e, po3=po3: e.tensor_tensor(out=hn[:], in0=po3[:, :, 0:64], in1=AP3(den, 0, [[GRP, 64], [1, GRP], [0, 64]]), op=ALU.mult),
                      reads=[pok, "o_den"], writes=["o_hn"])
                kb.op("act", lambda e: e.activation(out=hsq[:], in_=hn[:], func=AF.Square), reads=["o_hn"], writes=["o_hsq"])
                kb.op("dve", lambda e: e.tensor_reduce(out=ss[:], in_=hsq[:], axis=AX.X, op=ALU.add), reads=["o_hsq"], writes=["o_ss"])
                kb.op("act", lambda e: e.activation(out=ss[:], in_=ss[:], func=AF.Sqrt, scale=1.0 / 64, bias=eps_t[0:64, 0:1]), reads=["o_ss", "m_eps"], writes=["o_ss"])
                kb.op("dve", lambda e: e.reciprocal(out=ss[:], in_=ss[:]), reads=["o_ss"], writes=["o_ss"])
                kb.op("dve", lambda e: e.tensor_tensor(out=hn[:], in0=hn[:], in1=AP3(ss, 0, [[GRP, 64], [1, GRP], [0, 64]]), op=ALU.mult),
                      reads=["o_hn", "o_ss"], writes=["o_hn"])
                pt, ptk = c.ps()
                for ci in range(GRP):
                    kb.op("pe", lambda e, ci=ci, pt=pt: e.transpose(out=pt[0:64, ci * 64:(ci + 1) * 64], in_=hn[:, ci, :], identity=c.ident_f[0:64, 0:64]),
                          reads=["o_hn", "ident_f"], writes=[ptk])
                cs_ = cst[g % 2]
                csk = f"o_cst{g % 2}"
                toks = slice(c0 * 64, (c0 + GRP) * 64)
                kb.op("dve", lambda e, pt=pt, cs_=cs_, toks=toks: e.scalar_tensor_tensor(out=cs_[:], in0=pt[0:64, 0:GRP * 64], scalar=vm[0:64, 10:11], in1=og[:, toks],
                                                                                       op0=ALU.mult, op1=ALU.mult),
                      reads=[ptk, "vecsM", ("og", (c0 * 64) // TT)], writes=[csk])
                kb.dma("sp", io["catm_out"][:, toks], cs_[:], reads=[csk])

            NG4 = NCH // GRP
            a4_stage1(0)
            for g in range(NG4):
                if g + 1 < NG4:
                    a4_stage1(g + 1)
                a4_stage2(g)
        c.barrier()


def phase_M_fox(c, io):
    nc, kb = c.nc, c.kb
    from contextlib import ExitStack
    NEG = -30000.0
    with ExitStack() as s0:
        vm = c.sb("vecsMf_sb", [128, 16], F32, s0)
        wfx = c.sb("wfx", [128, 8, 386], BF16, s0)
        fq = [c.sb(f"f_q{h}", [128, SEQ], BF16, s0) for h in range(2)]
        fkk = c.sb("f_kk", [128, SEQ], BF16, s0)
        kb.op("pool", lambda e: e.memset(fq[0][64:128, :], 0.0), writes=[("fqz", 0)])
        kb.op("pool", lambda e: e.memset(fq[1][0:64, :], 0.0), writes=[("fqz", 1)])
        fV = [c.sb(f"f_V{h}", [128, NKT, 65], BF16, s0) for h in range(2)]
        fC = [c.sb(f"f_C{h}", [64, 128], F32, s0) for h in range(2)]
        kb.dma("sp", vm[:], io["vecsM"], writes=["vecsM"])
        kb.dma("pool", wfx[:], io["w_fx"].rearrange("(k p) n -> p k n", p=128), writes=["wfx"])
        for h in range(2):
            kb.op("pool", lambda e, h=h: e.memset(fV[h][:, :, 64:65], 1.0), writes=[("fV1", h)])
        with ExitStack() as s1:
            hTb = [c.sb(f"f_hT{i}", [128, 8, TT], BF16, s1) for i in range(2)]
            vt = c.sb("f_vt", [128, TT], F32, s1)
            rows = [c.sb(f"f_rows{i}", [2, TT], F32, s1) for i in range(2)]
            for tt in range(NQT):
                j, off = tt // 4, (tt % 4) * TT
                hT = hTb[tt % 2]
                hk = f"f_hT{tt % 2}"
                tok = slice(tt * TT, (tt + 1) * TT)
                h_load(c, io["hT_all"], hT, off, TT, [hk], j=j)
                p, pk = c.ps()
                for k in range(8):
                    mm(c, p[:, :], wfx[:, k, 0:128], hT[:, k, :], k == 0, k == 7, ["wfx", hk], [pk])
                kb.op("act", lambda e, p=p, tok=tok: e.activation(out=fq[0][0:64, tok], in_=p[0:64, :], func=AF.Copy, scale=0.125), reads=[pk], writes=[("fq", 0, tt)])
                kb.op("act", lambda e, p=p, tok=tok: e.activation(out=fq[1][64:128, tok], in_=p[64:128, :], func=AF.Copy, scale=0.125), reads=[pk], writes=[("fq", 1, tt)])
                p, pk = c.ps()
                for k in range(8):
                    mm(c, p[:, :], wfx[:, k, 128:256], hT[:, k, :], k == 0, k == 7, ["wfx", hk], [pk])
                kb.op("act", lambda e, p=p, tok=tok: e.activation(out=fkk[:, tok], in_=p[:, :], func=AF.Copy), reads=[pk], writes=[("fk", tt)])
                p, pk = c.ps()
                for k in range(8):
                    mm(c, p[:, :], wfx[:, k, 256:384], hT[:, k, :], k == 0, k == 7, ["wfx", hk], [pk])
                kb.op("act", lambda e, p=p: e.activation(out=vt[:], in_=p[:, :], func=AF.Copy), reads=[pk], writes=["f_vt"])
                p2, p2k = c.ps()
                for ci in range(4):
                    kb.op("pe", lambda e, ci=ci, p2=p2: e.transpose(out=p2[:, ci * 128:(ci + 1) * 128], in_=vt[:, ci * 128:(ci + 1) * 128], identity=c.ident_f[:]),
                          reads=["f_vt", "ident_f"], writes=[p2k])
                for h in range(2):
                    kb.op("dve", lambda e, p2=p2, tt=tt, h=h: e.tensor_copy(out=fV[h][:, tt * 4:(tt + 1) * 4, 0:64],
                                                                         in_=p2[:, :].rearrange("p (c d) -> p c d", d=128)[:, :, h * 64:(h + 1) * 64]),
                          reads=[p2k], writes=[("fV", h, tt)])
                p, pk = c.ps()
                for k in range(8):
                    mm(c, p[0:2, :], wfx[:, k, 384:386], hT[:, k, :], k == 0, k == 7, ["wfx", hk], [pk])
                rw = rows[tt % 2]
                rk = f"f_rows{tt % 2}"
                kb.op("act", lambda e, p=p, rw=rw: e.activation(out=rw[:], in_=p[0:2, :], func=AF.Copy), reads=[pk], writes=[rk])
                for h in range(2):
                    kb.dma("sp", fC[h][tt * 4:(tt + 1) * 4, :], AP3(rw, h * TT, [[TT, 1], [128, 4], [1, 128]]), reads=[rk], writes=[("fC", h)])
        c.barrier()
        ckT = [c.sb(f"f_ckT{h}", [128, NKT], F32, s0) for h in range(2)]
        cC = [c.sb(f"f_cC{h}", [64, 128], F32, s0) for h in range(2)]
        negm = c.sb("f_negm", [128, 4, TT], F32, s0)
        Ls = c.sb("f_Ls", [64, 64], F32, s0)
        with ExitStack() as s2:
            t1 = c.sb("f_t1", [64, 128], F32, s2)
            t2 = c.sb("f_t2", [64, 128], F32, s2)
            lf = c.sb("f_lf", [64, 128], F32, s2)
            on = c.sb("f_on", [64, 128], F32, s2)
            pre = c.sb("f_pre", [64, 1], F32, s2)
            kb.op("pool", lambda e: e.memset(on[:], 1.0), writes=["f_on"])
            kb.op("pool", lambda e: e.memset(Ls[:], 1.0), writes=["f_Ls"])
            kb.op("pool", lambda e: e.affine_select(out=Ls[:], in_=Ls[:], pattern=[[1, 64]], compare_op=ALU.is_ge, fill=0.0, base=-1, channel_multiplier=-1),
                  reads=["f_Ls"], writes=["f_Ls"])
            for r in range(4):
                kb.op("pool", lambda e, r=r: e.memset(negm[:, r, :], 0.0), writes=[("negm", r)])
                kb.op("pool", lambda e, r=r: e.affine_select(out=negm[:, r, :], in_=negm[:, r, :], pattern=[[1, TT]], compare_op=ALU.is_ge, fill=NEG,
                                                             base=-128 * r, channel_multiplier=-1), reads=[("negm", r)], writes=[("negm", r)])
            for h in range(2):
                log_sigmoid_tile(c, fC[h][:], lf[:], t1[:], t2[:], vm[0:64, 13 + h:14 + h], (("fC", h), "f_lf", "f_t1", "f_t2"))
                kb.op("dve", lambda e, h=h: e.tensor_tensor_scan(out=cC[h][:], data0=on[:], data1=lf[:], initial=0.0, op0=ALU.mult, op1=ALU.add),
                      reads=["f_on", "f_lf"], writes=[("cC", h)])
                p, pk = c.ps()
                mm(c, p[0:64, 0:1], Ls[:], cC[h][:, 127:128], True, True, ["f_Ls", ("cC", h)], [pk])
                kb.op("dve", lambda e, p=p: e.tensor_copy(out=pre[:], in_=p[0:64, 0:1]), reads=[pk], writes=["f_pre"])
                kb.op("dve", lambda e, h=h: e.tensor_scalar(out=cC[h][:], in0=cC[h][:], scalar1=pre[:, 0:1], scalar2=None, op0=ALU.add), reads=[("cC", h), "f_pre"], writes=[("cC", h)])
                p, pk = c.ps()
                kb.op("pe", lambda e, p=p, h=h: e.transpose(out=p[:, 0:64], in_=cC[h][:], identity=c.ident_f[0:64, 0:64]), reads=[("cC", h), "ident_f"], writes=[pk])
                kb.op("dve", lambda e, p=p, h=h: e.tensor_scalar(out=ckT[h][:], in0=p[:, 0:64], scalar1=-1.0, scalar2=None, op0=ALU.mult), reads=[pk], writes=[("ckT", h)])
        c.barrier()
        with ExitStack() as s3:
            X = c.sb("f_X", [64, 4, 128], F32, s3)
            cqB = c.sb("f_cqB", [128, TT], F32, s3)
            cqD = c.sb("f_cqD", [128, 4, TT], F32, s3)
            NB = 5
            tmpb = [c.sb(f"f_tmp{i}", [128, TT], F32, s3) for i in range(NB)]
            pTb = [c.sb(f"f_pT{i}", [128, TT], BF16, s3) for i in range(NB)]
            osb = c.sb("f_osb", [65, TT], F32, s3)
            rden = c.sb("f_rden", [64, TT], F32, s3)
            outb = [c.sb(f"f_out{i}", [64, TT], BF16, s3) for i in range(2)]
            it = 0
            for h in range(2):
                for qi in range(NQT):
                    qs = slice(qi * TT, (qi + 1) * TT)
                    kb.op("dve", lambda e, h=h, qi=qi: e.tensor_tensor(out=X[:], in0=AP3(c.ident_f, 4 * qi, [[128, 64], [1, 4], [0, 128]]),
                                                                       in1=AP3(cC[h], 0, [[128, 64], [0, 4], [1, 128]]), op=ALU.mult),
                          reads=["ident_f", ("cC", h)], writes=["f_X"])
                    p, pk = c.ps()
                    mm(c, p[:, :], c.ones_f[0:64, :], X[:].rearrange("p r s -> p (r s)"), True, True, ["ones_f", "f_X"], [pk])
                    kb.op("act", lambda e, p=p: e.activation(out=cqB[:], in_=p[:, :], func=AF.Copy), reads=[pk], writes=["f_cqB"])
                    for r in range(4):
                        kb.op("pool", lambda e, r=r: e.tensor_tensor(out=cqD[:, r, :], in0=cqB[:], in1=negm[:, r, :], op=ALU.add),
                              reads=["f_cqB", ("negm", r)], writes=[("f_cqD", r)])
                    c.rot = list(range(6))
                    po, pok = c.psb[6 + qi % 2], f"psb{6 + qi % 2}"
                    nk = 4 * (qi + 1)
                    LA = 4
                    sbank = {}

                    def emit_S(kt):
                        ps_, psk = c.ps()
                        mm(c, ps_[:, :], fkk[:, kt * 128:(kt + 1) * 128], fq[h][:, qs], True, True, [("fk", kt // 4), ("fq", h, qi), ("fqz", h)], [psk])
                        sbank[kt] = (ps_, psk)

                    for kt in range(min(LA, nk)):
                        emit_S(kt)
                    for kt in range(nk):
                        ps_, psk = sbank.pop(kt)
                        tb = tmpb[it % NB]; tbk = f"f_tmp{it % NB}"
                        pb = pTb[it % NB]; pbk = f"f_pT{it % NB}"
                        it += 1
                        r = kt - 4 * qi
                        if r >= 0:
                            kb.op("dve", lambda e, ps_=ps_, tb=tb, r=r: e.tensor_tensor(out=tb[:], in0=ps_[:, :], in1=cqD[:, r, :], op=ALU.add),
                                  reads=[psk, ("f_cqD", r)], writes=[tbk])
                        else:
                            kb.op("dve", lambda e, ps_=ps_, tb=tb: e.tensor_tensor(out=tb[:], in0=ps_[:, :], in1=cqB[:], op=ALU.add),
                                  reads=[psk, "f_cqB"], writes=[tbk])
                        kb.op("act", lambda e, tb=tb, pb=pb, h=h, kt=kt: e.activation(out=pb[:], in_=tb[:], func=AF.Exp, bias=ckT[h][:, kt:kt + 1]),
                              reads=[tbk, ("ckT", h)], writes=[pbk])
                        if kt + LA < nk:
                            emit_S(kt + LA)
                        mm(c, po[0:65, :], fV[h][:, kt, :], pb[:], kt == 0, kt == nk - 1, [("fV", h, kt // 4), ("fV1", h), pbk], [pok])
                    kb.op("act", lambda e, po=po: e.activation(out=osb[:], in_=po[0:65, :], func=AF.Copy), reads=[pok], writes=["f_osb"])
                    pd, pdk = c.ps()
                    mm(c, pd[0:64, :], c.ones_f[64:65, 0:64], osb[64:65, :], True, True, ["ones_f", "f_osb"], [pdk])
                    kb.op("dve", lambda e, pd=pd: e.reciprocal(out=rden[:], in_=pd[0:64, :]), reads=[pdk], writes=["f_rden"])
                    ob = outb[qi % 2]; obk = f"f_out{qi % 2}"
                    kb.op("dve", lambda e, ob=ob: e.tensor_tensor(out=ob[:], in0=osb[0:64, :], in1=rden[:], op=ALU.mult), reads=["f_osb", "f_rden"], writes=[obk])
                    cfo = io["catf_out"]
                    kb.dma("sp", (cfo[h][:, qs] if isinstance(cfo, list) else cfo[h * 64:(h + 1) * 64, qs]), ob[:], reads=[obk])
            c.rot = None
        c.barrier()


def build_M(which="both"):
    nc = bass.Bass("TRN2", target_bir_lowering=False)
    io = {}

    def din(name, shape, dt=F32):
        io[name] = nc.dram_tensor(name, shape, dt, kind="ExternalInput").ap()

    def dout(name, shape, dt=F32):
        io[name] = nc.dram_tensor(name, shape, dt, kind="ExternalOutput").ap()

    din("hT_all", [4, D, NT], BF16); din("w_ml", [D, 258]); din("w_fx", [D, 386]); din("vecsM", [128, 16])
    dout("catm_out", [64, SEQ], BF16); dout("catf_out", [128, SEQ], BF16)
    with _ES() as st:
        c = Ctx(nc, st)
        c.setup()
        if which in ("both", "mlstm"):
            phase_M_mlstm(c, io)
        if which in ("both", "fox"):
            phase_M_fox(c, io)
        c.barrier()
        c.kb.flush()
    return nc


def inputs_M(inp, l, g):
    w_in = np.asarray(inp["w_in"][l], np.float32)
    cols = np.concatenate([
        np.arange(g * 64, g * 64 + 64), 256 + np.arange(g * 64, g * 64 + 64),
        512 + np.arange(g * 64, g * 64 + 64), 768 + np.arange(g * 64, g * 64 + 64),
        [1024 + g, 1028 + g]])
    w_ml = np.ascontiguousarray(w_in[:, cols])
    fcols = []
    for base in (1544, 2056, 2568):
        for hh in (2 * g, 2 * g + 1):
            fcols.append(base + np.arange(hh * 64, hh * 64 + 64))
    fcols.append(np.array([3080 + 2 * g, 3080 + 2 * g + 1]))
    w_fx = np.ascontiguousarray(w_in[:, np.concatenate(fcols)])
    v = np.zeros((128, 16), np.float32)
    cw = np.asarray(inp["mlstm_conv_w"][l], np.float32)
    cb = np.asarray(inp["mlstm_conv_b"][l], np.float32)
    v[0:64, 0:4] = cw[:, g * 64:g * 64 + 64].T
    v[0:64, 4] = cb[g * 64:g * 64 + 64]
    v[0:64, 5:9] = cw[:, 256 + g * 64:256 + g * 64 + 64].T
    v[0:64, 9] = cb[256 + g * 64:256 + g * 64 + 64]
    v[0:64, 10] = np.asarray(inp["mlstm_norm_w"][l], np.float32)[g * 64:g * 64 + 64]
    v[:, 11] = inp["mlstm_b_i"][l][g]
    v[:, 12] = inp["mlstm_b_f"][l][g]
    v[:, 13] = inp["fox_b_f"][l][2 * g]
    v[:, 14] = inp["fox_b_f"][l][2 * g + 1]
    return {"w_ml": w_ml, "w_fx": w_fx, "vecsM": v}


def phase_P(c, io, xT):
    kb = c.kb
    from contextlib import ExitStack
    with ExitStack() as s0:
        vecs = c.sb("vecsP_sb", [128, 8], F32, s0)
        eps_t = c.sb("p_eps", [128, 1], F32, s0)
        sq = c.sb("p_sq", [128, 8, TT], BF16, s0)
        rstd = c.sb("p_rstd", [128, TT], F32, s0)
        hT = c.sb("p_hT", [128, 8, TT], BF16, s0)
        xt = [c.sb(f"p_xt{i}", [128, 4, D], F32, s0) for i in range(2)]
        tmp = {"sq": sq, "rstd": rstd, "eps": eps_t}
        kb.dma("sp", vecs[:], io["vecsP"], writes=["vecs"])
        kb.op("pool", lambda e: e.memset(eps_t[:], EPS), writes=["eps"])
        for t in range(NTT):
            t0 = t * TT
            xb = xt[t % 2]
            xk = f"p_xt{t % 2}"
            kb.dma("sp", xb[:], io["x_tok"][t0:t0 + TT, :].rearrange("(s p) d -> p s d", p=128), writes=[xk])
            for k in range(8):
                p, pk = c.ps()
                for s in range(4):
                    kb.op("pe", lambda e, k=k, s=s, p=p, xb=xb: e.transpose(out=p[:, s * 128:(s + 1) * 128], in_=xb[:, s, k * 128:(k + 1) * 128], identity=c.ident_f[:]),
                          reads=[xk, "ident_f"], writes=[pk])
                kb.op("act", lambda e, k=k, p=p, t0=t0: e.activation(out=xT[:, k, t0:t0 + TT], in_=p[:, :], func=AF.Copy), reads=[pk], writes=[("xT", k)])
            rmsnorm_tile(c, xT, "xT", t0, TT, vecs[:, 0:8], tmp, hT, "hT")
            h_store(c, io["h_next"], hT, t0, TT, [("hT", k) for k in range(8)], ["h_next_d"])
            if t == NTT - 1 and "tail_next" in io:
                kb.dma("sp", io["tail_next"].rearrange("(k p) n -> p k n", p=128), hT[:, :, TT - 32:TT],
                       reads=[("hT", k) for k in range(8)], writes=["tail_next_d"])
    c.barrier()


def build_P():
    nc = bass.Bass("TRN2", target_bir_lowering=False)
    io = {}
    io["x_tok"] = nc.dram_tensor("x_tok", [NT, D], F32, kind="ExternalInput").ap()
    io["vecsP"] = nc.dram_tensor("vecsP", [128, 8], F32, kind="ExternalInput").ap()
    io["xT_out"] = nc.dram_tensor("xT_out", [D, NT], F32, kind="ExternalOutput").ap()
    io["h_next"] = nc.dram_tensor("h_next", [D, NT], BF16, kind="ExternalOutput").ap()
    with _ES() as st:
        c = Ctx(nc, st)
        c.setup()
        xT = c.sb("xT", [128, 8, NT], F32)
        phase_P(c, io, xT)
        c.kb.dma("sp", io["xT_out"].rearrange("(k p) n -> p k n", p=128), xT[:], reads=[("xT", k) for k in range(8)])
        c.barrier()
        c.kb.flush()
    return nc


_CACHE = {}


def _get(name, fn):
    if name not in _CACHE:
        _CACHE[name] = fn()
    return _CACHE[name]


def kernel(**inp):
    inp = {k: np.asarray(v) for k, v in inp.items()}
    cores = list(range(8))
    B = 2
    x = inp["x"].astype(np.float32, copy=False)
    fm = lambda w: np.ascontiguousarray(np.asarray(w, np.float32).reshape(-1, 128).T)
    ncP = _get("P", build_P)
    maps = []
    for cid in cores:
        b, j = cid // 4, cid % 4
        maps.append({"x_tok": np.ascontiguousarray(x[b, j * NT:(j + 1) * NT]), "vecsP": fm(inp["norm_mix_w"][0])})
    res = run_bass_kernel_spmd(ncP, maps, core_ids=cores).results
    xT = [r["xT_out"] for r in res]
    hN = [r["h_next"] for r in res]
    out = None
    for l in range(2):
        last = (l == 1)
        E = 1 if l == 0 else 8
        ncM = _get("M", build_M)
        maps = []
        for cid in cores:
            b, g = cid // 4, cid % 4
            m = inputs_M(inp, l, g)
            m["hT_all"] = np.ascontiguousarray(np.stack([hN[b * 4 + jj] for jj in range(4)], axis=0))
            maps.append(m)
        resM = run_bass_kernel_spmd(ncM, maps, core_ids=cores).results
        ncT = _get(("T", E, last), lambda: build_T(E, last))
        maps = []
        for cid in cores:
            b, j = cid // 4, cid % 4
            tk = slice(j * NT, (j + 1) * NT)
            m = {"xT_in": xT[cid], "h_own": hN[cid]}
            m["h_halo"] = (np.ascontiguousarray(hN[cid - 1][:, NT - 32:NT]) if j > 0 else np.zeros((D, 32), NPBF))
            m["catm"] = np.ascontiguousarray(np.stack([resM[b * 4 + g]["catm_out"][:, tk] for g in range(4)], axis=0))
            m["catf"] = np.ascontiguousarray(np.stack([resM[b * 4 + g]["catf_out"][:, tk] for g in range(4)], axis=0))
            m["mem"] = np.ascontiguousarray(inp["mem"][b], dtype=np.float32)
            m["vecs"] = vecs_T(inp, l, last)
            m["w_c"] = np.ascontiguousarray(inp["w_in"][l][:, 1032:1544])
            m["w_out"] = inp["w_out"][l]; m["w_q"] = inp["xattn_w_q"][l]
            m["w_kv"] = inp["xattn_w_kv"][l]; m["w_o"] = inp["xattn_w_o"][l]
            if E == 1:
                m["w_gate"] = inp["ffn_w_gate"]; m["w_up"] = inp["ffn_w_up"]; m["w_down"] = inp["ffn_w_down"]
            else:
                m["w_gate"] = inp["moe_w_gate"][0]; m["w_up"] = inp["moe_w_up"][0]; m["w_down"] = inp["moe_w_down"][0]
                m["router_w"] = inp["router_w"][0]
            maps.append(m)
        resT = run_bass_kernel_spmd(ncT, maps, core_ids=cores).results
        if not last:
            xT = [r["xT_out"] for r in resT]
            hN = [r["h_next"] for r in resT]
        else:
            out = np.zeros((B, SEQ, D), np.float32)
            for cid in cores:
                b, j = cid // 4, cid % 4
                out[b, j * NT:(j + 1) * NT] = resT[cid]["out"]
    return out


RG = [[0, 1, 2, 3], [4, 5, 6, 7]]
_STOP = None


def build_fused(stop=None):
    nc = bass.Bass("TRN2", target_bir_lowering=False)
    io = {}
    if stop:
        io["dbg1"] = nc.dram_tensor("dbg1", [4 * D, NT], BF16, kind="ExternalOutput").ap()
        io["dbg2"] = nc.dram_tensor("dbg2", [512, SEQ], BF16, kind="ExternalOutput").ap()
        io["dbg3"] = nc.dram_tensor("dbg3", [D, NT], F32, kind="ExternalOutput").ap()

    def din(name, shape, dt=F32):
        io[name] = nc.dram_tensor(name, shape, dt, kind="ExternalInput").ap()
        return io[name]

    def dint(name, shape, dt=BF16):
        io[name] = nc.dram_tensor(name, shape, dt, kind="Internal").ap()
        return io[name]

    din("x_tok", [NT, D]); din("vecsP", [128, 8])
    if stop != "AG":
        din("sel", [128, 8]); din("mem", [256, D])
    for l in range(2 if stop != "AG" else 0):
        din(f"w_ml{l}", [D, 258]); din(f"w_fx{l}", [D, 386]); din(f"vecsM{l}", [128, 16]); din(f"vecs{l}", [128, NV_T])
        din(f"w_c{l}", [D, 512]); din(f"w_out{l}", [D, D]); din(f"w_q{l}", [D, 512]); din(f"w_kv{l}", [D, D]); din(f"w_o{l}", [512, D])
    if stop != "AG":
        din("w_gate0", [1, D, DFF]); din("w_up0", [1, D, DFF]); din("w_down0", [1, DFF, D])
        din("w_gate1", [8, D, DFF]); din("w_up1", [8, D, DFF]); din("w_down1", [8, DFF, D]); din("router_w", [D, 8])
    io["out"] = nc.dram_tensor("out", [NT, D], F32, kind="ExternalOutput").ap()
    for l in range(2):
        io[f"h_own{l}"] = [dint(f"h_own{l}_{a}", [256, NT]) for a in range(4)]
        io[f"hT_all{l}"] = [dint(f"hT_all{l}_{a}", [4 * 256, NT]) for a in range(4)]
        dint(f"tail{l}", [D, 32]); dint(f"tails{l}", [4 * D, 32])
        dint(f"catm{l}", [64, SEQ]); dint(f"catm_all{l}", [256, SEQ])
        io[f"catf{l}"] = [dint(f"catf{l}_{h}", [64, SEQ]) for h in range(2)]
        io[f"catf_all{l}"] = [dint(f"catf_all{l}_{h}", [256, SEQ]) for h in range(2)]
    with _ES() as st:
        c = Ctx(nc, st)
        c.setup()
        kb = c.kb
        xT = c.sb("xT", [128, 8, NT], F32)
        phase_P(c, {"x_tok": io["x_tok"], "vecsP": io["vecsP"], "h_next": io["h_own0"], "tail_next": io["tail0"]}, xT)
        for l in range(2):
            last = (l == 1)
            for a in range(4):
                kb.collective("AllGather", RG, io[f"h_own{l}"][a], io[f"hT_all{l}"][a], reads=["h_next_d"], writes=["hT_all_d"])
            kb.collective("AllGather", RG, io[f"tail{l}"], io[f"tails{l}"], reads=["tail_next_d"], writes=["tails_d"])
            c.barrier()
            if stop == "AG":
                for a in range(4):
                    for jj in range(4):
                        kb.dma("sp", io["dbg1"][jj * D + a * 256:jj * D + (a + 1) * 256, :], io[f"hT_all{l}"][a][jj * 256:(jj + 1) * 256, :], reads=["hT_all_d"])
                break
            ioM = {"hT_all": io[f"hT_all{l}"], "w_ml": io[f"w_ml{l}"], "w_fx": io[f"w_fx{l}"],
                   "vecsM": io[f"vecsM{l}"], "catm_out": io[f"catm{l}"], "catf_out": io[f"catf{l}"]}
            c.sfx = f"_{l}"
            phase_M_mlstm(c, ioM)
            phase_M_fox(c, ioM)
            kb.collective("AllGather", RG, io[f"catm{l}"], io[f"catm_all{l}"], writes=["catm_all_d"])
            for h in range(2):
                kb.collective("AllGather", RG, io[f"catf{l}"][h], io[f"catf_all{l}"][h], writes=["catf_all_d"])
            c.barrier()
            if stop == "M":
                for h in range(2):
                    for g in range(4):
                        kb.dma("sp", io["dbg2"][g * 128 + h * 64:g * 128 + (h + 1) * 64, :], io[f"catf_all{l}"][h][g * 64:(g + 1) * 64, :], reads=["catf_all_d"])
                break
            ioT = {"h_own": io[f"h_own{l}"], "tails": io[f"tails{l}"], "sel": io["sel"], "catm_all": io[f"catm_all{l}"], "catf_all": io[f"catf_all{l}"],
                   "mem": io["mem"], "vecs": io[f"vecs{l}"], "w_c": io[f"w_c{l}"], "w_out": io[f"w_out{l}"], "w_q": io[f"w_q{l}"],
                   "w_kv": io[f"w_kv{l}"], "w_o": io[f"w_o{l}"], "w_gate": io[f"w_gate{l}"], "w_up": io[f"w_up{l}"], "w_down": io[f"w_down{l}"]}
            if last:
                ioT["router_w"] = io["router_w"]; ioT["out"] = io["out"]
            else:
                ioT["h_next"] = io["h_own1"]; ioT["tail_next"] = io["tail1"]
            phase_T(c, ioT, 8 if last else 1, last, xT)
            if stop == "T":
                kb.dma("sp", io["dbg3"].rearrange("(k p) n -> p k n", p=128), xT[:], reads=[("xT", k) for k in range(8)])
                break
        c.barrier()
        kb.flush()
    return nc


def kernel_unfused(**inp):
    return _kernel_unfused(**inp)


_kernel_unfused = kernel


def kernel(**inp):
    inp = {k: np.asarray(v) for k, v in inp.items()}
    cores = list(range(8))
    x = inp["x"].astype(np.float32, copy=False)
    fm = lambda w: np.ascontiguousarray(np.asarray(w, np.float32).reshape(-1, 128).T)
    nc = _get("fused", lambda: build_fused(_STOP))
    shared = {"vecsP": fm(inp["norm_mix_w"][0]),
              "w_gate0": inp["ffn_w_gate"], "w_up0": inp["ffn_w_up"], "w_down0": inp["ffn_w_down"],
              "w_gate1": inp["moe_w_gate"][0], "w_up1": inp["moe_w_up"][0], "w_down1": inp["moe_w_down"][0],
              "router_w": inp["router_w"][0]}
    for l in range(2):
        shared[f"vecs{l}"] = vecs_T(inp, l, l == 1)
        shared[f"w_c{l}"] = np.ascontiguousarray(inp["w_in"][l][:, 1032:1544])
        shared[f"w_out{l}"] = inp["w_out"][l]; shared[f"w_q{l}"] = inp["xattn_w_q"][l]
        shared[f"w_kv{l}"] = inp["xattn_w_kv"][l]; shared[f"w_o{l}"] = inp["xattn_w_o"][l]
    perg = []
    for g in range(4):
        d = {}
        for l in range(2):
            m = inputs_M(inp, l, g)
            d[f"w_ml{l}"] = m["w_ml"]; d[f"w_fx{l}"] = m["w_fx"]; d[f"vecsM{l}"] = m["vecsM"]
        perg.append(d)
    maps = []
    for cid in cores:
        b, j = cid // 4, cid % 4
        m = dict(shared)
        m.update(perg[j])
        m["x_tok"] = np.ascontiguousarray(x[b, j * NT:(j + 1) * NT])
        m["mem"] = np.ascontiguousarray(inp["mem"][b], dtype=np.float32)
        sel = np.zeros((128, 8), np.float32)
        sel[:, j] = 1.0
        if j > 0:
            sel[:, 4 + j - 1] = 1.0
        m["sel"] = sel
        maps.append(m)
    if _STOP == "AG":
        maps = [{k: m[k] for k in ("x_tok", "vecsP")} for m in maps]
    res = run_bass_kernel_spmd(nc, maps, core_ids=cores).results
    if _STOP:
        return res
    out = np.zeros((2, SEQ, D), np.float32)
    for cid in cores:
        b, j = cid // 4, cid % 4
        out[b, j * NT:(j + 1) * NT] = res[cid]["out"]
    return out
```

```python
import numpy as np
import concourse.bass as bass
import concourse.mybir as mybir
from concourse.bass_utils import run_bass_kernel_spmd

F32 = mybir.dt.float32
BF16 = mybir.dt.bfloat16
AF = mybir.ActivationFunctionType
ALU = mybir.AluOpType
AX = mybir.AxisListType

ENGS = ("pe", "act", "dve", "pool", "sp")


class KB:
    SEM_ROLL = 2000

    def __init__(self, nc, n_dma_sems=32):
        self.nc = nc
        self.q = {e: [] for e in ENGS}
        self.cnt = {e: 0 for e in ENGS}
        self.cur_sem = {}
        self.sem_pool = []
        self.waited = {e: {} for e in ENGS}
        self.last_w = {}
        self.reads = {}
        self.n_dma_sems = n_dma_sems
        self.dma_sems = []
        self.dma_cnt = []
        self.dma_rr = 0
        self.dma_rr_sw = 0
        self._stack = None
        self.n_inst = 0

    def _new_sem(self, name):
        s = self._stack.enter_context(self.nc.semaphore(name))
        return s

    def start(self, stack):
        self._stack = stack
        for e in ENGS:
            self.cur_sem[e] = self._new_sem(f"p_{e}_0")
        for i in range(self.n_dma_sems):
            self.dma_sems.append(self._new_sem(f"dma{i}"))
            self.dma_cnt.append(0)

    def _wait(self, eng, ev):
        if ev is None:
            return
        if len(ev) == 3 and ev[2] == "pe" and eng == "pe":
            return
        sem, val = ev[0], ev[1]
        w = self.waited[eng]
        if w.get(id(sem), (None, 0))[1] >= val:
            return
        w[id(sem)] = (sem, val)
        self.q[eng].append(lambda e, sem=sem, val=val: e.wait_ge(sem, val))

    def _wait_w(self, eng, k):
        lw = self.last_w.get(k)
        if isinstance(lw, list):
            for ev in lw:
                self._wait(eng, ev)
        else:
            self._wait(eng, lw)

    def _deps(self, eng, reads, writes):
        for k in reads:
            self._wait_w(eng, k)
        for k in writes:
            self._wait_w(eng, k)
            for ev in self.reads.get(k, ()):
                self._wait(eng, ev)

    def _commit(self, ev, reads, writes, is_dma=False):
        for k in writes:
            lw = self.last_w.get(k)
            if is_dma and isinstance(lw, list) and not self.reads.get(k):
                lw.append(ev)
            else:
                self.last_w[k] = [ev] if is_dma else ev
            self.reads[k] = []
        for k in reads:
            self.reads.setdefault(k, []).append(ev)

    def op(self, eng, fn, reads=(), writes=()):
        self._deps(eng, reads, writes)
        if self.cnt[eng] >= self.SEM_ROLL:
            self.cur_sem[eng] = self._new_sem(f"p_{eng}_{self.n_inst}")
            self.cnt[eng] = 0
        self.cnt[eng] += 1
        sem = self.cur_sem[eng]
        ev = (sem, self.cnt[eng], eng)
        self.q[eng].append(lambda e, sem=sem: fn(e).then_inc(sem, 1))
        self._commit(ev, reads, writes)
        self.n_inst += 1
        return ev

    def dma(self, eng, out, in_, reads=(), writes=(), **kw):
        self._deps(eng, reads, writes)
        half = self.n_dma_sems // 2
        if eng == "pool":
            i = half + self.dma_rr_sw
            self.dma_rr_sw = (self.dma_rr_sw + 1) % (self.n_dma_sems - half)
        else:
            i = self.dma_rr
            self.dma_rr = (self.dma_rr + 1) % half
        sem = self.dma_sems[i]
        if self.dma_cnt[i] >= 2048:
            self.dma_sems[i] = self._new_sem(f"dma{i}_{self.n_inst}")
            self.dma_cnt[i] = 0
            sem = self.dma_sems[i]
        if self.dma_cnt[i] > 0:
            self._wait(eng, (sem, self.dma_cnt[i]))
        self.dma_cnt[i] += 16
        ev = (sem, self.dma_cnt[i])
        self.q[eng].append(lambda e, sem=sem: e.dma_start(out=out, in_=in_, **kw).then_inc(sem, 16))
        self._commit(ev, reads, writes, is_dma=True)
        self.n_inst += 1
        return ev

    def collective(self, kind, rg, in_ap, out_ap, reads=(), writes=()):
        eng = "pool"
        self._deps(eng, reads, writes)
        sem = self._new_sem(f"cc_{self.n_inst}")
        ev = (sem, 1)
        self.q[eng].append(lambda e: e.collective_compute(kind, ALU.bypass, replica_groups=rg, ins=[in_ap.opt()],
                                                          outs=[out_ap.opt()]).then_inc(sem, 1))
        self._commit(ev, reads, writes)
        self.n_inst += 1
        self.cc_events = getattr(self, "cc_events", []) + [ev]
        return ev

    def wait_all(self, eng, evs):
        for ev in evs:
            self._wait(eng, ev)

    def flush(self):
        nc = self.nc
        q = self.q
        with nc.Block() as block:
            @block.tensor
            def _(e):
                for f in q["pe"]:
                    f(e)

            @block.scalar
            def _(e):
                for f in q["act"]:
                    f(e)

            @block.vector
            def _(e):
                for f in q["dve"]:
                    f(e)

            @block.gpsimd
            def _(e):
                for f in q["pool"]:
                    f(e)

            @block.sync
            def _(e):
                for f in q["sp"]:
                    f(e)
        self.q = {e: [] for e in ENGS}


D = 1024
NT = 2048
TT = 512
NTT = NT // TT
DFF = 2816
NF = DFF // 128
SEQ = 8192
EPS = 1e-6
NV_T = 100


class Ctx:
    def __init__(self, nc, st):
        self.nc = nc
        self.st = st
        self.kb = KB(nc)
        self.kb.start(st)
        self.ps_rr = 0
        self.uid = 0

    def sb(self, name, shape, dt, st=None):
        self.uid += 1
        return (st or self.st).enter_context(self.nc.sbuf_tensor(f"{name}_u{self.uid}", shape, dt))

    def barrier(self):
        kb = self.kb
        evs = []
        for e in ENGS:
            if kb.cnt[e] > 0:
                evs.append((kb.cur_sem[e], kb.cnt[e]))
        for i, s in enumerate(kb.dma_sems):
            if kb.dma_cnt[i] > 0:
                evs.append((s, kb.dma_cnt[i]))
        evs += getattr(kb, "cc_events", [])
        kb.cc_events = []
        for e in ENGS:
            for ev in evs:
                kb._wait(e, ev)
        kb.last_w = {}
        kb.reads = {}

    def setup(self):
        nc, kb = self.nc, self.kb
        self.ident_f = self.sb("ident_f", [128, 128], F32)
        self.ident_b = self.sb("ident_b", [128, 128], BF16)
        self.ones_b = self.sb("ones_b", [128, 128], BF16)
        self.ones_f = self.sb("ones_f", [128, 128], F32)
        self.psb = [self.st.enter_context(nc.psum_tensor(f"psb{i}", [128, 512], F32)) for i in range(8)]
        idf, idb, ob, of = self.ident_f, self.ident_b, self.ones_b, self.ones_f
        kb.op("pool", lambda e: e.memset(idf[:], 0.0), writes=["ident_f"])
        kb.op("pool", lambda e: e.affine_select(out=idf[:], in_=idf[:], pattern=[[-1, 128]],
                                                compare_op=ALU.not_equal, fill=1.0, base=0,
                                                channel_multiplier=1),
              reads=["ident_f"], writes=["ident_f"])
        kb.op("pool", lambda e: e.tensor_copy(out=idb[:], in_=idf[:]), reads=["ident_f"], writes=["ident_b"])
        kb.op("pool", lambda e: e.memset(ob[:], 1.0), writes=["ones_b"])
        kb.op("pool", lambda e: e.memset(of[:], 1.0), writes=["ones_f"])

    def ps(self):
        rot = getattr(self, "rot", None) or list(range(8))
        i = rot[self.ps_rr % len(rot)]
        self.ps_rr += 1
        return self.psb[i], f"psb{i}"


def h_store(c, dst, hT, c0, n, reads, writes=()):
    if isinstance(dst, list):
        for a, d in enumerate(dst):
            c.kb.dma("sp", d[:, c0:c0 + n].rearrange("(k p) n -> p k n", p=128), hT[:, 2 * a:2 * a + 2, 0:n], reads=reads, writes=writes)
    else:
        c.kb.dma("sp", dst[:, c0:c0 + n].rearrange("(k p) n -> p k n", p=128), hT[:, :, 0:n], reads=reads, writes=writes)


def h_load(c, src, hT, c0, n, writes, j=None):
    if isinstance(src, list):
        for a, d in enumerate(src):
            v = d if j is None else d.rearrange("(j r) n -> j r n", j=4)[j]
            c.kb.dma("sp", hT[:, 2 * a:2 * a + 2, 0:n], v[:, c0:c0 + n].rearrange("(k p) n -> p k n", p=128), writes=writes)
    else:
        v = src if j is None else src[j]
        c.kb.dma("sp", hT[:, :, 0:n], v[:, c0:c0 + n].rearrange("(k p) n -> p k n", p=128), writes=writes)


def mm(c, out, lhsT, rhs, start, stop, reads, writes):
    return c.kb.op("pe", lambda e: e.matmul(out, lhsT=lhsT, rhs=rhs, start=start, stop=stop),
                   reads=reads, writes=writes)


def rmsnorm_tile(c, xT, xkey, t0, n, wv, tmp, out_bf, okey, out_f=None):
    kb = c.kb
    sq, rstd = tmp["sq"], tmp["rstd"]
    for k in range(8):
        kb.op("act", lambda e, k=k: e.activation(out=sq[:, k, 0:n], in_=xT[:, k, t0:t0 + n], func=AF.Square),
              reads=[(xkey, k)], writes=[("sq", k)])
    p, pk = c.ps()
    for k in range(8):
        mm(c, p[:, 0:n], c.ones_b[:], sq[:, k, 0:n], k == 0, k == 7, ["ones_b", ("sq", k)], [pk])
    kb.op("act", lambda e: e.activation(out=rstd[:, 0:n], in_=p[:, 0:n], func=AF.Sqrt, scale=1.0 / D, bias=tmp["eps"][:, 0:1]),
          reads=[pk, "eps"], writes=["rstd"])
    kb.op("dve", lambda e: e.reciprocal(out=rstd[:, 0:n], in_=rstd[:, 0:n]), reads=["rstd"], writes=["rstd"])
    for k in range(8):
        kb.op("dve", lambda e, k=k: e.scalar_tensor_tensor(out=out_bf[:, k, 0:n], in0=xT[:, k, t0:t0 + n],
                                                           scalar=wv[:, k:k + 1], in1=rstd[:, 0:n],
                                                           op0=ALU.mult, op1=ALU.mult),
              reads=[(xkey, k), "rstd", "vecs"], writes=[(okey, k)])
        if out_f is not None:
            kb.op("dve", lambda e, k=k: e.scalar_tensor_tensor(out=out_f[:, k, 0:n], in0=xT[:, k, t0:t0 + n],
                                                                scalar=wv[:, k:k + 1], in1=rstd[:, 0:n],
                                                                op0=ALU.mult, op1=ALU.mult),
                  reads=[(xkey, k), "rstd", "vecs"], writes=[(okey + "_f", k)])


def phase_T(c, io, E, last, xT):
    nc, kb = c.nc, c.kb
    from contextlib import ExitStack
    vec_st = ExitStack()
    vecs = c.sb("vecsT", [128, NV_T], F32, vec_st)
    eps_t = c.sb("eps_t", [128, 1], F32, vec_st)
    sq = c.sb("sq", [128, 8, TT], BF16, vec_st)
    rstd = c.sb("rstd", [128, TT], F32, vec_st)
    tmp = {"sq": sq, "rstd": rstd, "eps": eps_t}
    kb.dma("sp", vecs[:], io["vecs"], writes=["vecs"])
    kb.op("pool", lambda e: e.memset(eps_t[:], EPS), writes=["eps"])
    V_XA, V_MEM, V_FFN, V_NEXT, V_CB, V_LNW, V_LNB, V_CW = 0, 8, 16, 24, 32, 34, 36, 38

    with ExitStack() as s1:
        hT = c.sb("hT", [128, 8, TT], BF16, s1)
        gluT = c.sb("gluT", [128, 2, 32 + NT], BF16, s1)
        hcT = c.sb("hcT", [128, 2, NT], BF16, s1)
        wc = c.sb("wc", [128, 8, 512], BF16, s1)
        dg = c.sb("dg", [128, 62, 128], BF16, s1)
        sig = c.sb("sig", [128, 2, TT], F32, s1)
        hcv = c.sb("hcv", [128, 2, TT], F32, s1)
        hsq = c.sb("hsq", [128, 2, TT], F32, s1)
        mean = c.sb("mean", [128, TT], F32, s1)
        var = c.sb("var", [128, TT], F32, s1)
        wo_m = c.sb("wo_m", [64, 4, D], BF16, s1)
        wo_c = c.sb("wo_c", [128, 2, D], BF16, s1)
        wo_f = c.sb("wo_f", [128, 4, D], BF16, s1)
        mT = c.sb("mT", [64, 4, TT], BF16, s1)
        fT = c.sb("fT", [128, 4, TT], BF16, s1)
        if "sel" in io:
            halo4 = c.sb("halo4", [128, 4, 8, 32], BF16, s1)
            selt = c.sb("selt", [128, 8], F32, s1)
            m4 = [c.sb(f"m4_{i}", [64, 4, TT], BF16, s1) for i in range(2)]
            f4 = [c.sb(f"f4_{i}", [128, 4, TT], BF16, s1) for i in range(2)]
            kb.dma("sp", selt[:], io["sel"], writes=["selt"])
        kb.dma("pool", wc[:], io["w_c"].rearrange("(k p) n -> p k n", p=128), writes=["wc"])
        kb.dma("pool", wo_m[:], io["w_out"][0:256, :].rearrange("(g p) n -> p g n", p=64), writes=["wo_m"])
        kb.dma("pool", wo_c[:], io["w_out"][256:512, :].rearrange("(g p) n -> p g n", p=128), writes=["wo_c"])
        kb.dma("pool", wo_f[:], io["w_out"][512:1024, :].rearrange("(g p) n -> p g n", p=128), writes=["wo_f"])
        for j in range(31):
            for ch in range(2):
                kb.op("dve", lambda e, j=j, ch=ch: e.tensor_scalar(
                    out=dg[:, j * 2 + ch, :], in0=c.ident_b[:], scalar1=vecs[:, V_CW + j * 2 + ch:V_CW + j * 2 + ch + 1],
                    scalar2=None, op0=ALU.mult), reads=["ident_b", "vecs"], writes=[("dg", j, ch)])
        tiles = [("halo", 0, 32)] + [("own", t * TT, TT) for t in range(NTT)]
        for kind, t0, n in tiles:
            if kind == "halo" and "sel" in io:
                tl = io["tails"].rearrange("(j k p) n -> j p k n", j=4, p=128)
                for jj in range(4):
                    kb.dma("sp", halo4[:, jj, :, :], tl[jj], writes=[("halo4", jj)])
                kb.op("dve", lambda e: e.tensor_scalar(out=hT[:, :, 0:32], in0=halo4[:, 0, :, :], scalar1=selt[:, 4:5], scalar2=None, op0=ALU.mult),
                      reads=[("halo4", 0), "selt"], writes=[("hT", k) for k in range(8)])
                for jj in range(1, 4):
                    kb.op("dve", lambda e, jj=jj: e.scalar_tensor_tensor(out=hT[:, :, 0:32], in0=halo4[:, jj, :, :], scalar=selt[:, 4 + jj:5 + jj], in1=hT[:, :, 0:32],
                                                                         op0=ALU.mult, op1=ALU.add),
                          reads=[("halo4", jj), "selt"] + [("hT", k) for k in range(8)], writes=[("hT", k) for k in range(8)])
                g0 = 0
            elif kind == "halo":
                kb.dma("sp", hT[:, :, 0:n], io["h_halo"].rearrange("(k p) n -> p k n", p=128),
                       writes=[("hT", k) for k in range(8)])
                g0 = 0
            else:
                h_load(c, io["h_own"], hT, t0, n, [("hT", k) for k in range(8)])
                g0 = 32 + t0
            for ch in range(2):
                pa, pak = c.ps()
                pg, pgk = c.ps()
                for k in range(8):
                    mm(c, pa[:, 0:n], wc[:, k, ch * 128:(ch + 1) * 128], hT[:, k, 0:n], k == 0, k == 7,
                       ["wc", ("hT", k)], [pak])
                for k in range(8):
                    mm(c, pg[:, 0:n], wc[:, k, 256 + ch * 128:256 + (ch + 1) * 128], hT[:, k, 0:n], k == 0, k == 7,
                       ["wc", ("hT", k)], [pgk])
                kb.op("act", lambda e, ch=ch, pg=pg, n=n: e.activation(out=sig[:, ch, 0:n], in_=pg[:, 0:n], func=AF.Sigmoid),
                      reads=[pgk], writes=[("sig", ch)])
                kb.op("dve", lambda e, ch=ch, pa=pa, n=n, g0=g0: e.tensor_tensor(
                    out=gluT[:, ch, g0:g0 + n], in0=pa[:, 0:n], in1=sig[:, ch, 0:n], op=ALU.mult),
                    reads=[pak, ("sig", ch)], writes=[("glu", ch, g0 // TT), ("glu", ch, (g0 + n - 1) // TT)])
        for t in range(NTT):
            t0 = t * TT
            gk = lambda ch: [("glu", ch, (32 + t0 - 30) // TT), ("glu", ch, (32 + t0 + TT - 1) // TT)]
            for ch in range(2):
                p, pk = c.ps()
                for j in range(31):
                    o = 32 + t0 - 30 + j
                    mm(c, p[:, :], dg[:, j * 2 + ch, :], gluT[:, ch, o:o + TT], j == 0, j == 30,
                       [("dg", j, ch)] + gk(ch), [pk])
                kb.op("act", lambda e, ch=ch, p=p: e.activation(out=hcv[:, ch, :], in_=p[:, :], func=AF.Identity,
                                                                bias=vecs[:, V_CB + ch:V_CB + ch + 1]),
                      reads=[pk, "vecs"], writes=[("hcv", ch)])
                kb.op("act", lambda e, ch=ch: e.activation(out=hsq[:, ch, :], in_=hcv[:, ch, :], func=AF.Square),
                      reads=[("hcv", ch)], writes=[("hsq", ch)])
            p1, p1k = c.ps()
            p2, p2k = c.ps()
            for ch in range(2):
                mm(c, p1[:, :], c.ones_f[:], hcv[:, ch, :], ch == 0, ch == 1, ["ones_f", ("hcv", ch)], [p1k])
            for ch in range(2):
                mm(c, p2[:, :], c.ones_f[:], hsq[:, ch, :], ch == 0, ch == 1, ["ones_f", ("hsq", ch)], [p2k])
            kb.op("dve", lambda e, p1=p1: e.tensor_scalar(out=mean[:], in0=p1[:, :], scalar1=1.0 / 256, scalar2=None, op0=ALU.mult),
                  reads=[p1k], writes=["mean"])
            kb.op("dve", lambda e: e.tensor_tensor(out=var[:], in0=mean[:], in1=mean[:], op=ALU.mult),
                  reads=["mean"], writes=["var"])
            kb.op("dve", lambda e, p2=p2: e.scalar_tensor_tensor(out=var[:], in0=p2[:, :], scalar=1.0 / 256, in1=var[:],
                                                                 op0=ALU.mult, op1=ALU.subtract),
                  reads=[p2k, "var"], writes=["var"])
            kb.op("act", lambda e: e.activation(out=var[:], in_=var[:], func=AF.Sqrt, bias=eps_t[:, 0:1]),
                  reads=["var", "eps"], writes=["var"])
            kb.op("dve", lambda e: e.reciprocal(out=var[:], in_=var[:]), reads=["var"], writes=["var"])
            for ch in range(2):
                kb.op("dve", lambda e, ch=ch: e.tensor_tensor(out=hcv[:, ch, :], in0=hcv[:, ch, :], in1=mean[:], op=ALU.subtract),
                      reads=[("hcv", ch), "mean"], writes=[("hcv", ch)])
                kb.op("dve", lambda e, ch=ch: e.tensor_tensor(out=hcv[:, ch, :], in0=hcv[:, ch, :], in1=var[:], op=ALU.mult),
                      reads=[("hcv", ch), "var"], writes=[("hcv", ch)])
                kb.op("dve", lambda e, ch=ch: e.tensor_scalar(out=hcv[:, ch, :], in0=hcv[:, ch, :],
                                                              scalar1=vecs[:, V_LNW + ch:V_LNW + ch + 1],
                                                              scalar2=vecs[:, V_LNB + ch:V_LNB + ch + 1],
                                                              op0=ALU.mult, op1=ALU.add),
                      reads=[("hcv", ch), "vecs"], writes=[("hcv", ch)])
                kb.op("act", lambda e, ch=ch, t0=t0: e.activation(out=hcT[:, ch, t0:t0 + TT], in_=hcv[:, ch, :], func=AF.Silu),
                      reads=[("hcv", ch)], writes=[("hcT", ch, t)])
            if "sel" in io:
                cm = io["catm_all"].rearrange("(g p) n -> p g n", p=64)
                cf = [a.rearrange("(g p) n -> p g n", p=64) for a in io["catf_all"]]
                for jj in range(4):
                    for dst, stg, src, nm, npart in ((mT, m4, cm, "m4", 64), (fT, f4, cf, "f4", 128)):
                        dk = "mT" if nm == "m4" else "fT"
                        sg = stg[jj % 2]
                        sk = f"{nm}_{jj % 2}"
                        if nm == "m4":
                            kb.dma("sp", sg[:], src[:, :, jj * NT + t0:jj * NT + t0 + TT], writes=[sk])
                        else:
                            for hh in range(2):
                                kb.dma("sp", sg[hh * 64:(hh + 1) * 64, :, :], src[hh][:, :, jj * NT + t0:jj * NT + t0 + TT], writes=[sk])
                        if jj == 0:
                            kb.op("dve", lambda e, dst=dst, sg=sg, npart=npart: e.tensor_scalar(out=dst[:], in0=sg[:], scalar1=selt[0:npart, 0:1], scalar2=None, op0=ALU.mult),
                                  reads=[sk, "selt"], writes=[dk])
                        else:
                            kb.op("dve", lambda e, dst=dst, sg=sg, jj=jj, npart=npart: e.scalar_tensor_tensor(out=dst[:], in0=sg[:], scalar=selt[0:npart, jj:jj + 1], in1=dst[:],
                                                                                                          op0=ALU.mult, op1=ALU.add),
                                  reads=[sk, "selt", dk], writes=[dk])
            else:
                kb.dma("sp", mT[:], io["catm"][:, :, t0:t0 + TT].rearrange("g p n -> p g n"), writes=["mT"])
                kb.dma("sp", fT[:], io["catf"][:, :, t0:t0 + TT].rearrange("g p n -> p g n"), writes=["fT"])
            for d in range(8):
                p, pk = c.ps()
                ds = slice(d * 128, (d + 1) * 128)
                for g in range(4):
                    mm(c, p[:, :], wo_m[:, g, ds], mT[:, g, :], g == 0, False, ["wo_m", "mT"], [pk])
                for ch in range(2):
                    mm(c, p[:, :], wo_c[:, ch, ds], hcT[:, ch, t0:t0 + TT], False, False, ["wo_c", ("hcT", ch, t)], [pk])
                for g in range(4):
                    mm(c, p[:, :], wo_f[:, g, ds], fT[:, g, :], False, g == 3, ["wo_f", "fT"], [pk])
                kb.op("dve", lambda e, d=d, p=p, t0=t0: e.tensor_tensor(out=xT[:, d, t0:t0 + TT], in0=xT[:, d, t0:t0 + TT],
                                                                        in1=p[:, :], op=ALU.add),
                      reads=[pk, ("xT", d)], writes=[("xT", d)])
    c.barrier()
    if io.get("dbg_stage") == 1:
        vec_st.close()
        return

    with ExitStack() as s2:
        hT = c.sb("hT", [128, 8, TT], BF16, s2)
        memt = c.sb("memt", [128, 2, D], F32, s2)
        mss = c.sb("mss", [128, 2], F32, s2)
        junk = c.sb("junk", [128, D], F32, s2)
        memnT = c.sb("memnT", [128, 8, 256], BF16, s2)
        wkv = c.sb("wkv", [128, 8, D], BF16, s2)
        wq = c.sb("wq", [128, 8, 512], BF16, s2)
        wo = c.sb("wo", [128, 4, D], BF16, s2)
        kT = c.sb("kT", [128, 4, 256], BF16, s2)
        Vt = c.sb("Vt", [128, 2, 512], BF16, s2)
        qT = c.sb("qT", [128, 4, TT], BF16, s2)
        pT = c.sb("pT", [128, 8, TT], BF16, s2)
        rden = c.sb("rden", [128, TT], F32, s2)
        oT = c.sb("oT", [128, 4, TT], BF16, s2)
        kb.dma("sp", memt[:], io["mem"].rearrange("(t p) d -> p t d", p=128), writes=["memt"])
        kb.dma("pool", wkv[:], io["w_kv"].rearrange("(k p) n -> p k n", p=128), writes=["wkv"])
        kb.dma("pool", wq[:], io["w_q"].rearrange("(k p) n -> p k n", p=128), writes=["wq"])
        kb.dma("pool", wo[:], io["w_o"].rearrange("(k p) n -> p k n", p=128), writes=["wo"])
        for mt in range(2):
            kb.op("act", lambda e, mt=mt: e.activation(out=junk[:], in_=memt[:, mt, :], func=AF.Square,
                                                       accum_out=mss[:, mt:mt + 1]),
                  reads=["memt"], writes=["junk", ("mss", mt)])
            kb.op("act", lambda e, mt=mt: e.activation(out=mss[:, mt:mt + 1], in_=mss[:, mt:mt + 1], func=AF.Sqrt,
                                                       scale=1.0 / D, bias=eps_t[:, 0:1]),
                  reads=[("mss", mt), "eps"], writes=[("mss", mt)])
            kb.op("dve", lambda e, mt=mt: e.reciprocal(out=mss[:, mt:mt + 1], in_=mss[:, mt:mt + 1]),
                  reads=[("mss", mt)], writes=[("mss", mt)])
            kb.op("dve", lambda e, mt=mt: e.tensor_scalar(out=memt[:, mt, :], in0=memt[:, mt, :], scalar1=mss[:, mt:mt + 1],
                                                          scalar2=None, op0=ALU.mult),
                  reads=["memt", ("mss", mt)], writes=["memt"])
        for k in range(8):
            p, pk = c.ps()
            for mt in range(2):
                kb.op("pe", lambda e, k=k, mt=mt, p=p: e.transpose(out=p[:, mt * 128:(mt + 1) * 128],
                                                                   in_=memt[:, mt, k * 128:(k + 1) * 128], identity=c.ident_f[:]),
                      reads=["memt", "ident_f"], writes=[pk])
            kb.op("dve", lambda e, k=k, p=p: e.tensor_scalar(out=memnT[:, k, :], in0=p[:, 0:256],
                                                             scalar1=vecs[:, V_MEM + k:V_MEM + k + 1], scalar2=None, op0=ALU.mult),
                  reads=[pk, "vecs"], writes=[("memnT", k)])
        for h in range(4):
            p, pk = c.ps()
            for k in range(8):
                mm(c, p[:, 0:256], wkv[:, k, h * 128:(h + 1) * 128], memnT[:, k, :], k == 0, k == 7, ["wkv", ("memnT", k)], [pk])
            kb.op("act", lambda e, h=h, p=p: e.activation(out=kT[:, h, :], in_=p[:, 0:256], func=AF.Copy),
                  reads=[pk], writes=[("kT", h)])
        for mt in range(2):
            p, pk = c.ps()
            for k in range(8):
                mm(c, p[:, :], memnT[:, k, mt * 128:(mt + 1) * 128], wkv[:, k, 512:1024], k == 0, k == 7, ["wkv", ("memnT", k)], [pk])
            kb.op("act", lambda e, mt=mt, p=p: e.activation(out=Vt[:, mt, :], in_=p[:, :], func=AF.Copy),
                  reads=[pk], writes=[("Vt", mt)])
        sc = 128 ** -0.5
        for t in range(NTT):
            t0 = t * TT
            rmsnorm_tile(c, xT, "xT", t0, TT, vecs[:, V_XA:V_XA + 8], tmp, hT, "hT")
            for h in range(4):
                p, pk = c.ps()
                for k in range(8):
                    mm(c, p[:, :], wq[:, k, h * 128:(h + 1) * 128], hT[:, k, :], k == 0, k == 7, ["wq", ("hT", k)], [pk])
                kb.op("act", lambda e, h=h, p=p: e.activation(out=qT[:, h, :], in_=p[:, :], func=AF.Copy),
                      reads=[pk], writes=[("qT", h)])
            for h in range(4):
                for mt in range(2):
                    p, pk = c.ps()
                    mm(c, p[:, :], kT[:, h, mt * 128:(mt + 1) * 128], qT[:, h, :], True, True, [("kT", h), ("qT", h)], [pk])
                    kb.op("act", lambda e, h=h, mt=mt, p=p: e.activation(out=pT[:, h * 2 + mt, :], in_=p[:, :], func=AF.Exp, scale=sc),
                          reads=[pk], writes=[("pT", h, mt)])
                pd, pdk = c.ps()
                for mt in range(2):
                    mm(c, pd[:, :], c.ones_b[:], pT[:, h * 2 + mt, :], mt == 0, mt == 1, ["ones_b", ("pT", h, mt)], [pdk])
                kb.op("dve", lambda e, pd=pd: e.reciprocal(out=rden[:], in_=pd[:, :]), reads=[pdk], writes=["rden"])
                po, pok = c.ps()
                for mt in range(2):
                    mm(c, po[:, :], Vt[:, mt, h * 128:(h + 1) * 128], pT[:, h * 2 + mt, :], mt == 0, mt == 1,
                       [("Vt", mt), ("pT", h, mt)], [pok])
                kb.op("dve", lambda e, h=h, po=po: e.tensor_tensor(out=oT[:, h, :], in0=po[:, :], in1=rden[:], op=ALU.mult),
                      reads=[pok, "rden"], writes=[("oT", h)])
            for d in range(8):
                p, pk = c.ps()
                for h in range(4):
                    mm(c, p[:, :], wo[:, h, d * 128:(d + 1) * 128], oT[:, h, :], h == 0, h == 3, ["wo", ("oT", h)], [pk])
                kb.op("dve", lambda e, d=d, p=p, t0=t0: e.tensor_tensor(out=xT[:, d, t0:t0 + TT], in0=xT[:, d, t0:t0 + TT],
                                                                        in1=p[:, :], op=ALU.add),
                      reads=[pk, ("xT", d)], writes=[("xT", d)])
    c.barrier()
    if io.get("dbg_stage") == 2:
        vec_st.close()
        return

    with ExitStack() as s3:
        hTall = c.sb("hTall", [128, 8, NT], BF16, s3)
        actT = c.sb("actT", [128, 8, NT], BF16, s3)
        wgu = [c.sb(f"wgu{i}", [128, 8, 256], BF16, s3) for i in range(3)]
        wdr = [c.sb(f"wdr{i}", [128, D], BF16, s3) for i in range(11)]
        sil = [c.sb(f"sil{i}", [128, TT], BF16, s3) for i in range(2)]
        if E > 1:
            wr = c.sb("wr", [128, 8, 8], F32, s3)
            lg = c.sb("lg", [128, 4, 8], F32, s3)
            top8 = c.sb("top8", [128, 4, 8], F32, s3)
            gts = c.sb("gts", [128, 16, 8], F32, s3)
            gsc = c.sb("gsc", [128, 4, 4], F32, s3)
            dgate = c.sb("dgate", [128, 128], F32, s3)
            gB = [c.sb(f"gB{i}", [128, NT], BF16, s3) for i in range(2)]
            ytmps = [c.sb(f"ytmp{i}", [128, TT], F32, s3) for i in range(2)]
            kb.dma("sp", wr[:], io["router_w"].rearrange("(k p) n -> p k n", p=128), writes=["wr"])
        with ExitStack() as s3a:
            hF = c.sb("hF", [128, 8, TT], F32, s3a) if E > 1 else None
            for t in range(NTT):
                t0 = t * TT
                rmsnorm_tile(c, xT, "xT", t0, TT, vecs[:, V_FFN:V_FFN + 8], tmp, hTall[:, :, t0:t0 + TT], f"hA{t}", out_f=hF)
                if E > 1:
                    for s in range(4):
                        p, pk = c.ps()
                        for k in range(8):
                            mm(c, p[:, 0:8], hF[:, k, s * 128:(s + 1) * 128], wr[:, k, :], k == 0, k == 7, [(f"hA{t}_f", k), "wr"], [pk])
                        kb.op("dve", lambda e, s=s, p=p: e.tensor_copy(out=lg[:, s, :], in_=p[:, 0:8]), reads=[pk], writes=[("lg", s)])
                        kb.op("dve", lambda e, s=s: e.max(out=top8[:, s, :], in_=lg[:, s, :]), reads=[("lg", s)], writes=[("top8", s)])
                        kb.op("dve", lambda e, s=s: e.tensor_scalar(out=gsc[:, s, 0:1], in0=top8[:, s, 0:1], scalar1=-1.0, scalar2=None, op0=ALU.mult),
                              reads=[("top8", s)], writes=[("gsc", s, 0)])
                        kb.op("act", lambda e, s=s: e.activation(out=gsc[:, s, 1:2], in_=top8[:, s, 1:2], func=AF.Exp, bias=gsc[:, s, 0:1]),
                              reads=[("top8", s), ("gsc", s, 0)], writes=[("gsc", s, 1)])
                        kb.op("dve", lambda e, s=s: e.tensor_scalar(out=gsc[:, s, 1:2], in0=gsc[:, s, 1:2], scalar1=1.0, scalar2=None, op0=ALU.add),
                              reads=[("gsc", s, 1)], writes=[("gsc", s, 1)])
                        kb.op("dve", lambda e, s=s: e.reciprocal(out=gsc[:, s, 1:2], in_=gsc[:, s, 1:2]),
                              reads=[("gsc", s, 1)], writes=[("gsc", s, 1)])
                        gi = t * 4 + s
                        kb.op("act", lambda e, s=s, gi=gi: e.activation(out=gts[:, gi, :], in_=lg[:, s, :], func=AF.Exp, bias=gsc[:, s, 0:1]),
                              reads=[("lg", s), ("gsc", s, 0)], writes=[("gts", gi)])
                        kb.op("dve", lambda e, s=s: e.tensor_scalar(out=lg[:, s, :], in0=lg[:, s, :], scalar1=top8[:, s, 1:2], scalar2=None, op0=ALU.is_ge),
                              reads=[("lg", s), ("top8", s)], writes=[("lg", s)])
                        kb.op("dve", lambda e, s=s, gi=gi: e.scalar_tensor_tensor(out=gts[:, gi, :], in0=gts[:, gi, :], scalar=gsc[:, s, 1:2], in1=lg[:, s, :],
                                                                                  op0=ALU.mult, op1=ALU.mult),
                              reads=[("gts", gi), ("gsc", s, 1), ("lg", s)], writes=[("gts", gi)])
        hkeys = lambda t: [(f"hA{t}", k) for k in range(8)]
        groups = [(0, 8), (8, 16), (16, 22)]
        wgu_i = 0
        wd_i = 0
        sil_i = 0
        for ex in range(E):
            if E > 1:
                gb = gB[ex % 2]
                gbk = f"gB{ex % 2}"
                for gi in range(16):
                    kb.op("dve", lambda e, gi=gi, ex=ex: e.tensor_scalar(out=dgate[:], in0=c.ident_f[:], scalar1=gts[:, gi, ex:ex + 1],
                                                                         scalar2=None, op0=ALU.mult),
                          reads=["ident_f", ("gts", gi)], writes=["dgate"])
                    p, pk = c.ps()
                    mm(c, p[:, 0:128], c.ones_f[:], dgate[:], True, True, ["ones_f", "dgate"], [pk])
                    kb.op("act", lambda e, gi=gi, p=p, gb=gb: e.activation(out=gb[:, gi * 128:(gi + 1) * 128], in_=p[:, 0:128], func=AF.Copy),
                          reads=[pk], writes=[(gbk, gi // 4)])
            for fa, fb in groups:
                nfg = fb - fa
                for f0 in range(fa, fb, 2):
                    nf = min(2, fb - f0)
                    sg, su = wgu[wgu_i % 3], wgu[(wgu_i + 1) % 3]
                    sgk, suk = f"wgu{wgu_i % 3}", f"wgu{(wgu_i + 1) % 3}"
                    wgu_i += 2
                    kb.dma("pool", sg[:, :, 0:nf * 128], io["w_gate"][ex][:, f0 * 128:(f0 + nf) * 128].rearrange("(k p) n -> p k n", p=128), writes=[sgk])
                    kb.dma("pool", su[:, :, 0:nf * 128], io["w_up"][ex][:, f0 * 128:(f0 + nf) * 128].rearrange("(k p) n -> p k n", p=128), writes=[suk])
                    for fi in range(nf):
                        fl = f0 + fi - fa
                        for t in range(NTT):
                            ts_ = slice(t * TT, (t + 1) * TT)
                            pg, pgk = c.ps()
                            pu, puk = c.ps()
                            for k in range(8):
                                mm(c, pg[:, :], sg[:, k, fi * 128:(fi + 1) * 128], hTall[:, k, ts_], k == 0, k == 7, [sgk, (f"hA{t}", k)], [pgk])
                            for k in range(8):
                                mm(c, pu[:, :], su[:, k, fi * 128:(fi + 1) * 128], hTall[:, k, ts_], k == 0, k == 7, [suk, (f"hA{t}", k)], [puk])
                            sl = sil[sil_i % 2]
                            slk = f"sil{sil_i % 2}"
                            sil_i += 1
                            kb.op("act", lambda e, pg=pg, sl=sl: e.activation(out=sl[:], in_=pg[:, :], func=AF.Silu), reads=[pgk], writes=[slk])
                            kb.op("dve", lambda e, pu=pu, sl=sl, fl=fl, ts_=ts_: e.tensor_tensor(out=actT[:, fl, ts_], in0=pu[:, :], in1=sl[:], op=ALU.mult),
                                  reads=[puk, slk], writes=[("actT", fl, t)])
                slots = []
                for fl in range(nfg):
                    f = fa + fl
                    wd = wdr[wd_i % 11]
                    wdk = f"wdr{wd_i % 11}"
                    wd_i += 1
                    kb.dma("pool", wd[:], io["w_down"][ex][f * 128:(f + 1) * 128, :], writes=[wdk])
                    slots.append((wd, wdk))
                for t in range(NTT):
                    ts_ = slice(t * TT, (t + 1) * TT)
                    for d in range(8):
                        p, pk = c.ps()
                        for fl in range(nfg):
                            wd, wdk = slots[fl]
                            mm(c, p[:, :], wd[:, d * 128:(d + 1) * 128], actT[:, fl, ts_], fl == 0, fl == nfg - 1, [wdk, ("actT", fl, t)], [pk])
                        if E > 1:
                            ytmp = ytmps[d % 2]
                            ytk = f"ytmp{d % 2}"
                            kb.op("dve", lambda e, p=p, gb=gb, ytmp=ytmp, ts_=ts_: e.tensor_tensor(out=ytmp[:], in0=p[:, :], in1=gb[:, ts_], op=ALU.mult),
                                  reads=[pk, (gbk, t)], writes=[ytk])
                            kb.op("dve", lambda e, d=d, ts_=ts_, ytmp=ytmp: e.tensor_tensor(out=xT[:, d, ts_], in0=xT[:, d, ts_], in1=ytmp[:], op=ALU.add),
                                  reads=[ytk, ("xT", d)], writes=[("xT", d)])
                        else:
                            kb.op("dve", lambda e, d=d, p=p, ts_=ts_: e.tensor_tensor(out=xT[:, d, ts_], in0=xT[:, d, ts_], in1=p[:, :], op=ALU.add),
                                  reads=[pk, ("xT", d)], writes=[("xT", d)])
    c.barrier()

    with ExitStack() as s4:
        hT = c.sb("hT", [128, 8, TT], BF16, s4)
        if not last:
            for t in range(NTT):
                t0 = t * TT
                rmsnorm_tile(c, xT, "xT", t0, TT, vecs[:, V_NEXT:V_NEXT + 8], tmp, hT, "hT")
                h_store(c, io["h_next"], hT, t0, TT, [("hT", k) for k in range(8)], ["h_next_d"])
                if t == NTT - 1 and "tail_next" in io:
                    kb.dma("sp", io["tail_next"].rearrange("(k p) n -> p k n", p=128), hT[:, :, TT - 32:TT],
                           reads=[("hT", k) for k in range(8)], writes=["tail_next_d"])
        else:
            hF2 = c.sb("hF2", [128, 8, TT], F32, s4)
            otm = c.sb("otm", [128, 4, D], F32, s4)
            for t in range(NTT):
                t0 = t * TT
                rmsnorm_tile(c, xT, "xT", t0, TT, vecs[:, V_NEXT:V_NEXT + 8], tmp, hT, "hT", out_f=hF2)
                for s in range(4):
                    for kk in range(2):
                        p, pk = c.ps()
                        for k4 in range(4):
                            k = kk * 4 + k4
                            kb.op("pe", lambda e, k=k, k4=k4, s=s, p=p: e.transpose(out=p[:, k4 * 128:(k4 + 1) * 128],
                                                                                    in_=hF2[:, k, s * 128:(s + 1) * 128], identity=c.ident_f[:]),
                                  reads=[("hT_f", k), "ident_f"], writes=[pk])
                        kb.op("act", lambda e, s=s, kk=kk, p=p: e.activation(out=otm[:, s, kk * 512:(kk + 1) * 512], in_=p[:, :], func=AF.Copy),
                              reads=[pk], writes=[("otm", s)])
                kb.dma("sp", io["out"][t0:t0 + TT, :].rearrange("(s p) d -> p s d", p=128), otm[:],
                       reads=[("otm", s) for s in range(4)])
    c.barrier()
    vec_st.close()


from contextlib import ExitStack as _ES
import ml_dtypes as _mld

NPBF = _mld.bfloat16


def build_T(E, last, dbg_stage=0):
    nc = bass.Bass("TRN2", target_bir_lowering=False)
    io = {}

    def din(name, shape, dt=F32):
        io[name] = nc.dram_tensor(name, shape, dt, kind="ExternalInput").ap()

    def dout(name, shape, dt=F32):
        io[name] = nc.dram_tensor(name, shape, dt, kind="ExternalOutput").ap()

    din("xT_in", [D, NT]); din("h_own", [D, NT], BF16); din("h_halo", [D, 32], BF16)
    din("catm", [4, 64, NT], BF16); din("catf", [4, 128, NT], BF16); din("mem", [256, D])
    din("vecs", [128, NV_T]); din("w_c", [D, 512]); din("w_out", [D, D]); din("w_q", [D, 512])
    din("w_kv", [D, D]); din("w_o", [512, D])
    din("w_gate", [E, D, DFF]); din("w_up", [E, D, DFF]); din("w_down", [E, DFF, D])
    if E > 1:
        din("router_w", [D, 8])
    if last:
        dout("out", [NT, D])
    else:
        dout("xT_out", [D, NT]); dout("h_next", [D, NT], BF16)
    io["dbg_stage"] = dbg_stage
    with _ES() as st:
        c = Ctx(nc, st)
        c.setup()
        xT = c.sb("xT", [128, 8, NT], F32)
        c.kb.dma("sp", xT[:], io["xT_in"].rearrange("(k p) n -> p k n", p=128), writes=[("xT", k) for k in range(8)])
        phase_T(c, io, E, last and not dbg_stage, xT)
        evs = []
        if not last or dbg_stage:
            key = "xT_out" if not last else "out"
            if last:
                io["xT_dbg"] = None
            evs.append(c.kb.dma("sp", io["xT_out"].rearrange("(k p) n -> p k n", p=128), xT[:],
                                reads=[("xT", k) for k in range(8)]))
        c.barrier()
        c.kb.flush()
    return nc


def vecs_T(inp, l, last):
    v = np.zeros((128, NV_T), np.float32)
    fm = lambda w: np.asarray(w, np.float32).reshape(-1, 128).T
    v[:, 0:8] = fm(inp["norm_xattn_w"][l]); v[:, 8:16] = fm(inp["norm_mem_w"][l]); v[:, 16:24] = fm(inp["norm_ffn_w"][l])
    v[:, 24:32] = fm(inp["norm_final_w"]) if last else fm(inp["norm_mix_w"][l + 1])
    v[:, 32:34] = fm(inp["conf_conv_b"][l]); v[:, 34:36] = fm(inp["conf_ln_w"][l]); v[:, 36:38] = fm(inp["conf_ln_b"][l])
    cw = np.asarray(inp["conf_conv_w"][l], np.float32)
    for j in range(31):
        v[:, 38 + 2 * j:40 + 2 * j] = fm(cw[j])
    return v


NCH = SEQ // 64
NKT = SEQ // 128
NQT = SEQ // TT
GRP = 4


def AP3(t, off, dims):
    return bass.AP(t[:].tensor, off, [list(d) for d in dims])


def log_sigmoid_tile(c, x, out, tmp1, tmp2, bias_ap, keys):
    kb = c.kb
    kx, ko, k1, k2 = keys
    kb.op("dve", lambda e: e.tensor_scalar(out=x, in0=x, scalar1=bias_ap, scalar2=None, op0=ALU.add), reads=[kx, "vecsM"], writes=[kx])
    kb.op("dve", lambda e: e.tensor_scalar(out=tmp1, in0=x, scalar1=-1.0, scalar2=None, op0=ALU.mult), reads=[kx], writes=[k1])
    kb.op("dve", lambda e: e.tensor_tensor(out=tmp1, in0=tmp1, in1=x, op=ALU.max), reads=[kx, k1], writes=[k1])
    kb.op("act", lambda e: e.activation(out=tmp1, in_=tmp1, func=AF.Exp, scale=-1.0), reads=[k1], writes=[k1])
    kb.op("dve", lambda e: e.tensor_scalar(out=tmp1, in0=tmp1, scalar1=1.0, scalar2=None, op0=ALU.add), reads=[k1], writes=[k1])
    kb.op("act", lambda e: e.activation(out=tmp1, in_=tmp1, func=AF.Ln), reads=[k1], writes=[k1])
    kb.op("dve", lambda e: e.tensor_scalar(out=tmp2, in0=x, scalar1=0.0, scalar2=None, op0=ALU.min), reads=[kx], writes=[k2])
    kb.op("dve", lambda e: e.tensor_tensor(out=out, in0=tmp2, in1=tmp1, op=ALU.subtract), reads=[k1, k2], writes=[ko])


def phase_M_mlstm(c, io):
    nc, kb = c.nc, c.kb
    from contextlib import ExitStack
    with ExitStack() as s0:
        vm = c.sb("vecsM_sb", [128, 16], F32, s0)
        wml = c.sb("wml", [128, 8, 258], BF16, s0)
        qT = c.sb("m_qT", [64, SEQ], BF16, s0)
        kT = c.sb("m_kT", [64, SEQ], BF16, s0)
        Vaug = c.sb("m_Vaug", [64, NCH, 65], BF16, s0)
        og = c.sb("m_og", [64, SEQ], BF16, s0)
        iC = c.sb("m_iC", [128, 64], F32, s0)
        fC = c.sb("m_fC", [128, 64], F32, s0)
        eps_t = c.sb("m_eps", [128, 1], F32, s0)
        kb.dma("sp", vm[:], io["vecsM"], writes=["vecsM"])
        kb.dma("pool", wml[:], io["w_ml"].rearrange("(k p) n -> p k n", p=128), writes=["wml"])
        kb.op("pool", lambda e: e.memset(Vaug[:, :, 64:65], 1.0), writes=["Vaug1"])
        kb.op("pool", lambda e: e.memset(eps_t[:], EPS), writes=["m_eps"])
        with ExitStack() as s1:
            hTb = [c.sb(f"m_hT{i}", [128, 8, TT], BF16, s1) for i in range(2)]
            zq = c.sb("m_zq", [64, TT + 3], F32, s1)
            zk = c.sb("m_zk", [64, TT + 3], F32, s1)
            cacc = [c.sb(f"m_cacc{i}", [64, TT], F32, s1) for i in range(2)]
            vt = c.sb("m_vt", [64, TT], F32, s1)
            rows = [c.sb(f"m_rows{i}", [2, TT], F32, s1) for i in range(2)]
            kb.op("pool", lambda e: e.memset(zq[:, 0:3], 0.0), writes=["zq"])
            kb.op("pool", lambda e: e.memset(zk[:, 0:3], 0.0), writes=["zk"])
            def m_load(tt):
                h_load(c, io["hT_all"], hTb[tt % 2], (tt % 4) * TT, TT, [f"m_hT{tt % 2}"], j=tt // 4)

            m_load(0)
            for tt in range(NQT):
                if tt + 1 < NQT:
                    m_load(tt + 1)
                hT = hTb[tt % 2]
                hk = f"m_hT{tt % 2}"
                tok = slice(tt * TT, (tt + 1) * TT)

                def proj(c0, c1, np_):
                    p, pk = c.ps()
                    for k in range(8):
                        mm(c, p[0:np_, :], wml[:, k, c0:c1], hT[:, k, :], k == 0, k == 7, ["wml", hk], [pk])
                    return p, pk
                pq, pqk = proj(0, 64, 64)
                pkk, pkkk = proj(64, 128, 64)
                pv, pvk = proj(128, 192, 64)
                pog, pogk = proj(192, 256, 64)
                pif, pifk = proj(256, 258, 2)
                rw = rows[tt % 2]
                rk = f"m_rows{tt % 2}"
                kb.op("act", lambda e, p=pq: e.activation(out=zq[:, 3:TT + 3], in_=p[0:64, :], func=AF.Copy), reads=[pqk], writes=["zq"])
                kb.op("act", lambda e, p=pkk: e.activation(out=zk[:, 3:TT + 3], in_=p[0:64, :], func=AF.Copy), reads=[pkkk], writes=["zk"])
                kb.op("act", lambda e, p=pv: e.activation(out=vt[:], in_=p[0:64, :], func=AF.Copy), reads=[pvk], writes=["m_vt"])
                kb.op("act", lambda e, p=pog, tok=tok: e.activation(out=og[:, tok], in_=p[0:64, :], func=AF.Sigmoid), reads=[pogk], writes=[("og", tt)])
                kb.op("act", lambda e, p=pif, rw=rw: e.activation(out=rw[:], in_=p[0:2, :], func=AF.Copy), reads=[pifk], writes=[rk])
                p2, p2k = c.ps()
                for ci in range(8):
                    kb.op("pe", lambda e, ci=ci, p2=p2: e.transpose(out=p2[0:64, ci * 64:(ci + 1) * 64], in_=vt[:, ci * 64:(ci + 1) * 64], identity=c.ident_f[0:64, 0:64]),
                          reads=["m_vt", "ident_f"], writes=[p2k])
                kb.op("dve", lambda e, p2=p2, tt=tt: e.tensor_copy(out=Vaug[:, tt * 8:(tt + 1) * 8, 0:64], in_=p2[0:64, :].rearrange("p (c d) -> p c d", d=64)),
                      reads=[p2k], writes=[("Vaug", tt)])
                for nm, z, vc, dst in (("q", zq, 0, qT), ("k", zk, 5, kT)):
                    zkey = "z" + nm
                    ca = cacc[0 if nm == "q" else 1]
                    ck = "cacc" + nm
                    kb.op("dve", lambda e, z=z, ca=ca, vc=vc: e.tensor_scalar(out=ca[:], in0=z[:, 0:TT], scalar1=vm[0:64, vc:vc + 1], scalar2=vm[0:64, vc + 4:vc + 5],
                                                                              op0=ALU.mult, op1=ALU.add), reads=[zkey, "vecsM"], writes=[ck])
                    for jj in range(1, 4):
                        kb.op("dve", lambda e, z=z, ca=ca, vc=vc, jj=jj: e.scalar_tensor_tensor(out=ca[:], in0=z[:, jj:jj + TT], scalar=vm[0:64, vc + jj:vc + jj + 1], in1=ca[:],
                                                                                                 op0=ALU.mult, op1=ALU.add), reads=[zkey, ck, "vecsM"], writes=[ck])
                    kb.op("act", lambda e, ca=ca, dst=dst, tok=tok: e.activation(out=dst[:, tok], in_=ca[:], func=AF.Silu), reads=[ck], writes=[("m_" + nm + "T", tt)])
                    kb.op("dve", lambda e, z=z: e.tensor_copy(out=z[:, 0:3], in_=z[:, TT:TT + 3]), reads=[zkey], writes=[zkey])
                kb.dma("sp", iC[tt * 8:(tt + 1) * 8, :], AP3(rw, 0, [[TT, 1], [64, 8], [1, 64]]), reads=[rk], writes=["iC"])
                kb.dma("sp", fC[tt * 8:(tt + 1) * 8, :], AP3(rw, TT, [[TT, 1], [64, 8], [1, 64]]), reads=[rk], writes=["fC"])
        c.barrier()
        Uall = c.sb("m_Uall", [64, 65, NCH], F32, s0)
        wgT = c.sb("m_wgT", [64, NCH], F32, s0)
        flT = c.sb("m_flT", [64, NCH], F32, s0)
        dB = c.sb("m_dB", [64, NCH], F32, s0)
        dB0 = c.sb("m_dB0", [64, NCH], F32, s0)
        with ExitStack() as s2:
            t1 = c.sb("g_t1", [128, 64], F32, s2)
            t2 = c.sb("g_t2", [128, 64], F32, s2)
            lf = c.sb("g_lf", [128, 64], F32, s2)
            bb = c.sb("g_b", [128, 64], F32, s2)
            aa = c.sb("g_a", [128, 64], F32, s2)
            AA = c.sb("g_A", [128, 64], F32, s2)
            MM = c.sb("g_M", [128, 64], F32, s2)
            wg = c.sb("g_wg", [128, 64], F32, s2)
            fl = c.sb("g_fl", [128, 64], F32, s2)
            on = c.sb("g_on", [128, 64], F32, s2)
            r1 = c.sb("g_r1", [1, 128], F32, s2)
            r2 = c.sb("g_r2", [1, 128], F32, s2)
            r3 = c.sb("g_r3", [1, 128], F32, s2)
            r4 = c.sb("g_r4", [1, 128], F32, s2)
            mcol = c.sb("g_mcol", [128, 1], F32, s2)
            nM63 = c.sb("g_nM63", [128, 1], F32, s2)
            dec = c.sb("g_dec", [128, 1], F32, s2)
            dgd = c.sb("g_dgd", [128, 128], F32, s2)
            Xb = [c.sb(f"g_X{i}", [128, 8, 64], F32, s2) for i in range(2)]
            kw32 = [c.sb(f"g_kw32{i}", [64, TT], F32, s2) for i in range(2)]
            kwTok = c.sb("g_kwTok", [64, NCH, 64], BF16, s2)
            log_sigmoid_tile(c, fC[:], lf[:], t1[:], t2[:], vm[:, 12:13], ("fC", "g_lf", "g_t1", "g_t2"))
            kb.op("dve", lambda e: e.tensor_scalar(out=iC[:], in0=iC[:], scalar1=vm[:, 11:12], scalar2=None, op0=ALU.add), reads=["iC", "vecsM"], writes=["iC"])
            kb.op("pool", lambda e: e.memset(on[:], 1.0), writes=["g_on"])
            kb.op("dve", lambda e: e.tensor_tensor_scan(out=bb[:], data0=on[:], data1=lf[:], initial=0.0, op0=ALU.mult, op1=ALU.add),
                  reads=["g_on", "g_lf"], writes=["g_b"])
            kb.op("dve", lambda e: e.tensor_tensor(out=aa[:], in0=iC[:], in1=bb[:], op=ALU.subtract), reads=["iC", "g_b"], writes=["g_a"])
            kb.op("dve", lambda e: e.tensor_tensor_scan(out=AA[:], data0=aa[:], data1=aa[:], initial=-1e30, op0=ALU.max, op1=ALU.max),
                  reads=["g_a"], writes=["g_A"])
            p, pk = c.ps()
            kb.op("pe", lambda e, p=p: e.transpose(out=p[0:1, 0:128], in_=AA[:, 63:64], identity=c.ident_f[:]), reads=["g_A", "ident_f"], writes=[pk])
            kb.op("pe", lambda e, p=p: e.transpose(out=p[0:1, 128:256], in_=bb[:, 63:64], identity=c.ident_f[:]), reads=["g_b", "ident_f"], writes=[pk])
            kb.op("dve", lambda e, p=p: e.tensor_copy(out=r1[:], in_=p[0:1, 0:128]), reads=[pk], writes=["g_r1"])
            kb.op("dve", lambda e, p=p: e.tensor_copy(out=r2[:], in_=p[0:1, 128:256]), reads=[pk], writes=["g_r2"])
            kb.op("dve", lambda e: e.tensor_tensor_scan(out=r3[:], data0=r1[:], data1=r2[:], initial=0.0, op0=ALU.max, op1=ALU.add),
                  reads=["g_r1", "g_r2"], writes=["g_r3"])
            kb.op("pool", lambda e: e.memset(r4[:, 0:1], 0.0), writes=["g_r4a"])
            kb.op("dve", lambda e: e.tensor_copy(out=r4[:, 1:128], in_=r3[:, 0:127]), reads=["g_r3"], writes=["g_r4b"])
            p, pk = c.ps()
            kb.op("pe", lambda e, p=p: e.transpose(out=p[:, 0:1], in_=r4[:], identity=c.ident_f[0:1, 0:1]), reads=["g_r4a", "g_r4b", "ident_f"], writes=[pk])
            kb.op("dve", lambda e, p=p: e.tensor_copy(out=mcol[:], in_=p[:, 0:1]), reads=[pk], writes=["g_mcol"])
            kb.op("dve", lambda e: e.tensor_scalar(out=MM[:], in0=AA[:], scalar1=mcol[:, 0:1], scalar2=None, op0=ALU.max), reads=["g_A", "g_mcol"], writes=["g_M"])
            kb.op("dve", lambda e: e.tensor_scalar(out=nM63[:], in0=MM[:, 63:64], scalar1=-1.0, scalar2=None, op0=ALU.mult), reads=["g_M"], writes=["g_nM63"])
            kb.op("act", lambda e: e.activation(out=wg[:], in_=aa[:], func=AF.Exp, bias=nM63[:, 0:1]), reads=["g_a", "g_nM63"], writes=["g_wg"])
            kb.op("act", lambda e: e.activation(out=dec[:], in_=mcol[:], func=AF.Exp, bias=nM63[:, 0:1]), reads=["g_mcol", "g_nM63"], writes=["g_dec"])
            kb.op("act", lambda e: e.activation(out=fl[:], in_=bb[:], func=AF.Exp, scale=-1.0, bias=nM63[:, 0:1]), reads=["g_b", "g_nM63"], writes=["g_fl"])
            p, pk = c.ps()
            kb.op("pe", lambda e, p=p: e.transpose(out=p[0:64, 0:128], in_=wg[:], identity=c.ident_f[:]), reads=["g_wg", "ident_f"], writes=[pk])
            kb.op("pe", lambda e, p=p: e.transpose(out=p[0:64, 128:256], in_=fl[:], identity=c.ident_f[:]), reads=["g_fl", "ident_f"], writes=[pk])
            kb.op("dve", lambda e, p=p: e.tensor_copy(out=wgT[:], in_=p[0:64, 0:128]), reads=[pk], writes=["m_wgT"])
            kb.op("dve", lambda e, p=p: e.tensor_copy(out=flT[:], in_=p[0:64, 128:256]), reads=[pk], writes=["m_flT"])
            kb.op("dve", lambda e: e.tensor_scalar(out=dgd[:], in0=c.ident_f[:], scalar1=dec[:, 0:1], scalar2=None, op0=ALU.mult), reads=["ident_f", "g_dec"], writes=["g_dgd"])
            p, pk = c.ps()
            mm(c, p[0:64, 0:128], c.ones_f[:, 0:64], dgd[:], True, True, ["ones_f", "g_dgd"], [pk])
            kb.op("dve", lambda e, p=p: e.tensor_copy(out=dB[:], in_=p[0:64, 0:128]), reads=[pk], writes=["m_dB"])
            kb.op("dve", lambda e, p=p: e.tensor_copy(out=dB0[:], in_=p[0:64, 0:128]), reads=[pk], writes=["m_dB0"])
            kb.op("pool", lambda e: e.memset(dB0[:, 0:1], 0.0), reads=["m_dB0"], writes=["m_dB0"])
            for tt in range(NQT):
                tok = slice(tt * TT, (tt + 1) * TT)
                X = Xb[tt % 2]
                Xk = f"g_X{tt % 2}"
                kb.op("dve", lambda e, X=X, tt=tt: e.tensor_tensor(out=X[:], in0=AP3(c.ident_f, 8 * tt, [[128, 128], [1, 8], [0, 64]]),
                                                                   in1=AP3(wg, 0, [[64, 128], [0, 8], [1, 64]]), op=ALU.mult),
                      reads=["ident_f", "g_wg"], writes=[Xk])
                p, pk = c.ps()
                mm(c, p[0:64, :], c.ones_f[:, 0:64], X[:].rearrange("p c s -> p (c s)"), True, True, ["ones_f", Xk], [pk])
                k32 = kw32[tt % 2]
                k32k = f"g_kw32{tt % 2}"
                kb.op("dve", lambda e, p=p, k32=k32, tok=tok: e.scalar_tensor_tensor(out=k32[:], in0=kT[:, tok], scalar=0.125, in1=p[0:64, :], op0=ALU.mult, op1=ALU.mult),
                      reads=[pk, ("m_kT", tt)], writes=[k32k])
                kb.op("act", lambda e, k32=k32, tok=tok: e.activation(out=kT[:, tok], in_=k32[:], func=AF.Copy), reads=[k32k], writes=[("m_kT", tt)])
                p2, p2k = c.ps()
                for ci in range(8):
                    kb.op("pe", lambda e, ci=ci, p2=p2, k32=k32: e.transpose(out=p2[0:64, ci * 64:(ci + 1) * 64], in_=k32[:, ci * 64:(ci + 1) * 64], identity=c.ident_f[0:64, 0:64]),
                          reads=[k32k, "ident_f"], writes=[p2k])
                kb.op("dve", lambda e, p2=p2, tt=tt: e.tensor_copy(out=kwTok[:, tt * 8:(tt + 1) * 8, :], in_=p2[0:64, :].rearrange("p (c d) -> p c d", d=64)),
                      reads=[p2k], writes=[("kwTok", tt)])
            for g in range(NCH // GRP):
                p, pk = c.ps()
                for ci in range(GRP):
                    ch = g * GRP + ci
                    mm(c, p[0:64, ci * 65:(ci + 1) * 65], kwTok[:, ch, :], Vaug[:, ch, :], True, True, [("kwTok", ch // 8), ("Vaug", ch // 8), "Vaug1"], [pk])
                kb.op("dve", lambda e, p=p, g=g: e.tensor_copy(out=AP3(Uall, g * GRP, [[65 * NCH, 64], [1, GRP], [NCH, 65]]),
                                                               in_=p[0:64, 0:GRP * 65].rearrange("p (c d) -> p c d", d=65)),
                      reads=[pk], writes=["Uall"])
        c.barrier()
        Cn = Uall
        Eb = c.sb("m_E", [64, NCH, 65], BF16, s0)
        for dv in range(65):
            kb.op("dve", lambda e, dv=dv: e.tensor_tensor_scan(out=Cn[:, dv, :], data0=dB0[:], data1=Uall[:, dv, :], initial=0.0, op0=ALU.mult, op1=ALU.add),
                  reads=["m_dB0", "Uall"], writes=[("Cn", dv), "Uall"])
        kb.op("pool", lambda e: e.memset(Eb[:, 0:1, :], 0.0), writes=["E0"])
        kb.op("dve", lambda e: e.tensor_tensor(out=Eb[:, 1:NCH, :], in0=AP3(Cn, 0, [[65 * NCH, 64], [1, NCH - 1], [NCH, 65]]),
                                               in1=AP3(dB, 1, [[NCH, 64], [1, NCH - 1], [0, 65]]), op=ALU.mult),
              reads=[("Cn", dv) for dv in range(65)] + ["m_dB"], writes=["E"])
        with ExitStack() as s4:
            mask = c.sb("o_mask", [64, 64], F32, s4)
            sT = [c.sb(f"o_sT{i}", [64, GRP * 64], BF16, s4) for i in range(2)]
            den = c.sb("o_den", [64, GRP], F32, s4)
            hn = c.sb("o_hn", [64, GRP, 64], F32, s4)
            hsq = c.sb("o_hsq", [64, GRP, 64], F32, s4)
            ss = c.sb("o_ss", [64, GRP], F32, s4)
            cst = [c.sb(f"o_cst{i}", [64, GRP * 64], BF16, s4) for i in range(2)]
            kb.op("pool", lambda e: e.memset(mask[:], 1.0), writes=["o_mask"])
            kb.op("pool", lambda e: e.affine_select(out=mask[:], in_=mask[:], pattern=[[1, 64]], compare_op=ALU.is_ge, fill=0.0, base=0, channel_multiplier=-1),
                  reads=["o_mask"], writes=["o_mask"])
            pend = {}

            def a4_stage1(g):
                c0 = g * GRP
                p, pk = c.ps()
                for ci in range(GRP):
                    ch = c0 + ci
                    cs = slice(ch * 64, (ch + 1) * 64)
                    mm(c, p[0:64, ci * 64:(ci + 1) * 64], kT[:, cs], qT[:, cs], True, True, [("m_kT", ch // 8), ("m_qT", ch // 8)], [pk])
                st_ = sT[g % 2]
                stk = f"o_sT{g % 2}"
                kb.op("dve", lambda e, p=p, st_=st_: e.tensor_tensor(out=st_[:].rearrange("p (c t) -> p c t", t=64), in0=p[0:64, 0:GRP * 64].rearrange("p (c t) -> p c t", t=64),
                                                                     in1=AP3(mask, 0, [[64, 64], [0, GRP], [1, 64]]), op=ALU.mult),
                      reads=[pk, "o_mask"], writes=[stk])
                po, pok = c.ps()
                for ci in range(GRP):
                    ch = c0 + ci
                    cs = slice(ch * 64, (ch + 1) * 64)
                    mm(c, po[0:64, ci * 65:(ci + 1) * 65], st_[:, ci * 64:(ci + 1) * 64], Vaug[:, ch, :], True, False, [stk, ("Vaug", ch // 8), "Vaug1"], [pok])
                    mm(c, po[0:64, ci * 65:(ci + 1) * 65], qT[:, cs], Eb[:, ch, :], False, True, [("m_qT", ch // 8), "E", "E0"], [pok])
                pend[g] = (po, pok)

            def a4_stage2(g):
                c0 = g * GRP
                po, pok = pend.pop(g)
                po3 = po[0:64, 0:GRP * 65].rearrange("p (c d) -> p c d", d=65)
                den3 = den[:].rearrange("p (c o) -> p c o", o=1)
                kb.op("dve", lambda e, po3=po3, den3=den3: e.tensor_scalar(out=den3, in0=po3[:, :, 64:65], scalar1=-1.0, scalar2=None, op0=ALU.mult),
                      reads=[pok], writes=["o_den"])
                kb.op("dve", lambda e, po3=po3, den3=den3: e.tensor_tensor(out=den3, in0=po3[:, :, 64:65], in1=den3, op=ALU.max),
                      reads=[pok, "o_den"], writes=["o_den"])
                kb.op("dve", lambda e, c0=c0: e.tensor_tensor(out=den[:], in0=den[:], in1=flT[:, c0:c0 + GRP], op=ALU.max),
                      reads=["o_den", "m_flT"], writes=["o_den"])
                kb.op("dve", lambda e: e.reciprocal(out=den[:], in_=den[:]), reads=["o_den"], writes=["o_den"])
                kb.op("dve", lambda e, po3=po3: e.tensor_tensor(out=hn[:], in0=po3[:, :, 0:64], in1=AP3(den, 0, [[GRP, 64], [1, GRP], [0, 64]]), op=ALU.mult),
                      reads=[pok, "o_den"], writes=["o_hn"])
                kb.op("act", lambda e: e.activation(out=hsq[:], in_=hn[:], func=AF.Square), reads=["o_hn"], writes=["o_hsq"])
                kb.op("dve", lambda e: e.tensor_reduce(out=ss[:], in_=hsq[:], axis=AX.X, op=ALU.add), reads=["o_hsq"], writes=["o_ss"])
                kb.op("act", lambda e: e.activation(out=ss[:], in_=ss[:], func=AF.Sqrt, scale=1.0 / 64, bias=eps_t[0:64, 0:1]), reads=["o_ss", "m_eps"], writes=["o_ss"])
                kb.op("dve", lambda e: e.reciprocal(out=ss[:], in_=ss[:]), reads=["o_ss"], writes=["o_ss"])
                kb.op("dve", lambda e: e.tensor_tensor(out=hn[:], in0=hn[:], in1=AP3(ss, 0, [[GRP, 64], [1, GRP], [0, 64]]), op=ALU.mult),
                      reads=["o_hn", "o_ss"], writes=["o_hn"])
                pt, ptk = c.ps()
                for ci in range(GRP):
                    kb.op("pe", lambda e, ci=ci, pt=pt: e.transpose(out=pt[0:64, ci * 64:(ci + 1) * 64], in_=hn[:, ci, :], identity=c.ident_f[0:64, 0:64]),
                          reads=["o_hn", "ident_f"], writes=[ptk])
                cs_ = cst[g % 2]
                csk = f"o_cst{g % 2}"
                toks = slice(c0 * 64, (c0 + GRP) * 64)
                kb.op("dve", lambda e, pt=pt, cs_=cs_, toks=toks: e.scalar_tensor_tensor(out=cs_[:], in0=pt[0:64, 0:GRP * 64], scalar=vm[0:64, 10:11], in1=og[:, toks],
                                                                                       op0=ALU.mult, op1=ALU.mult),
                      reads=[ptk, "vecsM", ("og", (c0 * 64) // TT)], writes=[csk])
                kb.dma("sp", io["catm_out"][:, toks], cs_[:], reads=[csk])

            NG4 = NCH // GRP
            a4_stage1(0)
            for g in range(NG4):
                if g + 1 < NG4:
                    a4_stage1(g + 1)
                a4_stage2(g)
        c.barrier()


def phase_M_fox(c, io):
    nc, kb = c.nc, c.kb
    from contextlib import ExitStack
    NEG = -30000.0
    with ExitStack() as s0:
        vm = c.sb("vecsMf_sb", [128, 16], F32, s0)
        wfx = c.sb("wfx", [128, 8, 386], BF16, s0)
        fq = [c.sb(f"f_q{h}", [128, SEQ], BF16, s0) for h in range(2)]
        fkk = c.sb("f_kk", [128, SEQ], BF16, s0)
        kb.op("pool", lambda e: e.memset(fq[0][64:128, :], 0.0), writes=[("fqz", 0)])
        kb.op("pool", lambda e: e.memset(fq[1][0:64, :], 0.0), writes=[("fqz", 1)])
        fV = [c.sb(f"f_V{h}", [128, NKT, 65], BF16, s0) for h in range(2)]
        fC = [c.sb(f"f_C{h}", [64, 128], F32, s0) for h in range(2)]
        kb.dma("sp", vm[:], io["vecsM"], writes=["vecsM"])
        kb.dma("pool", wfx[:], io["w_fx"].rearrange("(k p) n -> p k n", p=128), writes=["wfx"])
        for h in range(2):
            kb.op("pool", lambda e, h=h: e.memset(fV[h][:, :, 64:65], 1.0), writes=[("fV1", h)])
        with ExitStack() as s1:
            hTb = [c.sb(f"f_hT{i}", [128, 8, TT], BF16, s1) for i in range(2)]
            vt = c.sb("f_vt", [128, TT], F32, s1)
            rows = [c.sb(f"f_rows{i}", [2, TT], F32, s1) for i in range(2)]
            def f_load(tt):
                h_load(c, io["hT_all"], hTb[tt % 2], (tt % 4) * TT, TT, [f"f_hT{tt % 2}"], j=tt // 4)

            f_load(0)
            for tt in range(NQT):
                if tt + 1 < NQT:
                    f_load(tt + 1)
                hT = hTb[tt % 2]
                hk = f"f_hT{tt % 2}"
                tok = slice(tt * TT, (tt + 1) * TT)
                p, pk = c.ps()
                for k in range(8):
                    mm(c, p[:, :], wfx[:, k, 0:128], hT[:, k, :], k == 0, k == 7, ["wfx", hk], [pk])
                kb.op("act", lambda e, p=p, tok=tok: e.activation(out=fq[0][0:64, tok], in_=p[0:64, :], func=AF.Copy, scale=0.125), reads=[pk], writes=[("fq", 0, tt)])
                kb.op("act", lambda e, p=p, tok=tok: e.activation(out=fq[1][64:128, tok], in_=p[64:128, :], func=AF.Copy, scale=0.125), reads=[pk], writes=[("fq", 1, tt)])
                p, pk = c.ps()
                for k in range(8):
                    mm(c, p[:, :], wfx[:, k, 128:256], hT[:, k, :], k == 0, k == 7, ["wfx", hk], [pk])
                kb.op("act", lambda e, p=p, tok=tok: e.activation(out=fkk[:, tok], in_=p[:, :], func=AF.Copy), reads=[pk], writes=[("fk", tt)])
                p, pk = c.ps()
                for k in range(8):
                    mm(c, p[:, :], wfx[:, k, 256:384], hT[:, k, :], k == 0, k == 7, ["wfx", hk], [pk])
                kb.op("act", lambda e, p=p: e.activation(out=vt[:], in_=p[:, :], func=AF.Copy), reads=[pk], writes=["f_vt"])
                p2, p2k = c.ps()
                for ci in range(4):
                    kb.op("pe", lambda e, ci=ci, p2=p2: e.transpose(out=p2[:, ci * 128:(ci + 1) * 128], in_=vt[:, ci * 128:(ci + 1) * 128], identity=c.ident_f[:]),
                          reads=["f_vt", "ident_f"], writes=[p2k])
                for h in range(2):
                    kb.op("dve", lambda e, p2=p2, tt=tt, h=h: e.tensor_copy(out=fV[h][:, tt * 4:(tt + 1) * 4, 0:64],
                                                                         in_=p2[:, :].rearrange("p (c d) -> p c d", d=128)[:, :, h * 64:(h + 1) * 64]),
                          reads=[p2k], writes=[("fV", h, tt)])
                p, pk = c.ps()
                for k in range(8):
                    mm(c, p[0:2, :], wfx[:, k, 384:386], hT[:, k, :], k == 0, k == 7, ["wfx", hk], [pk])
                rw = rows[tt % 2]
                rk = f"f_rows{tt % 2}"
                kb.op("act", lambda e, p=p, rw=rw: e.activation(out=rw[:], in_=p[0:2, :], func=AF.Copy), reads=[pk], writes=[rk])
                for h in range(2):
                    kb.dma("sp", fC[h][tt * 4:(tt + 1) * 4, :], AP3(rw, h * TT, [[TT, 1], [128, 4], [1, 128]]), reads=[rk], writes=[("fC", h)])
        c.barrier()
        ckT = [c.sb(f"f_ckT{h}", [128, NKT], F32, s0) for h in range(2)]
        cC = [c.sb(f"f_cC{h}", [64, 128], F32, s0) for h in range(2)]
        negm = c.sb("f_negm", [128, 4, TT], F32, s0)
        Ls = c.sb("f_Ls", [64, 64], F32, s0)
        with ExitStack() as s2:
            t1 = c.sb("f_t1", [64, 128], F32, s2)
            t2 = c.sb("f_t2", [64, 128], F32, s2)
            lf = c.sb("f_lf", [64, 128], F32, s2)
            on = c.sb("f_on", [64, 128], F32, s2)
            pre = c.sb("f_pre", [64, 1], F32, s2)
            kb.op("pool", lambda e: e.memset(on[:], 1.0), writes=["f_on"])
            kb.op("pool", lambda e: e.memset(Ls[:], 1.0), writes=["f_Ls"])
            kb.op("pool", lambda e: e.affine_select(out=Ls[:], in_=Ls[:], pattern=[[1, 64]], compare_op=ALU.is_ge, fill=0.0, base=-1, channel_multiplier=-1),
                  reads=["f_Ls"], writes=["f_Ls"])
            for r in range(4):
                kb.op("pool", lambda e, r=r: e.memset(negm[:, r, :], 0.0), writes=[("negm", r)])
                kb.op("pool", lambda e, r=r: e.affine_select(out=negm[:, r, :], in_=negm[:, r, :], pattern=[[1, TT]], compare_op=ALU.is_ge, fill=NEG,
                                                             base=-128 * r, channel_multiplier=-1), reads=[("negm", r)], writes=[("negm", r)])
            for h in range(2):
                log_sigmoid_tile(c, fC[h][:], lf[:], t1[:], t2[:], vm[0:64, 13 + h:14 + h], (("fC", h), "f_lf", "f_t1", "f_t2"))
                kb.op("dve", lambda e, h=h: e.tensor_tensor_scan(out=cC[h][:], data0=on[:], data1=lf[:], initial=0.0, op0=ALU.mult, op1=ALU.add),
                      reads=["f_on", "f_lf"], writes=[("cC", h)])
                p, pk = c.ps()
                mm(c, p[0:64, 0:1], Ls[:], cC[h][:, 127:128], True, True, ["f_Ls", ("cC", h)], [pk])
                kb.op("dve", lambda e, p=p: e.tensor_copy(out=pre[:], in_=p[0:64, 0:1]), reads=[pk], writes=["f_pre"])
                kb.op("dve", lambda e, h=h: e.tensor_scalar(out=cC[h][:], in0=cC[h][:], scalar1=pre[:, 0:1], scalar2=None, op0=ALU.add), reads=[("cC", h), "f_pre"], writes=[("cC", h)])
                p, pk = c.ps()
                kb.op("pe", lambda e, p=p, h=h: e.transpose(out=p[:, 0:64], in_=cC[h][:], identity=c.ident_f[0:64, 0:64]), reads=[("cC", h), "ident_f"], writes=[pk])
                kb.op("dve", lambda e, p=p, h=h: e.tensor_scalar(out=ckT[h][:], in0=p[:, 0:64], scalar1=-1.0, scalar2=None, op0=ALU.mult), reads=[pk], writes=[("ckT", h)])
        c.barrier()
        with ExitStack() as s3:
            X = c.sb("f_X", [64, 4, 128], F32, s3)
            cqB = c.sb("f_cqB", [128, TT], F32, s3)
            cqD = c.sb("f_cqD", [128, 4, TT], F32, s3)
            NB = 5
            tmpb = [c.sb(f"f_tmp{i}", [128, TT], F32, s3) for i in range(NB)]
            pTb = [c.sb(f"f_pT{i}", [128, TT], BF16, s3) for i in range(NB)]
            osb = c.sb("f_osb", [65, TT], F32, s3)
            rden = c.sb("f_rden", [64, TT], F32, s3)
            outb = [c.sb(f"f_out{i}", [64, TT], BF16, s3) for i in range(2)]
            it = 0
            for h in range(2):
                for qi in range(NQT):
                    qs = slice(qi * TT, (qi + 1) * TT)
                    kb.op("dve", lambda e, h=h, qi=qi: e.tensor_tensor(out=X[:], in0=AP3(c.ident_f, 4 * qi, [[128, 64], [1, 4], [0, 128]]),
                                                                       in1=AP3(cC[h], 0, [[128, 64], [0, 4], [1, 128]]), op=ALU.mult),
                          reads=["ident_f", ("cC", h)], writes=["f_X"])
                    p, pk = c.ps()
                    mm(c, p[:, :], c.ones_f[0:64, :], X[:].rearrange("p r s -> p (r s)"), True, True, ["ones_f", "f_X"], [pk])
                    kb.op("act", lambda e, p=p: e.activation(out=cqB[:], in_=p[:, :], func=AF.Copy), reads=[pk], writes=["f_cqB"])
                    for r in range(4):
                        kb.op("pool", lambda e, r=r: e.tensor_tensor(out=cqD[:, r, :], in0=cqB[:], in1=negm[:, r, :], op=ALU.add),
                              reads=["f_cqB", ("negm", r)], writes=[("f_cqD", r)])
                    c.rot = list(range(6))
                    po, pok = c.psb[6 + qi % 2], f"psb{6 + qi % 2}"
                    nk = 4 * (qi + 1)
                    LA = 4
                    sbank = {}

                    def emit_S(kt):
                        ps_, psk = c.ps()
                        mm(c, ps_[:, :], fkk[:, kt * 128:(kt + 1) * 128], fq[h][:, qs], True, True, [("fk", kt // 4), ("fq", h, qi), ("fqz", h)], [psk])
                        sbank[kt] = (ps_, psk)

                    for kt in range(min(LA, nk)):
                        emit_S(kt)
                    for kt in range(nk):
                        ps_, psk = sbank.pop(kt)
                        tb = tmpb[it % NB]; tbk = f"f_tmp{it % NB}"
                        pb = pTb[it % NB]; pbk = f"f_pT{it % NB}"
                        it += 1
                        r = kt - 4 * qi
                        if r >= 0:
                            kb.op("dve", lambda e, ps_=ps_, tb=tb, r=r: e.tensor_tensor(out=tb[:], in0=ps_[:, :], in1=cqD[:, r, :], op=ALU.add),
                                  reads=[psk, ("f_cqD", r)], writes=[tbk])
                        else:
                            kb.op("dve", lambda e, ps_=ps_, tb=tb: e.tensor_tensor(out=tb[:], in0=ps_[:, :], in1=cqB[:], op=ALU.add),
                                  reads=[psk, "f_cqB"], writes=[tbk])
                        kb.op("act", lambda e, tb=tb, pb=pb, h=h, kt=kt: e.activation(out=pb[:], in_=tb[:], func=AF.Exp, bias=ckT[h][:, kt:kt + 1]),
                              reads=[tbk, ("ckT", h)], writes=[pbk])
                        if kt + LA < nk:
                            emit_S(kt + LA)
                        mm(c, po[0:65, :], fV[h][:, kt, :], pb[:], kt == 0, kt == nk - 1, [("fV", h, kt // 4), ("fV1", h), pbk], [pok])
                    kb.op("act", lambda e, po=po: e.activation(out=osb[:], in_=po[0:65, :], func=AF.Copy), reads=[pok], writes=["f_osb"])
                    pd, pdk = c.ps()
                    mm(c, pd[0:64, :], c.ones_f[64:65, 0:64], osb[64:65, :], True, True, ["ones_f", "f_osb"], [pdk])
                    kb.op("dve", lambda e, pd=pd: e.reciprocal(out=rden[:], in_=pd[0:64, :]), reads=[pdk], writes=["f_rden"])
                    ob = outb[qi % 2]; obk = f"f_out{qi % 2}"
                    kb.op("dve", lambda e, ob=ob: e.tensor_tensor(out=ob[:], in0=osb[0:64, :], in1=rden[:], op=ALU.mult), reads=["f_osb", "f_rden"], writes=[obk])
                    cfo = io["catf_out"]
                    kb.dma("sp", (cfo[h][:, qs] if isinstance(cfo, list) else cfo[h * 64:(h + 1) * 64, qs]), ob[:], reads=[obk])
            c.rot = None
        c.barrier()


def build_M(which="both"):
    nc = bass.Bass("TRN2", target_bir_lowering=False)
    io = {}

    def din(name, shape, dt=F32):
        io[name] = nc.dram_tensor(name, shape, dt, kind="ExternalInput").ap()

    def dout(name, shape, dt=F32):
        io[name] = nc.dram_tensor(name, shape, dt, kind="ExternalOutput").ap()

    din("hT_all", [4, D, NT], BF16); din("w_ml", [D, 258]); din("w_fx", [D, 386]); din("vecsM", [128, 16])
    dout("catm_out", [64, SEQ], BF16); dout("catf_out", [128, SEQ], BF16)
    with _ES() as st:
        c = Ctx(nc, st)
        c.setup()
        if which in ("both", "mlstm"):
            phase_M_mlstm(c, io)
        if which in ("both", "fox"):
            phase_M_fox(c, io)
        c.barrier()
        c.kb.flush()
    return nc


def inputs_M(inp, l, g):
    w_in = np.asarray(inp["w_in"][l], np.float32)
    cols = np.concatenate([
        np.arange(g * 64, g * 64 + 64), 256 + np.arange(g * 64, g * 64 + 64),
        512 + np.arange(g * 64, g * 64 + 64), 768 + np.arange(g * 64, g * 64 + 64),
        [1024 + g, 1028 + g]])
    w_ml = np.ascontiguousarray(w_in[:, cols])
    fcols = []
    for base in (1544, 2056, 2568):
        for hh in (2 * g, 2 * g + 1):
            fcols.append(base + np.arange(hh * 64, hh * 64 + 64))
    fcols.append(np.array([3080 + 2 * g, 3080 + 2 * g + 1]))
    w_fx = np.ascontiguousarray(w_in[:, np.concatenate(fcols)])
    v = np.zeros((128, 16), np.float32)
    cw = np.asarray(inp["mlstm_conv_w"][l], np.float32)
    cb = np.asarray(inp["mlstm_conv_b"][l], np.float32)
    v[0:64, 0:4] = cw[:, g * 64:g * 64 + 64].T
    v[0:64, 4] = cb[g * 64:g * 64 + 64]
    v[0:64, 5:9] = cw[:, 256 + g * 64:256 + g * 64 + 64].T
    v[0:64, 9] = cb[256 + g * 64:256 + g * 64 + 64]
    v[0:64, 10] = np.asarray(inp["mlstm_norm_w"][l], np.float32)[g * 64:g * 64 + 64]
    v[:, 11] = inp["mlstm_b_i"][l][g]
    v[:, 12] = inp["mlstm_b_f"][l][g]
    v[:, 13] = inp["fox_b_f"][l][2 * g]
    v[:, 14] = inp["fox_b_f"][l][2 * g + 1]
    return {"w_ml": w_ml, "w_fx": w_fx, "vecsM": v}


def phase_P(c, io, xT):
    kb = c.kb
    from contextlib import ExitStack
    with ExitStack() as s0:
        vecs = c.sb("vecsP_sb", [128, 8], F32, s0)
        eps_t = c.sb("p_eps", [128, 1], F32, s0)
        sq = c.sb("p_sq", [128, 8, TT], BF16, s0)
        rstd = c.sb("p_rstd", [128, TT], F32, s0)
        hT = c.sb("p_hT", [128, 8, TT], BF16, s0)
        xt = [c.sb(f"p_xt{i}", [128, 4, D], F32, s0) for i in range(2)]
        tmp = {"sq": sq, "rstd": rstd, "eps": eps_t}
        kb.dma("sp", vecs[:], io["vecsP"], writes=["vecs"])
        kb.op("pool", lambda e: e.memset(eps_t[:], EPS), writes=["eps"])
        for t in range(NTT):
            t0 = t * TT
            xb = xt[t % 2]
            xk = f"p_xt{t % 2}"
            kb.dma("sp", xb[:], io["x_tok"][t0:t0 + TT, :].rearrange("(s p) d -> p s d", p=128), writes=[xk])
            for k in range(8):
                p, pk = c.ps()
                for s in range(4):
                    kb.op("pe", lambda e, k=k, s=s, p=p, xb=xb: e.transpose(out=p[:, s * 128:(s + 1) * 128], in_=xb[:, s, k * 128:(k + 1) * 128], identity=c.ident_f[:]),
                          reads=[xk, "ident_f"], writes=[pk])
                kb.op("act", lambda e, k=k, p=p, t0=t0: e.activation(out=xT[:, k, t0:t0 + TT], in_=p[:, :], func=AF.Copy), reads=[pk], writes=[("xT", k)])
            rmsnorm_tile(c, xT, "xT", t0, TT, vecs[:, 0:8], tmp, hT, "hT")
            h_store(c, io["h_next"], hT, t0, TT, [("hT", k) for k in range(8)], ["h_next_d"])
            if t == NTT - 1 and "tail_next" in io:
                kb.dma("sp", io["tail_next"].rearrange("(k p) n -> p k n", p=128), hT[:, :, TT - 32:TT],
                       reads=[("hT", k) for k in range(8)], writes=["tail_next_d"])
    c.barrier()


def build_P():
    nc = bass.Bass("TRN2", target_bir_lowering=False)
    io = {}
    io["x_tok"] = nc.dram_tensor("x_tok", [NT, D], F32, kind="ExternalInput").ap()
    io["vecsP"] = nc.dram_tensor("vecsP", [128, 8], F32, kind="ExternalInput").ap()
    io["xT_out"] = nc.dram_tensor("xT_out", [D, NT], F32, kind="ExternalOutput").ap()
    io["h_next"] = nc.dram_tensor("h_next", [D, NT], BF16, kind="ExternalOutput").ap()
    with _ES() as st:
        c = Ctx(nc, st)
        c.setup()
        xT = c.sb("xT", [128, 8, NT], F32)
        phase_P(c, io, xT)
        c.kb.dma("sp", io["xT_out"].rearrange("(k p) n -> p k n", p=128), xT[:], reads=[("xT", k) for k in range(8)])
        c.barrier()
        c.kb.flush()
    return nc


_CACHE = {}


def _get(name, fn):
    if name not in _CACHE:
        _CACHE[name] = fn()
    return _CACHE[name]


def kernel(**inp):
    inp = {k: np.asarray(v) for k, v in inp.items()}
    cores = list(range(8))
    B = 2
    x = inp["x"].astype(np.float32, copy=False)
    fm = lambda w: np.ascontiguousarray(np.asarray(w, np.float32).reshape(-1, 128).T)
    ncP = _get("P", build_P)
    maps = []
    for cid in cores:
        b, j = cid // 4, cid % 4
        maps.append({"x_tok": np.ascontiguousarray(x[b, j * NT:(j + 1) * NT]), "vecsP": fm(inp["norm_mix_w"][0])})
    res = run_bass_kernel_spmd(ncP, maps, core_ids=cores).results
    xT = [r["xT_out"] for r in res]
    hN = [r["h_next"] for r in res]
    out = None
    for l in range(2):
        last = (l == 1)
        E = 1 if l == 0 else 8
        ncM = _get("M", build_M)
        maps = []
        for cid in cores:
            b, g = cid // 4, cid % 4
            m = inputs_M(inp, l, g)
            m["hT_all"] = np.ascontiguousarray(np.stack([hN[b * 4 + jj] for jj in range(4)], axis=0))
            maps.append(m)
        resM = run_bass_kernel_spmd(ncM, maps, core_ids=cores).results
        ncT = _get(("T", E, last), lambda: build_T(E, last))
        maps = []
        for cid in cores:
            b, j = cid // 4, cid % 4
            tk = slice(j * NT, (j + 1) * NT)
            m = {"xT_in": xT[cid], "h_own": hN[cid]}
            m["h_halo"] = (np.ascontiguousarray(hN[cid - 1][:, NT - 32:NT]) if j > 0 else np.zeros((D, 32), NPBF))
            m["catm"] = np.ascontiguousarray(np.stack([resM[b * 4 + g]["catm_out"][:, tk] for g in range(4)], axis=0))
            m["catf"] = np.ascontiguousarray(np.stack([resM[b * 4 + g]["catf_out"][:, tk] for g in range(4)], axis=0))
            m["mem"] = np.ascontiguousarray(inp["mem"][b], dtype=np.float32)
            m["vecs"] = vecs_T(inp, l, last)
            m["w_c"] = np.ascontiguousarray(inp["w_in"][l][:, 1032:1544])
            m["w_out"] = inp["w_out"][l]; m["w_q"] = inp["xattn_w_q"][l]
            m["w_kv"] = inp["xattn_w_kv"][l]; m["w_o"] = inp["xattn_w_o"][l]
            if E == 1:
                m["w_gate"] = inp["ffn_w_gate"]; m["w_up"] = inp["ffn_w_up"]; m["w_down"] = inp["ffn_w_down"]
            else:
                m["w_gate"] = inp["moe_w_gate"][0]; m["w_up"] = inp["moe_w_up"][0]; m["w_down"] = inp["moe_w_down"][0]
                m["router_w"] = inp["router_w"][0]
            maps.append(m)
        resT = run_bass_kernel_spmd(ncT, maps, core_ids=cores).results
        if not last:
            xT = [r["xT_out"] for r in resT]
            hN = [r["h_next"] for r in resT]
        else:
            out = np.zeros((B, SEQ, D), np.float32)
            for cid in cores:
                b, j = cid // 4, cid % 4
                out[b, j * NT:(j + 1) * NT] = resT[cid]["out"]
    return out


RG = [[0, 1, 2, 3], [4, 5, 6, 7]]
_STOP = None


def build_fused(stop=None):
    nc = bass.Bass("TRN2", target_bir_lowering=False)
    io = {}
    if stop:
        io["dbg1"] = nc.dram_tensor("dbg1", [4 * D, NT], BF16, kind="ExternalOutput").ap()
        io["dbg2"] = nc.dram_tensor("dbg2", [512, SEQ], BF16, kind="ExternalOutput").ap()
        io["dbg3"] = nc.dram_tensor("dbg3", [D, NT], F32, kind="ExternalOutput").ap()

    def din(name, shape, dt=F32):
        io[name] = nc.dram_tensor(name, shape, dt, kind="ExternalInput").ap()
        return io[name]

    def dint(name, shape, dt=BF16):
        io[name] = nc.dram_tensor(name, shape, dt, kind="Internal").ap()
        return io[name]

    din("x_tok", [NT, D]); din("vecsP", [128, 8])
    if stop != "AG":
        din("sel", [128, 8]); din("mem", [256, D])
    for l in range(2 if stop != "AG" else 0):
        din(f"w_ml{l}", [D, 258]); din(f"w_fx{l}", [D, 386]); din(f"vecsM{l}", [128, 16]); din(f"vecs{l}", [128, NV_T])
        din(f"w_c{l}", [D, 512]); din(f"w_out{l}", [D, D]); din(f"w_q{l}", [D, 512]); din(f"w_kv{l}", [D, D]); din(f"w_o{l}", [512, D])
    if stop != "AG":
        din("w_gate0", [1, D, DFF]); din("w_up0", [1, D, DFF]); din("w_down0", [1, DFF, D])
        din("w_gate1", [8, D, DFF]); din("w_up1", [8, D, DFF]); din("w_down1", [8, DFF, D]); din("router_w", [D, 8])
    io["out"] = nc.dram_tensor("out", [NT, D], F32, kind="ExternalOutput").ap()
    for l in range(2):
        io[f"h_own{l}"] = [dint(f"h_own{l}_{a}", [256, NT]) for a in range(4)]
        io[f"hT_all{l}"] = [dint(f"hT_all{l}_{a}", [4 * 256, NT]) for a in range(4)]
        dint(f"tail{l}", [D, 32]); dint(f"tails{l}", [4 * D, 32])
        dint(f"catm{l}", [64, SEQ]); dint(f"catm_all{l}", [256, SEQ])
        io[f"catf{l}"] = [dint(f"catf{l}_{h}", [64, SEQ]) for h in range(2)]
        io[f"catf_all{l}"] = [dint(f"catf_all{l}_{h}", [256, SEQ]) for h in range(2)]
    with _ES() as st:
        c = Ctx(nc, st)
        c.setup()
        kb = c.kb
        xT = c.sb("xT", [128, 8, NT], F32)
        phase_P(c, {"x_tok": io["x_tok"], "vecsP": io["vecsP"], "h_next": io["h_own0"], "tail_next": io["tail0"]}, xT)
        for l in range(2):
            last = (l == 1)
            for a in range(4):
                kb.collective("AllGather", RG, io[f"h_own{l}"][a], io[f"hT_all{l}"][a], reads=["h_next_d"], writes=["hT_all_d"])
            kb.collective("AllGather", RG, io[f"tail{l}"], io[f"tails{l}"], reads=["tail_next_d"], writes=["tails_d"])
            c.barrier()
            if stop == "AG":
                for a in range(4):
                    for jj in range(4):
                        kb.dma("sp", io["dbg1"][jj * D + a * 256:jj * D + (a + 1) * 256, :], io[f"hT_all{l}"][a][jj * 256:(jj + 1) * 256, :], reads=["hT_all_d"])
                break
            ioM = {"hT_all": io[f"hT_all{l}"], "w_ml": io[f"w_ml{l}"], "w_fx": io[f"w_fx{l}"],
                   "vecsM": io[f"vecsM{l}"], "catm_out": io[f"catm{l}"], "catf_out": io[f"catf{l}"]}
            c.sfx = f"_{l}"
            phase_M_mlstm(c, ioM)
            phase_M_fox(c, ioM)
            kb.collective("AllGather", RG, io[f"catm{l}"], io[f"catm_all{l}"], writes=["catm_all_d"])
            for h in range(2):
                kb.collective("AllGather", RG, io[f"catf{l}"][h], io[f"catf_all{l}"][h], writes=["catf_all_d"])
            c.barrier()
            if stop == "M":
                for h in range(2):
                    for g in range(4):
                        kb.dma("sp", io["dbg2"][g * 128 + h * 64:g * 128 + (h + 1) * 64, :], io[f"catf_all{l}"][h][g * 64:(g + 1) * 64, :], reads=["catf_all_d"])
                break
            ioT = {"h_own": io[f"h_own{l}"], "tails": io[f"tails{l}"], "sel": io["sel"], "catm_all": io[f"catm_all{l}"], "catf_all": io[f"catf_all{l}"],
                   "mem": io["mem"], "vecs": io[f"vecs{l}"], "w_c": io[f"w_c{l}"], "w_out": io[f"w_out{l}"], "w_q": io[f"w_q{l}"],
                   "w_kv": io[f"w_kv{l}"], "w_o": io[f"w_o{l}"], "w_gate": io[f"w_gate{l}"], "w_up": io[f"w_up{l}"], "w_down": io[f"w_down{l}"]}
            if last:
                ioT["router_w"] = io["router_w"]; ioT["out"] = io["out"]
            else:
                ioT["h_next"] = io["h_own1"]; ioT["tail_next"] = io["tail1"]
            phase_T(c, ioT, 8 if last else 1, last, xT)
            if stop == "T":
                kb.dma("sp", io["dbg3"].rearrange("(k p) n -> p k n", p=128), xT[:], reads=[("xT", k) for k in range(8)])
                break
        c.barrier()
        kb.flush()
    return nc


def kernel_unfused(**inp):
    return _kernel_unfused(**inp)


_kernel_unfused = kernel


def kernel(**inp):
    inp = {k: np.asarray(v) for k, v in inp.items()}
    cores = list(range(8))
    x = inp["x"].astype(np.float32, copy=False)
    fm = lambda w: np.ascontiguousarray(np.asarray(w, np.float32).reshape(-1, 128).T)
    nc = _get("fused", lambda: build_fused(_STOP))
    shared = {"vecsP": fm(inp["norm_mix_w"][0]),
              "w_gate0": inp["ffn_w_gate"], "w_up0": inp["ffn_w_up"], "w_down0": inp["ffn_w_down"],
              "w_gate1": inp["moe_w_gate"][0], "w_up1": inp["moe_w_up"][0], "w_down1": inp["moe_w_down"][0],
              "router_w": inp["router_w"][0]}
    for l in range(2):
        shared[f"vecs{l}"] = vecs_T(inp, l, l == 1)
        shared[f"w_c{l}"] = np.ascontiguousarray(inp["w_in"][l][:, 1032:1544])
        shared[f"w_out{l}"] = inp["w_out"][l]; shared[f"w_q{l}"] = inp["xattn_w_q"][l]
        shared[f"w_kv{l}"] = inp["xattn_w_kv"][l]; shared[f"w_o{l}"] = inp["xattn_w_o"][l]
    perg = []
    for g in range(4):
        d = {}
        for l in range(2):
            m = inputs_M(inp, l, g)
            d[f"w_ml{l}"] = m["w_ml"]; d[f"w_fx{l}"] = m["w_fx"]; d[f"vecsM{l}"] = m["vecsM"]
        perg.append(d)
    maps = []
    for cid in cores:
        b, j = cid // 4, cid % 4
        m = dict(shared)
        m.update(perg[j])
        m["x_tok"] = np.ascontiguousarray(x[b, j * NT:(j + 1) * NT])
        m["mem"] = np.ascontiguousarray(inp["mem"][b], dtype=np.float32)
        sel = np.zeros((128, 8), np.float32)
        sel[:, j] = 1.0
        if j > 0:
            sel[:, 4 + j - 1] = 1.0
        m["sel"] = sel
        maps.append(m)
    if _STOP == "AG":
        maps = [{k: m[k] for k in ("x_tok", "vecsP")} for m in maps]
    res = run_bass_kernel_spmd(nc, maps, core_ids=cores).results
    if _STOP:
        return res
    out = np.zeros((2, SEQ, D), np.float32)
    for cid in cores:
        b, j = cid // 4, cid % 4
        out[b, j * NT:(j + 1) * NT] = res[cid]["out"]
    return out
```

```python
import numpy as np
import concourse.bass as bass
import concourse.mybir as mybir
from concourse.bass_utils import run_bass_kernel_spmd

F32 = mybir.dt.float32
BF16 = mybir.dt.bfloat16
AF = mybir.ActivationFunctionType
ALU = mybir.AluOpType
AX = mybir.AxisListType

ENGS = ("pe", "act", "dve", "pool", "sp")


class KB:
    SEM_ROLL = 2000

    def __init__(self, nc, n_dma_sems=32):
        self.nc = nc
        self.q = {e: [] for e in ENGS}
        self.cnt = {e: 0 for e in ENGS}
        self.cur_sem = {}
        self.sem_pool = []
        self.waited = {e: {} for e in ENGS}
        self.last_w = {}
        self.reads = {}
        self.n_dma_sems = n_dma_sems
        self.dma_sems = []
        self.dma_cnt = []
        self.dma_rr = 0
        self.dma_rr_sw = 0
        self._stack = None
        self.n_inst = 0

    def _new_sem(self, name):
        s = self._stack.enter_context(self.nc.semaphore(name))
        return s

    def start(self, stack):
        self._stack = stack
        for e in ENGS:
            self.cur_sem[e] = self._new_sem(f"p_{e}_0")
        for i in range(self.n_dma_sems):
            self.dma_sems.append(self._new_sem(f"dma{i}"))
            self.dma_cnt.append(0)

    def _wait(self, eng, ev):
        if ev is None:
            return
        if len(ev) == 3 and ev[2] == "pe" and eng == "pe":
            return
        sem, val = ev[0], ev[1]
        w = self.waited[eng]
        if w.get(id(sem), (None, 0))[1] >= val:
            return
        w[id(sem)] = (sem, val)
        self.q[eng].append(lambda e, sem=sem, val=val: e.wait_ge(sem, val))

    def _wait_w(self, eng, k):
        lw = self.last_w.get(k)
        if isinstance(lw, list):
            for ev in lw:
                self._wait(eng, ev)
        else:
            self._wait(eng, lw)

    def _deps(self, eng, reads, writes):
        for k in reads:
            self._wait_w(eng, k)
        for k in writes:
            self._wait_w(eng, k)
            for ev in self.reads.get(k, ()):
                self._wait(eng, ev)

    def _commit(self, ev, reads, writes, is_dma=False):
        for k in writes:
            lw = self.last_w.get(k)
            if is_dma and isinstance(lw, list) and not self.reads.get(k):
                lw.append(ev)
            else:
                self.last_w[k] = [ev] if is_dma else ev
            self.reads[k] = []
        for k in reads:
            self.reads.setdefault(k, []).append(ev)

    def op(self, eng, fn, reads=(), writes=()):
        self._deps(eng, reads, writes)
        if self.cnt[eng] >= self.SEM_ROLL:
            self.cur_sem[eng] = self._new_sem(f"p_{eng}_{self.n_inst}")
            self.cnt[eng] = 0
        self.cnt[eng] += 1
        sem = self.cur_sem[eng]
        ev = (sem, self.cnt[eng], eng)
        self.q[eng].append(lambda e, sem=sem: fn(e).then_inc(sem, 1))
        self._commit(ev, reads, writes)
        self.n_inst += 1
        return ev

    def dma(self, eng, out, in_, reads=(), writes=(), **kw):
        self._deps(eng, reads, writes)
        half = self.n_dma_sems // 2
        if eng == "pool":
            i = half + self.dma_rr_sw
            self.dma_rr_sw = (self.dma_rr_sw + 1) % (self.n_dma_sems - half)
        else:
            i = self.dma_rr
            self.dma_rr = (self.dma_rr + 1) % half
        sem = self.dma_sems[i]
        if self.dma_cnt[i] >= 2048:
            self.dma_sems[i] = self._new_sem(f"dma{i}_{self.n_inst}")
            self.dma_cnt[i] = 0
            sem = self.dma_sems[i]
        if self.dma_cnt[i] > 0:
            self._wait(eng, (sem, self.dma_cnt[i]))
        self.dma_cnt[i] += 16
        ev = (sem, self.dma_cnt[i])
        self.q[eng].append(lambda e, sem=sem: e.dma_start(out=out, in_=in_, **kw).then_inc(sem, 16))
        self._commit(ev, reads, writes, is_dma=True)
        self.n_inst += 1
        return ev

    def collective(self, kind, rg, in_ap, out_ap, reads=(), writes=()):
        eng = "pool"
        self._deps(eng, reads, writes)
        sem = self._new_sem(f"cc_{self.n_inst}")
        ev = (sem, 1)
        self.q[eng].append(lambda e: e.collective_compute(kind, ALU.bypass, replica_groups=rg, ins=[in_ap.opt()],
                                                          outs=[out_ap.opt()]).then_inc(sem, 1))
        self._commit(ev, reads, writes)
        self.n_inst += 1
        self.cc_events = getattr(self, "cc_events", []) + [ev]
        return ev

    def wait_all(self, eng, evs):
        for ev in evs:
            self._wait(eng, ev)

    def flush(self):
        nc = self.nc
        q = self.q
        with nc.Block() as block:
            @block.tensor
            def _(e):
                for f in q["pe"]:
                    f(e)

            @block.scalar
            def _(e):
                for f in q["act"]:
                    f(e)

            @block.vector
            def _(e):
                for f in q["dve"]:
                    f(e)

            @block.gpsimd
            def _(e):
                for f in q["pool"]:
                    f(e)

            @block.sync
            def _(e):
                for f in q["sp"]:
                    f(e)
        self.q = {e: [] for e in ENGS}


D = 1024
NT = 2048
TT = 512
NTT = NT // TT
DFF = 2816
NF = DFF // 128
SEQ = 8192
EPS = 1e-6
NV_T = 100


class Ctx:
    def __init__(self, nc, st):
        self.nc = nc
        self.st = st
        self.kb = KB(nc)
        self.kb.start(st)
        self.ps_rr = 0
        self.uid = 0

    def sb(self, name, shape, dt, st=None):
        self.uid += 1
        return (st or self.st).enter_context(self.nc.sbuf_tensor(f"{name}_u{self.uid}", shape, dt))

    def barrier(self):
        kb = self.kb
        evs = []
        for e in ENGS:
            if kb.cnt[e] > 0:
                evs.append((kb.cur_sem[e], kb.cnt[e]))
        for i, s in enumerate(kb.dma_sems):
            if kb.dma_cnt[i] > 0:
                evs.append((s, kb.dma_cnt[i]))
        evs += getattr(kb, "cc_events", [])
        kb.cc_events = []
        for e in ENGS:
            for ev in evs:
                kb._wait(e, ev)
        kb.last_w = {}
        kb.reads = {}

    def setup(self):
        nc, kb = self.nc, self.kb
        self.ident_f = self.sb("ident_f", [128, 128], F32)
        self.ident_b = self.sb("ident_b", [128, 128], BF16)
        self.ones_b = self.sb("ones_b", [128, 128], BF16)
        self.ones_f = self.sb("ones_f", [128, 128], F32)
        self.psb = [self.st.enter_context(nc.psum_tensor(f"psb{i}", [128, 512], F32)) for i in range(8)]
        idf, idb, ob, of = self.ident_f, self.ident_b, self.ones_b, self.ones_f
        kb.op("pool", lambda e: e.memset(idf[:], 0.0), writes=["ident_f"])
        kb.op("pool", lambda e: e.affine_select(out=idf[:], in_=idf[:], pattern=[[-1, 128]],
                                                compare_op=ALU.not_equal, fill=1.0, base=0,
                                                channel_multiplier=1),
              reads=["ident_f"], writes=["ident_f"])
        kb.op("pool", lambda e: e.tensor_copy(out=idb[:], in_=idf[:]), reads=["ident_f"], writes=["ident_b"])
        kb.op("pool", lambda e: e.memset(ob[:], 1.0), writes=["ones_b"])
        kb.op("pool", lambda e: e.memset(of[:], 1.0), writes=["ones_f"])

    def ps(self):
        rot = getattr(self, "rot", None) or list(range(8))
        i = rot[self.ps_rr % len(rot)]
        self.ps_rr += 1
        return self.psb[i], f"psb{i}"


def h_store(c, dst, hT, c0, n, reads, writes=()):
    if isinstance(dst, list):
        for a, d in enumerate(dst):
            c.kb.dma("sp", d[:, c0:c0 + n].rearrange("(k p) n -> p k n", p=128), hT[:, 2 * a:2 * a + 2, 0:n], reads=reads, writes=writes)
    else:
        c.kb.dma("sp", dst[:, c0:c0 + n].rearrange("(k p) n -> p k n", p=128), hT[:, :, 0:n], reads=reads, writes=writes)


def h_load(c, src, hT, c0, n, writes, j=None):
    if isinstance(src, list):
        for a, d in enumerate(src):
            v = d if j is None else d.rearrange("(j r) n -> j r n", j=4)[j]
            c.kb.dma("sp", hT[:, 2 * a:2 * a + 2, 0:n], v[:, c0:c0 + n].rearrange("(k p) n -> p k n", p=128), writes=writes)
    else:
        v = src if j is None else src[j]
        c.kb.dma("sp", hT[:, :, 0:n], v[:, c0:c0 + n].rearrange("(k p) n -> p k n", p=128), writes=writes)


def mm(c, out, lhsT, rhs, start, stop, reads, writes):
    return c.kb.op("pe", lambda e: e.matmul(out, lhsT=lhsT, rhs=rhs, start=start, stop=stop),
                   reads=reads, writes=writes)


def rmsnorm_tile(c, xT, xkey, t0, n, wv, tmp, out_bf, okey, out_f=None):
    kb = c.kb
    sq, rstd = tmp["sq"], tmp["rstd"]
    for k in range(8):
        kb.op("act", lambda e, k=k: e.activation(out=sq[:, k, 0:n], in_=xT[:, k, t0:t0 + n], func=AF.Square),
              reads=[(xkey, k)], writes=[("sq", k)])
    p, pk = c.ps()
    for k in range(8):
        mm(c, p[:, 0:n], c.ones_b[:], sq[:, k, 0:n], k == 0, k == 7, ["ones_b", ("sq", k)], [pk])
    kb.op("act", lambda e: e.activation(out=rstd[:, 0:n], in_=p[:, 0:n], func=AF.Sqrt, scale=1.0 / D, bias=tmp["eps"][:, 0:1]),
          reads=[pk, "eps"], writes=["rstd"])
    kb.op("dve", lambda e: e.reciprocal(out=rstd[:, 0:n], in_=rstd[:, 0:n]), reads=["rstd"], writes=["rstd"])
    for k in range(8):
        kb.op("dve", lambda e, k=k: e.scalar_tensor_tensor(out=out_bf[:, k, 0:n], in0=xT[:, k, t0:t0 + n],
                                                           scalar=wv[:, k:k + 1], in1=rstd[:, 0:n],
                                                           op0=ALU.mult, op1=ALU.mult),
              reads=[(xkey, k), "rstd", "vecs"], writes=[(okey, k)])
        if out_f is not None:
            kb.op("dve", lambda e, k=k: e.scalar_tensor_tensor(out=out_f[:, k, 0:n], in0=xT[:, k, t0:t0 + n],
                                                                scalar=wv[:, k:k + 1], in1=rstd[:, 0:n],
                                                                op0=ALU.mult, op1=ALU.mult),
                  reads=[(xkey, k), "rstd", "vecs"], writes=[(okey + "_f", k)])


def phase_T(c, io, E, last, xT):
    nc, kb = c.nc, c.kb
    from contextlib import ExitStack
    vec_st = ExitStack()
    vecs = c.sb("vecsT", [128, NV_T], F32, vec_st)
    eps_t = c.sb("eps_t", [128, 1], F32, vec_st)
    sq = c.sb("sq", [128, 8, TT], BF16, vec_st)
    rstd = c.sb("rstd", [128, TT], F32, vec_st)
    tmp = {"sq": sq, "rstd": rstd, "eps": eps_t}
    kb.dma("sp", vecs[:], io["vecs"], writes=["vecs"])
    kb.op("pool", lambda e: e.memset(eps_t[:], EPS), writes=["eps"])
    V_XA, V_MEM, V_FFN, V_NEXT, V_CB, V_LNW, V_LNB, V_CW = 0, 8, 16, 24, 32, 34, 36, 38

    with ExitStack() as s1:
        hT = c.sb("hT", [128, 8, TT], BF16, s1)
        gluT = c.sb("gluT", [128, 2, 32 + NT], BF16, s1)
        hcT = c.sb("hcT", [128, 2, NT], BF16, s1)
        wc = c.sb("wc", [128, 8, 512], BF16, s1)
        dg = c.sb("dg", [128, 62, 128], BF16, s1)
        sig = c.sb("sig", [128, 2, TT], F32, s1)
        hcv = c.sb("hcv", [128, 2, TT], F32, s1)
        hsq = c.sb("hsq", [128, 2, TT], F32, s1)
        mean = c.sb("mean", [128, TT], F32, s1)
        var = c.sb("var", [128, TT], F32, s1)
        wo_m = c.sb("wo_m", [64, 4, D], BF16, s1)
        wo_c = c.sb("wo_c", [128, 2, D], BF16, s1)
        wo_f = c.sb("wo_f", [128, 4, D], BF16, s1)
        mT = c.sb("mT", [64, 4, TT], BF16, s1)
        fT = c.sb("fT", [128, 4, TT], BF16, s1)
        if "sel" in io:
            halo4 = c.sb("halo4", [128, 4, 8, 32], BF16, s1)
            selt = c.sb("selt", [128, 8], F32, s1)
            m4 = [c.sb(f"m4_{i}", [64, 4, TT], BF16, s1) for i in range(2)]
            f4 = [c.sb(f"f4_{i}", [128, 4, TT], BF16, s1) for i in range(2)]
            kb.dma("sp", selt[:], io["sel"], writes=["selt"])
        kb.dma("pool", wc[:], io["w_c"].rearrange("(k p) n -> p k n", p=128), writes=["wc"])
        kb.dma("pool", wo_m[:], io["w_out"][0:256, :].rearrange("(g p) n -> p g n", p=64), writes=["wo_m"])
        kb.dma("pool", wo_c[:], io["w_out"][256:512, :].rearrange("(g p) n -> p g n", p=128), writes=["wo_c"])
        kb.dma("pool", wo_f[:], io["w_out"][512:1024, :].rearrange("(g p) n -> p g n", p=128), writes=["wo_f"])
        for j in range(31):
            for ch in range(2):
                kb.op("dve", lambda e, j=j, ch=ch: e.tensor_scalar(
                    out=dg[:, j * 2 + ch, :], in0=c.ident_b[:], scalar1=vecs[:, V_CW + j * 2 + ch:V_CW + j * 2 + ch + 1],
                    scalar2=None, op0=ALU.mult), reads=["ident_b", "vecs"], writes=[("dg", j, ch)])
        tiles = [("halo", 0, 32)] + [("own", t * TT, TT) for t in range(NTT)]
        for kind, t0, n in tiles:
            if kind == "halo" and "sel" in io:
                tl = io["tails"].rearrange("(j k p) n -> j p k n", j=4, p=128)
                for jj in range(4):
                    kb.dma("sp", halo4[:, jj, :, :], tl[jj], writes=[("halo4", jj)])
                kb.op("dve", lambda e: e.tensor_scalar(out=hT[:, :, 0:32], in0=halo4[:, 0, :, :], scalar1=selt[:, 4:5], scalar2=None, op0=ALU.mult),
                      reads=[("halo4", 0), "selt"], writes=[("hT", k) for k in range(8)])
                for jj in range(1, 4):
                    kb.op("dve", lambda e, jj=jj: e.scalar_tensor_tensor(out=hT[:, :, 0:32], in0=halo4[:, jj, :, :], scalar=selt[:, 4 + jj:5 + jj], in1=hT[:, :, 0:32],
                                                                         op0=ALU.mult, op1=ALU.add),
                          reads=[("halo4", jj), "selt"] + [("hT", k) for k in range(8)], writes=[("hT", k) for k in range(8)])
                g0 = 0
            elif kind == "halo":
                kb.dma("sp", hT[:, :, 0:n], io["h_halo"].rearrange("(k p) n -> p k n", p=128),
                       writes=[("hT", k) for k in range(8)])
                g0 = 0
            else:
                h_load(c, io["h_own"], hT, t0, n, [("hT", k) for k in range(8)])
                g0 = 32 + t0
            for ch in range(2):
                pa, pak = c.ps()
                pg, pgk = c.ps()
                for k in range(8):
                    mm(c, pa[:, 0:n], wc[:, k, ch * 128:(ch + 1) * 128], hT[:, k, 0:n], k == 0, k == 7,
                       ["wc", ("hT", k)], [pak])
                for k in range(8):
                    mm(c, pg[:, 0:n], wc[:, k, 256 + ch * 128:256 + (ch + 1) * 128], hT[:, k, 0:n], k == 0, k == 7,
                       ["wc", ("hT", k)], [pgk])
                kb.op("act", lambda e, ch=ch, pg=pg, n=n: e.activation(out=sig[:, ch, 0:n], in_=pg[:, 0:n], func=AF.Sigmoid),
                      reads=[pgk], writes=[("sig", ch)])
                kb.op("dve", lambda e, ch=ch, pa=pa, n=n, g0=g0: e.tensor_tensor(
                    out=gluT[:, ch, g0:g0 + n], in0=pa[:, 0:n], in1=sig[:, ch, 0:n], op=ALU.mult),
                    reads=[pak, ("sig", ch)], writes=[("glu", ch, g0 // TT), ("glu", ch, (g0 + n - 1) // TT)])
        for t in range(NTT):
            t0 = t * TT
            gk = lambda ch: [("glu", ch, (32 + t0 - 30) // TT), ("glu", ch, (32 + t0 + TT - 1) // TT)]
            for ch in range(2):
                p, pk = c.ps()
                for j in range(31):
                    o = 32 + t0 - 30 + j
                    mm(c, p[:, :], dg[:, j * 2 + ch, :], gluT[:, ch, o:o + TT], j == 0, j == 30,
                       [("dg", j, ch)] + gk(ch), [pk])
                kb.op("act", lambda e, ch=ch, p=p: e.activation(out=hcv[:, ch, :], in_=p[:, :], func=AF.Identity,
                                                                bias=vecs[:, V_CB + ch:V_CB + ch + 1]),
                      reads=[pk, "vecs"], writes=[("hcv", ch)])
                kb.op("act", lambda e, ch=ch: e.activation(out=hsq[:, ch, :], in_=hcv[:, ch, :], func=AF.Square),
                      reads=[("hcv", ch)], writes=[("hsq", ch)])
            p1, p1k = c.ps()
            p2, p2k = c.ps()
            for ch in range(2):
                mm(c, p1[:, :], c.ones_f[:], hcv[:, ch, :], ch == 0, ch == 1, ["ones_f", ("hcv", ch)], [p1k])
            for ch in range(2):
                mm(c, p2[:, :], c.ones_f[:], hsq[:, ch, :], ch == 0, ch == 1, ["ones_f", ("hsq", ch)], [p2k])
            kb.op("dve", lambda e, p1=p1: e.tensor_scalar(out=mean[:], in0=p1[:, :], scalar1=1.0 / 256, scalar2=None, op0=ALU.mult),
                  reads=[p1k], writes=["mean"])
            kb.op("dve", lambda e: e.tensor_tensor(out=var[:], in0=mean[:], in1=mean[:], op=ALU.mult),
                  reads=["mean"], writes=["var"])
            kb.op("dve", lambda e, p2=p2: e.scalar_tensor_tensor(out=var[:], in0=p2[:, :], scalar=1.0 / 256, in1=var[:],
                                                                 op0=ALU.mult, op1=ALU.subtract),
                  reads=[p2k, "var"], writes=["var"])
            kb.op("act", lambda e: e.activation(out=var[:], in_=var[:], func=AF.Sqrt, bias=eps_t[:, 0:1]),
                  reads=["var", "eps"], writes=["var"])
            kb.op("dve", lambda e: e.reciprocal(out=var[:], in_=var[:]), reads=["var"], writes=["var"])
            for ch in range(2):
                kb.op("dve", lambda e, ch=ch: e.tensor_tensor(out=hcv[:, ch, :], in0=hcv[:, ch, :], in1=mean[:], op=ALU.subtract),
                      reads=[("hcv", ch), "mean"], writes=[("hcv", ch)])
                kb.op("dve", lambda e, ch=ch: e.tensor_tensor(out=hcv[:, ch, :], in0=hcv[:, ch, :], in1=var[:], op=ALU.mult),
                      reads=[("hcv", ch), "var"], writes=[("hcv", ch)])
                kb.op("dve", lambda e, ch=ch: e.tensor_scalar(out=hcv[:, ch, :], in0=hcv[:, ch, :],
                                                              scalar1=vecs[:, V_LNW + ch:V_LNW + ch + 1],
                                                              scalar2=vecs[:, V_LNB + ch:V_LNB + ch + 1],
                                                              op0=ALU.mult, op1=ALU.add),
                      reads=[("hcv", ch), "vecs"], writes=[("hcv", ch)])
                kb.op("act", lambda e, ch=ch, t0=t0: e.activation(out=hcT[:, ch, t0:t0 + TT], in_=hcv[:, ch, :], func=AF.Silu),
                      reads=[("hcv", ch)], writes=[("hcT", ch, t)])
            if "sel" in io:
                cm = io["catm_all"].rearrange("(g p) n -> p g n", p=64)
                cf = [a.rearrange("(g p) n -> p g n", p=64) for a in io["catf_all"]]
                for jj in range(4):
                    for dst, stg, src, nm, npart in ((mT, m4, cm, "m4", 64), (fT, f4, cf, "f4", 128)):
                        dk = "mT" if nm == "m4" else "fT"
                        sg = stg[jj % 2]
                        sk = f"{nm}_{jj % 2}"
                        if nm == "m4":
                            kb.dma("sp", sg[:], src[:, :, jj * NT + t0:jj * NT + t0 + TT], writes=[sk])
                        else:
                            for hh in range(2):
                                kb.dma("sp", sg[hh * 64:(hh + 1) * 64, :, :], src[hh][:, :, jj * NT + t0:jj * NT + t0 + TT], writes=[sk])
                        if jj == 0:
                            kb.op("dve", lambda e, dst=dst, sg=sg, npart=npart: e.tensor_scalar(out=dst[:], in0=sg[:], scalar1=selt[0:npart, 0:1], scalar2=None, op0=ALU.mult),
                                  reads=[sk, "selt"], writes=[dk])
                        else:
                            kb.op("dve", lambda e, dst=dst, sg=sg, jj=jj, npart=npart: e.scalar_tensor_tensor(out=dst[:], in0=sg[:], scalar=selt[0:npart, jj:jj + 1], in1=dst[:],
                                                                                                          op0=ALU.mult, op1=ALU.add),
                                  reads=[sk, "selt", dk], writes=[dk])
            else:
                kb.dma("sp", mT[:], io["catm"][:, :, t0:t0 + TT].rearrange("g p n -> p g n"), writes=["mT"])
                kb.dma("sp", fT[:], io["catf"][:, :, t0:t0 + TT].rearrange("g p n -> p g n"), writes=["fT"])
            for d in range(8):
                p, pk = c.ps()
                ds = slice(d * 128, (d + 1) * 128)
                for g in range(4):
                    mm(c, p[:, :], wo_m[:, g, ds], mT[:, g, :], g == 0, False, ["wo_m", "mT"], [pk])
                for ch in range(2):
                    mm(c, p[:, :], wo_c[:, ch, ds], hcT[:, ch, t0:t0 + TT], False, False, ["wo_c", ("hcT", ch, t)], [pk])
                for g in range(4):
                    mm(c, p[:, :], wo_f[:, g, ds], fT[:, g, :], False, g == 3, ["wo_f", "fT"], [pk])
                kb.op("dve", lambda e, d=d, p=p, t0=t0: e.tensor_tensor(out=xT[:, d, t0:t0 + TT], in0=xT[:, d, t0:t0 + TT],
                                                                        in1=p[:, :], op=ALU.add),
                      reads=[pk, ("xT", d)], writes=[("xT", d)])
    c.barrier()
    if io.get("dbg_stage") == 1:
        vec_st.close()
        return

    with ExitStack() as s2:
        hT = c.sb("hT", [128, 8, TT], BF16, s2)
        memt = c.sb("memt", [128, 2, D], F32, s2)
        mss = c.sb("mss", [128, 2], F32, s2)
        junk = c.sb("junk", [128, D], F32, s2)
        memnT = c.sb("memnT", [128, 8, 256], BF16, s2)
        wkv = c.sb("wkv", [128, 8, D], BF16, s2)
        wq = c.sb("wq", [128, 8, 512], BF16, s2)
        wo = c.sb("wo", [128, 4, D], BF16, s2)
        kT = c.sb("kT", [128, 4, 256], BF16, s2)
        Vt = c.sb("Vt", [128, 2, 512], BF16, s2)
        qT = c.sb("qT", [128, 4, TT], BF16, s2)
        pT = c.sb("pT", [128, 8, TT], BF16, s2)
        rden = c.sb("rden", [128, TT], F32, s2)
        oT = c.sb("oT", [128, 4, TT], BF16, s2)
        kb.dma("sp", memt[:], io["mem"].rearrange("(t p) d -> p t d", p=128), writes=["memt"])
        kb.dma("pool", wkv[:], io["w_kv"].rearrange("(k p) n -> p k n", p=128), writes=["wkv"])
        kb.dma("pool", wq[:], io["w_q"].rearrange("(k p) n -> p k n", p=128), writes=["wq"])
        kb.dma("pool", wo[:], io["w_o"].rearrange("(k p) n -> p k n", p=128), writes=["wo"])
        for mt in range(2):
            kb.op("act", lambda e, mt=mt: e.activation(out=junk[:], in_=memt[:, mt, :], func=AF.Square,
                                                       accum_out=mss[:, mt:mt + 1]),
                  reads=["memt"], writes=["junk", ("mss", mt)])
            kb.op("act", lambda e, mt=mt: e.activation(out=mss[:, mt:mt + 1], in_=mss[:, mt:mt + 1], func=AF.Sqrt,
                                                       scale=1.0 / D, bias=eps_t[:, 0:1]),
                  reads=[("mss", mt), "eps"], writes=[("mss", mt)])
            kb.op("dve", lambda e, mt=mt: e.reciprocal(out=mss[:, mt:mt + 1], in_=mss[:, mt:mt + 1]),
                  reads=[("mss", mt)], writes=[("mss", mt)])
            kb.op("dve", lambda e, mt=mt: e.tensor_scalar(out=memt[:, mt, :], in0=memt[:, mt, :], scalar1=mss[:, mt:mt + 1],
                                                          scalar2=None, op0=ALU.mult),
                  reads=["memt", ("mss", mt)], writes=["memt"])
        for k in range(8):
            p, pk = c.ps()
            for mt in range(2):
                kb.op("pe", lambda e, k=k, mt=mt, p=p: e.transpose(out=p[:, mt * 128:(mt + 1) * 128],
                                                                   in_=memt[:, mt, k * 128:(k + 1) * 128], identity=c.ident_f[:]),
                      reads=["memt", "ident_f"], writes=[pk])
            kb.op("dve", lambda e, k=k, p=p: e.tensor_scalar(out=memnT[:, k, :], in0=p[:, 0:256],
                                                             scalar1=vecs[:, V_MEM + k:V_MEM + k + 1], scalar2=None, op0=ALU.mult),
                  reads=[pk, "vecs"], writes=[("memnT", k)])
        for h in range(4):
            p, pk = c.ps()
            for k in range(8):
                mm(c, p[:, 0:256], wkv[:, k, h * 128:(h + 1) * 128], memnT[:, k, :], k == 0, k == 7, ["wkv", ("memnT", k)], [pk])
            kb.op("act", lambda e, h=h, p=p: e.activation(out=kT[:, h, :], in_=p[:, 0:256], func=AF.Copy),
                  reads=[pk], writes=[("kT", h)])
        for mt in range(2):
            p, pk = c.ps()
            for k in range(8):
                mm(c, p[:, :], memnT[:, k, mt * 128:(mt + 1) * 128], wkv[:, k, 512:1024], k == 0, k == 7, ["wkv", ("memnT", k)], [pk])
            kb.op("act", lambda e, mt=mt, p=p: e.activation(out=Vt[:, mt, :], in_=p[:, :], func=AF.Copy),
                  reads=[pk], writes=[("Vt", mt)])
        sc = 128 ** -0.5
        for t in range(NTT):
            t0 = t * TT
            rmsnorm_tile(c, xT, "xT", t0, TT, vecs[:, V_XA:V_XA + 8], tmp, hT, "hT")
            for h in range(4):
                p, pk = c.ps()
                for k in range(8):
                    mm(c, p[:, :], wq[:, k, h * 128:(h + 1) * 128], hT[:, k, :], k == 0, k == 7, ["wq", ("hT", k)], [pk])
                kb.op("act", lambda e, h=h, p=p: e.activation(out=qT[:, h, :], in_=p[:, :], func=AF.Copy),
                      reads=[pk], writes=[("qT", h)])
            for h in range(4):
                for mt in range(2):
                    p, pk = c.ps()
                    mm(c, p[:, :], kT[:, h, mt * 128:(mt + 1) * 128], qT[:, h, :], True, True, [("kT", h), ("qT", h)], [pk])
                    kb.op("act", lambda e, h=h, mt=mt, p=p: e.activation(out=pT[:, h * 2 + mt, :], in_=p[:, :], func=AF.Exp, scale=sc),
                          reads=[pk], writes=[("pT", h, mt)])
                pd, pdk = c.ps()
                for mt in range(2):
                    mm(c, pd[:, :], c.ones_b[:], pT[:, h * 2 + mt, :], mt == 0, mt == 1, ["ones_b", ("pT", h, mt)], [pdk])
                kb.op("dve", lambda e, pd=pd: e.reciprocal(out=rden[:], in_=pd[:, :]), reads=[pdk], writes=["rden"])
                po, pok = c.ps()
                for mt in range(2):
                    mm(c, po[:, :], Vt[:, mt, h * 128:(h + 1) * 128], pT[:, h * 2 + mt, :], mt == 0, mt == 1,
                       [("Vt", mt), ("pT", h, mt)], [pok])
                kb.op("dve", lambda e, h=h, po=po: e.tensor_tensor(out=oT[:, h, :], in0=po[:, :], in1=rden[:], op=ALU.mult),
                      reads=[pok, "rden"], writes=[("oT", h)])
            for d in range(8):
                p, pk = c.ps()
                for h in range(4):
                    mm(c, p[:, :], wo[:, h, d * 128:(d + 1) * 128], oT[:, h, :], h == 0, h == 3, ["wo", ("oT", h)], [pk])
                kb.op("dve", lambda e, d=d, p=p, t0=t0: e.tensor_tensor(out=xT[:, d, t0:t0 + TT], in0=xT[:, d, t0:t0 + TT],
                                                                        in1=p[:, :], op=ALU.add),
                      reads=[pk, ("xT", d)], writes=[("xT", d)])
    c.barrier()
    if io.get("dbg_stage") == 2:
        vec_st.close()
        return

    with ExitStack() as s3:
        hTall = c.sb("hTall", [128, 8, NT], BF16, s3)
        actT = c.sb("actT", [128, 8, NT], BF16, s3)
        wgu = [c.sb(f"wgu{i}", [128, 8, 256], BF16, s3) for i in range(3)]
        wdr = [c.sb(f"wdr{i}", [128, D], BF16, s3) for i in range(11)]
        sil = [c.sb(f"sil{i}", [128, TT], BF16, s3) for i in range(2)]
        if E > 1:
            wr = c.sb("wr", [128, 8, 8], F32, s3)
            lg = c.sb("lg", [128, 4, 8], F32, s3)
            top8 = c.sb("top8", [128, 4, 8], F32, s3)
            gts = c.sb("gts", [128, 16, 8], F32, s3)
            gsc = c.sb("gsc", [128, 4, 4], F32, s3)
            dgate = c.sb("dgate", [128, 128], F32, s3)
            gB = [c.sb(f"gB{i}", [128, NT], BF16, s3) for i in range(2)]
            ytmps = [c.sb(f"ytmp{i}", [128, TT], F32, s3) for i in range(2)]
            kb.dma("sp", wr[:], io["router_w"].rearrange("(k p) n -> p k n", p=128), writes=["wr"])
        with ExitStack() as s3a:
            hF = c.sb("hF", [128, 8, TT], F32, s3a) if E > 1 else None
            for t in range(NTT):
                t0 = t * TT
                rmsnorm_tile(c, xT, "xT", t0, TT, vecs[:, V_FFN:V_FFN + 8], tmp, hTall[:, :, t0:t0 + TT], f"hA{t}", out_f=hF)
                if E > 1:
                    for s in range(4):
                        p, pk = c.ps()
                        for k in range(8):
                            mm(c, p[:, 0:8], hF[:, k, s * 128:(s + 1) * 128], wr[:, k, :], k == 0, k == 7, [(f"hA{t}_f", k), "wr"], [pk])
                        kb.op("dve", lambda e, s=s, p=p: e.tensor_copy(out=lg[:, s, :], in_=p[:, 0:8]), reads=[pk], writes=[("lg", s)])
                        kb.op("dve", lambda e, s=s: e.max(out=top8[:, s, :], in_=lg[:, s, :]), reads=[("lg", s)], writes=[("top8", s)])
                        kb.op("dve", lambda e, s=s: e.tensor_scalar(out=gsc[:, s, 0:1], in0=top8[:, s, 0:1], scalar1=-1.0, scalar2=None, op0=ALU.mult),
                              reads=[("top8", s)], writes=[("gsc", s, 0)])
                        kb.op("act", lambda e, s=s: e.activation(out=gsc[:, s, 1:2], in_=top8[:, s, 1:2], func=AF.Exp, bias=gsc[:, s, 0:1]),
                              reads=[("top8", s), ("gsc", s, 0)], writes=[("gsc", s, 1)])
                        kb.op("dve", lambda e, s=s: e.tensor_scalar(out=gsc[:, s, 1:2], in0=gsc[:, s, 1:2], scalar1=1.0, scalar2=None, op0=ALU.add),
                              reads=[("gsc", s, 1)], writes=[("gsc", s, 1)])
                        kb.op("dve", lambda e, s=s: e.reciprocal(out=gsc[:, s, 1:2], in_=gsc[:, s, 1:2]),
                              reads=[("gsc", s, 1)], writes=[("gsc", s, 1)])
                        gi = t * 4 + s
                        kb.op("act", lambda e, s=s, gi=gi: e.activation(out=gts[:, gi, :], in_=lg[:, s, :], func=AF.Exp, bias=gsc[:, s, 0:1]),
                              reads=[("lg", s), ("gsc", s, 0)], writes=[("gts", gi)])
                        kb.op("dve", lambda e, s=s: e.tensor_scalar(out=lg[:, s, :], in0=lg[:, s, :], scalar1=top8[:, s, 1:2], scalar2=None, op0=ALU.is_ge),
                              reads=[("lg", s), ("top8", s)], writes=[("lg", s)])
                        kb.op("dve", lambda e, s=s, gi=gi: e.scalar_tensor_tensor(out=gts[:, gi, :], in0=gts[:, gi, :], scalar=gsc[:, s, 1:2], in1=lg[:, s, :],
                                                                                  op0=ALU.mult, op1=ALU.mult),
                              reads=[("gts", gi), ("gsc", s, 1), ("lg", s)], writes=[("gts", gi)])
        hkeys = lambda t: [(f"hA{t}", k) for k in range(8)]
        groups = [(0, 8), (8, 16), (16, 22)]
        wgu_i = 0
        wd_i = 0
        sil_i = 0
        for ex in range(E):
            if E > 1:
                gb = gB[ex % 2]
                gbk = f"gB{ex % 2}"
                for gi in range(16):
                    kb.op("dve", lambda e, gi=gi, ex=ex: e.tensor_scalar(out=dgate[:], in0=c.ident_f[:], scalar1=gts[:, gi, ex:ex + 1],
                                                                         scalar2=None, op0=ALU.mult),
                          reads=["ident_f", ("gts", gi)], writes=["dgate"])
                    p, pk = c.ps()
                    mm(c, p[:, 0:128], c.ones_f[:], dgate[:], True, True, ["ones_f", "dgate"], [pk])
                    kb.op("act", lambda e, gi=gi, p=p, gb=gb: e.activation(out=gb[:, gi * 128:(gi + 1) * 128], in_=p[:, 0:128], func=AF.Copy),
                          reads=[pk], writes=[(gbk, gi // 4)])
            for fa, fb in groups:
                nfg = fb - fa
                for f0 in range(fa, fb, 2):
                    nf = min(2, fb - f0)
                    sg, su = wgu[wgu_i % 3], wgu[(wgu_i + 1) % 3]
                    sgk, suk = f"wgu{wgu_i % 3}", f"wgu{(wgu_i + 1) % 3}"
                    wgu_i += 2
                    kb.dma("pool", sg[:, :, 0:nf * 128], io["w_gate"][ex][:, f0 * 128:(f0 + nf) * 128].rearrange("(k p) n -> p k n", p=128), writes=[sgk])
                    kb.dma("pool", su[:, :, 0:nf * 128], io["w_up"][ex][:, f0 * 128:(f0 + nf) * 128].rearrange("(k p) n -> p k n", p=128), writes=[suk])
                    for fi in range(nf):
                        fl = f0 + fi - fa
                        for t in range(NTT):
                            ts_ = slice(t * TT, (t + 1) * TT)
                            pg, pgk = c.ps()
                            pu, puk = c.ps()
                            for k in range(8):
                                mm(c, pg[:, :], sg[:, k, fi * 128:(fi + 1) * 128], hTall[:, k, ts_], k == 0, k == 7, [sgk, (f"hA{t}", k)], [pgk])
                            for k in range(8):
                                mm(c, pu[:, :], su[:, k, fi * 128:(fi + 1) * 128], hTall[:, k, ts_], k == 0, k == 7, [suk, (f"hA{t}", k)], [puk])
                            sl = sil[sil_i % 2]
                            slk = f"sil{sil_i % 2}"
                            sil_i += 1
                            kb.op("act", lambda e, pg=pg, sl=sl: e.activation(out=sl[:], in_=pg[:, :], func=AF.Silu), reads=[pgk], writes=[slk])
                            kb.op("dve", lambda e, pu=pu, sl=sl, fl=fl, ts_=ts_: e.tensor_tensor(out=actT[:, fl, ts_], in0=pu[:, :], in1=sl[:], op=ALU.mult),
                                  reads=[puk, slk], writes=[("actT", fl, t)])
                slots = []
                for fl in range(nfg):
                    f = fa + fl
                    wd = wdr[wd_i % 11]
                    wdk = f"wdr{wd_i % 11}"
                    wd_i += 1
                    kb.dma("pool", wd[:], io["w_down"][ex][f * 128:(f + 1) * 128, :], writes=[wdk])
                    slots.append((wd, wdk))
                for t in range(NTT):
                    ts_ = slice(t * TT, (t + 1) * TT)
                    for d in range(8):
                        p, pk = c.ps()
                        for fl in range(nfg):
                            wd, wdk = slots[fl]
                            mm(c, p[:, :], wd[:, d * 128:(d + 1) * 128], actT[:, fl, ts_], fl == 0, fl == nfg - 1, [wdk, ("actT", fl, t)], [pk])
                        if E > 1:
                            ytmp = ytmps[d % 2]
                            ytk = f"ytmp{d % 2}"
                            kb.op("dve", lambda e, p=p, gb=gb, ytmp=ytmp, ts_=ts_: e.tensor_tensor(out=ytmp[:], in0=p[:, :], in1=gb[:, ts_], op=ALU.mult),
                                  reads=[pk, (gbk, t)], writes=[ytk])
                            kb.op("dve", lambda e, d=d, ts_=ts_, ytmp=ytmp: e.tensor_tensor(out=xT[:, d, ts_], in0=xT[:, d, ts_], in1=ytmp[:], op=ALU.add),
                                  reads=[ytk, ("xT", d)], writes=[("xT", d)])
                        else:
                            kb.op("dve", lambda e, d=d, p=p, ts_=ts_: e.tensor_tensor(out=xT[:, d, ts_], in0=xT[:, d, ts_], in1=p[:, :], op=ALU.add),
                                  reads=[pk, ("xT", d)], writes=[("xT", d)])
    c.barrier()

    with ExitStack() as s4:
        hT = c.sb("hT", [128, 8, TT], BF16, s4)
        if not last:
            for t in range(NTT):
                t0 = t * TT
                rmsnorm_tile(c, xT, "xT", t0, TT, vecs[:, V_NEXT:V_NEXT + 8], tmp, hT, "hT")
                h_store(c, io["h_next"], hT, t0, TT, [("hT", k) for k in range(8)], ["h_next_d"])
                if t == NTT - 1 and "tail_next" in io:
                    kb.dma("sp", io["tail_next"].rearrange("(k p) n -> p k n", p=128), hT[:, :, TT - 32:TT],
                           reads=[("hT", k) for k in range(8)], writes=["tail_next_d"])
        else:
            hF2 = c.sb("hF2", [128, 8, TT], F32, s4)
            otm = c.sb("otm", [128, 4, D], F32, s4)
            for t in range(NTT):
                t0 = t * TT
                rmsnorm_tile(c, xT, "xT", t0, TT, vecs[:, V_NEXT:V_NEXT + 8], tmp, hT, "hT", out_f=hF2)
                for s in range(4):
                    for kk in range(2):
                        p, pk = c.ps()
                        for k4 in range(4):
                            k = kk * 4 + k4
                            kb.op("pe", lambda e, k=k, k4=k4, s=s, p=p: e.transpose(out=p[:, k4 * 128:(k4 + 1) * 128],
                                                                                    in_=hF2[:, k, s * 128:(s + 1) * 128], identity=c.ident_f[:]),
                                  reads=[("hT_f", k), "ident_f"], writes=[pk])
                        kb.op("act", lambda e, s=s, kk=kk, p=p: e.activation(out=otm[:, s, kk * 512:(kk + 1) * 512], in_=p[:, :], func=AF.Copy),
                              reads=[pk], writes=[("otm", s)])
                kb.dma("sp", io["out"][t0:t0 + TT, :].rearrange("(s p) d -> p s d", p=128), otm[:],
                       reads=[("otm", s) for s in range(4)])
    c.barrier()
    vec_st.close()


from contextlib import ExitStack as _ES
import ml_dtypes as _mld

NPBF = _mld.bfloat16


def build_T(E, last, dbg_stage=0):
    nc = bass.Bass("TRN2", target_bir_lowering=False)
    io = {}

    def din(name, shape, dt=F32):
        io[name] = nc.dram_tensor(name, shape, dt, kind="ExternalInput").ap()

    def dout(name, shape, dt=F32):
        io[name] = nc.dram_tensor(name, shape, dt, kind="ExternalOutput").ap()

    din("xT_in", [D, NT]); din("h_own", [D, NT], BF16); din("h_halo", [D, 32], BF16)
    din("catm", [4, 64, NT], BF16); din("catf", [4, 128, NT], BF16); din("mem", [256, D])
    din("vecs", [128, NV_T]); din("w_c", [D, 512]); din("w_out", [D, D]); din("w_q", [D, 512])
    din("w_kv", [D, D]); din("w_o", [512, D])
    din("w_gate", [E, D, DFF]); din("w_up", [E, D, DFF]); din("w_down", [E, DFF, D])
    if E > 1:
        din("router_w", [D, 8])
    if last:
        dout("out", [NT, D])
    else:
        dout("xT_out", [D, NT]); dout("h_next", [D, NT], BF16)
    io["dbg_stage"] = dbg_stage
    with _ES() as st:
        c = Ctx(nc, st)
        c.setup()
        xT = c.sb("xT", [128, 8, NT], F32)
        c.kb.dma("sp", xT[:], io["xT_in"].rearrange("(k p) n -> p k n", p=128), writes=[("xT", k) for k in range(8)])
        phase_T(c, io, E, last and not dbg_stage, xT)
        evs = []
        if not last or dbg_stage:
            key = "xT_out" if not last else "out"
            if last:
                io["xT_dbg"] = None
            evs.append(c.kb.dma("sp", io["xT_out"].rearrange("(k p) n -> p k n", p=128), xT[:],
                                reads=[("xT", k) for k in range(8)]))
        c.barrier()
        c.kb.flush()
    return nc


def vecs_T(inp, l, last):
    v = np.zeros((128, NV_T), np.float32)
    fm = lambda w: np.asarray(w, np.float32).reshape(-1, 128).T
    v[:, 0:8] = fm(inp["norm_xattn_w"][l]); v[:, 8:16] = fm(inp["norm_mem_w"][l]); v[:, 16:24] = fm(inp["norm_ffn_w"][l])
    v[:, 24:32] = fm(inp["norm_final_w"]) if last else fm(inp["norm_mix_w"][l + 1])
    v[:, 32:34] = fm(inp["conf_conv_b"][l]); v[:, 34:36] = fm(inp["conf_ln_w"][l]); v[:, 36:38] = fm(inp["conf_ln_b"][l])
    cw = np.asarray(inp["conf_conv_w"][l], np.float32)
    for j in range(31):
        v[:, 38 + 2 * j:40 + 2 * j] = fm(cw[j])
    return v


NCH = SEQ // 64
NKT = SEQ // 128
NQT = SEQ // TT
GRP = 4


def AP3(t, off, dims):
    return bass.AP(t[:].tensor, off, [list(d) for d in dims])


def log_sigmoid_tile(c, x, out, tmp1, tmp2, bias_ap, keys):
    kb = c.kb
    kx, ko, k1, k2 = keys
    kb.op("dve", lambda e: e.tensor_scalar(out=x, in0=x, scalar1=bias_ap, scalar2=None, op0=ALU.add), reads=[kx, "vecsM"], writes=[kx])
    kb.op("dve", lambda e: e.tensor_scalar(out=tmp1, in0=x, scalar1=-1.0, scalar2=None, op0=ALU.mult), reads=[kx], writes=[k1])
    kb.op("dve", lambda e: e.tensor_tensor(out=tmp1, in0=tmp1, in1=x, op=ALU.max), reads=[kx, k1], writes=[k1])
    kb.op("act", lambda e: e.activation(out=tmp1, in_=tmp1, func=AF.Exp, scale=-1.0), reads=[k1], writes=[k1])
    kb.op("dve", lambda e: e.tensor_scalar(out=tmp1, in0=tmp1, scalar1=1.0, scalar2=None, op0=ALU.add), reads=[k1], writes=[k1])
    kb.op("act", lambda e: e.activation(out=tmp1, in_=tmp1, func=AF.Ln), reads=[k1], writes=[k1])
    kb.op("dve", lambda e: e.tensor_scalar(out=tmp2, in0=x, scalar1=0.0, scalar2=None, op0=ALU.min), reads=[kx], writes=[k2])
    kb.op("dve", lambda e: e.tensor_tensor(out=out, in0=tmp2, in1=tmp1, op=ALU.subtract), reads=[k1, k2], writes=[ko])


def phase_M_mlstm(c, io):
    nc, kb = c.nc, c.kb
    from contextlib import ExitStack
    with ExitStack() as s0:
        vm = c.sb("vecsM_sb", [128, 16], F32, s0)
        wml = c.sb("wml", [128, 8, 258], BF16, s0)
        qT = c.sb("m_qT", [64, SEQ], BF16, s0)
        kT = c.sb("m_kT", [64, SEQ], BF16, s0)
        Vaug = c.sb("m_Vaug", [64, NCH, 65], BF16, s0)
        og = c.sb("m_og", [64, SEQ], BF16, s0)
        iC = c.sb("m_iC", [128, 64], F32, s0)
        fC = c.sb("m_fC", [128, 64], F32, s0)
        eps_t = c.sb("m_eps", [128, 1], F32, s0)
        kb.dma("sp", vm[:], io["vecsM"], writes=["vecsM"])
        kb.dma("pool", wml[:], io["w_ml"].rearrange("(k p) n -> p k n", p=128), writes=["wml"])
        kb.op("pool", lambda e: e.memset(Vaug[:, :, 64:65], 1.0), writes=["Vaug1"])
        kb.op("pool", lambda e: e.memset(eps_t[:], EPS), writes=["m_eps"])
        with ExitStack() as s1:
            hTb = [c.sb(f"m_hT{i}", [128, 8, TT], BF16, s1) for i in range(2)]
            zq = c.sb("m_zq", [64, TT + 3], F32, s1)
            zk = c.sb("m_zk", [64, TT + 3], F32, s1)
            cacc = [c.sb(f"m_cacc{i}", [64, TT], F32, s1) for i in range(2)]
            vt = c.sb("m_vt", [64, TT], F32, s1)
            rows = [c.sb(f"m_rows{i}", [2, TT], F32, s1) for i in range(2)]
            kb.op("pool", lambda e: e.memset(zq[:, 0:3], 0.0), writes=["zq"])
            kb.op("pool", lambda e: e.memset(zk[:, 0:3], 0.0), writes=["zk"])
            def m_load(tt):
                h_load(c, io["hT_all"], hTb[tt % 2], (tt % 4) * TT, TT, [f"m_hT{tt % 2}"], j=tt // 4)

            m_load(0)
            for tt in range(NQT):
                if tt + 1 < NQT:
                    m_load(tt + 1)
                hT = hTb[tt % 2]
                hk = f"m_hT{tt % 2}"
                tok = slice(tt * TT, (tt + 1) * TT)

                def proj(c0, c1, np_):
                    p, pk = c.ps()
                    for k in range(8):
                        mm(c, p[0:np_, :], wml[:, k, c0:c1], hT[:, k, :], k == 0, k == 7, ["wml", hk], [pk])
                    return p, pk
                pq, pqk = proj(0, 64, 64)
                pkk, pkkk = proj(64, 128, 64)
                pv, pvk = proj(128, 192, 64)
                pog, pogk = proj(192, 256, 64)
                pif, pifk = proj(256, 258, 2)
                rw = rows[tt % 2]
                rk = f"m_rows{tt % 2}"
                kb.op("act", lambda e, p=pq: e.activation(out=zq[:, 3:TT + 3], in_=p[0:64, :], func=AF.Copy), reads=[pqk], writes=["zq"])
                kb.op("act", lambda e, p=pkk: e.activation(out=zk[:, 3:TT + 3], in_=p[0:64, :], func=AF.Copy), reads=[pkkk], writes=["zk"])
                kb.op("act", lambda e, p=pv: e.activation(out=vt[:], in_=p[0:64, :], func=AF.Copy), reads=[pvk], writes=["m_vt"])
                kb.op("act", lambda e, p=pog, tok=tok: e.activation(out=og[:, tok], in_=p[0:64, :], func=AF.Sigmoid), reads=[pogk], writes=[("og", tt)])
                kb.op("act", lambda e, p=pif, rw=rw: e.activation(out=rw[:], in_=p[0:2, :], func=AF.Copy), reads=[pifk], writes=[rk])
                p2, p2k = c.ps()
                for ci in range(8):
                    kb.op("pe", lambda e, ci=ci, p2=p2: e.transpose(out=p2[0:64, ci * 64:(ci + 1) * 64], in_=vt[:, ci * 64:(ci + 1) * 64], identity=c.ident_f[0:64, 0:64]),
                          reads=["m_vt", "ident_f"], writes=[p2k])
                kb.op("dve", lambda e, p2=p2, tt=tt: e.tensor_copy(out=Vaug[:, tt * 8:(tt + 1) * 8, 0:64], in_=p2[0:64, :].rearrange("p (c d) -> p c d", d=64)),
                      reads=[p2k], writes=[("Vaug", tt)])
                for nm, z, vc, dst in (("q", zq, 0, qT), ("k", zk, 5, kT)):
                    zkey = "z" + nm
                    ca = cacc[0 if nm == "q" else 1]
                    ck = "cacc" + nm
                    kb.op("dve", lambda e, z=z, ca=ca, vc=vc: e.tensor_scalar(out=ca[:], in0=z[:, 0:TT], scalar1=vm[0:64, vc:vc + 1], scalar2=vm[0:64, vc + 4:vc + 5],
                                                                              op0=ALU.mult, op1=ALU.add), reads=[zkey, "vecsM"], writes=[ck])
                    for jj in range(1, 4):
                        kb.op("dve", lambda e, z=z, ca=ca, vc=vc, jj=jj: e.scalar_tensor_tensor(out=ca[:], in0=z[:, jj:jj + TT], scalar=vm[0:64, vc + jj:vc + jj + 1], in1=ca[:],
                                                                                                 op0=ALU.mult, op1=ALU.add), reads=[zkey, ck, "vecsM"], writes=[ck])
                    kb.op("act", lambda e, ca=ca, dst=dst, tok=tok: e.activation(out=dst[:, tok], in_=ca[:], func=AF.Silu), reads=[ck], writes=[("m_" + nm + "T", tt)])
                    kb.op("dve", lambda e, z=z: e.tensor_copy(out=z[:, 0:3], in_=z[:, TT:TT + 3]), reads=[zkey], writes=[zkey])
                kb.dma("sp", iC[tt * 8:(tt + 1) * 8, :], AP3(rw, 0, [[TT, 1], [64, 8], [1, 64]]), reads=[rk], writes=["iC"])
                kb.dma("sp", fC[tt * 8:(tt + 1) * 8, :], AP3(rw, TT, [[TT, 1], [64, 8], [1, 64]]), reads=[rk], writes=["fC"])
        c.barrier()
        Uall = c.sb("m_Uall", [64, 65, NCH], F32, s0)
        wgT = c.sb("m_wgT", [64, NCH], F32, s0)
        flT = c.sb("m_flT", [64, NCH], F32, s0)
        dB = c.sb("m_dB", [64, NCH], F32, s0)
        dB0 = c.sb("m_dB0", [64, NCH], F32, s0)
        with ExitStack() as s2:
            t1 = c.sb("g_t1", [128, 64], F32, s2)
            t2 = c.sb("g_t2", [128, 64], F32, s2)
            lf = c.sb("g_lf", [128, 64], F32, s2)
            bb = c.sb("g_b", [128, 64], F32, s2)
            aa = c.sb("g_a", [128, 64], F32, s2)
            AA = c.sb("g_A", [128, 64], F32, s2)
            MM = c.sb("g_M", [128, 64], F32, s2)
            wg = c.sb("g_wg", [128, 64], F32, s2)
            fl = c.sb("g_fl", [128, 64], F32, s2)
            on = c.sb("g_on", [128, 64], F32, s2)
            r1 = c.sb("g_r1", [1, 128], F32, s2)
            r2 = c.sb("g_r2", [1, 128], F32, s2)
            r3 = c.sb("g_r3", [1, 128], F32, s2)
            r4 = c.sb("g_r4", [1, 128], F32, s2)
            mcol = c.sb("g_mcol", [128, 1], F32, s2)
            nM63 = c.sb("g_nM63", [128, 1], F32, s2)
            dec = c.sb("g_dec", [128, 1], F32, s2)
            dgd = c.sb("g_dgd", [128, 128], F32, s2)
            Xb = [c.sb(f"g_X{i}", [128, 8, 64], F32, s2) for i in range(2)]
            kw32 = [c.sb(f"g_kw32{i}", [64, TT], F32, s2) for i in range(2)]
            kwTok = c.sb("g_kwTok", [64, NCH, 64], BF16, s2)
            log_sigmoid_tile(c, fC[:], lf[:], t1[:], t2[:], vm[:, 12:13], ("fC", "g_lf", "g_t1", "g_t2"))
            kb.op("dve", lambda e: e.tensor_scalar(out=iC[:], in0=iC[:], scalar1=vm[:, 11:12], scalar2=None, op0=ALU.add), reads=["iC", "vecsM"], writes=["iC"])
            kb.op("pool", lambda e: e.memset(on[:], 1.0), writes=["g_on"])
            kb.op("dve", lambda e: e.tensor_tensor_scan(out=bb[:], data0=on[:], data1=lf[:], initial=0.0, op0=ALU.mult, op1=ALU.add),
                  reads=["g_on", "g_lf"], writes=["g_b"])
            kb.op("dve", lambda e: e.tensor_tensor(out=aa[:], in0=iC[:], in1=bb[:], op=ALU.subtract), reads=["iC", "g_b"], writes=["g_a"])
            kb.op("dve", lambda e: e.tensor_tensor_scan(out=AA[:], data0=aa[:], data1=aa[:], initial=-1e30, op0=ALU.max, op1=ALU.max),
                  reads=["g_a"], writes=["g_A"])
            p, pk = c.ps()
            kb.op("pe", lambda e, p=p: e.transpose(out=p[0:1, 0:128], in_=AA[:, 63:64], identity=c.ident_f[:]), reads=["g_A", "ident_f"], writes=[pk])
            kb.op("pe", lambda e, p=p: e.transpose(out=p[0:1, 128:256], in_=bb[:, 63:64], identity=c.ident_f[:]), reads=["g_b", "ident_f"], writes=[pk])
            kb.op("dve", lambda e, p=p: e.tensor_copy(out=r1[:], in_=p[0:1, 0:128]), reads=[pk], writes=["g_r1"])
            kb.op("dve", lambda e, p=p: e.tensor_copy(out=r2[:], in_=p[0:1, 128:256]), reads=[pk], writes=["g_r2"])
            kb.op("dve", lambda e: e.tensor_tensor_scan(out=r3[:], data0=r1[:], data1=r2[:], initial=0.0, op0=ALU.max, op1=ALU.add),
                  reads=["g_r1", "g_r2"], writes=["g_r3"])
            kb.op("pool", lambda e: e.memset(r4[:, 0:1], 0.0), writes=["g_r4a"])
            kb.op("dve", lambda e: e.tensor_copy(out=r4[:, 1:128], in_=r3[:, 0:127]), reads=["g_r3"], writes=["g_r4b"])
            p, pk = c.ps()
            kb.op("pe", lambda e, p=p: e.transpose(out=p[:, 0:1], in_=r4[:], identity=c.ident_f[0:1, 0:1]), reads=["g_r4a", "g_r4b", "ident_f"], writes=[pk])
            kb.op("dve", lambda e, p=p: e.tensor_copy(out=mcol[:], in_=p[:, 0:1]), reads=[pk], writes=["g_mcol"])
            kb.op("dve", lambda e: e.tensor_scalar(out=MM[:], in0=AA[:], scalar1=mcol[:, 0:1], scalar2=None, op0=ALU.max), reads=["g_A", "g_mcol"], writes=["g_M"])
            kb.op("dve", lambda e: e.tensor_scalar(out=nM63[:], in0=MM[:, 63:64], scalar1=-1.0, scalar2=None, op0=ALU.mult), reads=["g_M"], writes=["g_nM63"])
            kb.op("act", lambda e: e.activation(out=wg[:], in_=aa[:], func=AF.Exp, bias=nM63[:, 0:1]), reads=["g_a", "g_nM63"], writes=["g_wg"])
            kb.op("act", lambda e: e.activation(out=dec[:], in_=mcol[:], func=AF.Exp, bias=nM63[:, 0:1]), reads=["g_mcol", "g_nM63"], writes=["g_dec"])
            kb.op("act", lambda e: e.activation(out=fl[:], in_=bb[:], func=AF.Exp, scale=-1.0, bias=nM63[:, 0:1]), reads=["g_b", "g_nM63"], writes=["g_fl"])
            p, pk = c.ps()
            kb.op("pe", lambda e, p=p: e.transpose(out=p[0:64, 0:128], in_=wg[:], identity=c.ident_f[:]), reads=["g_wg", "ident_f"], writes=[pk])
            kb.op("pe", lambda e, p=p: e.transpose(out=p[0:64, 128:256], in_=fl[:], identity=c.ident_f[:]), reads=["g_fl", "ident_f"], writes=[pk])
            kb.op("dve", lambda e, p=p: e.tensor_copy(out=wgT[:], in_=p[0:64, 0:128]), reads=[pk], writes=["m_wgT"])
            kb.op("dve", lambda e, p=p: e.tensor_copy(out=flT[:], in_=p[0:64, 128:256]), reads=[pk], writes=["m_flT"])
            kb.op("dve", lambda e: e.tensor_scalar(out=dgd[:], in0=c.ident_f[:], scalar1=dec[:, 0:1], scalar2=None, op0=ALU.mult), reads=["ident_f", "g_dec"], writes=["g_dgd"])
            p, pk = c.ps()
            mm(c, p[0:64, 0:128], c.ones_f[:, 0:64], dgd[:], True, True, ["ones_f", "g_dgd"], [pk])
            kb.op("dve", lambda e, p=p: e.tensor_copy(out=dB[:], in_=p[0:64, 0:128]), reads=[pk], writes=["m_dB"])
            kb.op("dve", lambda e, p=p: e.tensor_copy(out=dB0[:], in_=p[0:64, 0:128]), reads=[pk], writes=["m_dB0"])
            kb.op("pool", lambda e: e.memset(dB0[:, 0:1], 0.0), reads=["m_dB0"], writes=["m_dB0"])
            wgb = {}

            def a2_bcast(tt):
                X = Xb[tt % 2]
                Xk = f"g_X{tt % 2}"
                kb.op("dve", lambda e, X=X, tt=tt: e.tensor_tensor(out=X[:], in0=AP3(c.ident_f, 8 * tt, [[128, 128], [1, 8], [0, 64]]),
                                                                   in1=AP3(wg, 0, [[64, 128], [0, 8], [1, 64]]), op=ALU.mult),
                      reads=["ident_f", "g_wg"], writes=[Xk])
                p, pk = c.ps()
                mm(c, p[0:64, :], c.ones_f[:, 0:64], X[:].rearrange("p c s -> p (c s)"), True, True, ["ones_f", Xk], [pk])
                wgb[tt] = (p, pk)

            a2_bcast(0)
            for tt in range(NQT):
                tok = slice(tt * TT, (tt + 1) * TT)
                if tt + 1 < NQT:
                    a2_bcast(tt + 1)
                p, pk = wgb.pop(tt)
                k32 = kw32[tt % 2]
                k32k = f"g_kw32{tt % 2}"
                kb.op("dve", lambda e, p=p, k32=k32, tok=tok: e.scalar_tensor_tensor(out=k32[:], in0=kT[:, tok], scalar=0.125, in1=p[0:64, :], op0=ALU.mult, op1=ALU.mult),
                      reads=[pk, ("m_kT", tt)], writes=[k32k])
                kb.op("act", lambda e, k32=k32, tok=tok: e.activation(out=kT[:, tok], in_=k32[:], func=AF.Copy), reads=[k32k], writes=[("m_kT", tt)])
                p2, p2k = c.ps()
                for ci in range(8):
                    kb.op("pe", lambda e, ci=ci, p2=p2, k32=k32: e.transpose(out=p2[0:64, ci * 64:(ci + 1) * 64], in_=k32[:, ci * 64:(ci + 1) * 64], identity=c.ident_f[0:64, 0:64]),
                          reads=[k32k, "ident_f"], writes=[p2k])
                kb.op("dve", lambda e, p2=p2, tt=tt: e.tensor_copy(out=kwTok[:, tt * 8:(tt + 1) * 8, :], in_=p2[0:64, :].rearrange("p (c d) -> p c d", d=64)),
                      reads=[p2k], writes=[("kwTok", tt)])
            for g in range(NCH // GRP):
                p, pk = c.ps()
                for ci in range(GRP):
                    ch = g * GRP + ci
                    mm(c, p[0:64, ci * 65:(ci + 1) * 65], kwTok[:, ch, :], Vaug[:, ch, :], True, True, [("kwTok", ch // 8), ("Vaug", ch // 8), "Vaug1"], [pk])
                kb.op("dve", lambda e, p=p, g=g: e.tensor_copy(out=AP3(Uall, g * GRP, [[65 * NCH, 64], [1, GRP], [NCH, 65]]),
                                                               in_=p[0:64, 0:GRP * 65].rearrange("p (c d) -> p c d", d=65)),
                      reads=[pk], writes=["Uall"])
        c.barrier()
        Cn = Uall
        Eb = c.sb("m_E", [64, NCH, 65], BF16, s0)
        for dv in range(65):
            kb.op("dve", lambda e, dv=dv: e.tensor_tensor_scan(out=Cn[:, dv, :], data0=dB0[:], data1=Uall[:, dv, :], initial=0.0, op0=ALU.mult, op1=ALU.add),
                  reads=["m_dB0", "Uall"], writes=[("Cn", dv), "Uall"])
        kb.op("pool", lambda e: e.memset(Eb[:, 0:1, :], 0.0), writes=["E0"])
        kb.op("dve", lambda e: e.tensor_tensor(out=Eb[:, 1:NCH, :], in0=AP3(Cn, 0, [[65 * NCH, 64], [1, NCH - 1], [NCH, 65]]),
                                               in1=AP3(dB, 1, [[NCH, 64], [1, NCH - 1], [0, 65]]), op=ALU.mult),
              reads=[("Cn", dv) for dv in range(65)] + ["m_dB"], writes=["E"])
        with ExitStack() as s4:
            mask = c.sb("o_mask", [64, 64], F32, s4)
            sT = [c.sb(f"o_sT{i}", [64, GRP * 64], BF16, s4) for i in range(2)]
            den = c.sb("o_den", [64, GRP], F32, s4)
            hn = c.sb("o_hn", [64, GRP, 64], F32, s4)
            hsq = c.sb("o_hsq", [64, GRP, 64], F32, s4)
            ss = c.sb("o_ss", [64, GRP], F32, s4)
            cst = [c.sb(f"o_cst{i}", [64, GRP * 64], BF16, s4) for i in range(2)]
            kb.op("pool", lambda e: e.memset(mask[:], 1.0), writes=["o_mask"])
            kb.op("pool", lambda e: e.affine_select(out=mask[:], in_=mask[:], pattern=[[1, 64]], compare_op=ALU.is_ge, fill=0.0, base=0, channel_multiplier=-1),
                  reads=["o_mask"], writes=["o_mask"])
            pend = {}

            def a4_stage1(g):
                c0 = g * GRP
                p, pk = c.ps()
                for ci in range(GRP):
                    ch = c0 + ci
                    cs = slice(ch * 64, (ch + 1) * 64)
                    mm(c, p[0:64, ci * 64:(ci + 1) * 64], kT[:, cs], qT[:, cs], True, True, [("m_kT", ch // 8), ("m_qT", ch // 8)], [pk])
                st_ = sT[g % 2]
                stk = f"o_sT{g % 2}"
                kb.op("dve", lambda e, p=p, st_=st_: e.tensor_tensor(out=st_[:].rearrange("p (c t) -> p c t", t=64), in0=p[0:64, 0:GRP * 64].rearrange("p (c t) -> p c t", t=64),
                                                                     in1=AP3(mask, 0, [[64, 64], [0, GRP], [1, 64]]), op=ALU.mult),
                      reads=[pk, "o_mask"], writes=[stk])
                po, pok = c.ps()
                for ci in range(GRP):
                    ch = c0 + ci
                    cs = slice(ch * 64, (ch + 1) * 64)
                    mm(c, po[0:64, ci * 65:(ci + 1) * 65], st_[:, ci * 64:(ci + 1) * 64], Vaug[:, ch, :], True, False, [stk, ("Vaug", ch // 8), "Vaug1"], [pok])
                    mm(c, po[0:64, ci * 65:(ci + 1) * 65], qT[:, cs], Eb[:, ch, :], False, True, [("m_qT", ch // 8), "E", "E0"], [pok])
                pend[g] = (po, pok)

            def a4_stage2(g):
                c0 = g * GRP
                po, pok = pend.pop(g)
                po3 = po[0:64, 0:GRP * 65].rearrange("p (c d) -> p c d", d=65)
                den3 = den[:].rearrange("p (c o) -> p c o", o=1)
                kb.op("dve", lambda e, po3=po3, den3=den3: e.tensor_scalar(out=den3, in0=po3[:, :, 64:65], scalar1=-1.0, scalar2=None, op0=ALU.mult),
                      reads=[pok], writes=["o_den"])
                kb.op("dve", lambda e, po3=po3, den3=den3: e.tensor_tensor(out=den3, in0=po3[:, :, 64:65], in1=den3, op=ALU.max),
                      reads=[pok, "o_den"], writes=["o_den"])
                kb.op("dve", lambda e, c0=c0: e.tensor_tensor(out=den[:], in0=den[:], in1=flT[:, c0:c0 + GRP], op=ALU.max),
                      reads=["o_den", "m_flT"], writes=["o_den"])
                kb.op("dve", lambda e: e.reciprocal(out=den[:], in_=den[:]), reads=["o_den"], writes=["o_den"])
                kb.op("dve", lambda e, po3=po3: e.tensor_tensor(out=hn[:], in0=po3[:, :, 0:64], in1=AP3(den, 0, [[GRP, 64], [1, GRP], [0, 64]]), op=ALU.mult),
                      reads=[pok, "o_den"], writes=["o_hn"])
                kb.op("act", lambda e: e.activation(out=hsq[:], in_=hn[:], func=AF.Square), reads=["o_hn"], writes=["o_hsq"])
                kb.op("dve", lambda e: e.tensor_reduce(out=ss[:], in_=hsq[:], axis=AX.X, op=ALU.add), reads=["o_hsq"], writes=["o_ss"])
                kb.op("act", lambda e: e.activation(out=ss[:], in_=ss[:], func=AF.Sqrt, scale=1.0 / 64, bias=eps_t[0:64, 0:1]), reads=["o_ss", "m_eps"], writes=["o_ss"])
                kb.op("dve", lambda e: e.reciprocal(out=ss[:], in_=ss[:]), reads=["o_ss"], writes=["o_ss"])
                kb.op("dve", lambda e: e.tensor_tensor(out=hn[:], in0=hn[:], in1=AP3(ss, 0, [[GRP, 64], [1, GRP], [0, 64]]), op=ALU.mult),
                      reads=["o_hn", "o_ss"], writes=["o_hn"])
                pt, ptk = c.ps()
                for ci in range(GRP):
                    kb.op("pe", lambda e, ci=ci, pt=pt: e.transpose(out=pt[0:64, ci * 64:(ci + 1) * 64], in_=hn[:, ci, :], identity=c.ident_f[0:64, 0:64]),
                          reads=["o_hn", "ident_f"], writes=[ptk])
                cs_ = cst[g % 2]
                csk = f"o_cst{g % 2}"
                toks = slice(c0 * 64, (c0 + GRP) * 64)
                kb.op("dve", lambda e, pt=pt, cs_=cs_, toks=toks: e.scalar_tensor_tensor(out=cs_[:], in0=pt[0:64, 0:GRP * 64], scalar=vm[0:64, 10:11], in1=og[:, toks],
                                                                                       op0=ALU.mult, op1=ALU.mult),
                      reads=[ptk, "vecsM", ("og", (c0 * 64) // TT)], writes=[csk])
                kb.dma("sp", io["catm_out"][:, toks], cs_[:], reads=[csk])

            NG4 = NCH // GRP
            a4_stage1(0)
            for g in range(NG4):
                if g + 1 < NG4:
                    a4_stage1(g + 1)
                a4_stage2(g)
        c.barrier()


def phase_M_fox(c, io):
    nc, kb = c.nc, c.kb
    from contextlib import ExitStack
    NEG = -30000.0
    with ExitStack() as s0:
        vm = c.sb("vecsMf_sb", [128, 16], F32, s0)
        wfx = c.sb("wfx", [128, 8, 386], BF16, s0)
        fq = [c.sb(f"f_q{h}", [128, SEQ], BF16, s0) for h in range(2)]
        fkk = c.sb("f_kk", [128, SEQ], BF16, s0)
        kb.op("pool", lambda e: e.memset(fq[0][64:128, :], 0.0), writes=[("fqz", 0)])
        kb.op("pool", lambda e: e.memset(fq[1][0:64, :], 0.0), writes=[("fqz", 1)])
        fV = [c.sb(f"f_V{h}", [128, NKT, 65], BF16, s0) for h in range(2)]
        fC = [c.sb(f"f_C{h}", [64, 128], F32, s0) for h in range(2)]
        kb.dma("sp", vm[:], io["vecsM"], writes=["vecsM"])
        kb.dma("pool", wfx[:], io["w_fx"].rearrange("(k p) n -> p k n", p=128), writes=["wfx"])
        for h in range(2):
            kb.op("pool", lambda e, h=h: e.memset(fV[h][:, :, 64:65], 1.0), writes=[("fV1", h)])
        with ExitStack() as s1:
            hTb = [c.sb(f"f_hT{i}", [128, 8, TT], BF16, s1) for i in range(2)]
            vt = c.sb("f_vt", [128, TT], F32, s1)
            rows = [c.sb(f"f_rows{i}", [2, TT], F32, s1) for i in range(2)]
            def f_load(tt):
                h_load(c, io["hT_all"], hTb[tt % 2], (tt % 4) * TT, TT, [f"f_hT{tt % 2}"], j=tt // 4)

            f_load(0)
            for tt in range(NQT):
                if tt + 1 < NQT:
                    f_load(tt + 1)
                hT = hTb[tt % 2]
                hk = f"f_hT{tt % 2}"
                tok = slice(tt * TT, (tt + 1) * TT)
                p, pk = c.ps()
                for k in range(8):
                    mm(c, p[:, :], wfx[:, k, 0:128], hT[:, k, :], k == 0, k == 7, ["wfx", hk], [pk])
                kb.op("act", lambda e, p=p, tok=tok: e.activation(out=fq[0][0:64, tok], in_=p[0:64, :], func=AF.Copy, scale=0.125), reads=[pk], writes=[("fq", 0, tt)])
                kb.op("act", lambda e, p=p, tok=tok: e.activation(out=fq[1][64:128, tok], in_=p[64:128, :], func=AF.Copy, scale=0.125), reads=[pk], writes=[("fq", 1, tt)])
                p, pk = c.ps()
                for k in range(8):
                    mm(c, p[:, :], wfx[:, k, 128:256], hT[:, k, :], k == 0, k == 7, ["wfx", hk], [pk])
                kb.op("act", lambda e, p=p, tok=tok: e.activation(out=fkk[:, tok], in_=p[:, :], func=AF.Copy), reads=[pk], writes=[("fk", tt)])
                p, pk = c.ps()
                for k in range(8):
                    mm(c, p[:, :], wfx[:, k, 256:384], hT[:, k, :], k == 0, k == 7, ["wfx", hk], [pk])
                kb.op("act", lambda e, p=p: e.activation(out=vt[:], in_=p[:, :], func=AF.Copy), reads=[pk], writes=["f_vt"])
                p2, p2k = c.ps()
                for ci in range(4):
                    kb.op("pe", lambda e, ci=ci, p2=p2: e.transpose(out=p2[:, ci * 128:(ci + 1) * 128], in_=vt[:, ci * 128:(ci + 1) * 128], identity=c.ident_f[:]),
                          reads=["f_vt", "ident_f"], writes=[p2k])
                for h in range(2):
                    kb.op("dve", lambda e, p2=p2, tt=tt, h=h: e.tensor_copy(out=fV[h][:, tt * 4:(tt + 1) * 4, 0:64],
                                                                         in_=p2[:, :].rearrange("p (c d) -> p c d", d=128)[:, :, h * 64:(h + 1) * 64]),
                          reads=[p2k], writes=[("fV", h, tt)])
                p, pk = c.ps()
                for k in range(8):
                    mm(c, p[0:2, :], wfx[:, k, 384:386], hT[:, k, :], k == 0, k == 7, ["wfx", hk], [pk])
                rw = rows[tt % 2]
                rk = f"f_rows{tt % 2}"
                kb.op("act", lambda e, p=p, rw=rw: e.activation(out=rw[:], in_=p[0:2, :], func=AF.Copy), reads=[pk], writes=[rk])
                for h in range(2):
                    kb.dma("sp", fC[h][tt * 4:(tt + 1) * 4, :], AP3(rw, h * TT, [[TT, 1], [128, 4], [1, 128]]), reads=[rk], writes=[("fC", h)])
        c.barrier()
        ckT = [c.sb(f"f_ckT{h}", [128, NKT], F32, s0) for h in range(2)]
        cC = [c.sb(f"f_cC{h}", [64, 128], F32, s0) for h in range(2)]
        negm = c.sb("f_negm", [128, 4, TT], F32, s0)
        Ls = c.sb("f_Ls", [64, 64], F32, s0)
        with ExitStack() as s2:
            t1 = c.sb("f_t1", [64, 128], F32, s2)
            t2 = c.sb("f_t2", [64, 128], F32, s2)
            lf = c.sb("f_lf", [64, 128], F32, s2)
            on = c.sb("f_on", [64, 128], F32, s2)
            pre = c.sb("f_pre", [64, 1], F32, s2)
            kb.op("pool", lambda e: e.memset(on[:], 1.0), writes=["f_on"])
            kb.op("pool", lambda e: e.memset(Ls[:], 1.0), writes=["f_Ls"])
            kb.op("pool", lambda e: e.affine_select(out=Ls[:], in_=Ls[:], pattern=[[1, 64]], compare_op=ALU.is_ge, fill=0.0, base=-1, channel_multiplier=-1),
                  reads=["f_Ls"], writes=["f_Ls"])
            for r in range(4):
                kb.op("pool", lambda e, r=r: e.memset(negm[:, r, :], 0.0), writes=[("negm", r)])
                kb.op("pool", lambda e, r=r: e.affine_select(out=negm[:, r, :], in_=negm[:, r, :], pattern=[[1, TT]], compare_op=ALU.is_ge, fill=NEG,
                                                             base=-128 * r, channel_multiplier=-1), reads=[("negm", r)], writes=[("negm", r)])
            for h in range(2):
                log_sigmoid_tile(c, fC[h][:], lf[:], t1[:], t2[:], vm[0:64, 13 + h:14 + h], (("fC", h), "f_lf", "f_t1", "f_t2"))
                kb.op("dve", lambda e, h=h: e.tensor_tensor_scan(out=cC[h][:], data0=on[:], data1=lf[:], initial=0.0, op0=ALU.mult, op1=ALU.add),
                      reads=["f_on", "f_lf"], writes=[("cC", h)])
                p, pk = c.ps()
                mm(c, p[0:64, 0:1], Ls[:], cC[h][:, 127:128], True, True, ["f_Ls", ("cC", h)], [pk])
                kb.op("dve", lambda e, p=p: e.tensor_copy(out=pre[:], in_=p[0:64, 0:1]), reads=[pk], writes=["f_pre"])
                kb.op("dve", lambda e, h=h: e.tensor_scalar(out=cC[h][:], in0=cC[h][:], scalar1=pre[:, 0:1], scalar2=None, op0=ALU.add), reads=[("cC", h), "f_pre"], writes=[("cC", h)])
                p, pk = c.ps()
                kb.op("pe", lambda e, p=p, h=h: e.transpose(out=p[:, 0:64], in_=cC[h][:], identity=c.ident_f[0:64, 0:64]), reads=[("cC", h), "ident_f"], writes=[pk])
                kb.op("dve", lambda e, p=p, h=h: e.tensor_scalar(out=ckT[h][:], in0=p[:, 0:64], scalar1=-1.0, scalar2=None, op0=ALU.mult), reads=[pk], writes=[("ckT", h)])
        c.barrier()
        with ExitStack() as s3:
            X = c.sb("f_X", [64, 4, 128], F32, s3)
            cqB = c.sb("f_cqB", [128, TT], F32, s3)
            cqD = c.sb("f_cqD", [128, 4, TT], F32, s3)
            NB = 5
            tmpb = [c.sb(f"f_tmp{i}", [128, TT], F32, s3) for i in range(NB)]
            pTb = [c.sb(f"f_pT{i}", [128, TT], BF16, s3) for i in range(NB)]
            osb = c.sb("f_osb", [65, TT], F32, s3)
            rden = c.sb("f_rden", [64, TT], F32, s3)
            outb = [c.sb(f"f_out{i}", [64, TT], BF16, s3) for i in range(2)]
            it = 0
            for h in range(2):
                for qi in range(NQT):
                    qs = slice(qi * TT, (qi + 1) * TT)
                    kb.op("dve", lambda e, h=h, qi=qi: e.tensor_tensor(out=X[:], in0=AP3(c.ident_f, 4 * qi, [[128, 64], [1, 4], [0, 128]]),
                                                                       in1=AP3(cC[h], 0, [[128, 64], [0, 4], [1, 128]]), op=ALU.mult),
                          reads=["ident_f", ("cC", h)], writes=["f_X"])
                    p, pk = c.ps()
                    mm(c, p[:, :], c.ones_f[0:64, :], X[:].rearrange("p r s -> p (r s)"), True, True, ["ones_f", "f_X"], [pk])
                    kb.op("act", lambda e, p=p: e.activation(out=cqB[:], in_=p[:, :], func=AF.Copy), reads=[pk], writes=["f_cqB"])
                    for r in range(4):
                        kb.op("pool", lambda e, r=r: e.tensor_tensor(out=cqD[:, r, :], in0=cqB[:], in1=negm[:, r, :], op=ALU.add),
                              reads=["f_cqB", ("negm", r)], writes=[("f_cqD", r)])
                    c.rot = list(range(6))
                    po, pok = c.psb[6 + qi % 2], f"psb{6 + qi % 2}"
                    nk = 4 * (qi + 1)
                    LA = 4
                    sbank = {}

                    def emit_S(kt):
                        ps_, psk = c.ps()
                        mm(c, ps_[:, :], fkk[:, kt * 128:(kt + 1) * 128], fq[h][:, qs], True, True, [("fk", kt // 4), ("fq", h, qi), ("fqz", h)], [psk])
                        sbank[kt] = (ps_, psk)

                    for kt in range(min(LA, nk)):
                        emit_S(kt)
                    for kt in range(nk):
                        ps_, psk = sbank.pop(kt)
                        tb = tmpb[it % NB]; tbk = f"f_tmp{it % NB}"
                        pb = pTb[it % NB]; pbk = f"f_pT{it % NB}"
                        it += 1
                        r = kt - 4 * qi
                        if r >= 0:
                            kb.op("dve", lambda e, ps_=ps_, tb=tb, r=r: e.tensor_tensor(out=tb[:], in0=ps_[:, :], in1=cqD[:, r, :], op=ALU.add),
                                  reads=[psk, ("f_cqD", r)], writes=[tbk])
                        else:
                            kb.op("dve", lambda e, ps_=ps_, tb=tb: e.tensor_tensor(out=tb[:], in0=ps_[:, :], in1=cqB[:], op=ALU.add),
                                  reads=[psk, "f_cqB"], writes=[tbk])
                        kb.op("act", lambda e, tb=tb, pb=pb, h=h, kt=kt: e.activation(out=pb[:], in_=tb[:], func=AF.Exp, bias=ckT[h][:, kt:kt + 1]),
                              reads=[tbk, ("ckT", h)], writes=[pbk])
                        if kt + LA < nk:
                            emit_S(kt + LA)
                        mm(c, po[0:65, :], fV[h][:, kt, :], pb[:], kt == 0, kt == nk - 1, [("fV", h, kt // 4), ("fV1", h), pbk], [pok])
                    kb.op("act", lambda e, po=po: e.activation(out=osb[:], in_=po[0:65, :], func=AF.Copy), reads=[pok], writes=["f_osb"])
                    pd, pdk = c.ps()
                    mm(c, pd[0:64, :], c.ones_f[64:65, 0:64], osb[64:65, :], True, True, ["ones_f", "f_osb"], [pdk])
                    kb.op("dve", lambda e, pd=pd: e.reciprocal(out=rden[:], in_=pd[0:64, :]), reads=[pdk], writes=["f_rden"])
                    ob = outb[qi % 2]; obk = f"f_out{qi % 2}"
                    kb.op("dve", lambda e, ob=ob: e.tensor_tensor(out=ob[:], in0=osb[0:64, :], in1=rden[:], op=ALU.mult), reads=["f_osb", "f_rden"], writes=[obk])
                    cfo = io["catf_out"]
                    kb.dma("sp", (cfo[h][:, qs] if isinstance(cfo, list) else cfo[h * 64:(h + 1) * 64, qs]), ob[:], reads=[obk])
            c.rot = None
        c.barrier()


def build_M(which="both"):
    nc = bass.Bass("TRN2", target_bir_lowering=False)
    io = {}

    def din(name, shape, dt=F32):
        io[name] = nc.dram_tensor(name, shape, dt, kind="ExternalInput").ap()

    def dout(name, shape, dt=F32):
        io[name] = nc.dram_tensor(name, shape, dt, kind="ExternalOutput").ap()

    din("hT_all", [4, D, NT], BF16); din("w_ml", [D, 258]); din("w_fx", [D, 386]); din("vecsM", [128, 16])
    dout("catm_out", [64, SEQ], BF16); dout("catf_out", [128, SEQ], BF16)
    with _ES() as st:
        c = Ctx(nc, st)
        c.setup()
        if which in ("both", "mlstm"):
            phase_M_mlstm(c, io)
        if which in ("both", "fox"):
            phase_M_fox(c, io)
        c.barrier()
        c.kb.flush()
    return nc


def inputs_M(inp, l, g):
    w_in = np.asarray(inp["w_in"][l], np.float32)
    cols = np.concatenate([
        np.arange(g * 64, g * 64 + 64), 256 + np.arange(g * 64, g * 64 + 64),
        512 + np.arange(g * 64, g * 64 + 64), 768 + np.arange(g * 64, g * 64 + 64),
        [1024 + g, 1028 + g]])
    w_ml = np.ascontiguousarray(w_in[:, cols])
    fcols = []
    for base in (1544, 2056, 2568):
        for hh in (2 * g, 2 * g + 1):
            fcols.append(base + np.arange(hh * 64, hh * 64 + 64))
    fcols.append(np.array([3080 + 2 * g, 3080 + 2 * g + 1]))
    w_fx = np.ascontiguousarray(w_in[:, np.concatenate(fcols)])
    v = np.zeros((128, 16), np.float32)
    cw = np.asarray(inp["mlstm_conv_w"][l], np.float32)
    cb = np.asarray(inp["mlstm_conv_b"][l], np.float32)
    v[0:64, 0:4] = cw[:, g * 64:g * 64 + 64].T
    v[0:64, 4] = cb[g * 64:g * 64 + 64]
    v[0:64, 5:9] = cw[:, 256 + g * 64:256 + g * 64 + 64].T
    v[0:64, 9] = cb[256 + g * 64:256 + g * 64 + 64]
    v[0:64, 10] = np.asarray(inp["mlstm_norm_w"][l], np.float32)[g * 64:g * 64 + 64]
    v[:, 11] = inp["mlstm_b_i"][l][g]
    v[:, 12] = inp["mlstm_b_f"][l][g]
    v[:, 13] = inp["fox_b_f"][l][2 * g]
    v[:, 14] = inp["fox_b_f"][l][2 * g + 1]
    return {"w_ml": w_ml, "w_fx": w_fx, "vecsM": v}


def phase_P(c, io, xT):
    kb = c.kb
    from contextlib import ExitStack
    with ExitStack() as s0:
        vecs = c.sb("vecsP_sb", [128, 8], F32, s0)
        eps_t = c.sb("p_eps", [128, 1], F32, s0)
        sq = c.sb("p_sq", [128, 8, TT], BF16, s0)
        rstd = c.sb("p_rstd", [128, TT], F32, s0)
        hT = c.sb("p_hT", [128, 8, TT], BF16, s0)
        xt = [c.sb(f"p_xt{i}", [128, 4, D], F32, s0) for i in range(2)]
        tmp = {"sq": sq, "rstd": rstd, "eps": eps_t}
        kb.dma("sp", vecs[:], io["vecsP"], writes=["vecs"])
        kb.op("pool", lambda e: e.memset(eps_t[:], EPS), writes=["eps"])
        def p_load(t):
            kb.dma("sp", xt[t % 2][:], io["x_tok"][t * TT:(t + 1) * TT, :].rearrange("(s p) d -> p s d", p=128), writes=[f"p_xt{t % 2}"])

        p_load(0)
        for t in range(NTT):
            t0 = t * TT
            xb = xt[t % 2]
            xk = f"p_xt{t % 2}"
            if t + 1 < NTT:
                p_load(t + 1)
            for k in range(8):
                p, pk = c.ps()
                for s in range(4):
                    kb.op("pe", lambda e, k=k, s=s, p=p, xb=xb: e.transpose(out=p[:, s * 128:(s + 1) * 128], in_=xb[:, s, k * 128:(k + 1) * 128], identity=c.ident_f[:]),
                          reads=[xk, "ident_f"], writes=[pk])
                kb.op("act", lambda e, k=k, p=p, t0=t0: e.activation(out=xT[:, k, t0:t0 + TT], in_=p[:, :], func=AF.Copy), reads=[pk], writes=[("xT", k)])
            rmsnorm_tile(c, xT, "xT", t0, TT, vecs[:, 0:8], tmp, hT, "hT")
            h_store(c, io["h_next"], hT, t0, TT, [("hT", k) for k in range(8)], ["h_next_d"])
            if t == NTT - 1 and "tail_next" in io:
                kb.dma("sp", io["tail_next"].rearrange("(k p) n -> p k n", p=128), hT[:, :, TT - 32:TT],
                       reads=[("hT", k) for k in range(8)], writes=["tail_next_d"])
    c.barrier()


def build_P():
    nc = bass.Bass("TRN2", target_bir_lowering=False)
    io = {}
    io["x_tok"] = nc.dram_tensor("x_tok", [NT, D], F32, kind="ExternalInput").ap()
    io["vecsP"] = nc.dram_tensor("vecsP", [128, 8], F32, kind="ExternalInput").ap()
    io["xT_out"] = nc.dram_tensor("xT_out", [D, NT], F32, kind="ExternalOutput").ap()
    io["h_next"] = nc.dram_tensor("h_next", [D, NT], BF16, kind="ExternalOutput").ap()
    with _ES() as st:
        c = Ctx(nc, st)
        c.setup()
        xT = c.sb("xT", [128, 8, NT], F32)
        phase_P(c, io, xT)
        c.kb.dma("sp", io["xT_out"].rearrange("(k p) n -> p k n", p=128), xT[:], reads=[("xT", k) for k in range(8)])
        c.barrier()
        c.kb.flush()
    return nc


_CACHE = {}


def _get(name, fn):
    if name not in _CACHE:
        _CACHE[name] = fn()
    return _CACHE[name]


def kernel(**inp):
    inp = {k: np.asarray(v) for k, v in inp.items()}
    cores = list(range(8))
    B = 2
    x = inp["x"].astype(np.float32, copy=False)
    fm = lambda w: np.ascontiguousarray(np.asarray(w, np.float32).reshape(-1, 128).T)
    ncP = _get("P", build_P)
    maps = []
    for cid in cores:
        b, j = cid // 4, cid % 4
        maps.append({"x_tok": np.ascontiguousarray(x[b, j * NT:(j + 1) * NT]), "vecsP": fm(inp["norm_mix_w"][0])})
    res = run_bass_kernel_spmd(ncP, maps, core_ids=cores).results
    xT = [r["xT_out"] for r in res]
    hN = [r["h_next"] for r in res]
    out = None
    for l in range(2):
        last = (l == 1)
        E = 1 if l == 0 else 8
        ncM = _get("M", build_M)
        maps = []
        for cid in cores:
            b, g = cid // 4, cid % 4
            m = inputs_M(inp, l, g)
            m["hT_all"] = np.ascontiguousarray(np.stack([hN[b * 4 + jj] for jj in range(4)], axis=0))
            maps.append(m)
        resM = run_bass_kernel_spmd(ncM, maps, core_ids=cores).results
        ncT = _get(("T", E, last), lambda: build_T(E, last))
        maps = []
        for cid in cores:
            b, j = cid // 4, cid % 4
            tk = slice(j * NT, (j + 1) * NT)
            m = {"xT_in": xT[cid], "h_own": hN[cid]}
            m["h_halo"] = (np.ascontiguousarray(hN[cid - 1][:, NT - 32:NT]) if j > 0 else np.zeros((D, 32), NPBF))
            m["catm"] = np.ascontiguousarray(np.stack([resM[b * 4 + g]["catm_out"][:, tk] for g in range(4)], axis=0))
            m["catf"] = np.ascontiguousarray(np.stack([resM[b * 4 + g]["catf_out"][:, tk] for g in range(4)], axis=0))
            m["mem"] = np.ascontiguousarray(inp["mem"][b], dtype=np.float32)
            m["vecs"] = vecs_T(inp, l, last)
            m["w_c"] = np.ascontiguousarray(inp["w_in"][l][:, 1032:1544])
            m["w_out"] = inp["w_out"][l]; m["w_q"] = inp["xattn_w_q"][l]
            m["w_kv"] = inp["xattn_w_kv"][l]; m["w_o"] = inp["xattn_w_o"][l]
            if E == 1:
                m["w_gate"] = inp["ffn_w_gate"]; m["w_up"] = inp["ffn_w_up"]; m["w_down"] = inp["ffn_w_down"]
            else:
                m["w_gate"] = inp["moe_w_gate"][0]; m["w_up"] = inp["moe_w_up"][0]; m["w_down"] = inp["moe_w_down"][0]
                m["router_w"] = inp["router_w"][0]
            maps.append(m)
        resT = run_bass_kernel_spmd(ncT, maps, core_ids=cores).results
        if not last:
            xT = [r["xT_out"] for r in resT]
            hN = [r["h_next"] for r in resT]
        else:
            out = np.zeros((B, SEQ, D), np.float32)
            for cid in cores:
                b, j = cid // 4, cid % 4
                out[b, j * NT:(j + 1) * NT] = resT[cid]["out"]
    return out


RG = [[0, 1, 2, 3], [4, 5, 6, 7]]
_STOP = None


def build_fused(stop=None):
    nc = bass.Bass("TRN2", target_bir_lowering=False)
    io = {}
    if stop:
        io["dbg1"] = nc.dram_tensor("dbg1", [4 * D, NT], BF16, kind="ExternalOutput").ap()
        io["dbg2"] = nc.dram_tensor("dbg2", [512, SEQ], BF16, kind="ExternalOutput").ap()
        io["dbg3"] = nc.dram_tensor("dbg3", [D, NT], F32, kind="ExternalOutput").ap()

    def din(name, shape, dt=F32):
        io[name] = nc.dram_tensor(name, shape, dt, kind="ExternalInput").ap()
        return io[name]

    def dint(name, shape, dt=BF16):
        io[name] = nc.dram_tensor(name, shape, dt, kind="Internal").ap()
        return io[name]

    din("x_tok", [NT, D]); din("vecsP", [128, 8])
    if stop != "AG":
        din("sel", [128, 8]); din("mem", [256, D])
    for l in range(2 if stop != "AG" else 0):
        din(f"w_ml{l}", [D, 258]); din(f"w_fx{l}", [D, 386]); din(f"vecsM{l}", [128, 16]); din(f"vecs{l}", [128, NV_T])
        din(f"w_c{l}", [D, 512]); din(f"w_out{l}", [D, D]); din(f"w_q{l}", [D, 512]); din(f"w_kv{l}", [D, D]); din(f"w_o{l}", [512, D])
    if stop != "AG":
        din("w_gate0", [1, D, DFF]); din("w_up0", [1, D, DFF]); din("w_down0", [1, DFF, D])
        din("w_gate1", [8, D, DFF]); din("w_up1", [8, D, DFF]); din("w_down1", [8, DFF, D]); din("router_w", [D, 8])
    io["out"] = nc.dram_tensor("out", [NT, D], F32, kind="ExternalOutput").ap()
    for l in range(2):
        io[f"h_own{l}"] = [dint(f"h_own{l}_{a}", [256, NT]) for a in range(4)]
        io[f"hT_all{l}"] = [dint(f"hT_all{l}_{a}", [4 * 256, NT]) for a in range(4)]
        dint(f"tail{l}", [D, 32]); dint(f"tails{l}", [4 * D, 32])
        dint(f"catm{l}", [64, SEQ]); dint(f"catm_all{l}", [256, SEQ])
        io[f"catf{l}"] = [dint(f"catf{l}_{h}", [64, SEQ]) for h in range(2)]
        io[f"catf_all{l}"] = [dint(f"catf_all{l}_{h}", [256, SEQ]) for h in range(2)]
    with _ES() as st:
        c = Ctx(nc, st)
        c.setup()
        kb = c.kb
        xT = c.sb("xT", [128, 8, NT], F32)
        phase_P(c, {"x_tok": io["x_tok"], "vecsP": io["vecsP"], "h_next": io["h_own0"], "tail_next": io["tail0"]}, xT)
        for l in range(2):
            last = (l == 1)
            for a in range(4):
                kb.collective("AllGather", RG, io[f"h_own{l}"][a], io[f"hT_all{l}"][a], reads=["h_next_d"], writes=["hT_all_d"])
            kb.collective("AllGather", RG, io[f"tail{l}"], io[f"tails{l}"], reads=["tail_next_d"], writes=["tails_d"])
            c.barrier()
            if stop == "AG":
                for a in range(4):
                    for jj in range(4):
                        kb.dma("sp", io["dbg1"][jj * D + a * 256:jj * D + (a + 1) * 256, :], io[f"hT_all{l}"][a][jj * 256:(jj + 1) * 256, :], reads=["hT_all_d"])
                break
            ioM = {"hT_all": io[f"hT_all{l}"], "w_ml": io[f"w_ml{l}"], "w_fx": io[f"w_fx{l}"],
                   "vecsM": io[f"vecsM{l}"], "catm_out": io[f"catm{l}"], "catf_out": io[f"catf{l}"]}
            c.sfx = f"_{l}"
            phase_M_mlstm(c, ioM)
            phase_M_fox(c, ioM)
            kb.collective("AllGather", RG, io[f"catm{l}"], io[f"catm_all{l}"], writes=["catm_all_d"])
            for h in range(2):
                kb.collective("AllGather", RG, io[f"catf{l}"][h], io[f"catf_all{l}"][h], writes=["catf_all_d"])
            c.barrier()
            if stop == "M":
                for h in range(2):
                    for g in range(4):
                        kb.dma("sp", io["dbg2"][g * 128 + h * 64:g * 128 + (h + 1) * 64, :], io[f"catf_all{l}"][h][g * 64:(g + 1) * 64, :], reads=["catf_all_d"])
                break
            ioT = {"h_own": io[f"h_own{l}"], "tails": io[f"tails{l}"], "sel": io["sel"], "catm_all": io[f"catm_all{l}"], "catf_all": io[f"catf_all{l}"],
                   "mem": io["mem"], "vecs": io[f"vecs{l}"], "w_c": io[f"w_c{l}"], "w_out": io[f"w_out{l}"], "w_q": io[f"w_q{l}"],
                   "w_kv": io[f"w_kv{l}"], "w_o": io[f"w_o{l}"], "w_gate": io[f"w_gate{l}"], "w_up": io[f"w_up{l}"], "w_down": io[f"w_down{l}"]}
            if last:
                ioT["router_w"] = io["router_w"]; ioT["out"] = io["out"]
            else:
                ioT["h_next"] = io["h_own1"]; ioT["tail_next"] = io["tail1"]
            phase_T(c, ioT, 8 if last else 1, last, xT)
            if stop == "T":
                kb.dma("sp", io["dbg3"].rearrange("(k p) n -> p k n", p=128), xT[:], reads=[("xT", k) for k in range(8)])
                break
        c.barrier()
        kb.flush()
    return nc


def kernel_unfused(**inp):
    return _kernel_unfused(**inp)


_kernel_unfused = kernel


def kernel(**inp):
    inp = {k: np.asarray(v) for k, v in inp.items()}
    cores = list(range(8))
    x = inp["x"].astype(np.float32, copy=False)
    fm = lambda w: np.ascontiguousarray(np.asarray(w, np.float32).reshape(-1, 128).T)
    nc = _get("fused", lambda: build_fused(_STOP))
    shared = {"vecsP": fm(inp["norm_mix_w"][0]),
              "w_gate0": inp["ffn_w_gate"], "w_up0": inp["ffn_w_up"], "w_down0": inp["ffn_w_down"],
              "w_gate1": inp["moe_w_gate"][0], "w_up1": inp["moe_w_up"][0], "w_down1": inp["moe_w_down"][0],
              "router_w": inp["router_w"][0]}
    for l in range(2):
        shared[f"vecs{l}"] = vecs_T(inp, l, l == 1)
        shared[f"w_c{l}"] = np.ascontiguousarray(inp["w_in"][l][:, 1032:1544])
        shared[f"w_out{l}"] = inp["w_out"][l]; shared[f"w_q{l}"] = inp["xattn_w_q"][l]
        shared[f"w_kv{l}"] = inp["xattn_w_kv"][l]; shared[f"w_o{l}"] = inp["xattn_w_o"][l]
    perg = []
    for g in range(4):
        d = {}
        for l in range(2):
            m = inputs_M(inp, l, g)
            d[f"w_ml{l}"] = m["w_ml"]; d[f"w_fx{l}"] = m["w_fx"]; d[f"vecsM{l}"] = m["vecsM"]
        perg.append(d)
    maps = []
    for cid in cores:
        b, j = cid // 4, cid % 4
        m = dict(shared)
        m.update(perg[j])
        m["x_tok"] = np.ascontiguousarray(x[b, j * NT:(j + 1) * NT])
        m["mem"] = np.ascontiguousarray(inp["mem"][b], dtype=np.float32)
        sel = np.zeros((128, 8), np.float32)
        sel[:, j] = 1.0
        if j > 0:
            sel[:, 4 + j - 1] = 1.0
        m["sel"] = sel
        maps.append(m)
    if _STOP == "AG":
        maps = [{k: m[k] for k in ("x_tok", "vecsP")} for m in maps]
    res = run_bass_kernel_spmd(nc, maps, core_ids=cores).results
    if _STOP:
        return res
    out = np.zeros((2, SEQ, D), np.float32)
    for cid in cores:
        b, j = cid // 4, cid % 4
        out[b, j * NT:(j + 1) * NT] = res[cid]["out"]
    return out
```

```python
import numpy as np
import concourse.bass as bass
import concourse.mybir as mybir
from concourse.bass_utils import run_bass_kernel_spmd

F32 = mybir.dt.float32
BF16 = mybir.dt.bfloat16
AF = mybir.ActivationFunctionType
ALU = mybir.AluOpType
AX = mybir.AxisListType

ENGS = ("pe", "act", "dve", "pool", "sp")


class KB:
    SEM_ROLL = 2000

    def __init__(self, nc, n_dma_sems=32):
        self.nc = nc
        self.q = {e: [] for e in ENGS}
        self.cnt = {e: 0 for e in ENGS}
        self.cur_sem = {}
        self.sem_pool = []
        self.waited = {e: {} for e in ENGS}
        self.last_w = {}
        self.reads = {}
        self.n_dma_sems = n_dma_sems
        self.dma_sems = []
        self.dma_cnt = []
        self.dma_rr = 0
        self.dma_rr_sw = 0
        self._stack = None
        self.n_inst = 0

    def _new_sem(self, name):
        s = self._stack.enter_context(self.nc.semaphore(name))
        return s

    def start(self, stack):
        self._stack = stack
        for e in ENGS:
            self.cur_sem[e] = self._new_sem(f"p_{e}_0")
        for i in range(self.n_dma_sems):
            self.dma_sems.append(self._new_sem(f"dma{i}"))
            self.dma_cnt.append(0)

    def _wait(self, eng, ev):
        if ev is None:
            return
        if len(ev) == 3 and ev[2] == "pe" and eng == "pe":
            return
        sem, val = ev[0], ev[1]
        w = self.waited[eng]
        if w.get(id(sem), (None, 0))[1] >= val:
            return
        w[id(sem)] = (sem, val)
        self.q[eng].append(lambda e, sem=sem, val=val: e.wait_ge(sem, val))

    def _wait_w(self, eng, k):
        lw = self.last_w.get(k)
        if isinstance(lw, list):
            for ev in lw:
                self._wait(eng, ev)
        else:
            self._wait(eng, lw)

    def _deps(self, eng, reads, writes):
        for k in reads:
            self._wait_w(eng, k)
        for k in writes:
            self._wait_w(eng, k)
            for ev in self.reads.get(k, ()):
                self._wait(eng, ev)

    def _commit(self, ev, reads, writes, is_dma=False):
        for k in writes:
            lw = self.last_w.get(k)
            if is_dma and isinstance(lw, list) and not self.reads.get(k):
                lw.append(ev)
            else:
                self.last_w[k] = [ev] if is_dma else ev
            self.reads[k] = []
        for k in reads:
            self.reads.setdefault(k, []).append(ev)

    def op(self, eng, fn, reads=(), writes=()):
        self._deps(eng, reads, writes)
        if self.cnt[eng] >= self.SEM_ROLL:
            self.cur_sem[eng] = self._new_sem(f"p_{eng}_{self.n_inst}")
            self.cnt[eng] = 0
        self.cnt[eng] += 1
        sem = self.cur_sem[eng]
        ev = (sem, self.cnt[eng], eng)
        self.q[eng].append(lambda e, sem=sem: fn(e).then_inc(sem, 1))
        self._commit(ev, reads, writes)
        self.n_inst += 1
        return ev

    def dma(self, eng, out, in_, reads=(), writes=(), **kw):
        self._deps(eng, reads, writes)
        half = self.n_dma_sems // 2
        if eng == "pool":
            i = half + self.dma_rr_sw
            self.dma_rr_sw = (self.dma_rr_sw + 1) % (self.n_dma_sems - half)
        else:
            i = self.dma_rr
            self.dma_rr = (self.dma_rr + 1) % half
        sem = self.dma_sems[i]
        if self.dma_cnt[i] >= 2048:
            self.dma_sems[i] = self._new_sem(f"dma{i}_{self.n_inst}")
            self.dma_cnt[i] = 0
            sem = self.dma_sems[i]
        if self.dma_cnt[i] > 0:
            self._wait(eng, (sem, self.dma_cnt[i]))
        self.dma_cnt[i] += 16
        ev = (sem, self.dma_cnt[i])
        self.q[eng].append(lambda e, sem=sem: e.dma_start(out=out, in_=in_, **kw).then_inc(sem, 16))
        self._commit(ev, reads, writes, is_dma=True)
        self.n_inst += 1
        return ev

    def collective(self, kind, rg, in_ap, out_ap, reads=(), writes=()):
        eng = "pool"
        self._deps(eng, reads, writes)
        sem = self._new_sem(f"cc_{self.n_inst}")
        ev = (sem, 1)
        self.q[eng].append(lambda e: e.collective_compute(kind, ALU.bypass, replica_groups=rg, ins=[in_ap.opt()],
                                                          outs=[out_ap.opt()]).then_inc(sem, 1))
        self._commit(ev, reads, writes)
        self.n_inst += 1
        self.cc_events = getattr(self, "cc_events", []) + [ev]
        return ev

    def wait_all(self, eng, evs):
        for ev in evs:
            self._wait(eng, ev)

    def flush(self):
        nc = self.nc
        q = self.q
        with nc.Block() as block:
            @block.tensor
            def _(e):
                for f in q["pe"]:
                    f(e)

            @block.scalar
            def _(e):
                for f in q["act"]:
                    f(e)

            @block.vector
            def _(e):
                for f in q["dve"]:
                    f(e)

            @block.gpsimd
            def _(e):
                for f in q["pool"]:
                    f(e)

            @block.sync
            def _(e):
                for f in q["sp"]:
                    f(e)
        self.q = {e: [] for e in ENGS}


D = 1024
NT = 2048
TT = 512
NTT = NT // TT
DFF = 2816
NF = DFF // 128
SEQ = 8192
EPS = 1e-6
NV_T = 100


class Ctx:
    def __init__(self, nc, st):
        self.nc = nc
        self.st = st
        self.kb = KB(nc)
        self.kb.start(st)
        self.ps_rr = 0
        self.uid = 0

    def sb(self, name, shape, dt, st=None):
        self.uid += 1
        return (st or self.st).enter_context(self.nc.sbuf_tensor(f"{name}_u{self.uid}", shape, dt))

    def barrier(self):
        kb = self.kb
        evs = []
        for e in ENGS:
            if kb.cnt[e] > 0:
                evs.append((kb.cur_sem[e], kb.cnt[e]))
        for i, s in enumerate(kb.dma_sems):
            if kb.dma_cnt[i] > 0:
                evs.append((s, kb.dma_cnt[i]))
        evs += getattr(kb, "cc_events", [])
        kb.cc_events = []
        for e in ENGS:
            for ev in evs:
                kb._wait(e, ev)
        kb.last_w = {}
        kb.reads = {}

    def setup(self):
        nc, kb = self.nc, self.kb
        self.ident_f = self.sb("ident_f", [128, 128], F32)
        self.ident_b = self.sb("ident_b", [128, 128], BF16)
        self.ones_b = self.sb("ones_b", [128, 128], BF16)
        self.ones_f = self.sb("ones_f", [128, 128], F32)
        self.psb = [self.st.enter_context(nc.psum_tensor(f"psb{i}", [128, 512], F32)) for i in range(8)]
        idf, idb, ob, of = self.ident_f, self.ident_b, self.ones_b, self.ones_f
        kb.op("pool", lambda e: e.memset(idf[:], 0.0), writes=["ident_f"])
        kb.op("pool", lambda e: e.affine_select(out=idf[:], in_=idf[:], pattern=[[-1, 128]],
                                                compare_op=ALU.not_equal, fill=1.0, base=0,
                                                channel_multiplier=1),
              reads=["ident_f"], writes=["ident_f"])
        kb.op("pool", lambda e: e.tensor_copy(out=idb[:], in_=idf[:]), reads=["ident_f"], writes=["ident_b"])
        kb.op("pool", lambda e: e.memset(ob[:], 1.0), writes=["ones_b"])
        kb.op("pool", lambda e: e.memset(of[:], 1.0), writes=["ones_f"])

    def ps(self):
        rot = getattr(self, "rot", None) or list(range(8))
        i = rot[self.ps_rr % len(rot)]
        self.ps_rr += 1
        return self.psb[i], f"psb{i}"


def h_store(c, dst, hT, c0, n, reads, writes=()):
    if isinstance(dst, list):
        for a, d in enumerate(dst):
            c.kb.dma("sp", d[:, c0:c0 + n].rearrange("(k p) n -> p k n", p=128), hT[:, 2 * a:2 * a + 2, 0:n], reads=reads, writes=writes)
    else:
        c.kb.dma("sp", dst[:, c0:c0 + n].rearrange("(k p) n -> p k n", p=128), hT[:, :, 0:n], reads=reads, writes=writes)


def h_load(c, src, hT, c0, n, writes, j=None):
    if isinstance(src, list):
        for a, d in enumerate(src):
            v = d if j is None else d.rearrange("(j r) n -> j r n", j=4)[j]
            c.kb.dma("sp", hT[:, 2 * a:2 * a + 2, 0:n], v[:, c0:c0 + n].rearrange("(k p) n -> p k n", p=128), writes=writes)
    else:
        v = src if j is None else src[j]
        c.kb.dma("sp", hT[:, :, 0:n], v[:, c0:c0 + n].rearrange("(k p) n -> p k n", p=128), writes=writes)


def mm(c, out, lhsT, rhs, start, stop, reads, writes):
    return c.kb.op("pe", lambda e: e.matmul(out, lhsT=lhsT, rhs=rhs, start=start, stop=stop),
                   reads=reads, writes=writes)


def rmsnorm_tile(c, xT, xkey, t0, n, wv, tmp, out_bf, okey, out_f=None):
    kb = c.kb
    sq, rstd = tmp["sq"], tmp["rstd"]
    for k in range(8):
        kb.op("act", lambda e, k=k: e.activation(out=sq[:, k, 0:n], in_=xT[:, k, t0:t0 + n], func=AF.Square),
              reads=[(xkey, k)], writes=[("sq", k)])
    p, pk = c.ps()
    for k in range(8):
        mm(c, p[:, 0:n], c.ones_b[:], sq[:, k, 0:n], k == 0, k == 7, ["ones_b", ("sq", k)], [pk])
    kb.op("act", lambda e: e.activation(out=rstd[:, 0:n], in_=p[:, 0:n], func=AF.Sqrt, scale=1.0 / D, bias=tmp["eps"][:, 0:1]),
          reads=[pk, "eps"], writes=["rstd"])
    kb.op("dve", lambda e: e.reciprocal(out=rstd[:, 0:n], in_=rstd[:, 0:n]), reads=["rstd"], writes=["rstd"])
    for k in range(8):
        kb.op("dve", lambda e, k=k: e.scalar_tensor_tensor(out=out_bf[:, k, 0:n], in0=xT[:, k, t0:t0 + n],
                                                           scalar=wv[:, k:k + 1], in1=rstd[:, 0:n],
                                                           op0=ALU.mult, op1=ALU.mult),
              reads=[(xkey, k), "rstd", "vecs"], writes=[(okey, k)])
        if out_f is not None:
            kb.op("dve", lambda e, k=k: e.scalar_tensor_tensor(out=out_f[:, k, 0:n], in0=xT[:, k, t0:t0 + n],
                                                                scalar=wv[:, k:k + 1], in1=rstd[:, 0:n],
                                                                op0=ALU.mult, op1=ALU.mult),
                  reads=[(xkey, k), "rstd", "vecs"], writes=[(okey + "_f", k)])


def phase_T(c, io, E, last, xT):
    nc, kb = c.nc, c.kb
    from contextlib import ExitStack
    vec_st = ExitStack()
    vecs = c.sb("vecsT", [128, NV_T], F32, vec_st)
    eps_t = c.sb("eps_t", [128, 1], F32, vec_st)
    sq = c.sb("sq", [128, 8, TT], BF16, vec_st)
    rstd = c.sb("rstd", [128, TT], F32, vec_st)
    tmp = {"sq": sq, "rstd": rstd, "eps": eps_t}
    kb.dma("sp", vecs[:], io["vecs"], writes=["vecs"])
    kb.op("pool", lambda e: e.memset(eps_t[:], EPS), writes=["eps"])
    V_XA, V_MEM, V_FFN, V_NEXT, V_CB, V_LNW, V_LNB, V_CW = 0, 8, 16, 24, 32, 34, 36, 38

    with ExitStack() as s1:
        hT = c.sb("hT", [128, 8, TT], BF16, s1)
        gluT = c.sb("gluT", [128, 2, 32 + NT], BF16, s1)
        hcT = c.sb("hcT", [128, 2, NT], BF16, s1)
        wc = c.sb("wc", [128, 8, 512], BF16, s1)
        dg = c.sb("dg", [128, 62, 128], BF16, s1)
        sig = c.sb("sig", [128, 2, TT], F32, s1)
        hcv = c.sb("hcv", [128, 2, TT], F32, s1)
        hsq = c.sb("hsq", [128, 2, TT], F32, s1)
        mean = c.sb("mean", [128, TT], F32, s1)
        var = c.sb("var", [128, TT], F32, s1)
        wo_m = c.sb("wo_m", [64, 4, D], BF16, s1)
        wo_c = c.sb("wo_c", [128, 2, D], BF16, s1)
        wo_f = c.sb("wo_f", [128, 4, D], BF16, s1)
        mT = c.sb("mT", [64, 4, TT], BF16, s1)
        fT = c.sb("fT", [128, 4, TT], BF16, s1)
        if "sel" in io:
            halo4 = c.sb("halo4", [128, 4, 8, 32], BF16, s1)
            selt = c.sb("selt", [128, 8], F32, s1)
            m4 = [c.sb(f"m4_{i}", [64, 4, TT], BF16, s1) for i in range(2)]
            f4 = [c.sb(f"f4_{i}", [128, 4, TT], BF16, s1) for i in range(2)]
            kb.dma("sp", selt[:], io["sel"], writes=["selt"])
        kb.dma("pool", wc[:], io["w_c"].rearrange("(k p) n -> p k n", p=128), writes=["wc"])
        kb.dma("pool", wo_m[:], io["w_out"][0:256, :].rearrange("(g p) n -> p g n", p=64), writes=["wo_m"])
        kb.dma("pool", wo_c[:], io["w_out"][256:512, :].rearrange("(g p) n -> p g n", p=128), writes=["wo_c"])
        kb.dma("pool", wo_f[:], io["w_out"][512:1024, :].rearrange("(g p) n -> p g n", p=128), writes=["wo_f"])
        for j in range(31):
            for ch in range(2):
                kb.op("dve", lambda e, j=j, ch=ch: e.tensor_scalar(
                    out=dg[:, j * 2 + ch, :], in0=c.ident_b[:], scalar1=vecs[:, V_CW + j * 2 + ch:V_CW + j * 2 + ch + 1],
                    scalar2=None, op0=ALU.mult), reads=["ident_b", "vecs"], writes=[("dg", j, ch)])
        tiles = [("halo", 0, 32)] + [("own", t * TT, TT) for t in range(NTT)]
        for kind, t0, n in tiles:
            if kind == "halo" and "sel" in io:
                tl = io["tails"].rearrange("(j k p) n -> j p k n", j=4, p=128)
                for jj in range(4):
                    kb.dma("sp", halo4[:, jj, :, :], tl[jj], writes=[("halo4", jj)])
                kb.op("dve", lambda e: e.tensor_scalar(out=hT[:, :, 0:32], in0=halo4[:, 0, :, :], scalar1=selt[:, 4:5], scalar2=None, op0=ALU.mult),
                      reads=[("halo4", 0), "selt"], writes=[("hT", k) for k in range(8)])
                for jj in range(1, 4):
                    kb.op("dve", lambda e, jj=jj: e.scalar_tensor_tensor(out=hT[:, :, 0:32], in0=halo4[:, jj, :, :], scalar=selt[:, 4 + jj:5 + jj], in1=hT[:, :, 0:32],
                                                                         op0=ALU.mult, op1=ALU.add),
                          reads=[("halo4", jj), "selt"] + [("hT", k) for k in range(8)], writes=[("hT", k) for k in range(8)])
                g0 = 0
            elif kind == "halo":
                kb.dma("sp", hT[:, :, 0:n], io["h_halo"].rearrange("(k p) n -> p k n", p=128),
                       writes=[("hT", k) for k in range(8)])
                g0 = 0
            else:
                h_load(c, io["h_own"], hT, t0, n, [("hT", k) for k in range(8)])
                g0 = 32 + t0
            for ch in range(2):
                pa, pak = c.ps()
                pg, pgk = c.ps()
                for k in range(8):
                    mm(c, pa[:, 0:n], wc[:, k, ch * 128:(ch + 1) * 128], hT[:, k, 0:n], k == 0, k == 7,
                       ["wc", ("hT", k)], [pak])
                for k in range(8):
                    mm(c, pg[:, 0:n], wc[:, k, 256 + ch * 128:256 + (ch + 1) * 128], hT[:, k, 0:n], k == 0, k == 7,
                       ["wc", ("hT", k)], [pgk])
                kb.op("act", lambda e, ch=ch, pg=pg, n=n: e.activation(out=sig[:, ch, 0:n], in_=pg[:, 0:n], func=AF.Sigmoid),
                      reads=[pgk], writes=[("sig", ch)])
                kb.op("dve", lambda e, ch=ch, pa=pa, n=n, g0=g0: e.tensor_tensor(
                    out=gluT[:, ch, g0:g0 + n], in0=pa[:, 0:n], in1=sig[:, ch, 0:n], op=ALU.mult),
                    reads=[pak, ("sig", ch)], writes=[("glu", ch, g0 // TT), ("glu", ch, (g0 + n - 1) // TT)])
        for t in range(NTT):
            t0 = t * TT
            gk = lambda ch: [("glu", ch, (32 + t0 - 30) // TT), ("glu", ch, (32 + t0 + TT - 1) // TT)]
            for ch in range(2):
                p, pk = c.ps()
                for j in range(31):
                    o = 32 + t0 - 30 + j
                    mm(c, p[:, :], dg[:, j * 2 + ch, :], gluT[:, ch, o:o + TT], j == 0, j == 30,
                       [("dg", j, ch)] + gk(ch), [pk])
                kb.op("act", lambda e, ch=ch, p=p: e.activation(out=hcv[:, ch, :], in_=p[:, :], func=AF.Identity,
                                                                bias=vecs[:, V_CB + ch:V_CB + ch + 1]),
                      reads=[pk, "vecs"], writes=[("hcv", ch)])
                kb.op("act", lambda e, ch=ch: e.activation(out=hsq[:, ch, :], in_=hcv[:, ch, :], func=AF.Square),
                      reads=[("hcv", ch)], writes=[("hsq", ch)])
            p1, p1k = c.ps()
            p2, p2k = c.ps()
            for ch in range(2):
                mm(c, p1[:, :], c.ones_f[:], hcv[:, ch, :], ch == 0, ch == 1, ["ones_f", ("hcv", ch)], [p1k])
            for ch in range(2):
                mm(c, p2[:, :], c.ones_f[:], hsq[:, ch, :], ch == 0, ch == 1, ["ones_f", ("hsq", ch)], [p2k])
            kb.op("dve", lambda e, p1=p1: e.tensor_scalar(out=mean[:], in0=p1[:, :], scalar1=1.0 / 256, scalar2=None, op0=ALU.mult),
                  reads=[p1k], writes=["mean"])
            kb.op("dve", lambda e: e.tensor_tensor(out=var[:], in0=mean[:], in1=mean[:], op=ALU.mult),
                  reads=["mean"], writes=["var"])
            kb.op("dve", lambda e, p2=p2: e.scalar_tensor_tensor(out=var[:], in0=p2[:, :], scalar=1.0 / 256, in1=var[:],
                                                                 op0=ALU.mult, op1=ALU.subtract),
                  reads=[p2k, "var"], writes=["var"])
            kb.op("act", lambda e: e.activation(out=var[:], in_=var[:], func=AF.Sqrt, bias=eps_t[:, 0:1]),
                  reads=["var", "eps"], writes=["var"])
            kb.op("dve", lambda e: e.reciprocal(out=var[:], in_=var[:]), reads=["var"], writes=["var"])
            for ch in range(2):
                kb.op("dve", lambda e, ch=ch: e.tensor_tensor(out=hcv[:, ch, :], in0=hcv[:, ch, :], in1=mean[:], op=ALU.subtract),
                      reads=[("hcv", ch), "mean"], writes=[("hcv", ch)])
                kb.op("dve", lambda e, ch=ch: e.tensor_tensor(out=hcv[:, ch, :], in0=hcv[:, ch, :], in1=var[:], op=ALU.mult),
                      reads=[("hcv", ch), "var"], writes=[("hcv", ch)])
                kb.op("dve", lambda e, ch=ch: e.tensor_scalar(out=hcv[:, ch, :], in0=hcv[:, ch, :],
                                                              scalar1=vecs[:, V_LNW + ch:V_LNW + ch + 1],
                                                              scalar2=vecs[:, V_LNB + ch:V_LNB + ch + 1],
                                                              op0=ALU.mult, op1=ALU.add),
                      reads=[("hcv", ch), "vecs"], writes=[("hcv", ch)])
                kb.op("act", lambda e, ch=ch, t0=t0: e.activation(out=hcT[:, ch, t0:t0 + TT], in_=hcv[:, ch, :], func=AF.Silu),
                      reads=[("hcv", ch)], writes=[("hcT", ch, t)])
            if "sel" in io:
                cm = io["catm_all"].rearrange("(g p) n -> p g n", p=64)
                cf = [a.rearrange("(g p) n -> p g n", p=64) for a in io["catf_all"]]
                for jj in range(4):
                    for dst, stg, src, nm, npart in ((mT, m4, cm, "m4", 64), (fT, f4, cf, "f4", 128)):
                        dk = "mT" if nm == "m4" else "fT"
                        sg = stg[jj % 2]
                        sk = f"{nm}_{jj % 2}"
                        if nm == "m4":
                            kb.dma("sp", sg[:], src[:, :, jj * NT + t0:jj * NT + t0 + TT], writes=[sk])
                        else:
                            for hh in range(2):
                                kb.dma("sp", sg[hh * 64:(hh + 1) * 64, :, :], src[hh][:, :, jj * NT + t0:jj * NT + t0 + TT], writes=[sk])
                        if jj == 0:
                            kb.op("dve", lambda e, dst=dst, sg=sg, npart=npart: e.tensor_scalar(out=dst[:], in0=sg[:], scalar1=selt[0:npart, 0:1], scalar2=None, op0=ALU.mult),
                                  reads=[sk, "selt"], writes=[dk])
                        else:
                            kb.op("dve", lambda e, dst=dst, sg=sg, jj=jj, npart=npart: e.scalar_tensor_tensor(out=dst[:], in0=sg[:], scalar=selt[0:npart, jj:jj + 1], in1=dst[:],
                                                                                                          op0=ALU.mult, op1=ALU.add),
                                  reads=[sk, "selt", dk], writes=[dk])
            else:
                kb.dma("sp", mT[:], io["catm"][:, :, t0:t0 + TT].rearrange("g p n -> p g n"), writes=["mT"])
                kb.dma("sp", fT[:], io["catf"][:, :, t0:t0 + TT].rearrange("g p n -> p g n"), writes=["fT"])
            for d in range(8):
                p, pk = c.ps()
                ds = slice(d * 128, (d + 1) * 128)
                for g in range(4):
                    mm(c, p[:, :], wo_m[:, g, ds], mT[:, g, :], g == 0, False, ["wo_m", "mT"], [pk])
                for ch in range(2):
                    mm(c, p[:, :], wo_c[:, ch, ds], hcT[:, ch, t0:t0 + TT], False, False, ["wo_c", ("hcT", ch, t)], [pk])
                for g in range(4):
                    mm(c, p[:, :], wo_f[:, g, ds], fT[:, g, :], False, g == 3, ["wo_f", "fT"], [pk])
                kb.op("dve", lambda e, d=d, p=p, t0=t0: e.tensor_tensor(out=xT[:, d, t0:t0 + TT], in0=xT[:, d, t0:t0 + TT],
                                                                        in1=p[:, :], op=ALU.add),
                      reads=[pk, ("xT", d)], writes=[("xT", d)])
    c.barrier()
    if io.get("dbg_stage") == 1:
        vec_st.close()
        return

    with ExitStack() as s2:
        hT = c.sb("hT", [128, 8, TT], BF16, s2)
        memt = c.sb("memt", [128, 2, D], F32, s2)
        mss = c.sb("mss", [128, 2], F32, s2)
        junk = c.sb("junk", [128, D], F32, s2)
        memnT = c.sb("memnT", [128, 8, 256], BF16, s2)
        wkv = c.sb("wkv", [128, 8, D], BF16, s2)
        wq = c.sb("wq", [128, 8, 512], BF16, s2)
        wo = c.sb("wo", [128, 4, D], BF16, s2)
        kT = c.sb("kT", [128, 4, 256], BF16, s2)
        Vt = c.sb("Vt", [128, 2, 512], BF16, s2)
        qT = c.sb("qT", [128, 4, TT], BF16, s2)
        pT = c.sb("pT", [128, 8, TT], BF16, s2)
        rden = c.sb("rden", [128, TT], F32, s2)
        oT = c.sb("oT", [128, 4, TT], BF16, s2)
        kb.dma("sp", memt[:], io["mem"].rearrange("(t p) d -> p t d", p=128), writes=["memt"])
        kb.dma("pool", wkv[:], io["w_kv"].rearrange("(k p) n -> p k n", p=128), writes=["wkv"])
        kb.dma("pool", wq[:], io["w_q"].rearrange("(k p) n -> p k n", p=128), writes=["wq"])
        kb.dma("pool", wo[:], io["w_o"].rearrange("(k p) n -> p k n", p=128), writes=["wo"])
        for mt in range(2):
            kb.op("act", lambda e, mt=mt: e.activation(out=junk[:], in_=memt[:, mt, :], func=AF.Square,
                                                       accum_out=mss[:, mt:mt + 1]),
                  reads=["memt"], writes=["junk", ("mss", mt)])
            kb.op("act", lambda e, mt=mt: e.activation(out=mss[:, mt:mt + 1], in_=mss[:, mt:mt + 1], func=AF.Sqrt,
                                                       scale=1.0 / D, bias=eps_t[:, 0:1]),
                  reads=[("mss", mt), "eps"], writes=[("mss", mt)])
            kb.op("dve", lambda e, mt=mt: e.reciprocal(out=mss[:, mt:mt + 1], in_=mss[:, mt:mt + 1]),
                  reads=[("mss", mt)], writes=[("mss", mt)])
            kb.op("dve", lambda e, mt=mt: e.tensor_scalar(out=memt[:, mt, :], in0=memt[:, mt, :], scalar1=mss[:, mt:mt + 1],
                                                          scalar2=None, op0=ALU.mult),
                  reads=["memt", ("mss", mt)], writes=["memt"])
        for k in range(8):
            p, pk = c.ps()
            for mt in range(2):
                kb.op("pe", lambda e, k=k, mt=mt, p=p: e.transpose(out=p[:, mt * 128:(mt + 1) * 128],
                                                                   in_=memt[:, mt, k * 128:(k + 1) * 128], identity=c.ident_f[:]),
                      reads=["memt", "ident_f"], writes=[pk])
            kb.op("dve", lambda e, k=k, p=p: e.tensor_scalar(out=memnT[:, k, :], in0=p[:, 0:256],
                                                             scalar1=vecs[:, V_MEM + k:V_MEM + k + 1], scalar2=None, op0=ALU.mult),
                  reads=[pk, "vecs"], writes=[("memnT", k)])
        for h in range(4):
            p, pk = c.ps()
            for k in range(8):
                mm(c, p[:, 0:256], wkv[:, k, h * 128:(h + 1) * 128], memnT[:, k, :], k == 0, k == 7, ["wkv", ("memnT", k)], [pk])
            kb.op("act", lambda e, h=h, p=p: e.activation(out=kT[:, h, :], in_=p[:, 0:256], func=AF.Copy),
                  reads=[pk], writes=[("kT", h)])
        for mt in range(2):
            p, pk = c.ps()
            for k in range(8):
                mm(c, p[:, :], memnT[:, k, mt * 128:(mt + 1) * 128], wkv[:, k, 512:1024], k == 0, k == 7, ["wkv", ("memnT", k)], [pk])
            kb.op("act", lambda e, mt=mt, p=p: e.activation(out=Vt[:, mt, :], in_=p[:, :], func=AF.Copy),
                  reads=[pk], writes=[("Vt", mt)])
        sc = 128 ** -0.5
        for t in range(NTT):
            t0 = t * TT
            rmsnorm_tile(c, xT, "xT", t0, TT, vecs[:, V_XA:V_XA + 8], tmp, hT, "hT")
            for h in range(4):
                p, pk = c.ps()
                for k in range(8):
                    mm(c, p[:, :], wq[:, k, h * 128:(h + 1) * 128], hT[:, k, :], k == 0, k == 7, ["wq", ("hT", k)], [pk])
                kb.op("act", lambda e, h=h, p=p: e.activation(out=qT[:, h, :], in_=p[:, :], func=AF.Copy),
                      reads=[pk], writes=[("qT", h)])
            for h in range(4):
                for mt in range(2):
                    p, pk = c.ps()
                    mm(c, p[:, :], kT[:, h, mt * 128:(mt + 1) * 128], qT[:, h, :], True, True, [("kT", h), ("qT", h)], [pk])
                    kb.op("act", lambda e, h=h, mt=mt, p=p: e.activation(out=pT[:, h * 2 + mt, :], in_=p[:, :], func=AF.Exp, scale=sc),
                          reads=[pk], writes=[("pT", h, mt)])
                pd, pdk = c.ps()
                for mt in range(2):
                    mm(c, pd[:, :], c.ones_b[:], pT[:, h * 2 + mt, :], mt == 0, mt == 1, ["ones_b", ("pT", h, mt)], [pdk])
                kb.op("dve", lambda e, pd=pd: e.reciprocal(out=rden[:], in_=pd[:, :]), reads=[pdk], writes=["rden"])
                po, pok = c.ps()
                for mt in range(2):
                    mm(c, po[:, :], Vt[:, mt, h * 128:(h + 1) * 128], pT[:, h * 2 + mt, :], mt == 0, mt == 1,
                       [("Vt", mt), ("pT", h, mt)], [pok])
                kb.op("dve", lambda e, h=h, po=po: e.tensor_tensor(out=oT[:, h, :], in0=po[:, :], in1=rden[:], op=ALU.mult),
                      reads=[pok, "rden"], writes=[("oT", h)])
            for d in range(8):
                p, pk = c.ps()
                for h in range(4):
                    mm(c, p[:, :], wo[:, h, d * 128:(d + 1) * 128], oT[:, h, :], h == 0, h == 3, ["wo", ("oT", h)], [pk])
                kb.op("dve", lambda e, d=d, p=p, t0=t0: e.tensor_tensor(out=xT[:, d, t0:t0 + TT], in0=xT[:, d, t0:t0 + TT],
                                                                        in1=p[:, :], op=ALU.add),
                      reads=[pk, ("xT", d)], writes=[("xT", d)])
    c.barrier()
    if io.get("dbg_stage") == 2:
        vec_st.close()
        return

    with ExitStack() as s3:
        hTall = c.sb("hTall", [128, 8, NT], BF16, s3)
        actT = c.sb("actT", [128, 8, NT], BF16, s3)
        wgu = [c.sb(f"wgu{i}", [128, 8, 256], BF16, s3) for i in range(3)]
        wdr = [c.sb(f"wdr{i}", [128, D], BF16, s3) for i in range(11)]
        sil = [c.sb(f"sil{i}", [128, TT], BF16, s3) for i in range(2)]
        if E > 1:
            wr = c.sb("wr", [128, 8, 8], F32, s3)
            lg = c.sb("lg", [128, 4, 8], F32, s3)
            top8 = c.sb("top8", [128, 4, 8], F32, s3)
            gts = c.sb("gts", [128, 16, 8], F32, s3)
            gsc = c.sb("gsc", [128, 4, 4], F32, s3)
            dgate = c.sb("dgate", [128, 128], F32, s3)
            gB = [c.sb(f"gB{i}", [128, NT], BF16, s3) for i in range(2)]
            ytmps = [c.sb(f"ytmp{i}", [128, TT], F32, s3) for i in range(2)]
            kb.dma("sp", wr[:], io["router_w"].rearrange("(k p) n -> p k n", p=128), writes=["wr"])
        with ExitStack() as s3a:
            hF = c.sb("hF", [128, 8, TT], F32, s3a) if E > 1 else None
            for t in range(NTT):
                t0 = t * TT
                rmsnorm_tile(c, xT, "xT", t0, TT, vecs[:, V_FFN:V_FFN + 8], tmp, hTall[:, :, t0:t0 + TT], f"hA{t}", out_f=hF)
                if E > 1:
                    for s in range(4):
                        p, pk = c.ps()
                        for k in range(8):
                            mm(c, p[:, 0:8], hF[:, k, s * 128:(s + 1) * 128], wr[:, k, :], k == 0, k == 7, [(f"hA{t}_f", k), "wr"], [pk])
                        kb.op("dve", lambda e, s=s, p=p: e.tensor_copy(out=lg[:, s, :], in_=p[:, 0:8]), reads=[pk], writes=[("lg", s)])
                        kb.op("dve", lambda e, s=s: e.max(out=top8[:, s, :], in_=lg[:, s, :]), reads=[("lg", s)], writes=[("top8", s)])
                        kb.op("dve", lambda e, s=s: e.tensor_scalar(out=gsc[:, s, 0:1], in0=top8[:, s, 0:1], scalar1=-1.0, scalar2=None, op0=ALU.mult),
                              reads=[("top8", s)], writes=[("gsc", s, 0)])
                        kb.op("act", lambda e, s=s: e.activation(out=gsc[:, s, 1:2], in_=top8[:, s, 1:2], func=AF.Exp, bias=gsc[:, s, 0:1]),
                              reads=[("top8", s), ("gsc", s, 0)], writes=[("gsc", s, 1)])
                        kb.op("dve", lambda e, s=s: e.tensor_scalar(out=gsc[:, s, 1:2], in0=gsc[:, s, 1:2], scalar1=1.0, scalar2=None, op0=ALU.add),
                              reads=[("gsc", s, 1)], writes=[("gsc", s, 1)])
                        kb.op("dve", lambda e, s=s: e.reciprocal(out=gsc[:, s, 1:2], in_=gsc[:, s, 1:2]),
                              reads=[("gsc", s, 1)], writes=[("gsc", s, 1)])
                        gi = t * 4 + s
                        kb.op("act", lambda e, s=s, gi=gi: e.activation(out=gts[:, gi, :], in_=lg[:, s, :], func=AF.Exp, bias=gsc[:, s, 0:1]),
                              reads=[("lg", s), ("gsc", s, 0)], writes=[("gts", gi)])
                        kb.op("dve", lambda e, s=s: e.tensor_scalar(out=lg[:, s, :], in0=lg[:, s, :], scalar1=top8[:, s, 1:2], scalar2=None, op0=ALU.is_ge),
                              reads=[("lg", s), ("top8", s)], writes=[("lg", s)])
                        kb.op("dve", lambda e, s=s, gi=gi: e.scalar_tensor_tensor(out=gts[:, gi, :], in0=gts[:, gi, :], scalar=gsc[:, s, 1:2], in1=lg[:, s, :],
                                                                                  op0=ALU.mult, op1=ALU.mult),
                              reads=[("gts", gi), ("gsc", s, 1), ("lg", s)], writes=[("gts", gi)])
        hkeys = lambda t: [(f"hA{t}", k) for k in range(8)]
        groups = [(0, 8), (8, 16), (16, 22)]
        wgu_i = 0
        wd_i = 0
        sil_i = 0
        for ex in range(E):
            if E > 1:
                gb = gB[ex % 2]
                gbk = f"gB{ex % 2}"
                for gi in range(16):
                    kb.op("dve", lambda e, gi=gi, ex=ex: e.tensor_scalar(out=dgate[:], in0=c.ident_f[:], scalar1=gts[:, gi, ex:ex + 1],
                                                                         scalar2=None, op0=ALU.mult),
                          reads=["ident_f", ("gts", gi)], writes=["dgate"])
                    p, pk = c.ps()
                    mm(c, p[:, 0:128], c.ones_f[:], dgate[:], True, True, ["ones_f", "dgate"], [pk])
                    kb.op("act", lambda e, gi=gi, p=p, gb=gb: e.activation(out=gb[:, gi * 128:(gi + 1) * 128], in_=p[:, 0:128], func=AF.Copy),
                          reads=[pk], writes=[(gbk, gi // 4)])
            for fa, fb in groups:
                nfg = fb - fa
                for f0 in range(fa, fb, 2):
                    nf = min(2, fb - f0)
                    sg, su = wgu[wgu_i % 3], wgu[(wgu_i + 1) % 3]
                    sgk, suk = f"wgu{wgu_i % 3}", f"wgu{(wgu_i + 1) % 3}"
                    wgu_i += 2
                    kb.dma("pool", sg[:, :, 0:nf * 128], io["w_gate"][ex][:, f0 * 128:(f0 + nf) * 128].rearrange("(k p) n -> p k n", p=128), writes=[sgk])
                    kb.dma("pool", su[:, :, 0:nf * 128], io["w_up"][ex][:, f0 * 128:(f0 + nf) * 128].rearrange("(k p) n -> p k n", p=128), writes=[suk])
                    for fi in range(nf):
                        fl = f0 + fi - fa
                        for t in range(NTT):
                            ts_ = slice(t * TT, (t + 1) * TT)
                            pg, pgk = c.ps()
                            pu, puk = c.ps()
                            for k in range(8):
                                mm(c, pg[:, :], sg[:, k, fi * 128:(fi + 1) * 128], hTall[:, k, ts_], k == 0, k == 7, [sgk, (f"hA{t}", k)], [pgk])
                            for k in range(8):
                                mm(c, pu[:, :], su[:, k, fi * 128:(fi + 1) * 128], hTall[:, k, ts_], k == 0, k == 7, [suk, (f"hA{t}", k)], [puk])
                            sl = sil[sil_i % 2]
                            slk = f"sil{sil_i % 2}"
                            sil_i += 1
                            kb.op("act", lambda e, pg=pg, sl=sl: e.activation(out=sl[:], in_=pg[:, :], func=AF.Silu), reads=[pgk], writes=[slk])
                            kb.op("dve", lambda e, pu=pu, sl=sl, fl=fl, ts_=ts_: e.tensor_tensor(out=actT[:, fl, ts_], in0=pu[:, :], in1=sl[:], op=ALU.mult),
                                  reads=[puk, slk], writes=[("actT", fl, t)])
                slots = []
                for fl in range(nfg):
                    f = fa + fl
                    wd = wdr[wd_i % 11]
                    wdk = f"wdr{wd_i % 11}"
                    wd_i += 1
                    kb.dma("pool", wd[:], io["w_down"][ex][f * 128:(f + 1) * 128, :], writes=[wdk])
                    slots.append((wd, wdk))
                for t in range(NTT):
                    ts_ = slice(t * TT, (t + 1) * TT)
                    for d in range(8):
                        p, pk = c.ps()
                        for fl in range(nfg):
                            wd, wdk = slots[fl]
                            mm(c, p[:, :], wd[:, d * 128:(d + 1) * 128], actT[:, fl, ts_], fl == 0, fl == nfg - 1, [wdk, ("actT", fl, t)], [pk])
                        if E > 1:
                            ytmp = ytmps[d % 2]
                            ytk = f"ytmp{d % 2}"
                            kb.op("dve", lambda e, p=p, gb=gb, ytmp=ytmp, ts_=ts_: e.tensor_tensor(out=ytmp[:], in0=p[:, :], in1=gb[:, ts_], op=ALU.mult),
                                  reads=[pk, (gbk, t)], writes=[ytk])
                            kb.op("dve", lambda e, d=d, ts_=ts_, ytmp=ytmp: e.tensor_tensor(out=xT[:, d, ts_], in0=xT[:, d, ts_], in1=ytmp[:], op=ALU.add),
                                  reads=[ytk, ("xT", d)], writes=[("xT", d)])
                        else:
                            kb.op("dve", lambda e, d=d, p=p, ts_=ts_: e.tensor_tensor(out=xT[:, d, ts_], in0=xT[:, d, ts_], in1=p[:, :], op=ALU.add),
                                  reads=[pk, ("xT", d)], writes=[("xT", d)])
    c.barrier()

    with ExitStack() as s4:
        hT = c.sb("hT", [128, 8, TT], BF16, s4)
        if not last:
            for t in range(NTT):
                t0 = t * TT
                rmsnorm_tile(c, xT, "xT", t0, TT, vecs[:, V_NEXT:V_NEXT + 8], tmp, hT, "hT")
                h_store(c, io["h_next"], hT, t0, TT, [("hT", k) for k in range(8)], ["h_next_d"])
                if t == NTT - 1 and "tail_next" in io:
                    kb.dma("sp", io["tail_next"].rearrange("(k p) n -> p k n", p=128), hT[:, :, TT - 32:TT],
                           reads=[("hT", k) for k in range(8)], writes=["tail_next_d"])
        else:
            hF2 = c.sb("hF2", [128, 8, TT], F32, s4)
            otm = c.sb("otm", [128, 4, D], F32, s4)
            for t in range(NTT):
                t0 = t * TT
                rmsnorm_tile(c, xT, "xT", t0, TT, vecs[:, V_NEXT:V_NEXT + 8], tmp, hT, "hT", out_f=hF2)
                for s in range(4):
                    for kk in range(2):
                        p, pk = c.ps()
                        for k4 in range(4):
                            k = kk * 4 + k4
                            kb.op("pe", lambda e, k=k, k4=k4, s=s, p=p: e.transpose(out=p[:, k4 * 128:(k4 + 1) * 128],
                                                                                    in_=hF2[:, k, s * 128:(s + 1) * 128], identity=c.ident_f[:]),
                                  reads=[("hT_f", k), "ident_f"], writes=[pk])
                        kb.op("act", lambda e, s=s, kk=kk, p=p: e.activation(out=otm[:, s, kk * 512:(kk + 1) * 512], in_=p[:, :], func=AF.Copy),
                              reads=[pk], writes=[("otm", s)])
                kb.dma("sp", io["out"][t0:t0 + TT, :].rearrange("(s p) d -> p s d", p=128), otm[:],
                       reads=[("otm", s) for s in range(4)])
    c.barrier()
    vec_st.close()


from contextlib import ExitStack as _ES
import ml_dtypes as _mld

NPBF = _mld.bfloat16


def build_T(E, last, dbg_stage=0):
    nc = bass.Bass("TRN2", target_bir_lowering=False)
    io = {}

    def din(name, shape, dt=F32):
        io[name] = nc.dram_tensor(name, shape, dt, kind="ExternalInput").ap()

    def dout(name, shape, dt=F32):
        io[name] = nc.dram_tensor(name, shape, dt, kind="ExternalOutput").ap()

    din("xT_in", [D, NT]); din("h_own", [D, NT], BF16); din("h_halo", [D, 32], BF16)
    din("catm", [4, 64, NT], BF16); din("catf", [4, 128, NT], BF16); din("mem", [256, D])
    din("vecs", [128, NV_T]); din("w_c", [D, 512]); din("w_out", [D, D]); din("w_q", [D, 512])
    din("w_kv", [D, D]); din("w_o", [512, D])
    din("w_gate", [E, D, DFF]); din("w_up", [E, D, DFF]); din("w_down", [E, DFF, D])
    if E > 1:
        din("router_w", [D, 8])
    if last:
        dout("out", [NT, D])
    else:
        dout("xT_out", [D, NT]); dout("h_next", [D, NT], BF16)
    io["dbg_stage"] = dbg_stage
    with _ES() as st:
        c = Ctx(nc, st)
        c.setup()
        xT = c.sb("xT", [128, 8, NT], F32)
        c.kb.dma("sp", xT[:], io["xT_in"].rearrange("(k p) n -> p k n", p=128), writes=[("xT", k) for k in range(8)])
        phase_T(c, io, E, last and not dbg_stage, xT)
        evs = []
        if not last or dbg_stage:
            key = "xT_out" if not last else "out"
            if last:
                io["xT_dbg"] = None
            evs.append(c.kb.dma("sp", io["xT_out"].rearrange("(k p) n -> p k n", p=128), xT[:],
                                reads=[("xT", k) for k in range(8)]))
        c.barrier()
        c.kb.flush()
    return nc


def vecs_T(inp, l, last):
    v = np.zeros((128, NV_T), np.float32)
    fm = lambda w: np.asarray(w, np.float32).reshape(-1, 128).T
    v[:, 0:8] = fm(inp["norm_xattn_w"][l]); v[:, 8:16] = fm(inp["norm_mem_w"][l]); v[:, 16:24] = fm(inp["norm_ffn_w"][l])
    v[:, 24:32] = fm(inp["norm_final_w"]) if last else fm(inp["norm_mix_w"][l + 1])
    v[:, 32:34] = fm(inp["conf_conv_b"][l]); v[:, 34:36] = fm(inp["conf_ln_w"][l]); v[:, 36:38] = fm(inp["conf_ln_b"][l])
    cw = np.asarray(inp["conf_conv_w"][l], np.float32)
    for j in range(31):
        v[:, 38 + 2 * j:40 + 2 * j] = fm(cw[j])
    return v


NCH = SEQ // 64
NKT = SEQ // 128
NQT = SEQ // TT
GRP = 4


def AP3(t, off, dims):
    return bass.AP(t[:].tensor, off, [list(d) for d in dims])


def log_sigmoid_tile(c, x, out, tmp1, tmp2, bias_ap, keys):
    kb = c.kb
    kx, ko, k1, k2 = keys
    kb.op("dve", lambda e: e.tensor_scalar(out=x, in0=x, scalar1=bias_ap, scalar2=None, op0=ALU.add), reads=[kx, "vecsM"], writes=[kx])
    kb.op("dve", lambda e: e.tensor_scalar(out=tmp1, in0=x, scalar1=-1.0, scalar2=None, op0=ALU.mult), reads=[kx], writes=[k1])
    kb.op("dve", lambda e: e.tensor_tensor(out=tmp1, in0=tmp1, in1=x, op=ALU.max), reads=[kx, k1], writes=[k1])
    kb.op("act", lambda e: e.activation(out=tmp1, in_=tmp1, func=AF.Exp, scale=-1.0), reads=[k1], writes=[k1])
    kb.op("dve", lambda e: e.tensor_scalar(out=tmp1, in0=tmp1, scalar1=1.0, scalar2=None, op0=ALU.add), reads=[k1], writes=[k1])
    kb.op("act", lambda e: e.activation(out=tmp1, in_=tmp1, func=AF.Ln), reads=[k1], writes=[k1])
    kb.op("dve", lambda e: e.tensor_scalar(out=tmp2, in0=x, scalar1=0.0, scalar2=None, op0=ALU.min), reads=[kx], writes=[k2])
    kb.op("dve", lambda e: e.tensor_tensor(out=out, in0=tmp2, in1=tmp1, op=ALU.subtract), reads=[k1, k2], writes=[ko])


def phase_M_mlstm(c, io):
    nc, kb = c.nc, c.kb
    from contextlib import ExitStack
    with ExitStack() as s0:
        vm = c.sb("vecsM_sb", [128, 16], F32, s0)
        wml = c.sb("wml", [128, 8, 258], BF16, s0)
        qT = c.sb("m_qT", [64, SEQ], BF16, s0)
        kT = c.sb("m_kT", [64, SEQ], BF16, s0)
        Vaug = c.sb("m_Vaug", [64, NCH, 65], BF16, s0)
        og = c.sb("m_og", [64, SEQ], BF16, s0)
        iC = c.sb("m_iC", [128, 64], F32, s0)
        fC = c.sb("m_fC", [128, 64], F32, s0)
        eps_t = c.sb("m_eps", [128, 1], F32, s0)
        kb.dma("sp", vm[:], io["vecsM"], writes=["vecsM"])
        kb.dma("pool", wml[:], io["w_ml"].rearrange("(k p) n -> p k n", p=128), writes=["wml"])
        kb.op("pool", lambda e: e.memset(Vaug[:, :, 64:65], 1.0), writes=["Vaug1"])
        kb.op("pool", lambda e: e.memset(eps_t[:], EPS), writes=["m_eps"])
        with ExitStack() as s1:
            hTb = [c.sb(f"m_hT{i}", [128, 8, TT], BF16, s1) for i in range(2)]
            zq = c.sb("m_zq", [64, TT + 3], F32, s1)
            zk = c.sb("m_zk", [64, TT + 3], F32, s1)
            cacc = [c.sb(f"m_cacc{i}", [64, TT], F32, s1) for i in range(2)]
            vt = c.sb("m_vt", [64, TT], F32, s1)
            rows = [c.sb(f"m_rows{i}", [2, TT], F32, s1) for i in range(2)]
            kb.op("pool", lambda e: e.memset(zq[:, 0:3], 0.0), writes=["zq"])
            kb.op("pool", lambda e: e.memset(zk[:, 0:3], 0.0), writes=["zk"])
            def m_load(tt):
                h_load(c, io["hT_all"], hTb[tt % 2], (tt % 4) * TT, TT, [f"m_hT{tt % 2}"], j=tt // 4)

            m_load(0)
            for tt in range(NQT):
                if tt + 1 < NQT:
                    m_load(tt + 1)
                hT = hTb[tt % 2]
                hk = f"m_hT{tt % 2}"
                tok = slice(tt * TT, (tt + 1) * TT)

                def proj(c0, c1, np_):
                    p, pk = c.ps()
                    for k in range(8):
                        mm(c, p[0:np_, :], wml[:, k, c0:c1], hT[:, k, :], k == 0, k == 7, ["wml", hk], [pk])
                    return p, pk
                pq, pqk = proj(0, 64, 64)
                pkk, pkkk = proj(64, 128, 64)
                pv, pvk = proj(128, 192, 64)
                pog, pogk = proj(192, 256, 64)
                pif, pifk = proj(256, 258, 2)
                rw = rows[tt % 2]
                rk = f"m_rows{tt % 2}"
                kb.op("act", lambda e, p=pq: e.activation(out=zq[:, 3:TT + 3], in_=p[0:64, :], func=AF.Copy), reads=[pqk], writes=["zq"])
                kb.op("act", lambda e, p=pkk: e.activation(out=zk[:, 3:TT + 3], in_=p[0:64, :], func=AF.Copy), reads=[pkkk], writes=["zk"])
                kb.op("act", lambda e, p=pv: e.activation(out=vt[:], in_=p[0:64, :], func=AF.Copy), reads=[pvk], writes=["m_vt"])
                kb.op("act", lambda e, p=pog, tok=tok: e.activation(out=og[:, tok], in_=p[0:64, :], func=AF.Sigmoid), reads=[pogk], writes=[("og", tt)])
                kb.op("act", lambda e, p=pif, rw=rw: e.activation(out=rw[:], in_=p[0:2, :], func=AF.Copy), reads=[pifk], writes=[rk])
                p2, p2k = c.ps()
                for ci in range(8):
                    kb.op("pe", lambda e, ci=ci, p2=p2: e.transpose(out=p2[0:64, ci * 64:(ci + 1) * 64], in_=vt[:, ci * 64:(ci + 1) * 64], identity=c.ident_f[0:64, 0:64]),
                          reads=["m_vt", "ident_f"], writes=[p2k])
                kb.op("dve", lambda e, p2=p2, tt=tt: e.tensor_copy(out=Vaug[:, tt * 8:(tt + 1) * 8, 0:64], in_=p2[0:64, :].rearrange("p (c d) -> p c d", d=64)),
                      reads=[p2k], writes=[("Vaug", tt)])
                for nm, z, vc, dst in (("q", zq, 0, qT), ("k", zk, 5, kT)):
                    zkey = "z" + nm
                    ca = cacc[0 if nm == "q" else 1]
                    ck = "cacc" + nm
                    kb.op("dve", lambda e, z=z, ca=ca, vc=vc: e.tensor_scalar(out=ca[:], in0=z[:, 0:TT], scalar1=vm[0:64, vc:vc + 1], scalar2=vm[0:64, vc + 4:vc + 5],
                                                                              op0=ALU.mult, op1=ALU.add), reads=[zkey, "vecsM"], writes=[ck])
                    for jj in range(1, 4):
                        kb.op("dve", lambda e, z=z, ca=ca, vc=vc, jj=jj: e.scalar_tensor_tensor(out=ca[:], in0=z[:, jj:jj + TT], scalar=vm[0:64, vc + jj:vc + jj + 1], in1=ca[:],
                                                                                                 op0=ALU.mult, op1=ALU.add), reads=[zkey, ck, "vecsM"], writes=[ck])
                    kb.op("act", lambda e, ca=ca, dst=dst, tok=tok: e.activation(out=dst[:, tok], in_=ca[:], func=AF.Silu), reads=[ck], writes=[("m_" + nm + "T", tt)])
                    kb.op("dve", lambda e, z=z: e.tensor_copy(out=z[:, 0:3], in_=z[:, TT:TT + 3]), reads=[zkey], writes=[zkey])
                kb.dma("sp", iC[tt * 8:(tt + 1) * 8, :], AP3(rw, 0, [[TT, 1], [64, 8], [1, 64]]), reads=[rk], writes=["iC"])
                kb.dma("sp", fC[tt * 8:(tt + 1) * 8, :], AP3(rw, TT, [[TT, 1], [64, 8], [1, 64]]), reads=[rk], writes=["fC"])
        c.barrier()
        Uall = c.sb("m_Uall", [64, 65, NCH], F32, s0)
        wgT = c.sb("m_wgT", [64, NCH], F32, s0)
        flT = c.sb("m_flT", [64, NCH], F32, s0)
        dB = c.sb("m_dB", [64, NCH], F32, s0)
        dB0 = c.sb("m_dB0", [64, NCH], F32, s0)
        with ExitStack() as s2:
            t1 = c.sb("g_t1", [128, 64], F32, s2)
            t2 = c.sb("g_t2", [128, 64], F32, s2)
            lf = c.sb("g_lf", [128, 64], F32, s2)
            bb = c.sb("g_b", [128, 64], F32, s2)
            aa = c.sb("g_a", [128, 64], F32, s2)
            AA = c.sb("g_A", [128, 64], F32, s2)
            MM = c.sb("g_M", [128, 64], F32, s2)
            wg = c.sb("g_wg", [128, 64], F32, s2)
            fl = c.sb("g_fl", [128, 64], F32, s2)
            on = c.sb("g_on", [128, 64], F32, s2)
            r1 = c.sb("g_r1", [1, 128], F32, s2)
            r2 = c.sb("g_r2", [1, 128], F32, s2)
            r3 = c.sb("g_r3", [1, 128], F32, s2)
            r4 = c.sb("g_r4", [1, 128], F32, s2)
            mcol = c.sb("g_mcol", [128, 1], F32, s2)
            nM63 = c.sb("g_nM63", [128, 1], F32, s2)
            dec = c.sb("g_dec", [128, 1], F32, s2)
            dgd = c.sb("g_dgd", [128, 128], F32, s2)
            Xb = [c.sb(f"g_X{i}", [128, 8, 64], F32, s2) for i in range(2)]
            kw32 = [c.sb(f"g_kw32{i}", [64, TT], F32, s2) for i in range(2)]
            kwTok = c.sb("g_kwTok", [64, NCH, 64], BF16, s2)
            log_sigmoid_tile(c, fC[:], lf[:], t1[:], t2[:], vm[:, 12:13], ("fC", "g_lf", "g_t1", "g_t2"))
            kb.op("dve", lambda e: e.tensor_scalar(out=iC[:], in0=iC[:], scalar1=vm[:, 11:12], scalar2=None, op0=ALU.add), reads=["iC", "vecsM"], writes=["iC"])
            kb.op("pool", lambda e: e.memset(on[:], 1.0), writes=["g_on"])
            kb.op("dve", lambda e: e.tensor_tensor_scan(out=bb[:], data0=on[:], data1=lf[:], initial=0.0, op0=ALU.mult, op1=ALU.add),
                  reads=["g_on", "g_lf"], writes=["g_b"])
            kb.op("dve", lambda e: e.tensor_tensor(out=aa[:], in0=iC[:], in1=bb[:], op=ALU.subtract), reads=["iC", "g_b"], writes=["g_a"])
            kb.op("dve", lambda e: e.tensor_tensor_scan(out=AA[:], data0=aa[:], data1=aa[:], initial=-1e30, op0=ALU.max, op1=ALU.max),
                  reads=["g_a"], writes=["g_A"])
            p, pk = c.ps()
            kb.op("pe", lambda e, p=p: e.transpose(out=p[0:1, 0:128], in_=AA[:, 63:64], identity=c.ident_f[:]), reads=["g_A", "ident_f"], writes=[pk])
            kb.op("pe", lambda e, p=p: e.transpose(out=p[0:1, 128:256], in_=bb[:, 63:64], identity=c.ident_f[:]), reads=["g_b", "ident_f"], writes=[pk])
            kb.op("dve", lambda e, p=p: e.tensor_copy(out=r1[:], in_=p[0:1, 0:128]), reads=[pk], writes=["g_r1"])
            kb.op("dve", lambda e, p=p: e.tensor_copy(out=r2[:], in_=p[0:1, 128:256]), reads=[pk], writes=["g_r2"])
            kb.op("dve", lambda e: e.tensor_tensor_scan(out=r3[:], data0=r1[:], data1=r2[:], initial=0.0, op0=ALU.max, op1=ALU.add),
                  reads=["g_r1", "g_r2"], writes=["g_r3"])
            kb.op("pool", lambda e: e.memset(r4[:, 0:1], 0.0), writes=["g_r4a"])
            kb.op("dve", lambda e: e.tensor_copy(out=r4[:, 1:128], in_=r3[:, 0:127]), reads=["g_r3"], writes=["g_r4b"])
            p, pk = c.ps()
            kb.op("pe", lambda e, p=p: e.transpose(out=p[:, 0:1], in_=r4[:], identity=c.ident_f[0:1, 0:1]), reads=["g_r4a", "g_r4b", "ident_f"], writes=[pk])
            kb.op("dve", lambda e, p=p: e.tensor_copy(out=mcol[:], in_=p[:, 0:1]), reads=[pk], writes=["g_mcol"])
            kb.op("dve", lambda e: e.tensor_scalar(out=MM[:], in0=AA[:], scalar1=mcol[:, 0:1], scalar2=None, op0=ALU.max), reads=["g_A", "g_mcol"], writes=["g_M"])
            kb.op("dve", lambda e: e.tensor_scalar(out=nM63[:], in0=MM[:, 63:64], scalar1=-1.0, scalar2=None, op0=ALU.mult), reads=["g_M"], writes=["g_nM63"])
            kb.op("act", lambda e: e.activation(out=wg[:], in_=aa[:], func=AF.Exp, bias=nM63[:, 0:1]), reads=["g_a", "g_nM63"], writes=["g_wg"])
            kb.op("act", lambda e: e.activation(out=dec[:], in_=mcol[:], func=AF.Exp, bias=nM63[:, 0:1]), reads=["g_mcol", "g_nM63"], writes=["g_dec"])
            kb.op("act", lambda e: e.activation(out=fl[:], in_=bb[:], func=AF.Exp, scale=-1.0, bias=nM63[:, 0:1]), reads=["g_b", "g_nM63"], writes=["g_fl"])
            p, pk = c.ps()
            kb.op("pe", lambda e, p=p: e.transpose(out=p[0:64, 0:128], in_=wg[:], identity=c.ident_f[:]), reads=["g_wg", "ident_f"], writes=[pk])
            kb.op("pe", lambda e, p=p: e.transpose(out=p[0:64, 128:256], in_=fl[:], identity=c.ident_f[:]), reads=["g_fl", "ident_f"], writes=[pk])
            kb.op("dve", lambda e, p=p: e.tensor_copy(out=wgT[:], in_=p[0:64, 0:128]), reads=[pk], writes=["m_wgT"])
            kb.op("dve", lambda e, p=p: e.tensor_copy(out=flT[:], in_=p[0:64, 128:256]), reads=[pk], writes=["m_flT"])
            kb.op("dve", lambda e: e.tensor_scalar(out=dgd[:], in0=c.ident_f[:], scalar1=dec[:, 0:1], scalar2=None, op0=ALU.mult), reads=["ident_f", "g_dec"], writes=["g_dgd"])
            p, pk = c.ps()
            mm(c, p[0:64, 0:128], c.ones_f[:, 0:64], dgd[:], True, True, ["ones_f", "g_dgd"], [pk])
            kb.op("dve", lambda e, p=p: e.tensor_copy(out=dB[:], in_=p[0:64, 0:128]), reads=[pk], writes=["m_dB"])
            kb.op("dve", lambda e, p=p: e.tensor_copy(out=dB0[:], in_=p[0:64, 0:128]), reads=[pk], writes=["m_dB0"])
            kb.op("pool", lambda e: e.memset(dB0[:, 0:1], 0.0), reads=["m_dB0"], writes=["m_dB0"])
            wgb = {}

            def a2_bcast(tt):
                X = Xb[tt % 2]
                Xk = f"g_X{tt % 2}"
                kb.op("dve", lambda e, X=X, tt=tt: e.tensor_tensor(out=X[:], in0=AP3(c.ident_f, 8 * tt, [[128, 128], [1, 8], [0, 64]]),
                                                                   in1=AP3(wg, 0, [[64, 128], [0, 8], [1, 64]]), op=ALU.mult),
                      reads=["ident_f", "g_wg"], writes=[Xk])
                p, pk = c.ps()
                mm(c, p[0:64, :], c.ones_f[:, 0:64], X[:].rearrange("p c s -> p (c s)"), True, True, ["ones_f", Xk], [pk])
                wgb[tt] = (p, pk)

            a2_bcast(0)
            for tt in range(NQT):
                tok = slice(tt * TT, (tt + 1) * TT)
                if tt + 1 < NQT:
                    a2_bcast(tt + 1)
                p, pk = wgb.pop(tt)
                k32 = kw32[tt % 2]
                k32k = f"g_kw32{tt % 2}"
                kb.op("dve", lambda e, p=p, k32=k32, tok=tok: e.scalar_tensor_tensor(out=k32[:], in0=kT[:, tok], scalar=0.125, in1=p[0:64, :], op0=ALU.mult, op1=ALU.mult),
                      reads=[pk, ("m_kT", tt)], writes=[k32k])
                kb.op("act", lambda e, k32=k32, tok=tok: e.activation(out=kT[:, tok], in_=k32[:], func=AF.Copy), reads=[k32k], writes=[("m_kT", tt)])
                p2, p2k = c.ps()
                for ci in range(8):
                    kb.op("pe", lambda e, ci=ci, p2=p2, k32=k32: e.transpose(out=p2[0:64, ci * 64:(ci + 1) * 64], in_=k32[:, ci * 64:(ci + 1) * 64], identity=c.ident_f[0:64, 0:64]),
                          reads=[k32k, "ident_f"], writes=[p2k])
                kb.op("dve", lambda e, p2=p2, tt=tt: e.tensor_copy(out=kwTok[:, tt * 8:(tt + 1) * 8, :], in_=p2[0:64, :].rearrange("p (c d) -> p c d", d=64)),
                      reads=[p2k], writes=[("kwTok", tt)])
            for g in range(NCH // GRP):
                p, pk = c.ps()
                for ci in range(GRP):
                    ch = g * GRP + ci
                    mm(c, p[0:64, ci * 65:(ci + 1) * 65], kwTok[:, ch, :], Vaug[:, ch, :], True, True, [("kwTok", ch // 8), ("Vaug", ch // 8), "Vaug1"], [pk])
                kb.op("dve", lambda e, p=p, g=g: e.tensor_copy(out=AP3(Uall, g * GRP, [[65 * NCH, 64], [1, GRP], [NCH, 65]]),
                                                               in_=p[0:64, 0:GRP * 65].rearrange("p (c d) -> p c d", d=65)),
                      reads=[pk], writes=["Uall"])
        c.barrier()
        Cn = Uall
        Eb = c.sb("m_E", [64, NCH, 65], BF16, s0)
        for dv in range(65):
            kb.op("dve", lambda e, dv=dv: e.tensor_tensor_scan(out=Cn[:, dv, :], data0=dB0[:], data1=Uall[:, dv, :], initial=0.0, op0=ALU.mult, op1=ALU.add),
                  reads=["m_dB0", "Uall"], writes=[("Cn", dv), "Uall"])
        kb.op("pool", lambda e: e.memset(Eb[:, 0:1, :], 0.0), writes=["E0"])
        kb.op("dve", lambda e: e.tensor_tensor(out=Eb[:, 1:NCH, :], in0=AP3(Cn, 0, [[65 * NCH, 64], [1, NCH - 1], [NCH, 65]]),
                                               in1=AP3(dB, 1, [[NCH, 64], [1, NCH - 1], [0, 65]]), op=ALU.mult),
              reads=[("Cn", dv) for dv in range(65)] + ["m_dB"], writes=["E"])
        with ExitStack() as s4:
            mask = c.sb("o_mask", [64, 64], F32, s4)
            sT = [c.sb(f"o_sT{i}", [64, GRP * 64], BF16, s4) for i in range(2)]
            den = c.sb("o_den", [64, GRP], F32, s4)
            hn = c.sb("o_hn", [64, GRP, 64], F32, s4)
            hsq = c.sb("o_hsq", [64, GRP, 64], F32, s4)
            ss = c.sb("o_ss", [64, GRP], F32, s4)
            cst = [c.sb(f"o_cst{i}", [64, GRP * 64], BF16, s4) for i in range(2)]
            kb.op("pool", lambda e: e.memset(mask[:], 1.0), writes=["o_mask"])
            kb.op("pool", lambda e: e.affine_select(out=mask[:], in_=mask[:], pattern=[[1, 64]], compare_op=ALU.is_ge, fill=0.0, base=0, channel_multiplier=-1),
                  reads=["o_mask"], writes=["o_mask"])
            pend = {}

            def a4_stage1(g):
                c0 = g * GRP
                p, pk = c.ps()
                for ci in range(GRP):
                    ch = c0 + ci
                    cs = slice(ch * 64, (ch + 1) * 64)
                    mm(c, p[0:64, ci * 64:(ci + 1) * 64], kT[:, cs], qT[:, cs], True, True, [("m_kT", ch // 8), ("m_qT", ch // 8)], [pk])
                st_ = sT[g % 2]
                stk = f"o_sT{g % 2}"
                kb.op("dve", lambda e, p=p, st_=st_: e.tensor_tensor(out=st_[:].rearrange("p (c t) -> p c t", t=64), in0=p[0:64, 0:GRP * 64].rearrange("p (c t) -> p c t", t=64),
                                                                     in1=AP3(mask, 0, [[64, 64], [0, GRP], [1, 64]]), op=ALU.mult),
                      reads=[pk, "o_mask"], writes=[stk])
                po, pok = c.ps()
                for ci in range(GRP):
                    ch = c0 + ci
                    cs = slice(ch * 64, (ch + 1) * 64)
                    mm(c, po[0:64, ci * 65:(ci + 1) * 65], st_[:, ci * 64:(ci + 1) * 64], Vaug[:, ch, :], True, False, [stk, ("Vaug", ch // 8), "Vaug1"], [pok])
                    mm(c, po[0:64, ci * 65:(ci + 1) * 65], qT[:, cs], Eb[:, ch, :], False, True, [("m_qT", ch // 8), "E", "E0"], [pok])
                pend[g] = (po, pok)

            def a4_stage2(g):
                c0 = g * GRP
                po, pok = pend.pop(g)
                po3 = po[0:64, 0:GRP * 65].rearrange("p (c d) -> p c d", d=65)
                den3 = den[:].rearrange("p (c o) -> p c o", o=1)
                kb.op("dve", lambda e, po3=po3, den3=den3: e.tensor_scalar(out=den3, in0=po3[:, :, 64:65], scalar1=-1.0, scalar2=None, op0=ALU.mult),
                      reads=[pok], writes=["o_den"])
                kb.op("dve", lambda e, po3=po3, den3=den3: e.tensor_tensor(out=den3, in0=po3[:, :, 64:65], in1=den3, op=ALU.max),
                      reads=[pok, "o_den"], writes=["o_den"])
                kb.op("dve", lambda e, c0=c0: e.tensor_tensor(out=den[:], in0=den[:], in1=flT[:, c0:c0 + GRP], op=ALU.max),
                      reads=["o_den", "m_flT"], writes=["o_den"])
                kb.op("dve", lambda e: e.reciprocal(out=den[:], in_=den[:]), reads=["o_den"], writes=["o_den"])
                kb.op("dve", lambda e, po3=po3: e.tensor_tensor(out=hn[:], in0=po3[:, :, 0:64], in1=AP3(den, 0, [[GRP, 64], [1, GRP], [0, 64]]), op=ALU.mult),
                      reads=[pok, "o_den"], writes=["o_hn"])
                kb.op("act", lambda e: e.activation(out=hsq[:], in_=hn[:], func=AF.Square), reads=["o_hn"], writes=["o_hsq"])
                kb.op("dve", lambda e: e.tensor_reduce(out=ss[:], in_=hsq[:], axis=AX.X, op=ALU.add), reads=["o_hsq"], writes=["o_ss"])
                kb.op("act", lambda e: e.activation(out=ss[:], in_=ss[:], func=AF.Sqrt, scale=1.0 / 64, bias=eps_t[0:64, 0:1]), reads=["o_ss", "m_eps"], writes=["o_ss"])
                kb.op("dve", lambda e: e.reciprocal(out=ss[:], in_=ss[:]), reads=["o_ss"], writes=["o_ss"])
                kb.op("dve", lambda e: e.tensor_tensor(out=hn[:], in0=hn[:], in1=AP3(ss, 0, [[GRP, 64], [1, GRP], [0, 64]]), op=ALU.mult),
                      reads=["o_hn", "o_ss"], writes=["o_hn"])
                pt, ptk = c.ps()
                for ci in range(GRP):
                    kb.op("pe", lambda e, ci=ci, pt=pt: e.transpose(out=pt[0:64, ci * 64:(ci + 1) * 64], in_=hn[:, ci, :], identity=c.ident_f[0:64, 0:64]),
                          reads=["o_hn", "ident_f"], writes=[ptk])
                cs_ = cst[g % 2]
                csk = f"o_cst{g % 2}"
                toks = slice(c0 * 64, (c0 + GRP) * 64)
                kb.op("dve", lambda e, pt=pt, cs_=cs_, toks=toks: e.scalar_tensor_tensor(out=cs_[:], in0=pt[0:64, 0:GRP * 64], scalar=vm[0:64, 10:11], in1=og[:, toks],
                                                                                       op0=ALU.mult, op1=ALU.mult),
                      reads=[ptk, "vecsM", ("og", (c0 * 64) // TT)], writes=[csk])
                kb.dma("sp", io["catm_out"][:, toks], cs_[:], reads=[csk])

            NG4 = NCH // GRP
            a4_stage1(0)
            for g in range(NG4):
                if g + 1 < NG4:
                    a4_stage1(g + 1)
                a4_stage2(g)
        c.barrier()


def phase_M_fox(c, io):
    nc, kb = c.nc, c.kb
    from contextlib import ExitStack
    NEG = -30000.0
    with ExitStack() as s0:
        vm = c.sb("vecsMf_sb", [128, 16], F32, s0)
        wfx = c.sb("wfx", [128, 8, 386], BF16, s0)
        fq = [c.sb(f"f_q{h}", [128, SEQ], BF16, s0) for h in range(2)]
        fkk = c.sb("f_kk", [128, SEQ], BF16, s0)
        kb.op("pool", lambda e: e.memset(fq[0][64:128, :], 0.0), writes=[("fqz", 0)])
        kb.op("pool", lambda e: e.memset(fq[1][0:64, :], 0.0), writes=[("fqz", 1)])
        fV = [c.sb(f"f_V{h}", [128, NKT, 65], BF16, s0) for h in range(2)]
        fC = [c.sb(f"f_C{h}", [64, 128], F32, s0) for h in range(2)]
        kb.dma("sp", vm[:], io["vecsM"], writes=["vecsM"])
        kb.dma("pool", wfx[:], io["w_fx"].rearrange("(k p) n -> p k n", p=128), writes=["wfx"])
        for h in range(2):
            kb.op("pool", lambda e, h=h: e.memset(fV[h][:, :, 64:65], 1.0), writes=[("fV1", h)])
        with ExitStack() as s1:
            hTb = [c.sb(f"f_hT{i}", [128, 8, TT], BF16, s1) for i in range(2)]
            vt = c.sb("f_vt", [128, TT], F32, s1)
            rows = [c.sb(f"f_rows{i}", [2, TT], F32, s1) for i in range(2)]
            def f_load(tt):
                h_load(c, io["hT_all"], hTb[tt % 2], (tt % 4) * TT, TT, [f"f_hT{tt % 2}"], j=tt // 4)

            f_load(0)
            for tt in range(NQT):
                if tt + 1 < NQT:
                    f_load(tt + 1)
                hT = hTb[tt % 2]
                hk = f"f_hT{tt % 2}"
                tok = slice(tt * TT, (tt + 1) * TT)
                p, pk = c.ps()
                for k in range(8):
                    mm(c, p[:, :], wfx[:, k, 0:128], hT[:, k, :], k == 0, k == 7, ["wfx", hk], [pk])
                kb.op("act", lambda e, p=p, tok=tok: e.activation(out=fq[0][0:64, tok], in_=p[0:64, :], func=AF.Copy, scale=0.125), reads=[pk], writes=[("fq", 0, tt)])
                kb.op("act", lambda e, p=p, tok=tok: e.activation(out=fq[1][64:128, tok], in_=p[64:128, :], func=AF.Copy, scale=0.125), reads=[pk], writes=[("fq", 1, tt)])
                p, pk = c.ps()
                for k in range(8):
                    mm(c, p[:, :], wfx[:, k, 128:256], hT[:, k, :], k == 0, k == 7, ["wfx", hk], [pk])
                kb.op("act", lambda e, p=p, tok=tok: e.activation(out=fkk[:, tok], in_=p[:, :], func=AF.Copy), reads=[pk], writes=[("fk", tt)])
                p, pk = c.ps()
                for k in range(8):
                    mm(c, p[:, :], wfx[:, k, 256:384], hT[:, k, :], k == 0, k == 7, ["wfx", hk], [pk])
                kb.op("act", lambda e, p=p: e.activation(out=vt[:], in_=p[:, :], func=AF.Copy), reads=[pk], writes=["f_vt"])
                p2, p2k = c.ps()
                for ci in range(4):
                    kb.op("pe", lambda e, ci=ci, p2=p2: e.transpose(out=p2[:, ci * 128:(ci + 1) * 128], in_=vt[:, ci * 128:(ci + 1) * 128], identity=c.ident_f[:]),
                          reads=["f_vt", "ident_f"], writes=[p2k])
                for h in range(2):
                    kb.op("dve", lambda e, p2=p2, tt=tt, h=h: e.tensor_copy(out=fV[h][:, tt * 4:(tt + 1) * 4, 0:64],
                                                                         in_=p2[:, :].rearrange("p (c d) -> p c d", d=128)[:, :, h * 64:(h + 1) * 64]),
                          reads=[p2k], writes=[("fV", h, tt)])
                p, pk = c.ps()
                for k in range(8):
                    mm(c, p[0:2, :], wfx[:, k, 384:386], hT[:, k, :], k == 0, k == 7, ["wfx", hk], [pk])
                rw = rows[tt % 2]
                rk = f"f_rows{tt % 2}"
                kb.op("act", lambda e, p=p, rw=rw: e.activation(out=rw[:], in_=p[0:2, :], func=AF.Copy), reads=[pk], writes=[rk])
                for h in range(2):
                    kb.dma("sp", fC[h][tt * 4:(tt + 1) * 4, :], AP3(rw, h * TT, [[TT, 1], [128, 4], [1, 128]]), reads=[rk], writes=[("fC", h)])
        c.barrier()
        ckT = [c.sb(f"f_ckT{h}", [128, NKT], F32, s0) for h in range(2)]
        cC = [c.sb(f"f_cC{h}", [64, 128], F32, s0) for h in range(2)]
        negm = c.sb("f_negm", [128, 4, TT], F32, s0)
        Ls = c.sb("f_Ls", [64, 64], F32, s0)
        with ExitStack() as s2:
            t1 = c.sb("f_t1", [64, 128], F32, s2)
            t2 = c.sb("f_t2", [64, 128], F32, s2)
            lf = c.sb("f_lf", [64, 128], F32, s2)
            on = c.sb("f_on", [64, 128], F32, s2)
            pre = c.sb("f_pre", [64, 1], F32, s2)
            kb.op("pool", lambda e: e.memset(on[:], 1.0), writes=["f_on"])
            kb.op("pool", lambda e: e.memset(Ls[:], 1.0), writes=["f_Ls"])
            kb.op("pool", lambda e: e.affine_select(out=Ls[:], in_=Ls[:], pattern=[[1, 64]], compare_op=ALU.is_ge, fill=0.0, base=-1, channel_multiplier=-1),
                  reads=["f_Ls"], writes=["f_Ls"])
            for r in range(4):
                kb.op("pool", lambda e, r=r: e.memset(negm[:, r, :], 0.0), writes=[("negm", r)])
                kb.op("pool", lambda e, r=r: e.affine_select(out=negm[:, r, :], in_=negm[:, r, :], pattern=[[1, TT]], compare_op=ALU.is_ge, fill=NEG,
                                                             base=-128 * r, channel_multiplier=-1), reads=[("negm", r)], writes=[("negm", r)])
            for h in range(2):
                log_sigmoid_tile(c, fC[h][:], lf[:], t1[:], t2[:], vm[0:64, 13 + h:14 + h], (("fC", h), "f_lf", "f_t1", "f_t2"))
                kb.op("dve", lambda e, h=h: e.tensor_tensor_scan(out=cC[h][:], data0=on[:], data1=lf[:], initial=0.0, op0=ALU.mult, op1=ALU.add),
                      reads=["f_on", "f_lf"], writes=[("cC", h)])
                p, pk = c.ps()
                mm(c, p[0:64, 0:1], Ls[:], cC[h][:, 127:128], True, True, ["f_Ls", ("cC", h)], [pk])
                kb.op("dve", lambda e, p=p: e.tensor_copy(out=pre[:], in_=p[0:64, 0:1]), reads=[pk], writes=["f_pre"])
                kb.op("dve", lambda e, h=h: e.tensor_scalar(out=cC[h][:], in0=cC[h][:], scalar1=pre[:, 0:1], scalar2=None, op0=ALU.add), reads=[("cC", h), "f_pre"], writes=[("cC", h)])
                p, pk = c.ps()
                kb.op("pe", lambda e, p=p, h=h: e.transpose(out=p[:, 0:64], in_=cC[h][:], identity=c.ident_f[0:64, 0:64]), reads=[("cC", h), "ident_f"], writes=[pk])
                kb.op("dve", lambda e, p=p, h=h: e.tensor_scalar(out=ckT[h][:], in0=p[:, 0:64], scalar1=-1.0, scalar2=None, op0=ALU.mult), reads=[pk], writes=[("ckT", h)])
        c.barrier()
        with ExitStack() as s3:
            X = c.sb("f_X", [64, 4, 128], F32, s3)
            cqB = c.sb("f_cqB", [128, TT], F32, s3)
            cqD = c.sb("f_cqD", [128, 4, TT], F32, s3)
            NB = 5
            tmpb = [c.sb(f"f_tmp{i}", [128, TT], F32, s3) for i in range(NB)]
            pTb = [c.sb(f"f_pT{i}", [128, TT], BF16, s3) for i in range(NB)]
            osb = c.sb("f_osb", [65, TT], F32, s3)
            rden = c.sb("f_rden", [64, TT], F32, s3)
            outb = [c.sb(f"f_out{i}", [64, TT], BF16, s3) for i in range(2)]
            it = 0
            for h in range(2):
                for qi in range(NQT):
                    qs = slice(qi * TT, (qi + 1) * TT)
                    kb.op("dve", lambda e, h=h, qi=qi: e.tensor_tensor(out=X[:], in0=AP3(c.ident_f, 4 * qi, [[128, 64], [1, 4], [0, 128]]),
                                                                       in1=AP3(cC[h], 0, [[128, 64], [0, 4], [1, 128]]), op=ALU.mult),
                          reads=["ident_f", ("cC", h)], writes=["f_X"])
                    p, pk = c.ps()
                    mm(c, p[:, :], c.ones_f[0:64, :], X[:].rearrange("p r s -> p (r s)"), True, True, ["ones_f", "f_X"], [pk])
                    kb.op("act", lambda e, p=p: e.activation(out=cqB[:], in_=p[:, :], func=AF.Copy), reads=[pk], writes=["f_cqB"])
                    for r in range(4):
                        kb.op("pool", lambda e, r=r: e.tensor_tensor(out=cqD[:, r, :], in0=cqB[:], in1=negm[:, r, :], op=ALU.add),
                              reads=["f_cqB", ("negm", r)], writes=[("f_cqD", r)])
                    c.rot = list(range(6))
                    po, pok = c.psb[6 + qi % 2], f"psb{6 + qi % 2}"
                    nk = 4 * (qi + 1)
                    LA = 4
                    sbank = {}

                    def emit_S(kt):
                        ps_, psk = c.ps()
                        mm(c, ps_[:, :], fkk[:, kt * 128:(kt + 1) * 128], fq[h][:, qs], True, True, [("fk", kt // 4), ("fq", h, qi), ("fqz", h)], [psk])
                        sbank[kt] = (ps_, psk)

                    for kt in range(min(LA, nk)):
                        emit_S(kt)
                    for kt in range(nk):
                        ps_, psk = sbank.pop(kt)
                        tb = tmpb[it % NB]; tbk = f"f_tmp{it % NB}"
                        pb = pTb[it % NB]; pbk = f"f_pT{it % NB}"
                        it += 1
                        r = kt - 4 * qi
                        if r >= 0:
                            kb.op("dve", lambda e, ps_=ps_, tb=tb, r=r: e.tensor_tensor(out=tb[:], in0=ps_[:, :], in1=cqD[:, r, :], op=ALU.add),
                                  reads=[psk, ("f_cqD", r)], writes=[tbk])
                        else:
                            kb.op("dve", lambda e, ps_=ps_, tb=tb: e.tensor_tensor(out=tb[:], in0=ps_[:, :], in1=cqB[:], op=ALU.add),
                                  reads=[psk, "f_cqB"], writes=[tbk])
                        kb.op("act", lambda e, tb=tb, pb=pb, h=h, kt=kt: e.activation(out=pb[:], in_=tb[:], func=AF.Exp, bias=ckT[h][:, kt:kt + 1]),
                              reads=[tbk, ("ckT", h)], writes=[pbk])
                        if kt + LA < nk:
                            emit_S(kt + LA)
                        mm(c, po[0:65, :], fV[h][:, kt, :], pb[:], kt == 0, kt == nk - 1, [("fV", h, kt // 4), ("fV1", h), pbk], [pok])
                    kb.op("act", lambda e, po=po: e.activation(out=osb[:], in_=po[0:65, :], func=AF.Copy), reads=[pok], writes=["f_osb"])
                    pd, pdk = c.ps()
                    mm(c, pd[0:64, :], c.ones_f[64:65, 0:64], osb[64:65, :], True, True, ["ones_f", "f_osb"], [pdk])
                    kb.op("dve", lambda e, pd=pd: e.reciprocal(out=rden[:], in_=pd[0:64, :]), reads=[pdk], writes=["f_rden"])
                    ob = outb[qi % 2]; obk = f"f_out{qi % 2}"
                    kb.op("dve", lambda e, ob=ob: e.tensor_tensor(out=ob[:], in0=osb[0:64, :], in1=rden[:], op=ALU.mult), reads=["f_osb", "f_rden"], writes=[obk])
                    cfo = io["catf_out"]
                    kb.dma("sp", (cfo[h][:, qs] if isinstance(cfo, list) else cfo[h * 64:(h + 1) * 64, qs]), ob[:], reads=[obk])
            c.rot = None
        c.barrier()


def build_M(which="both"):
    nc = bass.Bass("TRN2", target_bir_lowering=False)
    io = {}

    def din(name, shape, dt=F32):
        io[name] = nc.dram_tensor(name, shape, dt, kind="ExternalInput").ap()

    def dout(name, shape, dt=F32):
        io[name] = nc.dram_tensor(name, shape, dt, kind="ExternalOutput").ap()

    din("hT_all", [4, D, NT], BF16); din("w_ml", [D, 258]); din("w_fx", [D, 386]); din("vecsM", [128, 16])
    dout("catm_out", [64, SEQ], BF16); dout("catf_out", [128, SEQ], BF16)
    with _ES() as st:
        c = Ctx(nc, st)
        c.setup()
        if which in ("both", "mlstm"):
            phase_M_mlstm(c, io)
        if which in ("both", "fox"):
            phase_M_fox(c, io)
        c.barrier()
        c.kb.flush()
    return nc


def inputs_M(inp, l, g):
    w_in = np.asarray(inp["w_in"][l], np.float32)
    cols = np.concatenate([
        np.arange(g * 64, g * 64 + 64), 256 + np.arange(g * 64, g * 64 + 64),
        512 + np.arange(g * 64, g * 64 + 64), 768 + np.arange(g * 64, g * 64 + 64),
        [1024 + g, 1028 + g]])
    w_ml = np.ascontiguousarray(w_in[:, cols])
    fcols = []
    for base in (1544, 2056, 2568):
        for hh in (2 * g, 2 * g + 1):
            fcols.append(base + np.arange(hh * 64, hh * 64 + 64))
    fcols.append(np.array([3080 + 2 * g, 3080 + 2 * g + 1]))
    w_fx = np.ascontiguousarray(w_in[:, np.concatenate(fcols)])
    v = np.zeros((128, 16), np.float32)
    cw = np.asarray(inp["mlstm_conv_w"][l], np.float32)
    cb = np.asarray(inp["mlstm_conv_b"][l], np.float32)
    v[0:64, 0:4] = cw[:, g * 64:g * 64 + 64].T
    v[0:64, 4] = cb[g * 64:g * 64 + 64]
    v[0:64, 5:9] = cw[:, 256 + g * 64:256 + g * 64 + 64].T
    v[0:64, 9] = cb[256 + g * 64:256 + g * 64 + 64]
    v[0:64, 10] = np.asarray(inp["mlstm_norm_w"][l], np.float32)[g * 64:g * 64 + 64]
    v[:, 11] = inp["mlstm_b_i"][l][g]
    v[:, 12] = inp["mlstm_b_f"][l][g]
    v[:, 13] = inp["fox_b_f"][l][2 * g]
    v[:, 14] = inp["fox_b_f"][l][2 * g + 1]
    return {"w_ml": w_ml, "w_fx": w_fx, "vecsM": v}


def phase_P(c, io, xT):
    kb = c.kb
    from contextlib import ExitStack
    with ExitStack() as s0:
        vecs = c.sb("vecsP_sb", [128, 8], F32, s0)
        eps_t = c.sb("p_eps", [128, 1], F32, s0)
        sq = c.sb("p_sq", [128, 8, TT], BF16, s0)
        rstd = c.sb("p_rstd", [128, TT], F32, s0)
        hT = c.sb("p_hT", [128, 8, TT], BF16, s0)
        xt = [c.sb(f"p_xt{i}", [128, 4, D], F32, s0) for i in range(2)]
        tmp = {"sq": sq, "rstd": rstd, "eps": eps_t}
        kb.dma("sp", vecs[:], io["vecsP"], writes=["vecs"])
        kb.op("pool", lambda e: e.memset(eps_t[:], EPS), writes=["eps"])
        def p_load(t):
            kb.dma("sp", xt[t % 2][:], io["x_tok"][t * TT:(t + 1) * TT, :].rearrange("(s p) d -> p s d", p=128), writes=[f"p_xt{t % 2}"])

        p_load(0)
        for t in range(NTT):
            t0 = t * TT
            xb = xt[t % 2]
            xk = f"p_xt{t % 2}"
            if t + 1 < NTT:
                p_load(t + 1)
            for k in range(8):
                p, pk = c.ps()
                for s in range(4):
                    kb.op("pe", lambda e, k=k, s=s, p=p, xb=xb: e.transpose(out=p[:, s * 128:(s + 1) * 128], in_=xb[:, s, k * 128:(k + 1) * 128], identity=c.ident_f[:]),
                          reads=[xk, "ident_f"], writes=[pk])
                kb.op("act", lambda e, k=k, p=p, t0=t0: e.activation(out=xT[:, k, t0:t0 + TT], in_=p[:, :], func=AF.Copy), reads=[pk], writes=[("xT", k)])
            rmsnorm_tile(c, xT, "xT", t0, TT, vecs[:, 0:8], tmp, hT, "hT")
            h_store(c, io["h_next"], hT, t0, TT, [("hT", k) for k in range(8)], ["h_next_d"])
            if t == NTT - 1 and "tail_next" in io:
                kb.dma("sp", io["tail_next"].rearrange("(k p) n -> p k n", p=128), hT[:, :, TT - 32:TT],
                       reads=[("hT", k) for k in range(8)], writes=["tail_next_d"])
    c.barrier()


def build_P():
    nc = bass.Bass("TRN2", target_bir_lowering=False)
    io = {}
    io["x_tok"] = nc.dram_tensor("x_tok", [NT, D], F32, kind="ExternalInput").ap()
    io["vecsP"] = nc.dram_tensor("vecsP", [128, 8], F32, kind="ExternalInput").ap()
    io["xT_out"] = nc.dram_tensor("xT_out", [D, NT], F32, kind="ExternalOutput").ap()
    io["h_next"] = nc.dram_tensor("h_next", [D, NT], BF16, kind="ExternalOutput").ap()
    with _ES() as st:
        c = Ctx(nc, st)
        c.setup()
        xT = c.sb("xT", [128, 8, NT], F32)
        phase_P(c, io, xT)
        c.kb.dma("sp", io["xT_out"].rearrange("(k p) n -> p k n", p=128), xT[:], reads=[("xT", k) for k in range(8)])
        c.barrier()
        c.kb.flush()
    return nc


_CACHE = {}


def _get(name, fn):
    if name not in _CACHE:
        _CACHE[name] = fn()
    return _CACHE[name]


def kernel(**inp):
    inp = {k: np.asarray(v) for k, v in inp.items()}
    cores = list(range(8))
    B = 2
    x = inp["x"].astype(np.float32, copy=False)
    fm = lambda w: np.ascontiguousarray(np.asarray(w, np.float32).reshape(-1, 128).T)
    ncP = _get("P", build_P)
    maps = []
    for cid in cores:
        b, j = cid // 4, cid % 4
        maps.append({"x_tok": np.ascontiguousarray(x[b, j * NT:(j + 1) * NT]), "vecsP": fm(inp["norm_mix_w"][0])})
    res = run_bass_kernel_spmd(ncP, maps, core_ids=cores).results
    xT = [r["xT_out"] for r in res]
    hN = [r["h_next"] for r in res]
    out = None
    for l in range(2):
        last = (l == 1)
        E = 1 if l == 0 else 8
        ncM = _get("M", build_M)
        maps = []
        for cid in cores:
            b, g = cid // 4, cid % 4
            m = inputs_M(inp, l, g)
            m["hT_all"] = np.ascontiguousarray(np.stack([hN[b * 4 + jj] for jj in range(4)], axis=0))
            maps.append(m)
        resM = run_bass_kernel_spmd(ncM, maps, core_ids=cores).results
        ncT = _get(("T", E, last), lambda: build_T(E, last))
        maps = []
        for cid in cores:
            b, j = cid // 4, cid % 4
            tk = slice(j * NT, (j + 1) * NT)
            m = {"xT_in": xT[cid], "h_own": hN[cid]}
            m["h_halo"] = (np.ascontiguousarray(hN[cid - 1][:, NT - 32:NT]) if j > 0 else np.zeros((D, 32), NPBF))
            m["catm"] = np.ascontiguousarray(np.stack([resM[b * 4 + g]["catm_out"][:, tk] for g in range(4)], axis=0))
            m["catf"] = np.ascontiguousarray(np.stack([resM[b * 4 + g]["catf_out"][:, tk] for g in range(4)], axis=0))
            m["mem"] = np.ascontiguousarray(inp["mem"][b], dtype=np.float32)
            m["vecs"] = vecs_T(inp, l, last)
            m["w_c"] = np.ascontiguousarray(inp["w_in"][l][:, 1032:1544])
            m["w_out"] = inp["w_out"][l]; m["w_q"] = inp["xattn_w_q"][l]
            m["w_kv"] = inp["xattn_w_kv"][l]; m["w_o"] = inp["xattn_w_o"][l]
            if E == 1:
                m["w_gate"] = inp["ffn_w_gate"]; m["w_up"] = inp["ffn_w_up"]; m["w_down"] = inp["ffn_w_down"]
            else:
                m["w_gate"] = inp["moe_w_gate"][0]; m["w_up"] = inp["moe_w_up"][0]; m["w_down"] = inp["moe_w_down"][0]
                m["router_w"] = inp["router_w"][0]
            maps.append(m)
        resT = run_bass_kernel_spmd(ncT, maps, core_ids=cores).results
        if not last:
            xT = [r["xT_out"] for r in resT]
            hN = [r["h_next"] for r in resT]
        else:
            out = np.zeros((B, SEQ, D), np.float32)
            for cid in cores:
                b, j = cid // 4, cid % 4
                out[b, j * NT:(j + 1) * NT] = resT[cid]["out"]
    return out


RG = [[0, 1, 2, 3], [4, 5, 6, 7]]
_STOP = None


def build_fused(stop=None):
    nc = bass.Bass("TRN2", target_bir_lowering=False)
    io = {}
    if stop:
        io["dbg1"] = nc.dram_tensor("dbg1", [4 * D, NT], BF16, kind="ExternalOutput").ap()
        io["dbg2"] = nc.dram_tensor("dbg2", [512, SEQ], BF16, kind="ExternalOutput").ap()
        io["dbg3"] = nc.dram_tensor("dbg3", [D, NT], F32, kind="ExternalOutput").ap()

    def din(name, shape, dt=F32):
        io[name] = nc.dram_tensor(name, shape, dt, kind="ExternalInput").ap()
        return io[name]

    def dint(name, shape, dt=BF16):
        io[name] = nc.dram_tensor(name, shape, dt, kind="Internal").ap()
        return io[name]

    din("x_tok", [NT, D]); din("vecsP", [128, 8])
    if stop != "AG":
        din("sel", [128, 8]); din("mem", [256, D])
    for l in range(2 if stop != "AG" else 0):
        din(f"w_ml{l}", [D, 258]); din(f"w_fx{l}", [D, 386]); din(f"vecsM{l}", [128, 16]); din(f"vecs{l}", [128, NV_T])
        din(f"w_c{l}", [D, 512]); din(f"w_out{l}", [D, D]); din(f"w_q{l}", [D, 512]); din(f"w_kv{l}", [D, D]); din(f"w_o{l}", [512, D])
    if stop != "AG":
        din("w_gate0", [1, D, DFF]); din("w_up0", [1, D, DFF]); din("w_down0", [1, DFF, D])
        din("w_gate1", [8, D, DFF]); din("w_up1", [8, D, DFF]); din("w_down1", [8, DFF, D]); din("router_w", [D, 8])
    io["out"] = nc.dram_tensor("out", [NT, D], F32, kind="ExternalOutput").ap()
    for l in range(2):
        io[f"h_own{l}"] = [dint(f"h_own{l}_{a}", [256, NT]) for a in range(4)]
        io[f"hT_all{l}"] = [dint(f"hT_all{l}_{a}", [4 * 256, NT]) for a in range(4)]
        dint(f"tail{l}", [D, 32]); dint(f"tails{l}", [4 * D, 32])
        dint(f"catm{l}", [64, SEQ]); dint(f"catm_all{l}", [256, SEQ])
        io[f"catf{l}"] = [dint(f"catf{l}_{h}", [64, SEQ]) for h in range(2)]
        io[f"catf_all{l}"] = [dint(f"catf_all{l}_{h}", [256, SEQ]) for h in range(2)]
    with _ES() as st:
        c = Ctx(nc, st)
        c.setup()
        kb = c.kb
        xT = c.sb("xT", [128, 8, NT], F32)
        phase_P(c, {"x_tok": io["x_tok"], "vecsP": io["vecsP"], "h_next": io["h_own0"], "tail_next": io["tail0"]}, xT)
        for l in range(2):
            last = (l == 1)
            for a in range(4):
                kb.collective("AllGather", RG, io[f"h_own{l}"][a], io[f"hT_all{l}"][a], reads=["h_next_d"], writes=["hT_all_d"])
            kb.collective("AllGather", RG, io[f"tail{l}"], io[f"tails{l}"], reads=["tail_next_d"], writes=["tails_d"])
            c.barrier()
            if stop == "AG":
                for a in range(4):
                    for jj in range(4):
                        kb.dma("sp", io["dbg1"][jj * D + a * 256:jj * D + (a + 1) * 256, :], io[f"hT_all{l}"][a][jj * 256:(jj + 1) * 256, :], reads=["hT_all_d"])
                break
            ioM = {"hT_all": io[f"hT_all{l}"], "w_ml": io[f"w_ml{l}"], "w_fx": io[f"w_fx{l}"],
                   "vecsM": io[f"vecsM{l}"], "catm_out": io[f"catm{l}"], "catf_out": io[f"catf{l}"]}
            c.sfx = f"_{l}"
            phase_M_mlstm(c, ioM)
            kb.collective("AllGather", RG, io[f"catm{l}"], io[f"catm_all{l}"], writes=["catm_all_d"])
            phase_M_fox(c, ioM)
            for h in range(2):
                kb.collective("AllGather", RG, io[f"catf{l}"][h], io[f"catf_all{l}"][h], writes=["catf_all_d"])
            c.barrier()
            if stop == "M":
                for h in range(2):
                    for g in range(4):
                        kb.dma("sp", io["dbg2"][g * 128 + h * 64:g * 128 + (h + 1) * 64, :], io[f"catf_all{l}"][h][g * 64:(g + 1) * 64, :], reads=["catf_all_d"])
                break
            ioT = {"h_own": io[f"h_own{l}"], "tails": io[f"tails{l}"], "sel": io["sel"], "catm_all": io[f"catm_all{l}"], "catf_all": io[f"catf_all{l}"],
                   "mem": io["mem"], "vecs": io[f"vecs{l}"], "w_c": io[f"w_c{l}"], "w_out": io[f"w_out{l}"], "w_q": io[f"w_q{l}"],
                   "w_kv": io[f"w_kv{l}"], "w_o": io[f"w_o{l}"], "w_gate": io[f"w_gate{l}"], "w_up": io[f"w_up{l}"], "w_down": io[f"w_down{l}"]}
            if last:
                ioT["router_w"] = io["router_w"]; ioT["out"] = io["out"]
            else:
                ioT["h_next"] = io["h_own1"]; ioT["tail_next"] = io["tail1"]
            phase_T(c, ioT, 8 if last else 1, last, xT)
            if stop == "T":
                kb.dma("sp", io["dbg3"].rearrange("(k p) n -> p k n", p=128), xT[:], reads=[("xT", k) for k in range(8)])
                break
        c.barrier()
        kb.flush()
    return nc


def kernel_unfused(**inp):
    return _kernel_unfused(**inp)


_kernel_unfused = kernel


def kernel(**inp):
    inp = {k: np.asarray(v) for k, v in inp.items()}
    cores = list(range(8))
    x = inp["x"].astype(np.float32, copy=False)
    fm = lambda w: np.ascontiguousarray(np.asarray(w, np.float32).reshape(-1, 128).T)
    nc = _get("fused", lambda: build_fused(_STOP))
    shared = {"vecsP": fm(inp["norm_mix_w"][0]),
              "w_gate0": inp["ffn_w_gate"], "w_up0": inp["ffn_w_up"], "w_down0": inp["ffn_w_down"],
              "w_gate1": inp["moe_w_gate"][0], "w_up1": inp["moe_w_up"][0], "w_down1": inp["moe_w_down"][0],
              "router_w": inp["router_w"][0]}
    for l in range(2):
        shared[f"vecs{l}"] = vecs_T(inp, l, l == 1)
        shared[f"w_c{l}"] = np.ascontiguousarray(inp["w_in"][l][:, 1032:1544])
        shared[f"w_out{l}"] = inp["w_out"][l]; shared[f"w_q{l}"] = inp["xattn_w_q"][l]
        shared[f"w_kv{l}"] = inp["xattn_w_kv"][l]; shared[f"w_o{l}"] = inp["xattn_w_o"][l]
    perg = []
    for g in range(4):
        d = {}
        for l in range(2):
            m = inputs_M(inp, l, g)
            d[f"w_ml{l}"] = m["w_ml"]; d[f"w_fx{l}"] = m["w_fx"]; d[f"vecsM{l}"] = m["vecsM"]
        perg.append(d)
    maps = []
    for cid in cores:
        b, j = cid // 4, cid % 4
        m = dict(shared)
        m.update(perg[j])
        m["x_tok"] = np.ascontiguousarray(x[b, j * NT:(j + 1) * NT])
        m["mem"] = np.ascontiguousarray(inp["mem"][b], dtype=np.float32)
        sel = np.zeros((128, 8), np.float32)
        sel[:, j] = 1.0
        if j > 0:
            sel[:, 4 + j - 1] = 1.0
        m["sel"] = sel
        maps.append(m)
    if _STOP == "AG":
        maps = [{k: m[k] for k in ("x_tok", "vecsP")} for m in maps]
    res = run_bass_kernel_spmd(nc, maps, core_ids=cores).results
    if _STOP:
        return res
    out = np.zeros((2, SEQ, D), np.float32)
    for cid in cores:
        b, j = cid // 4, cid % 4
        out[b, j * NT:(j + 1) * NT] = res[cid]["out"]
    return out
```
